# Optimizing a Trainium2 kernel written in Bass

```python
import jax, jax.numpy as jnp
from jax import lax
import numpy as np

D_MODEL = 1024
BATCH = 8
SEQ = 8192
DEPTH = 2

GRID_W = 64
CTX_LEN = 256
N_EVEN = (DEPTH + 1) // 2
N_ODD = DEPTH // 2
NORM_EPS = 1e-6

GLA_HEADS = 4
GLA_DK = 64
GLA_DV = 128
GLA_GATE_RANK = 16
GLA_GATE_NORM = 16.0
GLA_CHUNK = 64
GLA_QK = GLA_HEADS * GLA_DK
GLA_V = GLA_HEADS * GLA_DV
GLA_COLS = 2 * GLA_QK + 2 * GLA_V + GLA_GATE_RANK
GLA_SPLITS = [GLA_QK, 2 * GLA_QK, 2 * GLA_QK + GLA_V, 2 * GLA_QK + 2 * GLA_V]

RW_HEADS = 8
RW_HEAD = 64
RW_W = RW_HEADS * RW_HEAD
RW_DECAY_RANK = 64
RW_ICLR_RANK = 64
RW_GATE_RANK = 160
RW_GN_EPS = 64e-5
RW_COLS = 3 * RW_W + RW_DECAY_RANK + RW_ICLR_RANK + RW_GATE_RANK
RW_SPLITS = [RW_W, 2 * RW_W, 3 * RW_W, 3 * RW_W + RW_DECAY_RANK, 3 * RW_W + RW_DECAY_RANK + RW_ICLR_RANK]

EVEN_IN = GLA_COLS + RW_COLS
EVEN_MIX = GLA_V + RW_W

MLA_HEADS = 8
MLA_NOPE = 128
MLA_ROPE = 64
MLA_V = 128
MLA_Q_RANK = 256
MLA_KV_RANK = 128
ODD_IN = MLA_Q_RANK + MLA_KV_RANK + MLA_ROPE
MLA_SPLITS = [MLA_Q_RANK, MLA_Q_RANK + MLA_KV_RANK]
ROPE_THETA = 10000.0
Q_BLOCK = 128

N_EXPERTS = 32
TOP_K = 4
D_EXPERT = 1024
SWIGLU_LIMIT = 7.0
SWIGLU_ALPHA = 1.702
MOE_BLOCK = 256

kernel_name = 'hybrid_gla_rwkv7_mla_moe_dit'


def rms_norm(x, g, eps=NORM_EPS):
    xf = x.astype(jnp.float32)
    y = xf * lax.rsqrt(jnp.mean(xf * xf, axis=-1, keepdims=True) + eps)
    return (y * g.astype(jnp.float32)).astype(x.dtype)


def modulate(x, g, shift, scale):
    return rms_norm(x, g) * (1.0 + scale) + shift


def shift_mix(p, mu_prev, mu_next):
    zero = jnp.zeros_like(p[:, :1])
    prev = jnp.concatenate([zero, p[:, :-1]], axis=1)
    nxt = jnp.concatenate([p[:, 1:], zero], axis=1)
    return p + mu_prev * (prev - p) + mu_next * (nxt - p)


def flip_if(a, d):
    return jnp.flip(a, 1) if d == 1 else a


def gla_chunked(q, k, v, g, s0):
    b, t, h, _ = q.shape
    dv = v.shape[-1]
    n = t // GLA_CHUNK
    def chunks(a):
        return a.reshape(b, n, GLA_CHUNK, h, a.shape[-1])
    q, k, v, g = chunks(q), chunks(k), chunks(v), chunks(g)
    G = jnp.cumsum(g, axis=2)
    G_last = G[:, :, -1]
    q_dec = q * jnp.exp(G)
    k_inv = k * jnp.exp(-G)
    k_end = k * jnp.exp(G_last[:, :, None] - G)
    mask = jnp.tril(jnp.ones((GLA_CHUNK, GLA_CHUNK), dtype=bool))
    att = jnp.where(mask, jnp.einsum('bnihd,bnjhd->bnhij', q_dec, k_inv), 0.0)
    o_intra = jnp.einsum('bnhij,bnjhv->bnihv', att, v)
    d_state = jnp.einsum('bnjhd,bnjhv->bnhdv', k_end, v)
    def step(s, inp):
        dec, ds = inp
        return s * dec[..., None] + ds, s
    s_fin, s_prev = lax.scan(step, s0, (jnp.moveaxis(jnp.exp(G_last), 1, 0), jnp.moveaxis(d_state, 1, 0)))
    o_inter = jnp.einsum('bnihd,nbhdv->bnihv', q_dec, s_prev)
    return (o_intra + o_inter).reshape(b, t, h, dv), s_fin


def gla_branch(p_lat, p_ctx, gk_up, gk_b, norm_g, need_ctx):
    def feats(p):
        bb, t, _ = p.shape
        q, k, v, og, gd = jnp.split(p, GLA_SPLITS, axis=-1)
        return (q.reshape(bb, t, GLA_HEADS, GLA_DK) * GLA_DK ** -0.5,
                k.reshape(bb, t, GLA_HEADS, GLA_DK),
                v.reshape(bb, t, GLA_HEADS, GLA_DV), og, gd)
    def dir_seq(f, d):
        q, k, v, _, gd = f
        bb, t, _ = gd.shape
        g = (jax.nn.log_sigmoid(gd @ gk_up[d] + gk_b[d]) / GLA_GATE_NORM).reshape(bb, t, GLA_HEADS, GLA_DK)
        return tuple(flip_if(a, d) for a in (q, k, v, g))
    lat, ctx = feats(p_lat), feats(p_ctx)
    b = p_lat.shape[0]
    o_l, o_c = [], []
    for d in range(2):
        s0 = jnp.zeros((b, GLA_HEADS, GLA_DK, GLA_DV), jnp.float32)
        oc, s_ctx = gla_chunked(*dir_seq(ctx, d), s0)
        ol, _ = gla_chunked(*dir_seq(lat, d), s_ctx)
        o_l.append(flip_if(ol, d))
        o_c.append(flip_if(oc, d))
    def finish(o, og):
        bb, t = o.shape[:2]
        return (rms_norm(o, norm_g) * jax.nn.silu(og).reshape(bb, t, GLA_HEADS, GLA_DV)).reshape(bb, t, GLA_V)
    out_lat = finish(o_l[0] + o_l[1], lat[3])
    out_ctx = finish(o_c[0] + o_c[1], ctx[3]) if need_ctx else None
    return out_lat, out_ctx


def rwkv7_scan(r, w, k, v, a, b, s0):
    def step(s, inp):
        r_t, w_t, k_t, v_t, a_t, b_t = inp
        sa = jnp.einsum('bhij,bhj->bhi', s, a_t)
        s = s * w_t[:, :, None, :] + sa[..., None] * b_t[:, :, None, :] + v_t[..., None] * k_t[:, :, None, :]
        return s, jnp.einsum('bhij,bhj->bhi', s, r_t)
    xs = tuple(jnp.swapaxes(t_, 0, 1) for t_ in (r, w, k, v, a, b))
    s_fin, y = lax.scan(step, s0, xs)
    return jnp.swapaxes(y, 0, 1), s_fin


def rwkv_branch(p_lat, p_ctx, mu, w0, w2, a0, a2, k_k, k_a, r_k, g2, ln_w, ln_b, need_ctx):
    def heads(a):
        bb, t, _ = a.shape
        return a.reshape(bb, t, RW_HEADS, RW_HEAD)
    def feats(p):
        return jnp.split(shift_mix(p, mu[0], mu[1]), RW_SPLITS, axis=-1)
    def dir_seq(f, d):
        r, k, v, wd, ad, _ = f
        w_log = -jax.nn.softplus(-(w0[d] + jnp.tanh(wd) @ w2[d])) - 0.5
        a = jax.nn.sigmoid(a0[d] + ad @ a2[d])
        kk = heads(k * k_k)
        kk = kk / jnp.maximum(jnp.sqrt(jnp.sum(kk * kk, axis=-1, keepdims=True)), 1e-12)
        kd = heads(k * (1.0 + (a - 1.0) * k_a))
        seq = (heads(r), heads(jnp.exp(-jnp.exp(w_log))), kd, heads(v), -kk, kk * heads(a))
        return tuple(flip_if(t_, d) for t_ in seq), kd
    lat, ctx = feats(p_lat), feats(p_ctx)
    b = p_lat.shape[0]
    y_l, y_c, kd_l, kd_c = [], [], [], []
    for d in range(2):
        seq_c, kdc = dir_seq(ctx, d)
        seq_l, kdl = dir_seq(lat, d)
        s0 = jnp.zeros((b, RW_HEADS, RW_HEAD, RW_HEAD), jnp.float32)
        yc, s_ctx = rwkv7_scan(*seq_c, s0)
        yl, _ = rwkv7_scan(*seq_l, s_ctx)
        y_l.append(flip_if(yl, d)); y_c.append(flip_if(yc, d))
        kd_l.append(kdl); kd_c.append(kdc)
    def finish(f, ys, kds):
        r, _, v, _, _, gd = f
        bb, t, _ = r.shape
        y = ys[0] + ys[1]
        mean = jnp.mean(y, axis=-1, keepdims=True)
        var = jnp.mean(jnp.square(y - mean), axis=-1, keepdims=True)
        y = ((y - mean) * lax.rsqrt(var + RW_GN_EPS)).reshape(bb, t, RW_W) * ln_w + ln_b
        rh, vh = heads(r), heads(v)
        bonus = (jnp.sum(rh * kds[0] * r_k, axis=-1, keepdims=True)
                 + jnp.sum(rh * kds[1] * r_k, axis=-1, keepdims=True)) * vh
        g = jax.nn.sigmoid(gd) @ g2
        return (y + bonus.reshape(bb, t, RW_W)) * g
    out_lat = finish(lat, y_l, kd_l)
    out_ctx = finish(ctx, y_c, kd_c) if need_ctx else None
    return out_lat, out_ctx


def even_mixer(h_lat, h_ctx, w_in, w_out, gk_up, gk_b, gla_g, mu, w0, w2, a0, a2, k_k, k_a, r_k, g2, ln_w, ln_b, need_ctx):
    p_lat = (h_lat @ w_in).astype(jnp.float32)
    p_ctx = (h_ctx @ w_in).astype(jnp.float32)
    gl, gc = gla_branch(p_lat[..., :GLA_COLS], p_ctx[..., :GLA_COLS], gk_up, gk_b, gla_g, need_ctx)
    rl, rc = rwkv_branch(p_lat[..., GLA_COLS:], p_ctx[..., GLA_COLS:], mu, w0, w2, a0, a2,
                         k_k, k_a, r_k, g2, ln_w, ln_b, need_ctx)
    o_lat = jnp.concatenate([gl, rl], axis=-1).astype(h_lat.dtype) @ w_out
    o_ctx = jnp.concatenate([gc, rc], axis=-1).astype(h_ctx.dtype) @ w_out if need_ctx else None
    return o_lat, o_ctx


def axial_angles(n_tok):
    rows = n_tok // GRID_W
    row = jnp.broadcast_to(jnp.arange(rows, dtype=jnp.float32)[:, None], (rows, GRID_W)).reshape(-1)
    col = jnp.broadcast_to(jnp.arange(GRID_W, dtype=jnp.float32)[None, :], (rows, GRID_W)).reshape(-1)
    quarter = MLA_ROPE // 4
    inv_freq = ROPE_THETA ** (-jnp.arange(quarter, dtype=jnp.float32) / quarter)
    return row[:, None] * inv_freq, col[:, None] * inv_freq


def apply_axial_rope(x, ang_row, ang_col):
    r1, r2, c1, c2 = jnp.split(x.astype(jnp.float32), 4, axis=-1)
    cr, sr = jnp.cos(ang_row)[:, None, :], jnp.sin(ang_row)[:, None, :]
    cc, sc = jnp.cos(ang_col)[:, None, :], jnp.sin(ang_col)[:, None, :]
    out = jnp.concatenate([r1 * cr - r2 * sr, r2 * cr + r1 * sr, c1 * cc - c2 * sc, c2 * cc + c1 * sc], axis=-1)
    return out.astype(x.dtype)


def odd_mixer(h_lat, h_ctx, w_in, q_norm, wq_up, kv_norm, wkv_up, w_out, need_ctx):
    def project(h):
        bb, t, _ = h.shape
        cq, ckv, k_rope = jnp.split(h @ w_in, MLA_SPLITS, axis=-1)
        q = (rms_norm(cq, q_norm) @ wq_up).reshape(bb, t, MLA_HEADS, MLA_NOPE + MLA_ROPE)
        kv = (rms_norm(ckv, kv_norm) @ wkv_up).reshape(bb, t, MLA_HEADS, MLA_NOPE + MLA_V)
        return q[..., :MLA_NOPE], q[..., MLA_NOPE:], kv[..., :MLA_NOPE], kv[..., MLA_NOPE:], k_rope[:, :, None, :]
    qn_l, qr_l, kn_l, v_l, kr_l = project(h_lat)
    qn_c, qr_c, kn_c, v_c, kr_c = project(h_ctx)
    b, n_lat, _ = h_lat.shape
    ang_r, ang_c = axial_angles(n_lat)
    qr_l = apply_axial_rope(qr_l, ang_r, ang_c)
    kr_l = apply_axial_rope(kr_l, ang_r, ang_c)
    kn = jnp.concatenate([kn_c, kn_l], axis=1)
    kr = jnp.concatenate([kr_c, kr_l], axis=1)[:, :, 0]
    v = jnp.concatenate([v_c, v_l], axis=1)
    scale = (MLA_NOPE + MLA_ROPE) ** -0.5
    def attend(qn, qr, kn_, kr_, v_):
        s = (jnp.einsum('bqhd,bkhd->bhqk', qn, kn_) + jnp.einsum('bqhr,bkr->bhqk', qr, kr_)).astype(jnp.float32) * scale
        p = jax.nn.softmax(s, axis=-1).astype(v_.dtype)
        return jnp.einsum('bhqk,bkhd->bqhd', p, v_)
    nb = n_lat // Q_BLOCK
    def blocks(a):
        return jnp.moveaxis(a.reshape(b, nb, Q_BLOCK, *a.shape[2:]), 1, 0)
    o = lax.map(lambda qb: attend(qb[0], qb[1], kn, kr, v), (blocks(qn_l), blocks(qr_l)))
    o_lat = jnp.moveaxis(o, 0, 1).reshape(b, n_lat, MLA_HEADS * MLA_V) @ w_out
    o_ctx = None
    if need_ctx:
        n_ctx = h_ctx.shape[1]
        o_ctx = attend(qn_c, qr_c, kn_c, kr_c[:, :, 0], v_c).reshape(b, n_ctx, MLA_HEADS * MLA_V) @ w_out
    return o_lat, o_ctx


def moe_ffn(h, w_r, b_r, w_gu, b_gu, w_dn, b_dn):
    n, d = h.shape
    logits = (h @ w_r + b_r).astype(jnp.float32)
    top_logit, top_e = lax.top_k(logits, TOP_K)
    gate = jax.nn.softmax(top_logit, axis=-1)
    n_assign = n * TOP_K
    flat_e = top_e.reshape(-1)
    flat_tok = jnp.arange(n_assign, dtype=jnp.int32) // TOP_K
    flat_g = gate.reshape(-1)
    order = jnp.argsort(flat_e)
    se, stok, sg = flat_e[order], flat_tok[order], flat_g[order]
    counts = jnp.bincount(flat_e, length=N_EXPERTS)
    padded = (counts + MOE_BLOCK - 1) // MOE_BLOCK * MOE_BLOCK
    pad_end = jnp.cumsum(padded)
    pad_start = pad_end - padded
    start = jnp.cumsum(counts) - counts
    dest = pad_start[se] + jnp.arange(n_assign, dtype=jnp.int32) - start[se]
    n_blocks = (n_assign + MOE_BLOCK - 1) // MOE_BLOCK + N_EXPERTS
    tok_buf = jnp.zeros((n_blocks * MOE_BLOCK,), jnp.int32).at[dest].set(stok)
    g_buf = jnp.zeros((n_blocks * MOE_BLOCK,), jnp.float32).at[dest].set(sg)
    blk_e = jnp.minimum(jnp.searchsorted(pad_end, jnp.arange(n_blocks, dtype=jnp.int32) * MOE_BLOCK, side='right'),
                        N_EXPERTS - 1)
    def step(acc, blk):
        tok, g, e = blk
        gu = (h[tok] @ w_gu[e] + b_gu[e]).astype(jnp.float32)
        x_glu = jnp.minimum(gu[:, :D_EXPERT], SWIGLU_LIMIT)
        x_lin = jnp.clip(gu[:, D_EXPERT:], -SWIGLU_LIMIT, SWIGLU_LIMIT)
        act = x_glu * jax.nn.sigmoid(SWIGLU_ALPHA * x_glu) * (x_lin + 1.0)
        y = (act.astype(h.dtype) @ w_dn[e] + b_dn[e]).astype(jnp.float32)
        return acc.at[tok].add(y * g[:, None]), None
    acc, _ = lax.scan(step, jnp.zeros((n, d), jnp.float32),
                      (tok_buf.reshape(n_blocks, MOE_BLOCK), g_buf.reshape(n_blocks, MOE_BLOCK), blk_e))
    return acc.astype(h.dtype)


def setup_inputs(seed: int = 0) -> dict:
    key = jax.random.key(seed)
    ks = iter(jax.random.split(key, 64))
    def nrm(shape, scale):
        return jax.random.normal(next(ks), shape, jnp.float32) * scale
    def gain(shape):
        return 1.0 + nrm(shape, 0.02)
    D = D_MODEL
    return {
        'x': nrm((BATCH, SEQ, D), 1.0),
        'c': nrm((BATCH, D), 1.0),
        'ctx': nrm((BATCH, CTX_LEN, D), 1.0),
        'c_ctx': nrm((D,), 1.0),
        'ada_w': nrm((DEPTH, D, 6 * D), 0.5 * D ** -0.5),
        'ada_b': nrm((DEPTH, 6 * D), 0.02),
        'norm1_g': gain((DEPTH, D)),
        'norm2_g': gain((DEPTH, D)),
        'ev_w_in': nrm((N_EVEN, D, EVEN_IN), D ** -0.5),
        'ev_w_out': nrm((N_EVEN, EVEN_MIX, D), EVEN_MIX ** -0.5),
        'gla_gk_up': nrm((N_EVEN, 2, GLA_GATE_RANK, GLA_QK), GLA_GATE_RANK ** -0.5),
        'gla_gk_b': nrm((N_EVEN, 2, GLA_QK), 0.1),
        'gla_norm_g': gain((N_EVEN, GLA_DV)),
        'rw_mu': jax.random.uniform(next(ks), (N_EVEN, 2, RW_COLS), jnp.float32) * 0.6,
        'rw_w0': nrm((N_EVEN, 2, RW_W), 0.5) - 2.0,
        'rw_w2': nrm((N_EVEN, 2, RW_DECAY_RANK, RW_W), 0.1),
        'rw_a0': nrm((N_EVEN, 2, RW_W), 0.5),
        'rw_a2': nrm((N_EVEN, 2, RW_ICLR_RANK, RW_W), 0.1),
        'rw_k_k': 0.85 + nrm((N_EVEN, RW_W), 0.02),
        'rw_k_a': gain((N_EVEN, RW_W)),
        'rw_r_k': nrm((N_EVEN, RW_HEADS, RW_HEAD), 0.1),
        'rw_g2': nrm((N_EVEN, RW_GATE_RANK, RW_W), RW_GATE_RANK ** -0.5),
        'rw_ln_w': gain((N_EVEN, RW_W)),
        'rw_ln_b': nrm((N_EVEN, RW_W), 0.02),
        'od_w_in': nrm((N_ODD, D, ODD_IN), D ** -0.5),
        'mla_q_norm': gain((N_ODD, MLA_Q_RANK)),
        'mla_wq_up': nrm((N_ODD, MLA_Q_RANK, MLA_HEADS * (MLA_NOPE + MLA_ROPE)), MLA_Q_RANK ** -0.5),
        'mla_kv_norm': gain((N_ODD, MLA_KV_RANK)),
        'mla_wkv_up': nrm((N_ODD, MLA_KV_RANK, MLA_HEADS * (MLA_NOPE + MLA_V)), MLA_KV_RANK ** -0.5),
        'od_w_out': nrm((N_ODD, MLA_HEADS * MLA_V, D), (MLA_HEADS * MLA_V) ** -0.5),
        'router_w': nrm((DEPTH, D, N_EXPERTS), D ** -0.5),
        'router_b': nrm((DEPTH, N_EXPERTS), 0.01),
        'moe_gu_w': nrm((DEPTH, N_EXPERTS, D, 2 * D_EXPERT), D ** -0.5),
        'moe_gu_b': nrm((DEPTH, N_EXPERTS, 2 * D_EXPERT), 0.02),
        'moe_down_w': nrm((DEPTH, N_EXPERTS, D_EXPERT, D), D_EXPERT ** -0.5),
        'moe_down_b': nrm((DEPTH, N_EXPERTS, D), 0.02),
        'final_norm_g': gain((D,)),
    }


def reference(x, c, ctx, c_ctx, ada_w, ada_b, norm1_g, norm2_g, ev_w_in, ev_w_out, gla_gk_up, gla_gk_b,
              gla_norm_g, rw_mu, rw_w0, rw_w2, rw_a0, rw_a2, rw_k_k, rw_k_a, rw_r_k, rw_g2, rw_ln_w, rw_ln_b,
              od_w_in, mla_q_norm, mla_wq_up, mla_kv_norm, mla_wkv_up, od_w_out, router_w, router_b,
              moe_gu_w, moe_gu_b, moe_down_w, moe_down_b, final_norm_g):
    b, n_lat, d = x.shape
    n_ctx = ctx.shape[1]
    x_lat, x_ctx = x, ctx
    for layer in range(DEPTH):
        need_ctx = layer < DEPTH - 1
        i = layer // 2
        mod_lat = jax.nn.silu(c) @ ada_w[layer] + ada_b[layer]
        mod_ctx = jax.nn.silu(c_ctx) @ ada_w[layer] + ada_b[layer]
        sh1, sc1, gt1, sh2, sc2, gt2 = jnp.split(mod_lat[:, None, :], 6, axis=-1)
        sh1c, sc1c, gt1c, sh2c, sc2c, gt2c = jnp.split(mod_ctx, 6, axis=-1)
        h_lat = modulate(x_lat, norm1_g[layer], sh1, sc1)
        h_ctx = modulate(x_ctx, norm1_g[layer], sh1c, sc1c)
        if layer % 2 == 0:
            o_lat, o_ctx = even_mixer(h_lat, h_ctx, ev_w_in[i], ev_w_out[i], gla_gk_up[i], gla_gk_b[i], gla_norm_g[i],
                                      rw_mu[i], rw_w0[i], rw_w2[i], rw_a0[i], rw_a2[i], rw_k_k[i], rw_k_a[i],
                                      rw_r_k[i], rw_g2[i], rw_ln_w[i], rw_ln_b[i], need_ctx)
        else:
            o_lat, o_ctx = odd_mixer(h_lat, h_ctx, od_w_in[i], mla_q_norm[i], mla_wq_up[i], mla_kv_norm[i],
                                     mla_wkv_up[i], od_w_out[i], need_ctx)
        x_lat = x_lat + gt1 * o_lat
        h_lat = modulate(x_lat, norm2_g[layer], sh2, sc2).reshape(b * n_lat, d)
        if need_ctx:
            x_ctx = x_ctx + gt1c * o_ctx
            h_ctx = modulate(x_ctx, norm2_g[layer], sh2c, sc2c).reshape(b * n_ctx, d)
            tokens = jnp.concatenate([h_lat, h_ctx], axis=0)
        else:
            tokens = h_lat
        y = moe_ffn(tokens, router_w[layer], router_b[layer], moe_gu_w[layer], moe_gu_b[layer],
                    moe_down_w[layer], moe_down_b[layer])
        x_lat = x_lat + gt2 * y[:b * n_lat].reshape(b, n_lat, d)
        if need_ctx:
            x_ctx = x_ctx + gt2c * y[b * n_lat:].reshape(b, n_ctx, d)
    return rms_norm(x_lat, final_norm_g)
```

```python
import contextlib
import numpy as np
import concourse.bass as bass
import concourse.mybir as mybir

F32 = mybir.dt.float32
BF16 = mybir.dt.bfloat16
I32 = mybir.dt.int32
U32 = mybir.dt.uint32
ALU = mybir.AluOpType
AF = mybir.ActivationFunctionType
AX = mybir.AxisListType

ENGS = ("pe", "act", "dve", "pool", "sp")
RESET_THRESH = 20000


class Prog:
    def __init__(self, nc, n_dma_slots=10):
        self.nc = nc
        self.es = contextlib.ExitStack()
        self.cur = self.es
        self.streams = {e: [] for e in ENGS}
        self.count = {e: 0 for e in ENGS}
        self.seen = {e: {} for e in ENGS}
        self.bufs = {}
        self.sems = {}
        for e in ("pe", "act", "dve", "pool"):
            self.sems[e] = self.es.enter_context(nc.semaphore("s_" + e))
        self.dma_slots = {}
        self.dma_rr = {}
        for q in ("sp", "act", "pool"):
            self.dma_slots[q] = []
            for i in range(n_dma_slots):
                s = self.es.enter_context(nc.semaphore(f"d_{q}{i}"))
                self.sems[f"d_{q}{i}"] = s
                self.dma_slots[q].append([f"d_{q}{i}", 0])
            self.dma_rr[q] = 0
        self.n_instr = 0
        self.bsem = self.es.enter_context(nc.semaphore("s_bar"))
        self.gsem = self.es.enter_context(nc.semaphore("s_go"))
        self.nreset = 0
        self.reset_thresh = RESET_THRESH

    def _maybe_reset(self):
        if max(self.count.values()) >= self.reset_thresh or \
                max(v for q in self.dma_slots for _, v in self.dma_slots[q]) >= 2 * self.reset_thresh:
            self.sync_reset()

    def sync_reset(self):
        self.barrier()
        self.nreset += 1
        k = self.nreset
        bs, gs = self.bsem, self.gsem
        for e in ENGS:
            self.streams[e].append(lambda eng, bs=bs: eng.sem_inc(bs, 1))
        self.streams["sp"].append(lambda eng, bs=bs, k=k: eng.wait_ge(bs, 5 * k))
        for name, sem in self.sems.items():
            self.streams["sp"].append(lambda eng, sem=sem: eng.sem_clear(sem))
        self.streams["sp"].append(lambda eng, gs=gs: eng.sem_inc(gs, 1))
        for e in ENGS:
            if e != "sp":
                self.streams[e].append(lambda eng, gs=gs, k=k: eng.wait_ge(gs, k))
        self.count = {e: 0 for e in ENGS}
        self.seen = {e: {} for e in ENGS}
        self.bufs = {}
        for q in self.dma_slots:
            for slot in self.dma_slots[q]:
                slot[1] = 0

    def sb(self, name, shape, dt=F32):
        self._uid = getattr(self, "_uid", 0) + 1
        name = f"{name}_u{self._uid}"
        return self.cur.enter_context(self.nc.sbuf_tensor(name, list(shape), dt))

    def ps(self, name, shape, dt=F32):
        self._uid = getattr(self, "_uid", 0) + 1
        name = f"{name}_u{self._uid}"
        return self.cur.enter_context(self.nc.psum_tensor(name, list(shape), dt))

    def _deps(self, r, w):
        deps = {}

        def add(tok):
            if tok is None:
                return
            s, v = tok
            if deps.get(s, 0) < v:
                deps[s] = v

        for k in r:
            b = self.bufs.setdefault(k, {"w": None, "r": {}})
            add(b["w"])
        for k in w:
            b = self.bufs.setdefault(k, {"w": None, "r": {}})
            add(b["w"])
            for s, v in b["r"].items():
                add((s, v))
        return deps

    def _commit(self, r, w, tok):
        for k in r:
            b = self.bufs[k]
            if b["r"].get(tok[0], 0) < tok[1]:
                b["r"][tok[0]] = tok[1]
        for k in w:
            b = self.bufs[k]
            b["w"] = tok
            b["r"] = {}

    def _emit_waits(self, eng, deps):
        seen = self.seen[eng]
        for s, v in deps.items():
            if eng == "pe" and s == "pe":
                continue
            if seen.get(s, 0) >= v:
                continue
            seen[s] = v
            sem = self.sems[s]
            self.streams[eng].append(lambda e, sem=sem, v=v: e.wait_ge(sem, v))

    def op(self, eng, fn, r=(), w=()):
        self._maybe_reset()
        deps = self._deps(r, w)
        self._emit_waits(eng, deps)
        self.count[eng] += 1
        n = self.count[eng]
        sem = self.sems[eng]
        self.streams[eng].append(lambda e, fn=fn, sem=sem: fn(e).then_inc(sem, 1))
        self._commit(r, w, (eng, n))
        self.n_instr += 1

    def dma(self, q, fn, r=(), w=()):
        self._maybe_reset()
        deps = self._deps(r, w)
        i = self.dma_rr[q]
        self.dma_rr[q] = (i + 1) % len(self.dma_slots[q])
        slot = self.dma_slots[q][i]
        if slot[1] > 0:
            if deps.get(slot[0], 0) < slot[1]:
                deps[slot[0]] = slot[1]
        self._emit_waits(q, deps)
        slot[1] += 16
        sem = self.sems[slot[0]]
        self.streams[q].append(lambda e, fn=fn, sem=sem: fn(e).then_inc(sem, 16))
        self._commit(r, w, (slot[0], slot[1]))
        self.n_instr += 1

    def wait_all(self, eng, keys):
        deps = self._deps(keys, ())
        self._emit_waits(eng, deps)

    def barrier(self):
        full = {}
        for e in ("pe", "act", "dve", "pool"):
            if self.count[e] > 0:
                full[e] = self.count[e]
        for q in self.dma_slots:
            for name, v in self.dma_slots[q]:
                if v > 0:
                    full[name] = v
        for e in ENGS:
            d = dict(full)
            d.pop(e, None)
            if e == "pe":
                pass
            self._emit_waits(e, d)

    @contextlib.contextmanager
    def phase(self):
        old = self.cur
        with contextlib.ExitStack() as st:
            self.cur = st
            yield
            self.barrier()
            self.flush()
        self.cur = old

    def flush(self):
        self._emit_block()
        self.streams = {e: [] for e in ENGS}

    def finish(self):
        self.barrier()
        self.flush()
        self.es.close()

    def _emit_block(self):
        nc = self.nc
        with nc.Block() as block:
            @block.tensor
            def _(e):
                for f in self.streams["pe"]:
                    f(e)

            @block.scalar
            def _(e):
                for f in self.streams["act"]:
                    f(e)

            @block.vector
            def _(e):
                for f in self.streams["dve"]:
                    f(e)

            @block.gpsimd
            def _(e):
                for f in self.streams["pool"]:
                    f(e)

            @block.sync
            def _(e):
                for f in self.streams["sp"]:
                    f(e)

import contextlib
import numpy as np

D = 1024
KC = 8
EPS = 1e-6
GLA_COLS = 1552
RW_COLS = 1824
EVEN_IN = 3376


def dram(nc, name, shape, dt=F32, kind="Internal"):
    return nc.dram_tensor(name, list(shape), dt, kind=kind).ap()


def make_consts(p):
    nc = p.nc
    C = {}
    iota_f = p.sb("iota_f", [128, 128], F32)
    iota_p = p.sb("iota_p", [128, 1], F32)
    ii = p.sb("iota_i", [128, 128], I32)
    ip = p.sb("iota_pi", [128, 1], I32)
    p.op("pool", lambda e: e.iota(ii[:], pattern=[[1, 128]], base=0, channel_multiplier=0), w=["iota_i"])
    p.op("pool", lambda e: e.iota(ip[:], pattern=[[1, 1]], base=0, channel_multiplier=1), w=["iota_pi"])
    p.op("dve", lambda e: e.tensor_copy(out=iota_f[:], in_=ii[:]), r=["iota_i"], w=["iota_f"])
    p.op("dve", lambda e: e.tensor_copy(out=iota_p[:], in_=ip[:]), r=["iota_pi"], w=["iota_p"])
    ident = p.sb("ident", [128, 128], F32)
    p.op("dve", lambda e: e.tensor_scalar(out=ident[:], in0=iota_f[:], scalar1=iota_p[:, 0:1], scalar2=None,
                                          op0=ALU.is_equal), r=["iota_f", "iota_p"], w=["ident"])
    identb = p.sb("identb", [128, 128], BF16)
    p.op("dve", lambda e: e.tensor_copy(out=identb[:], in_=ident[:]), r=["ident"], w=["identb"])
    C.update(iota_f=iota_f, iota_p=iota_p, ident=ident, identb=identb)
    return C


def phase_ada(p, C, io, L, G):
    nc = p.nc
    cvec, ada_w, ada_b = io["cvec"], io["ada_w"], io["ada_b"]
    modrow = io["modrow"]
    with p.phase():
        cT = p.sb("cT", [128, KC, 2], F32)
        sT = p.sb("sT", [128, KC, 2], F32)
        for r in range(2):
            p.dma("sp", lambda e, r=r: e.dma_start(out=cT[:, :, r], in_=cvec[r, :].rearrange("(c q) -> q c", q=128),
                                                   allow_slow_non_contiguous=True), w=["cT"])
        p.op("act", lambda e: e.activation(out=sT[:], in_=cT[:], func=AF.Silu), r=["cT"], w=["sT"])
        wts = [p.sb(f"adaw{i}", [128, KC, 512], F32) for i in range(2)]
        mrow = p.sb("mrow", [2, 6144], F32)
        brow = p.sb("brow", [2, 6144], F32)
        mps = p.ps("mps", [2, 512], F32)
        tps = p.ps("tps", [128, 48, 2], F32)
        mcol = p.sb("mcol", [128, 48, 2], F32)
        gcol = p.sb("gcol", [128, 2, KC], F32)
        it = 0
        for l in range(L):
            for r in range(2):
                p.dma("sp", lambda e, r=r, l=l: e.dma_start(out=brow[r:r + 1, :], in_=ada_b[l:l + 1, :]), w=["brow"])
            p.dma("sp", lambda e, l=l: e.dma_start(out=gcol[:, 0, :], in_=io["norm1_g"][l, :].rearrange("(c q) -> q c", q=128),
                                                   allow_slow_non_contiguous=True), w=["gcol"])
            p.dma("sp", lambda e, l=l: e.dma_start(out=gcol[:, 1, :], in_=io["norm2_g"][l, :].rearrange("(c q) -> q c", q=128),
                                                   allow_slow_non_contiguous=True), w=["gcol"])
            for cc in range(12):
                wt = wts[it % 2]
                wk = f"adaw{it % 2}"
                it += 1
                p.dma("sp", lambda e, wt=wt, l=l, cc=cc: e.dma_start(
                    out=wt[:], in_=ada_w[l, :, cc * 512:(cc + 1) * 512].rearrange("(c q) n -> q c n", q=128)), w=[wk])
                for kc in range(KC):
                    p.op("pe", lambda e, wt=wt, kc=kc: e.matmul(mps[:], lhsT=sT[:, kc, :], rhs=wt[:, kc, :],
                                                                 start=(kc == 0), stop=(kc == KC - 1)),
                         r=["sT", wk], w=["mps"])
                p.op("dve", lambda e, cc=cc: e.tensor_tensor(out=mrow[:, cc * 512:(cc + 1) * 512], in0=mps[:],
                                                             in1=brow[:, cc * 512:(cc + 1) * 512], op=ALU.add),
                     r=["mps", "brow"], w=["mrow"])
            p.dma("sp", lambda e, l=l: e.dma_start(out=modrow[l], in_=mrow[:]), r=["mrow"], w=[f"modrow{l}"])
            for c in range(48):
                p.op("pe", lambda e, c=c: e.transpose(tps[:, c, :], mrow[0:2, c * 128:(c + 1) * 128], C["ident"][0:2, 0:2]),
                     r=["mrow", "ident"], w=["tps"])
            p.op("dve", lambda e: e.tensor_copy(out=mcol[:], in_=tps[:]), r=["tps"], w=["mcol"])
            for r in range(2):
                for (nm, sc_c, sh_c, gi) in (("1", 1, 0, 0), ("2", 4, 3, 1)):
                    A = G["A" + nm]
                    B = G["B" + nm]
                    p.op("dve", lambda e, A=A, r=r, l=l, sc_c=sc_c, gi=gi: e.scalar_tensor_tensor(
                        out=A[:, l, r, :], in0=mcol[:, sc_c * 8:(sc_c + 1) * 8, r], scalar=1.0, in1=gcol[:, gi, :],
                        op0=ALU.add, op1=ALU.mult), r=["mcol", "gcol"], w=["G"])
                    p.op("dve", lambda e, B=B, r=r, l=l, sh_c=sh_c: e.tensor_copy(
                        out=B[:, l, r, :], in_=mcol[:, sh_c * 8:(sh_c + 1) * 8, r]), r=["mcol"], w=["G"])


def norm_tile(p, C, T, xt_key, xt, Acol, Bcol, hT_dst, hT_key, hT32_dst=None):
    ss, rstd, xn, junk = T["ss"], T["rstd"], T["xn"], T["junk"]
    p.op("act", lambda e: e.activation(out=junk[:], in_=xt[:], func=AF.Square, accum_out=ss[:, 0:1]),
         r=[xt_key], w=["junk", "ss"])
    p.op("dve", lambda e: e.tensor_scalar(out=rstd[:], in0=ss[:], scalar1=1.0 / D, scalar2=EPS, op0=ALU.mult, op1=ALU.add),
         r=["ss"], w=["rstd"])
    p.op("act", lambda e: e.activation(out=ss[:], in_=rstd[:], func=AF.Sqrt), r=["rstd"], w=["ss"])
    p.op("dve", lambda e: e.reciprocal(out=rstd[:], in_=ss[:]), r=["ss"], w=["rstd"])
    p.op("dve", lambda e: e.tensor_scalar(out=xn[:], in0=xt[:], scalar1=rstd[:, 0:1], scalar2=None, op0=ALU.mult),
         r=[xt_key, "rstd"], w=["xn"])
    for half in range(2):
        tp = T["tp"][half]
        tk = f"tp{half}"
        for j in range(4):
            kc = half * 4 + j
            p.op("pe", lambda e, tp=tp, j=j, kc=kc: e.transpose(tp[:, j, :], xn[:, kc * 128:(kc + 1) * 128], C["ident"][:]),
                 r=["xn", "ident"], w=[tk])
        for j in range(4):
            kc = half * 4 + j
            if hT32_dst is None:
                p.op("act", lambda e, tp=tp, j=j, kc=kc: e.activation(out=hT_dst(kc), in_=tp[:, j, :], func=AF.Identity,
                                                                       scale=Acol[:, kc:kc + 1], bias=Bcol[:, kc:kc + 1]),
                     r=[tk, "G"], w=[hT_key])
            else:
                p.op("act", lambda e, tp=tp, j=j, kc=kc: e.activation(out=hT32_dst(kc), in_=tp[:, j, :], func=AF.Identity,
                                                                       scale=Acol[:, kc:kc + 1], bias=Bcol[:, kc:kc + 1]),
                     r=[tk, "G"], w=[hT_key + "32"])
                p.op("pool", lambda e, kc=kc: e.tensor_copy(out=hT_dst(kc), in_=hT32_dst(kc)), r=[hT_key + "32"], w=[hT_key])


def norm_scratch(p):
    T = {}
    T["ss"] = p.sb("ss", [128, 1], F32)
    T["rstd"] = p.sb("rstd", [128, 1], F32)
    T["xn"] = p.sb("xn", [128, D], F32)
    T["junk"] = p.sb("junk", [128, D], BF16)
    T["tp"] = [p.ps(f"tp{h}", [128, 4, 128], F32) for h in range(2)]
    return T


def load_cast_weight(p, dst, dst_key, src_ap, rows_kc, ncols, stage, stage_key, eng_cycle=("dve", "pool")):
    for kc in range(rows_kc):
        st = stage[kc % len(stage)]
        sk = f"{stage_key}{kc % len(stage)}"
        p.dma("sp", lambda e, st=st, kc=kc: e.dma_start(out=st[:, 0:ncols], in_=src_ap[kc * 128:(kc + 1) * 128, :]), w=[sk])
        eng = eng_cycle[kc % len(eng_cycle)]
        p.op(eng, lambda e, st=st, kc=kc: e.tensor_copy(out=dst[:, kc, :], in_=st[:, 0:ncols]), r=[sk], w=[dst_key])


def phase_inproj(p, C, io, G, l, n_ctx, n_tok):
    xres, PT, PTOK, w_in = io["xres"], io["PT"], io["PTOK"], io["ev_w_in"]
    with p.phase():
        T = norm_scratch(p)
        wbf = p.sb("winbf", [128, KC, EVEN_IN], BF16)
        stage = [p.sb(f"wstage{i}", [128, EVEN_IN], F32) for i in range(2)]
        load_cast_weight(p, wbf, "winbf", w_in, KC, EVEN_IN, stage, "wstage")
        xts = [p.sb(f"xt{i}", [128, D], F32) for i in range(2)]
        hT = [p.sb(f"hT{i}", [128, KC, 512], BF16) for i in range(2)]
        pps = [p.ps(f"pps{i}", [128, 512], F32) for i in range(3)]
        ost = [p.sb(f"ost{i}", [128, 512], F32) for i in range(4)]
        ntile = n_tok // 128
        nsup = (ntile + 3) // 4
        oi = 0
        pi = 0
        tok_cols = [(512, 512), (1024, 512), (GLA_COLS + 1024, 512)]
        for s in range(nsup):
            tiles = list(range(s * 4, min(ntile, s * 4 + 4)))
            N = len(tiles) * 128
            h = hT[s % 2]
            hk = f"hT{s % 2}"
            for j, ti in enumerate(tiles):
                r = 1 if ti * 128 < n_ctx else 0
                xt = xts[ti % 2]
                xk = f"xt{ti % 2}"
                p.dma("sp", lambda e, xt=xt, ti=ti: e.dma_start(out=xt[:], in_=xres[ti * 128:(ti + 1) * 128, :]), w=[xk])
                norm_tile(p, C, T, xk, xt, G["A1"][:, l, r, :], G["B1"][:, l, r, :],
                          lambda kc, h=h, j=j: h[:, kc, j * 128:(j + 1) * 128], hk)
            ncc = (EVEN_IN + 127) // 128
            for cc in range(ncc):
                cw = min(128, EVEN_IN - cc * 128)
                ps = pps[pi % 3]
                pk = f"pps{pi % 3}"
                pi += 1
                for kc in range(KC):
                    p.op("pe", lambda e, ps=ps, kc=kc, cc=cc, cw=cw, h=h, N=N: e.matmul(
                        ps[0:cw, 0:N], lhsT=wbf[:, kc, cc * 128:cc * 128 + cw], rhs=h[:, kc, 0:N],
                        start=(kc == 0), stop=(kc == KC - 1)), r=["winbf", hk], w=[pk])
                o = ost[oi % 4]
                ok = f"ost{oi % 4}"
                eng = "dve" if oi % 2 == 0 else "act"
                oi += 1
                if eng == "dve":
                    p.op("dve", lambda e, o=o, ps=ps, cw=cw, N=N: e.tensor_copy(out=o[0:cw, 0:N], in_=ps[0:cw, 0:N]), r=[pk], w=[ok])
                else:
                    p.op("act", lambda e, o=o, ps=ps, cw=cw, N=N: e.copy(out=o[0:cw, 0:N], in_=ps[0:cw, 0:N]), r=[pk], w=[ok])
                p.dma("sp", lambda e, o=o, cc=cc, cw=cw, N=N, s=s: e.dma_start(
                    out=PT[cc * 128:cc * 128 + cw, s * 512:s * 512 + N], in_=o[0:cw, 0:N]), r=[ok], w=["PT"])
            for j, ti in enumerate(tiles):
                for gi, (c0, cn) in enumerate(tok_cols):
                    ps = pps[pi % 3]
                    pk = f"pps{pi % 3}"
                    pi += 1
                    for kc in range(KC):
                        p.op("pe", lambda e, ps=ps, kc=kc, c0=c0, cn=cn, h=h, j=j: e.matmul(
                            ps[:, 0:cn], lhsT=h[:, kc, j * 128:(j + 1) * 128], rhs=wbf[:, kc, c0:c0 + cn],
                            start=(kc == 0), stop=(kc == KC - 1)), r=["winbf", hk], w=[pk])
                    o = ost[oi % 4]
                    ok = f"ost{oi % 4}"
                    eng = "dve" if oi % 2 == 0 else "act"
                    oi += 1
                    if eng == "dve":
                        p.op("dve", lambda e, o=o, ps=ps, cn=cn: e.tensor_copy(out=o[:, 0:cn], in_=ps[:, 0:cn]), r=[pk], w=[ok])
                    else:
                        p.op("act", lambda e, o=o, ps=ps, cn=cn: e.copy(out=o[:, 0:cn], in_=ps[:, 0:cn]), r=[pk], w=[ok])
                    p.dma("sp", lambda e, o=o, ti=ti, gi=gi, cn=cn: e.dma_start(
                        out=PTOK[ti * 128:(ti + 1) * 128, gi * 512:gi * 512 + cn], in_=o[:, 0:cn]), r=[ok], w=["PTOK"])

import os


class Ring:
    def __init__(self, p, name, shape, dt=F32, n=2, psum=False):
        self.t = [(p.ps if psum else p.sb)(f"{name}{i}", shape, dt) for i in range(n)]
        self.k = [f"{name}{i}" for i in range(n)]
        self.i = 0

    def next(self):
        i = self.i
        self.i = (i + 1) % len(self.t)
        return self.t[i], self.k[i]


class PsRing:
    def __init__(self, p, name, nbanks):
        self.banks = [p.ps(f"{name}{i}", [128, 512], F32) for i in range(nbanks)]
        self.n = nbanks * 1
        self.sub = 1
        self.name = name
        self.i = 0

    def next(self):
        i = self.i
        self.i = (i + 1) % self.n
        sb_ = self.sub
        return self.banks[i // sb_][:, (i % sb_) * 128:(i % sb_ + 1) * 128], f"{self.name}_{i}"


def make_masks(p, C):
    f, q = C["iota_f"], C["iota_p"]
    for nm, op in (("incl0", ALU.is_ge), ("incl1", ALU.is_le), ("strict0", ALU.is_gt), ("strict1", ALU.is_lt)):
        m = p.sb("m_" + nm, [128, 128], F32)
        p.op("dve", lambda e, m=m, op=op: e.tensor_scalar(out=m[:], in0=f[:], scalar1=q[:, 0:1], scalar2=None, op0=op),
             r=["iota_f", "iota_p"], w=["masks"])
        C[nm] = m
    for nm, val in (("ones128", 1.0 / 128), ("ones64", 1.0), ("ones64m", 1.0 / 64)):
        m = p.sb("m_" + nm, [128, 128], F32)
        p.op("dve", lambda e, m=m, val=val: e.memset(m[:], val), w=["masks"])
        C[nm] = m


def chunk_order(n_ctx, n_tok):
    nc_, nt = n_ctx // 128, n_tok // 128
    fwd = list(range(nt))
    bwd = list(range(nc_ - 1, -1, -1)) + list(range(nt - 1, nc_ - 1, -1))
    return [fwd, bwd]


def phase_gla(p, C, io, n_ctx, n_tok):
    PT, PTOK, YT = io["PT"], io["PTOK"], io["YTG"]
    gk_up, gk_b = io["gla_gk_up"], io["gla_gk_b"]
    order = chunk_order(n_ctx, n_tok)
    nt = n_tok // 128
    with p.phase():
        gkup = p.sb("gkup", [16, 2, 256], F32)
        gkb = p.sb("gkb", [64, 2, 4], F32)
        for d in range(2):
            p.dma("sp", lambda e, d=d: e.dma_start(out=gkup[:, d, :], in_=gk_up[d]), w=["gkw"])
            p.dma("sp", lambda e, d=d: e.dma_start(out=gkb[:, d, :], in_=gk_b[d].rearrange("(h q) -> q h", q=64),
                                                   allow_slow_non_contiguous=True), w=["gkw"])
        S = [[p.sb(f"S{d}{h}", [64, 128], F32) for h in range(4)] for d in range(2)]
        for d in range(2):
            for h in range(4):
                p.op("pool", lambda e, t=S[d][h]: e.memset(t[:], 0.0), w=[f"S{d}{h}"])
        gd = Ring(p, "gd", [16, 128], n=2)
        qk = Ring(p, "qk", [64, 2, 128], n=4)
        vv = Ring(p, "vv", [128, 128], n=4)
        sg = Ring(p, "sg", [64, 128], n=3)
        lg = Ring(p, "lg", [64, 128], n=3)
        lgT = Ring(p, "lgT", [128, 64], n=3)
        eW = Ring(p, "eW", [64, 128], n=3)
        eWi = Ring(p, "eWi", [64, 128], n=3)
        rt = Ring(p, "rt", [64, 128], n=3)
        kt = Ring(p, "kt", [64, 128], n=3)
        ktok = Ring(p, "ktok", [128, 64], n=3)
        mrk = Ring(p, "mrk", [128, 128], n=3)
        yts = Ring(p, "yts", [128, 128], n=3)
        s2 = Ring(p, "s2", [64, 128], n=3)
        psA = Ring(p, "psA", [128, 128], n=6, psum=True)
        for i in range(nt):
            for d in range(2):
                c = order[d][i]
                t0 = c * 128
                tend = 127 if d == 0 else 0
                gdt, gdk = gd.next()
                p.dma("sp", lambda e, gdt=gdt, t0=t0: e.dma_start(out=gdt[:], in_=PT[1536:1552, t0:t0 + 128]), w=[gdk])
                for h in range(4):
                    Sk = f"S{d}{h}"
                    St = S[d][h]
                    qt, qkk = qk.next()
                    p.dma("sp", lambda e, qt=qt, h=h, t0=t0: e.dma_start(out=qt[:, 0, :], in_=PT[h * 64:(h + 1) * 64, t0:t0 + 128]), w=[qkk])
                    p.dma("sp", lambda e, qt=qt, h=h, t0=t0: e.dma_start(out=qt[:, 1, :], in_=PT[256 + h * 64:256 + (h + 1) * 64, t0:t0 + 128]), w=[qkk])
                    vt, vk = vv.next()
                    p.dma("sp", lambda e, vt=vt, h=h, t0=t0: e.dma_start(out=vt[:], in_=PTOK[t0:t0 + 128, h * 128:(h + 1) * 128]), w=[vk])
                    ps, pk = psA.next()
                    p.op("pe", lambda e, ps=ps, d=d, h=h, gdt=gdt: e.matmul(ps[0:64, :], lhsT=gkup[:, d, h * 64:(h + 1) * 64], rhs=gdt[:],
                                                                            start=True, stop=True), r=["gkw", gdk], w=[pk])
                    sgt, sgk = sg.next()
                    p.op("act", lambda e, sgt=sgt, ps=ps, d=d, h=h: e.activation(out=sgt[:], in_=ps[0:64, :], func=AF.Sigmoid,
                                                                                bias=gkb[:, d, h:h + 1]), r=[pk, "gkw"], w=[sgk])
                    lgt, lgk = lg.next()
                    p.op("act", lambda e, lgt=lgt, sgt=sgt: e.activation(out=lgt[:], in_=sgt[:], func=AF.Ln), r=[sgk], w=[lgk])
                    ps, pk = psA.next()
                    p.op("pe", lambda e, ps=ps, lgt=lgt: e.transpose(ps[:, 0:64], lgt[:], C["ident"][0:64, 0:64]), r=[lgk, "ident"], w=[pk])
                    lTt, lTk = lgT.next()
                    p.op("dve", lambda e, lTt=lTt, ps=ps: e.tensor_copy(out=lTt[:], in_=ps[:, 0:64]), r=[pk], w=[lTk])
                    ps, pk = psA.next()
                    p.op("pe", lambda e, ps=ps, lTt=lTt, d=d: e.matmul(ps[0:64, :], lhsT=lTt[:], rhs=C[f"incl{d}"][:], start=True, stop=True),
                         r=[lTk, "masks"], w=[pk])
                    eWt, eWk = eW.next()
                    eWit, eWik = eWi.next()
                    p.op("act", lambda e, eWt=eWt, ps=ps: e.activation(out=eWt[:], in_=ps[0:64, :], func=AF.Exp, scale=1.0 / 16), r=[pk], w=[eWk])
                    p.op("act", lambda e, eWit=eWit, ps=ps: e.activation(out=eWit[:], in_=ps[0:64, :], func=AF.Exp, scale=-1.0 / 16), r=[pk], w=[eWik])
                    rtt, rtk = rt.next()
                    ktt, ktk = kt.next()
                    p.op("dve", lambda e, rtt=rtt, qt=qt, eWt=eWt: e.scalar_tensor_tensor(out=rtt[:], in0=qt[:, 0, :], scalar=0.125, in1=eWt[:],
                                                                                         op0=ALU.mult, op1=ALU.mult), r=[qkk, eWk], w=[rtk])
                    p.op("dve", lambda e, ktt=ktt, qt=qt, eWit=eWit: e.tensor_tensor(out=ktt[:], in0=qt[:, 1, :], in1=eWit[:], op=ALU.mult),
                         r=[qkk, eWik], w=[ktk])
                    ps, pk = psA.next()
                    p.op("pe", lambda e, ps=ps, ktt=ktt: e.transpose(ps[:, 0:64], ktt[:], C["ident"][0:64, 0:64]), r=[ktk, "ident"], w=[pk])
                    kTt, kTk = ktok.next()
                    p.op("dve", lambda e, kTt=kTt, ps=ps: e.tensor_copy(out=kTt[:], in_=ps[:, 0:64]), r=[pk], w=[kTk])
                    ps, pk = psA.next()
                    p.op("pe", lambda e, ps=ps, ktt=ktt, rtt=rtt: e.matmul(ps[:], lhsT=ktt[:], rhs=rtt[:], start=True, stop=True), r=[ktk, rtk], w=[pk])
                    mt, mk = mrk.next()
                    p.op("dve", lambda e, mt=mt, ps=ps, d=d: e.tensor_tensor(out=mt[:], in0=ps[:], in1=C[f"incl{d}"][:], op=ALU.mult),
                         r=[pk, "masks"], w=[mk])
                    ps, pk = psA.next()
                    p.op("pe", lambda e, ps=ps, St=St, rtt=rtt: e.matmul(ps[:], lhsT=St[:], rhs=rtt[:], start=True, stop=False), r=[Sk, rtk], w=[pk])
                    p.op("pe", lambda e, ps=ps, vt=vt, mt=mt: e.matmul(ps[:], lhsT=vt[:], rhs=mt[:], start=False, stop=True), r=[vk, mk], w=[pk])
                    yt, yk = yts.next()
                    p.op("act", lambda e, yt=yt, ps=ps: e.copy(out=yt[:], in_=ps[:]), r=[pk], w=[yk])
                    p.dma("sp", lambda e, yt=yt, d=d, h=h, t0=t0: e.dma_start(out=YT[d, h * 128:(h + 1) * 128, t0:t0 + 128], in_=yt[:]),
                          r=[yk], w=["YTG"])
                    ps, pk = psA.next()
                    p.op("pe", lambda e, ps=ps, kTt=kTt, vt=vt: e.matmul(ps[0:64, :], lhsT=kTt[:], rhs=vt[:], start=True, stop=True), r=[kTk, vk], w=[pk])
                    s2t, s2k = s2.next()
                    p.op("dve", lambda e, s2t=s2t, St=St, eWt=eWt, tend=tend: e.tensor_scalar(out=s2t[:], in0=St[:], scalar1=eWt[:, tend:tend + 1],
                                                                                             scalar2=None, op0=ALU.mult), r=[Sk, eWk], w=[s2k])
                    p.op("dve", lambda e, s2t=s2t, St=St, eWt=eWt, ps=ps, tend=tend: e.scalar_tensor_tensor(
                        out=St[:], in0=ps[0:64, :], scalar=eWt[:, tend:tend + 1], in1=s2t[:], op0=ALU.mult, op1=ALU.add),
                        r=[pk, eWk, s2k], w=[Sk])


def phase_gla_finish(p, C, io, n_tok):
    PT, YT, MIXT = io["PT"], io["YTG"], io["MIXT"]
    with p.phase():
        gcol = p.sb("gng", [128, 1], F32)
        p.dma("sp", lambda e: e.dma_start(out=gcol[:, 0:1], in_=io["gla_norm_g"].rearrange("(q o) -> q o", o=1)), w=["gng"])
        y0 = Ring(p, "y0", [128, 512], n=2)
        y1 = Ring(p, "y1", [128, 512], n=2)
        og = Ring(p, "og", [128, 512], n=2)
        sq = Ring(p, "sq", [128, 512], n=2)
        rs = Ring(p, "rs", [128, 512], n=2)
        ob = Ring(p, "ob", [128, 512], BF16, n=2)
        psF = Ring(p, "psF", [128, 512], n=2, psum=True)
        for t0 in range(0, n_tok, 512):
            N = min(512, n_tok - t0)
            for h in range(4):
                a, ak = y0.next()
                b, bk = y1.next()
                o, ok = og.next()
                p.dma("sp", lambda e, a=a, h=h, t0=t0, N=N: e.dma_start(out=a[:, 0:N], in_=YT[0, h * 128:(h + 1) * 128, t0:t0 + N]), r=["YTG"], w=[ak])
                p.dma("sp", lambda e, b=b, h=h, t0=t0, N=N: e.dma_start(out=b[:, 0:N], in_=YT[1, h * 128:(h + 1) * 128, t0:t0 + N]), r=["YTG"], w=[bk])
                p.dma("sp", lambda e, o=o, h=h, t0=t0, N=N: e.dma_start(out=o[:, 0:N], in_=PT[1024 + h * 128:1024 + (h + 1) * 128, t0:t0 + N]), r=["PT"], w=[ok])
                p.op("dve", lambda e, a=a, b=b, N=N: e.tensor_tensor(out=a[:, 0:N], in0=a[:, 0:N], in1=b[:, 0:N], op=ALU.add), r=[ak, bk], w=[ak])
                s, sk = sq.next()
                p.op("act", lambda e, s=s, a=a, N=N: e.activation(out=s[:, 0:N], in_=a[:, 0:N], func=AF.Square), r=[ak], w=[sk])
                ps, pk = psF.next()
                p.op("pe", lambda e, ps=ps, s=s, N=N: e.matmul(ps[:, 0:N], lhsT=C["ones128"][:], rhs=s[:, 0:N], start=True, stop=True), r=[sk, "masks"], w=[pk])
                r_, rk = rs.next()
                p.op("dve", lambda e, r_=r_, ps=ps, N=N: e.tensor_scalar(out=r_[:, 0:N], in0=ps[:, 0:N], scalar1=EPS, scalar2=None, op0=ALU.add), r=[pk], w=[rk])
                p.op("act", lambda e, r_=r_, N=N: e.activation(out=r_[:, 0:N], in_=r_[:, 0:N], func=AF.Sqrt), r=[rk], w=[rk])
                p.op("dve", lambda e, r_=r_, N=N: e.reciprocal(out=r_[:, 0:N], in_=r_[:, 0:N]), r=[rk], w=[rk])
                p.op("dve", lambda e, a=a, r_=r_, N=N: e.scalar_tensor_tensor(out=a[:, 0:N], in0=a[:, 0:N], scalar=gcol[:, 0:1], in1=r_[:, 0:N],
                                                                             op0=ALU.mult, op1=ALU.mult), r=[ak, rk, "gng"], w=[ak])
                p.op("act", lambda e, s=s, o=o, N=N: e.activation(out=s[:, 0:N], in_=o[:, 0:N], func=AF.Silu), r=[ok], w=[sk])
                ot, otk = ob.next()
                p.op("dve", lambda e, ot=ot, a=a, s=s, N=N: e.tensor_tensor(out=ot[:, 0:N], in0=a[:, 0:N], in1=s[:, 0:N], op=ALU.mult), r=[ak, sk], w=[otk])
                p.dma("sp", lambda e, ot=ot, h=h, t0=t0, N=N: e.dma_start(out=MIXT[h * 128:(h + 1) * 128, t0:t0 + N], in_=ot[:, 0:N]), r=[otk], w=["MIXT"])


import os
STAGE = 99
R0 = GLA_COLS
NLW = 0.6065306597126334


def phase_shift(p, C, io, n_ctx, n_tok):
    PT, SMT, mu = io["PT"], io["SMT"], io["rw_mu"]
    with p.phase():
        nch = (RW_COLS + 127) // 128
        muc = p.sb("muc", [128, nch, 2], F32)
        p.op("pool", lambda e: e.memset(muc[:], 0.0), w=["muc"])
        for j in range(2):
            for c in range(nch):
                cw = min(128, RW_COLS - c * 128)
                p.dma("sp", lambda e, j=j, c=c, cw=cw: e.dma_start(out=muc[0:cw, c, j:j + 1],
                                                                   in_=mu[j, c * 128:c * 128 + cw].rearrange("(q o) -> q o", o=1)), w=["muc"])
        xin = Ring(p, "shx", [128, 514], n=3)
        d0 = Ring(p, "shd", [128, 512], n=3)
        so = Ring(p, "sho", [128, 512], n=3)
        blocks = []
        for (a, b) in ((0, n_ctx), (n_ctx, n_tok)):
            t = a
            while t < b:
                N = min(512, b - t)
                blocks.append((t, N, t == a, t + N == b))
                t += N
        for (t0, N, first, last) in blocks:
            for c in range(nch):
                cw = min(128, RW_COLS - c * 128)
                x, xk = xin.next()
                lo = 0 if not first else 1
                hi = N + 2 if not last else N + 1
                if first or last:
                    p.op("pool", lambda e, x=x: e.memset(x[:], 0.0), w=[xk])
                p.dma("sp", lambda e, x=x, c=c, cw=cw, lo=lo, hi=hi, t0=t0: e.dma_start(
                    out=x[0:cw, lo:hi], in_=PT[R0 + c * 128:R0 + c * 128 + cw, t0 - 1 + lo:t0 - 1 + hi]), w=[xk])
                dd, dk = d0.next()
                o, ok = so.next()
                p.op("dve", lambda e, dd=dd, x=x, cw=cw, N=N: e.tensor_tensor(out=dd[0:cw, 0:N], in0=x[0:cw, 0:N], in1=x[0:cw, 1:N + 1], op=ALU.subtract),
                     r=[xk], w=[dk])
                p.op("dve", lambda e, o=o, dd=dd, x=x, c=c, cw=cw, N=N: e.scalar_tensor_tensor(
                    out=o[0:cw, 0:N], in0=dd[0:cw, 0:N], scalar=muc[0:cw, c, 0:1], in1=x[0:cw, 1:N + 1], op0=ALU.mult, op1=ALU.add),
                    r=[dk, xk, "muc"], w=[ok])
                p.op("pool", lambda e, dd=dd, x=x, cw=cw, N=N: e.tensor_tensor(out=dd[0:cw, 0:N], in0=x[0:cw, 2:N + 2], in1=x[0:cw, 1:N + 1], op=ALU.subtract),
                     r=[xk, ok], w=[dk])
                p.op("dve", lambda e, o=o, dd=dd, c=c, cw=cw, N=N: e.scalar_tensor_tensor(
                    out=o[0:cw, 0:N], in0=dd[0:cw, 0:N], scalar=muc[0:cw, c, 1:2], in1=o[0:cw, 0:N], op0=ALU.mult, op1=ALU.add),
                    r=[dk, ok, "muc"], w=[ok])
                p.dma("sp", lambda e, o=o, c=c, cw=cw, N=N, t0=t0: e.dma_start(out=SMT[c * 128:c * 128 + cw, t0:t0 + N], in_=o[0:cw, 0:N]),
                      r=[ok], w=["SMT"])


def colload(p, dst, key, src_vec, nh, j=None):
    out = dst[:, :] if j is None else dst[:, j, :]
    p.dma("sp", lambda e: e.dma_start(out=out, in_=src_vec.rearrange("(h q) -> q h", q=64), allow_slow_non_contiguous=True), w=[key])


def phase_rwkv(p, C, io, n_ctx, n_tok):
    SMT, YT, RK = io["SMT"], io["YTR"], io["RK"]
    order = chunk_order(n_ctx, n_tok)
    nt = n_tok // 128
    with p.phase():
        w2 = p.sb("rw2", [64, 2, 512], F32)
        a2 = p.sb("ra2", [64, 2, 512], F32)
        for d in range(2):
            p.dma("sp", lambda e, d=d: e.dma_start(out=w2[:, d, :], in_=io["rw_w2"][d]), w=["rww"])
            p.dma("sp", lambda e, d=d: e.dma_start(out=a2[:, d, :], in_=io["rw_a2"][d]), w=["rww"])
        w0 = p.sb("rw0", [64, 2, 8], F32)
        a0 = p.sb("ra0", [64, 2, 8], F32)
        for d in range(2):
            colload(p, w0, "rww", io["rw_w0"][d], 8, d)
            colload(p, a0, "rww", io["rw_a0"][d], 8, d)
        kkc = p.sb("rkk", [64, 8], F32)
        kac = p.sb("rka", [64, 8], F32)
        kam = p.sb("rkam", [64, 8], F32)
        rkc = p.sb("rrk", [64, 8], F32)
        colload(p, kkc, "rww", io["rw_k_k"], 8)
        colload(p, kac, "rww", io["rw_k_a"], 8)
        colload(p, rkc, "rww", io["rw_r_k"].rearrange("h n -> (h n)"), 8)
        p.op("dve", lambda e: e.tensor_scalar(out=kam[:], in0=kac[:], scalar1=-1.0, scalar2=1.0, op0=ALU.mult, op1=ALU.add), r=["rww"], w=["rkam"])
        T = [[p.sb(f"T{d}{h}", [64, 64], F32) for h in range(8)] for d in range(2)]
        for d in range(2):
            for h in range(8):
                p.op("pool", lambda e, t=T[d][h]: e.memset(t[:], 0.0), w=[f"T{d}{h}"])
        wa = Ring(p, "wa", [64, 2, 128], n=2)
        rkv = Ring(p, "rkv", [64, 3, 128], n=3)
        f64 = {nm: Ring(p, nm, [64, 128], n=3) for nm in
               ("sgw", "alp", "kq", "sqk", "nrm", "kk", "tmk", "kd", "eW", "eWi", "eWx", "cx", "rt", "kt", "bt", "at", "rk_", "yo")}
        tokr = {nm: Ring(p, nm, [128, 64], n=3) for nm in ("sgT", "Ktok", "Btok", "Vtok", "Xs", "Us")}
        sqm = {nm: Ring(p, nm, [128, 128], n=3) for nm in ("At", "Am", "Akt", "Mrbt", "Mrkt", "Pt")}
        tw = Ring(p, "tw", [64, 64], n=3)
        psA = PsRing(p, "psR", 6)

        def mm(out_ap, pk, lhsT, rhs, r, start=True, stop=True):
            p.op("pe", lambda e: e.matmul(out_ap, lhsT=lhsT, rhs=rhs, start=start, stop=stop), r=r, w=[pk])

        def tr(out_ap, pk, in_ap, n, r):
            p.op("pe", lambda e: e.transpose(out_ap, in_ap, C["ident"][0:n, 0:n]), r=r + ["ident"], w=[pk])

        for i in range(nt):
            for d in range(2):
                c = order[d][i]
                t0 = c * 128
                tend = 127 if d == 0 else 0
                sl = slice(t0, t0 + 128)
                wat, wak = wa.next()
                p.dma("sp", lambda e, wat=wat, sl=sl: e.dma_start(out=wat[:, 0, :], in_=SMT[1536:1600, sl]), w=[wak])
                p.dma("sp", lambda e, wat=wat, sl=sl: e.dma_start(out=wat[:, 1, :], in_=SMT[1600:1664, sl]), w=[wak])
                p.op("act", lambda e, wat=wat: e.activation(out=wat[:, 0, :], in_=wat[:, 0, :], func=AF.Tanh), r=[wak], w=[wak])
                for h in range(8):
                    hs = slice(h * 64, (h + 1) * 64)
                    Tt, Tk = T[d][h], f"T{d}{h}"
                    x, xk = rkv.next()
                    for j in range(3):
                        p.dma("sp", lambda e, x=x, j=j, h=h, sl=sl: e.dma_start(out=x[:, j, :], in_=SMT[j * 512 + h * 64:j * 512 + (h + 1) * 64, sl]), w=[xk])
                    r_, k_, v_ = x[:, 0, :], x[:, 1, :], x[:, 2, :]
                    ps, pk = psA.next()
                    mm(ps[0:64, :], pk, w2[:, d, hs], wat[:, 0, :], ["rww", wak])
                    sgw, sgwk = f64["sgw"].next()
                    p.op("act", lambda e, sgw=sgw, ps=ps, d=d, h=h: e.activation(out=sgw[:], in_=ps[0:64, :], func=AF.Sigmoid, bias=w0[:, d, h:h + 1]),
                         r=[pk, "rww"], w=[sgwk])
                    ps, pk = psA.next()
                    mm(ps[0:64, :], pk, a2[:, d, hs], wat[:, 1, :], ["rww", wak])
                    alp, alpk = f64["alp"].next()
                    p.op("act", lambda e, alp=alp, ps=ps, d=d, h=h: e.activation(out=alp[:], in_=ps[0:64, :], func=AF.Sigmoid, bias=a0[:, d, h:h + 1]),
                         r=[pk, "rww"], w=[alpk])
                    if STAGE < 1:
                        continue
                    kq, kqk = f64["kq"].next()
                    p.op("dve", lambda e, kq=kq, k_=k_, h=h: e.tensor_scalar(out=kq[:], in0=k_, scalar1=kkc[:, h:h + 1], scalar2=None, op0=ALU.mult),
                         r=[xk, "rww"], w=[kqk])
                    sqk, sqkk = f64["sqk"].next()
                    p.op("act", lambda e, sqk=sqk, kq=kq: e.activation(out=sqk[:], in_=kq[:], func=AF.Square), r=[kqk], w=[sqkk])
                    ps, pk = psA.next()
                    mm(ps[0:64, :], pk, C["ones64"][0:64, 0:64], sqk[:], ["masks", sqkk])
                    nrm, nrmk = f64["nrm"].next()
                    p.op("act", lambda e, nrm=nrm, ps=ps: e.activation(out=nrm[:], in_=ps[0:64, :], func=AF.Sqrt), r=[pk], w=[nrmk])
                    p.op("dve", lambda e, nrm=nrm: e.tensor_scalar(out=nrm[:], in0=nrm[:], scalar1=1e-12, scalar2=None, op0=ALU.max), r=[nrmk], w=[nrmk])
                    p.op("dve", lambda e, nrm=nrm: e.reciprocal(out=nrm[:], in_=nrm[:]), r=[nrmk], w=[nrmk])
                    kk, kkk = f64["kk"].next()
                    p.op("dve", lambda e, kk=kk, kq=kq, nrm=nrm: e.tensor_tensor(out=kk[:], in0=kq[:], in1=nrm[:], op=ALU.mult), r=[kqk, nrmk], w=[kkk])
                    tmk, tmkk = f64["tmk"].next()
                    p.op("dve", lambda e, tmk=tmk, alp=alp, h=h: e.tensor_scalar(out=tmk[:], in0=alp[:], scalar1=kac[:, h:h + 1], scalar2=kam[:, h:h + 1],
                                                                                op0=ALU.mult, op1=ALU.add), r=[alpk, "rww", "rkam"], w=[tmkk])
                    kd, kdk = f64["kd"].next()
                    p.op("dve", lambda e, kd=kd, tmk=tmk, k_=k_: e.tensor_tensor(out=kd[:], in0=tmk[:], in1=k_, op=ALU.mult), r=[tmkk, xk], w=[kdk])
                    rk_, rkk = f64["rk_"].next()
                    p.op("dve", lambda e, rk_=rk_, r_=r_, kd=kd, h=h: e.scalar_tensor_tensor(out=rk_[:], in0=r_, scalar=rkc[:, h:h + 1], in1=kd[:],
                                                                                             op0=ALU.mult, op1=ALU.mult), r=[xk, kdk, "rww"], w=[rkk])
                    p.dma("sp", lambda e, rk_=rk_, d=d, hs=hs, sl=sl: e.dma_start(out=RK[d, hs, sl], in_=rk_[:]), r=[rkk], w=["RK"])
                    if STAGE < 2:
                        continue
                    ps, pk = psA.next()
                    tr(ps[:, 0:64], pk, sgw[:], 64, [sgwk])
                    sgT, sgTk = tokr["sgT"].next()
                    p.op("dve", lambda e, sgT=sgT, ps=ps: e.tensor_copy(out=sgT[:], in_=ps[:, 0:64]), r=[pk], w=[sgTk])
                    ps, pk = psA.next()
                    mm(ps[0:64, :], pk, sgT[:], C[f"incl{d}"][:], [sgTk, "masks"])
                    eW, eWk = f64["eW"].next()
                    eWi, eWik = f64["eWi"].next()
                    cx, cxk = f64["cx"].next()
                    eWx, eWxk = f64["eWx"].next()
                    p.op("act", lambda e, eW=eW, ps=ps: e.activation(out=eW[:], in_=ps[0:64, :], func=AF.Exp, scale=-NLW), r=[pk], w=[eWk])
                    p.op("act", lambda e, eWi=eWi, ps=ps: e.activation(out=eWi[:], in_=ps[0:64, :], func=AF.Exp, scale=NLW), r=[pk], w=[eWik])
                    p.op("dve", lambda e, cx=cx, ps=ps, sgw=sgw: e.tensor_tensor(out=cx[:], in0=ps[0:64, :], in1=sgw[:], op=ALU.subtract), r=[pk, sgwk], w=[cxk])
                    p.op("act", lambda e, eWx=eWx, cx=cx: e.activation(out=eWx[:], in_=cx[:], func=AF.Exp, scale=-NLW), r=[cxk], w=[eWxk])
                    rt, rtk = f64["rt"].next()
                    kt, ktk = f64["kt"].next()
                    bt, btk = f64["bt"].next()
                    at, atk = f64["at"].next()
                    p.op("dve", lambda e, rt=rt, r_=r_, eW=eW: e.tensor_tensor(out=rt[:], in0=r_, in1=eW[:], op=ALU.mult), r=[xk, eWk], w=[rtk])
                    p.op("pool", lambda e, kt=kt, kd=kd, eWi=eWi: e.tensor_tensor(out=kt[:], in0=kd[:], in1=eWi[:], op=ALU.mult), r=[kdk, eWik], w=[ktk])
                    p.op("dve", lambda e, bt=bt, kk=kk, alp=alp: e.tensor_tensor(out=bt[:], in0=kk[:], in1=alp[:], op=ALU.mult), r=[kkk, alpk], w=[btk])
                    p.op("dve", lambda e, bt=bt, eWi=eWi: e.tensor_tensor(out=bt[:], in0=bt[:], in1=eWi[:], op=ALU.mult), r=[btk, eWik], w=[btk])
                    p.op("dve", lambda e, at=at, kk=kk, eWx=eWx: e.scalar_tensor_tensor(out=at[:], in0=kk[:], scalar=-1.0, in1=eWx[:], op0=ALU.mult, op1=ALU.mult),
                         r=[kkk, eWxk], w=[atk])
                    if STAGE < 3:
                        continue
                    toks = {}
                    for nm, src, sk in (("Ktok", kt[:], ktk), ("Btok", bt[:], btk), ("Vtok", v_, xk)):
                        ps, pk = psA.next()
                        tr(ps[:, 0:64], pk, src, 64, [sk])
                        tt, ttk = tokr[nm].next()
                        p.op("act" if nm != "Vtok" else "dve", (lambda e, tt=tt, ps=ps: e.copy(out=tt[:], in_=ps[:, 0:64])) if nm != "Vtok" else
                             (lambda e, tt=tt, ps=ps: e.tensor_copy(out=tt[:], in_=ps[:, 0:64])), r=[pk], w=[ttk])
                        toks[nm] = (tt, ttk)
                    Ktok, Ktokk = toks["Ktok"]
                    Btok, Btokk = toks["Btok"]
                    Vtok, Vtokk = toks["Vtok"]
                    if STAGE < 4:
                        continue
                    def gram(nm, lhsT, lk, rhs, rk2, mask):
                        ps, pk = psA.next()
                        mm(ps[:, :], pk, lhsT, rhs, [lk, rk2])
                        m, mk = sqm[nm].next()
                        p.op("dve", lambda e: e.tensor_tensor(out=m[:], in0=ps[:, :], in1=C[mask][:], op=ALU.mult), r=[pk, "masks"], w=[mk])
                        return m, mk
                    At, Atk = gram("At", bt[:], btk, at[:], atk, f"strict{d}")
                    Am, Amk = gram("Am", at[:], atk, bt[:], btk, f"strict{1 - d}")
                    Akt, Aktk = gram("Akt", kt[:], ktk, at[:], atk, f"strict{d}")
                    Mrbt, Mrbtk = gram("Mrbt", bt[:], btk, rt[:], rtk, f"incl{d}")
                    Mrkt, Mrktk = gram("Mrkt", kt[:], ktk, rt[:], rtk, f"incl{d}")
                    Pt, Ptk = sqm["Pt"].next()
                    p.op("pool", lambda e, Pt=Pt, At=At: e.tensor_tensor(out=Pt[:], in0=At[:], in1=C["ident"][:], op=ALU.add), r=[Atk, "ident"], w=[Ptk])
                    for step in range(6 if STAGE >= 45 else 0):
                        ps, pk = psA.next()
                        mm(ps[:, :], pk, At[:], Am[:], [Atk, Amk])
                        Am2, Am2k = sqm["Am"].next()
                        p.op("act", lambda e, Am2=Am2, ps=ps: e.copy(out=Am2[:], in_=ps[:, :]), r=[pk], w=[Am2k])
                        if step < 5:
                            ps, pk = psA.next()
                            mm(ps[:, :], pk, Am[:], At[:], [Atk, Amk])
                            At2, At2k = sqm["At"].next()
                            p.op("dve", lambda e, At2=At2, ps=ps: e.tensor_copy(out=At2[:], in_=ps[:, :]), r=[pk], w=[At2k])
                        ps, pk = psA.next()
                        mm(ps[:, :], pk, Am2[:], Pt[:], [Am2k, Ptk])
                        Pt2, Pt2k = sqm["Pt"].next()
                        p.op("dve", lambda e, Pt2=Pt2, ps=ps, Pt=Pt: e.tensor_tensor(out=Pt2[:], in0=ps[:, :], in1=Pt[:], op=ALU.add), r=[pk, Ptk], w=[Pt2k])
                        Pt, Ptk = Pt2, Pt2k
                        Am, Amk = Am2, Am2k
                        if step < 5:
                            At, Atk = At2, At2k
                    if STAGE < 5:
                        continue
                    ps, pk = psA.next()
                    mm(ps[:, 0:64], pk, at[:], Tt[:], [atk, Tk], start=True, stop=False)
                    mm(ps[:, 0:64], pk, Akt[:], Vtok[:], [Aktk, Vtokk], start=False, stop=True)
                    Xs, Xsk = tokr["Xs"].next()
                    p.op("act", lambda e, Xs=Xs, ps=ps: e.copy(out=Xs[:], in_=ps[:, 0:64]), r=[pk], w=[Xsk])
                    ps, pk = psA.next()
                    mm(ps[:, 0:64], pk, Pt[:], Xs[:], [Ptk, Xsk])
                    Us, Usk = tokr["Us"].next()
                    p.op("dve", lambda e, Us=Us, ps=ps: e.tensor_copy(out=Us[:], in_=ps[:, 0:64]), r=[pk], w=[Usk])
                    if STAGE < 6:
                        continue
                    ps, pk = psA.next()
                    mm(ps[0:64, :], pk, Tt[:], rt[:], [Tk, rtk], start=True, stop=False)
                    mm(ps[0:64, :], pk, Us[:], Mrbt[:], [Usk, Mrbtk], start=False, stop=False)
                    mm(ps[0:64, :], pk, Vtok[:], Mrkt[:], [Vtokk, Mrktk], start=False, stop=True)
                    yo, yok = f64["yo"].next()
                    p.op("act", lambda e, yo=yo, ps=ps: e.copy(out=yo[:], in_=ps[0:64, :]), r=[pk], w=[yok])
                    p.dma("sp", lambda e, yo=yo, d=d, hs=hs, sl=sl: e.dma_start(out=YT[d, hs, sl], in_=yo[:]), r=[yok], w=["YTR"])
                    if STAGE < 7:
                        continue
                    ps, pk = psA.next()
                    mm(ps[0:64, 0:64], pk, Btok[:], Us[:], [Btokk, Usk], start=True, stop=False)
                    mm(ps[0:64, 0:64], pk, Ktok[:], Vtok[:], [Ktokk, Vtokk], start=False, stop=True)
                    t2, t2k = tw.next()
                    p.op("dve", lambda e, t2=t2, Tt=Tt, eW=eW, tend=tend: e.tensor_scalar(out=t2[:], in0=Tt[:], scalar1=eW[:, tend:tend + 1], scalar2=None, op0=ALU.mult),
                         r=[Tk, eWk], w=[t2k])
                    p.op("dve", lambda e, t2=t2, Tt=Tt, eW=eW, ps=ps, tend=tend: e.scalar_tensor_tensor(
                        out=Tt[:], in0=ps[0:64, 0:64], scalar=eW[:, tend:tend + 1], in1=t2[:], op0=ALU.mult, op1=ALU.add), r=[pk, eWk, t2k], w=[Tk])


def phase_rwkv_finish(p, C, io, n_tok):
    SMT, YT, RK, MIXT = io["SMT"], io["YTR"], io["RK"], io["MIXT"]
    with p.phase():
        g2a = p.sb("g2a", [128, 512], F32)
        g2b = p.sb("g2b", [32, 512], F32)
        p.dma("sp", lambda e: e.dma_start(out=g2a[:], in_=io["rw_g2"][0:128, :]), w=["fw"])
        p.dma("sp", lambda e: e.dma_start(out=g2b[:], in_=io["rw_g2"][128:160, :]), w=["fw"])
        lnw = p.sb("lnw", [64, 8], F32)
        lnb = p.sb("lnb", [64, 8], F32)
        colload(p, lnw, "fw", io["rw_ln_w"], 8)
        colload(p, lnb, "fw", io["rw_ln_b"], 8)
        sga = Ring(p, "sga", [128, 512], n=2)
        sgb = Ring(p, "sgb", [32, 512], n=2)
        R = {nm: Ring(p, nm, [64, 512], n=2) for nm in ("fy0", "fy1", "fr0", "fr1", "fv", "fyc", "fsq", "frs", "fbn")}
        ob = Ring(p, "fob", [64, 512], BF16, n=2)
        psF = Ring(p, "psG", [64, 512], n=4, psum=True)
        for t0 in range(0, n_tok, 512):
            N = min(512, n_tok - t0)
            sl = slice(t0, t0 + N)
            a_, ak = sga.next()
            b_, bk = sgb.next()
            p.dma("sp", lambda e, a_=a_, sl=sl, N=N: e.dma_start(out=a_[:, 0:N], in_=SMT[1664:1792, sl]), w=[ak])
            p.dma("sp", lambda e, b_=b_, sl=sl, N=N: e.dma_start(out=b_[:, 0:N], in_=SMT[1792:1824, sl]), w=[bk])
            p.op("act", lambda e, a_=a_, N=N: e.activation(out=a_[:, 0:N], in_=a_[:, 0:N], func=AF.Sigmoid), r=[ak], w=[ak])
            p.op("act", lambda e, b_=b_, N=N: e.activation(out=b_[:, 0:N], in_=b_[:, 0:N], func=AF.Sigmoid), r=[bk], w=[bk])
            for h in range(8):
                hs = slice(h * 64, (h + 1) * 64)
                y0, y0k = R["fy0"].next()
                y1, y1k = R["fy1"].next()
                r0, r0k = R["fr0"].next()
                r1, r1k = R["fr1"].next()
                v, vk = R["fv"].next()
                for (t, k_, src) in ((y0, y0k, YT[0, hs, sl]), (y1, y1k, YT[1, hs, sl]), (r0, r0k, RK[0, hs, sl]), (r1, r1k, RK[1, hs, sl]),
                                     (v, vk, SMT[1024 + h * 64:1024 + (h + 1) * 64, sl])):
                    p.dma("sp", lambda e, t=t, src=src, N=N: e.dma_start(out=t[:, 0:N], in_=src), w=[k_])
                p.op("dve", lambda e, y0=y0, y1=y1, N=N: e.tensor_tensor(out=y0[:, 0:N], in0=y0[:, 0:N], in1=y1[:, 0:N], op=ALU.add), r=[y0k, y1k], w=[y0k])
                p.op("pool", lambda e, r0=r0, r1=r1, N=N: e.tensor_tensor(out=r0[:, 0:N], in0=r0[:, 0:N], in1=r1[:, 0:N], op=ALU.add), r=[r0k, r1k], w=[r0k])
                ps, pk = psF.next()
                p.op("pe", lambda e, ps=ps, y0=y0, N=N: e.matmul(ps[:, 0:N], lhsT=C["ones64m"][0:64, 0:64], rhs=y0[:, 0:N], start=True, stop=True), r=[y0k, "masks"], w=[pk])
                yc, yck = R["fyc"].next()
                p.op("dve", lambda e, yc=yc, y0=y0, ps=ps, N=N: e.tensor_tensor(out=yc[:, 0:N], in0=y0[:, 0:N], in1=ps[:, 0:N], op=ALU.subtract), r=[y0k, pk], w=[yck])
                sq, sqk = R["fsq"].next()
                p.op("act", lambda e, sq=sq, yc=yc, N=N: e.activation(out=sq[:, 0:N], in_=yc[:, 0:N], func=AF.Square), r=[yck], w=[sqk])
                ps, pk = psF.next()
                p.op("pe", lambda e, ps=ps, sq=sq, N=N: e.matmul(ps[:, 0:N], lhsT=C["ones64m"][0:64, 0:64], rhs=sq[:, 0:N], start=True, stop=True), r=[sqk, "masks"], w=[pk])
                rs, rsk = R["frs"].next()
                p.op("dve", lambda e, rs=rs, ps=ps, N=N: e.tensor_scalar(out=rs[:, 0:N], in0=ps[:, 0:N], scalar1=64e-5, scalar2=None, op0=ALU.add), r=[pk], w=[rsk])
                p.op("act", lambda e, rs=rs, N=N: e.activation(out=rs[:, 0:N], in_=rs[:, 0:N], func=AF.Sqrt), r=[rsk], w=[rsk])
                p.op("dve", lambda e, rs=rs, N=N: e.reciprocal(out=rs[:, 0:N], in_=rs[:, 0:N]), r=[rsk], w=[rsk])
                p.op("dve", lambda e, yc=yc, rs=rs, N=N: e.tensor_tensor(out=yc[:, 0:N], in0=yc[:, 0:N], in1=rs[:, 0:N], op=ALU.mult), r=[yck, rsk], w=[yck])
                p.op("dve", lambda e, yc=yc, h=h, N=N: e.tensor_scalar(out=yc[:, 0:N], in0=yc[:, 0:N], scalar1=lnw[:, h:h + 1], scalar2=lnb[:, h:h + 1],
                                                                      op0=ALU.mult, op1=ALU.add), r=[yck, "fw"], w=[yck])
                ps, pk = psF.next()
                p.op("pe", lambda e, ps=ps, r0=r0, N=N: e.matmul(ps[:, 0:N], lhsT=C["ones64"][0:64, 0:64], rhs=r0[:, 0:N], start=True, stop=True), r=[r0k, "masks"], w=[pk])
                bn, bnk = R["fbn"].next()
                p.op("dve", lambda e, bn=bn, ps=ps, v=v, N=N: e.tensor_tensor(out=bn[:, 0:N], in0=ps[:, 0:N], in1=v[:, 0:N], op=ALU.mult), r=[pk, vk], w=[bnk])
                p.op("pool", lambda e, bn=bn, yc=yc, N=N: e.tensor_tensor(out=bn[:, 0:N], in0=bn[:, 0:N], in1=yc[:, 0:N], op=ALU.add), r=[bnk, yck], w=[bnk])
                ps, pk = psF.next()
                p.op("pe", lambda e, ps=ps, a_=a_, hs=hs, N=N: e.matmul(ps[:, 0:N], lhsT=g2a[:, hs], rhs=a_[:, 0:N], start=True, stop=False), r=["fw", ak], w=[pk])
                p.op("pe", lambda e, ps=ps, b_=b_, hs=hs, N=N: e.matmul(ps[:, 0:N], lhsT=g2b[:, hs], rhs=b_[:, 0:N], start=False, stop=True), r=["fw", bk], w=[pk])
                o, ok = ob.next()
                p.op("dve", lambda e, o=o, bn=bn, ps=ps, N=N: e.tensor_tensor(out=o[:, 0:N], in0=ps[:, 0:N], in1=bn[:, 0:N], op=ALU.mult), r=[pk, bnk], w=[ok])
                p.dma("sp", lambda e, o=o, h=h, sl=sl, N=N: e.dma_start(out=MIXT[512 + h * 64:512 + (h + 1) * 64, sl], in_=o[:, 0:N]), r=[ok], w=["MIXT"])


def phase_outproj(p, C, io, l, w_out, n_ctx, n_tok, t_lo=0):
    xres, MIXT, modrow = io["xres"], io["MIXT"], io["modrow"]
    with p.phase():
        wbf = p.sb("woutbf", [128, KC, D], BF16)
        stage = [p.sb(f"wostage{i}", [128, D], F32) for i in range(2)]
        load_cast_weight(p, wbf, "woutbf", w_out, KC, D, stage, "wostage")
        gt = p.sb("gt1row", [128, 2, D], F32)
        for r in range(2):
            p.dma("sp", lambda e, r=r: e.dma_start(out=gt[:, r, :], in_=modrow[l, r, 2048:3072].partition_broadcast(128)), w=["gt1row"])
        mx = Ring(p, "mx", [128, KC, 128], BF16, n=2)
        xt = Ring(p, "oxt", [128, D], n=2)
        tmp = Ring(p, "otmp", [128, D], n=2)
        pso = Ring(p, "pso", [128, 512], n=4, psum=True)
        for ti in range(t_lo // 128, n_tok // 128):
            r = 1 if ti * 128 < n_ctx else 0
            sl = slice(ti * 128, (ti + 1) * 128)
            m, mk = mx.next()
            p.dma("sp", lambda e, m=m, sl=sl: e.dma_start(out=m[:], in_=MIXT[:, sl].rearrange("(c q) t -> q c t", q=128)), w=[mk])
            x, xk = xt.next()
            p.dma("sp", lambda e, x=x, sl=sl: e.dma_start(out=x[:], in_=xres[sl, :]), w=[xk])
            t, tk = tmp.next()
            for n in range(2):
                ps, pk = pso.next()
                for kc in range(KC):
                    p.op("pe", lambda e, ps=ps, m=m, kc=kc, n=n: e.matmul(ps[:], lhsT=m[:, kc, :], rhs=wbf[:, kc, n * 512:(n + 1) * 512],
                                                                          start=(kc == 0), stop=(kc == KC - 1)), r=[mk, "woutbf"], w=[pk])
                p.op("dve", lambda e, t=t, ps=ps, n=n, r=r: e.tensor_tensor(out=t[:, n * 512:(n + 1) * 512], in0=ps[:], in1=gt[:, r, n * 512:(n + 1) * 512], op=ALU.mult),
                     r=[pk, "gt1row"], w=[tk])
            p.op("pool", lambda e, t=t, x=x: e.tensor_tensor(out=t[:], in0=t[:], in1=x[:], op=ALU.add), r=[tk, xk], w=[tk])
            p.dma("sp", lambda e, t=t, sl=sl: e.dma_start(out=xres[sl, :], in_=t[:]), r=[tk], w=["xres"])


def phase_final(p, C, io, n_ctx, n_tok):
    xres, out = io["xres"], io["out"]
    with p.phase():
        g = p.sb("fng", [128, D], F32)
        p.dma("sp", lambda e: e.dma_start(out=g[:], in_=io["final_norm_g"].partition_broadcast(128)), w=["fng"])
        xt = Ring(p, "fxt", [128, D], n=3)
        junk = p.sb("fjunk", [128, D], BF16)
        ss = Ring(p, "fss", [128, 1], n=3)
        rs = Ring(p, "frst", [128, 1], n=3)
        for ti in range(n_ctx // 128, n_tok // 128):
            sl = slice(ti * 128, (ti + 1) * 128)
            x, xk = xt.next()
            s, sk = ss.next()
            r_, rk = rs.next()
            p.dma("sp", lambda e, x=x, sl=sl: e.dma_start(out=x[:], in_=xres[sl, :]), w=[xk])
            p.op("act", lambda e, x=x, s=s: e.activation(out=junk[:], in_=x[:], func=AF.Square, accum_out=s[:, 0:1]), r=[xk], w=["fjunk", sk])
            p.op("dve", lambda e, r_=r_, s=s: e.tensor_scalar(out=r_[:], in0=s[:], scalar1=1.0 / D, scalar2=EPS, op0=ALU.mult, op1=ALU.add), r=[sk], w=[rk])
            p.op("act", lambda e, r_=r_: e.activation(out=r_[:], in_=r_[:], func=AF.Sqrt), r=[rk], w=[rk])
            p.op("dve", lambda e, r_=r_: e.reciprocal(out=r_[:], in_=r_[:]), r=[rk], w=[rk])
            p.op("dve", lambda e, x=x, r_=r_: e.scalar_tensor_tensor(out=x[:], in0=x[:], scalar=r_[:, 0:1], in1=g[:], op0=ALU.mult, op1=ALU.mult),
                 r=[xk, rk, "fng"], w=[xk])
            o0 = ti * 128 - n_ctx
            p.dma("sp", lambda e, x=x, o0=o0: e.dma_start(out=out[o0:o0 + 128, :], in_=x[:]), r=[xk], w=["out"])

RST = 9

DE = 1024
SW_LIMIT = 7.0
SW_ALPHA = 1.702


def phase_moe_cast(p, C, io, l, NE):
    gu, dn, WGU, WDN = io["moe_gu_w"], io["moe_down_w"], io["WGU"], io["WDN"]
    with p.phase():
        sg = Ring(p, "cg32", [128, 2, 2048], n=2)
        sb = Ring(p, "cg16", [128, 2, 2048], BF16, n=2)
        sd = Ring(p, "cd32", [128, 2, 1024], n=2)
        sdb = Ring(p, "cd16", [128, 2, 1024], BF16, n=2)
        engs = ("dve", "pool", "act")
        it = 0
        for e_ in range(NE):
            for k2 in range(4):
                for (src, dst, r32, r16) in ((gu, WGU, sg, sb), (dn, WDN, sd, sdb)):
                    a, ak = r32.next()
                    b, bk = r16.next()
                    rows = slice(k2 * 256, (k2 + 1) * 256)
                    p.dma("sp", lambda e, a=a, src=src, e_=e_, rows=rows: e.dma_start(out=a[:], in_=src[l, e_, rows, :].rearrange("(c q) n -> q c n", q=128)), w=[ak])
                    eng = engs[it % 3]
                    it += 1
                    if eng == "act":
                        p.op("act", lambda e, a=a, b=b: e.copy(out=b[:], in_=a[:]), r=[ak], w=[bk])
                    else:
                        p.op(eng, lambda e, a=a, b=b: e.tensor_copy(out=b[:], in_=a[:]), r=[ak], w=[bk])
                    p.dma("act", lambda e, b=b, dst=dst, e_=e_, rows=rows: e.dma_start(out=dst[e_, rows, :].rearrange("(c q) n -> q c n", q=128), in_=b[:]), r=[bk], w=["WBF"])


def phase_moe_route(p, C, io, G, l, NE, t_lo, t_hi, n_ctx):
    xres, HT, GATE = io["xres"], io["HT"], io["GATE"]
    with p.phase():
        T = norm_scratch(p)
        wr = p.sb("wr", [128, KC, NE], F32)
        p.dma("sp", lambda e: e.dma_start(out=wr[:], in_=io["router_w"][l].rearrange("(c q) n -> q c n", q=128)), w=["wr"])
        br = p.sb("br", [128, NE], F32)
        p.dma("sp", lambda e: e.dma_start(out=br[:], in_=io["router_b"][l].partition_broadcast(128)), w=["wr"])
        xt = Ring(p, "mxt", [128, D], n=2)
        hb = Ring(p, "mhb", [128, KC, 128], BF16, n=2)
        h32 = Ring(p, "mh32", [128, KC, 128], n=2)
        lg = Ring(p, "mlg", [128, NE], n=2)
        ex = Ring(p, "mex", [128, NE], n=2)
        mk = Ring(p, "mmk", [128, NE], n=2)
        m8 = Ring(p, "mm8", [128, 8], n=2)
        sc = Ring(p, "msc", [128, 2], n=2)
        psr = p.ps("psr", [128, NE], F32)
        for ti in range(t_lo // 128, t_hi // 128):
            r = 1 if ti * 128 < n_ctx else 0
            sl = slice(ti * 128, (ti + 1) * 128)
            x, xk = xt.next()
            p.dma("sp", lambda e, x=x, sl=sl: e.dma_start(out=x[:], in_=xres[sl, :]), w=[xk])
            b, bk = hb.next()
            f, fk = h32.next()
            norm_tile(p, C, T, xk, x, G["A2"][:, l, r, :], G["B2"][:, l, r, :], lambda kc, b=b: b[:, kc, :], bk,
                      hT32_dst=lambda kc, f=f: f[:, kc, :])
            fk32 = bk + "32"
            p.dma("sp", lambda e, b=b, sl=sl: e.dma_start(out=HT[:, sl].rearrange("(c q) t -> q c t", q=128), in_=b[:]), r=[bk], w=["HT"])
            if RST < 2:
                continue
            for kc in range(KC):
                p.op("pe", lambda e, f=f, kc=kc: e.matmul(psr[:], lhsT=f[:, kc, :], rhs=wr[:, kc, :], start=(kc == 0), stop=(kc == KC - 1)),
                     r=[fk32, "wr"], w=["psr"])
            g, gk = lg.next()
            p.op("dve", lambda e, g=g: e.tensor_tensor(out=g[:], in0=psr[:], in1=br[:], op=ALU.add), r=["psr", "wr"], w=[gk])
            if RST < 3:
                continue
            m, mk8 = m8.next()
            p.op("dve", lambda e, m=m, g=g: e.max(out=m[:], in_=g[:]), r=[gk], w=[mk8])
            msk, mskk = mk.next()
            p.op("dve", lambda e, msk=msk, g=g, m=m: e.tensor_scalar(out=msk[:], in0=g[:], scalar1=m[:, 3:4], scalar2=None, op0=ALU.is_ge), r=[gk, mk8], w=[mskk])
            if RST < 4:
                continue
            s, sk = sc.next()
            p.op("dve", lambda e, s=s, m=m: e.tensor_scalar(out=s[:, 0:1], in0=m[:, 0:1], scalar1=-1.0, scalar2=None, op0=ALU.mult), r=[mk8], w=[sk])
            x_, xk_ = ex.next()
            p.op("act", lambda e, x_=x_, g=g, s=s: e.activation(out=x_[:], in_=g[:], func=AF.Exp, bias=s[:, 0:1]), r=[gk, sk], w=[xk_])
            p.op("dve", lambda e, x_=x_, msk=msk: e.tensor_tensor(out=x_[:], in0=x_[:], in1=msk[:], op=ALU.mult), r=[xk_, mskk], w=[xk_])
            p.op("dve", lambda e, s=s, x_=x_: e.reduce_sum(out=s[:, 1:2], in_=x_[:], axis=AX.X), r=[xk_], w=[sk])
            p.op("dve", lambda e, s=s: e.reciprocal(out=s[:, 1:2], in_=s[:, 1:2]), r=[sk], w=[sk])
            p.op("dve", lambda e, x_=x_, s=s: e.tensor_scalar(out=x_[:], in0=x_[:], scalar1=s[:, 1:2], scalar2=None, op0=ALU.mult), r=[xk_, sk], w=[xk_])
            p.dma("sp", lambda e, x_=x_, sl=sl: e.dma_start(out=GATE[sl, :], in_=x_[:]), r=[xk_], w=["GATE"])


def phase_moe_experts(p, C, io, l, NE, t_lo, t_hi, n_ctx, TB=512):
    xres, HT, GATE, WGU, WDN, modrow = io["xres"], io["HT"], io["GATE"], io["WGU"], io["WDN"], io["modrow"]
    with p.phase():
        bg_rows = p.sb("bg_rows", [NE, 2048], F32)
        p.dma("sp", lambda e: e.dma_start(out=bg_rows[:], in_=io["moe_gu_b"][l]), w=["bg_rows"])
        bgc = p.sb("bgc", [128, 16, NE], F32)
        pst = p.ps("pst", [128, 16, NE], F32)
        for c in range(16):
            p.op("pe", lambda e, c=c: e.transpose(pst[:, c, :], bg_rows[:, c * 128:(c + 1) * 128], C["ident"][0:NE, 0:NE]), r=["bg_rows", "ident"], w=["pst"])
        p.op("dve", lambda e: e.tensor_copy(out=bgc[:], in_=pst[:]), r=["pst"], w=["bgc"])
        ones_b = p.sb("ones_b", [1, 128], BF16)
        p.op("dve", lambda e: e.memset(ones_b[:], 1.0), w=["ones_b"])
        gt = p.sb("gt2row", [128, 2, D], F32)
        for r in range(2):
            p.dma("sp", lambda e, r=r: e.dma_start(out=gt[:, r, :], in_=modrow[l, r, 5120:6144].partition_broadcast(128)), w=["gt2row"])
        wg = Ring(p, "wg", [128, KC, 2048], BF16, n=2)
        wd = Ring(p, "wd", [128, KC, 1024], BF16, n=2)
        bd32 = Ring(p, "bd32", [1, 1024], F32, n=2)
        bd16 = Ring(p, "bd16", [1, 1024], BF16, n=2)
        acc = p.sb("macc", [128, TB // 128, D], F32)
        hT = p.sb("mhT", [128, KC, TB], BF16)
        gate = p.sb("mgate", [128, TB // 128, NE], F32)
        actT = Ring(p, "actT", [128, KC, 512], BF16, n=2)
        t1 = Ring(p, "et1", [128, 512], n=2)
        sgm = Ring(p, "esg", [128, 512], n=2)
        t2 = Ring(p, "et2", [128, 512], n=2)
        xt = Ring(p, "ext", [128, D], n=2)
        psg = Ring(p, "psg", [128, 512], n=4, psum=True)
        psd = Ring(p, "psd", [128, 512], n=3, psum=True)
        t = t_lo
        while t < t_hi:
            nb = min(TB, t_hi - t)
            ntile = nb // 128
            p.dma("sp", lambda e, t=t, nb=nb: e.dma_start(out=hT[:, :, 0:nb], in_=HT[:, t:t + nb].rearrange("(c q) n -> q c n", q=128)), w=["mhT"])
            p.dma("sp", lambda e, t=t, nb=nb, ntile=ntile: e.dma_start(out=gate[:, 0:ntile, :], in_=GATE[t:t + nb, :].rearrange("(j q) n -> q j n", q=128)), w=["mgate"])
            p.op("pool", lambda e: e.memset(acc[:], 0.0), w=["macc"])
            for e_ in range(NE):
                g_, gk = wg.next()
                d_, dk = wd.next()
                b32, b32k = bd32.next()
                b16, b16k = bd16.next()
                p.dma("sp", lambda e, g_=g_, e_=e_: e.dma_start(out=g_[:], in_=WGU[e_].rearrange("(c q) n -> q c n", q=128)), w=[gk])
                p.dma("act", lambda e, d_=d_, e_=e_: e.dma_start(out=d_[:], in_=WDN[e_].rearrange("(c q) n -> q c n", q=128)), w=[dk])
                p.dma("sp", lambda e, b32=b32, e_=e_: e.dma_start(out=b32[:], in_=io["moe_down_b"][l, e_:e_ + 1, :]), w=[b32k])
                p.op("pool", lambda e, b32=b32, b16=b16: e.tensor_copy(out=b16[:], in_=b32[:]), r=[b32k], w=[b16k])
                for s0 in range(0, nb, 512):
                    N = min(512, nb - s0)
                    aT, aTk = actT.next()
                    for c in range(8):
                        pa, pak = psg.next()
                        pb, pbk = psg.next()
                        for (ps_, pk_, col0) in ((pa, pak, c * 128), (pb, pbk, DE + c * 128)):
                            for kc in range(KC):
                                p.op("pe", lambda e, ps_=ps_, g_=g_, kc=kc, col0=col0, s0=s0, N=N: e.matmul(
                                    ps_[:, 0:N], lhsT=g_[:, kc, col0:col0 + 128], rhs=hT[:, kc, s0:s0 + N], start=(kc == 0), stop=(kc == KC - 1)),
                                    r=[gk, "mhT"], w=[pk_])
                        a1, a1k = t1.next()
                        p.op("dve", lambda e, a1=a1, pa=pa, c=c, e_=e_, N=N: e.tensor_scalar(out=a1[:, 0:N], in0=pa[:, 0:N], scalar1=bgc[:, c, e_:e_ + 1], scalar2=SW_LIMIT,
                                                                                          op0=ALU.add, op1=ALU.min), r=[pak, "bgc"], w=[a1k])
                        s_, sk_ = sgm.next()
                        p.op("act", lambda e, s_=s_, a1=a1, N=N: e.activation(out=s_[:, 0:N], in_=a1[:, 0:N], func=AF.Sigmoid, scale=SW_ALPHA), r=[a1k], w=[sk_])
                        a2, a2k = t2.next()
                        p.op("dve", lambda e, a2=a2, pb=pb, c=c, e_=e_, N=N: e.tensor_scalar(out=a2[:, 0:N], in0=pb[:, 0:N], scalar1=bgc[:, 8 + c, e_:e_ + 1], scalar2=SW_LIMIT,
                                                                                          op0=ALU.add, op1=ALU.min), r=[pbk, "bgc"], w=[a2k])
                        p.op("pool", lambda e, a2=a2, N=N: e.tensor_scalar(out=a2[:, 0:N], in0=a2[:, 0:N], scalar1=-SW_LIMIT, scalar2=1.0, op0=ALU.max, op1=ALU.add),
                             r=[a2k], w=[a2k])
                        p.op("pool", lambda e, a1=a1, s_=s_, N=N: e.tensor_tensor(out=a1[:, 0:N], in0=a1[:, 0:N], in1=s_[:, 0:N], op=ALU.mult), r=[a1k, sk_], w=[a1k])
                        p.op("dve", lambda e, aT=aT, a1=a1, a2=a2, c=c, N=N: e.tensor_tensor(out=aT[:, c, 0:N], in0=a1[:, 0:N], in1=a2[:, 0:N], op=ALU.mult),
                             r=[a1k, a2k], w=[aTk])
                    for j in range(N // 128):
                        tile_i = (s0 // 128) + j
                        for n in range(2):
                            pd, pdk = psd.next()
                            for kc in range(KC):
                                p.op("pe", lambda e, pd=pd, aT=aT, d_=d_, kc=kc, j=j, n=n: e.matmul(
                                    pd[:], lhsT=aT[:, kc, j * 128:(j + 1) * 128], rhs=d_[:, kc, n * 512:(n + 1) * 512], start=(kc == 0), stop=False),
                                    r=[aTk, dk], w=[pdk])
                            p.op("pe", lambda e, pd=pd, b16=b16, n=n: e.matmul(pd[:], lhsT=ones_b[:], rhs=b16[:, n * 512:(n + 1) * 512], start=False, stop=True),
                                 r=["ones_b", b16k], w=[pdk])
                            p.op("dve", lambda e, pd=pd, tile_i=tile_i, n=n, e_=e_: e.scalar_tensor_tensor(
                                out=acc[:, tile_i, n * 512:(n + 1) * 512], in0=pd[:], scalar=gate[:, tile_i, e_:e_ + 1],
                                in1=acc[:, tile_i, n * 512:(n + 1) * 512], op0=ALU.mult, op1=ALU.add), r=[pdk, "mgate", "macc"], w=["macc"])
            for j in range(ntile):
                tok = t + j * 128
                r = 1 if tok < n_ctx else 0
                x, xk = xt.next()
                p.dma("sp", lambda e, x=x, tok=tok: e.dma_start(out=x[:], in_=xres[tok:tok + 128, :]), w=[xk])
                p.op("dve", lambda e, j=j, r=r: e.tensor_tensor(out=acc[:, j, :], in0=acc[:, j, :], in1=gt[:, r, :], op=ALU.mult), r=["macc", "gt2row"], w=["macc"])
                p.op("pool", lambda e, x=x, j=j: e.tensor_tensor(out=x[:], in0=x[:], in1=acc[:, j, :], op=ALU.add), r=[xk, "macc"], w=[xk])
                p.dma("sp", lambda e, x=x, tok=tok: e.dma_start(out=xres[tok:tok + 128, :], in_=x[:]), r=[xk], w=["xres"])
            t += nb

import math

MLA_SCALE = 192 ** -0.5


def make_rope_consts(p, C):
    f, q = C["iota_f"], C["iota_p"]
    d = p.sb("rp_d", [64, 64], F32)
    e1 = p.sb("rp_e1", [64, 64], F32)
    e2 = p.sb("rp_e2", [64, 64], F32)
    ge = p.sb("rp_ge", [64, 64], F32)
    PR = p.sb("rp_PR", [64, 64], F32)
    k = ["rope_c"]
    p.op("dve", lambda e: e.tensor_scalar(out=d[:], in0=f[0:64, 0:64], scalar1=q[0:64, 0:1], scalar2=None, op0=ALU.subtract), r=["iota_f", "iota_p"], w=k)
    p.op("dve", lambda e: e.tensor_scalar(out=e1[:], in0=d[:], scalar1=16.0, scalar2=None, op0=ALU.is_equal), r=k, w=k)
    p.op("dve", lambda e: e.tensor_scalar(out=e2[:], in0=d[:], scalar1=-16.0, scalar2=None, op0=ALU.is_equal), r=k, w=k)
    ii = p.sb("rp_ii", [64, 64], I32)
    p.op("pool", lambda e: e.iota(ii[:], pattern=[[1, 64]], base=0, channel_multiplier=0), w=["rp_ii"])
    p.op("dve", lambda e: e.tensor_single_scalar(out=ii[:], in_=ii[:], scalar=16, op=ALU.bitwise_and), r=["rp_ii"], w=["rp_ii"])
    p.op("dve", lambda e: e.tensor_copy(out=ge[:], in_=ii[:]), r=["rp_ii"], w=k)
    p.op("dve", lambda e: e.tensor_scalar(out=ge[:], in0=ge[:], scalar1=1.0 / 16, scalar2=None, op0=ALU.mult), r=k, w=k)
    p.op("dve", lambda e: e.tensor_tensor(out=e1[:], in0=e1[:], in1=ge[:], op=ALU.mult), r=k, w=k)
    p.op("dve", lambda e: e.tensor_scalar(out=ge[:], in0=ge[:], scalar1=-1.0, scalar2=1.0, op0=ALU.mult, op1=ALU.add), r=k, w=k)
    p.op("dve", lambda e: e.tensor_tensor(out=e2[:], in0=e2[:], in1=ge[:], op=ALU.mult), r=k, w=k)
    p.op("dve", lambda e: e.tensor_tensor(out=PR[:], in0=e1[:], in1=e2[:], op=ALU.subtract), r=k, w=k)
    invf = p.sb("rp_invf", [64, 1], F32)
    isrow = p.sb("rp_isrow", [64, 1], F32)
    pi_ = p.sb("rp_pi", [64, 1], I32)
    p.op("pool", lambda e: e.iota(pi_[:], pattern=[[1, 1]], base=0, channel_multiplier=1), w=["rp_pi"])
    p.op("dve", lambda e: e.tensor_single_scalar(out=pi_[:], in_=pi_[:], scalar=15, op=ALU.bitwise_and), r=["rp_pi"], w=["rp_pi"])
    p.op("dve", lambda e: e.tensor_copy(out=invf[:], in_=pi_[:]), r=["rp_pi"], w=k)
    p.op("act", lambda e: e.activation(out=invf[:], in_=invf[:], func=AF.Exp, scale=-math.log(10000.0) / 16.0), r=k, w=k)
    p.op("dve", lambda e: e.tensor_scalar(out=isrow[:], in0=q[0:64, 0:1], scalar1=32.0, scalar2=None, op0=ALU.is_lt), r=["iota_p"], w=k)
    C.update(PR=PR, invf=invf, isrow=isrow)


def phase_mla_proj(p, C, io, G, l, n_ctx, n_tok):
    xres = io["xres"]
    QN, QR, KN, KR, V = io["QN"], io["QR"], io["KN"], io["KRo"], io["Vt"]
    n_lat = n_tok - n_ctx
    with p.phase():
        T = norm_scratch(p)
        win = p.sb("mwin", [128, KC, 448], BF16)
        st = [p.sb(f"mst{i}", [128, 2048], F32) for i in range(2)]
        load_cast_weight(p, win, "mwin", io["od_w_in"], KC, 448, st, "mst")
        wq = p.sb("mwq", [128, 2, 1536], BF16)
        load_cast_weight(p, wq, "mwq", io["mla_wq_up"], 2, 1536, st, "mst")
        wkn = p.sb("mwkn", [128, 8, 128], BF16)
        wv = p.sb("mwv", [128, 8, 128], BF16)
        p.dma("sp", lambda e: e.dma_start(out=st[0][:, 0:2048], in_=io["mla_wkv_up"][:, :]), w=["mst0"])
        p.op("dve", lambda e: e.tensor_copy(out=wkn[:], in_=st[0][:, 0:2048].rearrange("q (h c) -> q h c", c=256)[:, :, 0:128]), r=["mst0"], w=["mwk"])
        p.op("pool", lambda e: e.tensor_copy(out=wv[:], in_=st[0][:, 0:2048].rearrange("q (h c) -> q h c", c=256)[:, :, 128:256]), r=["mst0"], w=["mwk"])
        qg = p.sb("mqg", [128, 2], F32)
        kg = p.sb("mkg", [128, 1], F32)
        p.dma("sp", lambda e: e.dma_start(out=qg[:], in_=io["mla_q_norm"].rearrange("(c q) -> q c", q=128), allow_slow_non_contiguous=True), w=["mg"])
        p.dma("sp", lambda e: e.dma_start(out=kg[:], in_=io["mla_kv_norm"].rearrange("(q o) -> q o", o=1)), w=["mg"])
        onesq = p.sb("monesq", [128, 128], F32)
        p.op("dve", lambda e: e.memset(onesq[:], 1.0 / 256), w=["monesq"])
        xts = Ring(p, "axt", [128, D], n=2)
        hT = p.sb("ahT", [128, KC, 512], BF16)
        cq = p.sb("acq", [128, 2, 512], F32)
        ckv = p.sb("ackv", [128, 512], F32)
        krr = p.sb("akrr", [64, 512], F32)
        sq = p.sb("asq", [128, 2, 512], F32)
        rs = Ring(p, "ars", [128, 512], n=2)
        cqn = p.sb("acqn", [128, 2, 512], BF16)
        ckvn = p.sb("ackvn", [128, 512], BF16)
        ang = p.sb("aang", [64, 512], F32)
        tti = p.sb("atti", [64, 512], I32)
        tt2 = p.sb("att2", [64, 512], I32)
        a2 = p.sb("aa2", [64, 512], F32)
        kf = p.sb("akf", [64, 512], F32)
        cosT = p.sb("acos", [64, 512], F32)
        sinT = p.sb("asin", [64, 512], F32)
        rx = Ring(p, "arx", [64, 512], n=2)
        ru = Ring(p, "aru", [64, 512], n=2)
        ob = Ring(p, "aob", [128, 512], BF16, n=3)
        vb = Ring(p, "avb", [128, 1024], BF16, n=2)
        pp = Ring(p, "app", [128, 512], n=5, psum=True)

        def rope(x, xk, N, scale, dst_ap, dst_key):
            p2, p2k = pp.next()
            p.op("pe", lambda e: e.matmul(p2[0:64, 0:N], lhsT=C["PR"][:], rhs=x, start=True, stop=True), r=["rope_c", xk], w=[p2k])
            u, uk = ru.next()
            p.op("dve", lambda e: e.tensor_tensor(out=u[:, 0:N], in0=p2[0:64, 0:N], in1=sinT[:, 0:N], op=ALU.mult), r=[p2k, "trig"], w=[uk])
            p.op("pool", lambda e: e.tensor_tensor(out=x, in0=x, in1=cosT[:, 0:N], op=ALU.mult), r=[xk, "trig"], w=[xk])
            if scale == 1.0:
                p.op("dve", lambda e: e.tensor_tensor(out=dst_ap, in0=x, in1=u[:, 0:N], op=ALU.add), r=[xk, uk], w=[dst_key])
            else:
                p.op("dve", lambda e: e.tensor_tensor(out=u[:, 0:N], in0=x, in1=u[:, 0:N], op=ALU.add), r=[xk, uk], w=[uk])
                p.op("dve", lambda e: e.tensor_scalar(out=dst_ap, in0=u[:, 0:N], scalar1=scale, scalar2=None, op0=ALU.mult), r=[uk], w=[dst_key])

        blocks = [(0, n_ctx, False)] + [(t, min(512, n_tok - t), True) for t in range(n_ctx, n_tok, 512)]
        def do_block(t0, N, is_lat):
            for j in range(N // 128):
                ti = t0 // 128 + j
                r = 0 if is_lat else 1
                x, xk = xts.next()
                p.dma("sp", lambda e, x=x, ti=ti: e.dma_start(out=x[:], in_=xres[ti * 128:(ti + 1) * 128, :]), w=[xk])
                norm_tile(p, C, T, xk, x, G["A1"][:, l, r, :], G["B1"][:, l, r, :], lambda kc, j=j: hT[:, kc, j * 128:(j + 1) * 128], "ahT")
            for (c0, cw, dst, dk) in ((0, 128, cq[:, 0, :], "acq"), (128, 128, cq[:, 1, :], "acq"), (256, 128, ckv[:, :], "ackv"), (384, 64, krr[:, :], "akrr")):
                ps, pk = pp.next()
                for kc in range(KC):
                    p.op("pe", lambda e, ps=ps, kc=kc, c0=c0, cw=cw, N=N: e.matmul(ps[0:cw, 0:N], lhsT=win[:, kc, c0:c0 + cw], rhs=hT[:, kc, 0:N],
                                                                                 start=(kc == 0), stop=(kc == KC - 1)), r=["mwin", "ahT"], w=[pk])
                p.op("act", lambda e, ps=ps, dst=dst, cw=cw, N=N: e.copy(out=dst[0:cw, 0:N], in_=ps[0:cw, 0:N]), r=[pk], w=[dk])

            def rmsn(src_chunks, src_key, ones_ap, gcols, dst_chunks, dst_key):
                n = len(src_chunks)
                for c in range(n):
                    p.op("act", lambda e, c=c: e.activation(out=sq[:, c, 0:N], in_=src_chunks[c], func=AF.Square), r=[src_key], w=["asq"])
                ps, pk = pp.next()
                for c in range(n):
                    p.op("pe", lambda e, ps=ps, c=c: e.matmul(ps[:, 0:N], lhsT=ones_ap, rhs=sq[:, c, 0:N], start=(c == 0), stop=(c == n - 1)),
                         r=["asq", "monesq", "masks"], w=[pk])
                r_, rk = rs.next()
                p.op("dve", lambda e: e.tensor_scalar(out=r_[:, 0:N], in0=ps[:, 0:N], scalar1=EPS, scalar2=None, op0=ALU.add), r=[pk], w=[rk])
                p.op("act", lambda e: e.activation(out=r_[:, 0:N], in_=r_[:, 0:N], func=AF.Sqrt), r=[rk], w=[rk])
                p.op("dve", lambda e: e.reciprocal(out=r_[:, 0:N], in_=r_[:, 0:N]), r=[rk], w=[rk])
                for c in range(n):
                    p.op("dve", lambda e, c=c: e.scalar_tensor_tensor(out=dst_chunks[c], in0=src_chunks[c], scalar=gcols[c], in1=r_[:, 0:N],
                                                                      op0=ALU.mult, op1=ALU.mult), r=[src_key, rk, "mg"], w=[dst_key])

            rmsn([ckv[:, 0:N]], "ackv", C["ones128"][:], [kg[:, 0:1]], [ckvn[:, 0:N]], "ackvn")
            if is_lat:
                rmsn([cq[:, 0, 0:N], cq[:, 1, 0:N]], "acq", onesq[:], [qg[:, 0:1], qg[:, 1:2]], [cqn[:, 0, 0:N], cqn[:, 1, 0:N]], "acqn")
                lat0 = t0 - n_ctx
                p.op("pool", lambda e, lat0=lat0: e.iota(tti[:, 0:N], pattern=[[1, N]], base=lat0, channel_multiplier=0), w=["atti"])
                p.op("dve", lambda e: e.tensor_single_scalar(out=tt2[:, 0:N], in_=tti[:, 0:N], scalar=63, op=ALU.bitwise_and), r=["atti"], w=["att2"])
                p.op("dve", lambda e: e.tensor_copy(out=cosT[:, 0:N], in_=tt2[:, 0:N]), r=["att2"], w=["trig"])
                p.op("dve", lambda e: e.tensor_single_scalar(out=tt2[:, 0:N], in_=tti[:, 0:N], scalar=6, op=ALU.arith_shift_right), r=["atti", "trig"], w=["att2"])
                p.op("dve", lambda e: e.tensor_copy(out=sinT[:, 0:N], in_=tt2[:, 0:N]), r=["att2"], w=["trig"])
                p.op("dve", lambda e: e.tensor_tensor(out=sinT[:, 0:N], in0=sinT[:, 0:N], in1=cosT[:, 0:N], op=ALU.subtract), r=["trig"], w=["trig"])
                p.op("dve", lambda e: e.scalar_tensor_tensor(out=ang[:, 0:N], in0=sinT[:, 0:N], scalar=C["isrow"][:, 0:1], in1=cosT[:, 0:N], op0=ALU.mult, op1=ALU.add),
                     r=["trig", "rope_c"], w=["aang"])
                p.op("dve", lambda e: e.tensor_scalar(out=ang[:, 0:N], in0=ang[:, 0:N], scalar1=C["invf"][:, 0:1], scalar2=None, op0=ALU.mult), r=["aang", "rope_c"], w=["aang"])
                for (dst, off) in ((sinT, 0.0), (cosT, 0.5 * math.pi)):
                    p.op("dve", lambda e, off=off: e.tensor_scalar(out=a2[:, 0:N], in0=ang[:, 0:N], scalar1=off, scalar2=None, op0=ALU.add), r=["aang"], w=["aa2"])
                    p.op("dve", lambda e: e.tensor_scalar(out=kf[:, 0:N], in0=a2[:, 0:N], scalar1=1.0 / (2 * math.pi), scalar2=None, op0=ALU.mult), r=["aa2"], w=["akf"])
                    p.op("dve", lambda e: e.tensor_copy(out=tt2[:, 0:N], in_=kf[:, 0:N]), r=["akf"], w=["att2"])
                    p.op("dve", lambda e: e.tensor_copy(out=kf[:, 0:N], in_=tt2[:, 0:N]), r=["att2"], w=["akf"])
                    p.op("dve", lambda e, dst=dst: e.scalar_tensor_tensor(out=dst[:, 0:N], in0=kf[:, 0:N], scalar=-2 * math.pi, in1=a2[:, 0:N], op0=ALU.mult, op1=ALU.add),
                         r=["akf", "aa2"], w=["trig"])
                    p.op("dve", lambda e, dst=dst: e.tensor_scalar(out=dst[:, 0:N], in0=dst[:, 0:N], scalar1=-math.pi, scalar2=math.pi, op0=ALU.max, op1=ALU.min), r=["trig"], w=["trig"])
                    p.op("act", lambda e, dst=dst: e.activation(out=dst[:, 0:N], in_=dst[:, 0:N], func=AF.Sin), r=["trig"], w=["trig"])
                lsl = slice(lat0, lat0 + N)
                for h in range(8):
                    ps, pk = pp.next()
                    for kc in range(2):
                        p.op("pe", lambda e, ps=ps, kc=kc, h=h: e.matmul(ps[:, 0:N], lhsT=wq[:, kc, h * 192:h * 192 + 128], rhs=cqn[:, kc, 0:N], start=(kc == 0), stop=(kc == 1)),
                             r=["mwq", "acqn"], w=[pk])
                    o, ok = ob.next()
                    p.op("act", lambda e, o=o, ps=ps: e.activation(out=o[:, 0:N], in_=ps[:, 0:N], func=AF.Copy, scale=MLA_SCALE), r=[pk], w=[ok])
                    p.dma("sp", lambda e, o=o, h=h, lsl=lsl: e.dma_start(out=QN[h, :, lsl], in_=o[:, 0:N]), r=[ok], w=["QN"])
                    ps, pk = pp.next()
                    for kc in range(2):
                        p.op("pe", lambda e, ps=ps, kc=kc, h=h: e.matmul(ps[0:64, 0:N], lhsT=wq[:, kc, h * 192 + 128:h * 192 + 192], rhs=cqn[:, kc, 0:N], start=(kc == 0), stop=(kc == 1)),
                             r=["mwq", "acqn"], w=[pk])
                    o, ok = ob.next()
                    x_, xk_ = rx.next()
                    p.op("act", lambda e, x_=x_, ps=ps: e.copy(out=x_[:, 0:N], in_=ps[0:64, 0:N]), r=[pk], w=[xk_])
                    rope(x_[:, 0:N], xk_, N, MLA_SCALE, o[0:64, 0:N], ok)
                    p.dma("sp", lambda e, o=o, h=h, lsl=lsl: e.dma_start(out=QR[h, :, lsl], in_=o[0:64, 0:N]), r=[ok], w=["QR"])
            for h in range(8):
                ps, pk = pp.next()
                p.op("pe", lambda e, ps=ps, h=h: e.matmul(ps[:, 0:N], lhsT=wkn[:, h, :], rhs=ckvn[:, 0:N], start=True, stop=True), r=["mwk", "ackvn"], w=[pk])
                o, ok = ob.next()
                p.op("act", lambda e, o=o, ps=ps: e.copy(out=o[:, 0:N], in_=ps[:, 0:N]), r=[pk], w=[ok])
                p.dma("sp", lambda e, o=o, h=h, t0=t0, N=N: e.dma_start(out=KN[h, :, t0:t0 + N], in_=o[:, 0:N]), r=[ok], w=["KN"])
            for j in range(N // 128):
                vt, vk = vb.next()
                for g in range(2):
                    ps, pk = pp.next()
                    p.op("pe", lambda e, ps=ps, j=j, g=g: e.matmul(ps[:, :], lhsT=ckvn[:, j * 128:(j + 1) * 128], rhs=wv[:, 4 * g:4 * g + 4, :], start=True, stop=True),
                         r=["mwk", "ackvn"], w=[pk])
                    p.op("dve", lambda e, vt=vt, ps=ps, g=g: e.tensor_copy(out=vt[:, g * 512:(g + 1) * 512], in_=ps[:, :]), r=[pk], w=[vk])
                tok = t0 + j * 128
                p.dma("sp", lambda e, vt=vt, tok=tok: e.dma_start(out=V[tok:tok + 128, :], in_=vt[:]), r=[vk], w=["Vt"])
            o, ok = ob.next()
            if is_lat:
                rope(krr[:, 0:N], "akrr", N, 1.0, o[0:64, 0:N], ok)
            else:
                p.op("dve", lambda e, o=o: e.tensor_copy(out=o[0:64, 0:N], in_=krr[:, 0:N]), r=["akrr"], w=[ok])
            p.dma("sp", lambda e, o=o, t0=t0, N=N: e.dma_start(out=KR[:, t0:t0 + N], in_=o[0:64, 0:N]), r=[ok], w=["KRo"])


        for (t0_, N_, lat_) in blocks:
            do_block(t0_, N_, lat_)

def phase_mla_attn(p, C, io, n_ctx, n_tok):
    QN, QR, KN, KR, V, MIXT = io["QN"], io["QR"], io["KN"], io["KRo"], io["Vt"], io["MIXT"]
    n_lat = n_tok - n_ctx
    nkt = n_tok // 128
    with p.phase():
        kr = p.sb("bkr", [64, n_tok], BF16)
        p.dma("sp", lambda e: e.dma_start(out=kr[:], in_=KR[:, :]), w=["bkr"])
        ones_bf = p.sb("bones", [128, 128], BF16)
        p.op("dve", lambda e: e.memset(ones_bf[:], 1.0), w=["bones"])
        kn = Ring(p, "bkn", [128, n_tok], BF16, n=2)
        vv = Ring(p, "bvv", [128, nkt, 128], BF16, n=2)
        qn = Ring(p, "bqn", [128, 512], BF16, n=2)
        qr = Ring(p, "bqr", [64, 512], BF16, n=2)
        pt = Ring(p, "bpt", [128, 512], BF16, n=3)
        rd = Ring(p, "brd", [128, 512], n=2)
        ob = Ring(p, "bob", [128, 512], BF16, n=2)
        psS = Ring(p, "bpsS", [128, 512], n=3, psum=True)
        psO = Ring(p, "bpsO", [128, 512], n=2, psum=True)
        psD = Ring(p, "bpsD", [128, 512], n=2, psum=True)
        for h in range(8):
            k_, kk = kn.next()
            v_, vk = vv.next()
            p.dma("sp", lambda e, k_=k_, h=h: e.dma_start(out=k_[:], in_=KN[h, :, :]), w=[kk])
            for j0 in range(0, nkt, 12):
                j1 = min(nkt, j0 + 12)
                p.dma("act", lambda e, v_=v_, h=h, j0=j0, j1=j1: e.dma_start(
                    out=v_[:, j0:j1, :], in_=V[j0 * 128:j1 * 128, h * 128:(h + 1) * 128].rearrange("(j q) c -> q j c", q=128)), w=[vk])
            for qb in range(n_lat // 512):
                qsl = slice(qb * 512, (qb + 1) * 512)
                a, ak = qn.next()
                b, bk = qr.next()
                p.dma("sp", lambda e, a=a, h=h, qsl=qsl: e.dma_start(out=a[:], in_=QN[h, :, qsl]), w=[ak])
                p.dma("sp", lambda e, b=b, h=h, qsl=qsl: e.dma_start(out=b[:], in_=QR[h, :, qsl]), w=[bk])
                po, pok = psO.next()
                pd, pdk = psD.next()
                for kt in range(nkt):
                    ks = slice(kt * 128, (kt + 1) * 128)
                    ps, psk = psS.next()
                    p.op("pe", lambda e, ps=ps, k_=k_, ks=ks, a=a: e.matmul(ps[:], lhsT=k_[:, ks], rhs=a[:], start=True, stop=False), r=[kk, ak], w=[psk])
                    p.op("pe", lambda e, ps=ps, ks=ks, b=b: e.matmul(ps[:], lhsT=kr[:, ks], rhs=b[:], start=False, stop=True), r=["bkr", bk], w=[psk])
                    t, tk = pt.next()
                    p.op("act", lambda e, t=t, ps=ps: e.activation(out=t[:], in_=ps[:], func=AF.Exp), r=[psk], w=[tk])
                    p.op("pe", lambda e, po=po, v_=v_, kt=kt, t=t: e.matmul(po[:], lhsT=v_[:, kt, :], rhs=t[:], start=(kt == 0), stop=(kt == nkt - 1)), r=[vk, tk], w=[pok])
                    p.op("pe", lambda e, pd=pd, t=t, kt=kt: e.matmul(pd[:], lhsT=ones_bf[:], rhs=t[:], start=(kt == 0), stop=(kt == nkt - 1)), r=["bones", tk], w=[pdk])
                r_, rk = rd.next()
                p.op("dve", lambda e, r_=r_, pd=pd: e.reciprocal(out=r_[:], in_=pd[:]), r=[pdk], w=[rk])
                o, ok = ob.next()
                p.op("dve", lambda e, o=o, po=po, r_=r_: e.tensor_tensor(out=o[:], in0=po[:], in1=r_[:], op=ALU.mult), r=[pok, rk], w=[ok])
                p.dma("sp", lambda e, o=o, h=h, qb=qb: e.dma_start(out=MIXT[h * 128:(h + 1) * 128, n_ctx + qb * 512:n_ctx + (qb + 1) * 512], in_=o[:]), r=[ok], w=["MIXT"])


from concourse.bass_utils import run_bass_kernel_spmd

N_CTX = 256
N_LAT = 8192
DEPTH = 2
NEXP = 32
_W_NAMES = ["ada_w", "ada_b", "norm1_g", "norm2_g", "ev_w_in", "ev_w_out", "gla_gk_up", "gla_gk_b", "gla_norm_g",
            "rw_mu", "rw_w0", "rw_w2", "rw_a0", "rw_a2", "rw_k_k", "rw_k_a", "rw_r_k", "rw_g2", "rw_ln_w", "rw_ln_b",
            "od_w_in", "mla_q_norm", "mla_wq_up", "mla_kv_norm", "mla_wkv_up", "od_w_out",
            "router_w", "router_b", "moe_gu_w", "moe_gu_b", "moe_down_w", "moe_down_b", "final_norm_g"]


def build(shapes, n_ctx=N_CTX, n_lat=N_LAT, NE=NEXP):
    n_tok = n_ctx + n_lat
    L = DEPTH
    nc = bass.Bass("TRN2", target_bir_lowering=False)
    io = {}
    for k, shp in shapes.items():
        io[k] = dram(nc, k, list(shp), kind="ExternalInput")
    for name, shp, dt in (("xres", [n_tok, 1024], F32), ("modrow", [L, 2, 6144], F32), ("PT", [3376, n_tok], F32),
                          ("PTOK", [n_tok, 1536], F32), ("SMT", [1824, n_tok], F32), ("YTG", [2, 512, n_tok], F32),
                          ("YTR", [2, 512, n_tok], F32), ("RK", [2, 512, n_tok], F32), ("MIXT", [1024, n_tok], BF16),
                          ("HT", [1024, n_tok], BF16), ("GATE", [n_tok, NE], F32), ("WGU", [NE, 1024, 2048], BF16),
                          ("WDN", [NE, 1024, 1024], BF16), ("QN", [8, 128, n_lat], BF16), ("QR", [8, 64, n_lat], BF16),
                          ("KN", [8, 128, n_tok], BF16), ("KRo", [64, n_tok], BF16), ("Vt", [n_tok, 1024], BF16)):
        io[name] = dram(nc, name, shp, dt)
    io["out"] = dram(nc, "out", [n_lat, 1024], kind="ExternalOutput")
    p = Prog(nc)
    C = make_consts(p)
    make_masks(p, C)
    make_rope_consts(p, C)
    G = {k: p.sb("G" + k, [128, L, 2, 8], F32) for k in ("A1", "B1", "A2", "B2")}
    with p.phase():
        cp = Ring(p, "cpx", [128, 1024], n=3)
        for ti in range(n_tok // 128):
            x_, xk = cp.next()
            p.dma("sp", lambda e, x_=x_, ti=ti: e.dma_start(out=x_[:], in_=io["xin"][ti * 128:(ti + 1) * 128, :]), w=[xk])
            p.dma("sp", lambda e, x_=x_, ti=ti: e.dma_start(out=io["xres"][ti * 128:(ti + 1) * 128, :], in_=x_[:]), r=[xk], w=["xres"])
    phase_ada(p, C, io, L, G)
    phase_inproj(p, C, io, G, 0, n_ctx, n_tok)
    phase_gla(p, C, io, n_ctx, n_tok)
    phase_gla_finish(p, C, io, n_tok)
    phase_shift(p, C, io, n_ctx, n_tok)
    phase_rwkv(p, C, io, n_ctx, n_tok)
    phase_rwkv_finish(p, C, io, n_tok)
    phase_outproj(p, C, io, 0, io["ev_w_out"], n_ctx, n_tok)
    phase_moe_cast(p, C, io, 0, NE)
    phase_moe_route(p, C, io, G, 0, NE, 0, n_tok, n_ctx)
    phase_moe_experts(p, C, io, 0, NE, 0, n_tok, n_ctx)
    phase_mla_proj(p, C, io, G, 1, n_ctx, n_tok)
    phase_mla_attn(p, C, io, n_ctx, n_tok)
    phase_outproj(p, C, io, 1, io["od_w_out"], n_ctx, n_tok, t_lo=n_ctx)
    phase_moe_cast(p, C, io, 1, NE)
    phase_moe_route(p, C, io, G, 1, NE, n_ctx, n_tok, n_ctx)
    phase_moe_experts(p, C, io, 1, NE, n_ctx, n_tok, n_ctx)
    phase_final(p, C, io, n_ctx, n_tok)
    p.finish()
    return nc


def kernel(**inputs):
    f = lambda a: np.ascontiguousarray(np.asarray(a, dtype=np.float32))
    x, c, ctx, c_ctx = f(inputs["x"]), f(inputs["c"]), f(inputs["ctx"]), f(inputs["c_ctx"])
    B = x.shape[0]
    shared = {}
    for k in _W_NAMES:
        a = f(inputs[k])
        if k.startswith(("ev_", "gla_", "rw_", "od_", "mla_")):
            a = np.ascontiguousarray(a[0])
        shared[k] = a
    in_maps = []
    for b in range(B):
        m = dict(shared)
        m["xin"] = np.ascontiguousarray(np.concatenate([ctx[b], x[b]], axis=0))
        m["cvec"] = np.ascontiguousarray(np.stack([c[b], c_ctx], axis=0))
        in_maps.append(m)
    shapes = {k: v.shape for k, v in in_maps[0].items()}
    nc = build(shapes, n_ctx=ctx.shape[1], n_lat=x.shape[1], NE=shared["router_w"].shape[-1])
    res = run_bass_kernel_spmd(nc, in_maps, core_ids=list(range(B)))
    return np.stack([np.asarray(r["out"], dtype=np.float32) for r in res.results], axis=0)
```

```python
import contextlib
import numpy as np
import concourse.bass as bass
import concourse.mybir as mybir

F32 = mybir.dt.float32
BF16 = mybir.dt.bfloat16
I32 = mybir.dt.int32
U32 = mybir.dt.uint32
ALU = mybir.AluOpType
AF = mybir.ActivationFunctionType
AX = mybir.AxisListType

ENGS = ("pe", "act", "dve", "pool", "sp")
RESET_THRESH = 20000


class Prog:
    def __init__(self, nc, n_dma_slots=10):
        self.nc = nc
        self.es = contextlib.ExitStack()
        self.cur = self.es
        self.streams = {e: [] for e in ENGS}
        self.count = {e: 0 for e in ENGS}
        self.seen = {e: {} for e in ENGS}
        self.bufs = {}
        self.sems = {}
        for e in ("pe", "act", "dve", "pool"):
            self.sems[e] = self.es.enter_context(nc.semaphore("s_" + e))
        self.dma_slots = {}
        self.dma_rr = {}
        for q in ("sp", "act", "pool"):
            self.dma_slots[q] = []
            for i in range(n_dma_slots):
                s = self.es.enter_context(nc.semaphore(f"d_{q}{i}"))
                self.sems[f"d_{q}{i}"] = s
                self.dma_slots[q].append([f"d_{q}{i}", 0])
            self.dma_rr[q] = 0
        self.n_instr = 0
        self.bsem = self.es.enter_context(nc.semaphore("s_bar"))
        self.gsem = self.es.enter_context(nc.semaphore("s_go"))
        self.nreset = 0
        self.reset_thresh = RESET_THRESH

    def _maybe_reset(self):
        if max(self.count.values()) >= self.reset_thresh or \
                max(v for q in self.dma_slots if q != "pool" for _, v in self.dma_slots[q]) >= 2 * self.reset_thresh:
            self.sync_reset()

    def sync_reset(self):
        self.barrier()
        self.nreset += 1
        k = self.nreset
        bs, gs = self.bsem, self.gsem
        for e in ENGS:
            self.streams[e].append(lambda eng, bs=bs: eng.sem_inc(bs, 1))
        self.streams["sp"].append(lambda eng, bs=bs, k=k: eng.wait_ge(bs, 5 * k))
        for name, sem in self.sems.items():
            if name.startswith("d_pool"):
                continue
            self.streams["sp"].append(lambda eng, sem=sem: eng.sem_clear(sem))
        self.streams["sp"].append(lambda eng, gs=gs: eng.sem_inc(gs, 1))
        for e in ENGS:
            if e != "sp":
                self.streams[e].append(lambda eng, gs=gs, k=k: eng.wait_ge(gs, k))
        self.count = {e: 0 for e in ENGS}
        self.seen = {e: {} for e in ENGS}
        self.bufs = {}
        for q in self.dma_slots:
            if q == "pool":
                continue
            for slot in self.dma_slots[q]:
                slot[1] = 0

    def sb(self, name, shape, dt=F32):
        self._uid = getattr(self, "_uid", 0) + 1
        name = f"{name}_u{self._uid}"
        return self.cur.enter_context(self.nc.sbuf_tensor(name, list(shape), dt))

    def ps(self, name, shape, dt=F32):
        self._uid = getattr(self, "_uid", 0) + 1
        name = f"{name}_u{self._uid}"
        return self.cur.enter_context(self.nc.psum_tensor(name, list(shape), dt))

    def _deps(self, r, w):
        deps = {}

        def add(tok):
            if tok is None:
                return
            s, v = tok
            if deps.get(s, 0) < v:
                deps[s] = v

        for k in r:
            b = self.bufs.setdefault(k, {"w": None, "r": {}})
            add(b["w"])
        for k in w:
            b = self.bufs.setdefault(k, {"w": None, "r": {}})
            add(b["w"])
            for s, v in b["r"].items():
                add((s, v))
        return deps

    def _commit(self, r, w, tok):
        for k in r:
            b = self.bufs[k]
            if b["r"].get(tok[0], 0) < tok[1]:
                b["r"][tok[0]] = tok[1]
        for k in w:
            b = self.bufs[k]
            b["w"] = tok
            b["r"] = {}

    def _emit_waits(self, eng, deps):
        seen = self.seen[eng]
        for s, v in deps.items():
            if eng == "pe" and s == "pe":
                continue
            if seen.get(s, 0) >= v:
                continue
            seen[s] = v
            sem = self.sems[s]
            self.streams[eng].append(lambda e, sem=sem, v=v: e.wait_ge(sem, v))

    def op(self, eng, fn, r=(), w=()):
        self._maybe_reset()
        deps = self._deps(r, w)
        self._emit_waits(eng, deps)
        self.count[eng] += 1
        n = self.count[eng]
        sem = self.sems[eng]
        self.streams[eng].append(lambda e, fn=fn, sem=sem: fn(e).then_inc(sem, 1))
        self._commit(r, w, (eng, n))
        self.n_instr += 1

    def dma(self, q, fn, r=(), w=()):
        self._maybe_reset()
        deps = self._deps(r, w)
        i = self.dma_rr[q]
        self.dma_rr[q] = (i + 1) % len(self.dma_slots[q])
        slot = self.dma_slots[q][i]
        if slot[1] > 0:
            if deps.get(slot[0], 0) < slot[1]:
                deps[slot[0]] = slot[1]
        self._emit_waits(q, deps)
        slot[1] += 16
        sem = self.sems[slot[0]]
        self.streams[q].append(lambda e, fn=fn, sem=sem: fn(e).then_inc(sem, 16))
        self._commit(r, w, (slot[0], slot[1]))
        self.n_instr += 1

    def wait_all(self, eng, keys):
        deps = self._deps(keys, ())
        self._emit_waits(eng, deps)

    def barrier(self):
        full = {}
        for e in ("pe", "act", "dve", "pool"):
            if self.count[e] > 0:
                full[e] = self.count[e]
        for q in self.dma_slots:
            for name, v in self.dma_slots[q]:
                if v > 0:
                    full[name] = v
        for e in ENGS:
            d = dict(full)
            d.pop(e, None)
            if e == "pe":
                pass
            self._emit_waits(e, d)

    @contextlib.contextmanager
    def phase(self):
        old = self.cur
        with contextlib.ExitStack() as st:
            self.cur = st
            yield
            self.barrier()
            self.flush()
        self.cur = old

    def flush(self):
        self._emit_block()
        self.streams = {e: [] for e in ENGS}

    def finish(self):
        self.barrier()
        self.flush()
        self.es.close()

    def _emit_block(self):
        nc = self.nc
        with nc.Block() as block:
            @block.tensor
            def _(e):
                for f in self.streams["pe"]:
                    f(e)

            @block.scalar
            def _(e):
                for f in self.streams["act"]:
                    f(e)

            @block.vector
            def _(e):
                for f in self.streams["dve"]:
                    f(e)

            @block.gpsimd
            def _(e):
                for f in self.streams["pool"]:
                    f(e)

            @block.sync
            def _(e):
                for f in self.streams["sp"]:
                    f(e)

import contextlib
import numpy as np

D = 1024
KC = 8
EPS = 1e-6
GLA_COLS = 1552
RW_COLS = 1824
EVEN_IN = 3376


def dram(nc, name, shape, dt=F32, kind="Internal"):
    return nc.dram_tensor(name, list(shape), dt, kind=kind).ap()


def make_consts(p):
    nc = p.nc
    C = {}
    iota_f = p.sb("iota_f", [128, 128], F32)
    iota_p = p.sb("iota_p", [128, 1], F32)
    ii = p.sb("iota_i", [128, 128], I32)
    ip = p.sb("iota_pi", [128, 1], I32)
    p.op("pool", lambda e: e.iota(ii[:], pattern=[[1, 128]], base=0, channel_multiplier=0), w=["iota_i"])
    p.op("pool", lambda e: e.iota(ip[:], pattern=[[1, 1]], base=0, channel_multiplier=1), w=["iota_pi"])
    p.op("dve", lambda e: e.tensor_copy(out=iota_f[:], in_=ii[:]), r=["iota_i"], w=["iota_f"])
    p.op("dve", lambda e: e.tensor_copy(out=iota_p[:], in_=ip[:]), r=["iota_pi"], w=["iota_p"])
    ident = p.sb("ident", [128, 128], F32)
    p.op("dve", lambda e: e.tensor_scalar(out=ident[:], in0=iota_f[:], scalar1=iota_p[:, 0:1], scalar2=None,
                                          op0=ALU.is_equal), r=["iota_f", "iota_p"], w=["ident"])
    identb = p.sb("identb", [128, 128], BF16)
    p.op("dve", lambda e: e.tensor_copy(out=identb[:], in_=ident[:]), r=["ident"], w=["identb"])
    C.update(iota_f=iota_f, iota_p=iota_p, ident=ident, identb=identb)
    return C


def phase_ada(p, C, io, L, G):
    nc = p.nc
    cvec, ada_w, ada_b = io["cvec"], io["ada_w"], io["ada_b"]
    modrow = io["modrow"]
    with p.phase():
        cT = p.sb("cT", [128, KC, 2], F32)
        sT = p.sb("sT", [128, KC, 2], F32)
        for r in range(2):
            p.dma("sp", lambda e, r=r: e.dma_start(out=cT[:, :, r], in_=cvec[r, :].rearrange("(c q) -> q c", q=128),
                                                   allow_slow_non_contiguous=True), w=["cT"])
        p.op("act", lambda e: e.activation(out=sT[:], in_=cT[:], func=AF.Silu), r=["cT"], w=["sT"])
        wts = [p.sb(f"adaw{i}", [128, KC, 512], F32) for i in range(2)]
        mrow = p.sb("mrow", [2, 6144], F32)
        brow = p.sb("brow", [2, 6144], F32)
        mps = p.ps("mps", [2, 512], F32)
        tps = p.ps("tps", [128, 48, 2], F32)
        mcol = p.sb("mcol", [128, 48, 2], F32)
        gcol = p.sb("gcol", [128, 2, KC], F32)
        it = 0
        for l in range(L):
            for r in range(2):
                p.dma("sp", lambda e, r=r, l=l: e.dma_start(out=brow[r:r + 1, :], in_=ada_b[l:l + 1, :]), w=["brow"])
            p.dma("sp", lambda e, l=l: e.dma_start(out=gcol[:, 0, :], in_=io["norm1_g"][l, :].rearrange("(c q) -> q c", q=128),
                                                   allow_slow_non_contiguous=True), w=["gcol"])
            p.dma("sp", lambda e, l=l: e.dma_start(out=gcol[:, 1, :], in_=io["norm2_g"][l, :].rearrange("(c q) -> q c", q=128),
                                                   allow_slow_non_contiguous=True), w=["gcol"])
            for cc in range(12):
                wt = wts[it % 2]
                wk = f"adaw{it % 2}"
                it += 1
                p.dma("sp", lambda e, wt=wt, l=l, cc=cc: e.dma_start(
                    out=wt[:], in_=ada_w[l, :, cc * 512:(cc + 1) * 512].rearrange("(c q) n -> q c n", q=128)), w=[wk])
                for kc in range(KC):
                    p.op("pe", lambda e, wt=wt, kc=kc: e.matmul(mps[:], lhsT=sT[:, kc, :], rhs=wt[:, kc, :],
                                                                 start=(kc == 0), stop=(kc == KC - 1)),
                         r=["sT", wk], w=["mps"])
                p.op("dve", lambda e, cc=cc: e.tensor_tensor(out=mrow[:, cc * 512:(cc + 1) * 512], in0=mps[:],
                                                             in1=brow[:, cc * 512:(cc + 1) * 512], op=ALU.add),
                     r=["mps", "brow"], w=["mrow"])
            p.dma("sp", lambda e, l=l: e.dma_start(out=modrow[l], in_=mrow[:]), r=["mrow"], w=[f"modrow{l}"])
            for c in range(48):
                p.op("pe", lambda e, c=c: e.transpose(tps[:, c, :], mrow[0:2, c * 128:(c + 1) * 128], C["ident"][0:2, 0:2]),
                     r=["mrow", "ident"], w=["tps"])
            p.op("dve", lambda e: e.tensor_copy(out=mcol[:], in_=tps[:]), r=["tps"], w=["mcol"])
            for r in range(2):
                for (nm, sc_c, sh_c, gi) in (("1", 1, 0, 0), ("2", 4, 3, 1)):
                    A = G["A" + nm]
                    B = G["B" + nm]
                    p.op("dve", lambda e, A=A, r=r, l=l, sc_c=sc_c, gi=gi: e.scalar_tensor_tensor(
                        out=A[:, l, r, :], in0=mcol[:, sc_c * 8:(sc_c + 1) * 8, r], scalar=1.0, in1=gcol[:, gi, :],
                        op0=ALU.add, op1=ALU.mult), r=["mcol", "gcol"], w=["G"])
                    p.op("dve", lambda e, B=B, r=r, l=l, sh_c=sh_c: e.tensor_copy(
                        out=B[:, l, r, :], in_=mcol[:, sh_c * 8:(sh_c + 1) * 8, r]), r=["mcol"], w=["G"])


def norm_tile(p, C, T, xt_key, xt, Acol, Bcol, hT_dst, hT_key, hT32_dst=None, want_bf=True):
    ss, rstd, xn, junk = T["ss"], T["rstd"], T["xn"], T["junk"]
    p.op("act", lambda e: e.activation(out=junk[:], in_=xt[:], func=AF.Square, accum_out=ss[:, 0:1]),
         r=[xt_key], w=["junk", "ss"])
    p.op("dve", lambda e: e.tensor_scalar(out=rstd[:], in0=ss[:], scalar1=1.0 / D, scalar2=EPS, op0=ALU.mult, op1=ALU.add),
         r=["ss"], w=["rstd"])
    p.op("act", lambda e: e.activation(out=ss[:], in_=rstd[:], func=AF.Sqrt), r=["rstd"], w=["ss"])
    p.op("dve", lambda e: e.reciprocal(out=rstd[:], in_=ss[:]), r=["ss"], w=["rstd"])
    p.op("dve", lambda e: e.tensor_scalar(out=xn[:], in0=xt[:], scalar1=rstd[:, 0:1], scalar2=None, op0=ALU.mult),
         r=[xt_key, "rstd"], w=["xn"])
    for half in range(2):
        tp = T["tp"][half]
        tk = f"tp{half}"
        for j in range(4):
            kc = half * 4 + j
            p.op("pe", lambda e, tp=tp, j=j, kc=kc: e.transpose(tp[:, j, :], xn[:, kc * 128:(kc + 1) * 128], C["ident"][:]),
                 r=["xn", "ident"], w=[tk])
        for j in range(4):
            kc = half * 4 + j
            if hT32_dst is None:
                p.op("act", lambda e, tp=tp, j=j, kc=kc: e.activation(out=hT_dst(kc), in_=tp[:, j, :], func=AF.Identity,
                                                                       scale=Acol[:, kc:kc + 1], bias=Bcol[:, kc:kc + 1]),
                     r=[tk, "G"], w=[hT_key])
            else:
                p.op("act", lambda e, tp=tp, j=j, kc=kc: e.activation(out=hT32_dst(kc), in_=tp[:, j, :], func=AF.Identity,
                                                                       scale=Acol[:, kc:kc + 1], bias=Bcol[:, kc:kc + 1]),
                     r=[tk, "G"], w=[hT_key + "32"])
                if want_bf:
                    p.op("pool", lambda e, kc=kc: e.tensor_copy(out=hT_dst(kc), in_=hT32_dst(kc)), r=[hT_key + "32"], w=[hT_key])


def norm_scratch(p):
    T = {}
    T["ss"] = p.sb("ss", [128, 1], F32)
    T["rstd"] = p.sb("rstd", [128, 1], F32)
    T["xn"] = p.sb("xn", [128, D], F32)
    T["junk"] = p.sb("junk", [128, D], BF16)
    T["tp"] = [p.ps(f"tp{h}", [128, 4, 128], F32) for h in range(2)]
    return T


def load_cast_weight(p, dst, dst_key, src_ap, rows_kc, ncols, stage, stage_key, eng_cycle=("dve", "pool")):
    for kc in range(rows_kc):
        st = stage[kc % len(stage)]
        sk = f"{stage_key}{kc % len(stage)}"
        p.dma("sp", lambda e, st=st, kc=kc: e.dma_start(out=st[:, 0:ncols], in_=src_ap[kc * 128:(kc + 1) * 128, :]), w=[sk])
        eng = eng_cycle[kc % len(eng_cycle)]
        p.op(eng, lambda e, st=st, kc=kc: e.tensor_copy(out=dst[:, kc, :], in_=st[:, 0:ncols]), r=[sk], w=[dst_key])


def phase_inproj(p, C, io, G, l, n_ctx, n_tok):
    xres, PT, PTOK, w_in = io["xres"], io["PT"], io["PTOK"], io["ev_w_in"]
    with p.phase():
        T = norm_scratch(p)
        wbf = p.sb("winbf", [128, KC, EVEN_IN], BF16)
        stage = [p.sb(f"wstage{i}", [128, EVEN_IN], F32) for i in range(2)]
        load_cast_weight(p, wbf, "winbf", w_in, KC, EVEN_IN, stage, "wstage")
        xts = [p.sb(f"xt{i}", [128, D], F32) for i in range(2)]
        hT = [p.sb(f"hT{i}", [128, KC, 512], BF16) for i in range(2)]
        pps = [p.ps(f"pps{i}", [128, 512], F32) for i in range(3)]
        ost = [p.sb(f"ost{i}", [128, 512], F32) for i in range(4)]
        ntile = n_tok // 128
        nsup = (ntile + 3) // 4
        oi = 0
        pi = 0
        tok_cols = [(512, 512), (1024, 512), (GLA_COLS + 1024, 512)]
        for s in range(nsup):
            tiles = list(range(s * 4, min(ntile, s * 4 + 4)))
            N = len(tiles) * 128
            h = hT[s % 2]
            hk = f"hT{s % 2}"
            for j, ti in enumerate(tiles):
                r = 1 if ti * 128 < n_ctx else 0
                xt = xts[ti % 2]
                xk = f"xt{ti % 2}"
                p.dma("sp", lambda e, xt=xt, ti=ti: e.dma_start(out=xt[:], in_=xres[ti * 128:(ti + 1) * 128, :]), w=[xk])
                norm_tile(p, C, T, xk, xt, G["A1"][:, l, r, :], G["B1"][:, l, r, :],
                          lambda kc, h=h, j=j: h[:, kc, j * 128:(j + 1) * 128], hk)
            ncc = (EVEN_IN + 127) // 128
            for cc in range(ncc):
                cw = min(128, EVEN_IN - cc * 128)
                ps = pps[pi % 3]
                pk = f"pps{pi % 3}"
                pi += 1
                for kc in range(KC):
                    p.op("pe", lambda e, ps=ps, kc=kc, cc=cc, cw=cw, h=h, N=N: e.matmul(
                        ps[0:cw, 0:N], lhsT=wbf[:, kc, cc * 128:cc * 128 + cw], rhs=h[:, kc, 0:N],
                        start=(kc == 0), stop=(kc == KC - 1)), r=["winbf", hk], w=[pk])
                o = ost[oi % 4]
                ok = f"ost{oi % 4}"
                eng = "dve" if oi % 2 == 0 else "act"
                oi += 1
                if eng == "dve":
                    p.op("dve", lambda e, o=o, ps=ps, cw=cw, N=N: e.tensor_copy(out=o[0:cw, 0:N], in_=ps[0:cw, 0:N]), r=[pk], w=[ok])
                else:
                    p.op("act", lambda e, o=o, ps=ps, cw=cw, N=N: e.copy(out=o[0:cw, 0:N], in_=ps[0:cw, 0:N]), r=[pk], w=[ok])
                p.dma("sp", lambda e, o=o, cc=cc, cw=cw, N=N, s=s: e.dma_start(
                    out=PT[cc * 128:cc * 128 + cw, s * 512:s * 512 + N], in_=o[0:cw, 0:N]), r=[ok], w=["PT"])
            for j, ti in enumerate(tiles):
                for gi, (c0, cn) in enumerate(tok_cols):
                    ps = pps[pi % 3]
                    pk = f"pps{pi % 3}"
                    pi += 1
                    for kc in range(KC):
                        p.op("pe", lambda e, ps=ps, kc=kc, c0=c0, cn=cn, h=h, j=j: e.matmul(
                            ps[:, 0:cn], lhsT=h[:, kc, j * 128:(j + 1) * 128], rhs=wbf[:, kc, c0:c0 + cn],
                            start=(kc == 0), stop=(kc == KC - 1)), r=["winbf", hk], w=[pk])
                    o = ost[oi % 4]
                    ok = f"ost{oi % 4}"
                    eng = "dve" if oi % 2 == 0 else "act"
                    oi += 1
                    if eng == "dve":
                        p.op("dve", lambda e, o=o, ps=ps, cn=cn: e.tensor_copy(out=o[:, 0:cn], in_=ps[:, 0:cn]), r=[pk], w=[ok])
                    else:
                        p.op("act", lambda e, o=o, ps=ps, cn=cn: e.copy(out=o[:, 0:cn], in_=ps[:, 0:cn]), r=[pk], w=[ok])
                    p.dma("sp", lambda e, o=o, ti=ti, gi=gi, cn=cn: e.dma_start(
                        out=PTOK[ti * 128:(ti + 1) * 128, gi * 512:gi * 512 + cn], in_=o[:, 0:cn]), r=[ok], w=["PTOK"])

import os


class Ring:
    def __init__(self, p, name, shape, dt=F32, n=2, psum=False):
        self.t = [(p.ps if psum else p.sb)(f"{name}{i}", shape, dt) for i in range(n)]
        self.k = [f"{name}{i}" for i in range(n)]
        self.i = 0

    def next(self):
        i = self.i
        self.i = (i + 1) % len(self.t)
        return self.t[i], self.k[i]


class PsRing:
    def __init__(self, p, name, nbanks):
        self.banks = [p.ps(f"{name}{i}", [128, 512], F32) for i in range(nbanks)]
        self.n = nbanks * 1
        self.sub = 1
        self.name = name
        self.i = 0

    def next(self):
        i = self.i
        self.i = (i + 1) % self.n
        sb_ = self.sub
        return self.banks[i // sb_][:, (i % sb_) * 128:(i % sb_ + 1) * 128], f"{self.name}_{i}"


def make_masks(p, C):
    f, q = C["iota_f"], C["iota_p"]
    for nm, op in (("incl0", ALU.is_ge), ("incl1", ALU.is_le), ("strict0", ALU.is_gt), ("strict1", ALU.is_lt)):
        m = p.sb("m_" + nm, [128, 128], F32)
        p.op("dve", lambda e, m=m, op=op: e.tensor_scalar(out=m[:], in0=f[:], scalar1=q[:, 0:1], scalar2=None, op0=op),
             r=["iota_f", "iota_p"], w=["masks"])
        C[nm] = m
    for nm, val in (("ones128", 1.0 / 128), ("ones64", 1.0), ("ones64m", 1.0 / 64)):
        m = p.sb("m_" + nm, [128, 128], F32)
        p.op("dve", lambda e, m=m, val=val: e.memset(m[:], val), w=["masks"])
        C[nm] = m


def chunk_order(n_ctx, n_tok):
    nc_, nt = n_ctx // 128, n_tok // 128
    fwd = list(range(nt))
    bwd = list(range(nc_ - 1, -1, -1)) + list(range(nt - 1, nc_ - 1, -1))
    return [fwd, bwd]


def phase_gla(p, C, io, n_ctx, n_tok):
    PT, PTOK, YT = io["PT"], io["PTOK"], io["YTG"]
    gk_up, gk_b = io["gla_gk_up"], io["gla_gk_b"]
    order = chunk_order(n_ctx, n_tok)
    nt = n_tok // 128
    with p.phase():
        gkup = p.sb("gkup", [16, 2, 256], F32)
        gkb = p.sb("gkb", [64, 2, 4], F32)
        for d in range(2):
            p.dma("sp", lambda e, d=d: e.dma_start(out=gkup[:, d, :], in_=gk_up[d]), w=["gkw"])
            p.dma("sp", lambda e, d=d: e.dma_start(out=gkb[:, d, :], in_=gk_b[d].rearrange("(h q) -> q h", q=64),
                                                   allow_slow_non_contiguous=True), w=["gkw"])
        S = [[p.sb(f"S{d}{h}", [64, 128], F32) for h in range(4)] for d in range(2)]
        for d in range(2):
            for h in range(4):
                p.op("pool", lambda e, t=S[d][h]: e.memset(t[:], 0.0), w=[f"S{d}{h}"])
        gd = Ring(p, "gd", [16, 128], n=4)
        qk = Ring(p, "qk", [64, 2, 128], n=9)
        vv = Ring(p, "vv", [128, 128], n=9)
        sg = Ring(p, "sg", [64, 128], n=9)
        lg = Ring(p, "lg", [64, 128], n=9)
        lgT = Ring(p, "lgT", [128, 64], n=9)
        eW = Ring(p, "eW", [64, 128], n=9)
        eWi = Ring(p, "eWi", [64, 128], n=9)
        rt = Ring(p, "rt", [64, 128], n=9)
        kt = Ring(p, "kt", [64, 128], n=9)
        ktok = Ring(p, "ktok", [128, 64], n=9)
        mrk = Ring(p, "mrk", [128, 128], n=9)
        yts = Ring(p, "yts", [128, 128], n=9)
        s2 = Ring(p, "s2", [64, 128], n=9)
        psA = Ring(p, "psA", [128, 128], n=8, psum=True)
        def unit(d, h, c, gdt, gdk):
            t0 = c * 128
            tend = 127 if d == 0 else 0
            Sk = f"S{d}{h}"
            St = S[d][h]
            qt, qkk = qk.next()
            p.dma("sp", lambda e, qt=qt, h=h, t0=t0: e.dma_start(out=qt[:, 0, :], in_=PT[h * 64:(h + 1) * 64, t0:t0 + 128]), w=[qkk])
            p.dma("sp", lambda e, qt=qt, h=h, t0=t0: e.dma_start(out=qt[:, 1, :], in_=PT[256 + h * 64:256 + (h + 1) * 64, t0:t0 + 128]), w=[qkk])
            vt, vk = vv.next()
            p.dma("sp", lambda e, vt=vt, h=h, t0=t0: e.dma_start(out=vt[:], in_=PTOK[t0:t0 + 128, h * 128:(h + 1) * 128]), w=[vk])
            yield
            ps, pk = psA.next()
            p.op("pe", lambda e, ps=ps, d=d, h=h, gdt=gdt: e.matmul(ps[0:64, :], lhsT=gkup[:, d, h * 64:(h + 1) * 64], rhs=gdt[:],
                                                                    start=True, stop=True), r=["gkw", gdk], w=[pk])
            sgt, sgk = sg.next()
            p.op("act", lambda e, sgt=sgt, ps=ps, d=d, h=h: e.activation(out=sgt[:], in_=ps[0:64, :], func=AF.Sigmoid,
                                                                        bias=gkb[:, d, h:h + 1]), r=[pk, "gkw"], w=[sgk])
            lgt, lgk = lg.next()
            p.op("act", lambda e, lgt=lgt, sgt=sgt: e.activation(out=lgt[:], in_=sgt[:], func=AF.Ln), r=[sgk], w=[lgk])
            yield
            ps, pk = psA.next()
            p.op("pe", lambda e, ps=ps, lgt=lgt: e.transpose(ps[:, 0:64], lgt[:], C["ident"][0:64, 0:64]), r=[lgk, "ident"], w=[pk])
            lTt, lTk = lgT.next()
            p.op("dve", lambda e, lTt=lTt, ps=ps: e.tensor_copy(out=lTt[:], in_=ps[:, 0:64]), r=[pk], w=[lTk])
            yield
            ps, pk = psA.next()
            p.op("pe", lambda e, ps=ps, lTt=lTt, d=d: e.matmul(ps[0:64, :], lhsT=lTt[:], rhs=C[f"incl{d}"][:], start=True, stop=True),
                 r=[lTk, "masks"], w=[pk])
            eWt, eWk = eW.next()
            eWit, eWik = eWi.next()
            p.op("act", lambda e, eWt=eWt, ps=ps: e.activation(out=eWt[:], in_=ps[0:64, :], func=AF.Exp, scale=1.0 / 16), r=[pk], w=[eWk])
            p.op("act", lambda e, eWit=eWit, ps=ps: e.activation(out=eWit[:], in_=ps[0:64, :], func=AF.Exp, scale=-1.0 / 16), r=[pk], w=[eWik])
            rtt, rtk = rt.next()
            ktt, ktk = kt.next()
            p.op("dve", lambda e, rtt=rtt, qt=qt, eWt=eWt: e.scalar_tensor_tensor(out=rtt[:], in0=qt[:, 0, :], scalar=0.125, in1=eWt[:],
                                                                                 op0=ALU.mult, op1=ALU.mult), r=[qkk, eWk], w=[rtk])
            p.op("dve", lambda e, ktt=ktt, qt=qt, eWit=eWit: e.tensor_tensor(out=ktt[:], in0=qt[:, 1, :], in1=eWit[:], op=ALU.mult),
                 r=[qkk, eWik], w=[ktk])
            yield
            ps, pk = psA.next()
            p.op("pe", lambda e, ps=ps, ktt=ktt: e.transpose(ps[:, 0:64], ktt[:], C["ident"][0:64, 0:64]), r=[ktk, "ident"], w=[pk])
            kTt, kTk = ktok.next()
            p.op("dve", lambda e, kTt=kTt, ps=ps: e.tensor_copy(out=kTt[:], in_=ps[:, 0:64]), r=[pk], w=[kTk])
            yield
            ps, pk = psA.next()
            p.op("pe", lambda e, ps=ps, ktt=ktt, rtt=rtt: e.matmul(ps[:], lhsT=ktt[:], rhs=rtt[:], start=True, stop=True), r=[ktk, rtk], w=[pk])
            mt, mk = mrk.next()
            p.op("dve", lambda e, mt=mt, ps=ps, d=d: e.tensor_tensor(out=mt[:], in0=ps[:], in1=C[f"incl{d}"][:], op=ALU.mult),
                 r=[pk, "masks"], w=[mk])
            yield
            ps, pk = psA.next()
            p.op("pe", lambda e, ps=ps, St=St, rtt=rtt: e.matmul(ps[:], lhsT=St[:], rhs=rtt[:], start=True, stop=False), r=[Sk, rtk], w=[pk])
            p.op("pe", lambda e, ps=ps, vt=vt, mt=mt: e.matmul(ps[:], lhsT=vt[:], rhs=mt[:], start=False, stop=True), r=[vk, mk], w=[pk])
            yt, yk = yts.next()
            p.op("act", lambda e, yt=yt, ps=ps: e.copy(out=yt[:], in_=ps[:]), r=[pk], w=[yk])
            p.dma("sp", lambda e, yt=yt, d=d, h=h, t0=t0: e.dma_start(out=YT[d, h * 128:(h + 1) * 128, t0:t0 + 128], in_=yt[:]),
                  r=[yk], w=["YTG"])
            yield
            ps, pk = psA.next()
            p.op("pe", lambda e, ps=ps, kTt=kTt, vt=vt: e.matmul(ps[0:64, :], lhsT=kTt[:], rhs=vt[:], start=True, stop=True), r=[kTk, vk], w=[pk])
            s2t, s2k = s2.next()
            p.op("dve", lambda e, s2t=s2t, St=St, eWt=eWt, tend=tend: e.tensor_scalar(out=s2t[:], in0=St[:], scalar1=eWt[:, tend:tend + 1],
                                                                                     scalar2=None, op0=ALU.mult), r=[Sk, eWk], w=[s2k])
            p.op("dve", lambda e, s2t=s2t, St=St, eWt=eWt, ps=ps, tend=tend: e.scalar_tensor_tensor(
                out=St[:], in0=ps[0:64, :], scalar=eWt[:, tend:tend + 1], in1=s2t[:], op0=ALU.mult, op1=ALU.add),
                r=[pk, eWk, s2k], w=[Sk])


        def drive(gens):
            gens = list(gens)
            while gens:
                for g_ in list(gens):
                    try:
                        next(g_)
                    except StopIteration:
                        gens.remove(g_)

        for i in range(nt):
            gens = []
            for d in range(2):
                c = order[d][i]
                t0 = c * 128
                gdt, gdk = gd.next()
                p.dma("sp", lambda e, gdt=gdt, t0=t0: e.dma_start(out=gdt[:], in_=PT[1536:1552, t0:t0 + 128]), w=[gdk])
                gens += [unit(d, h, c, gdt, gdk) for h in range(4)]
            drive(gens)


def phase_gla_finish(p, C, io, n_tok):
    PT, YT, MIXT = io["PT"], io["YTG"], io["MIXT"]
    with p.phase():
        gcol = p.sb("gng", [128, 1], F32)
        p.dma("sp", lambda e: e.dma_start(out=gcol[:, 0:1], in_=io["gla_norm_g"].rearrange("(q o) -> q o", o=1)), w=["gng"])
        y0 = Ring(p, "y0", [128, 512], n=2)
        y1 = Ring(p, "y1", [128, 512], n=2)
        og = Ring(p, "og", [128, 512], n=2)
        sq = Ring(p, "sq", [128, 512], n=2)
        rs = Ring(p, "rs", [128, 512], n=2)
        ob = Ring(p, "ob", [128, 512], BF16, n=2)
        psF = Ring(p, "psF", [128, 512], n=2, psum=True)
        for t0 in range(0, n_tok, 512):
            N = min(512, n_tok - t0)
            for h in range(4):
                a, ak = y0.next()
                b, bk = y1.next()
                o, ok = og.next()
                p.dma("sp", lambda e, a=a, h=h, t0=t0, N=N: e.dma_start(out=a[:, 0:N], in_=YT[0, h * 128:(h + 1) * 128, t0:t0 + N]), r=["YTG"], w=[ak])
                p.dma("sp", lambda e, b=b, h=h, t0=t0, N=N: e.dma_start(out=b[:, 0:N], in_=YT[1, h * 128:(h + 1) * 128, t0:t0 + N]), r=["YTG"], w=[bk])
                p.dma("sp", lambda e, o=o, h=h, t0=t0, N=N: e.dma_start(out=o[:, 0:N], in_=PT[1024 + h * 128:1024 + (h + 1) * 128, t0:t0 + N]), r=["PT"], w=[ok])
                p.op("dve", lambda e, a=a, b=b, N=N: e.tensor_tensor(out=a[:, 0:N], in0=a[:, 0:N], in1=b[:, 0:N], op=ALU.add), r=[ak, bk], w=[ak])
                s, sk = sq.next()
                p.op("act", lambda e, s=s, a=a, N=N: e.activation(out=s[:, 0:N], in_=a[:, 0:N], func=AF.Square), r=[ak], w=[sk])
                ps, pk = psF.next()
                p.op("pe", lambda e, ps=ps, s=s, N=N: e.matmul(ps[:, 0:N], lhsT=C["ones128"][:], rhs=s[:, 0:N], start=True, stop=True), r=[sk, "masks"], w=[pk])
                r_, rk = rs.next()
                p.op("dve", lambda e, r_=r_, ps=ps, N=N: e.tensor_scalar(out=r_[:, 0:N], in0=ps[:, 0:N], scalar1=EPS, scalar2=None, op0=ALU.add), r=[pk], w=[rk])
                p.op("act", lambda e, r_=r_, N=N: e.activation(out=r_[:, 0:N], in_=r_[:, 0:N], func=AF.Sqrt), r=[rk], w=[rk])
                p.op("dve", lambda e, r_=r_, N=N: e.reciprocal(out=r_[:, 0:N], in_=r_[:, 0:N]), r=[rk], w=[rk])
                p.op("dve", lambda e, a=a, r_=r_, N=N: e.scalar_tensor_tensor(out=a[:, 0:N], in0=a[:, 0:N], scalar=gcol[:, 0:1], in1=r_[:, 0:N],
                                                                             op0=ALU.mult, op1=ALU.mult), r=[ak, rk, "gng"], w=[ak])
                p.op("act", lambda e, s=s, o=o, N=N: e.activation(out=s[:, 0:N], in_=o[:, 0:N], func=AF.Silu), r=[ok], w=[sk])
                ot, otk = ob.next()
                p.op("dve", lambda e, ot=ot, a=a, s=s, N=N: e.tensor_tensor(out=ot[:, 0:N], in0=a[:, 0:N], in1=s[:, 0:N], op=ALU.mult), r=[ak, sk], w=[otk])
                p.dma("sp", lambda e, ot=ot, h=h, t0=t0, N=N: e.dma_start(out=MIXT[h * 128:(h + 1) * 128, t0:t0 + N], in_=ot[:, 0:N]), r=[otk], w=["MIXT"])


import os
STAGE = 99
R0 = GLA_COLS
NSQ = 12
NLW = 0.6065306597126334


def phase_shift(p, C, io, n_ctx, n_tok):
    PT, SMT, mu = io["PT"], io["SMT"], io["rw_mu"]
    with p.phase():
        nch = (RW_COLS + 127) // 128
        muc = p.sb("muc", [128, nch, 2], F32)
        p.op("pool", lambda e: e.memset(muc[:], 0.0), w=["muc"])
        for j in range(2):
            for c in range(nch):
                cw = min(128, RW_COLS - c * 128)
                p.dma("sp", lambda e, j=j, c=c, cw=cw: e.dma_start(out=muc[0:cw, c, j:j + 1],
                                                                   in_=mu[j, c * 128:c * 128 + cw].rearrange("(q o) -> q o", o=1)), w=["muc"])
        xin = Ring(p, "shx", [128, 514], n=3)
        d0 = Ring(p, "shd", [128, 512], n=3)
        so = Ring(p, "sho", [128, 512], n=3)
        blocks = []
        for (a, b) in ((0, n_ctx), (n_ctx, n_tok)):
            t = a
            while t < b:
                N = min(512, b - t)
                blocks.append((t, N, t == a, t + N == b))
                t += N
        for (t0, N, first, last) in blocks:
            for c in range(nch):
                cw = min(128, RW_COLS - c * 128)
                x, xk = xin.next()
                lo = 0 if not first else 1
                hi = N + 2 if not last else N + 1
                if first or last:
                    p.op("pool", lambda e, x=x: e.memset(x[:], 0.0), w=[xk])
                p.dma("sp", lambda e, x=x, c=c, cw=cw, lo=lo, hi=hi, t0=t0: e.dma_start(
                    out=x[0:cw, lo:hi], in_=PT[R0 + c * 128:R0 + c * 128 + cw, t0 - 1 + lo:t0 - 1 + hi]), w=[xk])
                dd, dk = d0.next()
                o, ok = so.next()
                p.op("dve", lambda e, dd=dd, x=x, cw=cw, N=N: e.tensor_tensor(out=dd[0:cw, 0:N], in0=x[0:cw, 0:N], in1=x[0:cw, 1:N + 1], op=ALU.subtract),
                     r=[xk], w=[dk])
                p.op("dve", lambda e, o=o, dd=dd, x=x, c=c, cw=cw, N=N: e.scalar_tensor_tensor(
                    out=o[0:cw, 0:N], in0=dd[0:cw, 0:N], scalar=muc[0:cw, c, 0:1], in1=x[0:cw, 1:N + 1], op0=ALU.mult, op1=ALU.add),
                    r=[dk, xk, "muc"], w=[ok])
                p.op("pool", lambda e, dd=dd, x=x, cw=cw, N=N: e.tensor_tensor(out=dd[0:cw, 0:N], in0=x[0:cw, 2:N + 2], in1=x[0:cw, 1:N + 1], op=ALU.subtract),
                     r=[xk, ok], w=[dk])
                p.op("dve", lambda e, o=o, dd=dd, c=c, cw=cw, N=N: e.scalar_tensor_tensor(
                    out=o[0:cw, 0:N], in0=dd[0:cw, 0:N], scalar=muc[0:cw, c, 1:2], in1=o[0:cw, 0:N], op0=ALU.mult, op1=ALU.add),
                    r=[dk, ok, "muc"], w=[ok])
                p.dma("sp", lambda e, o=o, c=c, cw=cw, N=N, t0=t0: e.dma_start(out=SMT[c * 128:c * 128 + cw, t0:t0 + N], in_=o[0:cw, 0:N]),
                      r=[ok], w=["SMT"])


def colload(p, dst, key, src_vec, nh, j=None):
    out = dst[:, :] if j is None else dst[:, j, :]
    p.dma("sp", lambda e: e.dma_start(out=out, in_=src_vec.rearrange("(h q) -> q h", q=64), allow_slow_non_contiguous=True), w=[key])


def phase_rwkv(p, C, io, n_ctx, n_tok):
    SMT, YT, RK = io["SMT"], io["YTR"], io["RK"]
    order = chunk_order(n_ctx, n_tok)
    nt = n_tok // 128
    with p.phase():
        w2 = p.sb("rw2", [64, 2, 512], F32)
        a2 = p.sb("ra2", [64, 2, 512], F32)
        for d in range(2):
            p.dma("sp", lambda e, d=d: e.dma_start(out=w2[:, d, :], in_=io["rw_w2"][d]), w=["rww"])
            p.dma("sp", lambda e, d=d: e.dma_start(out=a2[:, d, :], in_=io["rw_a2"][d]), w=["rww"])
        w0 = p.sb("rw0", [64, 2, 8], F32)
        a0 = p.sb("ra0", [64, 2, 8], F32)
        for d in range(2):
            colload(p, w0, "rww", io["rw_w0"][d], 8, d)
            colload(p, a0, "rww", io["rw_a0"][d], 8, d)
        kkc = p.sb("rkk", [64, 8], F32)
        kac = p.sb("rka", [64, 8], F32)
        kam = p.sb("rkam", [64, 8], F32)
        rkc = p.sb("rrk", [64, 8], F32)
        colload(p, kkc, "rww", io["rw_k_k"], 8)
        colload(p, kac, "rww", io["rw_k_a"], 8)
        colload(p, rkc, "rww", io["rw_r_k"].rearrange("h n -> (h n)"), 8)
        p.op("dve", lambda e: e.tensor_scalar(out=kam[:], in0=kac[:], scalar1=-1.0, scalar2=1.0, op0=ALU.mult, op1=ALU.add), r=["rww"], w=["rkam"])
        T = [[p.sb(f"T{d}{h}", [64, 64], F32) for h in range(8)] for d in range(2)]
        for d in range(2):
            for h in range(8):
                p.op("pool", lambda e, t=T[d][h]: e.memset(t[:], 0.0), w=[f"T{d}{h}"])
        wa = Ring(p, "wa", [64, 2, 128], n=3)
        rkv = Ring(p, "rkv", [64, 3, 128], n=9)
        f64 = {nm: Ring(p, nm, [64, 128], n=9) for nm in
               ("sgw", "alp", "kq", "sqk", "nrm", "kk", "tmk", "kd", "eW", "eWi", "eWx", "cx", "rt", "kt", "bt", "at", "rk_", "yo")}
        tokr = {nm: Ring(p, nm, [128, 64], n=9) for nm in ("sgT", "Ktok", "Btok", "Vtok", "Xs", "Us")}
        sqm = {nm: Ring(p, nm, [128, 128], n=9) for nm in ("Akt", "Mrbt", "Mrkt")}
        sqh = [{nm: Ring(p, f"{nm}h{h_}", [128, 128], n=3) for nm in ("At", "Am", "Pt")} for h_ in range(8)]
        tw = Ring(p, "tw", [64, 64], n=9)
        psA = PsRing(p, "psR", 8)

        def mm(out_ap, pk, lhsT, rhs, r, start=True, stop=True):
            p.op("pe", lambda e: e.matmul(out_ap, lhsT=lhsT, rhs=rhs, start=start, stop=stop), r=r, w=[pk])

        def tr(out_ap, pk, in_ap, n, r):
            p.op("pe", lambda e: e.transpose(out_ap, in_ap, C["ident"][0:n, 0:n]), r=r + ["ident"], w=[pk])

        def unit(d, h, c, wat, wak):
            t0 = c * 128
            tend = 127 if d == 0 else 0
            sl = slice(t0, t0 + 128)
            hs = slice(h * 64, (h + 1) * 64)
            Tt, Tk = T[d][h], f"T{d}{h}"
            x, xk = rkv.next()
            for j in range(3):
                p.dma("sp", lambda e, x=x, j=j, h=h, sl=sl: e.dma_start(out=x[:, j, :], in_=SMT[j * 512 + h * 64:j * 512 + (h + 1) * 64, sl]), w=[xk])
            r_, k_, v_ = x[:, 0, :], x[:, 1, :], x[:, 2, :]
            yield
            ps, pk = psA.next()
            mm(ps[0:64, :], pk, w2[:, d, hs], wat[:, 0, :], ["rww", wak])
            sgw, sgwk = f64["sgw"].next()
            p.op("act", lambda e, sgw=sgw, ps=ps, d=d, h=h: e.activation(out=sgw[:], in_=ps[0:64, :], func=AF.Sigmoid, bias=w0[:, d, h:h + 1]),
                 r=[pk, "rww"], w=[sgwk])
            yield
            ps, pk = psA.next()
            mm(ps[0:64, :], pk, a2[:, d, hs], wat[:, 1, :], ["rww", wak])
            alp, alpk = f64["alp"].next()
            p.op("act", lambda e, alp=alp, ps=ps, d=d, h=h: e.activation(out=alp[:], in_=ps[0:64, :], func=AF.Sigmoid, bias=a0[:, d, h:h + 1]),
                 r=[pk, "rww"], w=[alpk])
            kq, kqk = f64["kq"].next()
            p.op("dve", lambda e, kq=kq, k_=k_, h=h: e.tensor_scalar(out=kq[:], in0=k_, scalar1=kkc[:, h:h + 1], scalar2=None, op0=ALU.mult),
                 r=[xk, "rww"], w=[kqk])
            sqk, sqkk = f64["sqk"].next()
            p.op("act", lambda e, sqk=sqk, kq=kq: e.activation(out=sqk[:], in_=kq[:], func=AF.Square), r=[kqk], w=[sqkk])
            yield
            ps, pk = psA.next()
            mm(ps[0:64, :], pk, C["ones64"][0:64, 0:64], sqk[:], ["masks", sqkk])
            nrm, nrmk = f64["nrm"].next()
            p.op("act", lambda e, nrm=nrm, ps=ps: e.activation(out=nrm[:], in_=ps[0:64, :], func=AF.Sqrt), r=[pk], w=[nrmk])
            p.op("dve", lambda e, nrm=nrm: e.tensor_scalar(out=nrm[:], in0=nrm[:], scalar1=1e-12, scalar2=None, op0=ALU.max), r=[nrmk], w=[nrmk])
            p.op("dve", lambda e, nrm=nrm: e.reciprocal(out=nrm[:], in_=nrm[:]), r=[nrmk], w=[nrmk])
            kk, kkk = f64["kk"].next()
            p.op("dve", lambda e, kk=kk, kq=kq, nrm=nrm: e.tensor_tensor(out=kk[:], in0=kq[:], in1=nrm[:], op=ALU.mult), r=[kqk, nrmk], w=[kkk])
            tmk, tmkk = f64["tmk"].next()
            p.op("dve", lambda e, tmk=tmk, alp=alp, h=h: e.tensor_scalar(out=tmk[:], in0=alp[:], scalar1=kac[:, h:h + 1], scalar2=kam[:, h:h + 1],
                                                                        op0=ALU.mult, op1=ALU.add), r=[alpk, "rww", "rkam"], w=[tmkk])
            kd, kdk = f64["kd"].next()
            p.op("dve", lambda e, kd=kd, tmk=tmk, k_=k_: e.tensor_tensor(out=kd[:], in0=tmk[:], in1=k_, op=ALU.mult), r=[tmkk, xk], w=[kdk])
            rk_, rkk = f64["rk_"].next()
            p.op("dve", lambda e, rk_=rk_, r_=r_, kd=kd, h=h: e.scalar_tensor_tensor(out=rk_[:], in0=r_, scalar=rkc[:, h:h + 1], in1=kd[:],
                                                                                     op0=ALU.mult, op1=ALU.mult), r=[xk, kdk, "rww"], w=[rkk])
            p.dma("sp", lambda e, rk_=rk_, d=d, hs=hs, sl=sl: e.dma_start(out=RK[d, hs, sl], in_=rk_[:]), r=[rkk], w=["RK"])
            yield
            ps, pk = psA.next()
            tr(ps[:, 0:64], pk, sgw[:], 64, [sgwk])
            sgT, sgTk = tokr["sgT"].next()
            p.op("dve", lambda e, sgT=sgT, ps=ps: e.tensor_copy(out=sgT[:], in_=ps[:, 0:64]), r=[pk], w=[sgTk])
            yield
            ps, pk = psA.next()
            mm(ps[0:64, :], pk, sgT[:], C[f"incl{d}"][:], [sgTk, "masks"])
            eW, eWk = f64["eW"].next()
            eWi, eWik = f64["eWi"].next()
            cx, cxk = f64["cx"].next()
            eWx, eWxk = f64["eWx"].next()
            p.op("act", lambda e, eW=eW, ps=ps: e.activation(out=eW[:], in_=ps[0:64, :], func=AF.Exp, scale=-NLW), r=[pk], w=[eWk])
            p.op("act", lambda e, eWi=eWi, ps=ps: e.activation(out=eWi[:], in_=ps[0:64, :], func=AF.Exp, scale=NLW), r=[pk], w=[eWik])
            p.op("dve", lambda e, cx=cx, ps=ps, sgw=sgw: e.tensor_tensor(out=cx[:], in0=ps[0:64, :], in1=sgw[:], op=ALU.subtract), r=[pk, sgwk], w=[cxk])
            p.op("act", lambda e, eWx=eWx, cx=cx: e.activation(out=eWx[:], in_=cx[:], func=AF.Exp, scale=-NLW), r=[cxk], w=[eWxk])
            rt, rtk = f64["rt"].next()
            kt, ktk = f64["kt"].next()
            bt, btk = f64["bt"].next()
            at, atk = f64["at"].next()
            p.op("dve", lambda e, rt=rt, r_=r_, eW=eW: e.tensor_tensor(out=rt[:], in0=r_, in1=eW[:], op=ALU.mult), r=[xk, eWk], w=[rtk])
            p.op("pool", lambda e, kt=kt, kd=kd, eWi=eWi: e.tensor_tensor(out=kt[:], in0=kd[:], in1=eWi[:], op=ALU.mult), r=[kdk, eWik], w=[ktk])
            p.op("dve", lambda e, bt=bt, kk=kk, alp=alp: e.tensor_tensor(out=bt[:], in0=kk[:], in1=alp[:], op=ALU.mult), r=[kkk, alpk], w=[btk])
            p.op("dve", lambda e, bt=bt, eWi=eWi: e.tensor_tensor(out=bt[:], in0=bt[:], in1=eWi[:], op=ALU.mult), r=[btk, eWik], w=[btk])
            p.op("dve", lambda e, at=at, kk=kk, eWx=eWx: e.scalar_tensor_tensor(out=at[:], in0=kk[:], scalar=-1.0, in1=eWx[:], op0=ALU.mult, op1=ALU.mult),
                 r=[kkk, eWxk], w=[atk])
            toks = {}
            for nm, src, sk in (("Ktok", kt[:], ktk), ("Btok", bt[:], btk), ("Vtok", v_, xk)):
                yield
                ps, pk = psA.next()
                tr(ps[:, 0:64], pk, src, 64, [sk])
                tt, ttk = tokr[nm].next()
                p.op("act" if nm != "Vtok" else "dve", (lambda e, tt=tt, ps=ps: e.copy(out=tt[:], in_=ps[:, 0:64])) if nm != "Vtok" else
                     (lambda e, tt=tt, ps=ps: e.tensor_copy(out=tt[:], in_=ps[:, 0:64])), r=[pk], w=[ttk])
                toks[nm] = (tt, ttk)
            Ktok, Ktokk = toks["Ktok"]
            Btok, Btokk = toks["Btok"]
            Vtok, Vtokk = toks["Vtok"]
            def gram(nm, lhsT, lk, rhs, rk2, mask):
                ps, pk = psA.next()
                mm(ps[:, :], pk, lhsT, rhs, [lk, rk2])
                m, mk = (sqh[h][nm] if nm in sqh[h] else sqm[nm]).next()
                p.op("dve", lambda e: e.tensor_tensor(out=m[:], in0=ps[:, :], in1=C[mask][:], op=ALU.mult), r=[pk, "masks"], w=[mk])
                return m, mk
            At, Atk = gram("At", bt[:], btk, at[:], atk, f"strict{d}")
            yield
            Am, Amk = gram("Am", at[:], atk, bt[:], btk, f"strict{1 - d}")
            yield
            Akt, Aktk = gram("Akt", kt[:], ktk, at[:], atk, f"strict{d}")
            yield
            Mrbt, Mrbtk = gram("Mrbt", bt[:], btk, rt[:], rtk, f"incl{d}")
            yield
            Mrkt, Mrktk = gram("Mrkt", kt[:], ktk, rt[:], rtk, f"incl{d}")
            yield
            Pt, Ptk = sqh[h]["Pt"].next()
            p.op("pool", lambda e, Pt=Pt, At=At: e.tensor_tensor(out=Pt[:], in0=At[:], in1=C["ident"][:], op=ALU.add), r=[Atk, "ident"], w=[Ptk])
            for step in range(6):
                yield
                ps, pk = psA.next()
                mm(ps[:, :], pk, At[:], Am[:], [Atk, Amk])
                Am2, Am2k = sqh[h]["Am"].next()
                p.op("act", lambda e, Am2=Am2, ps=ps: e.copy(out=Am2[:], in_=ps[:, :]), r=[pk], w=[Am2k])
                if step < 5:
                    yield
                    ps, pk = psA.next()
                    mm(ps[:, :], pk, Am[:], At[:], [Atk, Amk])
                    At2, At2k = sqh[h]["At"].next()
                    p.op("dve", lambda e, At2=At2, ps=ps: e.tensor_copy(out=At2[:], in_=ps[:, :]), r=[pk], w=[At2k])
                yield
                ps, pk = psA.next()
                mm(ps[:, :], pk, Am2[:], Pt[:], [Am2k, Ptk])
                Pt2, Pt2k = sqh[h]["Pt"].next()
                p.op("dve", lambda e, Pt2=Pt2, ps=ps, Pt=Pt: e.tensor_tensor(out=Pt2[:], in0=ps[:, :], in1=Pt[:], op=ALU.add), r=[pk, Ptk], w=[Pt2k])
                Pt, Ptk = Pt2, Pt2k
                Am, Amk = Am2, Am2k
                if step < 5:
                    At, Atk = At2, At2k
            yield
            ps, pk = psA.next()
            mm(ps[:, 0:64], pk, at[:], Tt[:], [atk, Tk], start=True, stop=False)
            mm(ps[:, 0:64], pk, Akt[:], Vtok[:], [Aktk, Vtokk], start=False, stop=True)
            Xs, Xsk = tokr["Xs"].next()
            p.op("act", lambda e, Xs=Xs, ps=ps: e.copy(out=Xs[:], in_=ps[:, 0:64]), r=[pk], w=[Xsk])
            yield
            ps, pk = psA.next()
            mm(ps[:, 0:64], pk, Pt[:], Xs[:], [Ptk, Xsk])
            Us, Usk = tokr["Us"].next()
            p.op("dve", lambda e, Us=Us, ps=ps: e.tensor_copy(out=Us[:], in_=ps[:, 0:64]), r=[pk], w=[Usk])
            yield
            ps, pk = psA.next()
            mm(ps[0:64, :], pk, Tt[:], rt[:], [Tk, rtk], start=True, stop=False)
            mm(ps[0:64, :], pk, Us[:], Mrbt[:], [Usk, Mrbtk], start=False, stop=False)
            mm(ps[0:64, :], pk, Vtok[:], Mrkt[:], [Vtokk, Mrktk], start=False, stop=True)
            yo, yok = f64["yo"].next()
            p.op("act", lambda e, yo=yo, ps=ps: e.copy(out=yo[:], in_=ps[0:64, :]), r=[pk], w=[yok])
            p.dma("sp", lambda e, yo=yo, d=d, hs=hs, sl=sl: e.dma_start(out=YT[d, hs, sl], in_=yo[:]), r=[yok], w=["YTR"])
            yield
            ps, pk = psA.next()
            mm(ps[0:64, 0:64], pk, Btok[:], Us[:], [Btokk, Usk], start=True, stop=False)
            mm(ps[0:64, 0:64], pk, Ktok[:], Vtok[:], [Ktokk, Vtokk], start=False, stop=True)
            t2, t2k = tw.next()
            p.op("dve", lambda e, t2=t2, Tt=Tt, eW=eW, tend=tend: e.tensor_scalar(out=t2[:], in0=Tt[:], scalar1=eW[:, tend:tend + 1], scalar2=None, op0=ALU.mult),
                 r=[Tk, eWk], w=[t2k])
            p.op("dve", lambda e, t2=t2, Tt=Tt, eW=eW, ps=ps, tend=tend: e.scalar_tensor_tensor(
                out=Tt[:], in0=ps[0:64, 0:64], scalar=eW[:, tend:tend + 1], in1=t2[:], op0=ALU.mult, op1=ALU.add), r=[pk, eWk, t2k], w=[Tk])


        def drive(gens):
            gens = list(gens)
            while gens:
                for g_ in list(gens):
                    try:
                        next(g_)
                    except StopIteration:
                        gens.remove(g_)

        for i in range(nt):
            for d in range(2):
                c = order[d][i]
                t0 = c * 128
                tend = 127 if d == 0 else 0
                sl = slice(t0, t0 + 128)
                wat, wak = wa.next()
                p.dma("sp", lambda e, wat=wat, sl=sl: e.dma_start(out=wat[:, 0, :], in_=SMT[1536:1600, sl]), w=[wak])
                p.dma("sp", lambda e, wat=wat, sl=sl: e.dma_start(out=wat[:, 1, :], in_=SMT[1600:1664, sl]), w=[wak])
                p.op("act", lambda e, wat=wat: e.activation(out=wat[:, 0, :], in_=wat[:, 0, :], func=AF.Tanh), r=[wak], w=[wak])
                drive([unit(d, h, c, wat, wak) for h in range(8)])


def phase_rwkv_finish(p, C, io, n_tok):
    SMT, YT, RK, MIXT = io["SMT"], io["YTR"], io["RK"], io["MIXT"]
    with p.phase():
        g2a = p.sb("g2a", [128, 512], F32)
        g2b = p.sb("g2b", [32, 512], F32)
        p.dma("sp", lambda e: e.dma_start(out=g2a[:], in_=io["rw_g2"][0:128, :]), w=["fw"])
        p.dma("sp", lambda e: e.dma_start(out=g2b[:], in_=io["rw_g2"][128:160, :]), w=["fw"])
        lnw = p.sb("lnw", [64, 8], F32)
        lnb = p.sb("lnb", [64, 8], F32)
        colload(p, lnw, "fw", io["rw_ln_w"], 8)
        colload(p, lnb, "fw", io["rw_ln_b"], 8)
        sga = Ring(p, "sga", [128, 512], n=2)
        sgb = Ring(p, "sgb", [32, 512], n=2)
        R = {nm: Ring(p, nm, [64, 512], n=2) for nm in ("fy0", "fy1", "fr0", "fr1", "fv", "fyc", "fsq", "frs", "fbn")}
        ob = Ring(p, "fob", [64, 512], BF16, n=2)
        psF = Ring(p, "psG", [64, 512], n=4, psum=True)
        for t0 in range(0, n_tok, 512):
            N = min(512, n_tok - t0)
            sl = slice(t0, t0 + N)
            a_, ak = sga.next()
            b_, bk = sgb.next()
            p.dma("sp", lambda e, a_=a_, sl=sl, N=N: e.dma_start(out=a_[:, 0:N], in_=SMT[1664:1792, sl]), w=[ak])
            p.dma("sp", lambda e, b_=b_, sl=sl, N=N: e.dma_start(out=b_[:, 0:N], in_=SMT[1792:1824, sl]), w=[bk])
            p.op("act", lambda e, a_=a_, N=N: e.activation(out=a_[:, 0:N], in_=a_[:, 0:N], func=AF.Sigmoid), r=[ak], w=[ak])
            p.op("act", lambda e, b_=b_, N=N: e.activation(out=b_[:, 0:N], in_=b_[:, 0:N], func=AF.Sigmoid), r=[bk], w=[bk])
            for h in range(8):
                hs = slice(h * 64, (h + 1) * 64)
                y0, y0k = R["fy0"].next()
                y1, y1k = R["fy1"].next()
                r0, r0k = R["fr0"].next()
                r1, r1k = R["fr1"].next()
                v, vk = R["fv"].next()
                for (t, k_, src) in ((y0, y0k, YT[0, hs, sl]), (y1, y1k, YT[1, hs, sl]), (r0, r0k, RK[0, hs, sl]), (r1, r1k, RK[1, hs, sl]),
                                     (v, vk, SMT[1024 + h * 64:1024 + (h + 1) * 64, sl])):
                    p.dma("sp", lambda e, t=t, src=src, N=N: e.dma_start(out=t[:, 0:N], in_=src), w=[k_])
                p.op("dve", lambda e, y0=y0, y1=y1, N=N: e.tensor_tensor(out=y0[:, 0:N], in0=y0[:, 0:N], in1=y1[:, 0:N], op=ALU.add), r=[y0k, y1k], w=[y0k])
                p.op("pool", lambda e, r0=r0, r1=r1, N=N: e.tensor_tensor(out=r0[:, 0:N], in0=r0[:, 0:N], in1=r1[:, 0:N], op=ALU.add), r=[r0k, r1k], w=[r0k])
                ps, pk = psF.next()
                p.op("pe", lambda e, ps=ps, y0=y0, N=N: e.matmul(ps[:, 0:N], lhsT=C["ones64m"][0:64, 0:64], rhs=y0[:, 0:N], start=True, stop=True), r=[y0k, "masks"], w=[pk])
                yc, yck = R["fyc"].next()
                p.op("dve", lambda e, yc=yc, y0=y0, ps=ps, N=N: e.tensor_tensor(out=yc[:, 0:N], in0=y0[:, 0:N], in1=ps[:, 0:N], op=ALU.subtract), r=[y0k, pk], w=[yck])
                sq, sqk = R["fsq"].next()
                p.op("act", lambda e, sq=sq, yc=yc, N=N: e.activation(out=sq[:, 0:N], in_=yc[:, 0:N], func=AF.Square), r=[yck], w=[sqk])
                ps, pk = psF.next()
                p.op("pe", lambda e, ps=ps, sq=sq, N=N: e.matmul(ps[:, 0:N], lhsT=C["ones64m"][0:64, 0:64], rhs=sq[:, 0:N], start=True, stop=True), r=[sqk, "masks"], w=[pk])
                rs, rsk = R["frs"].next()
                p.op("dve", lambda e, rs=rs, ps=ps, N=N: e.tensor_scalar(out=rs[:, 0:N], in0=ps[:, 0:N], scalar1=64e-5, scalar2=None, op0=ALU.add), r=[pk], w=[rsk])
                p.op("act", lambda e, rs=rs, N=N: e.activation(out=rs[:, 0:N], in_=rs[:, 0:N], func=AF.Sqrt), r=[rsk], w=[rsk])
                p.op("dve", lambda e, rs=rs, N=N: e.reciprocal(out=rs[:, 0:N], in_=rs[:, 0:N]), r=[rsk], w=[rsk])
                p.op("dve", lambda e, yc=yc, rs=rs, N=N: e.tensor_tensor(out=yc[:, 0:N], in0=yc[:, 0:N], in1=rs[:, 0:N], op=ALU.mult), r=[yck, rsk], w=[yck])
                p.op("dve", lambda e, yc=yc, h=h, N=N: e.tensor_scalar(out=yc[:, 0:N], in0=yc[:, 0:N], scalar1=lnw[:, h:h + 1], scalar2=lnb[:, h:h + 1],
                                                                      op0=ALU.mult, op1=ALU.add), r=[yck, "fw"], w=[yck])
                ps, pk = psF.next()
                p.op("pe", lambda e, ps=ps, r0=r0, N=N: e.matmul(ps[:, 0:N], lhsT=C["ones64"][0:64, 0:64], rhs=r0[:, 0:N], start=True, stop=True), r=[r0k, "masks"], w=[pk])
                bn, bnk = R["fbn"].next()
                p.op("dve", lambda e, bn=bn, ps=ps, v=v, N=N: e.tensor_tensor(out=bn[:, 0:N], in0=ps[:, 0:N], in1=v[:, 0:N], op=ALU.mult), r=[pk, vk], w=[bnk])
                p.op("pool", lambda e, bn=bn, yc=yc, N=N: e.tensor_tensor(out=bn[:, 0:N], in0=bn[:, 0:N], in1=yc[:, 0:N], op=ALU.add), r=[bnk, yck], w=[bnk])
                ps, pk = psF.next()
                p.op("pe", lambda e, ps=ps, a_=a_, hs=hs, N=N: e.matmul(ps[:, 0:N], lhsT=g2a[:, hs], rhs=a_[:, 0:N], start=True, stop=False), r=["fw", ak], w=[pk])
                p.op("pe", lambda e, ps=ps, b_=b_, hs=hs, N=N: e.matmul(ps[:, 0:N], lhsT=g2b[:, hs], rhs=b_[:, 0:N], start=False, stop=True), r=["fw", bk], w=[pk])
                o, ok = ob.next()
                p.op("dve", lambda e, o=o, bn=bn, ps=ps, N=N: e.tensor_tensor(out=o[:, 0:N], in0=ps[:, 0:N], in1=bn[:, 0:N], op=ALU.mult), r=[pk, bnk], w=[ok])
                p.dma("sp", lambda e, o=o, h=h, sl=sl, N=N: e.dma_start(out=MIXT[512 + h * 64:512 + (h + 1) * 64, sl], in_=o[:, 0:N]), r=[ok], w=["MIXT"])


def phase_outproj(p, C, io, l, w_out, n_ctx, n_tok, t_lo=0):
    xres, MIXT, modrow = io["xres"], io["MIXT"], io["modrow"]
    with p.phase():
        wbf = p.sb("woutbf", [128, KC, D], BF16)
        stage = [p.sb(f"wostage{i}", [128, D], F32) for i in range(2)]
        load_cast_weight(p, wbf, "woutbf", w_out, KC, D, stage, "wostage")
        gt = p.sb("gt1row", [128, 2, D], F32)
        for r in range(2):
            p.dma("sp", lambda e, r=r: e.dma_start(out=gt[:, r, :], in_=modrow[l, r, 2048:3072].partition_broadcast(128)), w=["gt1row"])
        mx = Ring(p, "mx", [128, KC, 128], BF16, n=2)
        xt = Ring(p, "oxt", [128, D], n=2)
        tmp = Ring(p, "otmp", [128, D], n=2)
        pso = Ring(p, "pso", [128, 512], n=4, psum=True)
        for ti in range(t_lo // 128, n_tok // 128):
            r = 1 if ti * 128 < n_ctx else 0
            sl = slice(ti * 128, (ti + 1) * 128)
            m, mk = mx.next()
            p.dma("sp", lambda e, m=m, sl=sl: e.dma_start(out=m[:], in_=MIXT[:, sl].rearrange("(c q) t -> q c t", q=128)), w=[mk])
            x, xk = xt.next()
            p.dma("sp", lambda e, x=x, sl=sl: e.dma_start(out=x[:], in_=xres[sl, :]), w=[xk])
            t, tk = tmp.next()
            for n in range(2):
                ps, pk = pso.next()
                for kc in range(KC):
                    p.op("pe", lambda e, ps=ps, m=m, kc=kc, n=n: e.matmul(ps[:], lhsT=m[:, kc, :], rhs=wbf[:, kc, n * 512:(n + 1) * 512],
                                                                          start=(kc == 0), stop=(kc == KC - 1)), r=[mk, "woutbf"], w=[pk])
                p.op("dve", lambda e, t=t, ps=ps, n=n, r=r: e.tensor_tensor(out=t[:, n * 512:(n + 1) * 512], in0=ps[:], in1=gt[:, r, n * 512:(n + 1) * 512], op=ALU.mult),
                     r=[pk, "gt1row"], w=[tk])
            p.op("pool", lambda e, t=t, x=x: e.tensor_tensor(out=t[:], in0=t[:], in1=x[:], op=ALU.add), r=[tk, xk], w=[tk])
            p.dma("sp", lambda e, t=t, sl=sl: e.dma_start(out=xres[sl, :], in_=t[:]), r=[tk], w=["xres"])


def phase_final(p, C, io, n_ctx, n_tok):
    xres, out = io["xres"], io["out"]
    with p.phase():
        g = p.sb("fng", [128, D], F32)
        p.dma("sp", lambda e: e.dma_start(out=g[:], in_=io["final_norm_g"].partition_broadcast(128)), w=["fng"])
        xt = Ring(p, "fxt", [128, D], n=3)
        junk = p.sb("fjunk", [128, D], BF16)
        ss = Ring(p, "fss", [128, 1], n=3)
        rs = Ring(p, "frst", [128, 1], n=3)
        for ti in range(n_ctx // 128, n_tok // 128):
            sl = slice(ti * 128, (ti + 1) * 128)
            x, xk = xt.next()
            s, sk = ss.next()
            r_, rk = rs.next()
            p.dma("sp", lambda e, x=x, sl=sl: e.dma_start(out=x[:], in_=xres[sl, :]), w=[xk])
            p.op("act", lambda e, x=x, s=s: e.activation(out=junk[:], in_=x[:], func=AF.Square, accum_out=s[:, 0:1]), r=[xk], w=["fjunk", sk])
            p.op("dve", lambda e, r_=r_, s=s: e.tensor_scalar(out=r_[:], in0=s[:], scalar1=1.0 / D, scalar2=EPS, op0=ALU.mult, op1=ALU.add), r=[sk], w=[rk])
            p.op("act", lambda e, r_=r_: e.activation(out=r_[:], in_=r_[:], func=AF.Sqrt), r=[rk], w=[rk])
            p.op("dve", lambda e, r_=r_: e.reciprocal(out=r_[:], in_=r_[:]), r=[rk], w=[rk])
            p.op("dve", lambda e, x=x, r_=r_: e.scalar_tensor_tensor(out=x[:], in0=x[:], scalar=r_[:, 0:1], in1=g[:], op0=ALU.mult, op1=ALU.mult),
                 r=[xk, rk, "fng"], w=[xk])
            o0 = ti * 128 - n_ctx
            p.dma("sp", lambda e, x=x, o0=o0: e.dma_start(out=out[o0:o0 + 128, :], in_=x[:]), r=[xk], w=["out"])

RST = 9

DE = 1024
SW_LIMIT = 7.0
SW_ALPHA = 1.702


def phase_moe_cast(p, C, io, l, NE):
    gu, dn, WGU, WDN = io["moe_gu_w"], io["moe_down_w"], io["WGU"], io["WDN"]
    with p.phase():
        sg = Ring(p, "cg32", [128, 2, 2048], n=2)
        sb = Ring(p, "cg16", [128, 2, 2048], BF16, n=2)
        sd = Ring(p, "cd32", [128, 2, 1024], n=2)
        sdb = Ring(p, "cd16", [128, 2, 1024], BF16, n=2)
        engs = ("dve", "pool", "act")
        it = 0
        for e_ in range(NE):
            for k2 in range(4):
                for (src, dst, r32, r16) in ((gu, WGU, sg, sb), (dn, WDN, sd, sdb)):
                    a, ak = r32.next()
                    b, bk = r16.next()
                    rows = slice(k2 * 256, (k2 + 1) * 256)
                    p.dma("sp", lambda e, a=a, src=src, e_=e_, rows=rows: e.dma_start(out=a[:], in_=src[l, e_, rows, :].rearrange("(c q) n -> q c n", q=128)), w=[ak])
                    eng = engs[it % 3]
                    it += 1
                    if eng == "act":
                        p.op("act", lambda e, a=a, b=b: e.copy(out=b[:], in_=a[:]), r=[ak], w=[bk])
                    else:
                        p.op(eng, lambda e, a=a, b=b: e.tensor_copy(out=b[:], in_=a[:]), r=[ak], w=[bk])
                    p.dma("act", lambda e, b=b, dst=dst, e_=e_, rows=rows: e.dma_start(out=dst[e_, rows, :].rearrange("(c q) n -> q c n", q=128), in_=b[:]), r=[bk], w=["WBF"])


def phase_moe_route(p, C, io, G, l, NE, t_lo, t_hi, n_ctx):
    xres, HT, GATE = io["xres"], io["HT"], io["GATE"]
    with p.phase():
        T = norm_scratch(p)
        wr = p.sb("wr", [128, KC, NE], F32)
        p.dma("sp", lambda e: e.dma_start(out=wr[:], in_=io["router_w"][l].rearrange("(c q) n -> q c n", q=128)), w=["wr"])
        br = p.sb("br", [128, NE], F32)
        p.dma("sp", lambda e: e.dma_start(out=br[:], in_=io["router_b"][l].partition_broadcast(128)), w=["wr"])
        xt = Ring(p, "mxt", [128, D], n=2)
        hb = Ring(p, "mhb", [128, KC, 128], BF16, n=2)
        h32 = Ring(p, "mh32", [128, KC, 128], n=2)
        lg = Ring(p, "mlg", [128, NE], n=2)
        ex = Ring(p, "mex", [128, NE], n=2)
        mk = Ring(p, "mmk", [128, NE], n=2)
        m8 = Ring(p, "mm8", [128, 8], n=2)
        sc = Ring(p, "msc", [128, 2], n=2)
        psr = p.ps("psr", [128, NE], F32)
        for ti in range(t_lo // 128, t_hi // 128):
            r = 1 if ti * 128 < n_ctx else 0
            sl = slice(ti * 128, (ti + 1) * 128)
            x, xk = xt.next()
            p.dma("sp", lambda e, x=x, sl=sl: e.dma_start(out=x[:], in_=xres[sl, :]), w=[xk])
            b, bk = hb.next()
            f, fk = h32.next()
            norm_tile(p, C, T, xk, x, G["A2"][:, l, r, :], G["B2"][:, l, r, :], lambda kc, b=b: b[:, kc, :], bk,
                      hT32_dst=lambda kc, f=f: f[:, kc, :])
            fk32 = bk + "32"
            p.dma("sp", lambda e, b=b, sl=sl: e.dma_start(out=HT[:, sl].rearrange("(c q) t -> q c t", q=128), in_=b[:]), r=[bk], w=["HT"])
            if RST < 2:
                continue
            for kc in range(KC):
                p.op("pe", lambda e, f=f, kc=kc: e.matmul(psr[:], lhsT=f[:, kc, :], rhs=wr[:, kc, :], start=(kc == 0), stop=(kc == KC - 1)),
                     r=[fk32, "wr"], w=["psr"])
            g, gk = lg.next()
            p.op("dve", lambda e, g=g: e.tensor_tensor(out=g[:], in0=psr[:], in1=br[:], op=ALU.add), r=["psr", "wr"], w=[gk])
            if RST < 3:
                continue
            m, mk8 = m8.next()
            p.op("dve", lambda e, m=m, g=g: e.max(out=m[:], in_=g[:]), r=[gk], w=[mk8])
            msk, mskk = mk.next()
            p.op("dve", lambda e, msk=msk, g=g, m=m: e.tensor_scalar(out=msk[:], in0=g[:], scalar1=m[:, 3:4], scalar2=None, op0=ALU.is_ge), r=[gk, mk8], w=[mskk])
            if RST < 4:
                continue
            s, sk = sc.next()
            p.op("dve", lambda e, s=s, m=m: e.tensor_scalar(out=s[:, 0:1], in0=m[:, 0:1], scalar1=-1.0, scalar2=None, op0=ALU.mult), r=[mk8], w=[sk])
            x_, xk_ = ex.next()
            p.op("act", lambda e, x_=x_, g=g, s=s: e.activation(out=x_[:], in_=g[:], func=AF.Exp, bias=s[:, 0:1]), r=[gk, sk], w=[xk_])
            p.op("dve", lambda e, x_=x_, msk=msk: e.tensor_tensor(out=x_[:], in0=x_[:], in1=msk[:], op=ALU.mult), r=[xk_, mskk], w=[xk_])
            p.op("dve", lambda e, s=s, x_=x_: e.reduce_sum(out=s[:, 1:2], in_=x_[:], axis=AX.X), r=[xk_], w=[sk])
            p.op("dve", lambda e, s=s: e.reciprocal(out=s[:, 1:2], in_=s[:, 1:2]), r=[sk], w=[sk])
            p.op("dve", lambda e, x_=x_, s=s: e.tensor_scalar(out=x_[:], in0=x_[:], scalar1=s[:, 1:2], scalar2=None, op0=ALU.mult), r=[xk_, sk], w=[xk_])
            p.dma("sp", lambda e, x_=x_, sl=sl: e.dma_start(out=GATE[sl, :], in_=x_[:]), r=[xk_], w=["GATE"])


def phase_moe_experts(p, C, io, l, NE, t_lo, t_hi, n_ctx, TB=512):
    xres, HT, GATE, WGU, WDN, modrow = io["xres"], io["HT"], io["GATE"], io["WGU"], io["WDN"], io["modrow"]
    with p.phase():
        bg_rows = p.sb("bg_rows", [NE, 2048], F32)
        p.dma("sp", lambda e: e.dma_start(out=bg_rows[:], in_=io["moe_gu_b"][l]), w=["bg_rows"])
        bgc = p.sb("bgc", [128, 16, NE], F32)
        pst = p.ps("pst", [128, 16, NE], F32)
        for c in range(16):
            p.op("pe", lambda e, c=c: e.transpose(pst[:, c, :], bg_rows[:, c * 128:(c + 1) * 128], C["ident"][0:NE, 0:NE]), r=["bg_rows", "ident"], w=["pst"])
        p.op("dve", lambda e: e.tensor_copy(out=bgc[:], in_=pst[:]), r=["pst"], w=["bgc"])
        ones_b = p.sb("ones_b", [1, 128], BF16)
        p.op("dve", lambda e: e.memset(ones_b[:], 1.0), w=["ones_b"])
        gt = p.sb("gt2row", [128, 2, D], F32)
        for r in range(2):
            p.dma("sp", lambda e, r=r: e.dma_start(out=gt[:, r, :], in_=modrow[l, r, 5120:6144].partition_broadcast(128)), w=["gt2row"])
        wg = Ring(p, "wg", [128, KC, 2048], BF16, n=2)
        wd = Ring(p, "wd", [128, KC, 1024], BF16, n=2)
        bd32 = Ring(p, "bd32", [1, 1024], F32, n=2)
        bd16 = Ring(p, "bd16", [1, 1024], BF16, n=2)
        acc = p.sb("macc", [128, TB // 128, D], F32)
        hT = p.sb("mhT", [128, KC, TB], BF16)
        gate = p.sb("mgate", [128, TB // 128, NE], F32)
        actT = Ring(p, "actT", [128, KC, 512], BF16, n=2)
        t1 = Ring(p, "et1", [128, 512], n=2)
        sgm = Ring(p, "esg", [128, 512], n=2)
        t2 = Ring(p, "et2", [128, 512], n=2)
        xt = Ring(p, "ext", [128, D], n=2)
        psg = Ring(p, "psg", [128, 512], n=4, psum=True)
        psd = Ring(p, "psd", [128, 512], n=3, psum=True)
        t = t_lo
        while t < t_hi:
            nb = min(TB, t_hi - t)
            ntile = nb // 128
            p.dma("sp", lambda e, t=t, nb=nb: e.dma_start(out=hT[:, :, 0:nb], in_=HT[:, t:t + nb].rearrange("(c q) n -> q c n", q=128)), w=["mhT"])
            p.dma("sp", lambda e, t=t, nb=nb, ntile=ntile: e.dma_start(out=gate[:, 0:ntile, :], in_=GATE[t:t + nb, :].rearrange("(j q) n -> q j n", q=128)), w=["mgate"])
            p.op("pool", lambda e: e.memset(acc[:], 0.0), w=["macc"])
            for e_ in range(NE):
                g_, gk = wg.next()
                d_, dk = wd.next()
                b32, b32k = bd32.next()
                b16, b16k = bd16.next()
                p.dma("sp", lambda e, g_=g_, e_=e_: e.dma_start(out=g_[:], in_=WGU[e_].rearrange("(c q) n -> q c n", q=128)), w=[gk])
                p.dma("act", lambda e, d_=d_, e_=e_: e.dma_start(out=d_[:], in_=WDN[e_].rearrange("(c q) n -> q c n", q=128)), w=[dk])
                p.dma("sp", lambda e, b32=b32, e_=e_: e.dma_start(out=b32[:], in_=io["moe_down_b"][l, e_:e_ + 1, :]), w=[b32k])
                p.op("pool", lambda e, b32=b32, b16=b16: e.tensor_copy(out=b16[:], in_=b32[:]), r=[b32k], w=[b16k])
                for s0 in range(0, nb, 512):
                    N = min(512, nb - s0)
                    aT, aTk = actT.next()
                    for c in range(8):
                        pa, pak = psg.next()
                        pb, pbk = psg.next()
                        for (ps_, pk_, col0) in ((pa, pak, c * 128), (pb, pbk, DE + c * 128)):
                            for kc in range(KC):
                                p.op("pe", lambda e, ps_=ps_, g_=g_, kc=kc, col0=col0, s0=s0, N=N: e.matmul(
                                    ps_[:, 0:N], lhsT=g_[:, kc, col0:col0 + 128], rhs=hT[:, kc, s0:s0 + N], start=(kc == 0), stop=(kc == KC - 1)),
                                    r=[gk, "mhT"], w=[pk_])
                        a1, a1k = t1.next()
                        p.op("dve", lambda e, a1=a1, pa=pa, c=c, e_=e_, N=N: e.tensor_scalar(out=a1[:, 0:N], in0=pa[:, 0:N], scalar1=bgc[:, c, e_:e_ + 1], scalar2=SW_LIMIT,
                                                                                          op0=ALU.add, op1=ALU.min), r=[pak, "bgc"], w=[a1k])
                        s_, sk_ = sgm.next()
                        p.op("act", lambda e, s_=s_, a1=a1, N=N: e.activation(out=s_[:, 0:N], in_=a1[:, 0:N], func=AF.Sigmoid, scale=SW_ALPHA), r=[a1k], w=[sk_])
                        a2, a2k = t2.next()
                        p.op("dve", lambda e, a2=a2, pb=pb, c=c, e_=e_, N=N: e.tensor_scalar(out=a2[:, 0:N], in0=pb[:, 0:N], scalar1=bgc[:, 8 + c, e_:e_ + 1], scalar2=SW_LIMIT,
                                                                                          op0=ALU.add, op1=ALU.min), r=[pbk, "bgc"], w=[a2k])
                        p.op("pool", lambda e, a2=a2, N=N: e.tensor_scalar(out=a2[:, 0:N], in0=a2[:, 0:N], scalar1=-SW_LIMIT, scalar2=1.0, op0=ALU.max, op1=ALU.add),
                             r=[a2k], w=[a2k])
                        p.op("pool", lambda e, a1=a1, s_=s_, N=N: e.tensor_tensor(out=a1[:, 0:N], in0=a1[:, 0:N], in1=s_[:, 0:N], op=ALU.mult), r=[a1k, sk_], w=[a1k])
                        p.op("dve", lambda e, aT=aT, a1=a1, a2=a2, c=c, N=N: e.tensor_tensor(out=aT[:, c, 0:N], in0=a1[:, 0:N], in1=a2[:, 0:N], op=ALU.mult),
                             r=[a1k, a2k], w=[aTk])
                    for j in range(N // 128):
                        tile_i = (s0 // 128) + j
                        for n in range(2):
                            pd, pdk = psd.next()
                            for kc in range(KC):
                                p.op("pe", lambda e, pd=pd, aT=aT, d_=d_, kc=kc, j=j, n=n: e.matmul(
                                    pd[:], lhsT=aT[:, kc, j * 128:(j + 1) * 128], rhs=d_[:, kc, n * 512:(n + 1) * 512], start=(kc == 0), stop=False),
                                    r=[aTk, dk], w=[pdk])
                            p.op("pe", lambda e, pd=pd, b16=b16, n=n: e.matmul(pd[:], lhsT=ones_b[:], rhs=b16[:, n * 512:(n + 1) * 512], start=False, stop=True),
                                 r=["ones_b", b16k], w=[pdk])
                            p.op("dve", lambda e, pd=pd, tile_i=tile_i, n=n, e_=e_: e.scalar_tensor_tensor(
                                out=acc[:, tile_i, n * 512:(n + 1) * 512], in0=pd[:], scalar=gate[:, tile_i, e_:e_ + 1],
                                in1=acc[:, tile_i, n * 512:(n + 1) * 512], op0=ALU.mult, op1=ALU.add), r=[pdk, "mgate", "macc"], w=["macc"])
            for j in range(ntile):
                tok = t + j * 128
                r = 1 if tok < n_ctx else 0
                x, xk = xt.next()
                p.dma("sp", lambda e, x=x, tok=tok: e.dma_start(out=x[:], in_=xres[tok:tok + 128, :]), w=[xk])
                p.op("dve", lambda e, j=j, r=r: e.tensor_tensor(out=acc[:, j, :], in0=acc[:, j, :], in1=gt[:, r, :], op=ALU.mult), r=["macc", "gt2row"], w=["macc"])
                p.op("pool", lambda e, x=x, j=j: e.tensor_tensor(out=x[:], in0=x[:], in1=acc[:, j, :], op=ALU.add), r=[xk, "macc"], w=[xk])
                p.dma("sp", lambda e, x=x, tok=tok: e.dma_start(out=xres[tok:tok + 128, :], in_=x[:]), r=[xk], w=["xres"])
            t += nb

import math

MLA_SCALE = 192 ** -0.5


def make_rope_consts(p, C):
    f, q = C["iota_f"], C["iota_p"]
    d = p.sb("rp_d", [64, 64], F32)
    e1 = p.sb("rp_e1", [64, 64], F32)
    e2 = p.sb("rp_e2", [64, 64], F32)
    ge = p.sb("rp_ge", [64, 64], F32)
    PR = p.sb("rp_PR", [64, 64], F32)
    k = ["rope_c"]
    p.op("dve", lambda e: e.tensor_scalar(out=d[:], in0=f[0:64, 0:64], scalar1=q[0:64, 0:1], scalar2=None, op0=ALU.subtract), r=["iota_f", "iota_p"], w=k)
    p.op("dve", lambda e: e.tensor_scalar(out=e1[:], in0=d[:], scalar1=16.0, scalar2=None, op0=ALU.is_equal), r=k, w=k)
    p.op("dve", lambda e: e.tensor_scalar(out=e2[:], in0=d[:], scalar1=-16.0, scalar2=None, op0=ALU.is_equal), r=k, w=k)
    ii = p.sb("rp_ii", [64, 64], I32)
    p.op("pool", lambda e: e.iota(ii[:], pattern=[[1, 64]], base=0, channel_multiplier=0), w=["rp_ii"])
    p.op("dve", lambda e: e.tensor_single_scalar(out=ii[:], in_=ii[:], scalar=16, op=ALU.bitwise_and), r=["rp_ii"], w=["rp_ii"])
    p.op("dve", lambda e: e.tensor_copy(out=ge[:], in_=ii[:]), r=["rp_ii"], w=k)
    p.op("dve", lambda e: e.tensor_scalar(out=ge[:], in0=ge[:], scalar1=1.0 / 16, scalar2=None, op0=ALU.mult), r=k, w=k)
    p.op("dve", lambda e: e.tensor_tensor(out=e1[:], in0=e1[:], in1=ge[:], op=ALU.mult), r=k, w=k)
    p.op("dve", lambda e: e.tensor_scalar(out=ge[:], in0=ge[:], scalar1=-1.0, scalar2=1.0, op0=ALU.mult, op1=ALU.add), r=k, w=k)
    p.op("dve", lambda e: e.tensor_tensor(out=e2[:], in0=e2[:], in1=ge[:], op=ALU.mult), r=k, w=k)
    p.op("dve", lambda e: e.tensor_tensor(out=PR[:], in0=e1[:], in1=e2[:], op=ALU.subtract), r=k, w=k)
    invf = p.sb("rp_invf", [64, 1], F32)
    isrow = p.sb("rp_isrow", [64, 1], F32)
    pi_ = p.sb("rp_pi", [64, 1], I32)
    p.op("pool", lambda e: e.iota(pi_[:], pattern=[[1, 1]], base=0, channel_multiplier=1), w=["rp_pi"])
    p.op("dve", lambda e: e.tensor_single_scalar(out=pi_[:], in_=pi_[:], scalar=15, op=ALU.bitwise_and), r=["rp_pi"], w=["rp_pi"])
    p.op("dve", lambda e: e.tensor_copy(out=invf[:], in_=pi_[:]), r=["rp_pi"], w=k)
    p.op("act", lambda e: e.activation(out=invf[:], in_=invf[:], func=AF.Exp, scale=-math.log(10000.0) / 16.0), r=k, w=k)
    p.op("dve", lambda e: e.tensor_scalar(out=isrow[:], in0=q[0:64, 0:1], scalar1=32.0, scalar2=None, op0=ALU.is_lt), r=["iota_p"], w=k)
    C.update(PR=PR, invf=invf, isrow=isrow)


def phase_mla_proj(p, C, io, G, l, n_ctx, n_tok):
    xres = io["xres"]
    QN, QR, KN, KR, V = io["QN"], io["QR"], io["KN"], io["KRo"], io["Vt"]
    n_lat = n_tok - n_ctx
    with p.phase():
        T = norm_scratch(p)
        win = p.sb("mwin", [128, KC, 448], BF16)
        st = [p.sb(f"mst{i}", [128, 2048], F32) for i in range(2)]
        load_cast_weight(p, win, "mwin", io["od_w_in"], KC, 448, st, "mst")
        wq = p.sb("mwq", [128, 2, 1536], BF16)
        load_cast_weight(p, wq, "mwq", io["mla_wq_up"], 2, 1536, st, "mst")
        wkn = p.sb("mwkn", [128, 8, 128], BF16)
        wv = p.sb("mwv", [128, 8, 128], BF16)
        p.dma("sp", lambda e: e.dma_start(out=st[0][:, 0:2048], in_=io["mla_wkv_up"][:, :]), w=["mst0"])
        p.op("dve", lambda e: e.tensor_copy(out=wkn[:], in_=st[0][:, 0:2048].rearrange("q (h c) -> q h c", c=256)[:, :, 0:128]), r=["mst0"], w=["mwk"])
        p.op("pool", lambda e: e.tensor_copy(out=wv[:], in_=st[0][:, 0:2048].rearrange("q (h c) -> q h c", c=256)[:, :, 128:256]), r=["mst0"], w=["mwk"])
        qg = p.sb("mqg", [128, 2], F32)
        kg = p.sb("mkg", [128, 1], F32)
        p.dma("sp", lambda e: e.dma_start(out=qg[:], in_=io["mla_q_norm"].rearrange("(c q) -> q c", q=128), allow_slow_non_contiguous=True), w=["mg"])
        p.dma("sp", lambda e: e.dma_start(out=kg[:], in_=io["mla_kv_norm"].rearrange("(q o) -> q o", o=1)), w=["mg"])
        onesq = p.sb("monesq", [128, 128], F32)
        p.op("dve", lambda e: e.memset(onesq[:], 1.0 / 256), w=["monesq"])
        xts = Ring(p, "axt", [128, D], n=2)
        hT = p.sb("ahT", [128, KC, 512], BF16)
        cq = p.sb("acq", [128, 2, 512], F32)
        ckv = p.sb("ackv", [128, 512], F32)
        krr = p.sb("akrr", [64, 512], F32)
        sq = p.sb("asq", [128, 2, 512], F32)
        rs = Ring(p, "ars", [128, 512], n=2)
        cqn = p.sb("acqn", [128, 2, 512], BF16)
        ckvn = p.sb("ackvn", [128, 512], BF16)
        ang = p.sb("aang", [64, 512], F32)
        tti = p.sb("atti", [64, 512], I32)
        tt2 = p.sb("att2", [64, 512], I32)
        a2 = p.sb("aa2", [64, 512], F32)
        kf = p.sb("akf", [64, 512], F32)
        cosT = p.sb("acos", [64, 512], F32)
        sinT = p.sb("asin", [64, 512], F32)
        rx = Ring(p, "arx", [64, 512], n=2)
        ru = Ring(p, "aru", [64, 512], n=2)
        ob = Ring(p, "aob", [128, 512], BF16, n=3)
        vb = Ring(p, "avb", [128, 1024], BF16, n=2)
        pp = Ring(p, "app", [128, 512], n=5, psum=True)

        def rope(x, xk, N, scale, dst_ap, dst_key):
            p2, p2k = pp.next()
            p.op("pe", lambda e: e.matmul(p2[0:64, 0:N], lhsT=C["PR"][:], rhs=x, start=True, stop=True), r=["rope_c", xk], w=[p2k])
            u, uk = ru.next()
            p.op("dve", lambda e: e.tensor_tensor(out=u[:, 0:N], in0=p2[0:64, 0:N], in1=sinT[:, 0:N], op=ALU.mult), r=[p2k, "trig"], w=[uk])
            p.op("pool", lambda e: e.tensor_tensor(out=x, in0=x, in1=cosT[:, 0:N], op=ALU.mult), r=[xk, "trig"], w=[xk])
            if scale == 1.0:
                p.op("dve", lambda e: e.tensor_tensor(out=dst_ap, in0=x, in1=u[:, 0:N], op=ALU.add), r=[xk, uk], w=[dst_key])
            else:
                p.op("dve", lambda e: e.tensor_tensor(out=u[:, 0:N], in0=x, in1=u[:, 0:N], op=ALU.add), r=[xk, uk], w=[uk])
                p.op("dve", lambda e: e.tensor_scalar(out=dst_ap, in0=u[:, 0:N], scalar1=scale, scalar2=None, op0=ALU.mult), r=[uk], w=[dst_key])

        blocks = [(0, n_ctx, False)] + [(t, min(512, n_tok - t), True) for t in range(n_ctx, n_tok, 512)]
        def do_block(t0, N, is_lat):
            for j in range(N // 128):
                ti = t0 // 128 + j
                r = 0 if is_lat else 1
                x, xk = xts.next()
                p.dma("sp", lambda e, x=x, ti=ti: e.dma_start(out=x[:], in_=xres[ti * 128:(ti + 1) * 128, :]), w=[xk])
                norm_tile(p, C, T, xk, x, G["A1"][:, l, r, :], G["B1"][:, l, r, :], lambda kc, j=j: hT[:, kc, j * 128:(j + 1) * 128], "ahT")
            for (c0, cw, dst, dk) in ((0, 128, cq[:, 0, :], "acq"), (128, 128, cq[:, 1, :], "acq"), (256, 128, ckv[:, :], "ackv"), (384, 64, krr[:, :], "akrr")):
                ps, pk = pp.next()
                for kc in range(KC):
                    p.op("pe", lambda e, ps=ps, kc=kc, c0=c0, cw=cw, N=N: e.matmul(ps[0:cw, 0:N], lhsT=win[:, kc, c0:c0 + cw], rhs=hT[:, kc, 0:N],
                                                                                 start=(kc == 0), stop=(kc == KC - 1)), r=["mwin", "ahT"], w=[pk])
                p.op("act", lambda e, ps=ps, dst=dst, cw=cw, N=N: e.copy(out=dst[0:cw, 0:N], in_=ps[0:cw, 0:N]), r=[pk], w=[dk])

            def rmsn(src_chunks, src_key, ones_ap, gcols, dst_chunks, dst_key):
                n = len(src_chunks)
                for c in range(n):
                    p.op("act", lambda e, c=c: e.activation(out=sq[:, c, 0:N], in_=src_chunks[c], func=AF.Square), r=[src_key], w=["asq"])
                ps, pk = pp.next()
                for c in range(n):
                    p.op("pe", lambda e, ps=ps, c=c: e.matmul(ps[:, 0:N], lhsT=ones_ap, rhs=sq[:, c, 0:N], start=(c == 0), stop=(c == n - 1)),
                         r=["asq", "monesq", "masks"], w=[pk])
                r_, rk = rs.next()
                p.op("dve", lambda e: e.tensor_scalar(out=r_[:, 0:N], in0=ps[:, 0:N], scalar1=EPS, scalar2=None, op0=ALU.add), r=[pk], w=[rk])
                p.op("act", lambda e: e.activation(out=r_[:, 0:N], in_=r_[:, 0:N], func=AF.Sqrt), r=[rk], w=[rk])
                p.op("dve", lambda e: e.reciprocal(out=r_[:, 0:N], in_=r_[:, 0:N]), r=[rk], w=[rk])
                for c in range(n):
                    p.op("dve", lambda e, c=c: e.scalar_tensor_tensor(out=dst_chunks[c], in0=src_chunks[c], scalar=gcols[c], in1=r_[:, 0:N],
                                                                      op0=ALU.mult, op1=ALU.mult), r=[src_key, rk, "mg"], w=[dst_key])

            rmsn([ckv[:, 0:N]], "ackv", C["ones128"][:], [kg[:, 0:1]], [ckvn[:, 0:N]], "ackvn")
            if is_lat:
                rmsn([cq[:, 0, 0:N], cq[:, 1, 0:N]], "acq", onesq[:], [qg[:, 0:1], qg[:, 1:2]], [cqn[:, 0, 0:N], cqn[:, 1, 0:N]], "acqn")
                lat0 = t0 - n_ctx
                p.op("pool", lambda e, lat0=lat0: e.iota(tti[:, 0:N], pattern=[[1, N]], base=lat0, channel_multiplier=0), w=["atti"])
                p.op("dve", lambda e: e.tensor_single_scalar(out=tt2[:, 0:N], in_=tti[:, 0:N], scalar=63, op=ALU.bitwise_and), r=["atti"], w=["att2"])
                p.op("dve", lambda e: e.tensor_copy(out=cosT[:, 0:N], in_=tt2[:, 0:N]), r=["att2"], w=["trig"])
                p.op("dve", lambda e: e.tensor_single_scalar(out=tt2[:, 0:N], in_=tti[:, 0:N], scalar=6, op=ALU.arith_shift_right), r=["atti", "trig"], w=["att2"])
                p.op("dve", lambda e: e.tensor_copy(out=sinT[:, 0:N], in_=tt2[:, 0:N]), r=["att2"], w=["trig"])
                p.op("dve", lambda e: e.tensor_tensor(out=sinT[:, 0:N], in0=sinT[:, 0:N], in1=cosT[:, 0:N], op=ALU.subtract), r=["trig"], w=["trig"])
                p.op("dve", lambda e: e.scalar_tensor_tensor(out=ang[:, 0:N], in0=sinT[:, 0:N], scalar=C["isrow"][:, 0:1], in1=cosT[:, 0:N], op0=ALU.mult, op1=ALU.add),
                     r=["trig", "rope_c"], w=["aang"])
                p.op("dve", lambda e: e.tensor_scalar(out=ang[:, 0:N], in0=ang[:, 0:N], scalar1=C["invf"][:, 0:1], scalar2=None, op0=ALU.mult), r=["aang", "rope_c"], w=["aang"])
                for (dst, off) in ((sinT, 0.0), (cosT, 0.5 * math.pi)):
                    p.op("dve", lambda e, off=off: e.tensor_scalar(out=a2[:, 0:N], in0=ang[:, 0:N], scalar1=off, scalar2=None, op0=ALU.add), r=["aang"], w=["aa2"])
                    p.op("dve", lambda e: e.tensor_scalar(out=kf[:, 0:N], in0=a2[:, 0:N], scalar1=1.0 / (2 * math.pi), scalar2=None, op0=ALU.mult), r=["aa2"], w=["akf"])
                    p.op("dve", lambda e: e.tensor_copy(out=tt2[:, 0:N], in_=kf[:, 0:N]), r=["akf"], w=["att2"])
                    p.op("dve", lambda e: e.tensor_copy(out=kf[:, 0:N], in_=tt2[:, 0:N]), r=["att2"], w=["akf"])
                    p.op("dve", lambda e, dst=dst: e.scalar_tensor_tensor(out=dst[:, 0:N], in0=kf[:, 0:N], scalar=-2 * math.pi, in1=a2[:, 0:N], op0=ALU.mult, op1=ALU.add),
                         r=["akf", "aa2"], w=["trig"])
                    p.op("dve", lambda e, dst=dst: e.tensor_scalar(out=dst[:, 0:N], in0=dst[:, 0:N], scalar1=-math.pi, scalar2=math.pi, op0=ALU.max, op1=ALU.min), r=["trig"], w=["trig"])
                    p.op("act", lambda e, dst=dst: e.activation(out=dst[:, 0:N], in_=dst[:, 0:N], func=AF.Sin), r=["trig"], w=["trig"])
                lsl = slice(lat0, lat0 + N)
                for h in range(8):
                    ps, pk = pp.next()
                    for kc in range(2):
                        p.op("pe", lambda e, ps=ps, kc=kc, h=h: e.matmul(ps[:, 0:N], lhsT=wq[:, kc, h * 192:h * 192 + 128], rhs=cqn[:, kc, 0:N], start=(kc == 0), stop=(kc == 1)),
                             r=["mwq", "acqn"], w=[pk])
                    o, ok = ob.next()
                    p.op("act", lambda e, o=o, ps=ps: e.activation(out=o[:, 0:N], in_=ps[:, 0:N], func=AF.Copy, scale=MLA_SCALE), r=[pk], w=[ok])
                    p.dma("sp", lambda e, o=o, h=h, lsl=lsl: e.dma_start(out=QN[h, :, lsl], in_=o[:, 0:N]), r=[ok], w=["QN"])
                    ps, pk = pp.next()
                    for kc in range(2):
                        p.op("pe", lambda e, ps=ps, kc=kc, h=h: e.matmul(ps[0:64, 0:N], lhsT=wq[:, kc, h * 192 + 128:h * 192 + 192], rhs=cqn[:, kc, 0:N], start=(kc == 0), stop=(kc == 1)),
                             r=["mwq", "acqn"], w=[pk])
                    o, ok = ob.next()
                    x_, xk_ = rx.next()
                    p.op("act", lambda e, x_=x_, ps=ps: e.copy(out=x_[:, 0:N], in_=ps[0:64, 0:N]), r=[pk], w=[xk_])
                    rope(x_[:, 0:N], xk_, N, MLA_SCALE, o[0:64, 0:N], ok)
                    p.dma("sp", lambda e, o=o, h=h, lsl=lsl: e.dma_start(out=QR[h, :, lsl], in_=o[0:64, 0:N]), r=[ok], w=["QR"])
            for h in range(8):
                ps, pk = pp.next()
                p.op("pe", lambda e, ps=ps, h=h: e.matmul(ps[:, 0:N], lhsT=wkn[:, h, :], rhs=ckvn[:, 0:N], start=True, stop=True), r=["mwk", "ackvn"], w=[pk])
                o, ok = ob.next()
                p.op("act", lambda e, o=o, ps=ps: e.copy(out=o[:, 0:N], in_=ps[:, 0:N]), r=[pk], w=[ok])
                p.dma("sp", lambda e, o=o, h=h, t0=t0, N=N: e.dma_start(out=KN[h, :, t0:t0 + N], in_=o[:, 0:N]), r=[ok], w=["KN"])
            for j in range(N // 128):
                vt, vk = vb.next()
                for g in range(2):
                    ps, pk = pp.next()
                    p.op("pe", lambda e, ps=ps, j=j, g=g: e.matmul(ps[:, :], lhsT=ckvn[:, j * 128:(j + 1) * 128], rhs=wv[:, 4 * g:4 * g + 4, :], start=True, stop=True),
                         r=["mwk", "ackvn"], w=[pk])
                    p.op("dve", lambda e, vt=vt, ps=ps, g=g: e.tensor_copy(out=vt[:, g * 512:(g + 1) * 512], in_=ps[:, :]), r=[pk], w=[vk])
                tok = t0 + j * 128
                p.dma("sp", lambda e, vt=vt, tok=tok: e.dma_start(out=V[tok:tok + 128, :], in_=vt[:]), r=[vk], w=["Vt"])
            o, ok = ob.next()
            if is_lat:
                rope(krr[:, 0:N], "akrr", N, 1.0, o[0:64, 0:N], ok)
            else:
                p.op("dve", lambda e, o=o: e.tensor_copy(out=o[0:64, 0:N], in_=krr[:, 0:N]), r=["akrr"], w=[ok])
            p.dma("sp", lambda e, o=o, t0=t0, N=N: e.dma_start(out=KR[:, t0:t0 + N], in_=o[0:64, 0:N]), r=[ok], w=["KRo"])


        for (t0_, N_, lat_) in blocks:
            do_block(t0_, N_, lat_)

def phase_mla_attn(p, C, io, n_ctx, n_tok):
    QN, QR, KN, KR, V, MIXT = io["QN"], io["QR"], io["KN"], io["KRo"], io["Vt"], io["MIXT"]
    n_lat = n_tok - n_ctx
    nkt = n_tok // 128
    with p.phase():
        kr = p.sb("bkr", [64, n_tok], BF16)
        p.dma("sp", lambda e: e.dma_start(out=kr[:], in_=KR[:, :]), w=["bkr"])
        ones_bf = p.sb("bones", [128, 128], BF16)
        p.op("dve", lambda e: e.memset(ones_bf[:], 1.0), w=["bones"])
        kn = Ring(p, "bkn", [128, n_tok], BF16, n=2)
        vv = Ring(p, "bvv", [128, nkt, 128], BF16, n=2)
        qn = Ring(p, "bqn", [128, 512], BF16, n=2)
        qr = Ring(p, "bqr", [64, 512], BF16, n=2)
        pt = Ring(p, "bpt", [128, 512], BF16, n=4)
        rd = Ring(p, "brd", [128, 512], n=2)
        ob = Ring(p, "bob", [128, 512], BF16, n=2)
        psS = Ring(p, "bpsS", [128, 512], n=4, psum=True)
        psO = Ring(p, "bpsO", [128, 512], n=2, psum=True)
        psD = Ring(p, "bpsD", [128, 512], n=2, psum=True)
        for h in range(8):
            k_, kk = kn.next()
            v_, vk = vv.next()
            p.dma("sp", lambda e, k_=k_, h=h: e.dma_start(out=k_[:], in_=KN[h, :, :]), w=[kk])
            for j0 in range(0, nkt, 12):
                j1 = min(nkt, j0 + 12)
                p.dma("act", lambda e, v_=v_, h=h, j0=j0, j1=j1: e.dma_start(
                    out=v_[:, j0:j1, :], in_=V[j0 * 128:j1 * 128, h * 128:(h + 1) * 128].rearrange("(j q) c -> q j c", q=128)), w=[vk])
            for qb in range(n_lat // 512):
                qsl = slice(qb * 512, (qb + 1) * 512)
                a, ak = qn.next()
                b, bk = qr.next()
                p.dma("sp", lambda e, a=a, h=h, qsl=qsl: e.dma_start(out=a[:], in_=QN[h, :, qsl]), w=[ak])
                p.dma("sp", lambda e, b=b, h=h, qsl=qsl: e.dma_start(out=b[:], in_=QR[h, :, qsl]), w=[bk])
                po, pok = psO.next()
                pd, pdk = psD.next()
                def S_(kt):
                    ks = slice(kt * 128, (kt + 1) * 128)
                    ps, psk = psS.next()
                    p.op("pe", lambda e, ps=ps, k_=k_, ks=ks, a=a: e.matmul(ps[:], lhsT=k_[:, ks], rhs=a[:], start=True, stop=False), r=[kk, ak], w=[psk])
                    p.op("pe", lambda e, ps=ps, ks=ks, b=b: e.matmul(ps[:], lhsT=kr[:, ks], rhs=b[:], start=False, stop=True), r=["bkr", bk], w=[psk])
                    return ps, psk
                LA = 2
                pend = [S_(kt) for kt in range(min(LA, nkt))]
                for kt in range(nkt):
                    if kt + LA < nkt:
                        pend.append(S_(kt + LA))
                    ps, psk = pend.pop(0)
                    t, tk = pt.next()
                    p.op("act", lambda e, t=t, ps=ps: e.activation(out=t[:], in_=ps[:], func=AF.Exp), r=[psk], w=[tk])
                    p.op("pe", lambda e, po=po, v_=v_, kt=kt, t=t: e.matmul(po[:], lhsT=v_[:, kt, :], rhs=t[:], start=(kt == 0), stop=(kt == nkt - 1)), r=[vk, tk], w=[pok])
                    p.op("pe", lambda e, pd=pd, t=t, kt=kt: e.matmul(pd[:], lhsT=ones_bf[:], rhs=t[:], start=(kt == 0), stop=(kt == nkt - 1)), r=["bones", tk], w=[pdk])
                r_, rk = rd.next()
                p.op("dve", lambda e, r_=r_, pd=pd: e.reciprocal(out=r_[:], in_=pd[:]), r=[pdk], w=[rk])
                o, ok = ob.next()
                p.op("dve", lambda e, o=o, po=po, r_=r_: e.tensor_tensor(out=o[:], in0=po[:], in1=r_[:], op=ALU.mult), r=[pok, rk], w=[ok])
                p.dma("sp", lambda e, o=o, h=h, qb=qb: e.dma_start(out=MIXT[h * 128:(h + 1) * 128, n_ctx + qb * 512:n_ctx + (qb + 1) * 512], in_=o[:]), r=[ok], w=["MIXT"])


def moe_tables(p, NE, max_tiles, max_nb):
    RT = {}
    for nm in ("e4", "rank4", "gate4", "destf"):
        RT[nm] = p.sb("rt_" + nm, [128, max_tiles, 4], F32)
    RT["desti"] = p.sb("rt_desti", [128, max_tiles, 4], I32)
    RT["pstart"] = p.sb("rt_pstart", [128, NE], F32)
    RT["blke"] = p.sb("rt_blke", [128, max_nb], F32)
    RT["widx"] = p.sb("rt_widx", [128, max_nb, 8], I32)
    return RT


def phase_moe_sparse_route(p, C, io, G, RT, l, NE, t_lo, t_hi, n_ctx, BLK=512):
    xres, HTOK, modrow = io["xres"], io["HTOK"], io["modrow"]
    T_ = t_hi - t_lo
    ntile = T_ // 128
    NB = (4 * T_ + BLK - 1) // BLK + NE
    LOG = BLK.bit_length() - 1
    e4, rank4, gate4 = RT["e4"], RT["rank4"], RT["gate4"]
    with p.phase():
        T = norm_scratch(p)
        wr = p.sb("wr", [128, KC, NE], F32)
        p.dma("sp", lambda e: e.dma_start(out=wr[:], in_=io["router_w"][l].rearrange("(c q) n -> q c n", q=128)), w=["wr"])
        br = p.sb("br", [128, NE], F32)
        p.dma("sp", lambda e: e.dma_start(out=br[:], in_=io["router_b"][l].partition_broadcast(128)), w=["wr"])
        Arow = p.sb("sArow", [128, 2, D], F32)
        Brow = p.sb("sBrow", [128, 2, D], F32)
        grow = p.sb("sgrow", [128, D], F32)
        p.dma("sp", lambda e: e.dma_start(out=grow[:], in_=io["norm2_g"][l].partition_broadcast(128)), w=["sgrow"])
        for r in range(2):
            p.dma("sp", lambda e, r=r: e.dma_start(out=Arow[:, r, :], in_=modrow[l, r, 4096:5120].partition_broadcast(128)), w=["sArow"])
            p.dma("sp", lambda e, r=r: e.dma_start(out=Brow[:, r, :], in_=modrow[l, r, 3072:4096].partition_broadcast(128)), w=["sBrow"])
            p.op("dve", lambda e, r=r: e.scalar_tensor_tensor(out=Arow[:, r, :], in0=Arow[:, r, :], scalar=1.0, in1=grow[:], op0=ALU.add, op1=ALU.mult),
                 r=["sArow", "sgrow"], w=["sArow"])
        cnt = p.sb("scnt", [128, NE], F32)
        p.op("dve", lambda e: e.memset(cnt[:], 0.0), w=["scnt"])
        xt = Ring(p, "sxt", [128, D], n=2)
        h32 = Ring(p, "sh32", [128, KC, 128], n=2)
        ht = Ring(p, "sht", [128, D], n=2)
        hb = Ring(p, "shb", [128, D], BF16, n=2)
        sm = {nm: Ring(p, "s_" + nm, [128, NE], n=2) for nm in ("lg", "ex", "mk", "rk", "oh", "tmp")}
        m8 = Ring(p, "sm8", [128, 8], n=2)
        sc = Ring(p, "ssc", [128, 2], n=2)
        psr = p.ps("spsr", [128, NE], F32)
        ps2 = p.ps("sps2", [128, NE], F32)
        ps3 = p.ps("sps3", [128, NE], F32)
        for ti in range(ntile):
            tok = t_lo + ti * 128
            r = 1 if tok < n_ctx else 0
            x, xk = xt.next()
            p.dma("sp", lambda e, x=x, tok=tok: e.dma_start(out=x[:], in_=xres[tok:tok + 128, :]), w=[xk])
            f, fk = h32.next()
            norm_tile(p, C, T, xk, x, G["A2"][:, l, r, :], G["B2"][:, l, r, :], None, fk, hT32_dst=lambda kc, f=f: f[:, kc, :], want_bf=False)
            fk32 = fk + "32"
            a, ak = ht.next()
            b, bk = hb.next()
            p.op("dve", lambda e, a=a, r=r: e.tensor_tensor(out=a[:], in0=T["xn"][:], in1=Arow[:, r, :], op=ALU.mult), r=["xn", "sArow"], w=[ak])
            p.op("pool", lambda e, a=a, b=b, r=r: e.tensor_tensor(out=b[:], in0=a[:], in1=Brow[:, r, :], op=ALU.add), r=[ak, "sBrow"], w=[bk])
            p.dma("sp", lambda e, b=b, tok=tok: e.dma_start(out=HTOK[tok:tok + 128, :], in_=b[:]), r=[bk], w=["HTOK"])
            for kc in range(KC):
                p.op("pe", lambda e, f=f, kc=kc: e.matmul(psr[:], lhsT=f[:, kc, :], rhs=wr[:, kc, :], start=(kc == 0), stop=(kc == KC - 1)),
                     r=[fk32, "wr"], w=["spsr"])
            g, gk = sm["lg"].next()
            p.op("dve", lambda e, g=g: e.tensor_tensor(out=g[:], in0=psr[:], in1=br[:], op=ALU.add), r=["spsr", "wr"], w=[gk])
            m, mk8 = m8.next()
            p.op("dve", lambda e, m=m, g=g: e.max(out=m[:], in_=g[:]), r=[gk], w=[mk8])
            msk, mskk = sm["mk"].next()
            p.op("dve", lambda e, msk=msk, g=g, m=m: e.tensor_scalar(out=msk[:], in0=g[:], scalar1=m[:, 3:4], scalar2=None, op0=ALU.is_ge), r=[gk, mk8], w=[mskk])
            s, sk = sc.next()
            p.op("dve", lambda e, s=s, m=m: e.tensor_scalar(out=s[:, 0:1], in0=m[:, 0:1], scalar1=-1.0, scalar2=None, op0=ALU.mult), r=[mk8], w=[sk])
            x_, xk_ = sm["ex"].next()
            p.op("act", lambda e, x_=x_, g=g, s=s: e.activation(out=x_[:], in_=g[:], func=AF.Exp, bias=s[:, 0:1]), r=[gk, sk], w=[xk_])
            p.op("dve", lambda e, x_=x_, msk=msk: e.tensor_tensor(out=x_[:], in0=x_[:], in1=msk[:], op=ALU.mult), r=[xk_, mskk], w=[xk_])
            p.op("dve", lambda e, s=s, x_=x_: e.reduce_sum(out=s[:, 1:2], in_=x_[:], axis=AX.X), r=[xk_], w=[sk])
            p.op("dve", lambda e, s=s: e.reciprocal(out=s[:, 1:2], in_=s[:, 1:2]), r=[sk], w=[sk])
            p.op("dve", lambda e, x_=x_, s=s: e.tensor_scalar(out=x_[:], in0=x_[:], scalar1=s[:, 1:2], scalar2=None, op0=ALU.mult), r=[xk_, sk], w=[xk_])
            p.op("pe", lambda e, msk=msk: e.matmul(ps2[:], lhsT=C["strict0"][:], rhs=msk[:], start=True, stop=True), r=["masks", mskk], w=["sps2"])
            p.op("pe", lambda e, msk=msk: e.matmul(ps3[:], lhsT=C["ones64"][:], rhs=msk[:], start=True, stop=True), r=["masks", mskk], w=["sps3"])
            rk, rkk = sm["rk"].next()
            p.op("dve", lambda e, rk=rk: e.tensor_tensor(out=rk[:], in0=ps2[:], in1=cnt[:], op=ALU.add), r=["sps2", "scnt"], w=[rkk])
            p.op("dve", lambda e: e.tensor_tensor(out=cnt[:], in0=ps3[:], in1=cnt[:], op=ALU.add), r=["sps3", "scnt", rkk], w=["scnt"])
            for k in range(4):
                oh, ohk = sm["oh"].next()
                p.op("dve", lambda e, oh=oh, g=g, m=m, k=k: e.tensor_scalar(out=oh[:], in0=g[:], scalar1=m[:, k:k + 1], scalar2=None, op0=ALU.is_equal), r=[gk, mk8], w=[ohk])
                for (src, srck, dst) in ((C["iota_f"][:, 0:NE], "iota_f", e4), (rk[:], rkk, rank4), (x_[:], xk_, gate4)):
                    tmp, tmpk = sm["tmp"].next()
                    p.op("dve", lambda e, tmp=tmp, oh=oh, src=src: e.tensor_tensor(out=tmp[:], in0=oh[:], in1=src, op=ALU.mult), r=[ohk, srck], w=[tmpk])
                    p.op("dve", lambda e, tmp=tmp, dst=dst, ti=ti, k=k: e.reduce_sum(out=dst[:, ti, k:k + 1], in_=tmp[:], axis=AX.X), r=[tmpk], w=["rt"])
        ci = p.sb("sci", [128, NE], I32)
        padf = p.sb("spadf", [128, NE], F32)
        ca = p.sb("sca", [128, NE], F32)
        cb = p.sb("scb", [128, NE], F32)
        p.op("dve", lambda e: e.tensor_copy(out=ci[:], in_=cnt[:]), r=["scnt"], w=["sci"])
        p.op("dve", lambda e: e.tensor_single_scalar(out=ci[:], in_=ci[:], scalar=BLK - 1, op=ALU.add), r=["sci"], w=["sci"])
        p.op("dve", lambda e: e.tensor_single_scalar(out=ci[:], in_=ci[:], scalar=LOG, op=ALU.arith_shift_right), r=["sci"], w=["sci"])
        p.op("dve", lambda e: e.tensor_single_scalar(out=ci[:], in_=ci[:], scalar=LOG, op=ALU.logical_shift_left), r=["sci"], w=["sci"])
        p.op("dve", lambda e: e.tensor_copy(out=padf[:], in_=ci[:]), r=["sci"], w=["spadf"])
        p.op("dve", lambda e: e.tensor_copy(out=ca[:], in_=padf[:]), r=["spadf"], w=["sca"])
        cur, curk, oth, othk = ca, "sca", cb, "scb"
        sft = 1
        while sft < NE:
            p.op("dve", lambda e, cur=cur, oth=oth: e.tensor_copy(out=oth[:], in_=cur[:]), r=[curk], w=[othk])
            p.op("dve", lambda e, cur=cur, oth=oth, sft=sft: e.tensor_tensor(out=oth[:, sft:NE], in0=cur[:, sft:NE], in1=cur[:, 0:NE - sft], op=ALU.add), r=[curk, othk], w=[othk])
            cur, curk, oth, othk = oth, othk, cur, curk
            sft *= 2
        pend, pendk = cur, curk
        p.op("dve", lambda e: e.tensor_tensor(out=RT["pstart"][:], in0=pend[:], in1=padf[:], op=ALU.subtract), r=[pendk, "spadf"], w=["rt"])
        bst = p.sb("sbst", [128, NB], F32)
        p.op("dve", lambda e: e.tensor_scalar(out=bst[:], in0=C["iota_f"][:, 0:NB], scalar1=float(BLK), scalar2=None, op0=ALU.mult), r=["iota_f"], w=["sbst"])
        blke = RT["blke"]
        p.op("dve", lambda e: e.memset(blke[:, 0:NB], 0.0), w=["rt"])
        for e_ in range(NE):
            p.op("dve", lambda e, e_=e_: e.scalar_tensor_tensor(out=blke[:, 0:NB], in0=bst[:], scalar=pend[:, e_:e_ + 1], in1=blke[:, 0:NB], op0=ALU.is_ge, op1=ALU.add),
                 r=["sbst", pendk, "rt"], w=["rt"])
        p.op("dve", lambda e: e.tensor_scalar(out=blke[:, 0:NB], in0=blke[:, 0:NB], scalar1=float(NE - 1), scalar2=None, op0=ALU.min), r=["rt"], w=["rt"])
        base8 = p.sb("sbase8", [128, 8], F32)
        p.op("dve", lambda e: e.tensor_scalar(out=base8[:], in0=C["iota_f"][:, 0:8], scalar1=128.0, scalar2=C["iota_p"][:, 0:1], op0=ALU.mult, op1=ALU.add),
             r=["iota_f", "iota_p"], w=["sbase8"])
        b1k = p.sb("sb1k", [128, NB], F32)
        p.op("dve", lambda e: e.tensor_scalar(out=b1k[:], in0=blke[:, 0:NB], scalar1=1024.0, scalar2=None, op0=ALU.mult), r=["rt"], w=["sb1k"])
        wf = p.sb("swf", [128, NB, 8], F32)
        for b_ in range(NB):
            p.op("dve" if b_ % 2 == 0 else "pool", lambda e, b_=b_: e.tensor_scalar(out=wf[:, b_, :], in0=base8[:], scalar1=b1k[:, b_:b_ + 1], scalar2=None, op0=ALU.add),
                 r=["sbase8", "sb1k"], w=["swf"])
        p.op("dve", lambda e: e.tensor_copy(out=RT["widx"][:, 0:NB, :], in_=wf[:]), r=["swf"], w=["rt"])
        destf, desti = RT["destf"], RT["desti"]
        for ti in range(ntile):
            for k in range(4):
                oh, ohk = sm["oh"].next()
                p.op("dve", lambda e, oh=oh, ti=ti, k=k: e.tensor_scalar(out=oh[:], in0=C["iota_f"][:, 0:NE], scalar1=e4[:, ti, k:k + 1], scalar2=None, op0=ALU.is_equal),
                     r=["iota_f", "rt"], w=[ohk])
                tmp, tmpk = sm["tmp"].next()
                p.op("dve", lambda e, tmp=tmp, oh=oh: e.tensor_tensor(out=tmp[:], in0=oh[:], in1=RT["pstart"][:], op=ALU.mult), r=[ohk, "rt"], w=[tmpk])
                p.op("dve", lambda e, tmp=tmp, ti=ti, k=k: e.reduce_sum(out=destf[:, ti, k:k + 1], in_=tmp[:], axis=AX.X), r=[tmpk], w=["rt"])
        p.op("dve", lambda e: e.tensor_tensor(out=destf[:, 0:ntile, :], in0=destf[:, 0:ntile, :], in1=rank4[:, 0:ntile, :], op=ALU.add), r=["rt"], w=["rt"])
        p.op("dve", lambda e: e.tensor_scalar(out=destf[:, 0:ntile, :], in0=destf[:, 0:ntile, :], scalar1=float(NB * BLK - 1), scalar2=0.0, op0=ALU.min, op1=ALU.max), r=["rt"], w=["rt"])
        p.op("dve", lambda e: e.tensor_copy(out=desti[:, 0:ntile, :], in_=destf[:, 0:ntile, :]), r=["rt"], w=["rt"])
    return NB


def phase_moe_sparse_dispatch(p, C, io, RT, t_lo, t_hi, NB, BLK=512):
    HTOK, HS = io["HTOK"], io["HS"]
    ntile = (t_hi - t_lo) // 128
    with p.phase():
        z = p.sb("dz", [128, 4, D], BF16)
        p.op("pool", lambda e: e.memset(z[:], 0.0), w=["dz"])
        for b_ in range(NB * BLK // 512):
            p.dma("sp" if b_ % 2 == 0 else "act", lambda e, b_=b_: e.dma_start(out=HS[b_ * 512:(b_ + 1) * 512, :].rearrange("(j q) d -> q j d", q=128), in_=z[:]),
                  r=["dz"], w=[f"HSz{b_ % 8}"])
        hb = Ring(p, "dhb", [128, D], BF16, n=3)
        zk = [f"HSz{i}" for i in range(8)]
        for ti in range(ntile):
            tok = t_lo + ti * 128
            h, hk = hb.next()
            p.dma("sp", lambda e, h=h, tok=tok: e.dma_start(out=h[:], in_=HTOK[tok:tok + 128, :]), w=[hk])
            for k in range(4):
                p.dma("pool", lambda e, h=h, ti=ti, k=k: e.indirect_dma_start(
                    out=HS[:, :], out_offset=bass.IndirectOffsetOnAxis(ap=RT["desti"][:, ti, k:k + 1], axis=0), in_=h[:, :], in_offset=None),
                    r=[hk, "rt"] + zk, w=[f"HSs{(ti * 4 + k) % 8}"])


def phase_moe_sparse_experts(p, C, io, RT, l, NE, NB, BLK=512):
    HS, Y, WGU, WDN = io["HS"], io["Y"], io["WGU"], io["WDN"]
    WGf = WGU.rearrange("e k n -> (e k) n")
    WDf = WDN.rearrange("e k n -> (e k) n")
    blke = RT["blke"]
    with p.phase():
        b32 = p.sb("xb32", [NE, 2048], F32)
        bgu = p.sb("xbgu", [NE, 2048], BF16)
        bdn = p.sb("xbdn", [NE, 1024], BF16)
        p.dma("sp", lambda e: e.dma_start(out=b32[:], in_=io["moe_gu_b"][l]), w=["xb32"])
        p.op("dve", lambda e: e.tensor_copy(out=bgu[:], in_=b32[:]), r=["xb32"], w=["xbias"])
        p.dma("sp", lambda e: e.dma_start(out=b32[:, 0:1024], in_=io["moe_down_b"][l]), r=["xbias"], w=["xb32"])
        p.op("dve", lambda e: e.tensor_copy(out=bdn[:], in_=b32[:, 0:1024]), r=["xb32"], w=["xbias"])
        ones5 = p.sb("xones", [NE, BLK], F32)
        p.op("dve", lambda e: e.memset(ones5[:], 1.0), w=["xones"])
        wg = Ring(p, "xwg", [128, KC, 2048], BF16, n=2)
        wd = Ring(p, "xwd", [128, KC, 1024], BF16, n=2)
        hs = Ring(p, "xhs", [128, BLK // 128, D], BF16, n=2)
        hsT = Ring(p, "xhsT", [128, KC, BLK], BF16, n=2)
        ohT = Ring(p, "xohT", [NE, BLK], BF16, n=2)
        actT = Ring(p, "xactT", [128, KC, BLK], BF16, n=1)
        t1 = Ring(p, "xt1", [128, BLK], n=2)
        sgm = Ring(p, "xsg", [128, BLK], n=2)
        t2 = Ring(p, "xt2", [128, BLK], n=2)
        yo = Ring(p, "xyo", [128, D], n=2)
        pst = Ring(p, "xpst", [128, KC, 128], BF16, n=2, psum=True)
        psg = Ring(p, "xpsg", [128, 512], n=4, psum=True)
        psd = Ring(p, "xpsd", [128, 512], n=2, psum=True)
        ev = 0
        for b_ in range(NB):
            g_, gk = wg.next()
            d_, dk = wd.next()
            for kc in range(KC):
                p.dma("pool", lambda e, g_=g_, kc=kc, b_=b_: e.indirect_dma_start(
                    out=g_[:, kc, :], out_offset=None, in_=WGf[:, :], in_offset=bass.IndirectOffsetOnAxis(ap=RT["widx"][:, b_, kc:kc + 1], axis=0)), r=["rt"], w=[gk])
                p.dma("pool", lambda e, d_=d_, kc=kc, b_=b_: e.indirect_dma_start(
                    out=d_[:, kc, :], out_offset=None, in_=WDf[:, :], in_offset=bass.IndirectOffsetOnAxis(ap=RT["widx"][:, b_, kc:kc + 1], axis=0)), r=["rt"], w=[dk])
            h_, hk = hs.next()
            p.dma("sp", lambda e, h_=h_, b_=b_: e.dma_start(out=h_[:], in_=HS[b_ * BLK:(b_ + 1) * BLK, :].rearrange("(j q) d -> q j d", q=128)), w=[hk])
            o_, ok_ = ohT.next()
            p.op("dve", lambda e, o_=o_, b_=b_: e.tensor_scalar(out=o_[:], in0=ones5[:], scalar1=blke[0:NE, b_:b_ + 1], scalar2=C["iota_p"][0:NE, 0:1],
                                                                 op0=ALU.mult, op1=ALU.is_equal), r=["xones", "rt", "iota_p"], w=[ok_])
            hT, hTk = hsT.next()
            for j in range(BLK // 128):
                pt_, ptk = pst.next()
                for kc in range(KC):
                    p.op("pe", lambda e, pt_=pt_, h_=h_, j=j, kc=kc: e.transpose(pt_[:, kc, :], h_[:, j, kc * 128:(kc + 1) * 128], C["identb"][:]), r=[hk, "identb"], w=[ptk])
                if j % 2 == 0:
                    p.op("act", lambda e, hT=hT, pt_=pt_, j=j: e.copy(out=hT[:, :, j * 128:(j + 1) * 128], in_=pt_[:]), r=[ptk], w=[hTk])
                else:
                    p.op("dve", lambda e, hT=hT, pt_=pt_, j=j: e.tensor_copy(out=hT[:, :, j * 128:(j + 1) * 128], in_=pt_[:]), r=[ptk], w=[hTk])
            aT, aTk = actT.next()
            N = BLK
            for c in range(8):
                pa, pak = psg.next()
                pb, pbk = psg.next()
                for (ps_, pk_, col0) in ((pa, pak, c * 128), (pb, pbk, DE + c * 128)):
                    for kc in range(KC):
                        p.op("pe", lambda e, ps_=ps_, g_=g_, kc=kc, col0=col0, hT=hT: e.matmul(
                            ps_[:, 0:N], lhsT=g_[:, kc, col0:col0 + 128], rhs=hT[:, kc, :], start=(kc == 0), stop=False), r=[gk, hTk], w=[pk_])
                    p.op("pe", lambda e, ps_=ps_, col0=col0, o_=o_: e.matmul(ps_[:, 0:N], lhsT=bgu[:, col0:col0 + 128], rhs=o_[:], start=False, stop=True),
                         r=["xbias", ok_], w=[pk_])
                a1, a1k = t1.next()
                p.op("dve", lambda e, a1=a1, pa=pa: e.tensor_scalar(out=a1[:], in0=pa[:], scalar1=SW_LIMIT, scalar2=None, op0=ALU.min), r=[pak], w=[a1k])
                s_, sk_ = sgm.next()
                p.op("act", lambda e, s_=s_, a1=a1: e.activation(out=s_[:], in_=a1[:], func=AF.Sigmoid, scale=SW_ALPHA), r=[a1k], w=[sk_])
                a2, a2k = t2.next()
                p.op("dve", lambda e, a2=a2, pb=pb: e.tensor_scalar(out=a2[:], in0=pb[:], scalar1=SW_LIMIT, scalar2=-SW_LIMIT, op0=ALU.min, op1=ALU.max), r=[pbk], w=[a2k])
                p.op("pool", lambda e, a1=a1, s_=s_: e.tensor_tensor(out=a1[:], in0=a1[:], in1=s_[:], op=ALU.mult), r=[a1k, sk_], w=[a1k])
                p.op("dve", lambda e, aT=aT, a1=a1, a2=a2, c=c: e.scalar_tensor_tensor(out=aT[:, c, :], in0=a2[:], scalar=1.0, in1=a1[:], op0=ALU.add, op1=ALU.mult),
                     r=[a1k, a2k], w=[aTk])
            for j in range(BLK // 128):
                y_, yk = yo.next()
                for n in range(2):
                    pd, pdk = psd.next()
                    for kc in range(KC):
                        p.op("pe", lambda e, pd=pd, aT=aT, d_=d_, kc=kc, j=j, n=n: e.matmul(
                            pd[:], lhsT=aT[:, kc, j * 128:(j + 1) * 128], rhs=d_[:, kc, n * 512:(n + 1) * 512], start=(kc == 0), stop=False), r=[aTk, dk], w=[pdk])
                    p.op("pe", lambda e, pd=pd, o_=o_, n=n: e.matmul(pd[:], lhsT=o_[:, 0:128], rhs=bdn[:, n * 512:(n + 1) * 512], start=False, stop=True),
                         r=[ok_, "xbias"], w=[pdk])
                    ev += 1
                    if ev % 2 == 0:
                        p.op("act", lambda e, y_=y_, pd=pd, n=n: e.copy(out=y_[:, n * 512:(n + 1) * 512], in_=pd[:]), r=[pdk], w=[yk])
                    else:
                        p.op("dve", lambda e, y_=y_, pd=pd, n=n: e.tensor_copy(out=y_[:, n * 512:(n + 1) * 512], in_=pd[:]), r=[pdk], w=[yk])
                row = b_ * BLK + j * 128
                p.dma("sp", lambda e, y_=y_, row=row: e.dma_start(out=Y[row:row + 128, :], in_=y_[:]), r=[yk], w=["Y"])


def phase_moe_sparse_combine(p, C, io, RT, l, t_lo, t_hi, n_ctx):
    xres, Y, modrow = io["xres"], io["Y"], io["modrow"]
    ntile = (t_hi - t_lo) // 128
    with p.phase():
        gt = p.sb("cgt2", [128, 2, D], F32)
        for r in range(2):
            p.dma("sp", lambda e, r=r: e.dma_start(out=gt[:, r, :], in_=modrow[l, r, 5120:6144].partition_broadcast(128)), w=["cgt2"])
        yk_ = Ring(p, "cyk", [128, D], n=8)
        acc = Ring(p, "cacc", [128, D], n=2)
        xt = Ring(p, "cxt", [128, D], n=2)
        for ti in range(ntile):
            tok = t_lo + ti * 128
            r = 1 if tok < n_ctx else 0
            x, xk = xt.next()
            p.dma("sp", lambda e, x=x, tok=tok: e.dma_start(out=x[:], in_=xres[tok:tok + 128, :]), w=[xk])
            a, ak = acc.next()
            for k in range(4):
                y, yk = yk_.next()
                p.dma("pool", lambda e, y=y, ti=ti, k=k: e.indirect_dma_start(
                    out=y[:, :], out_offset=None, in_=Y[:, :], in_offset=bass.IndirectOffsetOnAxis(ap=RT["desti"][:, ti, k:k + 1], axis=0)), r=["rt", "Y"], w=[yk])
                if k == 0:
                    p.op("dve", lambda e, a=a, y=y, ti=ti: e.tensor_scalar(out=a[:], in0=y[:], scalar1=RT["gate4"][:, ti, 0:1], scalar2=None, op0=ALU.mult), r=[yk, "rt"], w=[ak])
                else:
                    p.op("dve", lambda e, a=a, y=y, ti=ti, k=k: e.scalar_tensor_tensor(out=a[:], in0=y[:], scalar=RT["gate4"][:, ti, k:k + 1], in1=a[:], op0=ALU.mult, op1=ALU.add),
                         r=[yk, "rt", ak], w=[ak])
            p.op("pool", lambda e, a=a, r=r: e.tensor_tensor(out=a[:], in0=a[:], in1=gt[:, r, :], op=ALU.mult), r=[ak, "cgt2"], w=[ak])
            p.op("dve", lambda e, a=a, x=x: e.tensor_tensor(out=x[:], in0=x[:], in1=a[:], op=ALU.add), r=[ak, xk], w=[xk])
            p.dma("sp", lambda e, x=x, tok=tok: e.dma_start(out=xres[tok:tok + 128, :], in_=x[:]), r=[xk], w=["xres"])

from concourse.bass_utils import run_bass_kernel_spmd

N_CTX = 256
N_LAT = 8192
DEPTH = 2
NEXP = 32
_W_NAMES = ["ada_w", "ada_b", "norm1_g", "norm2_g", "ev_w_in", "ev_w_out", "gla_gk_up", "gla_gk_b", "gla_norm_g",
            "rw_mu", "rw_w0", "rw_w2", "rw_a0", "rw_a2", "rw_k_k", "rw_k_a", "rw_r_k", "rw_g2", "rw_ln_w", "rw_ln_b",
            "od_w_in", "mla_q_norm", "mla_wq_up", "mla_kv_norm", "mla_wkv_up", "od_w_out",
            "router_w", "router_b", "moe_gu_w", "moe_gu_b", "moe_down_w", "moe_down_b", "final_norm_g"]


def build(shapes, n_ctx=N_CTX, n_lat=N_LAT, NE=NEXP):
    n_tok = n_ctx + n_lat
    L = DEPTH
    nc = bass.Bass("TRN2", target_bir_lowering=False)
    NBmax = (4 * n_tok + 511) // 512 + NE
    io = {}
    for k, shp in shapes.items():
        io[k] = dram(nc, k, list(shp), kind="ExternalInput")
    for name, shp, dt in (("xres", [n_tok, 1024], F32), ("modrow", [L, 2, 6144], F32), ("PT", [3376, n_tok], F32),
                          ("PTOK", [n_tok, 1536], F32), ("SMT", [1824, n_tok], F32), ("YTG", [2, 512, n_tok], F32),
                          ("YTR", [2, 512, n_tok], F32), ("RK", [2, 512, n_tok], F32), ("MIXT", [1024, n_tok], BF16),
                          ("HT", [1024, n_tok], BF16), ("GATE", [n_tok, NE], F32), ("WGU", [NE, 1024, 2048], BF16),
                          ("WDN", [NE, 1024, 1024], BF16), ("QN", [8, 128, n_lat], BF16), ("QR", [8, 64, n_lat], BF16),
                          ("KN", [8, 128, n_tok], BF16), ("KRo", [64, n_tok], BF16), ("Vt", [n_tok, 1024], BF16),
                          ("HTOK", [n_tok, 1024], BF16), ("HS", [NBmax * 512, 1024], BF16), ("Y", [NBmax * 512, 1024], F32)):
        io[name] = dram(nc, name, shp, dt)
    io["out"] = dram(nc, "out", [n_lat, 1024], kind="ExternalOutput")
    p = Prog(nc)
    C = make_consts(p)
    make_masks(p, C)
    make_rope_consts(p, C)
    G = {k: p.sb("G" + k, [128, L, 2, 8], F32) for k in ("A1", "B1", "A2", "B2")}
    RT = moe_tables(p, NE, n_tok // 128, NBmax)
    with p.phase():
        cp = Ring(p, "cpx", [128, 1024], n=3)
        for ti in range(n_tok // 128):
            x_, xk = cp.next()
            p.dma("sp", lambda e, x_=x_, ti=ti: e.dma_start(out=x_[:], in_=io["xin"][ti * 128:(ti + 1) * 128, :]), w=[xk])
            p.dma("sp", lambda e, x_=x_, ti=ti: e.dma_start(out=io["xres"][ti * 128:(ti + 1) * 128, :], in_=x_[:]), r=[xk], w=["xres"])
    phase_ada(p, C, io, L, G)
    phase_inproj(p, C, io, G, 0, n_ctx, n_tok)
    phase_gla(p, C, io, n_ctx, n_tok)
    phase_gla_finish(p, C, io, n_tok)
    phase_shift(p, C, io, n_ctx, n_tok)
    phase_rwkv(p, C, io, n_ctx, n_tok)
    phase_rwkv_finish(p, C, io, n_tok)
    phase_outproj(p, C, io, 0, io["ev_w_out"], n_ctx, n_tok)
    phase_moe_cast(p, C, io, 0, NE)
    NB = phase_moe_sparse_route(p, C, io, G, RT, 0, NE, 0, n_tok, n_ctx)
    phase_moe_sparse_dispatch(p, C, io, RT, 0, n_tok, NB)
    phase_moe_sparse_experts(p, C, io, RT, 0, NE, NB)
    phase_moe_sparse_combine(p, C, io, RT, 0, 0, n_tok, n_ctx)
    phase_mla_proj(p, C, io, G, 1, n_ctx, n_tok)
    phase_mla_attn(p, C, io, n_ctx, n_tok)
    phase_outproj(p, C, io, 1, io["od_w_out"], n_ctx, n_tok, t_lo=n_ctx)
    phase_moe_cast(p, C, io, 1, NE)
    NB = phase_moe_sparse_route(p, C, io, G, RT, 1, NE, n_ctx, n_tok, n_ctx)
    phase_moe_sparse_dispatch(p, C, io, RT, n_ctx, n_tok, NB)
    phase_moe_sparse_experts(p, C, io, RT, 1, NE, NB)
    phase_moe_sparse_combine(p, C, io, RT, 1, n_ctx, n_tok, n_ctx)
    phase_final(p, C, io, n_ctx, n_tok)
    p.finish()
    return nc


def kernel(**inputs):
    f = lambda a: np.ascontiguousarray(np.asarray(a, dtype=np.float32))
    x, c, ctx, c_ctx = f(inputs["x"]), f(inputs["c"]), f(inputs["ctx"]), f(inputs["c_ctx"])
    B = x.shape[0]
    shared = {}
    for k in _W_NAMES:
        a = f(inputs[k])
        if k.startswith(("ev_", "gla_", "rw_", "od_", "mla_")):
            a = np.ascontiguousarray(a[0])
        shared[k] = a
    in_maps = []
    for b in range(B):
        m = dict(shared)
        m["xin"] = np.ascontiguousarray(np.concatenate([ctx[b], x[b]], axis=0))
        m["cvec"] = np.ascontiguousarray(np.stack([c[b], c_ctx], axis=0))
        in_maps.append(m)
    shapes = {k: v.shape for k, v in in_maps[0].items()}
    nc = build(shapes, n_ctx=ctx.shape[1], n_lat=x.shape[1], NE=shared["router_w"].shape[-1])
    res = run_bass_kernel_spmd(nc, in_maps, core_ids=list(range(B)))
    return np.stack([np.asarray(r["out"], dtype=np.float32) for r in res.results], axis=0)
```

```python
import contextlib
import numpy as np
import concourse.bass as bass
import concourse.mybir as mybir

F32 = mybir.dt.float32
BF16 = mybir.dt.bfloat16
I32 = mybir.dt.int32
U32 = mybir.dt.uint32
ALU = mybir.AluOpType
AF = mybir.ActivationFunctionType
AX = mybir.AxisListType

ENGS = ("pe", "act", "dve", "pool", "sp")
RESET_THRESH = 20000


class Prog:
    def __init__(self, nc, n_dma_slots=10):
        self.nc = nc
        self.es = contextlib.ExitStack()
        self.cur = self.es
        self.streams = {e: [] for e in ENGS}
        self.count = {e: 0 for e in ENGS}
        self.seen = {e: {} for e in ENGS}
        self.bufs = {}
        self.sems = {}
        for e in ("pe", "act", "dve", "pool"):
            self.sems[e] = self.es.enter_context(nc.semaphore("s_" + e))
        self.dma_slots = {}
        self.dma_rr = {}
        for q in ("sp", "act", "pool"):
            self.dma_slots[q] = []
            for i in range(n_dma_slots):
                s = self.es.enter_context(nc.semaphore(f"d_{q}{i}"))
                self.sems[f"d_{q}{i}"] = s
                self.dma_slots[q].append([f"d_{q}{i}", 0])
            self.dma_rr[q] = 0
        self.n_instr = 0
        self.bsem = self.es.enter_context(nc.semaphore("s_bar"))
        self.gsem = self.es.enter_context(nc.semaphore("s_go"))
        self.nreset = 0
        self.reset_thresh = RESET_THRESH

    def _maybe_reset(self):
        if max(self.count.values()) >= self.reset_thresh or \
                max(v for q in self.dma_slots if q != "pool" for _, v in self.dma_slots[q]) >= 2 * self.reset_thresh:
            self.sync_reset()

    def sync_reset(self):
        self.barrier()
        self.nreset += 1
        k = self.nreset
        bs, gs = self.bsem, self.gsem
        for e in ENGS:
            self.streams[e].append(lambda eng, bs=bs: eng.sem_inc(bs, 1))
        self.streams["sp"].append(lambda eng, bs=bs, k=k: eng.wait_ge(bs, 5 * k))
        for name, sem in self.sems.items():
            if name.startswith("d_pool"):
                continue
            self.streams["sp"].append(lambda eng, sem=sem: eng.sem_clear(sem))
        self.streams["sp"].append(lambda eng, gs=gs: eng.sem_inc(gs, 1))
        for e in ENGS:
            if e != "sp":
                self.streams[e].append(lambda eng, gs=gs, k=k: eng.wait_ge(gs, k))
        self.count = {e: 0 for e in ENGS}
        self.seen = {e: {} for e in ENGS}
        self.bufs = {}
        for q in self.dma_slots:
            if q == "pool":
                continue
            for slot in self.dma_slots[q]:
                slot[1] = 0

    def sb(self, name, shape, dt=F32):
        self._uid = getattr(self, "_uid", 0) + 1
        name = f"{name}_u{self._uid}"
        return self.cur.enter_context(self.nc.sbuf_tensor(name, list(shape), dt))

    def ps(self, name, shape, dt=F32):
        self._uid = getattr(self, "_uid", 0) + 1
        name = f"{name}_u{self._uid}"
        return self.cur.enter_context(self.nc.psum_tensor(name, list(shape), dt))

    def _deps(self, r, w):
        deps = {}

        def add(tok):
            if tok is None:
                return
            s, v = tok
            if deps.get(s, 0) < v:
                deps[s] = v

        for k in r:
            b = self.bufs.setdefault(k, {"w": None, "r": {}})
            add(b["w"])
        for k in w:
            b = self.bufs.setdefault(k, {"w": None, "r": {}})
            add(b["w"])
            for s, v in b["r"].items():
                add((s, v))
        return deps

    def _commit(self, r, w, tok):
        for k in r:
            b = self.bufs[k]
            if b["r"].get(tok[0], 0) < tok[1]:
                b["r"][tok[0]] = tok[1]
        for k in w:
            b = self.bufs[k]
            b["w"] = tok
            b["r"] = {}

    def _emit_waits(self, eng, deps):
        seen = self.seen[eng]
        for s, v in deps.items():
            if eng == "pe" and s == "pe":
                continue
            if seen.get(s, 0) >= v:
                continue
            seen[s] = v
            sem = self.sems[s]
            self.streams[eng].append(lambda e, sem=sem, v=v: e.wait_ge(sem, v))

    def op(self, eng, fn, r=(), w=()):
        self._maybe_reset()
        deps = self._deps(r, w)
        self._emit_waits(eng, deps)
        self.count[eng] += 1
        n = self.count[eng]
        sem = self.sems[eng]
        self.streams[eng].append(lambda e, fn=fn, sem=sem: fn(e).then_inc(sem, 1))
        self._commit(r, w, (eng, n))
        self.n_instr += 1

    def dma(self, q, fn, r=(), w=()):
        self._maybe_reset()
        deps = self._deps(r, w)
        i = self.dma_rr[q]
        self.dma_rr[q] = (i + 1) % len(self.dma_slots[q])
        slot = self.dma_slots[q][i]
        if slot[1] > 0:
            if deps.get(slot[0], 0) < slot[1]:
                deps[slot[0]] = slot[1]
        self._emit_waits(q, deps)
        slot[1] += 16
        sem = self.sems[slot[0]]
        self.streams[q].append(lambda e, fn=fn, sem=sem: fn(e).then_inc(sem, 16))
        self._commit(r, w, (slot[0], slot[1]))
        self.n_instr += 1

    def wait_all(self, eng, keys):
        deps = self._deps(keys, ())
        self._emit_waits(eng, deps)

    def barrier(self):
        full = {}
        for e in ("pe", "act", "dve", "pool"):
            if self.count[e] > 0:
                full[e] = self.count[e]
        for q in self.dma_slots:
            for name, v in self.dma_slots[q]:
                if v > 0:
                    full[name] = v
        for e in ENGS:
            d = dict(full)
            d.pop(e, None)
            if e == "pe":
                pass
            self._emit_waits(e, d)

    @contextlib.contextmanager
    def phase(self):
        old = self.cur
        with contextlib.ExitStack() as st:
            self.cur = st
            yield
            self.barrier()
            self.flush()
        self.cur = old

    def flush(self):
        self._emit_block()
        self.streams = {e: [] for e in ENGS}

    def finish(self):
        self.barrier()
        self.flush()
        self.es.close()

    def _emit_block(self):
        nc = self.nc
        with nc.Block() as block:
            @block.tensor
            def _(e):
                for f in self.streams["pe"]:
                    f(e)

            @block.scalar
            def _(e):
                for f in self.streams["act"]:
                    f(e)

            @block.vector
            def _(e):
                for f in self.streams["dve"]:
                    f(e)

            @block.gpsimd
            def _(e):
                for f in self.streams["pool"]:
                    f(e)

            @block.sync
            def _(e):
                for f in self.streams["sp"]:
                    f(e)

import contextlib
import numpy as np

D = 1024
KC = 8
EPS = 1e-6
GLA_COLS = 1552
RW_COLS = 1824
EVEN_IN = 3376


def dram(nc, name, shape, dt=F32, kind="Internal"):
    return nc.dram_tensor(name, list(shape), dt, kind=kind).ap()


def make_consts(p):
    nc = p.nc
    C = {}
    iota_f = p.sb("iota_f", [128, 128], F32)
    iota_p = p.sb("iota_p", [128, 1], F32)
    ii = p.sb("iota_i", [128, 128], I32)
    ip = p.sb("iota_pi", [128, 1], I32)
    p.op("pool", lambda e: e.iota(ii[:], pattern=[[1, 128]], base=0, channel_multiplier=0), w=["iota_i"])
    p.op("pool", lambda e: e.iota(ip[:], pattern=[[1, 1]], base=0, channel_multiplier=1), w=["iota_pi"])
    p.op("dve", lambda e: e.tensor_copy(out=iota_f[:], in_=ii[:]), r=["iota_i"], w=["iota_f"])
    p.op("dve", lambda e: e.tensor_copy(out=iota_p[:], in_=ip[:]), r=["iota_pi"], w=["iota_p"])
    ident = p.sb("ident", [128, 128], F32)
    p.op("dve", lambda e: e.tensor_scalar(out=ident[:], in0=iota_f[:], scalar1=iota_p[:, 0:1], scalar2=None,
                                          op0=ALU.is_equal), r=["iota_f", "iota_p"], w=["ident"])
    identb = p.sb("identb", [128, 128], BF16)
    p.op("dve", lambda e: e.tensor_copy(out=identb[:], in_=ident[:]), r=["ident"], w=["identb"])
    C.update(iota_f=iota_f, iota_p=iota_p, ident=ident, identb=identb)
    return C


def phase_ada(p, C, io, L, G):
    nc = p.nc
    cvec, ada_w, ada_b = io["cvec"], io["ada_w"], io["ada_b"]
    modrow = io["modrow"]
    with p.phase():
        cT = p.sb("cT", [128, KC, 2], F32)
        sT = p.sb("sT", [128, KC, 2], F32)
        for r in range(2):
            p.dma("sp", lambda e, r=r: e.dma_start(out=cT[:, :, r], in_=cvec[r, :].rearrange("(c q) -> q c", q=128),
                                                   allow_slow_non_contiguous=True), w=["cT"])
        p.op("act", lambda e: e.activation(out=sT[:], in_=cT[:], func=AF.Silu), r=["cT"], w=["sT"])
        wts = [p.sb(f"adaw{i}", [128, KC, 512], F32) for i in range(2)]
        mrow = p.sb("mrow", [2, 6144], F32)
        brow = p.sb("brow", [2, 6144], F32)
        mps = p.ps("mps", [2, 512], F32)
        tps = p.ps("tps", [128, 48, 2], F32)
        mcol = p.sb("mcol", [128, 48, 2], F32)
        gcol = p.sb("gcol", [128, 2, KC], F32)
        it = 0
        for l in range(L):
            for r in range(2):
                p.dma("sp", lambda e, r=r, l=l: e.dma_start(out=brow[r:r + 1, :], in_=ada_b[l:l + 1, :]), w=["brow"])
            p.dma("sp", lambda e, l=l: e.dma_start(out=gcol[:, 0, :], in_=io["norm1_g"][l, :].rearrange("(c q) -> q c", q=128),
                                                   allow_slow_non_contiguous=True), w=["gcol"])
            p.dma("sp", lambda e, l=l: e.dma_start(out=gcol[:, 1, :], in_=io["norm2_g"][l, :].rearrange("(c q) -> q c", q=128),
                                                   allow_slow_non_contiguous=True), w=["gcol"])
            for cc in range(12):
                wt = wts[it % 2]
                wk = f"adaw{it % 2}"
                it += 1
                p.dma("sp", lambda e, wt=wt, l=l, cc=cc: e.dma_start(
                    out=wt[:], in_=ada_w[l, :, cc * 512:(cc + 1) * 512].rearrange("(c q) n -> q c n", q=128)), w=[wk])
                for kc in range(KC):
                    p.op("pe", lambda e, wt=wt, kc=kc: e.matmul(mps[:], lhsT=sT[:, kc, :], rhs=wt[:, kc, :],
                                                                 start=(kc == 0), stop=(kc == KC - 1)),
                         r=["sT", wk], w=["mps"])
                p.op("dve", lambda e, cc=cc: e.tensor_tensor(out=mrow[:, cc * 512:(cc + 1) * 512], in0=mps[:],
                                                             in1=brow[:, cc * 512:(cc + 1) * 512], op=ALU.add),
                     r=["mps", "brow"], w=["mrow"])
            p.dma("sp", lambda e, l=l: e.dma_start(out=modrow[l], in_=mrow[:]), r=["mrow"], w=[f"modrow{l}"])
            for c in range(48):
                p.op("pe", lambda e, c=c: e.transpose(tps[:, c, :], mrow[0:2, c * 128:(c + 1) * 128], C["ident"][0:2, 0:2]),
                     r=["mrow", "ident"], w=["tps"])
            p.op("dve", lambda e: e.tensor_copy(out=mcol[:], in_=tps[:]), r=["tps"], w=["mcol"])
            for r in range(2):
                for (nm, sc_c, sh_c, gi) in (("1", 1, 0, 0), ("2", 4, 3, 1)):
                    A = G["A" + nm]
                    B = G["B" + nm]
                    p.op("dve", lambda e, A=A, r=r, l=l, sc_c=sc_c, gi=gi: e.scalar_tensor_tensor(
                        out=A[:, l, r, :], in0=mcol[:, sc_c * 8:(sc_c + 1) * 8, r], scalar=1.0, in1=gcol[:, gi, :],
                        op0=ALU.add, op1=ALU.mult), r=["mcol", "gcol"], w=["G"])
                    p.op("dve", lambda e, B=B, r=r, l=l, sh_c=sh_c: e.tensor_copy(
                        out=B[:, l, r, :], in_=mcol[:, sh_c * 8:(sh_c + 1) * 8, r]), r=["mcol"], w=["G"])


def norm_tile(p, C, T, xt_key, xt, Acol, Bcol, hT_dst, hT_key, hT32_dst=None, want_bf=True):
    ss, rstd, xn, junk = T["ss"], T["rstd"], T["xn"], T["junk"]
    p.op("act", lambda e: e.activation(out=junk[:], in_=xt[:], func=AF.Square, accum_out=ss[:, 0:1]),
         r=[xt_key], w=["junk", "ss"])
    p.op("dve", lambda e: e.tensor_scalar(out=rstd[:], in0=ss[:], scalar1=1.0 / D, scalar2=EPS, op0=ALU.mult, op1=ALU.add),
         r=["ss"], w=["rstd"])
    p.op("act", lambda e: e.activation(out=ss[:], in_=rstd[:], func=AF.Sqrt), r=["rstd"], w=["ss"])
    p.op("dve", lambda e: e.reciprocal(out=rstd[:], in_=ss[:]), r=["ss"], w=["rstd"])
    p.op("dve", lambda e: e.tensor_scalar(out=xn[:], in0=xt[:], scalar1=rstd[:, 0:1], scalar2=None, op0=ALU.mult),
         r=[xt_key, "rstd"], w=["xn"])
    for half in range(2):
        tp = T["tp"][half]
        tk = f"tp{half}"
        for j in range(4):
            kc = half * 4 + j
            p.op("pe", lambda e, tp=tp, j=j, kc=kc: e.transpose(tp[:, j, :], xn[:, kc * 128:(kc + 1) * 128], C["ident"][:]),
                 r=["xn", "ident"], w=[tk])
        for j in range(4):
            kc = half * 4 + j
            if hT32_dst is None:
                p.op("act", lambda e, tp=tp, j=j, kc=kc: e.activation(out=hT_dst(kc), in_=tp[:, j, :], func=AF.Identity,
                                                                       scale=Acol[:, kc:kc + 1], bias=Bcol[:, kc:kc + 1]),
                     r=[tk, "G"], w=[hT_key])
            else:
                p.op("act", lambda e, tp=tp, j=j, kc=kc: e.activation(out=hT32_dst(kc), in_=tp[:, j, :], func=AF.Identity,
                                                                       scale=Acol[:, kc:kc + 1], bias=Bcol[:, kc:kc + 1]),
                     r=[tk, "G"], w=[hT_key + "32"])
                if want_bf:
                    p.op("pool", lambda e, kc=kc: e.tensor_copy(out=hT_dst(kc), in_=hT32_dst(kc)), r=[hT_key + "32"], w=[hT_key])


def norm_scratch(p):
    T = {}
    T["ss"] = p.sb("ss", [128, 1], F32)
    T["rstd"] = p.sb("rstd", [128, 1], F32)
    T["xn"] = p.sb("xn", [128, D], F32)
    T["junk"] = p.sb("junk", [128, D], BF16)
    T["tp"] = [p.ps(f"tp{h}", [128, 4, 128], F32) for h in range(2)]
    return T


def load_cast_weight(p, dst, dst_key, src_ap, rows_kc, ncols, stage, stage_key, eng_cycle=("dve", "pool")):
    for kc in range(rows_kc):
        st = stage[kc % len(stage)]
        sk = f"{stage_key}{kc % len(stage)}"
        p.dma("sp", lambda e, st=st, kc=kc: e.dma_start(out=st[:, 0:ncols], in_=src_ap[kc * 128:(kc + 1) * 128, :]), w=[sk])
        eng = eng_cycle[kc % len(eng_cycle)]
        p.op(eng, lambda e, st=st, kc=kc: e.tensor_copy(out=dst[:, kc, :], in_=st[:, 0:ncols]), r=[sk], w=[dst_key])


def phase_inproj(p, C, io, G, l, n_ctx, n_tok):
    xres, PT, PTOK, w_in = io["xres"], io["PT"], io["PTOK"], io["ev_w_in"]
    with p.phase():
        T = norm_scratch(p)
        wbf = p.sb("winbf", [128, KC, EVEN_IN], BF16)
        stage = [p.sb(f"wstage{i}", [128, EVEN_IN], F32) for i in range(2)]
        load_cast_weight(p, wbf, "winbf", w_in, KC, EVEN_IN, stage, "wstage")
        xts = [p.sb(f"xt{i}", [128, D], F32) for i in range(2)]
        hT = [p.sb(f"hT{i}", [128, KC, 512], BF16) for i in range(2)]
        pps = [p.ps(f"pps{i}", [128, 512], F32) for i in range(3)]
        ost = [p.sb(f"ost{i}", [128, 512], F32) for i in range(4)]
        ntile = n_tok // 128
        nsup = (ntile + 3) // 4
        oi = 0
        pi = 0
        tok_cols = [(512, 512), (1024, 512), (GLA_COLS + 1024, 512)]
        for s in range(nsup):
            tiles = list(range(s * 4, min(ntile, s * 4 + 4)))
            N = len(tiles) * 128
            h = hT[s % 2]
            hk = f"hT{s % 2}"
            for j, ti in enumerate(tiles):
                r = 1 if ti * 128 < n_ctx else 0
                xt = xts[ti % 2]
                xk = f"xt{ti % 2}"
                p.dma("sp", lambda e, xt=xt, ti=ti: e.dma_start(out=xt[:], in_=xres[ti * 128:(ti + 1) * 128, :]), w=[xk])
                norm_tile(p, C, T, xk, xt, G["A1"][:, l, r, :], G["B1"][:, l, r, :],
                          lambda kc, h=h, j=j: h[:, kc, j * 128:(j + 1) * 128], hk)
            ncc = (EVEN_IN + 127) // 128
            for cc in range(ncc):
                cw = min(128, EVEN_IN - cc * 128)
                ps = pps[pi % 3]
                pk = f"pps{pi % 3}"
                pi += 1
                for kc in range(KC):
                    p.op("pe", lambda e, ps=ps, kc=kc, cc=cc, cw=cw, h=h, N=N: e.matmul(
                        ps[0:cw, 0:N], lhsT=wbf[:, kc, cc * 128:cc * 128 + cw], rhs=h[:, kc, 0:N],
                        start=(kc == 0), stop=(kc == KC - 1)), r=["winbf", hk], w=[pk])
                o = ost[oi % 4]
                ok = f"ost{oi % 4}"
                eng = "dve" if oi % 2 == 0 else "act"
                oi += 1
                if eng == "dve":
                    p.op("dve", lambda e, o=o, ps=ps, cw=cw, N=N: e.tensor_copy(out=o[0:cw, 0:N], in_=ps[0:cw, 0:N]), r=[pk], w=[ok])
                else:
                    p.op("act", lambda e, o=o, ps=ps, cw=cw, N=N: e.copy(out=o[0:cw, 0:N], in_=ps[0:cw, 0:N]), r=[pk], w=[ok])
                p.dma("sp", lambda e, o=o, cc=cc, cw=cw, N=N, s=s: e.dma_start(
                    out=PT[cc * 128:cc * 128 + cw, s * 512:s * 512 + N], in_=o[0:cw, 0:N]), r=[ok], w=["PT"])
            for j, ti in enumerate(tiles):
                for gi, (c0, cn) in enumerate(tok_cols):
                    ps = pps[pi % 3]
                    pk = f"pps{pi % 3}"
                    pi += 1
                    for kc in range(KC):
                        p.op("pe", lambda e, ps=ps, kc=kc, c0=c0, cn=cn, h=h, j=j: e.matmul(
                            ps[:, 0:cn], lhsT=h[:, kc, j * 128:(j + 1) * 128], rhs=wbf[:, kc, c0:c0 + cn],
                            start=(kc == 0), stop=(kc == KC - 1)), r=["winbf", hk], w=[pk])
                    o = ost[oi % 4]
                    ok = f"ost{oi % 4}"
                    eng = "dve" if oi % 2 == 0 else "act"
                    oi += 1
                    if eng == "dve":
                        p.op("dve", lambda e, o=o, ps=ps, cn=cn: e.tensor_copy(out=o[:, 0:cn], in_=ps[:, 0:cn]), r=[pk], w=[ok])
                    else:
                        p.op("act", lambda e, o=o, ps=ps, cn=cn: e.copy(out=o[:, 0:cn], in_=ps[:, 0:cn]), r=[pk], w=[ok])
                    p.dma("sp", lambda e, o=o, ti=ti, gi=gi, cn=cn: e.dma_start(
                        out=PTOK[ti * 128:(ti + 1) * 128, gi * 512:gi * 512 + cn], in_=o[:, 0:cn]), r=[ok], w=["PTOK"])

import os


class Ring:
    def __init__(self, p, name, shape, dt=F32, n=2, psum=False):
        self.t = [(p.ps if psum else p.sb)(f"{name}{i}", shape, dt) for i in range(n)]
        self.k = [f"{name}{i}" for i in range(n)]
        self.i = 0

    def next(self):
        i = self.i
        self.i = (i + 1) % len(self.t)
        return self.t[i], self.k[i]


class PsRing:
    def __init__(self, p, name, nbanks):
        self.banks = [p.ps(f"{name}{i}", [128, 512], F32) for i in range(nbanks)]
        self.n = nbanks * 1
        self.sub = 1
        self.name = name
        self.i = 0

    def next(self):
        i = self.i
        self.i = (i + 1) % self.n
        sb_ = self.sub
        return self.banks[i // sb_][:, (i % sb_) * 128:(i % sb_ + 1) * 128], f"{self.name}_{i}"


def make_masks(p, C):
    f, q = C["iota_f"], C["iota_p"]
    for nm, op in (("incl0", ALU.is_ge), ("incl1", ALU.is_le), ("strict0", ALU.is_gt), ("strict1", ALU.is_lt)):
        m = p.sb("m_" + nm, [128, 128], F32)
        p.op("dve", lambda e, m=m, op=op: e.tensor_scalar(out=m[:], in0=f[:], scalar1=q[:, 0:1], scalar2=None, op0=op),
             r=["iota_f", "iota_p"], w=["masks"])
        C[nm] = m
    for nm, val in (("ones128", 1.0 / 128), ("ones64", 1.0), ("ones64m", 1.0 / 64)):
        m = p.sb("m_" + nm, [128, 128], F32)
        p.op("dve", lambda e, m=m, val=val: e.memset(m[:], val), w=["masks"])
        C[nm] = m


def chunk_order(n_ctx, n_tok):
    nc_, nt = n_ctx // 128, n_tok // 128
    fwd = list(range(nt))
    bwd = list(range(nc_ - 1, -1, -1)) + list(range(nt - 1, nc_ - 1, -1))
    return [fwd, bwd]


def phase_gla(p, C, io, n_ctx, n_tok):
    PT, PTOK, YT = io["PT"], io["PTOK"], io["YTG"]
    gk_up, gk_b = io["gla_gk_up"], io["gla_gk_b"]
    order = chunk_order(n_ctx, n_tok)
    nt = n_tok // 128
    with p.phase():
        gkup = p.sb("gkup", [16, 2, 256], F32)
        gkb = p.sb("gkb", [64, 2, 4], F32)
        for d in range(2):
            p.dma("sp", lambda e, d=d: e.dma_start(out=gkup[:, d, :], in_=gk_up[d]), w=["gkw"])
            p.dma("sp", lambda e, d=d: e.dma_start(out=gkb[:, d, :], in_=gk_b[d].rearrange("(h q) -> q h", q=64),
                                                   allow_slow_non_contiguous=True), w=["gkw"])
        S = [[p.sb(f"S{d}{h}", [64, 128], F32) for h in range(4)] for d in range(2)]
        for d in range(2):
            for h in range(4):
                p.op("pool", lambda e, t=S[d][h]: e.memset(t[:], 0.0), w=[f"S{d}{h}"])
        gd = Ring(p, "gd", [16, 128], n=4)
        qk = Ring(p, "qk", [64, 2, 128], n=9)
        vv = Ring(p, "vv", [128, 128], n=9)
        sg = Ring(p, "sg", [64, 128], n=9)
        lg = Ring(p, "lg", [64, 128], n=9)
        lgT = Ring(p, "lgT", [128, 64], n=9)
        eW = Ring(p, "eW", [64, 128], n=9)
        eWi = Ring(p, "eWi", [64, 128], n=9)
        rt = Ring(p, "rt", [64, 128], n=9)
        kt = Ring(p, "kt", [64, 128], n=9)
        ktok = Ring(p, "ktok", [128, 64], n=9)
        mrk = Ring(p, "mrk", [128, 128], n=9)
        yts = Ring(p, "yts", [128, 128], n=9)
        s2 = Ring(p, "s2", [64, 128], n=9)
        psA = Ring(p, "psA", [128, 128], n=8, psum=True)
        def unit(d, h, c, gdt, gdk):
            t0 = c * 128
            tend = 127 if d == 0 else 0
            Sk = f"S{d}{h}"
            St = S[d][h]
            qt, qkk = qk.next()
            p.dma("sp", lambda e, qt=qt, h=h, t0=t0: e.dma_start(out=qt[:, 0, :], in_=PT[h * 64:(h + 1) * 64, t0:t0 + 128]), w=[qkk])
            p.dma("sp", lambda e, qt=qt, h=h, t0=t0: e.dma_start(out=qt[:, 1, :], in_=PT[256 + h * 64:256 + (h + 1) * 64, t0:t0 + 128]), w=[qkk])
            vt, vk = vv.next()
            p.dma("sp", lambda e, vt=vt, h=h, t0=t0: e.dma_start(out=vt[:], in_=PTOK[t0:t0 + 128, h * 128:(h + 1) * 128]), w=[vk])
            yield
            ps, pk = psA.next()
            p.op("pe", lambda e, ps=ps, d=d, h=h, gdt=gdt: e.matmul(ps[0:64, :], lhsT=gkup[:, d, h * 64:(h + 1) * 64], rhs=gdt[:],
                                                                    start=True, stop=True), r=["gkw", gdk], w=[pk])
            sgt, sgk = sg.next()
            p.op("act", lambda e, sgt=sgt, ps=ps, d=d, h=h: e.activation(out=sgt[:], in_=ps[0:64, :], func=AF.Sigmoid,
                                                                        bias=gkb[:, d, h:h + 1]), r=[pk, "gkw"], w=[sgk])
            lgt, lgk = lg.next()
            p.op("act", lambda e, lgt=lgt, sgt=sgt: e.activation(out=lgt[:], in_=sgt[:], func=AF.Ln), r=[sgk], w=[lgk])
            yield
            ps, pk = psA.next()
            p.op("pe", lambda e, ps=ps, lgt=lgt: e.transpose(ps[:, 0:64], lgt[:], C["ident"][0:64, 0:64]), r=[lgk, "ident"], w=[pk])
            lTt, lTk = lgT.next()
            p.op("dve", lambda e, lTt=lTt, ps=ps: e.tensor_copy(out=lTt[:], in_=ps[:, 0:64]), r=[pk], w=[lTk])
            yield
            ps, pk = psA.next()
            p.op("pe", lambda e, ps=ps, lTt=lTt, d=d: e.matmul(ps[0:64, :], lhsT=lTt[:], rhs=C[f"incl{d}"][:], start=True, stop=True),
                 r=[lTk, "masks"], w=[pk])
            eWt, eWk = eW.next()
            eWit, eWik = eWi.next()
            p.op("act", lambda e, eWt=eWt, ps=ps: e.activation(out=eWt[:], in_=ps[0:64, :], func=AF.Exp, scale=1.0 / 16), r=[pk], w=[eWk])
            p.op("act", lambda e, eWit=eWit, ps=ps: e.activation(out=eWit[:], in_=ps[0:64, :], func=AF.Exp, scale=-1.0 / 16), r=[pk], w=[eWik])
            rtt, rtk = rt.next()
            ktt, ktk = kt.next()
            p.op("dve", lambda e, rtt=rtt, qt=qt, eWt=eWt: e.scalar_tensor_tensor(out=rtt[:], in0=qt[:, 0, :], scalar=0.125, in1=eWt[:],
                                                                                 op0=ALU.mult, op1=ALU.mult), r=[qkk, eWk], w=[rtk])
            p.op("dve", lambda e, ktt=ktt, qt=qt, eWit=eWit: e.tensor_tensor(out=ktt[:], in0=qt[:, 1, :], in1=eWit[:], op=ALU.mult),
                 r=[qkk, eWik], w=[ktk])
            yield
            ps, pk = psA.next()
            p.op("pe", lambda e, ps=ps, ktt=ktt: e.transpose(ps[:, 0:64], ktt[:], C["ident"][0:64, 0:64]), r=[ktk, "ident"], w=[pk])
            kTt, kTk = ktok.next()
            p.op("dve", lambda e, kTt=kTt, ps=ps: e.tensor_copy(out=kTt[:], in_=ps[:, 0:64]), r=[pk], w=[kTk])
            yield
            ps, pk = psA.next()
            p.op("pe", lambda e, ps=ps, ktt=ktt, rtt=rtt: e.matmul(ps[:], lhsT=ktt[:], rhs=rtt[:], start=True, stop=True), r=[ktk, rtk], w=[pk])
            mt, mk = mrk.next()
            p.op("dve", lambda e, mt=mt, ps=ps, d=d: e.tensor_tensor(out=mt[:], in0=ps[:], in1=C[f"incl{d}"][:], op=ALU.mult),
                 r=[pk, "masks"], w=[mk])
            yield
            ps, pk = psA.next()
            p.op("pe", lambda e, ps=ps, St=St, rtt=rtt: e.matmul(ps[:], lhsT=St[:], rhs=rtt[:], start=True, stop=False), r=[Sk, rtk], w=[pk])
            p.op("pe", lambda e, ps=ps, vt=vt, mt=mt: e.matmul(ps[:], lhsT=vt[:], rhs=mt[:], start=False, stop=True), r=[vk, mk], w=[pk])
            yt, yk = yts.next()
            p.op("act", lambda e, yt=yt, ps=ps: e.copy(out=yt[:], in_=ps[:]), r=[pk], w=[yk])
            p.dma("sp", lambda e, yt=yt, d=d, h=h, t0=t0: e.dma_start(out=YT[d, h * 128:(h + 1) * 128, t0:t0 + 128], in_=yt[:]),
                  r=[yk], w=["YTG"])
            yield
            ps, pk = psA.next()
            p.op("pe", lambda e, ps=ps, kTt=kTt, vt=vt: e.matmul(ps[0:64, :], lhsT=kTt[:], rhs=vt[:], start=True, stop=True), r=[kTk, vk], w=[pk])
            s2t, s2k = s2.next()
            p.op("dve", lambda e, s2t=s2t, St=St, eWt=eWt, tend=tend: e.tensor_scalar(out=s2t[:], in0=St[:], scalar1=eWt[:, tend:tend + 1],
                                                                                     scalar2=None, op0=ALU.mult), r=[Sk, eWk], w=[s2k])
            p.op("dve", lambda e, s2t=s2t, St=St, eWt=eWt, ps=ps, tend=tend: e.scalar_tensor_tensor(
                out=St[:], in0=ps[0:64, :], scalar=eWt[:, tend:tend + 1], in1=s2t[:], op0=ALU.mult, op1=ALU.add),
                r=[pk, eWk, s2k], w=[Sk])


        def drive(gens):
            gens = list(gens)
            while gens:
                for g_ in list(gens):
                    try:
                        next(g_)
                    except StopIteration:
                        gens.remove(g_)

        for i in range(nt):
            gens = []
            for d in range(2):
                c = order[d][i]
                t0 = c * 128
                gdt, gdk = gd.next()
                p.dma("sp", lambda e, gdt=gdt, t0=t0: e.dma_start(out=gdt[:], in_=PT[1536:1552, t0:t0 + 128]), w=[gdk])
                gens += [unit(d, h, c, gdt, gdk) for h in range(4)]
            drive(gens)


def phase_gla_finish(p, C, io, n_tok):
    PT, YT, MIXT = io["PT"], io["YTG"], io["MIXT"]
    with p.phase():
        gcol = p.sb("gng", [128, 1], F32)
        p.dma("sp", lambda e: e.dma_start(out=gcol[:, 0:1], in_=io["gla_norm_g"].rearrange("(q o) -> q o", o=1)), w=["gng"])
        y0 = Ring(p, "y0", [128, 512], n=2)
        y1 = Ring(p, "y1", [128, 512], n=2)
        og = Ring(p, "og", [128, 512], n=2)
        sq = Ring(p, "sq", [128, 512], n=2)
        rs = Ring(p, "rs", [128, 512], n=2)
        ob = Ring(p, "ob", [128, 512], BF16, n=2)
        psF = Ring(p, "psF", [128, 512], n=2, psum=True)
        for t0 in range(0, n_tok, 512):
            N = min(512, n_tok - t0)
            for h in range(4):
                a, ak = y0.next()
                b, bk = y1.next()
                o, ok = og.next()
                p.dma("sp", lambda e, a=a, h=h, t0=t0, N=N: e.dma_start(out=a[:, 0:N], in_=YT[0, h * 128:(h + 1) * 128, t0:t0 + N]), r=["YTG"], w=[ak])
                p.dma("sp", lambda e, b=b, h=h, t0=t0, N=N: e.dma_start(out=b[:, 0:N], in_=YT[1, h * 128:(h + 1) * 128, t0:t0 + N]), r=["YTG"], w=[bk])
                p.dma("sp", lambda e, o=o, h=h, t0=t0, N=N: e.dma_start(out=o[:, 0:N], in_=PT[1024 + h * 128:1024 + (h + 1) * 128, t0:t0 + N]), r=["PT"], w=[ok])
                p.op("dve", lambda e, a=a, b=b, N=N: e.tensor_tensor(out=a[:, 0:N], in0=a[:, 0:N], in1=b[:, 0:N], op=ALU.add), r=[ak, bk], w=[ak])
                s, sk = sq.next()
                p.op("act", lambda e, s=s, a=a, N=N: e.activation(out=s[:, 0:N], in_=a[:, 0:N], func=AF.Square), r=[ak], w=[sk])
                ps, pk = psF.next()
                p.op("pe", lambda e, ps=ps, s=s, N=N: e.matmul(ps[:, 0:N], lhsT=C["ones128"][:], rhs=s[:, 0:N], start=True, stop=True), r=[sk, "masks"], w=[pk])
                r_, rk = rs.next()
                p.op("dve", lambda e, r_=r_, ps=ps, N=N: e.tensor_scalar(out=r_[:, 0:N], in0=ps[:, 0:N], scalar1=EPS, scalar2=None, op0=ALU.add), r=[pk], w=[rk])
                p.op("act", lambda e, r_=r_, N=N: e.activation(out=r_[:, 0:N], in_=r_[:, 0:N], func=AF.Sqrt), r=[rk], w=[rk])
                p.op("dve", lambda e, r_=r_, N=N: e.reciprocal(out=r_[:, 0:N], in_=r_[:, 0:N]), r=[rk], w=[rk])
                p.op("dve", lambda e, a=a, r_=r_, N=N: e.scalar_tensor_tensor(out=a[:, 0:N], in0=a[:, 0:N], scalar=gcol[:, 0:1], in1=r_[:, 0:N],
                                                                             op0=ALU.mult, op1=ALU.mult), r=[ak, rk, "gng"], w=[ak])
                p.op("act", lambda e, s=s, o=o, N=N: e.activation(out=s[:, 0:N], in_=o[:, 0:N], func=AF.Silu), r=[ok], w=[sk])
                ot, otk = ob.next()
                p.op("dve", lambda e, ot=ot, a=a, s=s, N=N: e.tensor_tensor(out=ot[:, 0:N], in0=a[:, 0:N], in1=s[:, 0:N], op=ALU.mult), r=[ak, sk], w=[otk])
                p.dma("sp", lambda e, ot=ot, h=h, t0=t0, N=N: e.dma_start(out=MIXT[h * 128:(h + 1) * 128, t0:t0 + N], in_=ot[:, 0:N]), r=[otk], w=["MIXT"])


import os
STAGE = 99
R0 = GLA_COLS
NSQ = 12
NLW = 0.6065306597126334


def phase_shift(p, C, io, n_ctx, n_tok):
    PT, SMT, mu = io["PT"], io["SMT"], io["rw_mu"]
    with p.phase():
        nch = (RW_COLS + 127) // 128
        muc = p.sb("muc", [128, nch, 2], F32)
        p.op("pool", lambda e: e.memset(muc[:], 0.0), w=["muc"])
        for j in range(2):
            for c in range(nch):
                cw = min(128, RW_COLS - c * 128)
                p.dma("sp", lambda e, j=j, c=c, cw=cw: e.dma_start(out=muc[0:cw, c, j:j + 1],
                                                                   in_=mu[j, c * 128:c * 128 + cw].rearrange("(q o) -> q o", o=1)), w=["muc"])
        xin = Ring(p, "shx", [128, 514], n=3)
        d0 = Ring(p, "shd", [128, 512], n=3)
        so = Ring(p, "sho", [128, 512], n=3)
        blocks = []
        for (a, b) in ((0, n_ctx), (n_ctx, n_tok)):
            t = a
            while t < b:
                N = min(512, b - t)
                blocks.append((t, N, t == a, t + N == b))
                t += N
        for (t0, N, first, last) in blocks:
            for c in range(nch):
                cw = min(128, RW_COLS - c * 128)
                x, xk = xin.next()
                lo = 0 if not first else 1
                hi = N + 2 if not last else N + 1
                if first or last:
                    p.op("pool", lambda e, x=x: e.memset(x[:], 0.0), w=[xk])
                p.dma("sp", lambda e, x=x, c=c, cw=cw, lo=lo, hi=hi, t0=t0: e.dma_start(
                    out=x[0:cw, lo:hi], in_=PT[R0 + c * 128:R0 + c * 128 + cw, t0 - 1 + lo:t0 - 1 + hi]), w=[xk])
                dd, dk = d0.next()
                o, ok = so.next()
                p.op("dve", lambda e, dd=dd, x=x, cw=cw, N=N: e.tensor_tensor(out=dd[0:cw, 0:N], in0=x[0:cw, 0:N], in1=x[0:cw, 1:N + 1], op=ALU.subtract),
                     r=[xk], w=[dk])
                p.op("dve", lambda e, o=o, dd=dd, x=x, c=c, cw=cw, N=N: e.scalar_tensor_tensor(
                    out=o[0:cw, 0:N], in0=dd[0:cw, 0:N], scalar=muc[0:cw, c, 0:1], in1=x[0:cw, 1:N + 1], op0=ALU.mult, op1=ALU.add),
                    r=[dk, xk, "muc"], w=[ok])
                p.op("pool", lambda e, dd=dd, x=x, cw=cw, N=N: e.tensor_tensor(out=dd[0:cw, 0:N], in0=x[0:cw, 2:N + 2], in1=x[0:cw, 1:N + 1], op=ALU.subtract),
                     r=[xk, ok], w=[dk])
                p.op("dve", lambda e, o=o, dd=dd, c=c, cw=cw, N=N: e.scalar_tensor_tensor(
                    out=o[0:cw, 0:N], in0=dd[0:cw, 0:N], scalar=muc[0:cw, c, 1:2], in1=o[0:cw, 0:N], op0=ALU.mult, op1=ALU.add),
                    r=[dk, ok, "muc"], w=[ok])
                p.dma("sp", lambda e, o=o, c=c, cw=cw, N=N, t0=t0: e.dma_start(out=SMT[c * 128:c * 128 + cw, t0:t0 + N], in_=o[0:cw, 0:N]),
                      r=[ok], w=["SMT"])


def colload(p, dst, key, src_vec, nh, j=None):
    out = dst[:, :] if j is None else dst[:, j, :]
    p.dma("sp", lambda e: e.dma_start(out=out, in_=src_vec.rearrange("(h q) -> q h", q=64), allow_slow_non_contiguous=True), w=[key])


def phase_rwkv(p, C, io, n_ctx, n_tok):
    SMT, YT, RK = io["SMT"], io["YTR"], io["RK"]
    order = chunk_order(n_ctx, n_tok)
    nt = n_tok // 128
    with p.phase():
        w2 = p.sb("rw2", [64, 2, 512], F32)
        a2 = p.sb("ra2", [64, 2, 512], F32)
        for d in range(2):
            p.dma("sp", lambda e, d=d: e.dma_start(out=w2[:, d, :], in_=io["rw_w2"][d]), w=["rww"])
            p.dma("sp", lambda e, d=d: e.dma_start(out=a2[:, d, :], in_=io["rw_a2"][d]), w=["rww"])
        w0 = p.sb("rw0", [64, 2, 8], F32)
        a0 = p.sb("ra0", [64, 2, 8], F32)
        for d in range(2):
            colload(p, w0, "rww", io["rw_w0"][d], 8, d)
            colload(p, a0, "rww", io["rw_a0"][d], 8, d)
        kkc = p.sb("rkk", [64, 8], F32)
        kac = p.sb("rka", [64, 8], F32)
        kam = p.sb("rkam", [64, 8], F32)
        rkc = p.sb("rrk", [64, 8], F32)
        colload(p, kkc, "rww", io["rw_k_k"], 8)
        colload(p, kac, "rww", io["rw_k_a"], 8)
        colload(p, rkc, "rww", io["rw_r_k"].rearrange("h n -> (h n)"), 8)
        p.op("dve", lambda e: e.tensor_scalar(out=kam[:], in0=kac[:], scalar1=-1.0, scalar2=1.0, op0=ALU.mult, op1=ALU.add), r=["rww"], w=["rkam"])
        T = [[p.sb(f"T{d}{h}", [64, 64], F32) for h in range(8)] for d in range(2)]
        for d in range(2):
            for h in range(8):
                p.op("pool", lambda e, t=T[d][h]: e.memset(t[:], 0.0), w=[f"T{d}{h}"])
        wa = Ring(p, "wa", [64, 2, 128], n=3)
        rkv = Ring(p, "rkv", [64, 3, 128], n=9)
        f64 = {nm: Ring(p, nm, [64, 128], n=9) for nm in
               ("sgw", "alp", "kq", "sqk", "nrm", "kk", "tmk", "kd", "eW", "eWi", "eWx", "cx", "rt", "kt", "bt", "at", "rk_", "yo")}
        tokr = {nm: Ring(p, nm, [128, 64], n=9) for nm in ("sgT", "Ktok", "Btok", "Vtok", "Xs", "Us")}
        sqm = {nm: Ring(p, nm, [128, 128], n=9) for nm in ("Akt", "Mrbt", "Mrkt")}
        sqh = [{nm: Ring(p, f"{nm}h{h_}", [128, 128], n=3) for nm in ("At", "Am", "Pt")} for h_ in range(8)]
        tw = Ring(p, "tw", [64, 64], n=9)
        psA = PsRing(p, "psR", 8)

        def mm(out_ap, pk, lhsT, rhs, r, start=True, stop=True):
            p.op("pe", lambda e: e.matmul(out_ap, lhsT=lhsT, rhs=rhs, start=start, stop=stop), r=r, w=[pk])

        def tr(out_ap, pk, in_ap, n, r):
            p.op("pe", lambda e: e.transpose(out_ap, in_ap, C["ident"][0:n, 0:n]), r=r + ["ident"], w=[pk])

        def unit(d, h, c, wat, wak):
            t0 = c * 128
            tend = 127 if d == 0 else 0
            sl = slice(t0, t0 + 128)
            hs = slice(h * 64, (h + 1) * 64)
            Tt, Tk = T[d][h], f"T{d}{h}"
            x, xk = rkv.next()
            for j in range(3):
                p.dma("sp", lambda e, x=x, j=j, h=h, sl=sl: e.dma_start(out=x[:, j, :], in_=SMT[j * 512 + h * 64:j * 512 + (h + 1) * 64, sl]), w=[xk])
            r_, k_, v_ = x[:, 0, :], x[:, 1, :], x[:, 2, :]
            yield
            ps, pk = psA.next()
            mm(ps[0:64, :], pk, w2[:, d, hs], wat[:, 0, :], ["rww", wak])
            sgw, sgwk = f64["sgw"].next()
            p.op("act", lambda e, sgw=sgw, ps=ps, d=d, h=h: e.activation(out=sgw[:], in_=ps[0:64, :], func=AF.Sigmoid, bias=w0[:, d, h:h + 1]),
                 r=[pk, "rww"], w=[sgwk])
            yield
            ps, pk = psA.next()
            mm(ps[0:64, :], pk, a2[:, d, hs], wat[:, 1, :], ["rww", wak])
            alp, alpk = f64["alp"].next()
            p.op("act", lambda e, alp=alp, ps=ps, d=d, h=h: e.activation(out=alp[:], in_=ps[0:64, :], func=AF.Sigmoid, bias=a0[:, d, h:h + 1]),
                 r=[pk, "rww"], w=[alpk])
            kq, kqk = f64["kq"].next()
            p.op("dve", lambda e, kq=kq, k_=k_, h=h: e.tensor_scalar(out=kq[:], in0=k_, scalar1=kkc[:, h:h + 1], scalar2=None, op0=ALU.mult),
                 r=[xk, "rww"], w=[kqk])
            sqk, sqkk = f64["sqk"].next()
            p.op("act", lambda e, sqk=sqk, kq=kq: e.activation(out=sqk[:], in_=kq[:], func=AF.Square), r=[kqk], w=[sqkk])
            yield
            ps, pk = psA.next()
            mm(ps[0:64, :], pk, C["ones64"][0:64, 0:64], sqk[:], ["masks", sqkk])
            nrm, nrmk = f64["nrm"].next()
            p.op("act", lambda e, nrm=nrm, ps=ps: e.activation(out=nrm[:], in_=ps[0:64, :], func=AF.Sqrt), r=[pk], w=[nrmk])
            p.op("dve", lambda e, nrm=nrm: e.tensor_scalar(out=nrm[:], in0=nrm[:], scalar1=1e-12, scalar2=None, op0=ALU.max), r=[nrmk], w=[nrmk])
            p.op("dve", lambda e, nrm=nrm: e.reciprocal(out=nrm[:], in_=nrm[:]), r=[nrmk], w=[nrmk])
            kk, kkk = f64["kk"].next()
            p.op("dve", lambda e, kk=kk, kq=kq, nrm=nrm: e.tensor_tensor(out=kk[:], in0=kq[:], in1=nrm[:], op=ALU.mult), r=[kqk, nrmk], w=[kkk])
            tmk, tmkk = f64["tmk"].next()
            p.op("dve", lambda e, tmk=tmk, alp=alp, h=h: e.tensor_scalar(out=tmk[:], in0=alp[:], scalar1=kac[:, h:h + 1], scalar2=kam[:, h:h + 1],
                                                                        op0=ALU.mult, op1=ALU.add), r=[alpk, "rww", "rkam"], w=[tmkk])
            kd, kdk = f64["kd"].next()
            p.op("dve", lambda e, kd=kd, tmk=tmk, k_=k_: e.tensor_tensor(out=kd[:], in0=tmk[:], in1=k_, op=ALU.mult), r=[tmkk, xk], w=[kdk])
            rk_, rkk = f64["rk_"].next()
            p.op("dve", lambda e, rk_=rk_, r_=r_, kd=kd, h=h: e.scalar_tensor_tensor(out=rk_[:], in0=r_, scalar=rkc[:, h:h + 1], in1=kd[:],
                                                                                     op0=ALU.mult, op1=ALU.mult), r=[xk, kdk, "rww"], w=[rkk])
            p.dma("sp", lambda e, rk_=rk_, d=d, hs=hs, sl=sl: e.dma_start(out=RK[d, hs, sl], in_=rk_[:]), r=[rkk], w=["RK"])
            yield
            ps, pk = psA.next()
            tr(ps[:, 0:64], pk, sgw[:], 64, [sgwk])
            sgT, sgTk = tokr["sgT"].next()
            p.op("dve", lambda e, sgT=sgT, ps=ps: e.tensor_copy(out=sgT[:], in_=ps[:, 0:64]), r=[pk], w=[sgTk])
            yield
            ps, pk = psA.next()
            mm(ps[0:64, :], pk, sgT[:], C[f"incl{d}"][:], [sgTk, "masks"])
            eW, eWk = f64["eW"].next()
            eWi, eWik = f64["eWi"].next()
            cx, cxk = f64["cx"].next()
            eWx, eWxk = f64["eWx"].next()
            p.op("act", lambda e, eW=eW, ps=ps: e.activation(out=eW[:], in_=ps[0:64, :], func=AF.Exp, scale=-NLW), r=[pk], w=[eWk])
            p.op("act", lambda e, eWi=eWi, ps=ps: e.activation(out=eWi[:], in_=ps[0:64, :], func=AF.Exp, scale=NLW), r=[pk], w=[eWik])
            p.op("dve", lambda e, cx=cx, ps=ps, sgw=sgw: e.tensor_tensor(out=cx[:], in0=ps[0:64, :], in1=sgw[:], op=ALU.subtract), r=[pk, sgwk], w=[cxk])
            p.op("act", lambda e, eWx=eWx, cx=cx: e.activation(out=eWx[:], in_=cx[:], func=AF.Exp, scale=-NLW), r=[cxk], w=[eWxk])
            rt, rtk = f64["rt"].next()
            kt, ktk = f64["kt"].next()
            bt, btk = f64["bt"].next()
            at, atk = f64["at"].next()
            p.op("dve", lambda e, rt=rt, r_=r_, eW=eW: e.tensor_tensor(out=rt[:], in0=r_, in1=eW[:], op=ALU.mult), r=[xk, eWk], w=[rtk])
            p.op("pool", lambda e, kt=kt, kd=kd, eWi=eWi: e.tensor_tensor(out=kt[:], in0=kd[:], in1=eWi[:], op=ALU.mult), r=[kdk, eWik], w=[ktk])
            p.op("dve", lambda e, bt=bt, kk=kk, alp=alp: e.tensor_tensor(out=bt[:], in0=kk[:], in1=alp[:], op=ALU.mult), r=[kkk, alpk], w=[btk])
            p.op("dve", lambda e, bt=bt, eWi=eWi: e.tensor_tensor(out=bt[:], in0=bt[:], in1=eWi[:], op=ALU.mult), r=[btk, eWik], w=[btk])
            p.op("dve", lambda e, at=at, kk=kk, eWx=eWx: e.scalar_tensor_tensor(out=at[:], in0=kk[:], scalar=-1.0, in1=eWx[:], op0=ALU.mult, op1=ALU.mult),
                 r=[kkk, eWxk], w=[atk])
            toks = {}
            for nm, src, sk in (("Ktok", kt[:], ktk), ("Btok", bt[:], btk), ("Vtok", v_, xk)):
                yield
                ps, pk = psA.next()
                tr(ps[:, 0:64], pk, src, 64, [sk])
                tt, ttk = tokr[nm].next()
                p.op("act" if nm != "Vtok" else "dve", (lambda e, tt=tt, ps=ps: e.copy(out=tt[:], in_=ps[:, 0:64])) if nm != "Vtok" else
                     (lambda e, tt=tt, ps=ps: e.tensor_copy(out=tt[:], in_=ps[:, 0:64])), r=[pk], w=[ttk])
                toks[nm] = (tt, ttk)
            Ktok, Ktokk = toks["Ktok"]
            Btok, Btokk = toks["Btok"]
            Vtok, Vtokk = toks["Vtok"]
            def gram(nm, lhsT, lk, rhs, rk2, mask):
                ps, pk = psA.next()
                mm(ps[:, :], pk, lhsT, rhs, [lk, rk2])
                m, mk = (sqh[h][nm] if nm in sqh[h] else sqm[nm]).next()
                p.op("dve", lambda e: e.tensor_tensor(out=m[:], in0=ps[:, :], in1=C[mask][:], op=ALU.mult), r=[pk, "masks"], w=[mk])
                return m, mk
            At, Atk = gram("At", bt[:], btk, at[:], atk, f"strict{d}")
            yield
            Am, Amk = gram("Am", at[:], atk, bt[:], btk, f"strict{1 - d}")
            yield
            Akt, Aktk = gram("Akt", kt[:], ktk, at[:], atk, f"strict{d}")
            yield
            Mrbt, Mrbtk = gram("Mrbt", bt[:], btk, rt[:], rtk, f"incl{d}")
            yield
            Mrkt, Mrktk = gram("Mrkt", kt[:], ktk, rt[:], rtk, f"incl{d}")
            yield
            Pt, Ptk = sqh[h]["Pt"].next()
            p.op("pool", lambda e, Pt=Pt, At=At: e.tensor_tensor(out=Pt[:], in0=At[:], in1=C["ident"][:], op=ALU.add), r=[Atk, "ident"], w=[Ptk])
            for step in range(6):
                yield
                ps, pk = psA.next()
                mm(ps[:, :], pk, At[:], Am[:], [Atk, Amk])
                Am2, Am2k = sqh[h]["Am"].next()
                p.op("act", lambda e, Am2=Am2, ps=ps: e.copy(out=Am2[:], in_=ps[:, :]), r=[pk], w=[Am2k])
                if step < 5:
                    yield
                    ps, pk = psA.next()
                    mm(ps[:, :], pk, Am[:], At[:], [Atk, Amk])
                    At2, At2k = sqh[h]["At"].next()
                    p.op("dve", lambda e, At2=At2, ps=ps: e.tensor_copy(out=At2[:], in_=ps[:, :]), r=[pk], w=[At2k])
                yield
                ps, pk = psA.next()
                mm(ps[:, :], pk, Am2[:], Pt[:], [Am2k, Ptk])
                Pt2, Pt2k = sqh[h]["Pt"].next()
                p.op("dve", lambda e, Pt2=Pt2, ps=ps, Pt=Pt: e.tensor_tensor(out=Pt2[:], in0=ps[:, :], in1=Pt[:], op=ALU.add), r=[pk, Ptk], w=[Pt2k])
                Pt, Ptk = Pt2, Pt2k
                Am, Amk = Am2, Am2k
                if step < 5:
                    At, Atk = At2, At2k
            yield
            ps, pk = psA.next()
            mm(ps[:, 0:64], pk, at[:], Tt[:], [atk, Tk], start=True, stop=False)
            mm(ps[:, 0:64], pk, Akt[:], Vtok[:], [Aktk, Vtokk], start=False, stop=True)
            Xs, Xsk = tokr["Xs"].next()
            p.op("act", lambda e, Xs=Xs, ps=ps: e.copy(out=Xs[:], in_=ps[:, 0:64]), r=[pk], w=[Xsk])
            yield
            ps, pk = psA.next()
            mm(ps[:, 0:64], pk, Pt[:], Xs[:], [Ptk, Xsk])
            Us, Usk = tokr["Us"].next()
            p.op("dve", lambda e, Us=Us, ps=ps: e.tensor_copy(out=Us[:], in_=ps[:, 0:64]), r=[pk], w=[Usk])
            yield
            ps, pk = psA.next()
            mm(ps[0:64, :], pk, Tt[:], rt[:], [Tk, rtk], start=True, stop=False)
            mm(ps[0:64, :], pk, Us[:], Mrbt[:], [Usk, Mrbtk], start=False, stop=False)
            mm(ps[0:64, :], pk, Vtok[:], Mrkt[:], [Vtokk, Mrktk], start=False, stop=True)
            yo, yok = f64["yo"].next()
            p.op("act", lambda e, yo=yo, ps=ps: e.copy(out=yo[:], in_=ps[0:64, :]), r=[pk], w=[yok])
            p.dma("sp", lambda e, yo=yo, d=d, hs=hs, sl=sl: e.dma_start(out=YT[d, hs, sl], in_=yo[:]), r=[yok], w=["YTR"])
            yield
            ps, pk = psA.next()
            mm(ps[0:64, 0:64], pk, Btok[:], Us[:], [Btokk, Usk], start=True, stop=False)
            mm(ps[0:64, 0:64], pk, Ktok[:], Vtok[:], [Ktokk, Vtokk], start=False, stop=True)
            t2, t2k = tw.next()
            p.op("dve", lambda e, t2=t2, Tt=Tt, eW=eW, tend=tend: e.tensor_scalar(out=t2[:], in0=Tt[:], scalar1=eW[:, tend:tend + 1], scalar2=None, op0=ALU.mult),
                 r=[Tk, eWk], w=[t2k])
            p.op("dve", lambda e, t2=t2, Tt=Tt, eW=eW, ps=ps, tend=tend: e.scalar_tensor_tensor(
                out=Tt[:], in0=ps[0:64, 0:64], scalar=eW[:, tend:tend + 1], in1=t2[:], op0=ALU.mult, op1=ALU.add), r=[pk, eWk, t2k], w=[Tk])


        def drive(gens):
            gens = list(gens)
            while gens:
                for g_ in list(gens):
                    try:
                        next(g_)
                    except StopIteration:
                        gens.remove(g_)

        for i in range(nt):
            for d in range(2):
                c = order[d][i]
                t0 = c * 128
                tend = 127 if d == 0 else 0
                sl = slice(t0, t0 + 128)
                wat, wak = wa.next()
                p.dma("sp", lambda e, wat=wat, sl=sl: e.dma_start(out=wat[:, 0, :], in_=SMT[1536:1600, sl]), w=[wak])
                p.dma("sp", lambda e, wat=wat, sl=sl: e.dma_start(out=wat[:, 1, :], in_=SMT[1600:1664, sl]), w=[wak])
                p.op("act", lambda e, wat=wat: e.activation(out=wat[:, 0, :], in_=wat[:, 0, :], func=AF.Tanh), r=[wak], w=[wak])
                drive([unit(d, h, c, wat, wak) for h in range(8)])


def phase_rwkv_finish(p, C, io, n_tok):
    SMT, YT, RK, MIXT = io["SMT"], io["YTR"], io["RK"], io["MIXT"]
    with p.phase():
        g2a = p.sb("g2a", [128, 512], F32)
        g2b = p.sb("g2b", [32, 512], F32)
        p.dma("sp", lambda e: e.dma_start(out=g2a[:], in_=io["rw_g2"][0:128, :]), w=["fw"])
        p.dma("sp", lambda e: e.dma_start(out=g2b[:], in_=io["rw_g2"][128:160, :]), w=["fw"])
        lnw = p.sb("lnw", [64, 8], F32)
        lnb = p.sb("lnb", [64, 8], F32)
        colload(p, lnw, "fw", io["rw_ln_w"], 8)
        colload(p, lnb, "fw", io["rw_ln_b"], 8)
        sga = Ring(p, "sga", [128, 512], n=2)
        sgb = Ring(p, "sgb", [32, 512], n=2)
        R = {nm: Ring(p, nm, [64, 512], n=2) for nm in ("fy0", "fy1", "fr0", "fr1", "fv", "fyc", "fsq", "frs", "fbn")}
        ob = Ring(p, "fob", [64, 512], BF16, n=2)
        psF = Ring(p, "psG", [64, 512], n=4, psum=True)
        for t0 in range(0, n_tok, 512):
            N = min(512, n_tok - t0)
            sl = slice(t0, t0 + N)
            a_, ak = sga.next()
            b_, bk = sgb.next()
            p.dma("sp", lambda e, a_=a_, sl=sl, N=N: e.dma_start(out=a_[:, 0:N], in_=SMT[1664:1792, sl]), w=[ak])
            p.dma("sp", lambda e, b_=b_, sl=sl, N=N: e.dma_start(out=b_[:, 0:N], in_=SMT[1792:1824, sl]), w=[bk])
            p.op("act", lambda e, a_=a_, N=N: e.activation(out=a_[:, 0:N], in_=a_[:, 0:N], func=AF.Sigmoid), r=[ak], w=[ak])
            p.op("act", lambda e, b_=b_, N=N: e.activation(out=b_[:, 0:N], in_=b_[:, 0:N], func=AF.Sigmoid), r=[bk], w=[bk])
            for h in range(8):
                hs = slice(h * 64, (h + 1) * 64)
                y0, y0k = R["fy0"].next()
                y1, y1k = R["fy1"].next()
                r0, r0k = R["fr0"].next()
                r1, r1k = R["fr1"].next()
                v, vk = R["fv"].next()
                for (t, k_, src) in ((y0, y0k, YT[0, hs, sl]), (y1, y1k, YT[1, hs, sl]), (r0, r0k, RK[0, hs, sl]), (r1, r1k, RK[1, hs, sl]),
                                     (v, vk, SMT[1024 + h * 64:1024 + (h + 1) * 64, sl])):
                    p.dma("sp", lambda e, t=t, src=src, N=N: e.dma_start(out=t[:, 0:N], in_=src), w=[k_])
                p.op("dve", lambda e, y0=y0, y1=y1, N=N: e.tensor_tensor(out=y0[:, 0:N], in0=y0[:, 0:N], in1=y1[:, 0:N], op=ALU.add), r=[y0k, y1k], w=[y0k])
                p.op("pool", lambda e, r0=r0, r1=r1, N=N: e.tensor_tensor(out=r0[:, 0:N], in0=r0[:, 0:N], in1=r1[:, 0:N], op=ALU.add), r=[r0k, r1k], w=[r0k])
                ps, pk = psF.next()
                p.op("pe", lambda e, ps=ps, y0=y0, N=N: e.matmul(ps[:, 0:N], lhsT=C["ones64m"][0:64, 0:64], rhs=y0[:, 0:N], start=True, stop=True), r=[y0k, "masks"], w=[pk])
                yc, yck = R["fyc"].next()
                p.op("dve", lambda e, yc=yc, y0=y0, ps=ps, N=N: e.tensor_tensor(out=yc[:, 0:N], in0=y0[:, 0:N], in1=ps[:, 0:N], op=ALU.subtract), r=[y0k, pk], w=[yck])
                sq, sqk = R["fsq"].next()
                p.op("act", lambda e, sq=sq, yc=yc, N=N: e.activation(out=sq[:, 0:N], in_=yc[:, 0:N], func=AF.Square), r=[yck], w=[sqk])
                ps, pk = psF.next()
                p.op("pe", lambda e, ps=ps, sq=sq, N=N: e.matmul(ps[:, 0:N], lhsT=C["ones64m"][0:64, 0:64], rhs=sq[:, 0:N], start=True, stop=True), r=[sqk, "masks"], w=[pk])
                rs, rsk = R["frs"].next()
                p.op("dve", lambda e, rs=rs, ps=ps, N=N: e.tensor_scalar(out=rs[:, 0:N], in0=ps[:, 0:N], scalar1=64e-5, scalar2=None, op0=ALU.add), r=[pk], w=[rsk])
                p.op("act", lambda e, rs=rs, N=N: e.activation(out=rs[:, 0:N], in_=rs[:, 0:N], func=AF.Sqrt), r=[rsk], w=[rsk])
                p.op("dve", lambda e, rs=rs, N=N: e.reciprocal(out=rs[:, 0:N], in_=rs[:, 0:N]), r=[rsk], w=[rsk])
                p.op("dve", lambda e, yc=yc, rs=rs, N=N: e.tensor_tensor(out=yc[:, 0:N], in0=yc[:, 0:N], in1=rs[:, 0:N], op=ALU.mult), r=[yck, rsk], w=[yck])
                p.op("dve", lambda e, yc=yc, h=h, N=N: e.tensor_scalar(out=yc[:, 0:N], in0=yc[:, 0:N], scalar1=lnw[:, h:h + 1], scalar2=lnb[:, h:h + 1],
                                                                      op0=ALU.mult, op1=ALU.add), r=[yck, "fw"], w=[yck])
                ps, pk = psF.next()
                p.op("pe", lambda e, ps=ps, r0=r0, N=N: e.matmul(ps[:, 0:N], lhsT=C["ones64"][0:64, 0:64], rhs=r0[:, 0:N], start=True, stop=True), r=[r0k, "masks"], w=[pk])
                bn, bnk = R["fbn"].next()
                p.op("dve", lambda e, bn=bn, ps=ps, v=v, N=N: e.tensor_tensor(out=bn[:, 0:N], in0=ps[:, 0:N], in1=v[:, 0:N], op=ALU.mult), r=[pk, vk], w=[bnk])
                p.op("pool", lambda e, bn=bn, yc=yc, N=N: e.tensor_tensor(out=bn[:, 0:N], in0=bn[:, 0:N], in1=yc[:, 0:N], op=ALU.add), r=[bnk, yck], w=[bnk])
                ps, pk = psF.next()
                p.op("pe", lambda e, ps=ps, a_=a_, hs=hs, N=N: e.matmul(ps[:, 0:N], lhsT=g2a[:, hs], rhs=a_[:, 0:N], start=True, stop=False), r=["fw", ak], w=[pk])
                p.op("pe", lambda e, ps=ps, b_=b_, hs=hs, N=N: e.matmul(ps[:, 0:N], lhsT=g2b[:, hs], rhs=b_[:, 0:N], start=False, stop=True), r=["fw", bk], w=[pk])
                o, ok = ob.next()
                p.op("dve", lambda e, o=o, bn=bn, ps=ps, N=N: e.tensor_tensor(out=o[:, 0:N], in0=ps[:, 0:N], in1=bn[:, 0:N], op=ALU.mult), r=[pk, bnk], w=[ok])
                p.dma("sp", lambda e, o=o, h=h, sl=sl, N=N: e.dma_start(out=MIXT[512 + h * 64:512 + (h + 1) * 64, sl], in_=o[:, 0:N]), r=[ok], w=["MIXT"])


def phase_outproj(p, C, io, l, w_out, n_ctx, n_tok, t_lo=0):
    xres, MIXT, modrow = io["xres"], io["MIXT"], io["modrow"]
    with p.phase():
        wbf = p.sb("woutbf", [128, KC, D], BF16)
        stage = [p.sb(f"wostage{i}", [128, D], F32) for i in range(2)]
        load_cast_weight(p, wbf, "woutbf", w_out, KC, D, stage, "wostage")
        gt = p.sb("gt1row", [128, 2, D], F32)
        for r in range(2):
            p.dma("sp", lambda e, r=r: e.dma_start(out=gt[:, r, :], in_=modrow[l, r, 2048:3072].partition_broadcast(128)), w=["gt1row"])
        mx = Ring(p, "mx", [128, KC, 128], BF16, n=2)
        xt = Ring(p, "oxt", [128, D], n=2)
        tmp = Ring(p, "otmp", [128, D], n=2)
        pso = Ring(p, "pso", [128, 512], n=4, psum=True)
        for ti in range(t_lo // 128, n_tok // 128):
            r = 1 if ti * 128 < n_ctx else 0
            sl = slice(ti * 128, (ti + 1) * 128)
            m, mk = mx.next()
            p.dma("sp", lambda e, m=m, sl=sl: e.dma_start(out=m[:], in_=MIXT[:, sl].rearrange("(c q) t -> q c t", q=128)), w=[mk])
            x, xk = xt.next()
            p.dma("sp", lambda e, x=x, sl=sl: e.dma_start(out=x[:], in_=xres[sl, :]), w=[xk])
            t, tk = tmp.next()
            for n in range(2):
                ps, pk = pso.next()
                for kc in range(KC):
                    p.op("pe", lambda e, ps=ps, m=m, kc=kc, n=n: e.matmul(ps[:], lhsT=m[:, kc, :], rhs=wbf[:, kc, n * 512:(n + 1) * 512],
                                                                          start=(kc == 0), stop=(kc == KC - 1)), r=[mk, "woutbf"], w=[pk])
                p.op("dve", lambda e, t=t, ps=ps, n=n, r=r: e.tensor_tensor(out=t[:, n * 512:(n + 1) * 512], in0=ps[:], in1=gt[:, r, n * 512:(n + 1) * 512], op=ALU.mult),
                     r=[pk, "gt1row"], w=[tk])
            p.op("pool", lambda e, t=t, x=x: e.tensor_tensor(out=t[:], in0=t[:], in1=x[:], op=ALU.add), r=[tk, xk], w=[tk])
            p.dma("sp", lambda e, t=t, sl=sl: e.dma_start(out=xres[sl, :], in_=t[:]), r=[tk], w=["xres"])


def phase_final(p, C, io, n_ctx, n_tok):
    xres, out = io["xres"], io["out"]
    with p.phase():
        g = p.sb("fng", [128, D], F32)
        p.dma("sp", lambda e: e.dma_start(out=g[:], in_=io["final_norm_g"].partition_broadcast(128)), w=["fng"])
        xt = Ring(p, "fxt", [128, D], n=3)
        junk = p.sb("fjunk", [128, D], BF16)
        ss = Ring(p, "fss", [128, 1], n=3)
        rs = Ring(p, "frst", [128, 1], n=3)
        for ti in range(n_ctx // 128, n_tok // 128):
            sl = slice(ti * 128, (ti + 1) * 128)
            x, xk = xt.next()
            s, sk = ss.next()
            r_, rk = rs.next()
            p.dma("sp", lambda e, x=x, sl=sl: e.dma_start(out=x[:], in_=xres[sl, :]), w=[xk])
            p.op("act", lambda e, x=x, s=s: e.activation(out=junk[:], in_=x[:], func=AF.Square, accum_out=s[:, 0:1]), r=[xk], w=["fjunk", sk])
            p.op("dve", lambda e, r_=r_, s=s: e.tensor_scalar(out=r_[:], in0=s[:], scalar1=1.0 / D, scalar2=EPS, op0=ALU.mult, op1=ALU.add), r=[sk], w=[rk])
            p.op("act", lambda e, r_=r_: e.activation(out=r_[:], in_=r_[:], func=AF.Sqrt), r=[rk], w=[rk])
            p.op("dve", lambda e, r_=r_: e.reciprocal(out=r_[:], in_=r_[:]), r=[rk], w=[rk])
            p.op("dve", lambda e, x=x, r_=r_: e.scalar_tensor_tensor(out=x[:], in0=x[:], scalar=r_[:, 0:1], in1=g[:], op0=ALU.mult, op1=ALU.mult),
                 r=[xk, rk, "fng"], w=[xk])
            o0 = ti * 128 - n_ctx
            p.dma("sp", lambda e, x=x, o0=o0: e.dma_start(out=out[o0:o0 + 128, :], in_=x[:]), r=[xk], w=["out"])

RST = 9

DE = 1024
SW_LIMIT = 7.0
SW_ALPHA = 1.702


def phase_moe_cast(p, C, io, l, NE):
    gu, dn, WGU, WDN = io["moe_gu_w"], io["moe_down_w"], io["WGU"], io["WDN"]
    with p.phase():
        sg = Ring(p, "cg32", [128, 2, 2048], n=2)
        sb = Ring(p, "cg16", [128, 2, 2048], BF16, n=2)
        sd = Ring(p, "cd32", [128, 2, 1024], n=2)
        sdb = Ring(p, "cd16", [128, 2, 1024], BF16, n=2)
        engs = ("dve", "pool", "act")
        it = 0
        for e_ in range(NE):
            for k2 in range(4):
                for (src, dst, r32, r16) in ((gu, WGU, sg, sb), (dn, WDN, sd, sdb)):
                    a, ak = r32.next()
                    b, bk = r16.next()
                    rows = slice(k2 * 256, (k2 + 1) * 256)
                    p.dma("sp", lambda e, a=a, src=src, e_=e_, rows=rows: e.dma_start(out=a[:], in_=src[l, e_, rows, :].rearrange("(c q) n -> q c n", q=128)), w=[ak])
                    eng = engs[it % 3]
                    it += 1
                    if eng == "act":
                        p.op("act", lambda e, a=a, b=b: e.copy(out=b[:], in_=a[:]), r=[ak], w=[bk])
                    else:
                        p.op(eng, lambda e, a=a, b=b: e.tensor_copy(out=b[:], in_=a[:]), r=[ak], w=[bk])
                    p.dma("act", lambda e, b=b, dst=dst, e_=e_, rows=rows: e.dma_start(out=dst[e_, rows, :].rearrange("(c q) n -> q c n", q=128), in_=b[:]), r=[bk], w=["WBF"])


def phase_moe_route(p, C, io, G, l, NE, t_lo, t_hi, n_ctx):
    xres, HT, GATE = io["xres"], io["HT"], io["GATE"]
    with p.phase():
        T = norm_scratch(p)
        wr = p.sb("wr", [128, KC, NE], F32)
        p.dma("sp", lambda e: e.dma_start(out=wr[:], in_=io["router_w"][l].rearrange("(c q) n -> q c n", q=128)), w=["wr"])
        br = p.sb("br", [128, NE], F32)
        p.dma("sp", lambda e: e.dma_start(out=br[:], in_=io["router_b"][l].partition_broadcast(128)), w=["wr"])
        xt = Ring(p, "mxt", [128, D], n=2)
        hb = Ring(p, "mhb", [128, KC, 128], BF16, n=2)
        h32 = Ring(p, "mh32", [128, KC, 128], n=2)
        lg = Ring(p, "mlg", [128, NE], n=2)
        ex = Ring(p, "mex", [128, NE], n=2)
        mk = Ring(p, "mmk", [128, NE], n=2)
        m8 = Ring(p, "mm8", [128, 8], n=2)
        sc = Ring(p, "msc", [128, 2], n=2)
        psr = p.ps("psr", [128, NE], F32)
        for ti in range(t_lo // 128, t_hi // 128):
            r = 1 if ti * 128 < n_ctx else 0
            sl = slice(ti * 128, (ti + 1) * 128)
            x, xk = xt.next()
            p.dma("sp", lambda e, x=x, sl=sl: e.dma_start(out=x[:], in_=xres[sl, :]), w=[xk])
            b, bk = hb.next()
            f, fk = h32.next()
            norm_tile(p, C, T, xk, x, G["A2"][:, l, r, :], G["B2"][:, l, r, :], lambda kc, b=b: b[:, kc, :], bk,
                      hT32_dst=lambda kc, f=f: f[:, kc, :])
            fk32 = bk + "32"
            p.dma("sp", lambda e, b=b, sl=sl: e.dma_start(out=HT[:, sl].rearrange("(c q) t -> q c t", q=128), in_=b[:]), r=[bk], w=["HT"])
            if RST < 2:
                continue
            for kc in range(KC):
                p.op("pe", lambda e, f=f, kc=kc: e.matmul(psr[:], lhsT=f[:, kc, :], rhs=wr[:, kc, :], start=(kc == 0), stop=(kc == KC - 1)),
                     r=[fk32, "wr"], w=["psr"])
            g, gk = lg.next()
            p.op("dve", lambda e, g=g: e.tensor_tensor(out=g[:], in0=psr[:], in1=br[:], op=ALU.add), r=["psr", "wr"], w=[gk])
            if RST < 3:
                continue
            m, mk8 = m8.next()
            p.op("dve", lambda e, m=m, g=g: e.max(out=m[:], in_=g[:]), r=[gk], w=[mk8])
            msk, mskk = mk.next()
            p.op("dve", lambda e, msk=msk, g=g, m=m: e.tensor_scalar(out=msk[:], in0=g[:], scalar1=m[:, 3:4], scalar2=None, op0=ALU.is_ge), r=[gk, mk8], w=[mskk])
            if RST < 4:
                continue
            s, sk = sc.next()
            p.op("dve", lambda e, s=s, m=m: e.tensor_scalar(out=s[:, 0:1], in0=m[:, 0:1], scalar1=-1.0, scalar2=None, op0=ALU.mult), r=[mk8], w=[sk])
            x_, xk_ = ex.next()
            p.op("act", lambda e, x_=x_, g=g, s=s: e.activation(out=x_[:], in_=g[:], func=AF.Exp, bias=s[:, 0:1]), r=[gk, sk], w=[xk_])
            p.op("dve", lambda e, x_=x_, msk=msk: e.tensor_tensor(out=x_[:], in0=x_[:], in1=msk[:], op=ALU.mult), r=[xk_, mskk], w=[xk_])
            p.op("dve", lambda e, s=s, x_=x_: e.reduce_sum(out=s[:, 1:2], in_=x_[:], axis=AX.X), r=[xk_], w=[sk])
            p.op("dve", lambda e, s=s: e.reciprocal(out=s[:, 1:2], in_=s[:, 1:2]), r=[sk], w=[sk])
            p.op("dve", lambda e, x_=x_, s=s: e.tensor_scalar(out=x_[:], in0=x_[:], scalar1=s[:, 1:2], scalar2=None, op0=ALU.mult), r=[xk_, sk], w=[xk_])
            p.dma("sp", lambda e, x_=x_, sl=sl: e.dma_start(out=GATE[sl, :], in_=x_[:]), r=[xk_], w=["GATE"])


def phase_moe_experts(p, C, io, l, NE, t_lo, t_hi, n_ctx, TB=512):
    xres, HT, GATE, WGU, WDN, modrow = io["xres"], io["HT"], io["GATE"], io["WGU"], io["WDN"], io["modrow"]
    with p.phase():
        bg_rows = p.sb("bg_rows", [NE, 2048], F32)
        p.dma("sp", lambda e: e.dma_start(out=bg_rows[:], in_=io["moe_gu_b"][l]), w=["bg_rows"])
        bgc = p.sb("bgc", [128, 16, NE], F32)
        pst = p.ps("pst", [128, 16, NE], F32)
        for c in range(16):
            p.op("pe", lambda e, c=c: e.transpose(pst[:, c, :], bg_rows[:, c * 128:(c + 1) * 128], C["ident"][0:NE, 0:NE]), r=["bg_rows", "ident"], w=["pst"])
        p.op("dve", lambda e: e.tensor_copy(out=bgc[:], in_=pst[:]), r=["pst"], w=["bgc"])
        ones_b = p.sb("ones_b", [1, 128], BF16)
        p.op("dve", lambda e: e.memset(ones_b[:], 1.0), w=["ones_b"])
        gt = p.sb("gt2row", [128, 2, D], F32)
        for r in range(2):
            p.dma("sp", lambda e, r=r: e.dma_start(out=gt[:, r, :], in_=modrow[l, r, 5120:6144].partition_broadcast(128)), w=["gt2row"])
        wg = Ring(p, "wg", [128, KC, 2048], BF16, n=2)
        wd = Ring(p, "wd", [128, KC, 1024], BF16, n=2)
        bd32 = Ring(p, "bd32", [1, 1024], F32, n=2)
        bd16 = Ring(p, "bd16", [1, 1024], BF16, n=2)
        acc = p.sb("macc", [128, TB // 128, D], F32)
        hT = p.sb("mhT", [128, KC, TB], BF16)
        gate = p.sb("mgate", [128, TB // 128, NE], F32)
        actT = Ring(p, "actT", [128, KC, 512], BF16, n=2)
        t1 = Ring(p, "et1", [128, 512], n=2)
        sgm = Ring(p, "esg", [128, 512], n=2)
        t2 = Ring(p, "et2", [128, 512], n=2)
        xt = Ring(p, "ext", [128, D], n=2)
        psg = Ring(p, "psg", [128, 512], n=4, psum=True)
        psd = Ring(p, "psd", [128, 512], n=3, psum=True)
        t = t_lo
        while t < t_hi:
            nb = min(TB, t_hi - t)
            ntile = nb // 128
            p.dma("sp", lambda e, t=t, nb=nb: e.dma_start(out=hT[:, :, 0:nb], in_=HT[:, t:t + nb].rearrange("(c q) n -> q c n", q=128)), w=["mhT"])
            p.dma("sp", lambda e, t=t, nb=nb, ntile=ntile: e.dma_start(out=gate[:, 0:ntile, :], in_=GATE[t:t + nb, :].rearrange("(j q) n -> q j n", q=128)), w=["mgate"])
            p.op("pool", lambda e: e.memset(acc[:], 0.0), w=["macc"])
            for e_ in range(NE):
                g_, gk = wg.next()
                d_, dk = wd.next()
                b32, b32k = bd32.next()
                b16, b16k = bd16.next()
                p.dma("sp", lambda e, g_=g_, e_=e_: e.dma_start(out=g_[:], in_=WGU[e_].rearrange("(c q) n -> q c n", q=128)), w=[gk])
                p.dma("act", lambda e, d_=d_, e_=e_: e.dma_start(out=d_[:], in_=WDN[e_].rearrange("(c q) n -> q c n", q=128)), w=[dk])
                p.dma("sp", lambda e, b32=b32, e_=e_: e.dma_start(out=b32[:], in_=io["moe_down_b"][l, e_:e_ + 1, :]), w=[b32k])
                p.op("pool", lambda e, b32=b32, b16=b16: e.tensor_copy(out=b16[:], in_=b32[:]), r=[b32k], w=[b16k])
                for s0 in range(0, nb, 512):
                    N = min(512, nb - s0)
                    aT, aTk = actT.next()
                    for c in range(8):
                        pa, pak = psg.next()
                        pb, pbk = psg.next()
                        for (ps_, pk_, col0) in ((pa, pak, c * 128), (pb, pbk, DE + c * 128)):
                            for kc in range(KC):
                                p.op("pe", lambda e, ps_=ps_, g_=g_, kc=kc, col0=col0, s0=s0, N=N: e.matmul(
                                    ps_[:, 0:N], lhsT=g_[:, kc, col0:col0 + 128], rhs=hT[:, kc, s0:s0 + N], start=(kc == 0), stop=(kc == KC - 1)),
                                    r=[gk, "mhT"], w=[pk_])
                        a1, a1k = t1.next()
                        p.op("dve", lambda e, a1=a1, pa=pa, c=c, e_=e_, N=N: e.tensor_scalar(out=a1[:, 0:N], in0=pa[:, 0:N], scalar1=bgc[:, c, e_:e_ + 1], scalar2=SW_LIMIT,
                                                                                          op0=ALU.add, op1=ALU.min), r=[pak, "bgc"], w=[a1k])
                        s_, sk_ = sgm.next()
                        p.op("act", lambda e, s_=s_, a1=a1, N=N: e.activation(out=s_[:, 0:N], in_=a1[:, 0:N], func=AF.Sigmoid, scale=SW_ALPHA), r=[a1k], w=[sk_])
                        a2, a2k = t2.next()
                        p.op("dve", lambda e, a2=a2, pb=pb, c=c, e_=e_, N=N: e.tensor_scalar(out=a2[:, 0:N], in0=pb[:, 0:N], scalar1=bgc[:, 8 + c, e_:e_ + 1], scalar2=SW_LIMIT,
                                                                                          op0=ALU.add, op1=ALU.min), r=[pbk, "bgc"], w=[a2k])
                        p.op("pool", lambda e, a2=a2, N=N: e.tensor_scalar(out=a2[:, 0:N], in0=a2[:, 0:N], scalar1=-SW_LIMIT, scalar2=1.0, op0=ALU.max, op1=ALU.add),
                             r=[a2k], w=[a2k])
                        p.op("pool", lambda e, a1=a1, s_=s_, N=N: e.tensor_tensor(out=a1[:, 0:N], in0=a1[:, 0:N], in1=s_[:, 0:N], op=ALU.mult), r=[a1k, sk_], w=[a1k])
                        p.op("dve", lambda e, aT=aT, a1=a1, a2=a2, c=c, N=N: e.tensor_tensor(out=aT[:, c, 0:N], in0=a1[:, 0:N], in1=a2[:, 0:N], op=ALU.mult),
                             r=[a1k, a2k], w=[aTk])
                    for j in range(N // 128):
                        tile_i = (s0 // 128) + j
                        for n in range(2):
                            pd, pdk = psd.next()
                            for kc in range(KC):
                                p.op("pe", lambda e, pd=pd, aT=aT, d_=d_, kc=kc, j=j, n=n: e.matmul(
                                    pd[:], lhsT=aT[:, kc, j * 128:(j + 1) * 128], rhs=d_[:, kc, n * 512:(n + 1) * 512], start=(kc == 0), stop=False),
                                    r=[aTk, dk], w=[pdk])
                            p.op("pe", lambda e, pd=pd, b16=b16, n=n: e.matmul(pd[:], lhsT=ones_b[:], rhs=b16[:, n * 512:(n + 1) * 512], start=False, stop=True),
                                 r=["ones_b", b16k], w=[pdk])
                            p.op("dve", lambda e, pd=pd, tile_i=tile_i, n=n, e_=e_: e.scalar_tensor_tensor(
                                out=acc[:, tile_i, n * 512:(n + 1) * 512], in0=pd[:], scalar=gate[:, tile_i, e_:e_ + 1],
                                in1=acc[:, tile_i, n * 512:(n + 1) * 512], op0=ALU.mult, op1=ALU.add), r=[pdk, "mgate", "macc"], w=["macc"])
            for j in range(ntile):
                tok = t + j * 128
                r = 1 if tok < n_ctx else 0
                x, xk = xt.next()
                p.dma("sp", lambda e, x=x, tok=tok: e.dma_start(out=x[:], in_=xres[tok:tok + 128, :]), w=[xk])
                p.op("dve", lambda e, j=j, r=r: e.tensor_tensor(out=acc[:, j, :], in0=acc[:, j, :], in1=gt[:, r, :], op=ALU.mult), r=["macc", "gt2row"], w=["macc"])
                p.op("pool", lambda e, x=x, j=j: e.tensor_tensor(out=x[:], in0=x[:], in1=acc[:, j, :], op=ALU.add), r=[xk, "macc"], w=[xk])
                p.dma("sp", lambda e, x=x, tok=tok: e.dma_start(out=xres[tok:tok + 128, :], in_=x[:]), r=[xk], w=["xres"])
            t += nb

import math

MLA_SCALE = 192 ** -0.5


def make_rope_consts(p, C):
    f, q = C["iota_f"], C["iota_p"]
    d = p.sb("rp_d", [64, 64], F32)
    e1 = p.sb("rp_e1", [64, 64], F32)
    e2 = p.sb("rp_e2", [64, 64], F32)
    ge = p.sb("rp_ge", [64, 64], F32)
    PR = p.sb("rp_PR", [64, 64], F32)
    k = ["rope_c"]
    p.op("dve", lambda e: e.tensor_scalar(out=d[:], in0=f[0:64, 0:64], scalar1=q[0:64, 0:1], scalar2=None, op0=ALU.subtract), r=["iota_f", "iota_p"], w=k)
    p.op("dve", lambda e: e.tensor_scalar(out=e1[:], in0=d[:], scalar1=16.0, scalar2=None, op0=ALU.is_equal), r=k, w=k)
    p.op("dve", lambda e: e.tensor_scalar(out=e2[:], in0=d[:], scalar1=-16.0, scalar2=None, op0=ALU.is_equal), r=k, w=k)
    ii = p.sb("rp_ii", [64, 64], I32)
    p.op("pool", lambda e: e.iota(ii[:], pattern=[[1, 64]], base=0, channel_multiplier=0), w=["rp_ii"])
    p.op("dve", lambda e: e.tensor_single_scalar(out=ii[:], in_=ii[:], scalar=16, op=ALU.bitwise_and), r=["rp_ii"], w=["rp_ii"])
    p.op("dve", lambda e: e.tensor_copy(out=ge[:], in_=ii[:]), r=["rp_ii"], w=k)
    p.op("dve", lambda e: e.tensor_scalar(out=ge[:], in0=ge[:], scalar1=1.0 / 16, scalar2=None, op0=ALU.mult), r=k, w=k)
    p.op("dve", lambda e: e.tensor_tensor(out=e1[:], in0=e1[:], in1=ge[:], op=ALU.mult), r=k, w=k)
    p.op("dve", lambda e: e.tensor_scalar(out=ge[:], in0=ge[:], scalar1=-1.0, scalar2=1.0, op0=ALU.mult, op1=ALU.add), r=k, w=k)
    p.op("dve", lambda e: e.tensor_tensor(out=e2[:], in0=e2[:], in1=ge[:], op=ALU.mult), r=k, w=k)
    p.op("dve", lambda e: e.tensor_tensor(out=PR[:], in0=e1[:], in1=e2[:], op=ALU.subtract), r=k, w=k)
    invf = p.sb("rp_invf", [64, 1], F32)
    isrow = p.sb("rp_isrow", [64, 1], F32)
    pi_ = p.sb("rp_pi", [64, 1], I32)
    p.op("pool", lambda e: e.iota(pi_[:], pattern=[[1, 1]], base=0, channel_multiplier=1), w=["rp_pi"])
    p.op("dve", lambda e: e.tensor_single_scalar(out=pi_[:], in_=pi_[:], scalar=15, op=ALU.bitwise_and), r=["rp_pi"], w=["rp_pi"])
    p.op("dve", lambda e: e.tensor_copy(out=invf[:], in_=pi_[:]), r=["rp_pi"], w=k)
    p.op("act", lambda e: e.activation(out=invf[:], in_=invf[:], func=AF.Exp, scale=-math.log(10000.0) / 16.0), r=k, w=k)
    p.op("dve", lambda e: e.tensor_scalar(out=isrow[:], in0=q[0:64, 0:1], scalar1=32.0, scalar2=None, op0=ALU.is_lt), r=["iota_p"], w=k)
    C.update(PR=PR, invf=invf, isrow=isrow)


def phase_mla_proj(p, C, io, G, l, n_ctx, n_tok):
    xres = io["xres"]
    QN, QR, KN, KR, V = io["QN"], io["QR"], io["KN"], io["KRo"], io["Vt"]
    n_lat = n_tok - n_ctx
    with p.phase():
        T = norm_scratch(p)
        win = p.sb("mwin", [128, KC, 448], BF16)
        st = [p.sb(f"mst{i}", [128, 2048], F32) for i in range(2)]
        load_cast_weight(p, win, "mwin", io["od_w_in"], KC, 448, st, "mst")
        wq = p.sb("mwq", [128, 2, 1536], BF16)
        load_cast_weight(p, wq, "mwq", io["mla_wq_up"], 2, 1536, st, "mst")
        wkn = p.sb("mwkn", [128, 8, 128], BF16)
        wv = p.sb("mwv", [128, 8, 128], BF16)
        p.dma("sp", lambda e: e.dma_start(out=st[0][:, 0:2048], in_=io["mla_wkv_up"][:, :]), w=["mst0"])
        p.op("dve", lambda e: e.tensor_copy(out=wkn[:], in_=st[0][:, 0:2048].rearrange("q (h c) -> q h c", c=256)[:, :, 0:128]), r=["mst0"], w=["mwk"])
        p.op("pool", lambda e: e.tensor_copy(out=wv[:], in_=st[0][:, 0:2048].rearrange("q (h c) -> q h c", c=256)[:, :, 128:256]), r=["mst0"], w=["mwk"])
        qg = p.sb("mqg", [128, 2], F32)
        kg = p.sb("mkg", [128, 1], F32)
        p.dma("sp", lambda e: e.dma_start(out=qg[:], in_=io["mla_q_norm"].rearrange("(c q) -> q c", q=128), allow_slow_non_contiguous=True), w=["mg"])
        p.dma("sp", lambda e: e.dma_start(out=kg[:], in_=io["mla_kv_norm"].rearrange("(q o) -> q o", o=1)), w=["mg"])
        onesq = p.sb("monesq", [128, 128], F32)
        p.op("dve", lambda e: e.memset(onesq[:], 1.0 / 256), w=["monesq"])
        xts = Ring(p, "axt", [128, D], n=2)
        hT = p.sb("ahT", [128, KC, 512], BF16)
        cq = p.sb("acq", [128, 2, 512], F32)
        ckv = p.sb("ackv", [128, 512], F32)
        krr = p.sb("akrr", [64, 512], F32)
        sq = p.sb("asq", [128, 2, 512], F32)
        rs = Ring(p, "ars", [128, 512], n=2)
        cqn = p.sb("acqn", [128, 2, 512], BF16)
        ckvn = p.sb("ackvn", [128, 512], BF16)
        ang = p.sb("aang", [64, 512], F32)
        tti = p.sb("atti", [64, 512], I32)
        tt2 = p.sb("att2", [64, 512], I32)
        a2 = p.sb("aa2", [64, 512], F32)
        kf = p.sb("akf", [64, 512], F32)
        cosT = p.sb("acos", [64, 512], F32)
        sinT = p.sb("asin", [64, 512], F32)
        rx = Ring(p, "arx", [64, 512], n=2)
        ru = Ring(p, "aru", [64, 512], n=2)
        ob = Ring(p, "aob", [128, 512], BF16, n=3)
        vb = Ring(p, "avb", [128, 1024], BF16, n=2)
        pp = Ring(p, "app", [128, 512], n=5, psum=True)

        def rope(x, xk, N, scale, dst_ap, dst_key):
            p2, p2k = pp.next()
            p.op("pe", lambda e: e.matmul(p2[0:64, 0:N], lhsT=C["PR"][:], rhs=x, start=True, stop=True), r=["rope_c", xk], w=[p2k])
            u, uk = ru.next()
            p.op("dve", lambda e: e.tensor_tensor(out=u[:, 0:N], in0=p2[0:64, 0:N], in1=sinT[:, 0:N], op=ALU.mult), r=[p2k, "trig"], w=[uk])
            p.op("pool", lambda e: e.tensor_tensor(out=x, in0=x, in1=cosT[:, 0:N], op=ALU.mult), r=[xk, "trig"], w=[xk])
            if scale == 1.0:
                p.op("dve", lambda e: e.tensor_tensor(out=dst_ap, in0=x, in1=u[:, 0:N], op=ALU.add), r=[xk, uk], w=[dst_key])
            else:
                p.op("dve", lambda e: e.tensor_tensor(out=u[:, 0:N], in0=x, in1=u[:, 0:N], op=ALU.add), r=[xk, uk], w=[uk])
                p.op("dve", lambda e: e.tensor_scalar(out=dst_ap, in0=u[:, 0:N], scalar1=scale, scalar2=None, op0=ALU.mult), r=[uk], w=[dst_key])

        blocks = [(0, n_ctx, False)] + [(t, min(512, n_tok - t), True) for t in range(n_ctx, n_tok, 512)]
        def do_block(t0, N, is_lat):
            for j in range(N // 128):
                ti = t0 // 128 + j
                r = 0 if is_lat else 1
                x, xk = xts.next()
                p.dma("sp", lambda e, x=x, ti=ti: e.dma_start(out=x[:], in_=xres[ti * 128:(ti + 1) * 128, :]), w=[xk])
                norm_tile(p, C, T, xk, x, G["A1"][:, l, r, :], G["B1"][:, l, r, :], lambda kc, j=j: hT[:, kc, j * 128:(j + 1) * 128], "ahT")
            for (c0, cw, dst, dk) in ((0, 128, cq[:, 0, :], "acq"), (128, 128, cq[:, 1, :], "acq"), (256, 128, ckv[:, :], "ackv"), (384, 64, krr[:, :], "akrr")):
                ps, pk = pp.next()
                for kc in range(KC):
                    p.op("pe", lambda e, ps=ps, kc=kc, c0=c0, cw=cw, N=N: e.matmul(ps[0:cw, 0:N], lhsT=win[:, kc, c0:c0 + cw], rhs=hT[:, kc, 0:N],
                                                                                 start=(kc == 0), stop=(kc == KC - 1)), r=["mwin", "ahT"], w=[pk])
                p.op("act", lambda e, ps=ps, dst=dst, cw=cw, N=N: e.copy(out=dst[0:cw, 0:N], in_=ps[0:cw, 0:N]), r=[pk], w=[dk])

            def rmsn(src_chunks, src_key, ones_ap, gcols, dst_chunks, dst_key):
                n = len(src_chunks)
                for c in range(n):
                    p.op("act", lambda e, c=c: e.activation(out=sq[:, c, 0:N], in_=src_chunks[c], func=AF.Square), r=[src_key], w=["asq"])
                ps, pk = pp.next()
                for c in range(n):
                    p.op("pe", lambda e, ps=ps, c=c: e.matmul(ps[:, 0:N], lhsT=ones_ap, rhs=sq[:, c, 0:N], start=(c == 0), stop=(c == n - 1)),
                         r=["asq", "monesq", "masks"], w=[pk])
                r_, rk = rs.next()
                p.op("dve", lambda e: e.tensor_scalar(out=r_[:, 0:N], in0=ps[:, 0:N], scalar1=EPS, scalar2=None, op0=ALU.add), r=[pk], w=[rk])
                p.op("act", lambda e: e.activation(out=r_[:, 0:N], in_=r_[:, 0:N], func=AF.Sqrt), r=[rk], w=[rk])
                p.op("dve", lambda e: e.reciprocal(out=r_[:, 0:N], in_=r_[:, 0:N]), r=[rk], w=[rk])
                for c in range(n):
                    p.op("dve", lambda e, c=c: e.scalar_tensor_tensor(out=dst_chunks[c], in0=src_chunks[c], scalar=gcols[c], in1=r_[:, 0:N],
                                                                      op0=ALU.mult, op1=ALU.mult), r=[src_key, rk, "mg"], w=[dst_key])

            rmsn([ckv[:, 0:N]], "ackv", C["ones128"][:], [kg[:, 0:1]], [ckvn[:, 0:N]], "ackvn")
            if is_lat:
                rmsn([cq[:, 0, 0:N], cq[:, 1, 0:N]], "acq", onesq[:], [qg[:, 0:1], qg[:, 1:2]], [cqn[:, 0, 0:N], cqn[:, 1, 0:N]], "acqn")
                lat0 = t0 - n_ctx
                p.op("pool", lambda e, lat0=lat0: e.iota(tti[:, 0:N], pattern=[[1, N]], base=lat0, channel_multiplier=0), w=["atti"])
                p.op("dve", lambda e: e.tensor_single_scalar(out=tt2[:, 0:N], in_=tti[:, 0:N], scalar=63, op=ALU.bitwise_and), r=["atti"], w=["att2"])
                p.op("dve", lambda e: e.tensor_copy(out=cosT[:, 0:N], in_=tt2[:, 0:N]), r=["att2"], w=["trig"])
                p.op("dve", lambda e: e.tensor_single_scalar(out=tt2[:, 0:N], in_=tti[:, 0:N], scalar=6, op=ALU.arith_shift_right), r=["atti", "trig"], w=["att2"])
                p.op("dve", lambda e: e.tensor_copy(out=sinT[:, 0:N], in_=tt2[:, 0:N]), r=["att2"], w=["trig"])
                p.op("dve", lambda e: e.tensor_tensor(out=sinT[:, 0:N], in0=sinT[:, 0:N], in1=cosT[:, 0:N], op=ALU.subtract), r=["trig"], w=["trig"])
                p.op("dve", lambda e: e.scalar_tensor_tensor(out=ang[:, 0:N], in0=sinT[:, 0:N], scalar=C["isrow"][:, 0:1], in1=cosT[:, 0:N], op0=ALU.mult, op1=ALU.add),
                     r=["trig", "rope_c"], w=["aang"])
                p.op("dve", lambda e: e.tensor_scalar(out=ang[:, 0:N], in0=ang[:, 0:N], scalar1=C["invf"][:, 0:1], scalar2=None, op0=ALU.mult), r=["aang", "rope_c"], w=["aang"])
                for (dst, off) in ((sinT, 0.0), (cosT, 0.5 * math.pi)):
                    p.op("dve", lambda e, off=off: e.tensor_scalar(out=a2[:, 0:N], in0=ang[:, 0:N], scalar1=off, scalar2=None, op0=ALU.add), r=["aang"], w=["aa2"])
                    p.op("dve", lambda e: e.tensor_scalar(out=kf[:, 0:N], in0=a2[:, 0:N], scalar1=1.0 / (2 * math.pi), scalar2=None, op0=ALU.mult), r=["aa2"], w=["akf"])
                    p.op("dve", lambda e: e.tensor_copy(out=tt2[:, 0:N], in_=kf[:, 0:N]), r=["akf"], w=["att2"])
                    p.op("dve", lambda e: e.tensor_copy(out=kf[:, 0:N], in_=tt2[:, 0:N]), r=["att2"], w=["akf"])
                    p.op("dve", lambda e, dst=dst: e.scalar_tensor_tensor(out=dst[:, 0:N], in0=kf[:, 0:N], scalar=-2 * math.pi, in1=a2[:, 0:N], op0=ALU.mult, op1=ALU.add),
                         r=["akf", "aa2"], w=["trig"])
                    p.op("dve", lambda e, dst=dst: e.tensor_scalar(out=dst[:, 0:N], in0=dst[:, 0:N], scalar1=-math.pi, scalar2=math.pi, op0=ALU.max, op1=ALU.min), r=["trig"], w=["trig"])
                    p.op("act", lambda e, dst=dst: e.activation(out=dst[:, 0:N], in_=dst[:, 0:N], func=AF.Sin), r=["trig"], w=["trig"])
                lsl = slice(lat0, lat0 + N)
                for h in range(8):
                    ps, pk = pp.next()
                    for kc in range(2):
                        p.op("pe", lambda e, ps=ps, kc=kc, h=h: e.matmul(ps[:, 0:N], lhsT=wq[:, kc, h * 192:h * 192 + 128], rhs=cqn[:, kc, 0:N], start=(kc == 0), stop=(kc == 1)),
                             r=["mwq", "acqn"], w=[pk])
                    o, ok = ob.next()
                    p.op("act", lambda e, o=o, ps=ps: e.activation(out=o[:, 0:N], in_=ps[:, 0:N], func=AF.Copy, scale=MLA_SCALE), r=[pk], w=[ok])
                    p.dma("sp", lambda e, o=o, h=h, lsl=lsl: e.dma_start(out=QN[h, :, lsl], in_=o[:, 0:N]), r=[ok], w=["QN"])
                    ps, pk = pp.next()
                    for kc in range(2):
                        p.op("pe", lambda e, ps=ps, kc=kc, h=h: e.matmul(ps[0:64, 0:N], lhsT=wq[:, kc, h * 192 + 128:h * 192 + 192], rhs=cqn[:, kc, 0:N], start=(kc == 0), stop=(kc == 1)),
                             r=["mwq", "acqn"], w=[pk])
                    o, ok = ob.next()
                    x_, xk_ = rx.next()
                    p.op("act", lambda e, x_=x_, ps=ps: e.copy(out=x_[:, 0:N], in_=ps[0:64, 0:N]), r=[pk], w=[xk_])
                    rope(x_[:, 0:N], xk_, N, MLA_SCALE, o[0:64, 0:N], ok)
                    p.dma("sp", lambda e, o=o, h=h, lsl=lsl: e.dma_start(out=QR[h, :, lsl], in_=o[0:64, 0:N]), r=[ok], w=["QR"])
            for h in range(8):
                ps, pk = pp.next()
                p.op("pe", lambda e, ps=ps, h=h: e.matmul(ps[:, 0:N], lhsT=wkn[:, h, :], rhs=ckvn[:, 0:N], start=True, stop=True), r=["mwk", "ackvn"], w=[pk])
                o, ok = ob.next()
                p.op("act", lambda e, o=o, ps=ps: e.copy(out=o[:, 0:N], in_=ps[:, 0:N]), r=[pk], w=[ok])
                p.dma("sp", lambda e, o=o, h=h, t0=t0, N=N: e.dma_start(out=KN[h, :, t0:t0 + N], in_=o[:, 0:N]), r=[ok], w=["KN"])
            for j in range(N // 128):
                vt, vk = vb.next()
                for g in range(2):
                    ps, pk = pp.next()
                    p.op("pe", lambda e, ps=ps, j=j, g=g: e.matmul(ps[:, :], lhsT=ckvn[:, j * 128:(j + 1) * 128], rhs=wv[:, 4 * g:4 * g + 4, :], start=True, stop=True),
                         r=["mwk", "ackvn"], w=[pk])
                    p.op("dve", lambda e, vt=vt, ps=ps, g=g: e.tensor_copy(out=vt[:, g * 512:(g + 1) * 512], in_=ps[:, :]), r=[pk], w=[vk])
                tok = t0 + j * 128
                p.dma("sp", lambda e, vt=vt, tok=tok: e.dma_start(out=V[tok:tok + 128, :], in_=vt[:]), r=[vk], w=["Vt"])
            o, ok = ob.next()
            if is_lat:
                rope(krr[:, 0:N], "akrr", N, 1.0, o[0:64, 0:N], ok)
            else:
                p.op("dve", lambda e, o=o: e.tensor_copy(out=o[0:64, 0:N], in_=krr[:, 0:N]), r=["akrr"], w=[ok])
            p.dma("sp", lambda e, o=o, t0=t0, N=N: e.dma_start(out=KR[:, t0:t0 + N], in_=o[0:64, 0:N]), r=[ok], w=["KRo"])


        for (t0_, N_, lat_) in blocks:
            do_block(t0_, N_, lat_)

def phase_mla_attn(p, C, io, n_ctx, n_tok):
    QN, QR, KN, KR, V, MIXT = io["QN"], io["QR"], io["KN"], io["KRo"], io["Vt"], io["MIXT"]
    n_lat = n_tok - n_ctx
    nkt = n_tok // 128
    with p.phase():
        kr = p.sb("bkr", [64, n_tok], BF16)
        p.dma("sp", lambda e: e.dma_start(out=kr[:], in_=KR[:, :]), w=["bkr"])
        ones_bf = p.sb("bones", [128, 128], BF16)
        p.op("dve", lambda e: e.memset(ones_bf[:], 1.0), w=["bones"])
        kn = Ring(p, "bkn", [128, n_tok], BF16, n=2)
        vv = Ring(p, "bvv", [128, nkt, 128], BF16, n=2)
        qn = Ring(p, "bqn", [128, 512], BF16, n=2)
        qr = Ring(p, "bqr", [64, 512], BF16, n=2)
        pt = Ring(p, "bpt", [128, 512], BF16, n=4)
        rd = Ring(p, "brd", [128, 512], n=2)
        ob = Ring(p, "bob", [128, 512], BF16, n=2)
        psS = Ring(p, "bpsS", [128, 512], n=4, psum=True)
        psO = Ring(p, "bpsO", [128, 512], n=2, psum=True)
        psD = Ring(p, "bpsD", [128, 512], n=2, psum=True)
        for h in range(8):
            k_, kk = kn.next()
            v_, vk = vv.next()
            p.dma("sp", lambda e, k_=k_, h=h: e.dma_start(out=k_[:], in_=KN[h, :, :]), w=[kk])
            for j0 in range(0, nkt, 12):
                j1 = min(nkt, j0 + 12)
                p.dma("act", lambda e, v_=v_, h=h, j0=j0, j1=j1: e.dma_start(
                    out=v_[:, j0:j1, :], in_=V[j0 * 128:j1 * 128, h * 128:(h + 1) * 128].rearrange("(j q) c -> q j c", q=128)), w=[vk])
            for qb in range(n_lat // 512):
                qsl = slice(qb * 512, (qb + 1) * 512)
                a, ak = qn.next()
                b, bk = qr.next()
                p.dma("sp", lambda e, a=a, h=h, qsl=qsl: e.dma_start(out=a[:], in_=QN[h, :, qsl]), w=[ak])
                p.dma("sp", lambda e, b=b, h=h, qsl=qsl: e.dma_start(out=b[:], in_=QR[h, :, qsl]), w=[bk])
                po, pok = psO.next()
                pd, pdk = psD.next()
                def S_(kt):
                    ks = slice(kt * 128, (kt + 1) * 128)
                    ps, psk = psS.next()
                    p.op("pe", lambda e, ps=ps, k_=k_, ks=ks, a=a: e.matmul(ps[:], lhsT=k_[:, ks], rhs=a[:], start=True, stop=False), r=[kk, ak], w=[psk])
                    p.op("pe", lambda e, ps=ps, ks=ks, b=b: e.matmul(ps[:], lhsT=kr[:, ks], rhs=b[:], start=False, stop=True), r=["bkr", bk], w=[psk])
                    return ps, psk
                LA = 2
                pend = [S_(kt) for kt in range(min(LA, nkt))]
                for kt in range(nkt):
                    if kt + LA < nkt:
                        pend.append(S_(kt + LA))
                    ps, psk = pend.pop(0)
                    t, tk = pt.next()
                    p.op("act", lambda e, t=t, ps=ps: e.activation(out=t[:], in_=ps[:], func=AF.Exp), r=[psk], w=[tk])
                    p.op("pe", lambda e, po=po, v_=v_, kt=kt, t=t: e.matmul(po[:], lhsT=v_[:, kt, :], rhs=t[:], start=(kt == 0), stop=(kt == nkt - 1)), r=[vk, tk], w=[pok])
                    p.op("pe", lambda e, pd=pd, t=t, kt=kt: e.matmul(pd[:], lhsT=ones_bf[:], rhs=t[:], start=(kt == 0), stop=(kt == nkt - 1)), r=["bones", tk], w=[pdk])
                r_, rk = rd.next()
                p.op("dve", lambda e, r_=r_, pd=pd: e.reciprocal(out=r_[:], in_=pd[:]), r=[pdk], w=[rk])
                o, ok = ob.next()
                p.op("dve", lambda e, o=o, po=po, r_=r_: e.tensor_tensor(out=o[:], in0=po[:], in1=r_[:], op=ALU.mult), r=[pok, rk], w=[ok])
                p.dma("sp", lambda e, o=o, h=h, qb=qb: e.dma_start(out=MIXT[h * 128:(h + 1) * 128, n_ctx + qb * 512:n_ctx + (qb + 1) * 512], in_=o[:]), r=[ok], w=["MIXT"])

CAST_IN_GATHER = 1


def moe_tables(p, NE, max_tiles, max_nb):
    RT = {}
    for nm in ("e4", "rank4", "gate4", "destf"):
        RT[nm] = p.sb("rt_" + nm, [128, max_tiles, 4], F32)
    RT["desti"] = p.sb("rt_desti", [128, max_tiles, 4], I32)
    RT["pstart"] = p.sb("rt_pstart", [128, NE], F32)
    RT["blke"] = p.sb("rt_blke", [128, max_nb], F32)
    RT["widx"] = p.sb("rt_widx", [128, max_nb, 8], I32)
    return RT


def phase_moe_sparse_route(p, C, io, G, RT, l, NE, t_lo, t_hi, n_ctx, BLK=512):
    xres, HTOK, modrow = io["xres"], io["HTOK"], io["modrow"]
    T_ = t_hi - t_lo
    ntile = T_ // 128
    NB = (4 * T_ + BLK - 1) // BLK + NE
    LOG = BLK.bit_length() - 1
    e4, rank4, gate4 = RT["e4"], RT["rank4"], RT["gate4"]
    with p.phase():
        T = norm_scratch(p)
        wr = p.sb("wr", [128, KC, NE], F32)
        p.dma("sp", lambda e: e.dma_start(out=wr[:], in_=io["router_w"][l].rearrange("(c q) n -> q c n", q=128)), w=["wr"])
        br = p.sb("br", [128, NE], F32)
        p.dma("sp", lambda e: e.dma_start(out=br[:], in_=io["router_b"][l].partition_broadcast(128)), w=["wr"])
        Arow = p.sb("sArow", [128, 2, D], F32)
        Brow = p.sb("sBrow", [128, 2, D], F32)
        grow = p.sb("sgrow", [128, D], F32)
        p.dma("sp", lambda e: e.dma_start(out=grow[:], in_=io["norm2_g"][l].partition_broadcast(128)), w=["sgrow"])
        for r in range(2):
            p.dma("sp", lambda e, r=r: e.dma_start(out=Arow[:, r, :], in_=modrow[l, r, 4096:5120].partition_broadcast(128)), w=["sArow"])
            p.dma("sp", lambda e, r=r: e.dma_start(out=Brow[:, r, :], in_=modrow[l, r, 3072:4096].partition_broadcast(128)), w=["sBrow"])
            p.op("dve", lambda e, r=r: e.scalar_tensor_tensor(out=Arow[:, r, :], in0=Arow[:, r, :], scalar=1.0, in1=grow[:], op0=ALU.add, op1=ALU.mult),
                 r=["sArow", "sgrow"], w=["sArow"])
        cnt = p.sb("scnt", [128, NE], F32)
        p.op("dve", lambda e: e.memset(cnt[:], 0.0), w=["scnt"])
        xt = Ring(p, "sxt", [128, D], n=2)
        h32 = Ring(p, "sh32", [128, KC, 128], n=2)
        ht = Ring(p, "sht", [128, D], n=2)
        hb = Ring(p, "shb", [128, D], BF16, n=2)
        sm = {nm: Ring(p, "s_" + nm, [128, NE], n=2) for nm in ("lg", "ex", "mk", "rk", "oh", "tmp")}
        m8 = Ring(p, "sm8", [128, 8], n=2)
        sc = Ring(p, "ssc", [128, 2], n=2)
        psr = p.ps("spsr", [128, NE], F32)
        ps2 = p.ps("sps2", [128, NE], F32)
        ps3 = p.ps("sps3", [128, NE], F32)
        for ti in range(ntile):
            tok = t_lo + ti * 128
            r = 1 if tok < n_ctx else 0
            x, xk = xt.next()
            p.dma("sp", lambda e, x=x, tok=tok: e.dma_start(out=x[:], in_=xres[tok:tok + 128, :]), w=[xk])
            f, fk = h32.next()
            norm_tile(p, C, T, xk, x, G["A2"][:, l, r, :], G["B2"][:, l, r, :], None, fk, hT32_dst=lambda kc, f=f: f[:, kc, :], want_bf=False)
            fk32 = fk + "32"
            a, ak = ht.next()
            b, bk = hb.next()
            p.op("dve", lambda e, a=a, r=r: e.tensor_tensor(out=a[:], in0=T["xn"][:], in1=Arow[:, r, :], op=ALU.mult), r=["xn", "sArow"], w=[ak])
            p.op("pool", lambda e, a=a, b=b, r=r: e.tensor_tensor(out=b[:], in0=a[:], in1=Brow[:, r, :], op=ALU.add), r=[ak, "sBrow"], w=[bk])
            p.dma("sp", lambda e, b=b, tok=tok: e.dma_start(out=HTOK[tok:tok + 128, :], in_=b[:]), r=[bk], w=["HTOK"])
            for kc in range(KC):
                p.op("pe", lambda e, f=f, kc=kc: e.matmul(psr[:], lhsT=f[:, kc, :], rhs=wr[:, kc, :], start=(kc == 0), stop=(kc == KC - 1)),
                     r=[fk32, "wr"], w=["spsr"])
            g, gk = sm["lg"].next()
            p.op("dve", lambda e, g=g: e.tensor_tensor(out=g[:], in0=psr[:], in1=br[:], op=ALU.add), r=["spsr", "wr"], w=[gk])
            m, mk8 = m8.next()
            p.op("dve", lambda e, m=m, g=g: e.max(out=m[:], in_=g[:]), r=[gk], w=[mk8])
            msk, mskk = sm["mk"].next()
            p.op("dve", lambda e, msk=msk, g=g, m=m: e.tensor_scalar(out=msk[:], in0=g[:], scalar1=m[:, 3:4], scalar2=None, op0=ALU.is_ge), r=[gk, mk8], w=[mskk])
            s, sk = sc.next()
            p.op("dve", lambda e, s=s, m=m: e.tensor_scalar(out=s[:, 0:1], in0=m[:, 0:1], scalar1=-1.0, scalar2=None, op0=ALU.mult), r=[mk8], w=[sk])
            x_, xk_ = sm["ex"].next()
            p.op("act", lambda e, x_=x_, g=g, s=s: e.activation(out=x_[:], in_=g[:], func=AF.Exp, bias=s[:, 0:1]), r=[gk, sk], w=[xk_])
            p.op("dve", lambda e, x_=x_, msk=msk: e.tensor_tensor(out=x_[:], in0=x_[:], in1=msk[:], op=ALU.mult), r=[xk_, mskk], w=[xk_])
            p.op("dve", lambda e, s=s, x_=x_: e.reduce_sum(out=s[:, 1:2], in_=x_[:], axis=AX.X), r=[xk_], w=[sk])
            p.op("dve", lambda e, s=s: e.reciprocal(out=s[:, 1:2], in_=s[:, 1:2]), r=[sk], w=[sk])
            p.op("dve", lambda e, x_=x_, s=s: e.tensor_scalar(out=x_[:], in0=x_[:], scalar1=s[:, 1:2], scalar2=None, op0=ALU.mult), r=[xk_, sk], w=[xk_])
            p.op("pe", lambda e, msk=msk: e.matmul(ps2[:], lhsT=C["strict0"][:], rhs=msk[:], start=True, stop=True), r=["masks", mskk], w=["sps2"])
            p.op("pe", lambda e, msk=msk: e.matmul(ps3[:], lhsT=C["ones64"][:], rhs=msk[:], start=True, stop=True), r=["masks", mskk], w=["sps3"])
            rk, rkk = sm["rk"].next()
            p.op("dve", lambda e, rk=rk: e.tensor_tensor(out=rk[:], in0=ps2[:], in1=cnt[:], op=ALU.add), r=["sps2", "scnt"], w=[rkk])
            p.op("dve", lambda e: e.tensor_tensor(out=cnt[:], in0=ps3[:], in1=cnt[:], op=ALU.add), r=["sps3", "scnt", rkk], w=["scnt"])
            for k in range(4):
                oh, ohk = sm["oh"].next()
                p.op("dve", lambda e, oh=oh, g=g, m=m, k=k: e.tensor_scalar(out=oh[:], in0=g[:], scalar1=m[:, k:k + 1], scalar2=None, op0=ALU.is_equal), r=[gk, mk8], w=[ohk])
                for (src, srck, dst) in ((C["iota_f"][:, 0:NE], "iota_f", e4), (rk[:], rkk, rank4), (x_[:], xk_, gate4)):
                    tmp, tmpk = sm["tmp"].next()
                    p.op("dve", lambda e, tmp=tmp, oh=oh, src=src: e.tensor_tensor(out=tmp[:], in0=oh[:], in1=src, op=ALU.mult), r=[ohk, srck], w=[tmpk])
                    p.op("dve", lambda e, tmp=tmp, dst=dst, ti=ti, k=k: e.reduce_sum(out=dst[:, ti, k:k + 1], in_=tmp[:], axis=AX.X), r=[tmpk], w=["rt"])
        ci = p.sb("sci", [128, NE], I32)
        padf = p.sb("spadf", [128, NE], F32)
        ca = p.sb("sca", [128, NE], F32)
        cb = p.sb("scb", [128, NE], F32)
        p.op("dve", lambda e: e.tensor_copy(out=ci[:], in_=cnt[:]), r=["scnt"], w=["sci"])
        p.op("dve", lambda e: e.tensor_single_scalar(out=ci[:], in_=ci[:], scalar=BLK - 1, op=ALU.add), r=["sci"], w=["sci"])
        p.op("dve", lambda e: e.tensor_single_scalar(out=ci[:], in_=ci[:], scalar=LOG, op=ALU.arith_shift_right), r=["sci"], w=["sci"])
        p.op("dve", lambda e: e.tensor_single_scalar(out=ci[:], in_=ci[:], scalar=LOG, op=ALU.logical_shift_left), r=["sci"], w=["sci"])
        p.op("dve", lambda e: e.tensor_copy(out=padf[:], in_=ci[:]), r=["sci"], w=["spadf"])
        p.op("dve", lambda e: e.tensor_copy(out=ca[:], in_=padf[:]), r=["spadf"], w=["sca"])
        cur, curk, oth, othk = ca, "sca", cb, "scb"
        sft = 1
        while sft < NE:
            p.op("dve", lambda e, cur=cur, oth=oth: e.tensor_copy(out=oth[:], in_=cur[:]), r=[curk], w=[othk])
            p.op("dve", lambda e, cur=cur, oth=oth, sft=sft: e.tensor_tensor(out=oth[:, sft:NE], in0=cur[:, sft:NE], in1=cur[:, 0:NE - sft], op=ALU.add), r=[curk, othk], w=[othk])
            cur, curk, oth, othk = oth, othk, cur, curk
            sft *= 2
        pend, pendk = cur, curk
        p.op("dve", lambda e: e.tensor_tensor(out=RT["pstart"][:], in0=pend[:], in1=padf[:], op=ALU.subtract), r=[pendk, "spadf"], w=["rt"])
        bst = p.sb("sbst", [128, NB], F32)
        p.op("dve", lambda e: e.tensor_scalar(out=bst[:], in0=C["iota_f"][:, 0:NB], scalar1=float(BLK), scalar2=None, op0=ALU.mult), r=["iota_f"], w=["sbst"])
        blke = RT["blke"]
        p.op("dve", lambda e: e.memset(blke[:, 0:NB], 0.0), w=["rt"])
        for e_ in range(NE):
            p.op("dve", lambda e, e_=e_: e.scalar_tensor_tensor(out=blke[:, 0:NB], in0=bst[:], scalar=pend[:, e_:e_ + 1], in1=blke[:, 0:NB], op0=ALU.is_ge, op1=ALU.add),
                 r=["sbst", pendk, "rt"], w=["rt"])
        p.op("dve", lambda e: e.tensor_scalar(out=blke[:, 0:NB], in0=blke[:, 0:NB], scalar1=float(NE - 1), scalar2=None, op0=ALU.min), r=["rt"], w=["rt"])
        base8 = p.sb("sbase8", [128, 8], F32)
        p.op("dve", lambda e: e.tensor_scalar(out=base8[:], in0=C["iota_f"][:, 0:8], scalar1=128.0, scalar2=C["iota_p"][:, 0:1], op0=ALU.mult, op1=ALU.add),
             r=["iota_f", "iota_p"], w=["sbase8"])
        b1k = p.sb("sb1k", [128, NB], F32)
        lofs = float(l * NE * 1024) if CAST_IN_GATHER else 0.0
        p.op("dve", lambda e: e.tensor_scalar(out=b1k[:], in0=blke[:, 0:NB], scalar1=1024.0, scalar2=lofs, op0=ALU.mult, op1=ALU.add), r=["rt"], w=["sb1k"])
        wf = p.sb("swf", [128, NB, 8], F32)
        for b_ in range(NB):
            p.op("dve" if b_ % 2 == 0 else "pool", lambda e, b_=b_: e.tensor_scalar(out=wf[:, b_, :], in0=base8[:], scalar1=b1k[:, b_:b_ + 1], scalar2=None, op0=ALU.add),
                 r=["sbase8", "sb1k"], w=["swf"])
        p.op("dve", lambda e: e.tensor_copy(out=RT["widx"][:, 0:NB, :], in_=wf[:]), r=["swf"], w=["rt"])
        destf, desti = RT["destf"], RT["desti"]
        for ti in range(ntile):
            for k in range(4):
                oh, ohk = sm["oh"].next()
                p.op("dve", lambda e, oh=oh, ti=ti, k=k: e.tensor_scalar(out=oh[:], in0=C["iota_f"][:, 0:NE], scalar1=e4[:, ti, k:k + 1], scalar2=None, op0=ALU.is_equal),
                     r=["iota_f", "rt"], w=[ohk])
                tmp, tmpk = sm["tmp"].next()
                p.op("dve", lambda e, tmp=tmp, oh=oh: e.tensor_tensor(out=tmp[:], in0=oh[:], in1=RT["pstart"][:], op=ALU.mult), r=[ohk, "rt"], w=[tmpk])
                p.op("dve", lambda e, tmp=tmp, ti=ti, k=k: e.reduce_sum(out=destf[:, ti, k:k + 1], in_=tmp[:], axis=AX.X), r=[tmpk], w=["rt"])
        p.op("dve", lambda e: e.tensor_tensor(out=destf[:, 0:ntile, :], in0=destf[:, 0:ntile, :], in1=rank4[:, 0:ntile, :], op=ALU.add), r=["rt"], w=["rt"])
        p.op("dve", lambda e: e.tensor_scalar(out=destf[:, 0:ntile, :], in0=destf[:, 0:ntile, :], scalar1=float(NB * BLK - 1), scalar2=0.0, op0=ALU.min, op1=ALU.max), r=["rt"], w=["rt"])
        p.op("dve", lambda e: e.tensor_copy(out=desti[:, 0:ntile, :], in_=destf[:, 0:ntile, :]), r=["rt"], w=["rt"])
    return NB


def phase_moe_sparse_dispatch(p, C, io, RT, t_lo, t_hi, NB, BLK=512):
    HTOK, HS = io["HTOK"], io["HS"]
    ntile = (t_hi - t_lo) // 128
    with p.phase():
        z = p.sb("dz", [128, 4, D], BF16)
        p.op("pool", lambda e: e.memset(z[:], 0.0), w=["dz"])
        for b_ in range(NB * BLK // 512):
            p.dma("sp" if b_ % 2 == 0 else "act", lambda e, b_=b_: e.dma_start(out=HS[b_ * 512:(b_ + 1) * 512, :].rearrange("(j q) d -> q j d", q=128), in_=z[:]),
                  r=["dz"], w=[f"HSz{b_ % 8}"])
        hb = Ring(p, "dhb", [128, D], BF16, n=3)
        zk = [f"HSz{i}" for i in range(8)]
        for ti in range(ntile):
            tok = t_lo + ti * 128
            h, hk = hb.next()
            p.dma("sp", lambda e, h=h, tok=tok: e.dma_start(out=h[:], in_=HTOK[tok:tok + 128, :]), w=[hk])
            for k in range(4):
                p.dma("pool", lambda e, h=h, ti=ti, k=k: e.indirect_dma_start(
                    out=HS[:, :], out_offset=bass.IndirectOffsetOnAxis(ap=RT["desti"][:, ti, k:k + 1], axis=0), in_=h[:, :], in_offset=None),
                    r=[hk, "rt"] + zk, w=[f"HSs{(ti * 4 + k) % 8}"])


def phase_moe_sparse_experts(p, C, io, RT, l, NE, NB, BLK=512):
    HS, Y = io["HS"], io["Y"]
    if CAST_IN_GATHER:
        WGf = io["moe_gu_w"].rearrange("l e k n -> (l e k) n")
        WDf = io["moe_down_w"].rearrange("l e k n -> (l e k) n")
    else:
        WGf = io["WGU"].rearrange("e k n -> (e k) n")
        WDf = io["WDN"].rearrange("e k n -> (e k) n")
    blke = RT["blke"]
    with p.phase():
        b32 = p.sb("xb32", [NE, 2048], F32)
        bgu = p.sb("xbgu", [NE, 2048], BF16)
        bdn = p.sb("xbdn", [NE, 1024], BF16)
        p.dma("sp", lambda e: e.dma_start(out=b32[:], in_=io["moe_gu_b"][l]), w=["xb32"])
        p.op("dve", lambda e: e.tensor_copy(out=bgu[:], in_=b32[:]), r=["xb32"], w=["xbias"])
        p.dma("sp", lambda e: e.dma_start(out=b32[:, 0:1024], in_=io["moe_down_b"][l]), r=["xbias"], w=["xb32"])
        p.op("dve", lambda e: e.tensor_copy(out=bdn[:], in_=b32[:, 0:1024]), r=["xb32"], w=["xbias"])
        ones5 = p.sb("xones", [NE, BLK], F32)
        p.op("dve", lambda e: e.memset(ones5[:], 1.0), w=["xones"])
        wg = Ring(p, "xwg", [128, KC, 2048], BF16, n=2)
        wd = Ring(p, "xwd", [128, KC, 1024], BF16, n=2)
        hs = Ring(p, "xhs", [128, BLK // 128, D], BF16, n=2)
        hsT = Ring(p, "xhsT", [128, KC, BLK], BF16, n=2)
        ohT = Ring(p, "xohT", [NE, BLK], BF16, n=2)
        actT = Ring(p, "xactT", [128, KC, BLK], BF16, n=1)
        t1 = Ring(p, "xt1", [128, BLK], n=2)
        sgm = Ring(p, "xsg", [128, BLK], n=2)
        t2 = Ring(p, "xt2", [128, BLK], n=2)
        yo = Ring(p, "xyo", [128, D], n=2)
        pst = Ring(p, "xpst", [128, KC, 128], BF16, n=2, psum=True)
        psg = Ring(p, "xpsg", [128, 512], n=4, psum=True)
        psd = Ring(p, "xpsd", [128, 512], n=2, psum=True)
        ev = 0
        for b_ in range(NB):
            g_, gk = wg.next()
            d_, dk = wd.next()
            for kc in range(KC):
                p.dma("pool", lambda e, g_=g_, kc=kc, b_=b_: e.indirect_dma_start(
                    out=g_[:, kc, :], out_offset=None, in_=WGf[:, :], in_offset=bass.IndirectOffsetOnAxis(ap=RT["widx"][:, b_, kc:kc + 1], axis=0)), r=["rt"], w=[gk])
                p.dma("pool", lambda e, d_=d_, kc=kc, b_=b_: e.indirect_dma_start(
                    out=d_[:, kc, :], out_offset=None, in_=WDf[:, :], in_offset=bass.IndirectOffsetOnAxis(ap=RT["widx"][:, b_, kc:kc + 1], axis=0)), r=["rt"], w=[dk])
            h_, hk = hs.next()
            p.dma("sp", lambda e, h_=h_, b_=b_: e.dma_start(out=h_[:], in_=HS[b_ * BLK:(b_ + 1) * BLK, :].rearrange("(j q) d -> q j d", q=128)), w=[hk])
            o_, ok_ = ohT.next()
            p.op("dve", lambda e, o_=o_, b_=b_: e.tensor_scalar(out=o_[:], in0=ones5[:], scalar1=blke[0:NE, b_:b_ + 1], scalar2=C["iota_p"][0:NE, 0:1],
                                                                 op0=ALU.mult, op1=ALU.is_equal), r=["xones", "rt", "iota_p"], w=[ok_])
            hT, hTk = hsT.next()
            for j in range(BLK // 128):
                pt_, ptk = pst.next()
                for kc in range(KC):
                    p.op("pe", lambda e, pt_=pt_, h_=h_, j=j, kc=kc: e.transpose(pt_[:, kc, :], h_[:, j, kc * 128:(kc + 1) * 128], C["identb"][:]), r=[hk, "identb"], w=[ptk])
                if j % 2 == 0:
                    p.op("act", lambda e, hT=hT, pt_=pt_, j=j: e.copy(out=hT[:, :, j * 128:(j + 1) * 128], in_=pt_[:]), r=[ptk], w=[hTk])
                else:
                    p.op("dve", lambda e, hT=hT, pt_=pt_, j=j: e.tensor_copy(out=hT[:, :, j * 128:(j + 1) * 128], in_=pt_[:]), r=[ptk], w=[hTk])
            aT, aTk = actT.next()
            N = BLK
            for c in range(8):
                pa, pak = psg.next()
                pb, pbk = psg.next()
                for (ps_, pk_, col0) in ((pa, pak, c * 128), (pb, pbk, DE + c * 128)):
                    for kc in range(KC):
                        p.op("pe", lambda e, ps_=ps_, g_=g_, kc=kc, col0=col0, hT=hT: e.matmul(
                            ps_[:, 0:N], lhsT=g_[:, kc, col0:col0 + 128], rhs=hT[:, kc, :], start=(kc == 0), stop=False), r=[gk, hTk], w=[pk_])
                    p.op("pe", lambda e, ps_=ps_, col0=col0, o_=o_: e.matmul(ps_[:, 0:N], lhsT=bgu[:, col0:col0 + 128], rhs=o_[:], start=False, stop=True),
                         r=["xbias", ok_], w=[pk_])
                a1, a1k = t1.next()
                p.op("dve", lambda e, a1=a1, pa=pa: e.tensor_scalar(out=a1[:], in0=pa[:], scalar1=SW_LIMIT, scalar2=None, op0=ALU.min), r=[pak], w=[a1k])
                s_, sk_ = sgm.next()
                p.op("act", lambda e, s_=s_, a1=a1: e.activation(out=s_[:], in_=a1[:], func=AF.Sigmoid, scale=SW_ALPHA), r=[a1k], w=[sk_])
                a2, a2k = t2.next()
                p.op("dve", lambda e, a2=a2, pb=pb: e.tensor_scalar(out=a2[:], in0=pb[:], scalar1=SW_LIMIT, scalar2=-SW_LIMIT, op0=ALU.min, op1=ALU.max), r=[pbk], w=[a2k])
                p.op("pool", lambda e, a1=a1, s_=s_: e.tensor_tensor(out=a1[:], in0=a1[:], in1=s_[:], op=ALU.mult), r=[a1k, sk_], w=[a1k])
                p.op("dve", lambda e, aT=aT, a1=a1, a2=a2, c=c: e.scalar_tensor_tensor(out=aT[:, c, :], in0=a2[:], scalar=1.0, in1=a1[:], op0=ALU.add, op1=ALU.mult),
                     r=[a1k, a2k], w=[aTk])
            for j in range(BLK // 128):
                y_, yk = yo.next()
                for n in range(2):
                    pd, pdk = psd.next()
                    for kc in range(KC):
                        p.op("pe", lambda e, pd=pd, aT=aT, d_=d_, kc=kc, j=j, n=n: e.matmul(
                            pd[:], lhsT=aT[:, kc, j * 128:(j + 1) * 128], rhs=d_[:, kc, n * 512:(n + 1) * 512], start=(kc == 0), stop=False), r=[aTk, dk], w=[pdk])
                    p.op("pe", lambda e, pd=pd, o_=o_, n=n: e.matmul(pd[:], lhsT=o_[:, 0:128], rhs=bdn[:, n * 512:(n + 1) * 512], start=False, stop=True),
                         r=[ok_, "xbias"], w=[pdk])
                    ev += 1
                    if ev % 2 == 0:
                        p.op("act", lambda e, y_=y_, pd=pd, n=n: e.copy(out=y_[:, n * 512:(n + 1) * 512], in_=pd[:]), r=[pdk], w=[yk])
                    else:
                        p.op("dve", lambda e, y_=y_, pd=pd, n=n: e.tensor_copy(out=y_[:, n * 512:(n + 1) * 512], in_=pd[:]), r=[pdk], w=[yk])
                row = b_ * BLK + j * 128
                p.dma("sp", lambda e, y_=y_, row=row: e.dma_start(out=Y[row:row + 128, :], in_=y_[:]), r=[yk], w=["Y"])


def phase_moe_sparse_combine(p, C, io, RT, l, t_lo, t_hi, n_ctx):
    xres, Y, modrow = io["xres"], io["Y"], io["modrow"]
    ntile = (t_hi - t_lo) // 128
    with p.phase():
        gt = p.sb("cgt2", [128, 2, D], F32)
        for r in range(2):
            p.dma("sp", lambda e, r=r: e.dma_start(out=gt[:, r, :], in_=modrow[l, r, 5120:6144].partition_broadcast(128)), w=["cgt2"])
        yk_ = Ring(p, "cyk", [128, D], n=8)
        acc = Ring(p, "cacc", [128, D], n=2)
        xt = Ring(p, "cxt", [128, D], n=2)
        for ti in range(ntile):
            tok = t_lo + ti * 128
            r = 1 if tok < n_ctx else 0
            x, xk = xt.next()
            p.dma("sp", lambda e, x=x, tok=tok: e.dma_start(out=x[:], in_=xres[tok:tok + 128, :]), w=[xk])
            a, ak = acc.next()
            for k in range(4):
                y, yk = yk_.next()
                p.dma("pool", lambda e, y=y, ti=ti, k=k: e.indirect_dma_start(
                    out=y[:, :], out_offset=None, in_=Y[:, :], in_offset=bass.IndirectOffsetOnAxis(ap=RT["desti"][:, ti, k:k + 1], axis=0)), r=["rt", "Y"], w=[yk])
                if k == 0:
                    p.op("dve", lambda e, a=a, y=y, ti=ti: e.tensor_scalar(out=a[:], in0=y[:], scalar1=RT["gate4"][:, ti, 0:1], scalar2=None, op0=ALU.mult), r=[yk, "rt"], w=[ak])
                else:
                    p.op("dve", lambda e, a=a, y=y, ti=ti, k=k: e.scalar_tensor_tensor(out=a[:], in0=y[:], scalar=RT["gate4"][:, ti, k:k + 1], in1=a[:], op0=ALU.mult, op1=ALU.add),
                         r=[yk, "rt", ak], w=[ak])
            p.op("pool", lambda e, a=a, r=r: e.tensor_tensor(out=a[:], in0=a[:], in1=gt[:, r, :], op=ALU.mult), r=[ak, "cgt2"], w=[ak])
            p.op("dve", lambda e, a=a, x=x: e.tensor_tensor(out=x[:], in0=x[:], in1=a[:], op=ALU.add), r=[ak, xk], w=[xk])
            p.dma("sp", lambda e, x=x, tok=tok: e.dma_start(out=xres[tok:tok + 128, :], in_=x[:]), r=[xk], w=["xres"])

from concourse.bass_utils import run_bass_kernel_spmd

N_CTX = 256
N_LAT = 8192
DEPTH = 2
NEXP = 32
_W_NAMES = ["ada_w", "ada_b", "norm1_g", "norm2_g", "ev_w_in", "ev_w_out", "gla_gk_up", "gla_gk_b", "gla_norm_g",
            "rw_mu", "rw_w0", "rw_w2", "rw_a0", "rw_a2", "rw_k_k", "rw_k_a", "rw_r_k", "rw_g2", "rw_ln_w", "rw_ln_b",
            "od_w_in", "mla_q_norm", "mla_wq_up", "mla_kv_norm", "mla_wkv_up", "od_w_out",
            "router_w", "router_b", "moe_gu_w", "moe_gu_b", "moe_down_w", "moe_down_b", "final_norm_g"]


def build(shapes, n_ctx=N_CTX, n_lat=N_LAT, NE=NEXP):
    n_tok = n_ctx + n_lat
    L = DEPTH
    nc = bass.Bass("TRN2", target_bir_lowering=False)
    NBmax = (4 * n_tok + 511) // 512 + NE
    io = {}
    for k, shp in shapes.items():
        io[k] = dram(nc, k, list(shp), kind="ExternalInput")
    for name, shp, dt in (("xres", [n_tok, 1024], F32), ("modrow", [L, 2, 6144], F32), ("PT", [3376, n_tok], F32),
                          ("PTOK", [n_tok, 1536], F32), ("SMT", [1824, n_tok], F32), ("YTG", [2, 512, n_tok], F32),
                          ("YTR", [2, 512, n_tok], F32), ("RK", [2, 512, n_tok], F32), ("MIXT", [1024, n_tok], BF16),
                          ("HT", [1024, n_tok], BF16), ("GATE", [n_tok, NE], F32), ("WGU", [NE, 1024, 2048], BF16),
                          ("WDN", [NE, 1024, 1024], BF16), ("QN", [8, 128, n_lat], BF16), ("QR", [8, 64, n_lat], BF16),
                          ("KN", [8, 128, n_tok], BF16), ("KRo", [64, n_tok], BF16), ("Vt", [n_tok, 1024], BF16),
                          ("HTOK", [n_tok, 1024], BF16), ("HS", [NBmax * 512, 1024], BF16), ("Y", [NBmax * 512, 1024], F32)):
        io[name] = dram(nc, name, shp, dt)
    io["out"] = dram(nc, "out", [n_lat, 1024], kind="ExternalOutput")
    p = Prog(nc)
    C = make_consts(p)
    make_masks(p, C)
    make_rope_consts(p, C)
    G = {k: p.sb("G" + k, [128, L, 2, 8], F32) for k in ("A1", "B1", "A2", "B2")}
    RT = moe_tables(p, NE, n_tok // 128, NBmax)
    with p.phase():
        cp = Ring(p, "cpx", [128, 1024], n=3)
        for ti in range(n_tok // 128):
            x_, xk = cp.next()
            p.dma("sp", lambda e, x_=x_, ti=ti: e.dma_start(out=x_[:], in_=io["xin"][ti * 128:(ti + 1) * 128, :]), w=[xk])
            p.dma("sp", lambda e, x_=x_, ti=ti: e.dma_start(out=io["xres"][ti * 128:(ti + 1) * 128, :], in_=x_[:]), r=[xk], w=["xres"])
    phase_ada(p, C, io, L, G)
    phase_inproj(p, C, io, G, 0, n_ctx, n_tok)
    phase_gla(p, C, io, n_ctx, n_tok)
    phase_gla_finish(p, C, io, n_tok)
    phase_shift(p, C, io, n_ctx, n_tok)
    phase_rwkv(p, C, io, n_ctx, n_tok)
    phase_rwkv_finish(p, C, io, n_tok)
    phase_outproj(p, C, io, 0, io["ev_w_out"], n_ctx, n_tok)
    NB = phase_moe_sparse_route(p, C, io, G, RT, 0, NE, 0, n_tok, n_ctx)
    phase_moe_sparse_dispatch(p, C, io, RT, 0, n_tok, NB)
    phase_moe_sparse_experts(p, C, io, RT, 0, NE, NB)
    phase_moe_sparse_combine(p, C, io, RT, 0, 0, n_tok, n_ctx)
    phase_mla_proj(p, C, io, G, 1, n_ctx, n_tok)
    phase_mla_attn(p, C, io, n_ctx, n_tok)
    phase_outproj(p, C, io, 1, io["od_w_out"], n_ctx, n_tok, t_lo=n_ctx)
    NB = phase_moe_sparse_route(p, C, io, G, RT, 1, NE, n_ctx, n_tok, n_ctx)
    phase_moe_sparse_dispatch(p, C, io, RT, n_ctx, n_tok, NB)
    phase_moe_sparse_experts(p, C, io, RT, 1, NE, NB)
    phase_moe_sparse_combine(p, C, io, RT, 1, n_ctx, n_tok, n_ctx)
    phase_final(p, C, io, n_ctx, n_tok)
    p.finish()
    return nc


def kernel(**inputs):
    f = lambda a: np.ascontiguousarray(np.asarray(a, dtype=np.float32))
    x, c, ctx, c_ctx = f(inputs["x"]), f(inputs["c"]), f(inputs["ctx"]), f(inputs["c_ctx"])
    B = x.shape[0]
    shared = {}
    for k in _W_NAMES:
        a = f(inputs[k])
        if k.startswith(("ev_", "gla_", "rw_", "od_", "mla_")):
            a = np.ascontiguousarray(a[0])
        shared[k] = a
    in_maps = []
    for b in range(B):
        m = dict(shared)
        m["xin"] = np.ascontiguousarray(np.concatenate([ctx[b], x[b]], axis=0))
        m["cvec"] = np.ascontiguousarray(np.stack([c[b], c_ctx], axis=0))
        in_maps.append(m)
    shapes = {k: v.shape for k, v in in_maps[0].items()}
    nc = build(shapes, n_ctx=ctx.shape[1], n_lat=x.shape[1], NE=shared["router_w"].shape[-1])
    res = run_bass_kernel_spmd(nc, in_maps, core_ids=list(range(B)))
    return np.stack([np.asarray(r["out"], dtype=np.float32) for r in res.results], axis=0)
```

```python
import contextlib
import numpy as np
import concourse.bass as bass
import concourse.mybir as mybir

F32 = mybir.dt.float32
BF16 = mybir.dt.bfloat16
I32 = mybir.dt.int32
U32 = mybir.dt.uint32
ALU = mybir.AluOpType
AF = mybir.ActivationFunctionType
AX = mybir.AxisListType

ENGS = ("pe", "act", "dve", "pool", "sp")
RESET_THRESH = 20000


class Prog:
    def __init__(self, nc, n_dma_slots=10):
        self.nc = nc
        self.es = contextlib.ExitStack()
        self.cur = self.es
        self.streams = {e: [] for e in ENGS}
        self.count = {e: 0 for e in ENGS}
        self.seen = {e: {} for e in ENGS}
        self.bufs = {}
        self.sems = {}
        for e in ("pe", "act", "dve", "pool"):
            self.sems[e] = self.es.enter_context(nc.semaphore("s_" + e))
        self.dma_slots = {}
        self.dma_rr = {}
        for q in ("sp", "act", "pool"):
            self.dma_slots[q] = []
            for i in range(n_dma_slots):
                s = self.es.enter_context(nc.semaphore(f"d_{q}{i}"))
                self.sems[f"d_{q}{i}"] = s
                self.dma_slots[q].append([f"d_{q}{i}", 0])
            self.dma_rr[q] = 0
        self.n_instr = 0
        self.bsem = self.es.enter_context(nc.semaphore("s_bar"))
        self.gsem = self.es.enter_context(nc.semaphore("s_go"))
        self.nreset = 0
        self.reset_thresh = RESET_THRESH

    def _maybe_reset(self):
        if max(self.count.values()) >= self.reset_thresh or \
                max(v for q in self.dma_slots if q != "pool" for _, v in self.dma_slots[q]) >= 2 * self.reset_thresh:
            self.sync_reset()

    def sync_reset(self):
        self.barrier()
        self.nreset += 1
        k = self.nreset
        bs, gs = self.bsem, self.gsem
        for e in ENGS:
            self.streams[e].append(lambda eng, bs=bs: eng.sem_inc(bs, 1))
        self.streams["sp"].append(lambda eng, bs=bs, k=k: eng.wait_ge(bs, 5 * k))
        for name, sem in self.sems.items():
            if name.startswith("d_pool"):
                continue
            self.streams["sp"].append(lambda eng, sem=sem: eng.sem_clear(sem))
        self.streams["sp"].append(lambda eng, gs=gs: eng.sem_inc(gs, 1))
        for e in ENGS:
            if e != "sp":
                self.streams[e].append(lambda eng, gs=gs, k=k: eng.wait_ge(gs, k))
        self.count = {e: 0 for e in ENGS}
        self.seen = {e: {} for e in ENGS}
        self.bufs = {}
        for q in self.dma_slots:
            if q == "pool":
                continue
            for slot in self.dma_slots[q]:
                slot[1] = 0

    def sb(self, name, shape, dt=F32):
        self._uid = getattr(self, "_uid", 0) + 1
        name = f"{name}_u{self._uid}"
        return self.cur.enter_context(self.nc.sbuf_tensor(name, list(shape), dt))

    def ps(self, name, shape, dt=F32):
        self._uid = getattr(self, "_uid", 0) + 1
        name = f"{name}_u{self._uid}"
        return self.cur.enter_context(self.nc.psum_tensor(name, list(shape), dt))

    def _deps(self, r, w):
        deps = {}

        def add(tok):
            if tok is None:
                return
            s, v = tok
            if deps.get(s, 0) < v:
                deps[s] = v

        for k in r:
            b = self.bufs.setdefault(k, {"w": None, "r": {}})
            add(b["w"])
        for k in w:
            b = self.bufs.setdefault(k, {"w": None, "r": {}})
            add(b["w"])
            for s, v in b["r"].items():
                add((s, v))
        return deps

    def _commit(self, r, w, tok):
        for k in r:
            b = self.bufs[k]
            if b["r"].get(tok[0], 0) < tok[1]:
                b["r"][tok[0]] = tok[1]
        for k in w:
            b = self.bufs[k]
            b["w"] = tok
            b["r"] = {}

    def _emit_waits(self, eng, deps):
        seen = self.seen[eng]
        for s, v in deps.items():
            if eng == "pe" and s == "pe":
                continue
            if seen.get(s, 0) >= v:
                continue
            seen[s] = v
            sem = self.sems[s]
            self.streams[eng].append(lambda e, sem=sem, v=v: e.wait_ge(sem, v))

    def op(self, eng, fn, r=(), w=()):
        self._maybe_reset()
        deps = self._deps(r, w)
        self._emit_waits(eng, deps)
        self.count[eng] += 1
        n = self.count[eng]
        sem = self.sems[eng]
        self.streams[eng].append(lambda e, fn=fn, sem=sem: fn(e).then_inc(sem, 1))
        self._commit(r, w, (eng, n))
        self.n_instr += 1

    def dma(self, q, fn, r=(), w=()):
        self._maybe_reset()
        deps = self._deps(r, w)
        i = self.dma_rr[q]
        self.dma_rr[q] = (i + 1) % len(self.dma_slots[q])
        slot = self.dma_slots[q][i]
        if slot[1] > 0:
            if deps.get(slot[0], 0) < slot[1]:
                deps[slot[0]] = slot[1]
        self._emit_waits(q, deps)
        slot[1] += 16
        sem = self.sems[slot[0]]
        self.streams[q].append(lambda e, fn=fn, sem=sem: fn(e).then_inc(sem, 16))
        self._commit(r, w, (slot[0], slot[1]))
        self.n_instr += 1

    def wait_all(self, eng, keys):
        deps = self._deps(keys, ())
        self._emit_waits(eng, deps)

    def barrier(self):
        full = {}
        for e in ("pe", "act", "dve", "pool"):
            if self.count[e] > 0:
                full[e] = self.count[e]
        for q in self.dma_slots:
            for name, v in self.dma_slots[q]:
                if v > 0:
                    full[name] = v
        for e in ENGS:
            d = dict(full)
            d.pop(e, None)
            if e == "pe":
                pass
            self._emit_waits(e, d)

    @contextlib.contextmanager
    def phase(self):
        old = self.cur
        with contextlib.ExitStack() as st:
            self.cur = st
            yield
            self.barrier()
            self.flush()
        self.cur = old

    def flush(self):
        self._emit_block()
        self.streams = {e: [] for e in ENGS}

    def finish(self):
        self.barrier()
        self.flush()
        self.es.close()

    def _emit_block(self):
        nc = self.nc
        with nc.Block() as block:
            @block.tensor
            def _(e):
                for f in self.streams["pe"]:
                    f(e)

            @block.scalar
            def _(e):
                for f in self.streams["act"]:
                    f(e)

            @block.vector
            def _(e):
                for f in self.streams["dve"]:
                    f(e)

            @block.gpsimd
            def _(e):
                for f in self.streams["pool"]:
                    f(e)

            @block.sync
            def _(e):
                for f in self.streams["sp"]:
                    f(e)

import contextlib
import numpy as np

D = 1024
KC = 8
EPS = 1e-6
GLA_COLS = 1552
RW_COLS = 1824
EVEN_IN = 3376


def dram(nc, name, shape, dt=F32, kind="Internal"):
    return nc.dram_tensor(name, list(shape), dt, kind=kind).ap()


def make_consts(p):
    nc = p.nc
    C = {}
    iota_f = p.sb("iota_f", [128, 128], F32)
    iota_p = p.sb("iota_p", [128, 1], F32)
    ii = p.sb("iota_i", [128, 128], I32)
    ip = p.sb("iota_pi", [128, 1], I32)
    p.op("pool", lambda e: e.iota(ii[:], pattern=[[1, 128]], base=0, channel_multiplier=0), w=["iota_i"])
    p.op("pool", lambda e: e.iota(ip[:], pattern=[[1, 1]], base=0, channel_multiplier=1), w=["iota_pi"])
    p.op("dve", lambda e: e.tensor_copy(out=iota_f[:], in_=ii[:]), r=["iota_i"], w=["iota_f"])
    p.op("dve", lambda e: e.tensor_copy(out=iota_p[:], in_=ip[:]), r=["iota_pi"], w=["iota_p"])
    ident = p.sb("ident", [128, 128], F32)
    p.op("dve", lambda e: e.tensor_scalar(out=ident[:], in0=iota_f[:], scalar1=iota_p[:, 0:1], scalar2=None,
                                          op0=ALU.is_equal), r=["iota_f", "iota_p"], w=["ident"])
    identb = p.sb("identb", [128, 128], BF16)
    p.op("dve", lambda e: e.tensor_copy(out=identb[:], in_=ident[:]), r=["ident"], w=["identb"])
    C.update(iota_f=iota_f, iota_p=iota_p, ident=ident, identb=identb)
    return C


def phase_ada(p, C, io, L, G):
    nc = p.nc
    cvec, ada_w, ada_b = io["cvec"], io["ada_w"], io["ada_b"]
    modrow = io["modrow"]
    with p.phase():
        cT = p.sb("cT", [128, KC, 2], F32)
        sT = p.sb("sT", [128, KC, 2], F32)
        for r in range(2):
            p.dma("sp", lambda e, r=r: e.dma_start(out=cT[:, :, r], in_=cvec[r, :].rearrange("(c q) -> q c", q=128),
                                                   allow_slow_non_contiguous=True), w=["cT"])
        p.op("act", lambda e: e.activation(out=sT[:], in_=cT[:], func=AF.Silu), r=["cT"], w=["sT"])
        wts = [p.sb(f"adaw{i}", [128, KC, 512], F32) for i in range(2)]
        mrow = p.sb("mrow", [2, 6144], F32)
        brow = p.sb("brow", [2, 6144], F32)
        mps = p.ps("mps", [2, 512], F32)
        tps = p.ps("tps", [128, 48, 2], F32)
        mcol = p.sb("mcol", [128, 48, 2], F32)
        gcol = p.sb("gcol", [128, 2, KC], F32)
        it = 0
        for l in range(L):
            for r in range(2):
                p.dma("sp", lambda e, r=r, l=l: e.dma_start(out=brow[r:r + 1, :], in_=ada_b[l:l + 1, :]), w=["brow"])
            p.dma("sp", lambda e, l=l: e.dma_start(out=gcol[:, 0, :], in_=io["norm1_g"][l, :].rearrange("(c q) -> q c", q=128),
                                                   allow_slow_non_contiguous=True), w=["gcol"])
            p.dma("sp", lambda e, l=l: e.dma_start(out=gcol[:, 1, :], in_=io["norm2_g"][l, :].rearrange("(c q) -> q c", q=128),
                                                   allow_slow_non_contiguous=True), w=["gcol"])
            for cc in range(12):
                wt = wts[it % 2]
                wk = f"adaw{it % 2}"
                it += 1
                p.dma("sp", lambda e, wt=wt, l=l, cc=cc: e.dma_start(
                    out=wt[:], in_=ada_w[l, :, cc * 512:(cc + 1) * 512].rearrange("(c q) n -> q c n", q=128)), w=[wk])
                for kc in range(KC):
                    p.op("pe", lambda e, wt=wt, kc=kc: e.matmul(mps[:], lhsT=sT[:, kc, :], rhs=wt[:, kc, :],
                                                                 start=(kc == 0), stop=(kc == KC - 1)),
                         r=["sT", wk], w=["mps"])
                p.op("dve", lambda e, cc=cc: e.tensor_tensor(out=mrow[:, cc * 512:(cc + 1) * 512], in0=mps[:],
                                                             in1=brow[:, cc * 512:(cc + 1) * 512], op=ALU.add),
                     r=["mps", "brow"], w=["mrow"])
            p.dma("sp", lambda e, l=l: e.dma_start(out=modrow[l], in_=mrow[:]), r=["mrow"], w=[f"modrow{l}"])
            for c in range(48):
                p.op("pe", lambda e, c=c: e.transpose(tps[:, c, :], mrow[0:2, c * 128:(c + 1) * 128], C["ident"][0:2, 0:2]),
                     r=["mrow", "ident"], w=["tps"])
            p.op("dve", lambda e: e.tensor_copy(out=mcol[:], in_=tps[:]), r=["tps"], w=["mcol"])
            for r in range(2):
                for (nm, sc_c, sh_c, gi) in (("1", 1, 0, 0), ("2", 4, 3, 1)):
                    A = G["A" + nm]
                    B = G["B" + nm]
                    p.op("dve", lambda e, A=A, r=r, l=l, sc_c=sc_c, gi=gi: e.scalar_tensor_tensor(
                        out=A[:, l, r, :], in0=mcol[:, sc_c * 8:(sc_c + 1) * 8, r], scalar=1.0, in1=gcol[:, gi, :],
                        op0=ALU.add, op1=ALU.mult), r=["mcol", "gcol"], w=["G"])
                    p.op("dve", lambda e, B=B, r=r, l=l, sh_c=sh_c: e.tensor_copy(
                        out=B[:, l, r, :], in_=mcol[:, sh_c * 8:(sh_c + 1) * 8, r]), r=["mcol"], w=["G"])


def norm_tile(p, C, T, xt_key, xt, Acol, Bcol, hT_dst, hT_key, hT32_dst=None, want_bf=True):
    ss, rstd, xn, junk = T["ss"], T["rstd"], T["xn"], T["junk"]
    p.op("act", lambda e: e.activation(out=junk[:], in_=xt[:], func=AF.Square, accum_out=ss[:, 0:1]),
         r=[xt_key], w=["junk", "ss"])
    p.op("dve", lambda e: e.tensor_scalar(out=rstd[:], in0=ss[:], scalar1=1.0 / D, scalar2=EPS, op0=ALU.mult, op1=ALU.add),
         r=["ss"], w=["rstd"])
    p.op("act", lambda e: e.activation(out=ss[:], in_=rstd[:], func=AF.Sqrt), r=["rstd"], w=["ss"])
    p.op("dve", lambda e: e.reciprocal(out=rstd[:], in_=ss[:]), r=["ss"], w=["rstd"])
    p.op("dve", lambda e: e.tensor_scalar(out=xn[:], in0=xt[:], scalar1=rstd[:, 0:1], scalar2=None, op0=ALU.mult),
         r=[xt_key, "rstd"], w=["xn"])
    for half in range(2):
        tp = T["tp"][half]
        tk = f"tp{half}"
        for j in range(4):
            kc = half * 4 + j
            p.op("pe", lambda e, tp=tp, j=j, kc=kc: e.transpose(tp[:, j, :], xn[:, kc * 128:(kc + 1) * 128], C["ident"][:]),
                 r=["xn", "ident"], w=[tk])
        for j in range(4):
            kc = half * 4 + j
            if hT32_dst is None:
                p.op("act", lambda e, tp=tp, j=j, kc=kc: e.activation(out=hT_dst(kc), in_=tp[:, j, :], func=AF.Identity,
                                                                       scale=Acol[:, kc:kc + 1], bias=Bcol[:, kc:kc + 1]),
                     r=[tk, "G"], w=[hT_key])
            else:
                p.op("act", lambda e, tp=tp, j=j, kc=kc: e.activation(out=hT32_dst(kc), in_=tp[:, j, :], func=AF.Identity,
                                                                       scale=Acol[:, kc:kc + 1], bias=Bcol[:, kc:kc + 1]),
                     r=[tk, "G"], w=[hT_key + "32"])
                if want_bf:
                    p.op("pool", lambda e, kc=kc: e.tensor_copy(out=hT_dst(kc), in_=hT32_dst(kc)), r=[hT_key + "32"], w=[hT_key])


def norm_scratch(p):
    T = {}
    T["ss"] = p.sb("ss", [128, 1], F32)
    T["rstd"] = p.sb("rstd", [128, 1], F32)
    T["xn"] = p.sb("xn", [128, D], F32)
    T["junk"] = p.sb("junk", [128, D], BF16)
    T["tp"] = [p.ps(f"tp{h}", [128, 4, 128], F32) for h in range(2)]
    return T


def load_cast_weight(p, dst, dst_key, src_ap, rows_kc, ncols, stage, stage_key, eng_cycle=("dve", "pool")):
    for kc in range(rows_kc):
        st = stage[kc % len(stage)]
        sk = f"{stage_key}{kc % len(stage)}"
        p.dma("sp", lambda e, st=st, kc=kc: e.dma_start(out=st[:, 0:ncols], in_=src_ap[kc * 128:(kc + 1) * 128, :]), w=[sk])
        eng = eng_cycle[kc % len(eng_cycle)]
        p.op(eng, lambda e, st=st, kc=kc: e.tensor_copy(out=dst[:, kc, :], in_=st[:, 0:ncols]), r=[sk], w=[dst_key])


def phase_inproj(p, C, io, G, l, n_ctx, n_tok):
    xres, PT, PTOK, w_in = io["xres"], io["PT"], io["PTOK"], io["ev_w_in"]
    with p.phase():
        T = norm_scratch(p)
        wbf = p.sb("winbf", [128, KC, EVEN_IN], BF16)
        stage = [p.sb(f"wstage{i}", [128, EVEN_IN], F32) for i in range(2)]
        load_cast_weight(p, wbf, "winbf", w_in, KC, EVEN_IN, stage, "wstage")
        xts = [p.sb(f"xt{i}", [128, D], F32) for i in range(2)]
        hT = [p.sb(f"hT{i}", [128, KC, 512], BF16) for i in range(2)]
        pps = [p.ps(f"pps{i}", [128, 512], F32) for i in range(3)]
        ost = [p.sb(f"ost{i}", [128, 512], F32) for i in range(4)]
        ntile = n_tok // 128
        nsup = (ntile + 3) // 4
        oi = 0
        pi = 0
        tok_cols = [(512, 512), (1024, 512), (GLA_COLS + 1024, 512)]
        for s in range(nsup):
            tiles = list(range(s * 4, min(ntile, s * 4 + 4)))
            N = len(tiles) * 128
            h = hT[s % 2]
            hk = f"hT{s % 2}"
            for j, ti in enumerate(tiles):
                r = 1 if ti * 128 < n_ctx else 0
                xt = xts[ti % 2]
                xk = f"xt{ti % 2}"
                p.dma("sp", lambda e, xt=xt, ti=ti: e.dma_start(out=xt[:], in_=xres[ti * 128:(ti + 1) * 128, :]), w=[xk])
                norm_tile(p, C, T, xk, xt, G["A1"][:, l, r, :], G["B1"][:, l, r, :],
                          lambda kc, h=h, j=j: h[:, kc, j * 128:(j + 1) * 128], hk)
            ncc = (EVEN_IN + 127) // 128
            for cc in range(ncc):
                cw = min(128, EVEN_IN - cc * 128)
                ps = pps[pi % 3]
                pk = f"pps{pi % 3}"
                pi += 1
                for kc in range(KC):
                    p.op("pe", lambda e, ps=ps, kc=kc, cc=cc, cw=cw, h=h, N=N: e.matmul(
                        ps[0:cw, 0:N], lhsT=wbf[:, kc, cc * 128:cc * 128 + cw], rhs=h[:, kc, 0:N],
                        start=(kc == 0), stop=(kc == KC - 1)), r=["winbf", hk], w=[pk])
                o = ost[oi % 4]
                ok = f"ost{oi % 4}"
                eng = "dve" if oi % 2 == 0 else "act"
                oi += 1
                if eng == "dve":
                    p.op("dve", lambda e, o=o, ps=ps, cw=cw, N=N: e.tensor_copy(out=o[0:cw, 0:N], in_=ps[0:cw, 0:N]), r=[pk], w=[ok])
                else:
                    p.op("act", lambda e, o=o, ps=ps, cw=cw, N=N: e.copy(out=o[0:cw, 0:N], in_=ps[0:cw, 0:N]), r=[pk], w=[ok])
                p.dma("sp", lambda e, o=o, cc=cc, cw=cw, N=N, s=s: e.dma_start(
                    out=PT[cc * 128:cc * 128 + cw, s * 512:s * 512 + N], in_=o[0:cw, 0:N]), r=[ok], w=["PT"])
            for j, ti in enumerate(tiles):
                for gi, (c0, cn) in enumerate(tok_cols):
                    ps = pps[pi % 3]
                    pk = f"pps{pi % 3}"
                    pi += 1
                    for kc in range(KC):
                        p.op("pe", lambda e, ps=ps, kc=kc, c0=c0, cn=cn, h=h, j=j: e.matmul(
                            ps[:, 0:cn], lhsT=h[:, kc, j * 128:(j + 1) * 128], rhs=wbf[:, kc, c0:c0 + cn],
                            start=(kc == 0), stop=(kc == KC - 1)), r=["winbf", hk], w=[pk])
                    o = ost[oi % 4]
                    ok = f"ost{oi % 4}"
                    eng = "dve" if oi % 2 == 0 else "act"
                    oi += 1
                    if eng == "dve":
                        p.op("dve", lambda e, o=o, ps=ps, cn=cn: e.tensor_copy(out=o[:, 0:cn], in_=ps[:, 0:cn]), r=[pk], w=[ok])
                    else:
                        p.op("act", lambda e, o=o, ps=ps, cn=cn: e.copy(out=o[:, 0:cn], in_=ps[:, 0:cn]), r=[pk], w=[ok])
                    p.dma("sp", lambda e, o=o, ti=ti, gi=gi, cn=cn: e.dma_start(
                        out=PTOK[ti * 128:(ti + 1) * 128, gi * 512:gi * 512 + cn], in_=o[:, 0:cn]), r=[ok], w=["PTOK"])

import os


class Ring:
    def __init__(self, p, name, shape, dt=F32, n=2, psum=False):
        self.t = [(p.ps if psum else p.sb)(f"{name}{i}", shape, dt) for i in range(n)]
        self.k = [f"{name}{i}" for i in range(n)]
        self.i = 0

    def next(self):
        i = self.i
        self.i = (i + 1) % len(self.t)
        return self.t[i], self.k[i]


class PsRing:
    def __init__(self, p, name, nbanks):
        self.banks = [p.ps(f"{name}{i}", [128, 512], F32) for i in range(nbanks)]
        self.n = nbanks * 1
        self.sub = 1
        self.name = name
        self.i = 0

    def next(self):
        i = self.i
        self.i = (i + 1) % self.n
        sb_ = self.sub
        return self.banks[i // sb_][:, (i % sb_) * 128:(i % sb_ + 1) * 128], f"{self.name}_{i}"


def make_masks(p, C):
    f, q = C["iota_f"], C["iota_p"]
    for nm, op in (("incl0", ALU.is_ge), ("incl1", ALU.is_le), ("strict0", ALU.is_gt), ("strict1", ALU.is_lt)):
        m = p.sb("m_" + nm, [128, 128], F32)
        p.op("dve", lambda e, m=m, op=op: e.tensor_scalar(out=m[:], in0=f[:], scalar1=q[:, 0:1], scalar2=None, op0=op),
             r=["iota_f", "iota_p"], w=["masks"])
        C[nm] = m
    for nm, val in (("ones128", 1.0 / 128), ("ones64", 1.0), ("ones64m", 1.0 / 64)):
        m = p.sb("m_" + nm, [128, 128], F32)
        p.op("dve", lambda e, m=m, val=val: e.memset(m[:], val), w=["masks"])
        C[nm] = m


def chunk_order(n_ctx, n_tok):
    nc_, nt = n_ctx // 128, n_tok // 128
    fwd = list(range(nt))
    bwd = list(range(nc_ - 1, -1, -1)) + list(range(nt - 1, nc_ - 1, -1))
    return [fwd, bwd]


def phase_gla(p, C, io, n_ctx, n_tok):
    PT, PTOK, YT = io["PT"], io["PTOK"], io["YTG"]
    gk_up, gk_b = io["gla_gk_up"], io["gla_gk_b"]
    order = chunk_order(n_ctx, n_tok)
    nt = n_tok // 128
    with p.phase():
        gkup = p.sb("gkup", [16, 2, 256], F32)
        gkb = p.sb("gkb", [64, 2, 4], F32)
        for d in range(2):
            p.dma("sp", lambda e, d=d: e.dma_start(out=gkup[:, d, :], in_=gk_up[d]), w=["gkw"])
            p.dma("sp", lambda e, d=d: e.dma_start(out=gkb[:, d, :], in_=gk_b[d].rearrange("(h q) -> q h", q=64),
                                                   allow_slow_non_contiguous=True), w=["gkw"])
        S = [[p.sb(f"S{d}{h}", [64, 128], F32) for h in range(4)] for d in range(2)]
        for d in range(2):
            for h in range(4):
                p.op("pool", lambda e, t=S[d][h]: e.memset(t[:], 0.0), w=[f"S{d}{h}"])
        gd = Ring(p, "gd", [16, 128], n=4)
        qk = Ring(p, "qk", [64, 2, 128], n=9)
        vv = Ring(p, "vv", [128, 128], n=9)
        sg = Ring(p, "sg", [64, 128], n=9)
        lg = Ring(p, "lg", [64, 128], n=9)
        lgT = Ring(p, "lgT", [128, 64], n=9)
        eW = Ring(p, "eW", [64, 128], n=9)
        eWi = Ring(p, "eWi", [64, 128], n=9)
        rt = Ring(p, "rt", [64, 128], n=9)
        kt = Ring(p, "kt", [64, 128], n=9)
        ktok = Ring(p, "ktok", [128, 64], n=9)
        mrk = Ring(p, "mrk", [128, 128], n=9)
        yts = Ring(p, "yts", [128, 128], n=9)
        s2 = Ring(p, "s2", [64, 128], n=9)
        psA = Ring(p, "psA", [128, 128], n=8, psum=True)
        def unit(d, h, c, gdt, gdk):
            t0 = c * 128
            tend = 127 if d == 0 else 0
            Sk = f"S{d}{h}"
            St = S[d][h]
            qt, qkk = qk.next()
            p.dma("sp", lambda e, qt=qt, h=h, t0=t0: e.dma_start(out=qt[:, 0, :], in_=PT[h * 64:(h + 1) * 64, t0:t0 + 128]), w=[qkk])
            p.dma("sp", lambda e, qt=qt, h=h, t0=t0: e.dma_start(out=qt[:, 1, :], in_=PT[256 + h * 64:256 + (h + 1) * 64, t0:t0 + 128]), w=[qkk])
            vt, vk = vv.next()
            p.dma("sp", lambda e, vt=vt, h=h, t0=t0: e.dma_start(out=vt[:], in_=PTOK[t0:t0 + 128, h * 128:(h + 1) * 128]), w=[vk])
            yield
            ps, pk = psA.next()
            p.op("pe", lambda e, ps=ps, d=d, h=h, gdt=gdt: e.matmul(ps[0:64, :], lhsT=gkup[:, d, h * 64:(h + 1) * 64], rhs=gdt[:],
                                                                    start=True, stop=True), r=["gkw", gdk], w=[pk])
            sgt, sgk = sg.next()
            p.op("act", lambda e, sgt=sgt, ps=ps, d=d, h=h: e.activation(out=sgt[:], in_=ps[0:64, :], func=AF.Sigmoid,
                                                                        bias=gkb[:, d, h:h + 1]), r=[pk, "gkw"], w=[sgk])
            lgt, lgk = lg.next()
            p.op("act", lambda e, lgt=lgt, sgt=sgt: e.activation(out=lgt[:], in_=sgt[:], func=AF.Ln), r=[sgk], w=[lgk])
            yield
            ps, pk = psA.next()
            p.op("pe", lambda e, ps=ps, lgt=lgt: e.transpose(ps[:, 0:64], lgt[:], C["ident"][0:64, 0:64]), r=[lgk, "ident"], w=[pk])
            lTt, lTk = lgT.next()
            p.op("dve", lambda e, lTt=lTt, ps=ps: e.tensor_copy(out=lTt[:], in_=ps[:, 0:64]), r=[pk], w=[lTk])
            yield
            ps, pk = psA.next()
            p.op("pe", lambda e, ps=ps, lTt=lTt, d=d: e.matmul(ps[0:64, :], lhsT=lTt[:], rhs=C[f"incl{d}"][:], start=True, stop=True),
                 r=[lTk, "masks"], w=[pk])
            eWt, eWk = eW.next()
            eWit, eWik = eWi.next()
            p.op("act", lambda e, eWt=eWt, ps=ps: e.activation(out=eWt[:], in_=ps[0:64, :], func=AF.Exp, scale=1.0 / 16), r=[pk], w=[eWk])
            p.op("act", lambda e, eWit=eWit, ps=ps: e.activation(out=eWit[:], in_=ps[0:64, :], func=AF.Exp, scale=-1.0 / 16), r=[pk], w=[eWik])
            rtt, rtk = rt.next()
            ktt, ktk = kt.next()
            p.op("dve", lambda e, rtt=rtt, qt=qt, eWt=eWt: e.scalar_tensor_tensor(out=rtt[:], in0=qt[:, 0, :], scalar=0.125, in1=eWt[:],
                                                                                 op0=ALU.mult, op1=ALU.mult), r=[qkk, eWk], w=[rtk])
            p.op("dve", lambda e, ktt=ktt, qt=qt, eWit=eWit: e.tensor_tensor(out=ktt[:], in0=qt[:, 1, :], in1=eWit[:], op=ALU.mult),
                 r=[qkk, eWik], w=[ktk])
            yield
            ps, pk = psA.next()
            p.op("pe", lambda e, ps=ps, ktt=ktt: e.transpose(ps[:, 0:64], ktt[:], C["ident"][0:64, 0:64]), r=[ktk, "ident"], w=[pk])
            kTt, kTk = ktok.next()
            p.op("dve", lambda e, kTt=kTt, ps=ps: e.tensor_copy(out=kTt[:], in_=ps[:, 0:64]), r=[pk], w=[kTk])
            yield
            ps, pk = psA.next()
            p.op("pe", lambda e, ps=ps, ktt=ktt, rtt=rtt: e.matmul(ps[:], lhsT=ktt[:], rhs=rtt[:], start=True, stop=True), r=[ktk, rtk], w=[pk])
            mt, mk = mrk.next()
            p.op("dve", lambda e, mt=mt, ps=ps, d=d: e.tensor_tensor(out=mt[:], in0=ps[:], in1=C[f"incl{d}"][:], op=ALU.mult),
                 r=[pk, "masks"], w=[mk])
            yield
            ps, pk = psA.next()
            p.op("pe", lambda e, ps=ps, St=St, rtt=rtt: e.matmul(ps[:], lhsT=St[:], rhs=rtt[:], start=True, stop=False), r=[Sk, rtk], w=[pk])
            p.op("pe", lambda e, ps=ps, vt=vt, mt=mt: e.matmul(ps[:], lhsT=vt[:], rhs=mt[:], start=False, stop=True), r=[vk, mk], w=[pk])
            yt, yk = yts.next()
            p.op("act", lambda e, yt=yt, ps=ps: e.copy(out=yt[:], in_=ps[:]), r=[pk], w=[yk])
            p.dma("sp", lambda e, yt=yt, d=d, h=h, t0=t0: e.dma_start(out=YT[d, h * 128:(h + 1) * 128, t0:t0 + 128], in_=yt[:]),
                  r=[yk], w=["YTG"])
            yield
            ps, pk = psA.next()
            p.op("pe", lambda e, ps=ps, kTt=kTt, vt=vt: e.matmul(ps[0:64, :], lhsT=kTt[:], rhs=vt[:], start=True, stop=True), r=[kTk, vk], w=[pk])
            s2t, s2k = s2.next()
            p.op("dve", lambda e, s2t=s2t, St=St, eWt=eWt, tend=tend: e.tensor_scalar(out=s2t[:], in0=St[:], scalar1=eWt[:, tend:tend + 1],
                                                                                     scalar2=None, op0=ALU.mult), r=[Sk, eWk], w=[s2k])
            p.op("dve", lambda e, s2t=s2t, St=St, eWt=eWt, ps=ps, tend=tend: e.scalar_tensor_tensor(
                out=St[:], in0=ps[0:64, :], scalar=eWt[:, tend:tend + 1], in1=s2t[:], op0=ALU.mult, op1=ALU.add),
                r=[pk, eWk, s2k], w=[Sk])


        def drive(gens):
            gens = list(gens)
            while gens:
                for g_ in list(gens):
                    try:
                        next(g_)
                    except StopIteration:
                        gens.remove(g_)

        for i in range(nt):
            gens = []
            for d in range(2):
                c = order[d][i]
                t0 = c * 128
                gdt, gdk = gd.next()
                p.dma("sp", lambda e, gdt=gdt, t0=t0: e.dma_start(out=gdt[:], in_=PT[1536:1552, t0:t0 + 128]), w=[gdk])
                gens += [unit(d, h, c, gdt, gdk) for h in range(4)]
            drive(gens)


def phase_gla_finish(p, C, io, n_tok):
    PT, YT, MIXT = io["PT"], io["YTG"], io["MIXT"]
    with p.phase():
        gcol = p.sb("gng", [128, 1], F32)
        p.dma("sp", lambda e: e.dma_start(out=gcol[:, 0:1], in_=io["gla_norm_g"].rearrange("(q o) -> q o", o=1)), w=["gng"])
        y0 = Ring(p, "y0", [128, 512], n=2)
        y1 = Ring(p, "y1", [128, 512], n=2)
        og = Ring(p, "og", [128, 512], n=2)
        sq = Ring(p, "sq", [128, 512], n=2)
        rs = Ring(p, "rs", [128, 512], n=2)
        ob = Ring(p, "ob", [128, 512], BF16, n=2)
        psF = Ring(p, "psF", [128, 512], n=2, psum=True)
        for t0 in range(0, n_tok, 512):
            N = min(512, n_tok - t0)
            for h in range(4):
                a, ak = y0.next()
                b, bk = y1.next()
                o, ok = og.next()
                p.dma("sp", lambda e, a=a, h=h, t0=t0, N=N: e.dma_start(out=a[:, 0:N], in_=YT[0, h * 128:(h + 1) * 128, t0:t0 + N]), r=["YTG"], w=[ak])
                p.dma("sp", lambda e, b=b, h=h, t0=t0, N=N: e.dma_start(out=b[:, 0:N], in_=YT[1, h * 128:(h + 1) * 128, t0:t0 + N]), r=["YTG"], w=[bk])
                p.dma("sp", lambda e, o=o, h=h, t0=t0, N=N: e.dma_start(out=o[:, 0:N], in_=PT[1024 + h * 128:1024 + (h + 1) * 128, t0:t0 + N]), r=["PT"], w=[ok])
                p.op("dve", lambda e, a=a, b=b, N=N: e.tensor_tensor(out=a[:, 0:N], in0=a[:, 0:N], in1=b[:, 0:N], op=ALU.add), r=[ak, bk], w=[ak])
                s, sk = sq.next()
                p.op("act", lambda e, s=s, a=a, N=N: e.activation(out=s[:, 0:N], in_=a[:, 0:N], func=AF.Square), r=[ak], w=[sk])
                ps, pk = psF.next()
                p.op("pe", lambda e, ps=ps, s=s, N=N: e.matmul(ps[:, 0:N], lhsT=C["ones128"][:], rhs=s[:, 0:N], start=True, stop=True), r=[sk, "masks"], w=[pk])
                r_, rk = rs.next()
                p.op("dve", lambda e, r_=r_, ps=ps, N=N: e.tensor_scalar(out=r_[:, 0:N], in0=ps[:, 0:N], scalar1=EPS, scalar2=None, op0=ALU.add), r=[pk], w=[rk])
                p.op("act", lambda e, r_=r_, N=N: e.activation(out=r_[:, 0:N], in_=r_[:, 0:N], func=AF.Sqrt), r=[rk], w=[rk])
                p.op("dve", lambda e, r_=r_, N=N: e.reciprocal(out=r_[:, 0:N], in_=r_[:, 0:N]), r=[rk], w=[rk])
                p.op("dve", lambda e, a=a, r_=r_, N=N: e.scalar_tensor_tensor(out=a[:, 0:N], in0=a[:, 0:N], scalar=gcol[:, 0:1], in1=r_[:, 0:N],
                                                                             op0=ALU.mult, op1=ALU.mult), r=[ak, rk, "gng"], w=[ak])
                p.op("act", lambda e, s=s, o=o, N=N: e.activation(out=s[:, 0:N], in_=o[:, 0:N], func=AF.Silu), r=[ok], w=[sk])
                ot, otk = ob.next()
                p.op("dve", lambda e, ot=ot, a=a, s=s, N=N: e.tensor_tensor(out=ot[:, 0:N], in0=a[:, 0:N], in1=s[:, 0:N], op=ALU.mult), r=[ak, sk], w=[otk])
                p.dma("sp", lambda e, ot=ot, h=h, t0=t0, N=N: e.dma_start(out=MIXT[h * 128:(h + 1) * 128, t0:t0 + N], in_=ot[:, 0:N]), r=[otk], w=["MIXT"])


import os
STAGE = 99
R0 = GLA_COLS
NSQ = 12
NLW = 0.6065306597126334


def phase_shift(p, C, io, n_ctx, n_tok):
    PT, SMT, mu = io["PT"], io["SMT"], io["rw_mu"]
    with p.phase():
        nch = (RW_COLS + 127) // 128
        muc = p.sb("muc", [128, nch, 2], F32)
        p.op("pool", lambda e: e.memset(muc[:], 0.0), w=["muc"])
        for j in range(2):
            for c in range(nch):
                cw = min(128, RW_COLS - c * 128)
                p.dma("sp", lambda e, j=j, c=c, cw=cw: e.dma_start(out=muc[0:cw, c, j:j + 1],
                                                                   in_=mu[j, c * 128:c * 128 + cw].rearrange("(q o) -> q o", o=1)), w=["muc"])
        xin = Ring(p, "shx", [128, 514], n=3)
        d0 = Ring(p, "shd", [128, 512], n=3)
        so = Ring(p, "sho", [128, 512], n=3)
        blocks = []
        for (a, b) in ((0, n_ctx), (n_ctx, n_tok)):
            t = a
            while t < b:
                N = min(512, b - t)
                blocks.append((t, N, t == a, t + N == b))
                t += N
        for (t0, N, first, last) in blocks:
            for c in range(nch):
                cw = min(128, RW_COLS - c * 128)
                x, xk = xin.next()
                lo = 0 if not first else 1
                hi = N + 2 if not last else N + 1
                if first or last:
                    p.op("pool", lambda e, x=x: e.memset(x[:], 0.0), w=[xk])
                p.dma("sp", lambda e, x=x, c=c, cw=cw, lo=lo, hi=hi, t0=t0: e.dma_start(
                    out=x[0:cw, lo:hi], in_=PT[R0 + c * 128:R0 + c * 128 + cw, t0 - 1 + lo:t0 - 1 + hi]), w=[xk])
                dd, dk = d0.next()
                o, ok = so.next()
                p.op("dve", lambda e, dd=dd, x=x, cw=cw, N=N: e.tensor_tensor(out=dd[0:cw, 0:N], in0=x[0:cw, 0:N], in1=x[0:cw, 1:N + 1], op=ALU.subtract),
                     r=[xk], w=[dk])
                p.op("dve", lambda e, o=o, dd=dd, x=x, c=c, cw=cw, N=N: e.scalar_tensor_tensor(
                    out=o[0:cw, 0:N], in0=dd[0:cw, 0:N], scalar=muc[0:cw, c, 0:1], in1=x[0:cw, 1:N + 1], op0=ALU.mult, op1=ALU.add),
                    r=[dk, xk, "muc"], w=[ok])
                p.op("pool", lambda e, dd=dd, x=x, cw=cw, N=N: e.tensor_tensor(out=dd[0:cw, 0:N], in0=x[0:cw, 2:N + 2], in1=x[0:cw, 1:N + 1], op=ALU.subtract),
                     r=[xk, ok], w=[dk])
                p.op("dve", lambda e, o=o, dd=dd, c=c, cw=cw, N=N: e.scalar_tensor_tensor(
                    out=o[0:cw, 0:N], in0=dd[0:cw, 0:N], scalar=muc[0:cw, c, 1:2], in1=o[0:cw, 0:N], op0=ALU.mult, op1=ALU.add),
                    r=[dk, ok, "muc"], w=[ok])
                p.dma("sp", lambda e, o=o, c=c, cw=cw, N=N, t0=t0: e.dma_start(out=SMT[c * 128:c * 128 + cw, t0:t0 + N], in_=o[0:cw, 0:N]),
                      r=[ok], w=["SMT"])


def colload(p, dst, key, src_vec, nh, j=None):
    out = dst[:, :] if j is None else dst[:, j, :]
    p.dma("sp", lambda e: e.dma_start(out=out, in_=src_vec.rearrange("(h q) -> q h", q=64), allow_slow_non_contiguous=True), w=[key])


def phase_rwkv(p, C, io, n_ctx, n_tok):
    SMT, YT, RK = io["SMT"], io["YTR"], io["RK"]
    order = chunk_order(n_ctx, n_tok)
    nt = n_tok // 128
    with p.phase():
        w2 = p.sb("rw2", [64, 2, 512], F32)
        a2 = p.sb("ra2", [64, 2, 512], F32)
        for d in range(2):
            p.dma("sp", lambda e, d=d: e.dma_start(out=w2[:, d, :], in_=io["rw_w2"][d]), w=["rww"])
            p.dma("sp", lambda e, d=d: e.dma_start(out=a2[:, d, :], in_=io["rw_a2"][d]), w=["rww"])
        w0 = p.sb("rw0", [64, 2, 8], F32)
        a0 = p.sb("ra0", [64, 2, 8], F32)
        for d in range(2):
            colload(p, w0, "rww", io["rw_w0"][d], 8, d)
            colload(p, a0, "rww", io["rw_a0"][d], 8, d)
        kkc = p.sb("rkk", [64, 8], F32)
        kac = p.sb("rka", [64, 8], F32)
        kam = p.sb("rkam", [64, 8], F32)
        rkc = p.sb("rrk", [64, 8], F32)
        colload(p, kkc, "rww", io["rw_k_k"], 8)
        colload(p, kac, "rww", io["rw_k_a"], 8)
        colload(p, rkc, "rww", io["rw_r_k"].rearrange("h n -> (h n)"), 8)
        p.op("dve", lambda e: e.tensor_scalar(out=kam[:], in0=kac[:], scalar1=-1.0, scalar2=1.0, op0=ALU.mult, op1=ALU.add), r=["rww"], w=["rkam"])
        T = [[p.sb(f"T{d}{h}", [64, 64], F32) for h in range(8)] for d in range(2)]
        for d in range(2):
            for h in range(8):
                p.op("pool", lambda e, t=T[d][h]: e.memset(t[:], 0.0), w=[f"T{d}{h}"])
        wa = Ring(p, "wa", [64, 2, 128], n=3)
        rkv = Ring(p, "rkv", [64, 3, 128], n=9)
        f64 = {nm: Ring(p, nm, [64, 128], n=9) for nm in
               ("sgw", "alp", "kq", "sqk", "nrm", "kk", "tmk", "kd", "eW", "eWi", "eWx", "cx", "rt", "kt", "bt", "at", "rk_", "yo")}
        tokr = {nm: Ring(p, nm, [128, 64], n=9) for nm in ("sgT", "Ktok", "Btok", "Vtok", "Xs", "Us")}
        sqm = {nm: Ring(p, nm, [128, 128], n=9) for nm in ("Akt", "Mrbt", "Mrkt")}
        sqh = [{nm: Ring(p, f"{nm}h{h_}", [128, 128], n=3) for nm in ("At", "Am", "Pt")} for h_ in range(8)]
        tw = Ring(p, "tw", [64, 64], n=9)
        psA = PsRing(p, "psR", 8)

        def mm(out_ap, pk, lhsT, rhs, r, start=True, stop=True):
            p.op("pe", lambda e: e.matmul(out_ap, lhsT=lhsT, rhs=rhs, start=start, stop=stop), r=r, w=[pk])

        def tr(out_ap, pk, in_ap, n, r):
            p.op("pe", lambda e: e.transpose(out_ap, in_ap, C["ident"][0:n, 0:n]), r=r + ["ident"], w=[pk])

        def unit(d, h, c, wat, wak):
            t0 = c * 128
            tend = 127 if d == 0 else 0
            sl = slice(t0, t0 + 128)
            hs = slice(h * 64, (h + 1) * 64)
            Tt, Tk = T[d][h], f"T{d}{h}"
            x, xk = rkv.next()
            for j in range(3):
                p.dma("sp", lambda e, x=x, j=j, h=h, sl=sl: e.dma_start(out=x[:, j, :], in_=SMT[j * 512 + h * 64:j * 512 + (h + 1) * 64, sl]), w=[xk])
            r_, k_, v_ = x[:, 0, :], x[:, 1, :], x[:, 2, :]
            yield
            ps, pk = psA.next()
            mm(ps[0:64, :], pk, w2[:, d, hs], wat[:, 0, :], ["rww", wak])
            sgw, sgwk = f64["sgw"].next()
            p.op("act", lambda e, sgw=sgw, ps=ps, d=d, h=h: e.activation(out=sgw[:], in_=ps[0:64, :], func=AF.Sigmoid, bias=w0[:, d, h:h + 1]),
                 r=[pk, "rww"], w=[sgwk])
            yield
            ps, pk = psA.next()
            mm(ps[0:64, :], pk, a2[:, d, hs], wat[:, 1, :], ["rww", wak])
            alp, alpk = f64["alp"].next()
            p.op("act", lambda e, alp=alp, ps=ps, d=d, h=h: e.activation(out=alp[:], in_=ps[0:64, :], func=AF.Sigmoid, bias=a0[:, d, h:h + 1]),
                 r=[pk, "rww"], w=[alpk])
            kq, kqk = f64["kq"].next()
            p.op("dve", lambda e, kq=kq, k_=k_, h=h: e.tensor_scalar(out=kq[:], in0=k_, scalar1=kkc[:, h:h + 1], scalar2=None, op0=ALU.mult),
                 r=[xk, "rww"], w=[kqk])
            sqk, sqkk = f64["sqk"].next()
            p.op("act", lambda e, sqk=sqk, kq=kq: e.activation(out=sqk[:], in_=kq[:], func=AF.Square), r=[kqk], w=[sqkk])
            yield
            ps, pk = psA.next()
            mm(ps[0:64, :], pk, C["ones64"][0:64, 0:64], sqk[:], ["masks", sqkk])
            nrm, nrmk = f64["nrm"].next()
            p.op("act", lambda e, nrm=nrm, ps=ps: e.activation(out=nrm[:], in_=ps[0:64, :], func=AF.Sqrt), r=[pk], w=[nrmk])
            p.op("dve", lambda e, nrm=nrm: e.tensor_scalar(out=nrm[:], in0=nrm[:], scalar1=1e-12, scalar2=None, op0=ALU.max), r=[nrmk], w=[nrmk])
            p.op("dve", lambda e, nrm=nrm: e.reciprocal(out=nrm[:], in_=nrm[:]), r=[nrmk], w=[nrmk])
            kk, kkk = f64["kk"].next()
            p.op("dve", lambda e, kk=kk, kq=kq, nrm=nrm: e.tensor_tensor(out=kk[:], in0=kq[:], in1=nrm[:], op=ALU.mult), r=[kqk, nrmk], w=[kkk])
            tmk, tmkk = f64["tmk"].next()
            p.op("dve", lambda e, tmk=tmk, alp=alp, h=h: e.tensor_scalar(out=tmk[:], in0=alp[:], scalar1=kac[:, h:h + 1], scalar2=kam[:, h:h + 1],
                                                                        op0=ALU.mult, op1=ALU.add), r=[alpk, "rww", "rkam"], w=[tmkk])
            kd, kdk = f64["kd"].next()
            p.op("dve", lambda e, kd=kd, tmk=tmk, k_=k_: e.tensor_tensor(out=kd[:], in0=tmk[:], in1=k_, op=ALU.mult), r=[tmkk, xk], w=[kdk])
            rk_, rkk = f64["rk_"].next()
            p.op("dve", lambda e, rk_=rk_, r_=r_, kd=kd, h=h: e.scalar_tensor_tensor(out=rk_[:], in0=r_, scalar=rkc[:, h:h + 1], in1=kd[:],
                                                                                     op0=ALU.mult, op1=ALU.mult), r=[xk, kdk, "rww"], w=[rkk])
            p.dma("sp", lambda e, rk_=rk_, d=d, hs=hs, sl=sl: e.dma_start(out=RK[d, hs, sl], in_=rk_[:]), r=[rkk], w=["RK"])
            yield
            ps, pk = psA.next()
            tr(ps[:, 0:64], pk, sgw[:], 64, [sgwk])
            sgT, sgTk = tokr["sgT"].next()
            p.op("dve", lambda e, sgT=sgT, ps=ps: e.tensor_copy(out=sgT[:], in_=ps[:, 0:64]), r=[pk], w=[sgTk])
            yield
            ps, pk = psA.next()
            mm(ps[0:64, :], pk, sgT[:], C[f"incl{d}"][:], [sgTk, "masks"])
            eW, eWk = f64["eW"].next()
            eWi, eWik = f64["eWi"].next()
            cx, cxk = f64["cx"].next()
            eWx, eWxk = f64["eWx"].next()
            p.op("act", lambda e, eW=eW, ps=ps: e.activation(out=eW[:], in_=ps[0:64, :], func=AF.Exp, scale=-NLW), r=[pk], w=[eWk])
            p.op("act", lambda e, eWi=eWi, ps=ps: e.activation(out=eWi[:], in_=ps[0:64, :], func=AF.Exp, scale=NLW), r=[pk], w=[eWik])
            p.op("dve", lambda e, cx=cx, ps=ps, sgw=sgw: e.tensor_tensor(out=cx[:], in0=ps[0:64, :], in1=sgw[:], op=ALU.subtract), r=[pk, sgwk], w=[cxk])
            p.op("act", lambda e, eWx=eWx, cx=cx: e.activation(out=eWx[:], in_=cx[:], func=AF.Exp, scale=-NLW), r=[cxk], w=[eWxk])
            rt, rtk = f64["rt"].next()
            kt, ktk = f64["kt"].next()
            bt, btk = f64["bt"].next()
            at, atk = f64["at"].next()
            p.op("dve", lambda e, rt=rt, r_=r_, eW=eW: e.tensor_tensor(out=rt[:], in0=r_, in1=eW[:], op=ALU.mult), r=[xk, eWk], w=[rtk])
            p.op("pool", lambda e, kt=kt, kd=kd, eWi=eWi: e.tensor_tensor(out=kt[:], in0=kd[:], in1=eWi[:], op=ALU.mult), r=[kdk, eWik], w=[ktk])
            p.op("dve", lambda e, bt=bt, kk=kk, alp=alp: e.tensor_tensor(out=bt[:], in0=kk[:], in1=alp[:], op=ALU.mult), r=[kkk, alpk], w=[btk])
            p.op("dve", lambda e, bt=bt, eWi=eWi: e.tensor_tensor(out=bt[:], in0=bt[:], in1=eWi[:], op=ALU.mult), r=[btk, eWik], w=[btk])
            p.op("dve", lambda e, at=at, kk=kk, eWx=eWx: e.scalar_tensor_tensor(out=at[:], in0=kk[:], scalar=-1.0, in1=eWx[:], op0=ALU.mult, op1=ALU.mult),
                 r=[kkk, eWxk], w=[atk])
            toks = {}
            for nm, src, sk in (("Ktok", kt[:], ktk), ("Btok", bt[:], btk), ("Vtok", v_, xk)):
                yield
                ps, pk = psA.next()
                tr(ps[:, 0:64], pk, src, 64, [sk])
                tt, ttk = tokr[nm].next()
                p.op("act" if nm != "Vtok" else "dve", (lambda e, tt=tt, ps=ps: e.copy(out=tt[:], in_=ps[:, 0:64])) if nm != "Vtok" else
                     (lambda e, tt=tt, ps=ps: e.tensor_copy(out=tt[:], in_=ps[:, 0:64])), r=[pk], w=[ttk])
                toks[nm] = (tt, ttk)
            Ktok, Ktokk = toks["Ktok"]
            Btok, Btokk = toks["Btok"]
            Vtok, Vtokk = toks["Vtok"]
            def gram(nm, lhsT, lk, rhs, rk2, mask):
                ps, pk = psA.next()
                mm(ps[:, :], pk, lhsT, rhs, [lk, rk2])
                m, mk = (sqh[h][nm] if nm in sqh[h] else sqm[nm]).next()
                p.op("dve", lambda e: e.tensor_tensor(out=m[:], in0=ps[:, :], in1=C[mask][:], op=ALU.mult), r=[pk, "masks"], w=[mk])
                return m, mk
            At, Atk = gram("At", bt[:], btk, at[:], atk, f"strict{d}")
            yield
            Am, Amk = gram("Am", at[:], atk, bt[:], btk, f"strict{1 - d}")
            yield
            Akt, Aktk = gram("Akt", kt[:], ktk, at[:], atk, f"strict{d}")
            yield
            Mrbt, Mrbtk = gram("Mrbt", bt[:], btk, rt[:], rtk, f"incl{d}")
            yield
            Mrkt, Mrktk = gram("Mrkt", kt[:], ktk, rt[:], rtk, f"incl{d}")
            yield
            Pt, Ptk = sqh[h]["Pt"].next()
            p.op("pool", lambda e, Pt=Pt, At=At: e.tensor_tensor(out=Pt[:], in0=At[:], in1=C["ident"][:], op=ALU.add), r=[Atk, "ident"], w=[Ptk])
            for step in range(6):
                yield
                ps, pk = psA.next()
                mm(ps[:, :], pk, At[:], Am[:], [Atk, Amk])
                Am2, Am2k = sqh[h]["Am"].next()
                p.op("act", lambda e, Am2=Am2, ps=ps: e.copy(out=Am2[:], in_=ps[:, :]), r=[pk], w=[Am2k])
                if step < 5:
                    yield
                    ps, pk = psA.next()
                    mm(ps[:, :], pk, Am[:], At[:], [Atk, Amk])
                    At2, At2k = sqh[h]["At"].next()
                    p.op("dve", lambda e, At2=At2, ps=ps: e.tensor_copy(out=At2[:], in_=ps[:, :]), r=[pk], w=[At2k])
                yield
                ps, pk = psA.next()
                mm(ps[:, :], pk, Am2[:], Pt[:], [Am2k, Ptk])
                Pt2, Pt2k = sqh[h]["Pt"].next()
                p.op("dve", lambda e, Pt2=Pt2, ps=ps, Pt=Pt: e.tensor_tensor(out=Pt2[:], in0=ps[:, :], in1=Pt[:], op=ALU.add), r=[pk, Ptk], w=[Pt2k])
                Pt, Ptk = Pt2, Pt2k
                Am, Amk = Am2, Am2k
                if step < 5:
                    At, Atk = At2, At2k
            yield
            ps, pk = psA.next()
            mm(ps[:, 0:64], pk, at[:], Tt[:], [atk, Tk], start=True, stop=False)
            mm(ps[:, 0:64], pk, Akt[:], Vtok[:], [Aktk, Vtokk], start=False, stop=True)
            Xs, Xsk = tokr["Xs"].next()
            p.op("act", lambda e, Xs=Xs, ps=ps: e.copy(out=Xs[:], in_=ps[:, 0:64]), r=[pk], w=[Xsk])
            yield
            ps, pk = psA.next()
            mm(ps[:, 0:64], pk, Pt[:], Xs[:], [Ptk, Xsk])
            Us, Usk = tokr["Us"].next()
            p.op("dve", lambda e, Us=Us, ps=ps: e.tensor_copy(out=Us[:], in_=ps[:, 0:64]), r=[pk], w=[Usk])
            yield
            ps, pk = psA.next()
            mm(ps[0:64, :], pk, Tt[:], rt[:], [Tk, rtk], start=True, stop=False)
            mm(ps[0:64, :], pk, Us[:], Mrbt[:], [Usk, Mrbtk], start=False, stop=False)
            mm(ps[0:64, :], pk, Vtok[:], Mrkt[:], [Vtokk, Mrktk], start=False, stop=True)
            yo, yok = f64["yo"].next()
            p.op("act", lambda e, yo=yo, ps=ps: e.copy(out=yo[:], in_=ps[0:64, :]), r=[pk], w=[yok])
            p.dma("sp", lambda e, yo=yo, d=d, hs=hs, sl=sl: e.dma_start(out=YT[d, hs, sl], in_=yo[:]), r=[yok], w=["YTR"])
            yield
            ps, pk = psA.next()
            mm(ps[0:64, 0:64], pk, Btok[:], Us[:], [Btokk, Usk], start=True, stop=False)
            mm(ps[0:64, 0:64], pk, Ktok[:], Vtok[:], [Ktokk, Vtokk], start=False, stop=True)
            t2, t2k = tw.next()
            p.op("dve", lambda e, t2=t2, Tt=Tt, eW=eW, tend=tend: e.tensor_scalar(out=t2[:], in0=Tt[:], scalar1=eW[:, tend:tend + 1], scalar2=None, op0=ALU.mult),
                 r=[Tk, eWk], w=[t2k])
            p.op("dve", lambda e, t2=t2, Tt=Tt, eW=eW, ps=ps, tend=tend: e.scalar_tensor_tensor(
                out=Tt[:], in0=ps[0:64, 0:64], scalar=eW[:, tend:tend + 1], in1=t2[:], op0=ALU.mult, op1=ALU.add), r=[pk, eWk, t2k], w=[Tk])


        def drive(gens):
            gens = list(gens)
            while gens:
                for g_ in list(gens):
                    try:
                        next(g_)
                    except StopIteration:
                        gens.remove(g_)

        for i in range(nt):
            for d in range(2):
                c = order[d][i]
                t0 = c * 128
                tend = 127 if d == 0 else 0
                sl = slice(t0, t0 + 128)
                wat, wak = wa.next()
                p.dma("sp", lambda e, wat=wat, sl=sl: e.dma_start(out=wat[:, 0, :], in_=SMT[1536:1600, sl]), w=[wak])
                p.dma("sp", lambda e, wat=wat, sl=sl: e.dma_start(out=wat[:, 1, :], in_=SMT[1600:1664, sl]), w=[wak])
                p.op("act", lambda e, wat=wat: e.activation(out=wat[:, 0, :], in_=wat[:, 0, :], func=AF.Tanh), r=[wak], w=[wak])
                drive([unit(d, h, c, wat, wak) for h in range(8)])


def phase_rwkv_finish(p, C, io, n_tok):
    SMT, YT, RK, MIXT = io["SMT"], io["YTR"], io["RK"], io["MIXT"]
    with p.phase():
        g2a = p.sb("g2a", [128, 512], F32)
        g2b = p.sb("g2b", [32, 512], F32)
        p.dma("sp", lambda e: e.dma_start(out=g2a[:], in_=io["rw_g2"][0:128, :]), w=["fw"])
        p.dma("sp", lambda e: e.dma_start(out=g2b[:], in_=io["rw_g2"][128:160, :]), w=["fw"])
        lnw = p.sb("lnw", [64, 8], F32)
        lnb = p.sb("lnb", [64, 8], F32)
        colload(p, lnw, "fw", io["rw_ln_w"], 8)
        colload(p, lnb, "fw", io["rw_ln_b"], 8)
        sga = Ring(p, "sga", [128, 512], n=2)
        sgb = Ring(p, "sgb", [32, 512], n=2)
        R = {nm: Ring(p, nm, [64, 512], n=2) for nm in ("fy0", "fy1", "fr0", "fr1", "fv", "fyc", "fsq", "frs", "fbn")}
        ob = Ring(p, "fob", [64, 512], BF16, n=2)
        psF = Ring(p, "psG", [64, 512], n=4, psum=True)
        for t0 in range(0, n_tok, 512):
            N = min(512, n_tok - t0)
            sl = slice(t0, t0 + N)
            a_, ak = sga.next()
            b_, bk = sgb.next()
            p.dma("sp", lambda e, a_=a_, sl=sl, N=N: e.dma_start(out=a_[:, 0:N], in_=SMT[1664:1792, sl]), w=[ak])
            p.dma("sp", lambda e, b_=b_, sl=sl, N=N: e.dma_start(out=b_[:, 0:N], in_=SMT[1792:1824, sl]), w=[bk])
            p.op("act", lambda e, a_=a_, N=N: e.activation(out=a_[:, 0:N], in_=a_[:, 0:N], func=AF.Sigmoid), r=[ak], w=[ak])
            p.op("act", lambda e, b_=b_, N=N: e.activation(out=b_[:, 0:N], in_=b_[:, 0:N], func=AF.Sigmoid), r=[bk], w=[bk])
            for h in range(8):
                hs = slice(h * 64, (h + 1) * 64)
                y0, y0k = R["fy0"].next()
                y1, y1k = R["fy1"].next()
                r0, r0k = R["fr0"].next()
                r1, r1k = R["fr1"].next()
                v, vk = R["fv"].next()
                for (t, k_, src) in ((y0, y0k, YT[0, hs, sl]), (y1, y1k, YT[1, hs, sl]), (r0, r0k, RK[0, hs, sl]), (r1, r1k, RK[1, hs, sl]),
                                     (v, vk, SMT[1024 + h * 64:1024 + (h + 1) * 64, sl])):
                    p.dma("sp", lambda e, t=t, src=src, N=N: e.dma_start(out=t[:, 0:N], in_=src), w=[k_])
                p.op("dve", lambda e, y0=y0, y1=y1, N=N: e.tensor_tensor(out=y0[:, 0:N], in0=y0[:, 0:N], in1=y1[:, 0:N], op=ALU.add), r=[y0k, y1k], w=[y0k])
                p.op("pool", lambda e, r0=r0, r1=r1, N=N: e.tensor_tensor(out=r0[:, 0:N], in0=r0[:, 0:N], in1=r1[:, 0:N], op=ALU.add), r=[r0k, r1k], w=[r0k])
                ps, pk = psF.next()
                p.op("pe", lambda e, ps=ps, y0=y0, N=N: e.matmul(ps[:, 0:N], lhsT=C["ones64m"][0:64, 0:64], rhs=y0[:, 0:N], start=True, stop=True), r=[y0k, "masks"], w=[pk])
                yc, yck = R["fyc"].next()
                p.op("dve", lambda e, yc=yc, y0=y0, ps=ps, N=N: e.tensor_tensor(out=yc[:, 0:N], in0=y0[:, 0:N], in1=ps[:, 0:N], op=ALU.subtract), r=[y0k, pk], w=[yck])
                sq, sqk = R["fsq"].next()
                p.op("act", lambda e, sq=sq, yc=yc, N=N: e.activation(out=sq[:, 0:N], in_=yc[:, 0:N], func=AF.Square), r=[yck], w=[sqk])
                ps, pk = psF.next()
                p.op("pe", lambda e, ps=ps, sq=sq, N=N: e.matmul(ps[:, 0:N], lhsT=C["ones64m"][0:64, 0:64], rhs=sq[:, 0:N], start=True, stop=True), r=[sqk, "masks"], w=[pk])
                rs, rsk = R["frs"].next()
                p.op("dve", lambda e, rs=rs, ps=ps, N=N: e.tensor_scalar(out=rs[:, 0:N], in0=ps[:, 0:N], scalar1=64e-5, scalar2=None, op0=ALU.add), r=[pk], w=[rsk])
                p.op("act", lambda e, rs=rs, N=N: e.activation(out=rs[:, 0:N], in_=rs[:, 0:N], func=AF.Sqrt), r=[rsk], w=[rsk])
                p.op("dve", lambda e, rs=rs, N=N: e.reciprocal(out=rs[:, 0:N], in_=rs[:, 0:N]), r=[rsk], w=[rsk])
                p.op("dve", lambda e, yc=yc, rs=rs, N=N: e.tensor_tensor(out=yc[:, 0:N], in0=yc[:, 0:N], in1=rs[:, 0:N], op=ALU.mult), r=[yck, rsk], w=[yck])
                p.op("dve", lambda e, yc=yc, h=h, N=N: e.tensor_scalar(out=yc[:, 0:N], in0=yc[:, 0:N], scalar1=lnw[:, h:h + 1], scalar2=lnb[:, h:h + 1],
                                                                      op0=ALU.mult, op1=ALU.add), r=[yck, "fw"], w=[yck])
                ps, pk = psF.next()
                p.op("pe", lambda e, ps=ps, r0=r0, N=N: e.matmul(ps[:, 0:N], lhsT=C["ones64"][0:64, 0:64], rhs=r0[:, 0:N], start=True, stop=True), r=[r0k, "masks"], w=[pk])
                bn, bnk = R["fbn"].next()
                p.op("dve", lambda e, bn=bn, ps=ps, v=v, N=N: e.tensor_tensor(out=bn[:, 0:N], in0=ps[:, 0:N], in1=v[:, 0:N], op=ALU.mult), r=[pk, vk], w=[bnk])
                p.op("pool", lambda e, bn=bn, yc=yc, N=N: e.tensor_tensor(out=bn[:, 0:N], in0=bn[:, 0:N], in1=yc[:, 0:N], op=ALU.add), r=[bnk, yck], w=[bnk])
                ps, pk = psF.next()
                p.op("pe", lambda e, ps=ps, a_=a_, hs=hs, N=N: e.matmul(ps[:, 0:N], lhsT=g2a[:, hs], rhs=a_[:, 0:N], start=True, stop=False), r=["fw", ak], w=[pk])
                p.op("pe", lambda e, ps=ps, b_=b_, hs=hs, N=N: e.matmul(ps[:, 0:N], lhsT=g2b[:, hs], rhs=b_[:, 0:N], start=False, stop=True), r=["fw", bk], w=[pk])
                o, ok = ob.next()
                p.op("dve", lambda e, o=o, bn=bn, ps=ps, N=N: e.tensor_tensor(out=o[:, 0:N], in0=ps[:, 0:N], in1=bn[:, 0:N], op=ALU.mult), r=[pk, bnk], w=[ok])
                p.dma("sp", lambda e, o=o, h=h, sl=sl, N=N: e.dma_start(out=MIXT[512 + h * 64:512 + (h + 1) * 64, sl], in_=o[:, 0:N]), r=[ok], w=["MIXT"])


def phase_outproj(p, C, io, l, w_out, n_ctx, n_tok, t_lo=0):
    xres, MIXT, modrow = io["xres"], io["MIXT"], io["modrow"]
    with p.phase():
        wbf = p.sb("woutbf", [128, KC, D], BF16)
        stage = [p.sb(f"wostage{i}", [128, D], F32) for i in range(2)]
        load_cast_weight(p, wbf, "woutbf", w_out, KC, D, stage, "wostage")
        gt = p.sb("gt1row", [128, 2, D], F32)
        for r in range(2):
            p.dma("sp", lambda e, r=r: e.dma_start(out=gt[:, r, :], in_=modrow[l, r, 2048:3072].partition_broadcast(128)), w=["gt1row"])
        mx = Ring(p, "mx", [128, KC, 128], BF16, n=2)
        xt = Ring(p, "oxt", [128, D], n=2)
        tmp = Ring(p, "otmp", [128, D], n=2)
        pso = Ring(p, "pso", [128, 512], n=4, psum=True)
        for ti in range(t_lo // 128, n_tok // 128):
            r = 1 if ti * 128 < n_ctx else 0
            sl = slice(ti * 128, (ti + 1) * 128)
            m, mk = mx.next()
            p.dma("sp", lambda e, m=m, sl=sl: e.dma_start(out=m[:], in_=MIXT[:, sl].rearrange("(c q) t -> q c t", q=128)), w=[mk])
            x, xk = xt.next()
            p.dma("sp", lambda e, x=x, sl=sl: e.dma_start(out=x[:], in_=xres[sl, :]), w=[xk])
            t, tk = tmp.next()
            for n in range(2):
                ps, pk = pso.next()
                for kc in range(KC):
                    p.op("pe", lambda e, ps=ps, m=m, kc=kc, n=n: e.matmul(ps[:], lhsT=m[:, kc, :], rhs=wbf[:, kc, n * 512:(n + 1) * 512],
                                                                          start=(kc == 0), stop=(kc == KC - 1)), r=[mk, "woutbf"], w=[pk])
                p.op("dve", lambda e, t=t, ps=ps, n=n, r=r: e.tensor_tensor(out=t[:, n * 512:(n + 1) * 512], in0=ps[:], in1=gt[:, r, n * 512:(n + 1) * 512], op=ALU.mult),
                     r=[pk, "gt1row"], w=[tk])
            p.op("pool", lambda e, t=t, x=x: e.tensor_tensor(out=t[:], in0=t[:], in1=x[:], op=ALU.add), r=[tk, xk], w=[tk])
            p.dma("sp", lambda e, t=t, sl=sl: e.dma_start(out=xres[sl, :], in_=t[:]), r=[tk], w=["xres"])


def phase_final(p, C, io, n_ctx, n_tok):
    xres, out = io["xres"], io["out"]
    with p.phase():
        g = p.sb("fng", [128, D], F32)
        p.dma("sp", lambda e: e.dma_start(out=g[:], in_=io["final_norm_g"].partition_broadcast(128)), w=["fng"])
        xt = Ring(p, "fxt", [128, D], n=3)
        junk = p.sb("fjunk", [128, D], BF16)
        ss = Ring(p, "fss", [128, 1], n=3)
        rs = Ring(p, "frst", [128, 1], n=3)
        for ti in range(n_ctx // 128, n_tok // 128):
            sl = slice(ti * 128, (ti + 1) * 128)
            x, xk = xt.next()
            s, sk = ss.next()
            r_, rk = rs.next()
            p.dma("sp", lambda e, x=x, sl=sl: e.dma_start(out=x[:], in_=xres[sl, :]), w=[xk])
            p.op("act", lambda e, x=x, s=s: e.activation(out=junk[:], in_=x[:], func=AF.Square, accum_out=s[:, 0:1]), r=[xk], w=["fjunk", sk])
            p.op("dve", lambda e, r_=r_, s=s: e.tensor_scalar(out=r_[:], in0=s[:], scalar1=1.0 / D, scalar2=EPS, op0=ALU.mult, op1=ALU.add), r=[sk], w=[rk])
            p.op("act", lambda e, r_=r_: e.activation(out=r_[:], in_=r_[:], func=AF.Sqrt), r=[rk], w=[rk])
            p.op("dve", lambda e, r_=r_: e.reciprocal(out=r_[:], in_=r_[:]), r=[rk], w=[rk])
            p.op("dve", lambda e, x=x, r_=r_: e.scalar_tensor_tensor(out=x[:], in0=x[:], scalar=r_[:, 0:1], in1=g[:], op0=ALU.mult, op1=ALU.mult),
                 r=[xk, rk, "fng"], w=[xk])
            o0 = ti * 128 - n_ctx
            p.dma("sp", lambda e, x=x, o0=o0: e.dma_start(out=out[o0:o0 + 128, :], in_=x[:]), r=[xk], w=["out"])

RST = 9

DE = 1024
SW_LIMIT = 7.0
SW_ALPHA = 1.702


def phase_moe_cast(p, C, io, l, NE):
    gu, dn, WGU, WDN = io["moe_gu_w"], io["moe_down_w"], io["WGU"], io["WDN"]
    with p.phase():
        sg = Ring(p, "cg32", [128, 2, 2048], n=2)
        sb = Ring(p, "cg16", [128, 2, 2048], BF16, n=2)
        sd = Ring(p, "cd32", [128, 2, 1024], n=2)
        sdb = Ring(p, "cd16", [128, 2, 1024], BF16, n=2)
        engs = ("dve", "pool", "act")
        it = 0
        for e_ in range(NE):
            for k2 in range(4):
                for (src, dst, r32, r16) in ((gu, WGU, sg, sb), (dn, WDN, sd, sdb)):
                    a, ak = r32.next()
                    b, bk = r16.next()
                    rows = slice(k2 * 256, (k2 + 1) * 256)
                    p.dma("sp", lambda e, a=a, src=src, e_=e_, rows=rows: e.dma_start(out=a[:], in_=src[l, e_, rows, :].rearrange("(c q) n -> q c n", q=128)), w=[ak])
                    eng = engs[it % 3]
                    it += 1
                    if eng == "act":
                        p.op("act", lambda e, a=a, b=b: e.copy(out=b[:], in_=a[:]), r=[ak], w=[bk])
                    else:
                        p.op(eng, lambda e, a=a, b=b: e.tensor_copy(out=b[:], in_=a[:]), r=[ak], w=[bk])
                    p.dma("act", lambda e, b=b, dst=dst, e_=e_, rows=rows: e.dma_start(out=dst[e_, rows, :].rearrange("(c q) n -> q c n", q=128), in_=b[:]), r=[bk], w=["WBF"])


def phase_moe_route(p, C, io, G, l, NE, t_lo, t_hi, n_ctx):
    xres, HT, GATE = io["xres"], io["HT"], io["GATE"]
    with p.phase():
        T = norm_scratch(p)
        wr = p.sb("wr", [128, KC, NE], F32)
        p.dma("sp", lambda e: e.dma_start(out=wr[:], in_=io["router_w"][l].rearrange("(c q) n -> q c n", q=128)), w=["wr"])
        br = p.sb("br", [128, NE], F32)
        p.dma("sp", lambda e: e.dma_start(out=br[:], in_=io["router_b"][l].partition_broadcast(128)), w=["wr"])
        xt = Ring(p, "mxt", [128, D], n=2)
        hb = Ring(p, "mhb", [128, KC, 128], BF16, n=2)
        h32 = Ring(p, "mh32", [128, KC, 128], n=2)
        lg = Ring(p, "mlg", [128, NE], n=2)
        ex = Ring(p, "mex", [128, NE], n=2)
        mk = Ring(p, "mmk", [128, NE], n=2)
        m8 = Ring(p, "mm8", [128, 8], n=2)
        sc = Ring(p, "msc", [128, 2], n=2)
        psr = p.ps("psr", [128, NE], F32)
        for ti in range(t_lo // 128, t_hi // 128):
            r = 1 if ti * 128 < n_ctx else 0
            sl = slice(ti * 128, (ti + 1) * 128)
            x, xk = xt.next()
            p.dma("sp", lambda e, x=x, sl=sl: e.dma_start(out=x[:], in_=xres[sl, :]), w=[xk])
            b, bk = hb.next()
            f, fk = h32.next()
            norm_tile(p, C, T, xk, x, G["A2"][:, l, r, :], G["B2"][:, l, r, :], lambda kc, b=b: b[:, kc, :], bk,
                      hT32_dst=lambda kc, f=f: f[:, kc, :])
            fk32 = bk + "32"
            p.dma("sp", lambda e, b=b, sl=sl: e.dma_start(out=HT[:, sl].rearrange("(c q) t -> q c t", q=128), in_=b[:]), r=[bk], w=["HT"])
            if RST < 2:
                continue
            for kc in range(KC):
                p.op("pe", lambda e, f=f, kc=kc: e.matmul(psr[:], lhsT=f[:, kc, :], rhs=wr[:, kc, :], start=(kc == 0), stop=(kc == KC - 1)),
                     r=[fk32, "wr"], w=["psr"])
            g, gk = lg.next()
            p.op("dve", lambda e, g=g: e.tensor_tensor(out=g[:], in0=psr[:], in1=br[:], op=ALU.add), r=["psr", "wr"], w=[gk])
            if RST < 3:
                continue
            m, mk8 = m8.next()
            p.op("dve", lambda e, m=m, g=g: e.max(out=m[:], in_=g[:]), r=[gk], w=[mk8])
            msk, mskk = mk.next()
            p.op("dve", lambda e, msk=msk, g=g, m=m: e.tensor_scalar(out=msk[:], in0=g[:], scalar1=m[:, 3:4], scalar2=None, op0=ALU.is_ge), r=[gk, mk8], w=[mskk])
            if RST < 4:
                continue
            s, sk = sc.next()
            p.op("dve", lambda e, s=s, m=m: e.tensor_scalar(out=s[:, 0:1], in0=m[:, 0:1], scalar1=-1.0, scalar2=None, op0=ALU.mult), r=[mk8], w=[sk])
            x_, xk_ = ex.next()
            p.op("act", lambda e, x_=x_, g=g, s=s: e.activation(out=x_[:], in_=g[:], func=AF.Exp, bias=s[:, 0:1]), r=[gk, sk], w=[xk_])
            p.op("dve", lambda e, x_=x_, msk=msk: e.tensor_tensor(out=x_[:], in0=x_[:], in1=msk[:], op=ALU.mult), r=[xk_, mskk], w=[xk_])
            p.op("dve", lambda e, s=s, x_=x_: e.reduce_sum(out=s[:, 1:2], in_=x_[:], axis=AX.X), r=[xk_], w=[sk])
            p.op("dve", lambda e, s=s: e.reciprocal(out=s[:, 1:2], in_=s[:, 1:2]), r=[sk], w=[sk])
            p.op("dve", lambda e, x_=x_, s=s: e.tensor_scalar(out=x_[:], in0=x_[:], scalar1=s[:, 1:2], scalar2=None, op0=ALU.mult), r=[xk_, sk], w=[xk_])
            p.dma("sp", lambda e, x_=x_, sl=sl: e.dma_start(out=GATE[sl, :], in_=x_[:]), r=[xk_], w=["GATE"])


def phase_moe_experts(p, C, io, l, NE, t_lo, t_hi, n_ctx, TB=512):
    xres, HT, GATE, WGU, WDN, modrow = io["xres"], io["HT"], io["GATE"], io["WGU"], io["WDN"], io["modrow"]
    with p.phase():
        bg_rows = p.sb("bg_rows", [NE, 2048], F32)
        p.dma("sp", lambda e: e.dma_start(out=bg_rows[:], in_=io["moe_gu_b"][l]), w=["bg_rows"])
        bgc = p.sb("bgc", [128, 16, NE], F32)
        pst = p.ps("pst", [128, 16, NE], F32)
        for c in range(16):
            p.op("pe", lambda e, c=c: e.transpose(pst[:, c, :], bg_rows[:, c * 128:(c + 1) * 128], C["ident"][0:NE, 0:NE]), r=["bg_rows", "ident"], w=["pst"])
        p.op("dve", lambda e: e.tensor_copy(out=bgc[:], in_=pst[:]), r=["pst"], w=["bgc"])
        ones_b = p.sb("ones_b", [1, 128], BF16)
        p.op("dve", lambda e: e.memset(ones_b[:], 1.0), w=["ones_b"])
        gt = p.sb("gt2row", [128, 2, D], F32)
        for r in range(2):
            p.dma("sp", lambda e, r=r: e.dma_start(out=gt[:, r, :], in_=modrow[l, r, 5120:6144].partition_broadcast(128)), w=["gt2row"])
        wg = Ring(p, "wg", [128, KC, 2048], BF16, n=2)
        wd = Ring(p, "wd", [128, KC, 1024], BF16, n=2)
        bd32 = Ring(p, "bd32", [1, 1024], F32, n=2)
        bd16 = Ring(p, "bd16", [1, 1024], BF16, n=2)
        acc = p.sb("macc", [128, TB // 128, D], F32)
        hT = p.sb("mhT", [128, KC, TB], BF16)
        gate = p.sb("mgate", [128, TB // 128, NE], F32)
        actT = Ring(p, "actT", [128, KC, 512], BF16, n=2)
        t1 = Ring(p, "et1", [128, 512], n=2)
        sgm = Ring(p, "esg", [128, 512], n=2)
        t2 = Ring(p, "et2", [128, 512], n=2)
        xt = Ring(p, "ext", [128, D], n=2)
        psg = Ring(p, "psg", [128, 512], n=4, psum=True)
        psd = Ring(p, "psd", [128, 512], n=3, psum=True)
        t = t_lo
        while t < t_hi:
            nb = min(TB, t_hi - t)
            ntile = nb // 128
            p.dma("sp", lambda e, t=t, nb=nb: e.dma_start(out=hT[:, :, 0:nb], in_=HT[:, t:t + nb].rearrange("(c q) n -> q c n", q=128)), w=["mhT"])
            p.dma("sp", lambda e, t=t, nb=nb, ntile=ntile: e.dma_start(out=gate[:, 0:ntile, :], in_=GATE[t:t + nb, :].rearrange("(j q) n -> q j n", q=128)), w=["mgate"])
            p.op("pool", lambda e: e.memset(acc[:], 0.0), w=["macc"])
            for e_ in range(NE):
                g_, gk = wg.next()
                d_, dk = wd.next()
                b32, b32k = bd32.next()
                b16, b16k = bd16.next()
                p.dma("sp", lambda e, g_=g_, e_=e_: e.dma_start(out=g_[:], in_=WGU[e_].rearrange("(c q) n -> q c n", q=128)), w=[gk])
                p.dma("act", lambda e, d_=d_, e_=e_: e.dma_start(out=d_[:], in_=WDN[e_].rearrange("(c q) n -> q c n", q=128)), w=[dk])
                p.dma("sp", lambda e, b32=b32, e_=e_: e.dma_start(out=b32[:], in_=io["moe_down_b"][l, e_:e_ + 1, :]), w=[b32k])
                p.op("pool", lambda e, b32=b32, b16=b16: e.tensor_copy(out=b16[:], in_=b32[:]), r=[b32k], w=[b16k])
                for s0 in range(0, nb, 512):
                    N = min(512, nb - s0)
                    aT, aTk = actT.next()
                    for c in range(8):
                        pa, pak = psg.next()
                        pb, pbk = psg.next()
                        for (ps_, pk_, col0) in ((pa, pak, c * 128), (pb, pbk, DE + c * 128)):
                            for kc in range(KC):
                                p.op("pe", lambda e, ps_=ps_, g_=g_, kc=kc, col0=col0, s0=s0, N=N: e.matmul(
                                    ps_[:, 0:N], lhsT=g_[:, kc, col0:col0 + 128], rhs=hT[:, kc, s0:s0 + N], start=(kc == 0), stop=(kc == KC - 1)),
                                    r=[gk, "mhT"], w=[pk_])
                        a1, a1k = t1.next()
                        p.op("dve", lambda e, a1=a1, pa=pa, c=c, e_=e_, N=N: e.tensor_scalar(out=a1[:, 0:N], in0=pa[:, 0:N], scalar1=bgc[:, c, e_:e_ + 1], scalar2=SW_LIMIT,
                                                                                          op0=ALU.add, op1=ALU.min), r=[pak, "bgc"], w=[a1k])
                        s_, sk_ = sgm.next()
                        p.op("act", lambda e, s_=s_, a1=a1, N=N: e.activation(out=s_[:, 0:N], in_=a1[:, 0:N], func=AF.Sigmoid, scale=SW_ALPHA), r=[a1k], w=[sk_])
                        a2, a2k = t2.next()
                        p.op("dve", lambda e, a2=a2, pb=pb, c=c, e_=e_, N=N: e.tensor_scalar(out=a2[:, 0:N], in0=pb[:, 0:N], scalar1=bgc[:, 8 + c, e_:e_ + 1], scalar2=SW_LIMIT,
                                                                                          op0=ALU.add, op1=ALU.min), r=[pbk, "bgc"], w=[a2k])
                        p.op("pool", lambda e, a2=a2, N=N: e.tensor_scalar(out=a2[:, 0:N], in0=a2[:, 0:N], scalar1=-SW_LIMIT, scalar2=1.0, op0=ALU.max, op1=ALU.add),
                             r=[a2k], w=[a2k])
                        p.op("pool", lambda e, a1=a1, s_=s_, N=N: e.tensor_tensor(out=a1[:, 0:N], in0=a1[:, 0:N], in1=s_[:, 0:N], op=ALU.mult), r=[a1k, sk_], w=[a1k])
                        p.op("dve", lambda e, aT=aT, a1=a1, a2=a2, c=c, N=N: e.tensor_tensor(out=aT[:, c, 0:N], in0=a1[:, 0:N], in1=a2[:, 0:N], op=ALU.mult),
                             r=[a1k, a2k], w=[aTk])
                    for j in range(N // 128):
                        tile_i = (s0 // 128) + j
                        for n in range(2):
                            pd, pdk = psd.next()
                            for kc in range(KC):
                                p.op("pe", lambda e, pd=pd, aT=aT, d_=d_, kc=kc, j=j, n=n: e.matmul(
                                    pd[:], lhsT=aT[:, kc, j * 128:(j + 1) * 128], rhs=d_[:, kc, n * 512:(n + 1) * 512], start=(kc == 0), stop=False),
                                    r=[aTk, dk], w=[pdk])
                            p.op("pe", lambda e, pd=pd, b16=b16, n=n: e.matmul(pd[:], lhsT=ones_b[:], rhs=b16[:, n * 512:(n + 1) * 512], start=False, stop=True),
                                 r=["ones_b", b16k], w=[pdk])
                            p.op("dve", lambda e, pd=pd, tile_i=tile_i, n=n, e_=e_: e.scalar_tensor_tensor(
                                out=acc[:, tile_i, n * 512:(n + 1) * 512], in0=pd[:], scalar=gate[:, tile_i, e_:e_ + 1],
                                in1=acc[:, tile_i, n * 512:(n + 1) * 512], op0=ALU.mult, op1=ALU.add), r=[pdk, "mgate", "macc"], w=["macc"])
            for j in range(ntile):
                tok = t + j * 128
                r = 1 if tok < n_ctx else 0
                x, xk = xt.next()
                p.dma("sp", lambda e, x=x, tok=tok: e.dma_start(out=x[:], in_=xres[tok:tok + 128, :]), w=[xk])
                p.op("dve", lambda e, j=j, r=r: e.tensor_tensor(out=acc[:, j, :], in0=acc[:, j, :], in1=gt[:, r, :], op=ALU.mult), r=["macc", "gt2row"], w=["macc"])
                p.op("pool", lambda e, x=x, j=j: e.tensor_tensor(out=x[:], in0=x[:], in1=acc[:, j, :], op=ALU.add), r=[xk, "macc"], w=[xk])
                p.dma("sp", lambda e, x=x, tok=tok: e.dma_start(out=xres[tok:tok + 128, :], in_=x[:]), r=[xk], w=["xres"])
            t += nb

import math

MLA_SCALE = 192 ** -0.5


def make_rope_consts(p, C):
    f, q = C["iota_f"], C["iota_p"]
    d = p.sb("rp_d", [64, 64], F32)
    e1 = p.sb("rp_e1", [64, 64], F32)
    e2 = p.sb("rp_e2", [64, 64], F32)
    ge = p.sb("rp_ge", [64, 64], F32)
    PR = p.sb("rp_PR", [64, 64], F32)
    k = ["rope_c"]
    p.op("dve", lambda e: e.tensor_scalar(out=d[:], in0=f[0:64, 0:64], scalar1=q[0:64, 0:1], scalar2=None, op0=ALU.subtract), r=["iota_f", "iota_p"], w=k)
    p.op("dve", lambda e: e.tensor_scalar(out=e1[:], in0=d[:], scalar1=16.0, scalar2=None, op0=ALU.is_equal), r=k, w=k)
    p.op("dve", lambda e: e.tensor_scalar(out=e2[:], in0=d[:], scalar1=-16.0, scalar2=None, op0=ALU.is_equal), r=k, w=k)
    ii = p.sb("rp_ii", [64, 64], I32)
    p.op("pool", lambda e: e.iota(ii[:], pattern=[[1, 64]], base=0, channel_multiplier=0), w=["rp_ii"])
    p.op("dve", lambda e: e.tensor_single_scalar(out=ii[:], in_=ii[:], scalar=16, op=ALU.bitwise_and), r=["rp_ii"], w=["rp_ii"])
    p.op("dve", lambda e: e.tensor_copy(out=ge[:], in_=ii[:]), r=["rp_ii"], w=k)
    p.op("dve", lambda e: e.tensor_scalar(out=ge[:], in0=ge[:], scalar1=1.0 / 16, scalar2=None, op0=ALU.mult), r=k, w=k)
    p.op("dve", lambda e: e.tensor_tensor(out=e1[:], in0=e1[:], in1=ge[:], op=ALU.mult), r=k, w=k)
    p.op("dve", lambda e: e.tensor_scalar(out=ge[:], in0=ge[:], scalar1=-1.0, scalar2=1.0, op0=ALU.mult, op1=ALU.add), r=k, w=k)
    p.op("dve", lambda e: e.tensor_tensor(out=e2[:], in0=e2[:], in1=ge[:], op=ALU.mult), r=k, w=k)
    p.op("dve", lambda e: e.tensor_tensor(out=PR[:], in0=e1[:], in1=e2[:], op=ALU.subtract), r=k, w=k)
    invf = p.sb("rp_invf", [64, 1], F32)
    isrow = p.sb("rp_isrow", [64, 1], F32)
    pi_ = p.sb("rp_pi", [64, 1], I32)
    p.op("pool", lambda e: e.iota(pi_[:], pattern=[[1, 1]], base=0, channel_multiplier=1), w=["rp_pi"])
    p.op("dve", lambda e: e.tensor_single_scalar(out=pi_[:], in_=pi_[:], scalar=15, op=ALU.bitwise_and), r=["rp_pi"], w=["rp_pi"])
    p.op("dve", lambda e: e.tensor_copy(out=invf[:], in_=pi_[:]), r=["rp_pi"], w=k)
    p.op("act", lambda e: e.activation(out=invf[:], in_=invf[:], func=AF.Exp, scale=-math.log(10000.0) / 16.0), r=k, w=k)
    p.op("dve", lambda e: e.tensor_scalar(out=isrow[:], in0=q[0:64, 0:1], scalar1=32.0, scalar2=None, op0=ALU.is_lt), r=["iota_p"], w=k)
    C.update(PR=PR, invf=invf, isrow=isrow)


def phase_mla_proj(p, C, io, G, l, n_ctx, n_tok):
    xres = io["xres"]
    QN, QR, KN, KR, V = io["QN"], io["QR"], io["KN"], io["KRo"], io["Vt"]
    n_lat = n_tok - n_ctx
    with p.phase():
        T = norm_scratch(p)
        win = p.sb("mwin", [128, KC, 448], BF16)
        st = [p.sb(f"mst{i}", [128, 2048], F32) for i in range(2)]
        load_cast_weight(p, win, "mwin", io["od_w_in"], KC, 448, st, "mst")
        wq = p.sb("mwq", [128, 2, 1536], BF16)
        load_cast_weight(p, wq, "mwq", io["mla_wq_up"], 2, 1536, st, "mst")
        wkn = p.sb("mwkn", [128, 8, 128], BF16)
        wv = p.sb("mwv", [128, 8, 128], BF16)
        p.dma("sp", lambda e: e.dma_start(out=st[0][:, 0:2048], in_=io["mla_wkv_up"][:, :]), w=["mst0"])
        p.op("dve", lambda e: e.tensor_copy(out=wkn[:], in_=st[0][:, 0:2048].rearrange("q (h c) -> q h c", c=256)[:, :, 0:128]), r=["mst0"], w=["mwk"])
        p.op("pool", lambda e: e.tensor_copy(out=wv[:], in_=st[0][:, 0:2048].rearrange("q (h c) -> q h c", c=256)[:, :, 128:256]), r=["mst0"], w=["mwk"])
        qg = p.sb("mqg", [128, 2], F32)
        kg = p.sb("mkg", [128, 1], F32)
        p.dma("sp", lambda e: e.dma_start(out=qg[:], in_=io["mla_q_norm"].rearrange("(c q) -> q c", q=128), allow_slow_non_contiguous=True), w=["mg"])
        p.dma("sp", lambda e: e.dma_start(out=kg[:], in_=io["mla_kv_norm"].rearrange("(q o) -> q o", o=1)), w=["mg"])
        onesq = p.sb("monesq", [128, 128], F32)
        p.op("dve", lambda e: e.memset(onesq[:], 1.0 / 256), w=["monesq"])
        xts = Ring(p, "axt", [128, D], n=2)
        hT = p.sb("ahT", [128, KC, 512], BF16)
        cq = p.sb("acq", [128, 2, 512], F32)
        ckv = p.sb("ackv", [128, 512], F32)
        krr = p.sb("akrr", [64, 512], F32)
        sq = p.sb("asq", [128, 2, 512], F32)
        rs = Ring(p, "ars", [128, 512], n=2)
        cqn = p.sb("acqn", [128, 2, 512], BF16)
        ckvn = p.sb("ackvn", [128, 512], BF16)
        ang = p.sb("aang", [64, 512], F32)
        tti = p.sb("atti", [64, 512], I32)
        tt2 = p.sb("att2", [64, 512], I32)
        a2 = p.sb("aa2", [64, 512], F32)
        kf = p.sb("akf", [64, 512], F32)
        cosT = p.sb("acos", [64, 512], F32)
        sinT = p.sb("asin", [64, 512], F32)
        rx = Ring(p, "arx", [64, 512], n=2)
        ru = Ring(p, "aru", [64, 512], n=2)
        ob = Ring(p, "aob", [128, 512], BF16, n=3)
        vb = Ring(p, "avb", [128, 1024], BF16, n=2)
        pp = Ring(p, "app", [128, 512], n=5, psum=True)

        def rope(x, xk, N, scale, dst_ap, dst_key):
            p2, p2k = pp.next()
            p.op("pe", lambda e: e.matmul(p2[0:64, 0:N], lhsT=C["PR"][:], rhs=x, start=True, stop=True), r=["rope_c", xk], w=[p2k])
            u, uk = ru.next()
            p.op("dve", lambda e: e.tensor_tensor(out=u[:, 0:N], in0=p2[0:64, 0:N], in1=sinT[:, 0:N], op=ALU.mult), r=[p2k, "trig"], w=[uk])
            p.op("pool", lambda e: e.tensor_tensor(out=x, in0=x, in1=cosT[:, 0:N], op=ALU.mult), r=[xk, "trig"], w=[xk])
            if scale == 1.0:
                p.op("dve", lambda e: e.tensor_tensor(out=dst_ap, in0=x, in1=u[:, 0:N], op=ALU.add), r=[xk, uk], w=[dst_key])
            else:
                p.op("dve", lambda e: e.tensor_tensor(out=u[:, 0:N], in0=x, in1=u[:, 0:N], op=ALU.add), r=[xk, uk], w=[uk])
                p.op("dve", lambda e: e.tensor_scalar(out=dst_ap, in0=u[:, 0:N], scalar1=scale, scalar2=None, op0=ALU.mult), r=[uk], w=[dst_key])

        blocks = [(0, n_ctx, False)] + [(t, min(512, n_tok - t), True) for t in range(n_ctx, n_tok, 512)]
        def do_block(t0, N, is_lat):
            for j in range(N // 128):
                ti = t0 // 128 + j
                r = 0 if is_lat else 1
                x, xk = xts.next()
                p.dma("sp", lambda e, x=x, ti=ti: e.dma_start(out=x[:], in_=xres[ti * 128:(ti + 1) * 128, :]), w=[xk])
                norm_tile(p, C, T, xk, x, G["A1"][:, l, r, :], G["B1"][:, l, r, :], lambda kc, j=j: hT[:, kc, j * 128:(j + 1) * 128], "ahT")
            for (c0, cw, dst, dk) in ((0, 128, cq[:, 0, :], "acq"), (128, 128, cq[:, 1, :], "acq"), (256, 128, ckv[:, :], "ackv"), (384, 64, krr[:, :], "akrr")):
                ps, pk = pp.next()
                for kc in range(KC):
                    p.op("pe", lambda e, ps=ps, kc=kc, c0=c0, cw=cw, N=N: e.matmul(ps[0:cw, 0:N], lhsT=win[:, kc, c0:c0 + cw], rhs=hT[:, kc, 0:N],
                                                                                 start=(kc == 0), stop=(kc == KC - 1)), r=["mwin", "ahT"], w=[pk])
                p.op("act", lambda e, ps=ps, dst=dst, cw=cw, N=N: e.copy(out=dst[0:cw, 0:N], in_=ps[0:cw, 0:N]), r=[pk], w=[dk])

            def rmsn(src_chunks, src_key, ones_ap, gcols, dst_chunks, dst_key):
                n = len(src_chunks)
                for c in range(n):
                    p.op("act", lambda e, c=c: e.activation(out=sq[:, c, 0:N], in_=src_chunks[c], func=AF.Square), r=[src_key], w=["asq"])
                ps, pk = pp.next()
                for c in range(n):
                    p.op("pe", lambda e, ps=ps, c=c: e.matmul(ps[:, 0:N], lhsT=ones_ap, rhs=sq[:, c, 0:N], start=(c == 0), stop=(c == n - 1)),
                         r=["asq", "monesq", "masks"], w=[pk])
                r_, rk = rs.next()
                p.op("dve", lambda e: e.tensor_scalar(out=r_[:, 0:N], in0=ps[:, 0:N], scalar1=EPS, scalar2=None, op0=ALU.add), r=[pk], w=[rk])
                p.op("act", lambda e: e.activation(out=r_[:, 0:N], in_=r_[:, 0:N], func=AF.Sqrt), r=[rk], w=[rk])
                p.op("dve", lambda e: e.reciprocal(out=r_[:, 0:N], in_=r_[:, 0:N]), r=[rk], w=[rk])
                for c in range(n):
                    p.op("dve", lambda e, c=c: e.scalar_tensor_tensor(out=dst_chunks[c], in0=src_chunks[c], scalar=gcols[c], in1=r_[:, 0:N],
                                                                      op0=ALU.mult, op1=ALU.mult), r=[src_key, rk, "mg"], w=[dst_key])

            rmsn([ckv[:, 0:N]], "ackv", C["ones128"][:], [kg[:, 0:1]], [ckvn[:, 0:N]], "ackvn")
            if is_lat:
                rmsn([cq[:, 0, 0:N], cq[:, 1, 0:N]], "acq", onesq[:], [qg[:, 0:1], qg[:, 1:2]], [cqn[:, 0, 0:N], cqn[:, 1, 0:N]], "acqn")
                lat0 = t0 - n_ctx
                p.op("pool", lambda e, lat0=lat0: e.iota(tti[:, 0:N], pattern=[[1, N]], base=lat0, channel_multiplier=0), w=["atti"])
                p.op("dve", lambda e: e.tensor_single_scalar(out=tt2[:, 0:N], in_=tti[:, 0:N], scalar=63, op=ALU.bitwise_and), r=["atti"], w=["att2"])
                p.op("dve", lambda e: e.tensor_copy(out=cosT[:, 0:N], in_=tt2[:, 0:N]), r=["att2"], w=["trig"])
                p.op("dve", lambda e: e.tensor_single_scalar(out=tt2[:, 0:N], in_=tti[:, 0:N], scalar=6, op=ALU.arith_shift_right), r=["atti", "trig"], w=["att2"])
                p.op("dve", lambda e: e.tensor_copy(out=sinT[:, 0:N], in_=tt2[:, 0:N]), r=["att2"], w=["trig"])
                p.op("dve", lambda e: e.tensor_tensor(out=sinT[:, 0:N], in0=sinT[:, 0:N], in1=cosT[:, 0:N], op=ALU.subtract), r=["trig"], w=["trig"])
                p.op("dve", lambda e: e.scalar_tensor_tensor(out=ang[:, 0:N], in0=sinT[:, 0:N], scalar=C["isrow"][:, 0:1], in1=cosT[:, 0:N], op0=ALU.mult, op1=ALU.add),
                     r=["trig", "rope_c"], w=["aang"])
                p.op("dve", lambda e: e.tensor_scalar(out=ang[:, 0:N], in0=ang[:, 0:N], scalar1=C["invf"][:, 0:1], scalar2=None, op0=ALU.mult), r=["aang", "rope_c"], w=["aang"])
                for (dst, off) in ((sinT, 0.0), (cosT, 0.5 * math.pi)):
                    p.op("dve", lambda e, off=off: e.tensor_scalar(out=a2[:, 0:N], in0=ang[:, 0:N], scalar1=off, scalar2=None, op0=ALU.add), r=["aang"], w=["aa2"])
                    p.op("dve", lambda e: e.tensor_scalar(out=kf[:, 0:N], in0=a2[:, 0:N], scalar1=1.0 / (2 * math.pi), scalar2=None, op0=ALU.mult), r=["aa2"], w=["akf"])
                    p.op("dve", lambda e: e.tensor_copy(out=tt2[:, 0:N], in_=kf[:, 0:N]), r=["akf"], w=["att2"])
                    p.op("dve", lambda e: e.tensor_copy(out=kf[:, 0:N], in_=tt2[:, 0:N]), r=["att2"], w=["akf"])
                    p.op("dve", lambda e, dst=dst: e.scalar_tensor_tensor(out=dst[:, 0:N], in0=kf[:, 0:N], scalar=-2 * math.pi, in1=a2[:, 0:N], op0=ALU.mult, op1=ALU.add),
                         r=["akf", "aa2"], w=["trig"])
                    p.op("dve", lambda e, dst=dst: e.tensor_scalar(out=dst[:, 0:N], in0=dst[:, 0:N], scalar1=-math.pi, scalar2=math.pi, op0=ALU.max, op1=ALU.min), r=["trig"], w=["trig"])
                    p.op("act", lambda e, dst=dst: e.activation(out=dst[:, 0:N], in_=dst[:, 0:N], func=AF.Sin), r=["trig"], w=["trig"])
                lsl = slice(lat0, lat0 + N)
                for h in range(8):
                    ps, pk = pp.next()
                    for kc in range(2):
                        p.op("pe", lambda e, ps=ps, kc=kc, h=h: e.matmul(ps[:, 0:N], lhsT=wq[:, kc, h * 192:h * 192 + 128], rhs=cqn[:, kc, 0:N], start=(kc == 0), stop=(kc == 1)),
                             r=["mwq", "acqn"], w=[pk])
                    o, ok = ob.next()
                    p.op("act", lambda e, o=o, ps=ps: e.activation(out=o[:, 0:N], in_=ps[:, 0:N], func=AF.Copy, scale=MLA_SCALE), r=[pk], w=[ok])
                    p.dma("sp", lambda e, o=o, h=h, lsl=lsl: e.dma_start(out=QN[h, :, lsl], in_=o[:, 0:N]), r=[ok], w=["QN"])
                    ps, pk = pp.next()
                    for kc in range(2):
                        p.op("pe", lambda e, ps=ps, kc=kc, h=h: e.matmul(ps[0:64, 0:N], lhsT=wq[:, kc, h * 192 + 128:h * 192 + 192], rhs=cqn[:, kc, 0:N], start=(kc == 0), stop=(kc == 1)),
                             r=["mwq", "acqn"], w=[pk])
                    o, ok = ob.next()
                    x_, xk_ = rx.next()
                    p.op("act", lambda e, x_=x_, ps=ps: e.copy(out=x_[:, 0:N], in_=ps[0:64, 0:N]), r=[pk], w=[xk_])
                    rope(x_[:, 0:N], xk_, N, MLA_SCALE, o[0:64, 0:N], ok)
                    p.dma("sp", lambda e, o=o, h=h, lsl=lsl: e.dma_start(out=QR[h, :, lsl], in_=o[0:64, 0:N]), r=[ok], w=["QR"])
            for h in range(8):
                ps, pk = pp.next()
                p.op("pe", lambda e, ps=ps, h=h: e.matmul(ps[:, 0:N], lhsT=wkn[:, h, :], rhs=ckvn[:, 0:N], start=True, stop=True), r=["mwk", "ackvn"], w=[pk])
                o, ok = ob.next()
                p.op("act", lambda e, o=o, ps=ps: e.copy(out=o[:, 0:N], in_=ps[:, 0:N]), r=[pk], w=[ok])
                p.dma("sp", lambda e, o=o, h=h, t0=t0, N=N: e.dma_start(out=KN[h, :, t0:t0 + N], in_=o[:, 0:N]), r=[ok], w=["KN"])
            for j in range(N // 128):
                vt, vk = vb.next()
                for g in range(2):
                    ps, pk = pp.next()
                    p.op("pe", lambda e, ps=ps, j=j, g=g: e.matmul(ps[:, :], lhsT=ckvn[:, j * 128:(j + 1) * 128], rhs=wv[:, 4 * g:4 * g + 4, :], start=True, stop=True),
                         r=["mwk", "ackvn"], w=[pk])
                    p.op("dve", lambda e, vt=vt, ps=ps, g=g: e.tensor_copy(out=vt[:, g * 512:(g + 1) * 512], in_=ps[:, :]), r=[pk], w=[vk])
                tok = t0 + j * 128
                p.dma("sp", lambda e, vt=vt, tok=tok: e.dma_start(out=V[tok:tok + 128, :], in_=vt[:]), r=[vk], w=["Vt"])
            o, ok = ob.next()
            if is_lat:
                rope(krr[:, 0:N], "akrr", N, 1.0, o[0:64, 0:N], ok)
            else:
                p.op("dve", lambda e, o=o: e.tensor_copy(out=o[0:64, 0:N], in_=krr[:, 0:N]), r=["akrr"], w=[ok])
            p.dma("sp", lambda e, o=o, t0=t0, N=N: e.dma_start(out=KR[:, t0:t0 + N], in_=o[0:64, 0:N]), r=[ok], w=["KRo"])


        for (t0_, N_, lat_) in blocks:
            do_block(t0_, N_, lat_)

def phase_mla_attn(p, C, io, n_ctx, n_tok):
    QN, QR, KN, KR, V, MIXT = io["QN"], io["QR"], io["KN"], io["KRo"], io["Vt"], io["MIXT"]
    n_lat = n_tok - n_ctx
    nkt = n_tok // 128
    with p.phase():
        kr = p.sb("bkr", [64, n_tok], BF16)
        p.dma("sp", lambda e: e.dma_start(out=kr[:], in_=KR[:, :]), w=["bkr"])
        ones_bf = p.sb("bones", [128, 128], BF16)
        p.op("dve", lambda e: e.memset(ones_bf[:], 1.0), w=["bones"])
        kn = Ring(p, "bkn", [128, n_tok], BF16, n=2)
        vv = Ring(p, "bvv", [128, nkt, 128], BF16, n=2)
        qn = Ring(p, "bqn", [128, 512], BF16, n=2)
        qr = Ring(p, "bqr", [64, 512], BF16, n=2)
        pt = Ring(p, "bpt", [128, 512], BF16, n=4)
        rd = Ring(p, "brd", [128, 512], n=2)
        ob = Ring(p, "bob", [128, 512], BF16, n=2)
        psS = Ring(p, "bpsS", [128, 512], n=4, psum=True)
        psO = Ring(p, "bpsO", [128, 512], n=2, psum=True)
        psD = Ring(p, "bpsD", [128, 512], n=2, psum=True)
        for h in range(8):
            k_, kk = kn.next()
            v_, vk = vv.next()
            p.dma("sp", lambda e, k_=k_, h=h: e.dma_start(out=k_[:], in_=KN[h, :, :]), w=[kk])
            for j0 in range(0, nkt, 12):
                j1 = min(nkt, j0 + 12)
                p.dma("act", lambda e, v_=v_, h=h, j0=j0, j1=j1: e.dma_start(
                    out=v_[:, j0:j1, :], in_=V[j0 * 128:j1 * 128, h * 128:(h + 1) * 128].rearrange("(j q) c -> q j c", q=128)), w=[vk])
            for qb in range(n_lat // 512):
                qsl = slice(qb * 512, (qb + 1) * 512)
                a, ak = qn.next()
                b, bk = qr.next()
                p.dma("sp", lambda e, a=a, h=h, qsl=qsl: e.dma_start(out=a[:], in_=QN[h, :, qsl]), w=[ak])
                p.dma("sp", lambda e, b=b, h=h, qsl=qsl: e.dma_start(out=b[:], in_=QR[h, :, qsl]), w=[bk])
                po, pok = psO.next()
                pd, pdk = psD.next()
                def S_(kt):
                    ks = slice(kt * 128, (kt + 1) * 128)
                    ps, psk = psS.next()
                    p.op("pe", lambda e, ps=ps, k_=k_, ks=ks, a=a: e.matmul(ps[:], lhsT=k_[:, ks], rhs=a[:], start=True, stop=False), r=[kk, ak], w=[psk])
                    p.op("pe", lambda e, ps=ps, ks=ks, b=b: e.matmul(ps[:], lhsT=kr[:, ks], rhs=b[:], start=False, stop=True), r=["bkr", bk], w=[psk])
                    return ps, psk
                LA = 2
                pend = [S_(kt) for kt in range(min(LA, nkt))]
                for kt in range(nkt):
                    if kt + LA < nkt:
                        pend.append(S_(kt + LA))
                    ps, psk = pend.pop(0)
                    t, tk = pt.next()
                    p.op("act", lambda e, t=t, ps=ps: e.activation(out=t[:], in_=ps[:], func=AF.Exp), r=[psk], w=[tk])
                    p.op("pe", lambda e, po=po, v_=v_, kt=kt, t=t: e.matmul(po[:], lhsT=v_[:, kt, :], rhs=t[:], start=(kt == 0), stop=(kt == nkt - 1)), r=[vk, tk], w=[pok])
                    p.op("pe", lambda e, pd=pd, t=t, kt=kt: e.matmul(pd[:], lhsT=ones_bf[:], rhs=t[:], start=(kt == 0), stop=(kt == nkt - 1)), r=["bones", tk], w=[pdk])
                r_, rk = rd.next()
                p.op("dve", lambda e, r_=r_, pd=pd: e.reciprocal(out=r_[:], in_=pd[:]), r=[pdk], w=[rk])
                o, ok = ob.next()
                p.op("dve", lambda e, o=o, po=po, r_=r_: e.tensor_tensor(out=o[:], in0=po[:], in1=r_[:], op=ALU.mult), r=[pok, rk], w=[ok])
                p.dma("sp", lambda e, o=o, h=h, qb=qb: e.dma_start(out=MIXT[h * 128:(h + 1) * 128, n_ctx + qb * 512:n_ctx + (qb + 1) * 512], in_=o[:]), r=[ok], w=["MIXT"])

CAST_IN_GATHER = 1


def moe_tables(p, NE, max_tiles, max_nb):
    RT = {}
    for nm in ("e4", "rank4", "gate4", "destf"):
        RT[nm] = p.sb("rt_" + nm, [128, max_tiles, 4], F32)
    RT["desti"] = p.sb("rt_desti", [128, max_tiles, 4], I32)
    RT["pstart"] = p.sb("rt_pstart", [128, NE], F32)
    RT["blke"] = p.sb("rt_blke", [128, max_nb], F32)
    RT["widx"] = p.sb("rt_widx", [128, max_nb, 8], I32)
    return RT


def phase_moe_sparse_route(p, C, io, G, RT, l, NE, t_lo, t_hi, n_ctx, BLK=512):
    xres, HTOK, modrow = io["xres"], io["HTOK"], io["modrow"]
    T_ = t_hi - t_lo
    ntile = T_ // 128
    NB = (4 * T_ + BLK - 1) // BLK + NE
    LOG = BLK.bit_length() - 1
    e4, rank4, gate4 = RT["e4"], RT["rank4"], RT["gate4"]
    with p.phase():
        T = norm_scratch(p)
        wr = p.sb("wr", [128, KC, NE], F32)
        p.dma("sp", lambda e: e.dma_start(out=wr[:], in_=io["router_w"][l].rearrange("(c q) n -> q c n", q=128)), w=["wr"])
        br = p.sb("br", [128, NE], F32)
        p.dma("sp", lambda e: e.dma_start(out=br[:], in_=io["router_b"][l].partition_broadcast(128)), w=["wr"])
        Arow = p.sb("sArow", [128, 2, D], F32)
        Brow = p.sb("sBrow", [128, 2, D], F32)
        grow = p.sb("sgrow", [128, D], F32)
        p.dma("sp", lambda e: e.dma_start(out=grow[:], in_=io["norm2_g"][l].partition_broadcast(128)), w=["sgrow"])
        for r in range(2):
            p.dma("sp", lambda e, r=r: e.dma_start(out=Arow[:, r, :], in_=modrow[l, r, 4096:5120].partition_broadcast(128)), w=["sArow"])
            p.dma("sp", lambda e, r=r: e.dma_start(out=Brow[:, r, :], in_=modrow[l, r, 3072:4096].partition_broadcast(128)), w=["sBrow"])
            p.op("dve", lambda e, r=r: e.scalar_tensor_tensor(out=Arow[:, r, :], in0=Arow[:, r, :], scalar=1.0, in1=grow[:], op0=ALU.add, op1=ALU.mult),
                 r=["sArow", "sgrow"], w=["sArow"])
        cnt = p.sb("scnt", [128, NE], F32)
        p.op("dve", lambda e: e.memset(cnt[:], 0.0), w=["scnt"])
        xt = Ring(p, "sxt", [128, D], n=2)
        h32 = Ring(p, "sh32", [128, KC, 128], n=2)
        ht = Ring(p, "sht", [128, D], n=2)
        hb = Ring(p, "shb", [128, D], BF16, n=2)
        sm = {nm: Ring(p, "s_" + nm, [128, NE], n=2) for nm in ("lg", "ex", "mk", "rk", "oh", "tmp")}
        m8 = Ring(p, "sm8", [128, 8], n=2)
        sc = Ring(p, "ssc", [128, 2], n=2)
        psr = p.ps("spsr", [128, NE], F32)
        ps2 = p.ps("sps2", [128, NE], F32)
        ps3 = p.ps("sps3", [128, NE], F32)
        for ti in range(ntile):
            tok = t_lo + ti * 128
            r = 1 if tok < n_ctx else 0
            x, xk = xt.next()
            p.dma("sp", lambda e, x=x, tok=tok: e.dma_start(out=x[:], in_=xres[tok:tok + 128, :]), w=[xk])
            f, fk = h32.next()
            norm_tile(p, C, T, xk, x, G["A2"][:, l, r, :], G["B2"][:, l, r, :], None, fk, hT32_dst=lambda kc, f=f: f[:, kc, :], want_bf=False)
            fk32 = fk + "32"
            a, ak = ht.next()
            b, bk = hb.next()
            p.op("dve", lambda e, a=a, r=r: e.tensor_tensor(out=a[:], in0=T["xn"][:], in1=Arow[:, r, :], op=ALU.mult), r=["xn", "sArow"], w=[ak])
            p.op("pool", lambda e, a=a, b=b, r=r: e.tensor_tensor(out=b[:], in0=a[:], in1=Brow[:, r, :], op=ALU.add), r=[ak, "sBrow"], w=[bk])
            p.dma("sp", lambda e, b=b, tok=tok: e.dma_start(out=HTOK[tok:tok + 128, :], in_=b[:]), r=[bk], w=["HTOK"])
            for kc in range(KC):
                p.op("pe", lambda e, f=f, kc=kc: e.matmul(psr[:], lhsT=f[:, kc, :], rhs=wr[:, kc, :], start=(kc == 0), stop=(kc == KC - 1)),
                     r=[fk32, "wr"], w=["spsr"])
            g, gk = sm["lg"].next()
            p.op("dve", lambda e, g=g: e.tensor_tensor(out=g[:], in0=psr[:], in1=br[:], op=ALU.add), r=["spsr", "wr"], w=[gk])
            m, mk8 = m8.next()
            p.op("dve", lambda e, m=m, g=g: e.max(out=m[:], in_=g[:]), r=[gk], w=[mk8])
            msk, mskk = sm["mk"].next()
            p.op("dve", lambda e, msk=msk, g=g, m=m: e.tensor_scalar(out=msk[:], in0=g[:], scalar1=m[:, 3:4], scalar2=None, op0=ALU.is_ge), r=[gk, mk8], w=[mskk])
            s, sk = sc.next()
            p.op("dve", lambda e, s=s, m=m: e.tensor_scalar(out=s[:, 0:1], in0=m[:, 0:1], scalar1=-1.0, scalar2=None, op0=ALU.mult), r=[mk8], w=[sk])
            x_, xk_ = sm["ex"].next()
            p.op("act", lambda e, x_=x_, g=g, s=s: e.activation(out=x_[:], in_=g[:], func=AF.Exp, bias=s[:, 0:1]), r=[gk, sk], w=[xk_])
            p.op("dve", lambda e, x_=x_, msk=msk: e.tensor_tensor(out=x_[:], in0=x_[:], in1=msk[:], op=ALU.mult), r=[xk_, mskk], w=[xk_])
            p.op("dve", lambda e, s=s, x_=x_: e.reduce_sum(out=s[:, 1:2], in_=x_[:], axis=AX.X), r=[xk_], w=[sk])
            p.op("dve", lambda e, s=s: e.reciprocal(out=s[:, 1:2], in_=s[:, 1:2]), r=[sk], w=[sk])
            p.op("dve", lambda e, x_=x_, s=s: e.tensor_scalar(out=x_[:], in0=x_[:], scalar1=s[:, 1:2], scalar2=None, op0=ALU.mult), r=[xk_, sk], w=[xk_])
            p.op("pe", lambda e, msk=msk: e.matmul(ps2[:], lhsT=C["strict0"][:], rhs=msk[:], start=True, stop=True), r=["masks", mskk], w=["sps2"])
            p.op("pe", lambda e, msk=msk: e.matmul(ps3[:], lhsT=C["ones64"][:], rhs=msk[:], start=True, stop=True), r=["masks", mskk], w=["sps3"])
            rk, rkk = sm["rk"].next()
            p.op("dve", lambda e, rk=rk: e.tensor_tensor(out=rk[:], in0=ps2[:], in1=cnt[:], op=ALU.add), r=["sps2", "scnt"], w=[rkk])
            p.op("dve", lambda e: e.tensor_tensor(out=cnt[:], in0=ps3[:], in1=cnt[:], op=ALU.add), r=["sps3", "scnt", rkk], w=["scnt"])
            for k in range(4):
                oh, ohk = sm["oh"].next()
                p.op("dve", lambda e, oh=oh, g=g, m=m, k=k: e.tensor_scalar(out=oh[:], in0=g[:], scalar1=m[:, k:k + 1], scalar2=None, op0=ALU.is_equal), r=[gk, mk8], w=[ohk])
                for (src, srck, dst) in ((C["iota_f"][:, 0:NE], "iota_f", e4), (rk[:], rkk, rank4), (x_[:], xk_, gate4)):
                    tmp, tmpk = sm["tmp"].next()
                    p.op("dve", lambda e, tmp=tmp, oh=oh, src=src: e.tensor_tensor(out=tmp[:], in0=oh[:], in1=src, op=ALU.mult), r=[ohk, srck], w=[tmpk])
                    p.op("dve", lambda e, tmp=tmp, dst=dst, ti=ti, k=k: e.reduce_sum(out=dst[:, ti, k:k + 1], in_=tmp[:], axis=AX.X), r=[tmpk], w=["rt"])
        ci = p.sb("sci", [128, NE], I32)
        padf = p.sb("spadf", [128, NE], F32)
        ca = p.sb("sca", [128, NE], F32)
        cb = p.sb("scb", [128, NE], F32)
        p.op("dve", lambda e: e.tensor_copy(out=ci[:], in_=cnt[:]), r=["scnt"], w=["sci"])
        p.op("dve", lambda e: e.tensor_single_scalar(out=ci[:], in_=ci[:], scalar=BLK - 1, op=ALU.add), r=["sci"], w=["sci"])
        p.op("dve", lambda e: e.tensor_single_scalar(out=ci[:], in_=ci[:], scalar=LOG, op=ALU.arith_shift_right), r=["sci"], w=["sci"])
        p.op("dve", lambda e: e.tensor_single_scalar(out=ci[:], in_=ci[:], scalar=LOG, op=ALU.logical_shift_left), r=["sci"], w=["sci"])
        p.op("dve", lambda e: e.tensor_copy(out=padf[:], in_=ci[:]), r=["sci"], w=["spadf"])
        p.op("dve", lambda e: e.tensor_copy(out=ca[:], in_=padf[:]), r=["spadf"], w=["sca"])
        cur, curk, oth, othk = ca, "sca", cb, "scb"
        sft = 1
        while sft < NE:
            p.op("dve", lambda e, cur=cur, oth=oth: e.tensor_copy(out=oth[:], in_=cur[:]), r=[curk], w=[othk])
            p.op("dve", lambda e, cur=cur, oth=oth, sft=sft: e.tensor_tensor(out=oth[:, sft:NE], in0=cur[:, sft:NE], in1=cur[:, 0:NE - sft], op=ALU.add), r=[curk, othk], w=[othk])
            cur, curk, oth, othk = oth, othk, cur, curk
            sft *= 2
        pend, pendk = cur, curk
        p.op("dve", lambda e: e.tensor_tensor(out=RT["pstart"][:], in0=pend[:], in1=padf[:], op=ALU.subtract), r=[pendk, "spadf"], w=["rt"])
        bst = p.sb("sbst", [128, NB], F32)
        p.op("dve", lambda e: e.tensor_scalar(out=bst[:], in0=C["iota_f"][:, 0:NB], scalar1=float(BLK), scalar2=None, op0=ALU.mult), r=["iota_f"], w=["sbst"])
        blke = RT["blke"]
        p.op("dve", lambda e: e.memset(blke[:, 0:NB], 0.0), w=["rt"])
        for e_ in range(NE):
            p.op("dve", lambda e, e_=e_: e.scalar_tensor_tensor(out=blke[:, 0:NB], in0=bst[:], scalar=pend[:, e_:e_ + 1], in1=blke[:, 0:NB], op0=ALU.is_ge, op1=ALU.add),
                 r=["sbst", pendk, "rt"], w=["rt"])
        p.op("dve", lambda e: e.tensor_scalar(out=blke[:, 0:NB], in0=blke[:, 0:NB], scalar1=float(NE - 1), scalar2=None, op0=ALU.min), r=["rt"], w=["rt"])
        base8 = p.sb("sbase8", [128, 8], F32)
        p.op("dve", lambda e: e.tensor_scalar(out=base8[:], in0=C["iota_f"][:, 0:8], scalar1=128.0, scalar2=C["iota_p"][:, 0:1], op0=ALU.mult, op1=ALU.add),
             r=["iota_f", "iota_p"], w=["sbase8"])
        b1k = p.sb("sb1k", [128, NB], F32)
        lofs = float(l * NE * 1024) if CAST_IN_GATHER else 0.0
        p.op("dve", lambda e: e.tensor_scalar(out=b1k[:], in0=blke[:, 0:NB], scalar1=1024.0, scalar2=lofs, op0=ALU.mult, op1=ALU.add), r=["rt"], w=["sb1k"])
        wf = p.sb("swf", [128, NB, 8], F32)
        for b_ in range(NB):
            p.op("dve" if b_ % 2 == 0 else "pool", lambda e, b_=b_: e.tensor_scalar(out=wf[:, b_, :], in0=base8[:], scalar1=b1k[:, b_:b_ + 1], scalar2=None, op0=ALU.add),
                 r=["sbase8", "sb1k"], w=["swf"])
        p.op("dve", lambda e: e.tensor_copy(out=RT["widx"][:, 0:NB, :], in_=wf[:]), r=["swf"], w=["rt"])
        destf, desti = RT["destf"], RT["desti"]
        for ti in range(ntile):
            for k in range(4):
                oh, ohk = sm["oh"].next()
                p.op("dve", lambda e, oh=oh, ti=ti, k=k: e.tensor_scalar(out=oh[:], in0=C["iota_f"][:, 0:NE], scalar1=e4[:, ti, k:k + 1], scalar2=None, op0=ALU.is_equal),
                     r=["iota_f", "rt"], w=[ohk])
                tmp, tmpk = sm["tmp"].next()
                p.op("dve", lambda e, tmp=tmp, oh=oh: e.tensor_tensor(out=tmp[:], in0=oh[:], in1=RT["pstart"][:], op=ALU.mult), r=[ohk, "rt"], w=[tmpk])
                p.op("dve", lambda e, tmp=tmp, ti=ti, k=k: e.reduce_sum(out=destf[:, ti, k:k + 1], in_=tmp[:], axis=AX.X), r=[tmpk], w=["rt"])
        p.op("dve", lambda e: e.tensor_tensor(out=destf[:, 0:ntile, :], in0=destf[:, 0:ntile, :], in1=rank4[:, 0:ntile, :], op=ALU.add), r=["rt"], w=["rt"])
        p.op("dve", lambda e: e.tensor_scalar(out=destf[:, 0:ntile, :], in0=destf[:, 0:ntile, :], scalar1=float(NB * BLK - 1), scalar2=0.0, op0=ALU.min, op1=ALU.max), r=["rt"], w=["rt"])
        p.op("dve", lambda e: e.tensor_copy(out=desti[:, 0:ntile, :], in_=destf[:, 0:ntile, :]), r=["rt"], w=["rt"])
    return NB


def phase_moe_sparse_dispatch(p, C, io, RT, t_lo, t_hi, NB, BLK=512):
    HTOK, HS = io["HTOK"], io["HS"]
    ntile = (t_hi - t_lo) // 128
    with p.phase():
        z = p.sb("dz", [128, 4, D], BF16)
        p.op("pool", lambda e: e.memset(z[:], 0.0), w=["dz"])
        for b_ in range(NB * BLK // 512):
            p.dma("sp" if b_ % 2 == 0 else "act", lambda e, b_=b_: e.dma_start(out=HS[b_ * 512:(b_ + 1) * 512, :].rearrange("(j q) d -> q j d", q=128), in_=z[:]),
                  r=["dz"], w=[f"HSz{b_ % 8}"])
        hb = Ring(p, "dhb", [128, D], BF16, n=3)
        zk = [f"HSz{i}" for i in range(8)]
        for ti in range(ntile):
            tok = t_lo + ti * 128
            h, hk = hb.next()
            p.dma("sp", lambda e, h=h, tok=tok: e.dma_start(out=h[:], in_=HTOK[tok:tok + 128, :]), w=[hk])
            for k in range(4):
                p.dma("pool", lambda e, h=h, ti=ti, k=k: e.indirect_dma_start(
                    out=HS[:, :], out_offset=bass.IndirectOffsetOnAxis(ap=RT["desti"][:, ti, k:k + 1], axis=0), in_=h[:, :], in_offset=None),
                    r=[hk, "rt"] + zk, w=[f"HSs{(ti * 4 + k) % 8}"])


def phase_moe_sparse_experts(p, C, io, RT, l, NE, NB, BLK=512):
    HS, Y = io["HS"], io["Y"]
    if CAST_IN_GATHER:
        WGf = io["moe_gu_w"].rearrange("l e k n -> (l e k) n")
        WDf = io["moe_down_w"].rearrange("l e k n -> (l e k) n")
    else:
        WGf = io["WGU"].rearrange("e k n -> (e k) n")
        WDf = io["WDN"].rearrange("e k n -> (e k) n")
    blke = RT["blke"]
    with p.phase():
        b32 = p.sb("xb32", [NE, 2048], F32)
        bgu = p.sb("xbgu", [NE, 2048], BF16)
        bdn = p.sb("xbdn", [NE, 1024], BF16)
        p.dma("sp", lambda e: e.dma_start(out=b32[:], in_=io["moe_gu_b"][l]), w=["xb32"])
        p.op("dve", lambda e: e.tensor_copy(out=bgu[:], in_=b32[:]), r=["xb32"], w=["xbias"])
        p.dma("sp", lambda e: e.dma_start(out=b32[:, 0:1024], in_=io["moe_down_b"][l]), r=["xbias"], w=["xb32"])
        p.op("dve", lambda e: e.tensor_copy(out=bdn[:], in_=b32[:, 0:1024]), r=["xb32"], w=["xbias"])
        ones5 = p.sb("xones", [NE, BLK], F32)
        p.op("dve", lambda e: e.memset(ones5[:], 1.0), w=["xones"])
        wg = Ring(p, "xwg", [128, KC, 2048], BF16, n=2)
        wd = Ring(p, "xwd", [128, KC, 1024], BF16, n=2)
        hs = Ring(p, "xhs", [128, BLK // 128, D], BF16, n=2)
        hsT = Ring(p, "xhsT", [128, KC, BLK], BF16, n=2)
        ohT = Ring(p, "xohT", [NE, BLK], BF16, n=2)
        actT = Ring(p, "xactT", [128, KC, BLK], BF16, n=2)
        t1 = Ring(p, "xt1", [128, BLK], n=2)
        sgm = Ring(p, "xsg", [128, BLK], n=2)
        t2 = Ring(p, "xt2", [128, BLK], n=2)
        yo = Ring(p, "xyo", [128, D], n=2)
        pst = Ring(p, "xpst", [128, KC, 128], BF16, n=2, psum=True)
        psg = Ring(p, "xpsg", [128, 512], n=4, psum=True)
        psd = Ring(p, "xpsd", [128, 512], n=2, psum=True)
        ev = 0
        for b_ in range(NB):
            g_, gk = wg.next()
            d_, dk = wd.next()
            for kc in range(KC):
                p.dma("pool", lambda e, g_=g_, kc=kc, b_=b_: e.indirect_dma_start(
                    out=g_[:, kc, :], out_offset=None, in_=WGf[:, :], in_offset=bass.IndirectOffsetOnAxis(ap=RT["widx"][:, b_, kc:kc + 1], axis=0)), r=["rt"], w=[gk])
                p.dma("pool", lambda e, d_=d_, kc=kc, b_=b_: e.indirect_dma_start(
                    out=d_[:, kc, :], out_offset=None, in_=WDf[:, :], in_offset=bass.IndirectOffsetOnAxis(ap=RT["widx"][:, b_, kc:kc + 1], axis=0)), r=["rt"], w=[dk])
            h_, hk = hs.next()
            p.dma("sp", lambda e, h_=h_, b_=b_: e.dma_start(out=h_[:], in_=HS[b_ * BLK:(b_ + 1) * BLK, :].rearrange("(j q) d -> q j d", q=128)), w=[hk])
            o_, ok_ = ohT.next()
            p.op("dve", lambda e, o_=o_, b_=b_: e.tensor_scalar(out=o_[:], in0=ones5[:], scalar1=blke[0:NE, b_:b_ + 1], scalar2=C["iota_p"][0:NE, 0:1],
                                                                 op0=ALU.mult, op1=ALU.is_equal), r=["xones", "rt", "iota_p"], w=[ok_])
            hT, hTk = hsT.next()
            for j in range(BLK // 128):
                pt_, ptk = pst.next()
                for kc in range(KC):
                    p.op("pe", lambda e, pt_=pt_, h_=h_, j=j, kc=kc: e.transpose(pt_[:, kc, :], h_[:, j, kc * 128:(kc + 1) * 128], C["identb"][:]), r=[hk, "identb"], w=[ptk])
                if j % 2 == 0:
                    p.op("act", lambda e, hT=hT, pt_=pt_, j=j: e.copy(out=hT[:, :, j * 128:(j + 1) * 128], in_=pt_[:]), r=[ptk], w=[hTk])
                else:
                    p.op("dve", lambda e, hT=hT, pt_=pt_, j=j: e.tensor_copy(out=hT[:, :, j * 128:(j + 1) * 128], in_=pt_[:]), r=[ptk], w=[hTk])
            aT, aTk = actT.next()
            N = BLK
            for c in range(8):
                pa, pak = psg.next()
                pb, pbk = psg.next()
                for (ps_, pk_, col0) in ((pa, pak, c * 128), (pb, pbk, DE + c * 128)):
                    for kc in range(KC):
                        p.op("pe", lambda e, ps_=ps_, g_=g_, kc=kc, col0=col0, hT=hT: e.matmul(
                            ps_[:, 0:N], lhsT=g_[:, kc, col0:col0 + 128], rhs=hT[:, kc, :], start=(kc == 0), stop=False), r=[gk, hTk], w=[pk_])
                    p.op("pe", lambda e, ps_=ps_, col0=col0, o_=o_: e.matmul(ps_[:, 0:N], lhsT=bgu[:, col0:col0 + 128], rhs=o_[:], start=False, stop=True),
                         r=["xbias", ok_], w=[pk_])
                a1, a1k = t1.next()
                p.op("dve", lambda e, a1=a1, pa=pa: e.tensor_scalar(out=a1[:], in0=pa[:], scalar1=SW_LIMIT, scalar2=None, op0=ALU.min), r=[pak], w=[a1k])
                s_, sk_ = sgm.next()
                p.op("act", lambda e, s_=s_, a1=a1: e.activation(out=s_[:], in_=a1[:], func=AF.Sigmoid, scale=SW_ALPHA), r=[a1k], w=[sk_])
                a2, a2k = t2.next()
                p.op("dve", lambda e, a2=a2, pb=pb: e.tensor_scalar(out=a2[:], in0=pb[:], scalar1=SW_LIMIT, scalar2=-SW_LIMIT, op0=ALU.min, op1=ALU.max), r=[pbk], w=[a2k])
                p.op("pool", lambda e, a1=a1, s_=s_: e.tensor_tensor(out=a1[:], in0=a1[:], in1=s_[:], op=ALU.mult), r=[a1k, sk_], w=[a1k])
                p.op("dve", lambda e, aT=aT, a1=a1, a2=a2, c=c: e.scalar_tensor_tensor(out=aT[:, c, :], in0=a2[:], scalar=1.0, in1=a1[:], op0=ALU.add, op1=ALU.mult),
                     r=[a1k, a2k], w=[aTk])
            for j in range(BLK // 128):
                y_, yk = yo.next()
                for n in range(2):
                    pd, pdk = psd.next()
                    for kc in range(KC):
                        p.op("pe", lambda e, pd=pd, aT=aT, d_=d_, kc=kc, j=j, n=n: e.matmul(
                            pd[:], lhsT=aT[:, kc, j * 128:(j + 1) * 128], rhs=d_[:, kc, n * 512:(n + 1) * 512], start=(kc == 0), stop=False), r=[aTk, dk], w=[pdk])
                    p.op("pe", lambda e, pd=pd, o_=o_, n=n: e.matmul(pd[:], lhsT=o_[:, 0:128], rhs=bdn[:, n * 512:(n + 1) * 512], start=False, stop=True),
                         r=[ok_, "xbias"], w=[pdk])
                    ev += 1
                    if ev % 2 == 0:
                        p.op("act", lambda e, y_=y_, pd=pd, n=n: e.copy(out=y_[:, n * 512:(n + 1) * 512], in_=pd[:]), r=[pdk], w=[yk])
                    else:
                        p.op("dve", lambda e, y_=y_, pd=pd, n=n: e.tensor_copy(out=y_[:, n * 512:(n + 1) * 512], in_=pd[:]), r=[pdk], w=[yk])
                row = b_ * BLK + j * 128
                p.dma("sp", lambda e, y_=y_, row=row: e.dma_start(out=Y[row:row + 128, :], in_=y_[:]), r=[yk], w=["Y"])


def phase_moe_sparse_combine(p, C, io, RT, l, t_lo, t_hi, n_ctx):
    xres, Y, modrow = io["xres"], io["Y"], io["modrow"]
    ntile = (t_hi - t_lo) // 128
    with p.phase():
        gt = p.sb("cgt2", [128, 2, D], F32)
        for r in range(2):
            p.dma("sp", lambda e, r=r: e.dma_start(out=gt[:, r, :], in_=modrow[l, r, 5120:6144].partition_broadcast(128)), w=["cgt2"])
        yk_ = Ring(p, "cyk", [128, D], n=8)
        acc = Ring(p, "cacc", [128, D], n=2)
        xt = Ring(p, "cxt", [128, D], n=2)
        for ti in range(ntile):
            tok = t_lo + ti * 128
            r = 1 if tok < n_ctx else 0
            x, xk = xt.next()
            p.dma("sp", lambda e, x=x, tok=tok: e.dma_start(out=x[:], in_=xres[tok:tok + 128, :]), w=[xk])
            a, ak = acc.next()
            for k in range(4):
                y, yk = yk_.next()
                p.dma("pool", lambda e, y=y, ti=ti, k=k: e.indirect_dma_start(
                    out=y[:, :], out_offset=None, in_=Y[:, :], in_offset=bass.IndirectOffsetOnAxis(ap=RT["desti"][:, ti, k:k + 1], axis=0)), r=["rt", "Y"], w=[yk])
                if k == 0:
                    p.op("dve", lambda e, a=a, y=y, ti=ti: e.tensor_scalar(out=a[:], in0=y[:], scalar1=RT["gate4"][:, ti, 0:1], scalar2=None, op0=ALU.mult), r=[yk, "rt"], w=[ak])
                else:
                    p.op("dve", lambda e, a=a, y=y, ti=ti, k=k: e.scalar_tensor_tensor(out=a[:], in0=y[:], scalar=RT["gate4"][:, ti, k:k + 1], in1=a[:], op0=ALU.mult, op1=ALU.add),
                         r=[yk, "rt", ak], w=[ak])
            p.op("pool", lambda e, a=a, r=r: e.tensor_tensor(out=a[:], in0=a[:], in1=gt[:, r, :], op=ALU.mult), r=[ak, "cgt2"], w=[ak])
            p.op("dve", lambda e, a=a, x=x: e.tensor_tensor(out=x[:], in0=x[:], in1=a[:], op=ALU.add), r=[ak, xk], w=[xk])
            p.dma("sp", lambda e, x=x, tok=tok: e.dma_start(out=xres[tok:tok + 128, :], in_=x[:]), r=[xk], w=["xres"])

from concourse.bass_utils import run_bass_kernel_spmd

N_CTX = 256
N_LAT = 8192
DEPTH = 2
NEXP = 32
_W_NAMES = ["ada_w", "ada_b", "norm1_g", "norm2_g", "ev_w_in", "ev_w_out", "gla_gk_up", "gla_gk_b", "gla_norm_g",
            "rw_mu", "rw_w0", "rw_w2", "rw_a0", "rw_a2", "rw_k_k", "rw_k_a", "rw_r_k", "rw_g2", "rw_ln_w", "rw_ln_b",
            "od_w_in", "mla_q_norm", "mla_wq_up", "mla_kv_norm", "mla_wkv_up", "od_w_out",
            "router_w", "router_b", "moe_gu_w", "moe_gu_b", "moe_down_w", "moe_down_b", "final_norm_g"]


def build(shapes, n_ctx=N_CTX, n_lat=N_LAT, NE=NEXP):
    n_tok = n_ctx + n_lat
    L = DEPTH
    nc = bass.Bass("TRN2", target_bir_lowering=False)
    NBmax = (4 * n_tok + 511) // 512 + NE
    io = {}
    for k, shp in shapes.items():
        io[k] = dram(nc, k, list(shp), kind="ExternalInput")
    for name, shp, dt in (("xres", [n_tok, 1024], F32), ("modrow", [L, 2, 6144], F32), ("PT", [3376, n_tok], F32),
                          ("PTOK", [n_tok, 1536], F32), ("SMT", [1824, n_tok], F32), ("YTG", [2, 512, n_tok], F32),
                          ("YTR", [2, 512, n_tok], F32), ("RK", [2, 512, n_tok], F32), ("MIXT", [1024, n_tok], BF16),
                          ("HT", [1024, n_tok], BF16), ("GATE", [n_tok, NE], F32), ("WGU", [NE, 1024, 2048], BF16),
                          ("WDN", [NE, 1024, 1024], BF16), ("QN", [8, 128, n_lat], BF16), ("QR", [8, 64, n_lat], BF16),
                          ("KN", [8, 128, n_tok], BF16), ("KRo", [64, n_tok], BF16), ("Vt", [n_tok, 1024], BF16),
                          ("HTOK", [n_tok, 1024], BF16), ("HS", [NBmax * 512, 1024], BF16), ("Y", [NBmax * 512, 1024], F32)):
        io[name] = dram(nc, name, shp, dt)
    io["out"] = dram(nc, "out", [n_lat, 1024], kind="ExternalOutput")
    p = Prog(nc)
    C = make_consts(p)
    make_masks(p, C)
    make_rope_consts(p, C)
    G = {k: p.sb("G" + k, [128, L, 2, 8], F32) for k in ("A1", "B1", "A2", "B2")}
    RT = moe_tables(p, NE, n_tok // 128, NBmax)
    with p.phase():
        cp = Ring(p, "cpx", [128, 1024], n=3)
        for ti in range(n_tok // 128):
            x_, xk = cp.next()
            p.dma("sp", lambda e, x_=x_, ti=ti: e.dma_start(out=x_[:], in_=io["xin"][ti * 128:(ti + 1) * 128, :]), w=[xk])
            p.dma("sp", lambda e, x_=x_, ti=ti: e.dma_start(out=io["xres"][ti * 128:(ti + 1) * 128, :], in_=x_[:]), r=[xk], w=["xres"])
    phase_ada(p, C, io, L, G)
    phase_inproj(p, C, io, G, 0, n_ctx, n_tok)
    phase_gla(p, C, io, n_ctx, n_tok)
    phase_gla_finish(p, C, io, n_tok)
    phase_shift(p, C, io, n_ctx, n_tok)
    phase_rwkv(p, C, io, n_ctx, n_tok)
    phase_rwkv_finish(p, C, io, n_tok)
    phase_outproj(p, C, io, 0, io["ev_w_out"], n_ctx, n_tok)
    NB = phase_moe_sparse_route(p, C, io, G, RT, 0, NE, 0, n_tok, n_ctx)
    phase_moe_sparse_dispatch(p, C, io, RT, 0, n_tok, NB)
    phase_moe_sparse_experts(p, C, io, RT, 0, NE, NB)
    phase_moe_sparse_combine(p, C, io, RT, 0, 0, n_tok, n_ctx)
    phase_mla_proj(p, C, io, G, 1, n_ctx, n_tok)
    phase_mla_attn(p, C, io, n_ctx, n_tok)
    phase_outproj(p, C, io, 1, io["od_w_out"], n_ctx, n_tok, t_lo=n_ctx)
    NB = phase_moe_sparse_route(p, C, io, G, RT, 1, NE, n_ctx, n_tok, n_ctx)
    phase_moe_sparse_dispatch(p, C, io, RT, n_ctx, n_tok, NB)
    phase_moe_sparse_experts(p, C, io, RT, 1, NE, NB)
    phase_moe_sparse_combine(p, C, io, RT, 1, n_ctx, n_tok, n_ctx)
    phase_final(p, C, io, n_ctx, n_tok)
    p.finish()
    return nc


def kernel(**inputs):
    f = lambda a: np.ascontiguousarray(np.asarray(a, dtype=np.float32))
    x, c, ctx, c_ctx = f(inputs["x"]), f(inputs["c"]), f(inputs["ctx"]), f(inputs["c_ctx"])
    B = x.shape[0]
    shared = {}
    for k in _W_NAMES:
        a = f(inputs[k])
        if k.startswith(("ev_", "gla_", "rw_", "od_", "mla_")):
            a = np.ascontiguousarray(a[0])
        shared[k] = a
    in_maps = []
    for b in range(B):
        m = dict(shared)
        m["xin"] = np.ascontiguousarray(np.concatenate([ctx[b], x[b]], axis=0))
        m["cvec"] = np.ascontiguousarray(np.stack([c[b], c_ctx], axis=0))
        in_maps.append(m)
    shapes = {k: v.shape for k, v in in_maps[0].items()}
    nc = build(shapes, n_ctx=ctx.shape[1], n_lat=x.shape[1], NE=shared["router_w"].shape[-1])
    res = run_bass_kernel_spmd(nc, in_maps, core_ids=list(range(B)))
    return np.stack([np.asarray(r["out"], dtype=np.float32) for r in res.results], axis=0)
```

```python
import contextlib
import numpy as np
import concourse.bass as bass
import concourse.mybir as mybir

F32 = mybir.dt.float32
BF16 = mybir.dt.bfloat16
I32 = mybir.dt.int32
U32 = mybir.dt.uint32
ALU = mybir.AluOpType
AF = mybir.ActivationFunctionType
AX = mybir.AxisListType

ENGS = ("pe", "act", "dve", "pool", "sp")
RESET_THRESH = 20000


class Prog:
    def __init__(self, nc, n_dma_slots=10):
        self.nc = nc
        self.es = contextlib.ExitStack()
        self.cur = self.es
        self.streams = {e: [] for e in ENGS}
        self.count = {e: 0 for e in ENGS}
        self.seen = {e: {} for e in ENGS}
        self.bufs = {}
        self.sems = {}
        for e in ("pe", "act", "dve", "pool"):
            self.sems[e] = self.es.enter_context(nc.semaphore("s_" + e))
        self.dma_slots = {}
        self.dma_rr = {}
        for q in ("sp", "act", "pool"):
            self.dma_slots[q] = []
            for i in range(n_dma_slots):
                s = self.es.enter_context(nc.semaphore(f"d_{q}{i}"))
                self.sems[f"d_{q}{i}"] = s
                self.dma_slots[q].append([f"d_{q}{i}", 0])
            self.dma_rr[q] = 0
        self.n_instr = 0
        self.bsem = self.es.enter_context(nc.semaphore("s_bar"))
        self.gsem = self.es.enter_context(nc.semaphore("s_go"))
        self.nreset = 0
        self.reset_thresh = RESET_THRESH

    def _maybe_reset(self):
        if max(self.count.values()) >= self.reset_thresh or \
                max(v for q in self.dma_slots if q != "pool" for _, v in self.dma_slots[q]) >= 2 * self.reset_thresh:
            self.sync_reset()

    def sync_reset(self):
        self.barrier()
        self.nreset += 1
        k = self.nreset
        bs, gs = self.bsem, self.gsem
        for e in ENGS:
            self.streams[e].append(lambda eng, bs=bs: eng.sem_inc(bs, 1))
        self.streams["sp"].append(lambda eng, bs=bs, k=k: eng.wait_ge(bs, 5 * k))
        for name, sem in self.sems.items():
            if name.startswith("d_pool"):
                continue
            self.streams["sp"].append(lambda eng, sem=sem: eng.sem_clear(sem))
        self.streams["sp"].append(lambda eng, gs=gs: eng.sem_inc(gs, 1))
        for e in ENGS:
            if e != "sp":
                self.streams[e].append(lambda eng, gs=gs, k=k: eng.wait_ge(gs, k))
        self.count = {e: 0 for e in ENGS}
        self.seen = {e: {} for e in ENGS}
        self.bufs = {}
        for q in self.dma_slots:
            if q == "pool":
                continue
            for slot in self.dma_slots[q]:
                slot[1] = 0

    def sb(self, name, shape, dt=F32):
        self._uid = getattr(self, "_uid", 0) + 1
        name = f"{name}_u{self._uid}"
        return self.cur.enter_context(self.nc.sbuf_tensor(name, list(shape), dt))

    def ps(self, name, shape, dt=F32):
        self._uid = getattr(self, "_uid", 0) + 1
        name = f"{name}_u{self._uid}"
        return self.cur.enter_context(self.nc.psum_tensor(name, list(shape), dt))

    def _deps(self, r, w):
        deps = {}

        def add(tok):
            if tok is None:
                return
            s, v = tok
            if deps.get(s, 0) < v:
                deps[s] = v

        for k in r:
            b = self.bufs.setdefault(k, {"w": None, "r": {}})
            add(b["w"])
        for k in w:
            b = self.bufs.setdefault(k, {"w": None, "r": {}})
            add(b["w"])
            for s, v in b["r"].items():
                add((s, v))
        return deps

    def _commit(self, r, w, tok):
        for k in r:
            b = self.bufs[k]
            if b["r"].get(tok[0], 0) < tok[1]:
                b["r"][tok[0]] = tok[1]
        for k in w:
            b = self.bufs[k]
            b["w"] = tok
            b["r"] = {}

    def _emit_waits(self, eng, deps):
        seen = self.seen[eng]
        for s, v in deps.items():
            if eng == "pe" and s == "pe":
                continue
            if seen.get(s, 0) >= v:
                continue
            seen[s] = v
            sem = self.sems[s]
            self.streams[eng].append(lambda e, sem=sem, v=v: e.wait_ge(sem, v))

    def op(self, eng, fn, r=(), w=()):
        self._maybe_reset()
        deps = self._deps(r, w)
        self._emit_waits(eng, deps)
        self.count[eng] += 1
        n = self.count[eng]
        sem = self.sems[eng]
        self.streams[eng].append(lambda e, fn=fn, sem=sem: fn(e).then_inc(sem, 1))
        self._commit(r, w, (eng, n))
        self.n_instr += 1

    def dma(self, q, fn, r=(), w=()):
        self._maybe_reset()
        deps = self._deps(r, w)
        i = self.dma_rr[q]
        self.dma_rr[q] = (i + 1) % len(self.dma_slots[q])
        slot = self.dma_slots[q][i]
        if slot[1] > 0:
            if deps.get(slot[0], 0) < slot[1]:
                deps[slot[0]] = slot[1]
        self._emit_waits(q, deps)
        slot[1] += 16
        sem = self.sems[slot[0]]
        self.streams[q].append(lambda e, fn=fn, sem=sem: fn(e).then_inc(sem, 16))
        self._commit(r, w, (slot[0], slot[1]))
        self.n_instr += 1

    def wait_all(self, eng, keys):
        deps = self._deps(keys, ())
        self._emit_waits(eng, deps)

    def barrier(self):
        full = {}
        for e in ("pe", "act", "dve", "pool"):
            if self.count[e] > 0:
                full[e] = self.count[e]
        for q in self.dma_slots:
            for name, v in self.dma_slots[q]:
                if v > 0:
                    full[name] = v
        for e in ENGS:
            d = dict(full)
            d.pop(e, None)
            if e == "pe":
                pass
            self._emit_waits(e, d)

    @contextlib.contextmanager
    def phase(self):
        old = self.cur
        with contextlib.ExitStack() as st:
            self.cur = st
            yield
            self.barrier()
            self.flush()
        self.cur = old

    def flush(self):
        self._emit_block()
        self.streams = {e: [] for e in ENGS}

    def finish(self):
        self.barrier()
        self.flush()
        self.es.close()

    def _emit_block(self):
        nc = self.nc
        with nc.Block() as block:
            @block.tensor
            def _(e):
                for f in self.streams["pe"]:
                    f(e)

            @block.scalar
            def _(e):
                for f in self.streams["act"]:
                    f(e)

            @block.vector
            def _(e):
                for f in self.streams["dve"]:
                    f(e)

            @block.gpsimd
            def _(e):
                for f in self.streams["pool"]:
                    f(e)

            @block.sync
            def _(e):
                for f in self.streams["sp"]:
                    f(e)

import contextlib
import numpy as np

D = 1024
KC = 8
EPS = 1e-6
GLA_COLS = 1552
RW_COLS = 1824
EVEN_IN = 3376


def dram(nc, name, shape, dt=F32, kind="Internal"):
    return nc.dram_tensor(name, list(shape), dt, kind=kind).ap()


def make_consts(p):
    nc = p.nc
    C = {}
    iota_f = p.sb("iota_f", [128, 128], F32)
    iota_p = p.sb("iota_p", [128, 1], F32)
    ii = p.sb("iota_i", [128, 128], I32)
    ip = p.sb("iota_pi", [128, 1], I32)
    p.op("pool", lambda e: e.iota(ii[:], pattern=[[1, 128]], base=0, channel_multiplier=0), w=["iota_i"])
    p.op("pool", lambda e: e.iota(ip[:], pattern=[[1, 1]], base=0, channel_multiplier=1), w=["iota_pi"])
    p.op("dve", lambda e: e.tensor_copy(out=iota_f[:], in_=ii[:]), r=["iota_i"], w=["iota_f"])
    p.op("dve", lambda e: e.tensor_copy(out=iota_p[:], in_=ip[:]), r=["iota_pi"], w=["iota_p"])
    ident = p.sb("ident", [128, 128], F32)
    p.op("dve", lambda e: e.tensor_scalar(out=ident[:], in0=iota_f[:], scalar1=iota_p[:, 0:1], scalar2=None,
                                          op0=ALU.is_equal), r=["iota_f", "iota_p"], w=["ident"])
    identb = p.sb("identb", [128, 128], BF16)
    p.op("dve", lambda e: e.tensor_copy(out=identb[:], in_=ident[:]), r=["ident"], w=["identb"])
    C.update(iota_f=iota_f, iota_p=iota_p, ident=ident, identb=identb)
    return C


def phase_ada(p, C, io, L, G):
    nc = p.nc
    cvec, ada_w, ada_b = io["cvec"], io["ada_w"], io["ada_b"]
    modrow = io["modrow"]
    with p.phase():
        cT = p.sb("cT", [128, KC, 2], F32)
        sT = p.sb("sT", [128, KC, 2], F32)
        for r in range(2):
            p.dma("sp", lambda e, r=r: e.dma_start(out=cT[:, :, r], in_=cvec[r, :].rearrange("(c q) -> q c", q=128),
                                                   allow_slow_non_contiguous=True), w=["cT"])
        p.op("act", lambda e: e.activation(out=sT[:], in_=cT[:], func=AF.Silu), r=["cT"], w=["sT"])
        wts = [p.sb(f"adaw{i}", [128, KC, 512], F32) for i in range(2)]
        mrow = p.sb("mrow", [2, 6144], F32)
        brow = p.sb("brow", [2, 6144], F32)
        mps = p.ps("mps", [2, 512], F32)
        tps = p.ps("tps", [128, 48, 2], F32)
        mcol = p.sb("mcol", [128, 48, 2], F32)
        gcol = p.sb("gcol", [128, 2, KC], F32)
        it = 0
        for l in range(L):
            for r in range(2):
                p.dma("sp", lambda e, r=r, l=l: e.dma_start(out=brow[r:r + 1, :], in_=ada_b[l:l + 1, :]), w=["brow"])
            p.dma("sp", lambda e, l=l: e.dma_start(out=gcol[:, 0, :], in_=io["norm1_g"][l, :].rearrange("(c q) -> q c", q=128),
                                                   allow_slow_non_contiguous=True), w=["gcol"])
            p.dma("sp", lambda e, l=l: e.dma_start(out=gcol[:, 1, :], in_=io["norm2_g"][l, :].rearrange("(c q) -> q c", q=128),
                                                   allow_slow_non_contiguous=True), w=["gcol"])
            for cc in range(12):
                wt = wts[it % 2]
                wk = f"adaw{it % 2}"
                it += 1
                p.dma("sp", lambda e, wt=wt, l=l, cc=cc: e.dma_start(
                    out=wt[:], in_=ada_w[l, :, cc * 512:(cc + 1) * 512].rearrange("(c q) n -> q c n", q=128)), w=[wk])
                for kc in range(KC):
                    p.op("pe", lambda e, wt=wt, kc=kc: e.matmul(mps[:], lhsT=sT[:, kc, :], rhs=wt[:, kc, :],
                                                                 start=(kc == 0), stop=(kc == KC - 1)),
                         r=["sT", wk], w=["mps"])
                p.op("dve", lambda e, cc=cc: e.tensor_tensor(out=mrow[:, cc * 512:(cc + 1) * 512], in0=mps[:],
                                                             in1=brow[:, cc * 512:(cc + 1) * 512], op=ALU.add),
                     r=["mps", "brow"], w=["mrow"])
            p.dma("sp", lambda e, l=l: e.dma_start(out=modrow[l], in_=mrow[:]), r=["mrow"], w=[f"modrow{l}"])
            for c in range(48):
                p.op("pe", lambda e, c=c: e.transpose(tps[:, c, :], mrow[0:2, c * 128:(c + 1) * 128], C["ident"][0:2, 0:2]),
                     r=["mrow", "ident"], w=["tps"])
            p.op("dve", lambda e: e.tensor_copy(out=mcol[:], in_=tps[:]), r=["tps"], w=["mcol"])
            for r in range(2):
                for (nm, sc_c, sh_c, gi) in (("1", 1, 0, 0), ("2", 4, 3, 1)):
                    A = G["A" + nm]
                    B = G["B" + nm]
                    p.op("dve", lambda e, A=A, r=r, l=l, sc_c=sc_c, gi=gi: e.scalar_tensor_tensor(
                        out=A[:, l, r, :], in0=mcol[:, sc_c * 8:(sc_c + 1) * 8, r], scalar=1.0, in1=gcol[:, gi, :],
                        op0=ALU.add, op1=ALU.mult), r=["mcol", "gcol"], w=["G"])
                    p.op("dve", lambda e, B=B, r=r, l=l, sh_c=sh_c: e.tensor_copy(
                        out=B[:, l, r, :], in_=mcol[:, sh_c * 8:(sh_c + 1) * 8, r]), r=["mcol"], w=["G"])


def norm_tile(p, C, T, xt_key, xt, Acol, Bcol, hT_dst, hT_key, hT32_dst=None, want_bf=True):
    ss, rstd, xn, junk = T["ss"], T["rstd"], T["xn"], T["junk"]
    p.op("act", lambda e: e.activation(out=junk[:], in_=xt[:], func=AF.Square, accum_out=ss[:, 0:1]),
         r=[xt_key], w=["junk", "ss"])
    p.op("dve", lambda e: e.tensor_scalar(out=rstd[:], in0=ss[:], scalar1=1.0 / D, scalar2=EPS, op0=ALU.mult, op1=ALU.add),
         r=["ss"], w=["rstd"])
    p.op("act", lambda e: e.activation(out=ss[:], in_=rstd[:], func=AF.Sqrt), r=["rstd"], w=["ss"])
    p.op("dve", lambda e: e.reciprocal(out=rstd[:], in_=ss[:]), r=["ss"], w=["rstd"])
    p.op("dve", lambda e: e.tensor_scalar(out=xn[:], in0=xt[:], scalar1=rstd[:, 0:1], scalar2=None, op0=ALU.mult),
         r=[xt_key, "rstd"], w=["xn"])
    for half in range(2):
        tp = T["tp"][half]
        tk = f"tp{half}"
        for j in range(4):
            kc = half * 4 + j
            p.op("pe", lambda e, tp=tp, j=j, kc=kc: e.transpose(tp[:, j, :], xn[:, kc * 128:(kc + 1) * 128], C["ident"][:]),
                 r=["xn", "ident"], w=[tk])
        for j in range(4):
            kc = half * 4 + j
            if hT32_dst is None:
                p.op("act", lambda e, tp=tp, j=j, kc=kc: e.activation(out=hT_dst(kc), in_=tp[:, j, :], func=AF.Identity,
                                                                       scale=Acol[:, kc:kc + 1], bias=Bcol[:, kc:kc + 1]),
                     r=[tk, "G"], w=[hT_key])
            else:
                p.op("act", lambda e, tp=tp, j=j, kc=kc: e.activation(out=hT32_dst(kc), in_=tp[:, j, :], func=AF.Identity,
                                                                       scale=Acol[:, kc:kc + 1], bias=Bcol[:, kc:kc + 1]),
                     r=[tk, "G"], w=[hT_key + "32"])
                if want_bf:
                    p.op("pool", lambda e, kc=kc: e.tensor_copy(out=hT_dst(kc), in_=hT32_dst(kc)), r=[hT_key + "32"], w=[hT_key])


def norm_scratch(p):
    T = {}
    T["ss"] = p.sb("ss", [128, 1], F32)
    T["rstd"] = p.sb("rstd", [128, 1], F32)
    T["xn"] = p.sb("xn", [128, D], F32)
    T["junk"] = p.sb("junk", [128, D], BF16)
    T["tp"] = [p.ps(f"tp{h}", [128, 4, 128], F32) for h in range(2)]
    return T


def load_cast_weight(p, dst, dst_key, src_ap, rows_kc, ncols, stage, stage_key, eng_cycle=("dve", "pool")):
    for kc in range(rows_kc):
        st = stage[kc % len(stage)]
        sk = f"{stage_key}{kc % len(stage)}"
        p.dma("sp", lambda e, st=st, kc=kc: e.dma_start(out=st[:, 0:ncols], in_=src_ap[kc * 128:(kc + 1) * 128, :]), w=[sk])
        eng = eng_cycle[kc % len(eng_cycle)]
        p.op(eng, lambda e, st=st, kc=kc: e.tensor_copy(out=dst[:, kc, :], in_=st[:, 0:ncols]), r=[sk], w=[dst_key])


def phase_inproj(p, C, io, G, l, n_ctx, n_tok):
    xres, PT, PTOK, w_in = io["xres"], io["PT"], io["PTOK"], io["ev_w_in"]
    with p.phase():
        T = norm_scratch(p)
        wbf = p.sb("winbf", [128, KC, EVEN_IN], BF16)
        stage = [p.sb(f"wstage{i}", [128, EVEN_IN], F32) for i in range(2)]
        load_cast_weight(p, wbf, "winbf", w_in, KC, EVEN_IN, stage, "wstage")
        xts = [p.sb(f"xt{i}", [128, D], F32) for i in range(2)]
        hT = [p.sb(f"hT{i}", [128, KC, 512], BF16) for i in range(2)]
        pps = [p.ps(f"pps{i}", [128, 512], F32) for i in range(3)]
        ost = [p.sb(f"ost{i}", [128, 512], F32) for i in range(4)]
        ntile = n_tok // 128
        nsup = (ntile + 3) // 4
        oi = 0
        pi = 0
        tok_cols = [(512, 512), (1024, 512), (GLA_COLS + 1024, 512)]
        for s in range(nsup):
            tiles = list(range(s * 4, min(ntile, s * 4 + 4)))
            N = len(tiles) * 128
            h = hT[s % 2]
            hk = f"hT{s % 2}"
            for j, ti in enumerate(tiles):
                r = 1 if ti * 128 < n_ctx else 0
                xt = xts[ti % 2]
                xk = f"xt{ti % 2}"
                p.dma("sp", lambda e, xt=xt, ti=ti: e.dma_start(out=xt[:], in_=xres[ti * 128:(ti + 1) * 128, :]), w=[xk])
                norm_tile(p, C, T, xk, xt, G["A1"][:, l, r, :], G["B1"][:, l, r, :],
                          lambda kc, h=h, j=j: h[:, kc, j * 128:(j + 1) * 128], hk)
            ncc = (EVEN_IN + 127) // 128
            for cc in range(ncc):
                cw = min(128, EVEN_IN - cc * 128)
                ps = pps[pi % 3]
                pk = f"pps{pi % 3}"
                pi += 1
                for kc in range(KC):
                    p.op("pe", lambda e, ps=ps, kc=kc, cc=cc, cw=cw, h=h, N=N: e.matmul(
                        ps[0:cw, 0:N], lhsT=wbf[:, kc, cc * 128:cc * 128 + cw], rhs=h[:, kc, 0:N],
                        start=(kc == 0), stop=(kc == KC - 1)), r=["winbf", hk], w=[pk])
                o = ost[oi % 4]
                ok = f"ost{oi % 4}"
                eng = "dve" if oi % 2 == 0 else "act"
                oi += 1
                if eng == "dve":
                    p.op("dve", lambda e, o=o, ps=ps, cw=cw, N=N: e.tensor_copy(out=o[0:cw, 0:N], in_=ps[0:cw, 0:N]), r=[pk], w=[ok])
                else:
                    p.op("act", lambda e, o=o, ps=ps, cw=cw, N=N: e.copy(out=o[0:cw, 0:N], in_=ps[0:cw, 0:N]), r=[pk], w=[ok])
                p.dma("sp", lambda e, o=o, cc=cc, cw=cw, N=N, s=s: e.dma_start(
                    out=PT[cc * 128:cc * 128 + cw, s * 512:s * 512 + N], in_=o[0:cw, 0:N]), r=[ok], w=["PT"])
            for j, ti in enumerate(tiles):
                for gi, (c0, cn) in enumerate(tok_cols):
                    ps = pps[pi % 3]
                    pk = f"pps{pi % 3}"
                    pi += 1
                    for kc in range(KC):
                        p.op("pe", lambda e, ps=ps, kc=kc, c0=c0, cn=cn, h=h, j=j: e.matmul(
                            ps[:, 0:cn], lhsT=h[:, kc, j * 128:(j + 1) * 128], rhs=wbf[:, kc, c0:c0 + cn],
                            start=(kc == 0), stop=(kc == KC - 1)), r=["winbf", hk], w=[pk])
                    o = ost[oi % 4]
                    ok = f"ost{oi % 4}"
                    eng = "dve" if oi % 2 == 0 else "act"
                    oi += 1
                    if eng == "dve":
                        p.op("dve", lambda e, o=o, ps=ps, cn=cn: e.tensor_copy(out=o[:, 0:cn], in_=ps[:, 0:cn]), r=[pk], w=[ok])
                    else:
                        p.op("act", lambda e, o=o, ps=ps, cn=cn: e.copy(out=o[:, 0:cn], in_=ps[:, 0:cn]), r=[pk], w=[ok])
                    p.dma("sp", lambda e, o=o, ti=ti, gi=gi, cn=cn: e.dma_start(
                        out=PTOK[ti * 128:(ti + 1) * 128, gi * 512:gi * 512 + cn], in_=o[:, 0:cn]), r=[ok], w=["PTOK"])

import os


class Ring:
    def __init__(self, p, name, shape, dt=F32, n=2, psum=False):
        self.t = [(p.ps if psum else p.sb)(f"{name}{i}", shape, dt) for i in range(n)]
        self.k = [f"{name}{i}" for i in range(n)]
        self.i = 0

    def next(self):
        i = self.i
        self.i = (i + 1) % len(self.t)
        return self.t[i], self.k[i]


class PsRing:
    def __init__(self, p, name, nbanks):
        self.banks = [p.ps(f"{name}{i}", [128, 512], F32) for i in range(nbanks)]
        self.n = nbanks * 1
        self.sub = 1
        self.name = name
        self.i = 0

    def next(self):
        i = self.i
        self.i = (i + 1) % self.n
        sb_ = self.sub
        return self.banks[i // sb_][:, (i % sb_) * 128:(i % sb_ + 1) * 128], f"{self.name}_{i}"


def make_masks(p, C):
    f, q = C["iota_f"], C["iota_p"]
    for nm, op in (("incl0", ALU.is_ge), ("incl1", ALU.is_le), ("strict0", ALU.is_gt), ("strict1", ALU.is_lt)):
        m = p.sb("m_" + nm, [128, 128], F32)
        p.op("dve", lambda e, m=m, op=op: e.tensor_scalar(out=m[:], in0=f[:], scalar1=q[:, 0:1], scalar2=None, op0=op),
             r=["iota_f", "iota_p"], w=["masks"])
        C[nm] = m
    for nm, val in (("ones128", 1.0 / 128), ("ones64", 1.0), ("ones64m", 1.0 / 64)):
        m = p.sb("m_" + nm, [128, 128], F32)
        p.op("dve", lambda e, m=m, val=val: e.memset(m[:], val), w=["masks"])
        C[nm] = m


def chunk_order(n_ctx, n_tok):
    nc_, nt = n_ctx // 128, n_tok // 128
    fwd = list(range(nt))
    bwd = list(range(nc_ - 1, -1, -1)) + list(range(nt - 1, nc_ - 1, -1))
    return [fwd, bwd]


def phase_gla(p, C, io, n_ctx, n_tok):
    PT, PTOK, YT = io["PT"], io["PTOK"], io["YTG"]
    gk_up, gk_b = io["gla_gk_up"], io["gla_gk_b"]
    order = chunk_order(n_ctx, n_tok)
    nt = n_tok // 128
    with p.phase():
        gkup = p.sb("gkup", [16, 2, 256], F32)
        gkb = p.sb("gkb", [64, 2, 4], F32)
        for d in range(2):
            p.dma("sp", lambda e, d=d: e.dma_start(out=gkup[:, d, :], in_=gk_up[d]), w=["gkw"])
            p.dma("sp", lambda e, d=d: e.dma_start(out=gkb[:, d, :], in_=gk_b[d].rearrange("(h q) -> q h", q=64),
                                                   allow_slow_non_contiguous=True), w=["gkw"])
        S = [[p.sb(f"S{d}{h}", [64, 128], F32) for h in range(4)] for d in range(2)]
        for d in range(2):
            for h in range(4):
                p.op("pool", lambda e, t=S[d][h]: e.memset(t[:], 0.0), w=[f"S{d}{h}"])
        gd = Ring(p, "gd", [16, 128], n=4)
        qk = Ring(p, "qk", [64, 2, 128], n=9)
        vv = Ring(p, "vv", [128, 128], n=9)
        sg = Ring(p, "sg", [64, 128], n=9)
        lg = Ring(p, "lg", [64, 128], n=9)
        lgT = Ring(p, "lgT", [128, 64], n=9)
        eW = Ring(p, "eW", [64, 128], n=9)
        eWi = Ring(p, "eWi", [64, 128], n=9)
        rt = Ring(p, "rt", [64, 128], n=9)
        kt = Ring(p, "kt", [64, 128], n=9)
        ktok = Ring(p, "ktok", [128, 64], n=9)
        mrk = Ring(p, "mrk", [128, 128], n=9)
        yts = Ring(p, "yts", [128, 128], n=9)
        s2 = Ring(p, "s2", [64, 128], n=9)
        psA = Ring(p, "psA", [128, 128], n=8, psum=True)
        def unit(d, h, c, gdt, gdk):
            t0 = c * 128
            tend = 127 if d == 0 else 0
            Sk = f"S{d}{h}"
            St = S[d][h]
            qt, qkk = qk.next()
            p.dma("sp", lambda e, qt=qt, h=h, t0=t0: e.dma_start(out=qt[:, 0, :], in_=PT[h * 64:(h + 1) * 64, t0:t0 + 128]), w=[qkk])
            p.dma("sp", lambda e, qt=qt, h=h, t0=t0: e.dma_start(out=qt[:, 1, :], in_=PT[256 + h * 64:256 + (h + 1) * 64, t0:t0 + 128]), w=[qkk])
            vt, vk = vv.next()
            p.dma("sp", lambda e, vt=vt, h=h, t0=t0: e.dma_start(out=vt[:], in_=PTOK[t0:t0 + 128, h * 128:(h + 1) * 128]), w=[vk])
            yield
            ps, pk = psA.next()
            p.op("pe", lambda e, ps=ps, d=d, h=h, gdt=gdt: e.matmul(ps[0:64, :], lhsT=gkup[:, d, h * 64:(h + 1) * 64], rhs=gdt[:],
                                                                    start=True, stop=True), r=["gkw", gdk], w=[pk])
            sgt, sgk = sg.next()
            p.op("act", lambda e, sgt=sgt, ps=ps, d=d, h=h: e.activation(out=sgt[:], in_=ps[0:64, :], func=AF.Sigmoid,
                                                                        bias=gkb[:, d, h:h + 1]), r=[pk, "gkw"], w=[sgk])
            lgt, lgk = lg.next()
            p.op("act", lambda e, lgt=lgt, sgt=sgt: e.activation(out=lgt[:], in_=sgt[:], func=AF.Ln), r=[sgk], w=[lgk])
            yield
            ps, pk = psA.next()
            p.op("pe", lambda e, ps=ps, lgt=lgt: e.transpose(ps[:, 0:64], lgt[:], C["ident"][0:64, 0:64]), r=[lgk, "ident"], w=[pk])
            lTt, lTk = lgT.next()
            p.op("dve", lambda e, lTt=lTt, ps=ps: e.tensor_copy(out=lTt[:], in_=ps[:, 0:64]), r=[pk], w=[lTk])
            yield
            ps, pk = psA.next()
            p.op("pe", lambda e, ps=ps, lTt=lTt, d=d: e.matmul(ps[0:64, :], lhsT=lTt[:], rhs=C[f"incl{d}"][:], start=True, stop=True),
                 r=[lTk, "masks"], w=[pk])
            eWt, eWk = eW.next()
            eWit, eWik = eWi.next()
            p.op("act", lambda e, eWt=eWt, ps=ps: e.activation(out=eWt[:], in_=ps[0:64, :], func=AF.Exp, scale=1.0 / 16), r=[pk], w=[eWk])
            p.op("act", lambda e, eWit=eWit, ps=ps: e.activation(out=eWit[:], in_=ps[0:64, :], func=AF.Exp, scale=-1.0 / 16), r=[pk], w=[eWik])
            rtt, rtk = rt.next()
            ktt, ktk = kt.next()
            p.op("dve", lambda e, rtt=rtt, qt=qt, eWt=eWt: e.scalar_tensor_tensor(out=rtt[:], in0=qt[:, 0, :], scalar=0.125, in1=eWt[:],
                                                                                 op0=ALU.mult, op1=ALU.mult), r=[qkk, eWk], w=[rtk])
            p.op("dve", lambda e, ktt=ktt, qt=qt, eWit=eWit: e.tensor_tensor(out=ktt[:], in0=qt[:, 1, :], in1=eWit[:], op=ALU.mult),
                 r=[qkk, eWik], w=[ktk])
            yield
            ps, pk = psA.next()
            p.op("pe", lambda e, ps=ps, ktt=ktt: e.transpose(ps[:, 0:64], ktt[:], C["ident"][0:64, 0:64]), r=[ktk, "ident"], w=[pk])
            kTt, kTk = ktok.next()
            p.op("dve", lambda e, kTt=kTt, ps=ps: e.tensor_copy(out=kTt[:], in_=ps[:, 0:64]), r=[pk], w=[kTk])
            yield
            ps, pk = psA.next()
            p.op("pe", lambda e, ps=ps, ktt=ktt, rtt=rtt: e.matmul(ps[:], lhsT=ktt[:], rhs=rtt[:], start=True, stop=True), r=[ktk, rtk], w=[pk])
            mt, mk = mrk.next()
            p.op("dve", lambda e, mt=mt, ps=ps, d=d: e.tensor_tensor(out=mt[:], in0=ps[:], in1=C[f"incl{d}"][:], op=ALU.mult),
                 r=[pk, "masks"], w=[mk])
            yield
            ps, pk = psA.next()
            p.op("pe", lambda e, ps=ps, St=St, rtt=rtt: e.matmul(ps[:], lhsT=St[:], rhs=rtt[:], start=True, stop=False), r=[Sk, rtk], w=[pk])
            p.op("pe", lambda e, ps=ps, vt=vt, mt=mt: e.matmul(ps[:], lhsT=vt[:], rhs=mt[:], start=False, stop=True), r=[vk, mk], w=[pk])
            yt, yk = yts.next()
            p.op("act", lambda e, yt=yt, ps=ps: e.copy(out=yt[:], in_=ps[:]), r=[pk], w=[yk])
            p.dma("sp", lambda e, yt=yt, d=d, h=h, t0=t0: e.dma_start(out=YT[d, h * 128:(h + 1) * 128, t0:t0 + 128], in_=yt[:]),
                  r=[yk], w=["YTG"])
            yield
            ps, pk = psA.next()
            p.op("pe", lambda e, ps=ps, kTt=kTt, vt=vt: e.matmul(ps[0:64, :], lhsT=kTt[:], rhs=vt[:], start=True, stop=True), r=[kTk, vk], w=[pk])
            s2t, s2k = s2.next()
            p.op("dve", lambda e, s2t=s2t, St=St, eWt=eWt, tend=tend: e.tensor_scalar(out=s2t[:], in0=St[:], scalar1=eWt[:, tend:tend + 1],
                                                                                     scalar2=None, op0=ALU.mult), r=[Sk, eWk], w=[s2k])
            p.op("dve", lambda e, s2t=s2t, St=St, eWt=eWt, ps=ps, tend=tend: e.scalar_tensor_tensor(
                out=St[:], in0=ps[0:64, :], scalar=eWt[:, tend:tend + 1], in1=s2t[:], op0=ALU.mult, op1=ALU.add),
                r=[pk, eWk, s2k], w=[Sk])


        def drive(gens):
            gens = list(gens)
            while gens:
                for g_ in list(gens):
                    try:
                        next(g_)
                    except StopIteration:
                        gens.remove(g_)

        for i in range(nt):
            gens = []
            for d in range(2):
                c = order[d][i]
                t0 = c * 128
                gdt, gdk = gd.next()
                p.dma("sp", lambda e, gdt=gdt, t0=t0: e.dma_start(out=gdt[:], in_=PT[1536:1552, t0:t0 + 128]), w=[gdk])
                gens += [unit(d, h, c, gdt, gdk) for h in range(4)]
            drive(gens)


def phase_gla_finish(p, C, io, n_tok):
    PT, YT, MIXT = io["PT"], io["YTG"], io["MIXT"]
    with p.phase():
        gcol = p.sb("gng", [128, 1], F32)
        p.dma("sp", lambda e: e.dma_start(out=gcol[:, 0:1], in_=io["gla_norm_g"].rearrange("(q o) -> q o", o=1)), w=["gng"])
        y0 = Ring(p, "y0", [128, 512], n=2)
        y1 = Ring(p, "y1", [128, 512], n=2)
        og = Ring(p, "og", [128, 512], n=2)
        sq = Ring(p, "sq", [128, 512], n=2)
        rs = Ring(p, "rs", [128, 512], n=2)
        ob = Ring(p, "ob", [128, 512], BF16, n=2)
        psF = Ring(p, "psF", [128, 512], n=2, psum=True)
        for t0 in range(0, n_tok, 512):
            N = min(512, n_tok - t0)
            for h in range(4):
                a, ak = y0.next()
                b, bk = y1.next()
                o, ok = og.next()
                p.dma("sp", lambda e, a=a, h=h, t0=t0, N=N: e.dma_start(out=a[:, 0:N], in_=YT[0, h * 128:(h + 1) * 128, t0:t0 + N]), r=["YTG"], w=[ak])
                p.dma("sp", lambda e, b=b, h=h, t0=t0, N=N: e.dma_start(out=b[:, 0:N], in_=YT[1, h * 128:(h + 1) * 128, t0:t0 + N]), r=["YTG"], w=[bk])
                p.dma("sp", lambda e, o=o, h=h, t0=t0, N=N: e.dma_start(out=o[:, 0:N], in_=PT[1024 + h * 128:1024 + (h + 1) * 128, t0:t0 + N]), r=["PT"], w=[ok])
                p.op("dve", lambda e, a=a, b=b, N=N: e.tensor_tensor(out=a[:, 0:N], in0=a[:, 0:N], in1=b[:, 0:N], op=ALU.add), r=[ak, bk], w=[ak])
                s, sk = sq.next()
                p.op("act", lambda e, s=s, a=a, N=N: e.activation(out=s[:, 0:N], in_=a[:, 0:N], func=AF.Square), r=[ak], w=[sk])
                ps, pk = psF.next()
                p.op("pe", lambda e, ps=ps, s=s, N=N: e.matmul(ps[:, 0:N], lhsT=C["ones128"][:], rhs=s[:, 0:N], start=True, stop=True), r=[sk, "masks"], w=[pk])
                r_, rk = rs.next()
                p.op("dve", lambda e, r_=r_, ps=ps, N=N: e.tensor_scalar(out=r_[:, 0:N], in0=ps[:, 0:N], scalar1=EPS, scalar2=None, op0=ALU.add), r=[pk], w=[rk])
                p.op("act", lambda e, r_=r_, N=N: e.activation(out=r_[:, 0:N], in_=r_[:, 0:N], func=AF.Sqrt), r=[rk], w=[rk])
                p.op("dve", lambda e, r_=r_, N=N: e.reciprocal(out=r_[:, 0:N], in_=r_[:, 0:N]), r=[rk], w=[rk])
                p.op("dve", lambda e, a=a, r_=r_, N=N: e.scalar_tensor_tensor(out=a[:, 0:N], in0=a[:, 0:N], scalar=gcol[:, 0:1], in1=r_[:, 0:N],
                                                                             op0=ALU.mult, op1=ALU.mult), r=[ak, rk, "gng"], w=[ak])
                p.op("act", lambda e, s=s, o=o, N=N: e.activation(out=s[:, 0:N], in_=o[:, 0:N], func=AF.Silu), r=[ok], w=[sk])
                ot, otk = ob.next()
                p.op("dve", lambda e, ot=ot, a=a, s=s, N=N: e.tensor_tensor(out=ot[:, 0:N], in0=a[:, 0:N], in1=s[:, 0:N], op=ALU.mult), r=[ak, sk], w=[otk])
                p.dma("sp", lambda e, ot=ot, h=h, t0=t0, N=N: e.dma_start(out=MIXT[h * 128:(h + 1) * 128, t0:t0 + N], in_=ot[:, 0:N]), r=[otk], w=["MIXT"])


import os
STAGE = 99
R0 = GLA_COLS
NSQ = 12
NLW = 0.6065306597126334


def phase_shift(p, C, io, n_ctx, n_tok):
    PT, SMT, mu = io["PT"], io["SMT"], io["rw_mu"]
    with p.phase():
        nch = (RW_COLS + 127) // 128
        muc = p.sb("muc", [128, nch, 2], F32)
        p.op("pool", lambda e: e.memset(muc[:], 0.0), w=["muc"])
        for j in range(2):
            for c in range(nch):
                cw = min(128, RW_COLS - c * 128)
                p.dma("sp", lambda e, j=j, c=c, cw=cw: e.dma_start(out=muc[0:cw, c, j:j + 1],
                                                                   in_=mu[j, c * 128:c * 128 + cw].rearrange("(q o) -> q o", o=1)), w=["muc"])
        xin = Ring(p, "shx", [128, 514], n=3)
        d0 = Ring(p, "shd", [128, 512], n=3)
        so = Ring(p, "sho", [128, 512], n=3)
        blocks = []
        for (a, b) in ((0, n_ctx), (n_ctx, n_tok)):
            t = a
            while t < b:
                N = min(512, b - t)
                blocks.append((t, N, t == a, t + N == b))
                t += N
        for (t0, N, first, last) in blocks:
            for c in range(nch):
                cw = min(128, RW_COLS - c * 128)
                x, xk = xin.next()
                lo = 0 if not first else 1
                hi = N + 2 if not last else N + 1
                if first or last:
                    p.op("pool", lambda e, x=x: e.memset(x[:], 0.0), w=[xk])
                p.dma("sp", lambda e, x=x, c=c, cw=cw, lo=lo, hi=hi, t0=t0: e.dma_start(
                    out=x[0:cw, lo:hi], in_=PT[R0 + c * 128:R0 + c * 128 + cw, t0 - 1 + lo:t0 - 1 + hi]), w=[xk])
                dd, dk = d0.next()
                o, ok = so.next()
                p.op("dve", lambda e, dd=dd, x=x, cw=cw, N=N: e.tensor_tensor(out=dd[0:cw, 0:N], in0=x[0:cw, 0:N], in1=x[0:cw, 1:N + 1], op=ALU.subtract),
                     r=[xk], w=[dk])
                p.op("dve", lambda e, o=o, dd=dd, x=x, c=c, cw=cw, N=N: e.scalar_tensor_tensor(
                    out=o[0:cw, 0:N], in0=dd[0:cw, 0:N], scalar=muc[0:cw, c, 0:1], in1=x[0:cw, 1:N + 1], op0=ALU.mult, op1=ALU.add),
                    r=[dk, xk, "muc"], w=[ok])
                p.op("pool", lambda e, dd=dd, x=x, cw=cw, N=N: e.tensor_tensor(out=dd[0:cw, 0:N], in0=x[0:cw, 2:N + 2], in1=x[0:cw, 1:N + 1], op=ALU.subtract),
                     r=[xk, ok], w=[dk])
                p.op("dve", lambda e, o=o, dd=dd, c=c, cw=cw, N=N: e.scalar_tensor_tensor(
                    out=o[0:cw, 0:N], in0=dd[0:cw, 0:N], scalar=muc[0:cw, c, 1:2], in1=o[0:cw, 0:N], op0=ALU.mult, op1=ALU.add),
                    r=[dk, ok, "muc"], w=[ok])
                p.dma("sp", lambda e, o=o, c=c, cw=cw, N=N, t0=t0: e.dma_start(out=SMT[c * 128:c * 128 + cw, t0:t0 + N], in_=o[0:cw, 0:N]),
                      r=[ok], w=["SMT"])


def colload(p, dst, key, src_vec, nh, j=None):
    out = dst[:, :] if j is None else dst[:, j, :]
    p.dma("sp", lambda e: e.dma_start(out=out, in_=src_vec.rearrange("(h q) -> q h", q=64), allow_slow_non_contiguous=True), w=[key])


def phase_rwkv(p, C, io, n_ctx, n_tok):
    SMT, YT, RK = io["SMT"], io["YTR"], io["RK"]
    order = chunk_order(n_ctx, n_tok)
    nt = n_tok // 128
    with p.phase():
        w2 = p.sb("rw2", [64, 2, 512], F32)
        a2 = p.sb("ra2", [64, 2, 512], F32)
        for d in range(2):
            p.dma("sp", lambda e, d=d: e.dma_start(out=w2[:, d, :], in_=io["rw_w2"][d]), w=["rww"])
            p.dma("sp", lambda e, d=d: e.dma_start(out=a2[:, d, :], in_=io["rw_a2"][d]), w=["rww"])
        w0 = p.sb("rw0", [64, 2, 8], F32)
        a0 = p.sb("ra0", [64, 2, 8], F32)
        for d in range(2):
            colload(p, w0, "rww", io["rw_w0"][d], 8, d)
            colload(p, a0, "rww", io["rw_a0"][d], 8, d)
        kkc = p.sb("rkk", [64, 8], F32)
        kac = p.sb("rka", [64, 8], F32)
        kam = p.sb("rkam", [64, 8], F32)
        rkc = p.sb("rrk", [64, 8], F32)
        colload(p, kkc, "rww", io["rw_k_k"], 8)
        colload(p, kac, "rww", io["rw_k_a"], 8)
        colload(p, rkc, "rww", io["rw_r_k"].rearrange("h n -> (h n)"), 8)
        p.op("dve", lambda e: e.tensor_scalar(out=kam[:], in0=kac[:], scalar1=-1.0, scalar2=1.0, op0=ALU.mult, op1=ALU.add), r=["rww"], w=["rkam"])
        T = [[p.sb(f"T{d}{h}", [64, 64], F32) for h in range(8)] for d in range(2)]
        for d in range(2):
            for h in range(8):
                p.op("pool", lambda e, t=T[d][h]: e.memset(t[:], 0.0), w=[f"T{d}{h}"])
        wa = Ring(p, "wa", [64, 2, 128], n=3)
        rkv = Ring(p, "rkv", [64, 3, 128], n=9)
        f64 = {nm: Ring(p, nm, [64, 128], n=9) for nm in
               ("sgw", "alp", "kq", "sqk", "nrm", "kk", "tmk", "kd", "eW", "eWi", "eWx", "cx", "rt", "kt", "bt", "at", "rk_", "yo")}
        tokr = {nm: Ring(p, nm, [128, 64], n=9) for nm in ("sgT", "Ktok", "Btok", "Vtok", "Xs", "Us")}
        sqm = {nm: Ring(p, nm, [128, 128], n=9) for nm in ("Akt", "Mrbt", "Mrkt")}
        sqh = [{nm: Ring(p, f"{nm}h{h_}", [128, 128], n=3) for nm in ("At", "Am", "Pt")} for h_ in range(8)]
        tw = Ring(p, "tw", [64, 64], n=9)
        psA = PsRing(p, "psR", 8)

        def mm(out_ap, pk, lhsT, rhs, r, start=True, stop=True):
            p.op("pe", lambda e: e.matmul(out_ap, lhsT=lhsT, rhs=rhs, start=start, stop=stop), r=r, w=[pk])

        def tr(out_ap, pk, in_ap, n, r):
            p.op("pe", lambda e: e.transpose(out_ap, in_ap, C["ident"][0:n, 0:n]), r=r + ["ident"], w=[pk])

        def unit(d, h, c, wat, wak):
            t0 = c * 128
            tend = 127 if d == 0 else 0
            sl = slice(t0, t0 + 128)
            hs = slice(h * 64, (h + 1) * 64)
            Tt, Tk = T[d][h], f"T{d}{h}"
            x, xk = rkv.next()
            for j in range(3):
                p.dma("sp", lambda e, x=x, j=j, h=h, sl=sl: e.dma_start(out=x[:, j, :], in_=SMT[j * 512 + h * 64:j * 512 + (h + 1) * 64, sl]), w=[xk])
            r_, k_, v_ = x[:, 0, :], x[:, 1, :], x[:, 2, :]
            yield
            ps, pk = psA.next()
            mm(ps[0:64, :], pk, w2[:, d, hs], wat[:, 0, :], ["rww", wak])
            sgw, sgwk = f64["sgw"].next()
            p.op("act", lambda e, sgw=sgw, ps=ps, d=d, h=h: e.activation(out=sgw[:], in_=ps[0:64, :], func=AF.Sigmoid, bias=w0[:, d, h:h + 1]),
                 r=[pk, "rww"], w=[sgwk])
            yield
            ps, pk = psA.next()
            mm(ps[0:64, :], pk, a2[:, d, hs], wat[:, 1, :], ["rww", wak])
            alp, alpk = f64["alp"].next()
            p.op("act", lambda e, alp=alp, ps=ps, d=d, h=h: e.activation(out=alp[:], in_=ps[0:64, :], func=AF.Sigmoid, bias=a0[:, d, h:h + 1]),
                 r=[pk, "rww"], w=[alpk])
            kq, kqk = f64["kq"].next()
            p.op("dve", lambda e, kq=kq, k_=k_, h=h: e.tensor_scalar(out=kq[:], in0=k_, scalar1=kkc[:, h:h + 1], scalar2=None, op0=ALU.mult),
                 r=[xk, "rww"], w=[kqk])
            sqk, sqkk = f64["sqk"].next()
            p.op("act", lambda e, sqk=sqk, kq=kq: e.activation(out=sqk[:], in_=kq[:], func=AF.Square), r=[kqk], w=[sqkk])
            yield
            ps, pk = psA.next()
            mm(ps[0:64, :], pk, C["ones64"][0:64, 0:64], sqk[:], ["masks", sqkk])
            nrm, nrmk = f64["nrm"].next()
            p.op("act", lambda e, nrm=nrm, ps=ps: e.activation(out=nrm[:], in_=ps[0:64, :], func=AF.Sqrt), r=[pk], w=[nrmk])
            p.op("dve", lambda e, nrm=nrm: e.tensor_scalar(out=nrm[:], in0=nrm[:], scalar1=1e-12, scalar2=None, op0=ALU.max), r=[nrmk], w=[nrmk])
            p.op("dve", lambda e, nrm=nrm: e.reciprocal(out=nrm[:], in_=nrm[:]), r=[nrmk], w=[nrmk])
            kk, kkk = f64["kk"].next()
            p.op("dve", lambda e, kk=kk, kq=kq, nrm=nrm: e.tensor_tensor(out=kk[:], in0=kq[:], in1=nrm[:], op=ALU.mult), r=[kqk, nrmk], w=[kkk])
            tmk, tmkk = f64["tmk"].next()
            p.op("dve", lambda e, tmk=tmk, alp=alp, h=h: e.tensor_scalar(out=tmk[:], in0=alp[:], scalar1=kac[:, h:h + 1], scalar2=kam[:, h:h + 1],
                                                                        op0=ALU.mult, op1=ALU.add), r=[alpk, "rww", "rkam"], w=[tmkk])
            kd, kdk = f64["kd"].next()
            p.op("dve", lambda e, kd=kd, tmk=tmk, k_=k_: e.tensor_tensor(out=kd[:], in0=tmk[:], in1=k_, op=ALU.mult), r=[tmkk, xk], w=[kdk])
            rk_, rkk = f64["rk_"].next()
            p.op("dve", lambda e, rk_=rk_, r_=r_, kd=kd, h=h: e.scalar_tensor_tensor(out=rk_[:], in0=r_, scalar=rkc[:, h:h + 1], in1=kd[:],
                                                                                     op0=ALU.mult, op1=ALU.mult), r=[xk, kdk, "rww"], w=[rkk])
            p.dma("sp", lambda e, rk_=rk_, d=d, hs=hs, sl=sl: e.dma_start(out=RK[d, hs, sl], in_=rk_[:]), r=[rkk], w=["RK"])
            yield
            ps, pk = psA.next()
            tr(ps[:, 0:64], pk, sgw[:], 64, [sgwk])
            sgT, sgTk = tokr["sgT"].next()
            p.op("dve", lambda e, sgT=sgT, ps=ps: e.tensor_copy(out=sgT[:], in_=ps[:, 0:64]), r=[pk], w=[sgTk])
            yield
            ps, pk = psA.next()
            mm(ps[0:64, :], pk, sgT[:], C[f"incl{d}"][:], [sgTk, "masks"])
            eW, eWk = f64["eW"].next()
            eWi, eWik = f64["eWi"].next()
            cx, cxk = f64["cx"].next()
            eWx, eWxk = f64["eWx"].next()
            p.op("act", lambda e, eW=eW, ps=ps: e.activation(out=eW[:], in_=ps[0:64, :], func=AF.Exp, scale=-NLW), r=[pk], w=[eWk])
            p.op("act", lambda e, eWi=eWi, ps=ps: e.activation(out=eWi[:], in_=ps[0:64, :], func=AF.Exp, scale=NLW), r=[pk], w=[eWik])
            p.op("dve", lambda e, cx=cx, ps=ps, sgw=sgw: e.tensor_tensor(out=cx[:], in0=ps[0:64, :], in1=sgw[:], op=ALU.subtract), r=[pk, sgwk], w=[cxk])
            p.op("act", lambda e, eWx=eWx, cx=cx: e.activation(out=eWx[:], in_=cx[:], func=AF.Exp, scale=-NLW), r=[cxk], w=[eWxk])
            rt, rtk = f64["rt"].next()
            kt, ktk = f64["kt"].next()
            bt, btk = f64["bt"].next()
            at, atk = f64["at"].next()
            p.op("dve", lambda e, rt=rt, r_=r_, eW=eW: e.tensor_tensor(out=rt[:], in0=r_, in1=eW[:], op=ALU.mult), r=[xk, eWk], w=[rtk])
            p.op("pool", lambda e, kt=kt, kd=kd, eWi=eWi: e.tensor_tensor(out=kt[:], in0=kd[:], in1=eWi[:], op=ALU.mult), r=[kdk, eWik], w=[ktk])
            p.op("dve", lambda e, bt=bt, kk=kk, alp=alp: e.tensor_tensor(out=bt[:], in0=kk[:], in1=alp[:], op=ALU.mult), r=[kkk, alpk], w=[btk])
            p.op("dve", lambda e, bt=bt, eWi=eWi: e.tensor_tensor(out=bt[:], in0=bt[:], in1=eWi[:], op=ALU.mult), r=[btk, eWik], w=[btk])
            p.op("dve", lambda e, at=at, kk=kk, eWx=eWx: e.scalar_tensor_tensor(out=at[:], in0=kk[:], scalar=-1.0, in1=eWx[:], op0=ALU.mult, op1=ALU.mult),
                 r=[kkk, eWxk], w=[atk])
            toks = {}
            for nm, src, sk in (("Ktok", kt[:], ktk), ("Btok", bt[:], btk), ("Vtok", v_, xk)):
                yield
                ps, pk = psA.next()
                tr(ps[:, 0:64], pk, src, 64, [sk])
                tt, ttk = tokr[nm].next()
                p.op("act" if nm != "Vtok" else "dve", (lambda e, tt=tt, ps=ps: e.copy(out=tt[:], in_=ps[:, 0:64])) if nm != "Vtok" else
                     (lambda e, tt=tt, ps=ps: e.tensor_copy(out=tt[:], in_=ps[:, 0:64])), r=[pk], w=[ttk])
                toks[nm] = (tt, ttk)
            Ktok, Ktokk = toks["Ktok"]
            Btok, Btokk = toks["Btok"]
            Vtok, Vtokk = toks["Vtok"]
            def gram(nm, lhsT, lk, rhs, rk2, mask):
                ps, pk = psA.next()
                mm(ps[:, :], pk, lhsT, rhs, [lk, rk2])
                m, mk = (sqh[h][nm] if nm in sqh[h] else sqm[nm]).next()
                p.op("dve", lambda e: e.tensor_tensor(out=m[:], in0=ps[:, :], in1=C[mask][:], op=ALU.mult), r=[pk, "masks"], w=[mk])
                return m, mk
            At, Atk = gram("At", bt[:], btk, at[:], atk, f"strict{d}")
            yield
            Am, Amk = gram("Am", at[:], atk, bt[:], btk, f"strict{1 - d}")
            yield
            Akt, Aktk = gram("Akt", kt[:], ktk, at[:], atk, f"strict{d}")
            yield
            Mrbt, Mrbtk = gram("Mrbt", bt[:], btk, rt[:], rtk, f"incl{d}")
            yield
            Mrkt, Mrktk = gram("Mrkt", kt[:], ktk, rt[:], rtk, f"incl{d}")
            yield
            Pt, Ptk = sqh[h]["Pt"].next()
            p.op("pool", lambda e, Pt=Pt, At=At: e.tensor_tensor(out=Pt[:], in0=At[:], in1=C["ident"][:], op=ALU.add), r=[Atk, "ident"], w=[Ptk])
            for step in range(6):
                yield
                ps, pk = psA.next()
                mm(ps[:, :], pk, At[:], Am[:], [Atk, Amk])
                Am2, Am2k = sqh[h]["Am"].next()
                p.op("act", lambda e, Am2=Am2, ps=ps: e.copy(out=Am2[:], in_=ps[:, :]), r=[pk], w=[Am2k])
                if step < 5:
                    yield
                    ps, pk = psA.next()
                    mm(ps[:, :], pk, Am[:], At[:], [Atk, Amk])
                    At2, At2k = sqh[h]["At"].next()
                    p.op("dve", lambda e, At2=At2, ps=ps: e.tensor_copy(out=At2[:], in_=ps[:, :]), r=[pk], w=[At2k])
                yield
                ps, pk = psA.next()
                mm(ps[:, :], pk, Am2[:], Pt[:], [Am2k, Ptk])
                Pt2, Pt2k = sqh[h]["Pt"].next()
                p.op("dve", lambda e, Pt2=Pt2, ps=ps, Pt=Pt: e.tensor_tensor(out=Pt2[:], in0=ps[:, :], in1=Pt[:], op=ALU.add), r=[pk, Ptk], w=[Pt2k])
                Pt, Ptk = Pt2, Pt2k
                Am, Amk = Am2, Am2k
                if step < 5:
                    At, Atk = At2, At2k
            yield
            ps, pk = psA.next()
            mm(ps[:, 0:64], pk, at[:], Tt[:], [atk, Tk], start=True, stop=False)
            mm(ps[:, 0:64], pk, Akt[:], Vtok[:], [Aktk, Vtokk], start=False, stop=True)
            Xs, Xsk = tokr["Xs"].next()
            p.op("act", lambda e, Xs=Xs, ps=ps: e.copy(out=Xs[:], in_=ps[:, 0:64]), r=[pk], w=[Xsk])
            yield
            ps, pk = psA.next()
            mm(ps[:, 0:64], pk, Pt[:], Xs[:], [Ptk, Xsk])
            Us, Usk = tokr["Us"].next()
            p.op("dve", lambda e, Us=Us, ps=ps: e.tensor_copy(out=Us[:], in_=ps[:, 0:64]), r=[pk], w=[Usk])
            yield
            ps, pk = psA.next()
            mm(ps[0:64, :], pk, Tt[:], rt[:], [Tk, rtk], start=True, stop=False)
            mm(ps[0:64, :], pk, Us[:], Mrbt[:], [Usk, Mrbtk], start=False, stop=False)
            mm(ps[0:64, :], pk, Vtok[:], Mrkt[:], [Vtokk, Mrktk], start=False, stop=True)
            yo, yok = f64["yo"].next()
            p.op("act", lambda e, yo=yo, ps=ps: e.copy(out=yo[:], in_=ps[0:64, :]), r=[pk], w=[yok])
            p.dma("sp", lambda e, yo=yo, d=d, hs=hs, sl=sl: e.dma_start(out=YT[d, hs, sl], in_=yo[:]), r=[yok], w=["YTR"])
            yield
            ps, pk = psA.next()
            mm(ps[0:64, 0:64], pk, Btok[:], Us[:], [Btokk, Usk], start=True, stop=False)
            mm(ps[0:64, 0:64], pk, Ktok[:], Vtok[:], [Ktokk, Vtokk], start=False, stop=True)
            t2, t2k = tw.next()
            p.op("dve", lambda e, t2=t2, Tt=Tt, eW=eW, tend=tend: e.tensor_scalar(out=t2[:], in0=Tt[:], scalar1=eW[:, tend:tend + 1], scalar2=None, op0=ALU.mult),
                 r=[Tk, eWk], w=[t2k])
            p.op("dve", lambda e, t2=t2, Tt=Tt, eW=eW, ps=ps, tend=tend: e.scalar_tensor_tensor(
                out=Tt[:], in0=ps[0:64, 0:64], scalar=eW[:, tend:tend + 1], in1=t2[:], op0=ALU.mult, op1=ALU.add), r=[pk, eWk, t2k], w=[Tk])


        def drive(gens):
            gens = list(gens)
            while gens:
                for g_ in list(gens):
                    try:
                        next(g_)
                    except StopIteration:
                        gens.remove(g_)

        for i in range(nt):
            for d in range(2):
                c = order[d][i]
                t0 = c * 128
                tend = 127 if d == 0 else 0
                sl = slice(t0, t0 + 128)
                wat, wak = wa.next()
                p.dma("sp", lambda e, wat=wat, sl=sl: e.dma_start(out=wat[:, 0, :], in_=SMT[1536:1600, sl]), w=[wak])
                p.dma("sp", lambda e, wat=wat, sl=sl: e.dma_start(out=wat[:, 1, :], in_=SMT[1600:1664, sl]), w=[wak])
                p.op("act", lambda e, wat=wat: e.activation(out=wat[:, 0, :], in_=wat[:, 0, :], func=AF.Tanh), r=[wak], w=[wak])
                drive([unit(d, h, c, wat, wak) for h in range(8)])


def phase_rwkv_finish(p, C, io, n_tok):
    SMT, YT, RK, MIXT = io["SMT"], io["YTR"], io["RK"], io["MIXT"]
    with p.phase():
        g2a = p.sb("g2a", [128, 512], F32)
        g2b = p.sb("g2b", [32, 512], F32)
        p.dma("sp", lambda e: e.dma_start(out=g2a[:], in_=io["rw_g2"][0:128, :]), w=["fw"])
        p.dma("sp", lambda e: e.dma_start(out=g2b[:], in_=io["rw_g2"][128:160, :]), w=["fw"])
        lnw = p.sb("lnw", [64, 8], F32)
        lnb = p.sb("lnb", [64, 8], F32)
        colload(p, lnw, "fw", io["rw_ln_w"], 8)
        colload(p, lnb, "fw", io["rw_ln_b"], 8)
        sga = Ring(p, "sga", [128, 512], n=2)
        sgb = Ring(p, "sgb", [32, 512], n=2)
        R = {nm: Ring(p, nm, [64, 512], n=2) for nm in ("fy0", "fy1", "fr0", "fr1", "fv", "fyc", "fsq", "frs", "fbn")}
        ob = Ring(p, "fob", [64, 512], BF16, n=2)
        psF = Ring(p, "psG", [64, 512], n=4, psum=True)
        for t0 in range(0, n_tok, 512):
            N = min(512, n_tok - t0)
            sl = slice(t0, t0 + N)
            a_, ak = sga.next()
            b_, bk = sgb.next()
            p.dma("sp", lambda e, a_=a_, sl=sl, N=N: e.dma_start(out=a_[:, 0:N], in_=SMT[1664:1792, sl]), w=[ak])
            p.dma("sp", lambda e, b_=b_, sl=sl, N=N: e.dma_start(out=b_[:, 0:N], in_=SMT[1792:1824, sl]), w=[bk])
            p.op("act", lambda e, a_=a_, N=N: e.activation(out=a_[:, 0:N], in_=a_[:, 0:N], func=AF.Sigmoid), r=[ak], w=[ak])
            p.op("act", lambda e, b_=b_, N=N: e.activation(out=b_[:, 0:N], in_=b_[:, 0:N], func=AF.Sigmoid), r=[bk], w=[bk])
            for h in range(8):
                hs = slice(h * 64, (h + 1) * 64)
                y0, y0k = R["fy0"].next()
                y1, y1k = R["fy1"].next()
                r0, r0k = R["fr0"].next()
                r1, r1k = R["fr1"].next()
                v, vk = R["fv"].next()
                for (t, k_, src) in ((y0, y0k, YT[0, hs, sl]), (y1, y1k, YT[1, hs, sl]), (r0, r0k, RK[0, hs, sl]), (r1, r1k, RK[1, hs, sl]),
                                     (v, vk, SMT[1024 + h * 64:1024 + (h + 1) * 64, sl])):
                    p.dma("sp", lambda e, t=t, src=src, N=N: e.dma_start(out=t[:, 0:N], in_=src), w=[k_])
                p.op("dve", lambda e, y0=y0, y1=y1, N=N: e.tensor_tensor(out=y0[:, 0:N], in0=y0[:, 0:N], in1=y1[:, 0:N], op=ALU.add), r=[y0k, y1k], w=[y0k])
                p.op("pool", lambda e, r0=r0, r1=r1, N=N: e.tensor_tensor(out=r0[:, 0:N], in0=r0[:, 0:N], in1=r1[:, 0:N], op=ALU.add), r=[r0k, r1k], w=[r0k])
                ps, pk = psF.next()
                p.op("pe", lambda e, ps=ps, y0=y0, N=N: e.matmul(ps[:, 0:N], lhsT=C["ones64m"][0:64, 0:64], rhs=y0[:, 0:N], start=True, stop=True), r=[y0k, "masks"], w=[pk])
                yc, yck = R["fyc"].next()
                p.op("dve", lambda e, yc=yc, y0=y0, ps=ps, N=N: e.tensor_tensor(out=yc[:, 0:N], in0=y0[:, 0:N], in1=ps[:, 0:N], op=ALU.subtract), r=[y0k, pk], w=[yck])
                sq, sqk = R["fsq"].next()
                p.op("act", lambda e, sq=sq, yc=yc, N=N: e.activation(out=sq[:, 0:N], in_=yc[:, 0:N], func=AF.Square), r=[yck], w=[sqk])
                ps, pk = psF.next()
                p.op("pe", lambda e, ps=ps, sq=sq, N=N: e.matmul(ps[:, 0:N], lhsT=C["ones64m"][0:64, 0:64], rhs=sq[:, 0:N], start=True, stop=True), r=[sqk, "masks"], w=[pk])
                rs, rsk = R["frs"].next()
                p.op("dve", lambda e, rs=rs, ps=ps, N=N: e.tensor_scalar(out=rs[:, 0:N], in0=ps[:, 0:N], scalar1=64e-5, scalar2=None, op0=ALU.add), r=[pk], w=[rsk])
                p.op("act", lambda e, rs=rs, N=N: e.activation(out=rs[:, 0:N], in_=rs[:, 0:N], func=AF.Sqrt), r=[rsk], w=[rsk])
                p.op("dve", lambda e, rs=rs, N=N: e.reciprocal(out=rs[:, 0:N], in_=rs[:, 0:N]), r=[rsk], w=[rsk])
                p.op("dve", lambda e, yc=yc, rs=rs, N=N: e.tensor_tensor(out=yc[:, 0:N], in0=yc[:, 0:N], in1=rs[:, 0:N], op=ALU.mult), r=[yck, rsk], w=[yck])
                p.op("dve", lambda e, yc=yc, h=h, N=N: e.tensor_scalar(out=yc[:, 0:N], in0=yc[:, 0:N], scalar1=lnw[:, h:h + 1], scalar2=lnb[:, h:h + 1],
                                                                      op0=ALU.mult, op1=ALU.add), r=[yck, "fw"], w=[yck])
                ps, pk = psF.next()
                p.op("pe", lambda e, ps=ps, r0=r0, N=N: e.matmul(ps[:, 0:N], lhsT=C["ones64"][0:64, 0:64], rhs=r0[:, 0:N], start=True, stop=True), r=[r0k, "masks"], w=[pk])
                bn, bnk = R["fbn"].next()
                p.op("dve", lambda e, bn=bn, ps=ps, v=v, N=N: e.tensor_tensor(out=bn[:, 0:N], in0=ps[:, 0:N], in1=v[:, 0:N], op=ALU.mult), r=[pk, vk], w=[bnk])
                p.op("pool", lambda e, bn=bn, yc=yc, N=N: e.tensor_tensor(out=bn[:, 0:N], in0=bn[:, 0:N], in1=yc[:, 0:N], op=ALU.add), r=[bnk, yck], w=[bnk])
                ps, pk = psF.next()
                p.op("pe", lambda e, ps=ps, a_=a_, hs=hs, N=N: e.matmul(ps[:, 0:N], lhsT=g2a[:, hs], rhs=a_[:, 0:N], start=True, stop=False), r=["fw", ak], w=[pk])
                p.op("pe", lambda e, ps=ps, b_=b_, hs=hs, N=N: e.matmul(ps[:, 0:N], lhsT=g2b[:, hs], rhs=b_[:, 0:N], start=False, stop=True), r=["fw", bk], w=[pk])
                o, ok = ob.next()
                p.op("dve", lambda e, o=o, bn=bn, ps=ps, N=N: e.tensor_tensor(out=o[:, 0:N], in0=ps[:, 0:N], in1=bn[:, 0:N], op=ALU.mult), r=[pk, bnk], w=[ok])
                p.dma("sp", lambda e, o=o, h=h, sl=sl, N=N: e.dma_start(out=MIXT[512 + h * 64:512 + (h + 1) * 64, sl], in_=o[:, 0:N]), r=[ok], w=["MIXT"])


def phase_outproj(p, C, io, l, w_out, n_ctx, n_tok, t_lo=0):
    xres, MIXT, modrow = io["xres"], io["MIXT"], io["modrow"]
    with p.phase():
        wbf = p.sb("woutbf", [128, KC, D], BF16)
        stage = [p.sb(f"wostage{i}", [128, D], F32) for i in range(2)]
        load_cast_weight(p, wbf, "woutbf", w_out, KC, D, stage, "wostage")
        gt = p.sb("gt1row", [128, 2, D], F32)
        for r in range(2):
            p.dma("sp", lambda e, r=r: e.dma_start(out=gt[:, r, :], in_=modrow[l, r, 2048:3072].partition_broadcast(128)), w=["gt1row"])
        mx = Ring(p, "mx", [128, KC, 128], BF16, n=2)
        xt = Ring(p, "oxt", [128, D], n=2)
        tmp = Ring(p, "otmp", [128, D], n=2)
        pso = Ring(p, "pso", [128, 512], n=4, psum=True)
        for ti in range(t_lo // 128, n_tok // 128):
            r = 1 if ti * 128 < n_ctx else 0
            sl = slice(ti * 128, (ti + 1) * 128)
            m, mk = mx.next()
            p.dma("sp", lambda e, m=m, sl=sl: e.dma_start(out=m[:], in_=MIXT[:, sl].rearrange("(c q) t -> q c t", q=128)), w=[mk])
            x, xk = xt.next()
            p.dma("sp", lambda e, x=x, sl=sl: e.dma_start(out=x[:], in_=xres[sl, :]), w=[xk])
            t, tk = tmp.next()
            for n in range(2):
                ps, pk = pso.next()
                for kc in range(KC):
                    p.op("pe", lambda e, ps=ps, m=m, kc=kc, n=n: e.matmul(ps[:], lhsT=m[:, kc, :], rhs=wbf[:, kc, n * 512:(n + 1) * 512],
                                                                          start=(kc == 0), stop=(kc == KC - 1)), r=[mk, "woutbf"], w=[pk])
                p.op("dve", lambda e, t=t, ps=ps, n=n, r=r: e.tensor_tensor(out=t[:, n * 512:(n + 1) * 512], in0=ps[:], in1=gt[:, r, n * 512:(n + 1) * 512], op=ALU.mult),
                     r=[pk, "gt1row"], w=[tk])
            p.op("pool", lambda e, t=t, x=x: e.tensor_tensor(out=t[:], in0=t[:], in1=x[:], op=ALU.add), r=[tk, xk], w=[tk])
            p.dma("sp", lambda e, t=t, sl=sl: e.dma_start(out=xres[sl, :], in_=t[:]), r=[tk], w=["xres"])


def phase_final(p, C, io, n_ctx, n_tok):
    xres, out = io["xres"], io["out"]
    with p.phase():
        g = p.sb("fng", [128, D], F32)
        p.dma("sp", lambda e: e.dma_start(out=g[:], in_=io["final_norm_g"].partition_broadcast(128)), w=["fng"])
        xt = Ring(p, "fxt", [128, D], n=3)
        junk = p.sb("fjunk", [128, D], BF16)
        ss = Ring(p, "fss", [128, 1], n=3)
        rs = Ring(p, "frst", [128, 1], n=3)
        for ti in range(n_ctx // 128, n_tok // 128):
            sl = slice(ti * 128, (ti + 1) * 128)
            x, xk = xt.next()
            s, sk = ss.next()
            r_, rk = rs.next()
            p.dma("sp", lambda e, x=x, sl=sl: e.dma_start(out=x[:], in_=xres[sl, :]), w=[xk])
            p.op("act", lambda e, x=x, s=s: e.activation(out=junk[:], in_=x[:], func=AF.Square, accum_out=s[:, 0:1]), r=[xk], w=["fjunk", sk])
            p.op("dve", lambda e, r_=r_, s=s: e.tensor_scalar(out=r_[:], in0=s[:], scalar1=1.0 / D, scalar2=EPS, op0=ALU.mult, op1=ALU.add), r=[sk], w=[rk])
            p.op("act", lambda e, r_=r_: e.activation(out=r_[:], in_=r_[:], func=AF.Sqrt), r=[rk], w=[rk])
            p.op("dve", lambda e, r_=r_: e.reciprocal(out=r_[:], in_=r_[:]), r=[rk], w=[rk])
            p.op("dve", lambda e, x=x, r_=r_: e.scalar_tensor_tensor(out=x[:], in0=x[:], scalar=r_[:, 0:1], in1=g[:], op0=ALU.mult, op1=ALU.mult),
                 r=[xk, rk, "fng"], w=[xk])
            o0 = ti * 128 - n_ctx
            p.dma("sp", lambda e, x=x, o0=o0: e.dma_start(out=out[o0:o0 + 128, :], in_=x[:]), r=[xk], w=["out"])

RST = 9

DE = 1024
SW_LIMIT = 7.0
SW_ALPHA = 1.702


def phase_moe_cast(p, C, io, l, NE):
    gu, dn, WGU, WDN = io["moe_gu_w"], io["moe_down_w"], io["WGU"], io["WDN"]
    with p.phase():
        sg = Ring(p, "cg32", [128, 2, 2048], n=2)
        sb = Ring(p, "cg16", [128, 2, 2048], BF16, n=2)
        sd = Ring(p, "cd32", [128, 2, 1024], n=2)
        sdb = Ring(p, "cd16", [128, 2, 1024], BF16, n=2)
        engs = ("dve", "pool", "act")
        it = 0
        for e_ in range(NE):
            for k2 in range(4):
                for (src, dst, r32, r16) in ((gu, WGU, sg, sb), (dn, WDN, sd, sdb)):
                    a, ak = r32.next()
                    b, bk = r16.next()
                    rows = slice(k2 * 256, (k2 + 1) * 256)
                    p.dma("sp", lambda e, a=a, src=src, e_=e_, rows=rows: e.dma_start(out=a[:], in_=src[l, e_, rows, :].rearrange("(c q) n -> q c n", q=128)), w=[ak])
                    eng = engs[it % 3]
                    it += 1
                    if eng == "act":
                        p.op("act", lambda e, a=a, b=b: e.copy(out=b[:], in_=a[:]), r=[ak], w=[bk])
                    else:
                        p.op(eng, lambda e, a=a, b=b: e.tensor_copy(out=b[:], in_=a[:]), r=[ak], w=[bk])
                    p.dma("act", lambda e, b=b, dst=dst, e_=e_, rows=rows: e.dma_start(out=dst[e_, rows, :].rearrange("(c q) n -> q c n", q=128), in_=b[:]), r=[bk], w=["WBF"])


def phase_moe_route(p, C, io, G, l, NE, t_lo, t_hi, n_ctx):
    xres, HT, GATE = io["xres"], io["HT"], io["GATE"]
    with p.phase():
        T = norm_scratch(p)
        wr = p.sb("wr", [128, KC, NE], F32)
        p.dma("sp", lambda e: e.dma_start(out=wr[:], in_=io["router_w"][l].rearrange("(c q) n -> q c n", q=128)), w=["wr"])
        br = p.sb("br", [128, NE], F32)
        p.dma("sp", lambda e: e.dma_start(out=br[:], in_=io["router_b"][l].partition_broadcast(128)), w=["wr"])
        xt = Ring(p, "mxt", [128, D], n=2)
        hb = Ring(p, "mhb", [128, KC, 128], BF16, n=2)
        h32 = Ring(p, "mh32", [128, KC, 128], n=2)
        lg = Ring(p, "mlg", [128, NE], n=2)
        ex = Ring(p, "mex", [128, NE], n=2)
        mk = Ring(p, "mmk", [128, NE], n=2)
        m8 = Ring(p, "mm8", [128, 8], n=2)
        sc = Ring(p, "msc", [128, 2], n=2)
        psr = p.ps("psr", [128, NE], F32)
        for ti in range(t_lo // 128, t_hi // 128):
            r = 1 if ti * 128 < n_ctx else 0
            sl = slice(ti * 128, (ti + 1) * 128)
            x, xk = xt.next()
            p.dma("sp", lambda e, x=x, sl=sl: e.dma_start(out=x[:], in_=xres[sl, :]), w=[xk])
            b, bk = hb.next()
            f, fk = h32.next()
            norm_tile(p, C, T, xk, x, G["A2"][:, l, r, :], G["B2"][:, l, r, :], lambda kc, b=b: b[:, kc, :], bk,
                      hT32_dst=lambda kc, f=f: f[:, kc, :])
            fk32 = bk + "32"
            p.dma("sp", lambda e, b=b, sl=sl: e.dma_start(out=HT[:, sl].rearrange("(c q) t -> q c t", q=128), in_=b[:]), r=[bk], w=["HT"])
            if RST < 2:
                continue
            for kc in range(KC):
                p.op("pe", lambda e, f=f, kc=kc: e.matmul(psr[:], lhsT=f[:, kc, :], rhs=wr[:, kc, :], start=(kc == 0), stop=(kc == KC - 1)),
                     r=[fk32, "wr"], w=["psr"])
            g, gk = lg.next()
            p.op("dve", lambda e, g=g: e.tensor_tensor(out=g[:], in0=psr[:], in1=br[:], op=ALU.add), r=["psr", "wr"], w=[gk])
            if RST < 3:
                continue
            m, mk8 = m8.next()
            p.op("dve", lambda e, m=m, g=g: e.max(out=m[:], in_=g[:]), r=[gk], w=[mk8])
            msk, mskk = mk.next()
            p.op("dve", lambda e, msk=msk, g=g, m=m: e.tensor_scalar(out=msk[:], in0=g[:], scalar1=m[:, 3:4], scalar2=None, op0=ALU.is_ge), r=[gk, mk8], w=[mskk])
            if RST < 4:
                continue
            s, sk = sc.next()
            p.op("dve", lambda e, s=s, m=m: e.tensor_scalar(out=s[:, 0:1], in0=m[:, 0:1], scalar1=-1.0, scalar2=None, op0=ALU.mult), r=[mk8], w=[sk])
            x_, xk_ = ex.next()
            p.op("act", lambda e, x_=x_, g=g, s=s: e.activation(out=x_[:], in_=g[:], func=AF.Exp, bias=s[:, 0:1]), r=[gk, sk], w=[xk_])
            p.op("dve", lambda e, x_=x_, msk=msk: e.tensor_tensor(out=x_[:], in0=x_[:], in1=msk[:], op=ALU.mult), r=[xk_, mskk], w=[xk_])
            p.op("dve", lambda e, s=s, x_=x_: e.reduce_sum(out=s[:, 1:2], in_=x_[:], axis=AX.X), r=[xk_], w=[sk])
            p.op("dve", lambda e, s=s: e.reciprocal(out=s[:, 1:2], in_=s[:, 1:2]), r=[sk], w=[sk])
            p.op("dve", lambda e, x_=x_, s=s: e.tensor_scalar(out=x_[:], in0=x_[:], scalar1=s[:, 1:2], scalar2=None, op0=ALU.mult), r=[xk_, sk], w=[xk_])
            p.dma("sp", lambda e, x_=x_, sl=sl: e.dma_start(out=GATE[sl, :], in_=x_[:]), r=[xk_], w=["GATE"])


def phase_moe_experts(p, C, io, l, NE, t_lo, t_hi, n_ctx, TB=512):
    xres, HT, GATE, WGU, WDN, modrow = io["xres"], io["HT"], io["GATE"], io["WGU"], io["WDN"], io["modrow"]
    with p.phase():
        bg_rows = p.sb("bg_rows", [NE, 2048], F32)
        p.dma("sp", lambda e: e.dma_start(out=bg_rows[:], in_=io["moe_gu_b"][l]), w=["bg_rows"])
        bgc = p.sb("bgc", [128, 16, NE], F32)
        pst = p.ps("pst", [128, 16, NE], F32)
        for c in range(16):
            p.op("pe", lambda e, c=c: e.transpose(pst[:, c, :], bg_rows[:, c * 128:(c + 1) * 128], C["ident"][0:NE, 0:NE]), r=["bg_rows", "ident"], w=["pst"])
        p.op("dve", lambda e: e.tensor_copy(out=bgc[:], in_=pst[:]), r=["pst"], w=["bgc"])
        ones_b = p.sb("ones_b", [1, 128], BF16)
        p.op("dve", lambda e: e.memset(ones_b[:], 1.0), w=["ones_b"])
        gt = p.sb("gt2row", [128, 2, D], F32)
        for r in range(2):
            p.dma("sp", lambda e, r=r: e.dma_start(out=gt[:, r, :], in_=modrow[l, r, 5120:6144].partition_broadcast(128)), w=["gt2row"])
        wg = Ring(p, "wg", [128, KC, 2048], BF16, n=2)
        wd = Ring(p, "wd", [128, KC, 1024], BF16, n=2)
        bd32 = Ring(p, "bd32", [1, 1024], F32, n=2)
        bd16 = Ring(p, "bd16", [1, 1024], BF16, n=2)
        acc = p.sb("macc", [128, TB // 128, D], F32)
        hT = p.sb("mhT", [128, KC, TB], BF16)
        gate = p.sb("mgate", [128, TB // 128, NE], F32)
        actT = Ring(p, "actT", [128, KC, 512], BF16, n=2)
        t1 = Ring(p, "et1", [128, 512], n=2)
        sgm = Ring(p, "esg", [128, 512], n=2)
        t2 = Ring(p, "et2", [128, 512], n=2)
        xt = Ring(p, "ext", [128, D], n=2)
        psg = Ring(p, "psg", [128, 512], n=4, psum=True)
        psd = Ring(p, "psd", [128, 512], n=3, psum=True)
        t = t_lo
        while t < t_hi:
            nb = min(TB, t_hi - t)
            ntile = nb // 128
            p.dma("sp", lambda e, t=t, nb=nb: e.dma_start(out=hT[:, :, 0:nb], in_=HT[:, t:t + nb].rearrange("(c q) n -> q c n", q=128)), w=["mhT"])
            p.dma("sp", lambda e, t=t, nb=nb, ntile=ntile: e.dma_start(out=gate[:, 0:ntile, :], in_=GATE[t:t + nb, :].rearrange("(j q) n -> q j n", q=128)), w=["mgate"])
            p.op("pool", lambda e: e.memset(acc[:], 0.0), w=["macc"])
            for e_ in range(NE):
                g_, gk = wg.next()
                d_, dk = wd.next()
                b32, b32k = bd32.next()
                b16, b16k = bd16.next()
                p.dma("sp", lambda e, g_=g_, e_=e_: e.dma_start(out=g_[:], in_=WGU[e_].rearrange("(c q) n -> q c n", q=128)), w=[gk])
                p.dma("act", lambda e, d_=d_, e_=e_: e.dma_start(out=d_[:], in_=WDN[e_].rearrange("(c q) n -> q c n", q=128)), w=[dk])
                p.dma("sp", lambda e, b32=b32, e_=e_: e.dma_start(out=b32[:], in_=io["moe_down_b"][l, e_:e_ + 1, :]), w=[b32k])
                p.op("pool", lambda e, b32=b32, b16=b16: e.tensor_copy(out=b16[:], in_=b32[:]), r=[b32k], w=[b16k])
                for s0 in range(0, nb, 512):
                    N = min(512, nb - s0)
                    aT, aTk = actT.next()
                    for c in range(8):
                        pa, pak = psg.next()
                        pb, pbk = psg.next()
                        for (ps_, pk_, col0) in ((pa, pak, c * 128), (pb, pbk, DE + c * 128)):
                            for kc in range(KC):
                                p.op("pe", lambda e, ps_=ps_, g_=g_, kc=kc, col0=col0, s0=s0, N=N: e.matmul(
                                    ps_[:, 0:N], lhsT=g_[:, kc, col0:col0 + 128], rhs=hT[:, kc, s0:s0 + N], start=(kc == 0), stop=(kc == KC - 1)),
                                    r=[gk, "mhT"], w=[pk_])
                        a1, a1k = t1.next()
                        p.op("dve", lambda e, a1=a1, pa=pa, c=c, e_=e_, N=N: e.tensor_scalar(out=a1[:, 0:N], in0=pa[:, 0:N], scalar1=bgc[:, c, e_:e_ + 1], scalar2=SW_LIMIT,
                                                                                          op0=ALU.add, op1=ALU.min), r=[pak, "bgc"], w=[a1k])
                        s_, sk_ = sgm.next()
                        p.op("act", lambda e, s_=s_, a1=a1, N=N: e.activation(out=s_[:, 0:N], in_=a1[:, 0:N], func=AF.Sigmoid, scale=SW_ALPHA), r=[a1k], w=[sk_])
                        a2, a2k = t2.next()
                        p.op("dve", lambda e, a2=a2, pb=pb, c=c, e_=e_, N=N: e.tensor_scalar(out=a2[:, 0:N], in0=pb[:, 0:N], scalar1=bgc[:, 8 + c, e_:e_ + 1], scalar2=SW_LIMIT,
                                                                                          op0=ALU.add, op1=ALU.min), r=[pbk, "bgc"], w=[a2k])
                        p.op("pool", lambda e, a2=a2, N=N: e.tensor_scalar(out=a2[:, 0:N], in0=a2[:, 0:N], scalar1=-SW_LIMIT, scalar2=1.0, op0=ALU.max, op1=ALU.add),
                             r=[a2k], w=[a2k])
                        p.op("pool", lambda e, a1=a1, s_=s_, N=N: e.tensor_tensor(out=a1[:, 0:N], in0=a1[:, 0:N], in1=s_[:, 0:N], op=ALU.mult), r=[a1k, sk_], w=[a1k])
                        p.op("dve", lambda e, aT=aT, a1=a1, a2=a2, c=c, N=N: e.tensor_tensor(out=aT[:, c, 0:N], in0=a1[:, 0:N], in1=a2[:, 0:N], op=ALU.mult),
                             r=[a1k, a2k], w=[aTk])
                    for j in range(N // 128):
                        tile_i = (s0 // 128) + j
                        for n in range(2):
                            pd, pdk = psd.next()
                            for kc in range(KC):
                                p.op("pe", lambda e, pd=pd, aT=aT, d_=d_, kc=kc, j=j, n=n: e.matmul(
                                    pd[:], lhsT=aT[:, kc, j * 128:(j + 1) * 128], rhs=d_[:, kc, n * 512:(n + 1) * 512], start=(kc == 0), stop=False),
                                    r=[aTk, dk], w=[pdk])
                            p.op("pe", lambda e, pd=pd, b16=b16, n=n: e.matmul(pd[:], lhsT=ones_b[:], rhs=b16[:, n * 512:(n + 1) * 512], start=False, stop=True),
                                 r=["ones_b", b16k], w=[pdk])
                            p.op("dve", lambda e, pd=pd, tile_i=tile_i, n=n, e_=e_: e.scalar_tensor_tensor(
                                out=acc[:, tile_i, n * 512:(n + 1) * 512], in0=pd[:], scalar=gate[:, tile_i, e_:e_ + 1],
                                in1=acc[:, tile_i, n * 512:(n + 1) * 512], op0=ALU.mult, op1=ALU.add), r=[pdk, "mgate", "macc"], w=["macc"])
            for j in range(ntile):
                tok = t + j * 128
                r = 1 if tok < n_ctx else 0
                x, xk = xt.next()
                p.dma("sp", lambda e, x=x, tok=tok: e.dma_start(out=x[:], in_=xres[tok:tok + 128, :]), w=[xk])
                p.op("dve", lambda e, j=j, r=r: e.tensor_tensor(out=acc[:, j, :], in0=acc[:, j, :], in1=gt[:, r, :], op=ALU.mult), r=["macc", "gt2row"], w=["macc"])
                p.op("pool", lambda e, x=x, j=j: e.tensor_tensor(out=x[:], in0=x[:], in1=acc[:, j, :], op=ALU.add), r=[xk, "macc"], w=[xk])
                p.dma("sp", lambda e, x=x, tok=tok: e.dma_start(out=xres[tok:tok + 128, :], in_=x[:]), r=[xk], w=["xres"])
            t += nb

import math

MLA_SCALE = 192 ** -0.5


def make_rope_consts(p, C):
    f, q = C["iota_f"], C["iota_p"]
    d = p.sb("rp_d", [64, 64], F32)
    e1 = p.sb("rp_e1", [64, 64], F32)
    e2 = p.sb("rp_e2", [64, 64], F32)
    ge = p.sb("rp_ge", [64, 64], F32)
    PR = p.sb("rp_PR", [64, 64], F32)
    k = ["rope_c"]
    p.op("dve", lambda e: e.tensor_scalar(out=d[:], in0=f[0:64, 0:64], scalar1=q[0:64, 0:1], scalar2=None, op0=ALU.subtract), r=["iota_f", "iota_p"], w=k)
    p.op("dve", lambda e: e.tensor_scalar(out=e1[:], in0=d[:], scalar1=16.0, scalar2=None, op0=ALU.is_equal), r=k, w=k)
    p.op("dve", lambda e: e.tensor_scalar(out=e2[:], in0=d[:], scalar1=-16.0, scalar2=None, op0=ALU.is_equal), r=k, w=k)
    ii = p.sb("rp_ii", [64, 64], I32)
    p.op("pool", lambda e: e.iota(ii[:], pattern=[[1, 64]], base=0, channel_multiplier=0), w=["rp_ii"])
    p.op("dve", lambda e: e.tensor_single_scalar(out=ii[:], in_=ii[:], scalar=16, op=ALU.bitwise_and), r=["rp_ii"], w=["rp_ii"])
    p.op("dve", lambda e: e.tensor_copy(out=ge[:], in_=ii[:]), r=["rp_ii"], w=k)
    p.op("dve", lambda e: e.tensor_scalar(out=ge[:], in0=ge[:], scalar1=1.0 / 16, scalar2=None, op0=ALU.mult), r=k, w=k)
    p.op("dve", lambda e: e.tensor_tensor(out=e1[:], in0=e1[:], in1=ge[:], op=ALU.mult), r=k, w=k)
    p.op("dve", lambda e: e.tensor_scalar(out=ge[:], in0=ge[:], scalar1=-1.0, scalar2=1.0, op0=ALU.mult, op1=ALU.add), r=k, w=k)
    p.op("dve", lambda e: e.tensor_tensor(out=e2[:], in0=e2[:], in1=ge[:], op=ALU.mult), r=k, w=k)
    p.op("dve", lambda e: e.tensor_tensor(out=PR[:], in0=e1[:], in1=e2[:], op=ALU.subtract), r=k, w=k)
    invf = p.sb("rp_invf", [64, 1], F32)
    isrow = p.sb("rp_isrow", [64, 1], F32)
    pi_ = p.sb("rp_pi", [64, 1], I32)
    p.op("pool", lambda e: e.iota(pi_[:], pattern=[[1, 1]], base=0, channel_multiplier=1), w=["rp_pi"])
    p.op("dve", lambda e: e.tensor_single_scalar(out=pi_[:], in_=pi_[:], scalar=15, op=ALU.bitwise_and), r=["rp_pi"], w=["rp_pi"])
    p.op("dve", lambda e: e.tensor_copy(out=invf[:], in_=pi_[:]), r=["rp_pi"], w=k)
    p.op("act", lambda e: e.activation(out=invf[:], in_=invf[:], func=AF.Exp, scale=-math.log(10000.0) / 16.0), r=k, w=k)
    p.op("dve", lambda e: e.tensor_scalar(out=isrow[:], in0=q[0:64, 0:1], scalar1=32.0, scalar2=None, op0=ALU.is_lt), r=["iota_p"], w=k)
    C.update(PR=PR, invf=invf, isrow=isrow)


def phase_mla_proj(p, C, io, G, l, n_ctx, n_tok):
    xres = io["xres"]
    QN, QR, KN, KR, V = io["QN"], io["QR"], io["KN"], io["KRo"], io["Vt"]
    n_lat = n_tok - n_ctx
    with p.phase():
        T = norm_scratch(p)
        win = p.sb("mwin", [128, KC, 448], BF16)
        st = [p.sb(f"mst{i}", [128, 2048], F32) for i in range(2)]
        load_cast_weight(p, win, "mwin", io["od_w_in"], KC, 448, st, "mst")
        wq = p.sb("mwq", [128, 2, 1536], BF16)
        load_cast_weight(p, wq, "mwq", io["mla_wq_up"], 2, 1536, st, "mst")
        wkn = p.sb("mwkn", [128, 8, 128], BF16)
        wv = p.sb("mwv", [128, 8, 128], BF16)
        p.dma("sp", lambda e: e.dma_start(out=st[0][:, 0:2048], in_=io["mla_wkv_up"][:, :]), w=["mst0"])
        p.op("dve", lambda e: e.tensor_copy(out=wkn[:], in_=st[0][:, 0:2048].rearrange("q (h c) -> q h c", c=256)[:, :, 0:128]), r=["mst0"], w=["mwk"])
        p.op("pool", lambda e: e.tensor_copy(out=wv[:], in_=st[0][:, 0:2048].rearrange("q (h c) -> q h c", c=256)[:, :, 128:256]), r=["mst0"], w=["mwk"])
        qg = p.sb("mqg", [128, 2], F32)
        kg = p.sb("mkg", [128, 1], F32)
        p.dma("sp", lambda e: e.dma_start(out=qg[:], in_=io["mla_q_norm"].rearrange("(c q) -> q c", q=128), allow_slow_non_contiguous=True), w=["mg"])
        p.dma("sp", lambda e: e.dma_start(out=kg[:], in_=io["mla_kv_norm"].rearrange("(q o) -> q o", o=1)), w=["mg"])
        onesq = p.sb("monesq", [128, 128], F32)
        p.op("dve", lambda e: e.memset(onesq[:], 1.0 / 256), w=["monesq"])
        xts = Ring(p, "axt", [128, D], n=2)
        hT = p.sb("ahT", [128, KC, 512], BF16)
        cq = p.sb("acq", [128, 2, 512], F32)
        ckv = p.sb("ackv", [128, 512], F32)
        krr = p.sb("akrr", [64, 512], F32)
        sq = p.sb("asq", [128, 2, 512], F32)
        rs = Ring(p, "ars", [128, 512], n=2)
        cqn = p.sb("acqn", [128, 2, 512], BF16)
        ckvn = p.sb("ackvn", [128, 512], BF16)
        ang = p.sb("aang", [64, 512], F32)
        tti = p.sb("atti", [64, 512], I32)
        tt2 = p.sb("att2", [64, 512], I32)
        a2 = p.sb("aa2", [64, 512], F32)
        kf = p.sb("akf", [64, 512], F32)
        cosT = p.sb("acos", [64, 512], F32)
        sinT = p.sb("asin", [64, 512], F32)
        rx = Ring(p, "arx", [64, 512], n=2)
        ru = Ring(p, "aru", [64, 512], n=2)
        ob = Ring(p, "aob", [128, 512], BF16, n=3)
        vb = Ring(p, "avb", [128, 1024], BF16, n=2)
        pp = Ring(p, "app", [128, 512], n=5, psum=True)

        def rope(x, xk, N, scale, dst_ap, dst_key):
            p2, p2k = pp.next()
            p.op("pe", lambda e: e.matmul(p2[0:64, 0:N], lhsT=C["PR"][:], rhs=x, start=True, stop=True), r=["rope_c", xk], w=[p2k])
            u, uk = ru.next()
            p.op("dve", lambda e: e.tensor_tensor(out=u[:, 0:N], in0=p2[0:64, 0:N], in1=sinT[:, 0:N], op=ALU.mult), r=[p2k, "trig"], w=[uk])
            p.op("pool", lambda e: e.tensor_tensor(out=x, in0=x, in1=cosT[:, 0:N], op=ALU.mult), r=[xk, "trig"], w=[xk])
            if scale == 1.0:
                p.op("dve", lambda e: e.tensor_tensor(out=dst_ap, in0=x, in1=u[:, 0:N], op=ALU.add), r=[xk, uk], w=[dst_key])
            else:
                p.op("dve", lambda e: e.tensor_tensor(out=u[:, 0:N], in0=x, in1=u[:, 0:N], op=ALU.add), r=[xk, uk], w=[uk])
                p.op("dve", lambda e: e.tensor_scalar(out=dst_ap, in0=u[:, 0:N], scalar1=scale, scalar2=None, op0=ALU.mult), r=[uk], w=[dst_key])

        blocks = [(0, n_ctx, False)] + [(t, min(512, n_tok - t), True) for t in range(n_ctx, n_tok, 512)]
        def do_block(t0, N, is_lat):
            for j in range(N // 128):
                ti = t0 // 128 + j
                r = 0 if is_lat else 1
                x, xk = xts.next()
                p.dma("sp", lambda e, x=x, ti=ti: e.dma_start(out=x[:], in_=xres[ti * 128:(ti + 1) * 128, :]), w=[xk])
                norm_tile(p, C, T, xk, x, G["A1"][:, l, r, :], G["B1"][:, l, r, :], lambda kc, j=j: hT[:, kc, j * 128:(j + 1) * 128], "ahT")
            for (c0, cw, dst, dk) in ((0, 128, cq[:, 0, :], "acq"), (128, 128, cq[:, 1, :], "acq"), (256, 128, ckv[:, :], "ackv"), (384, 64, krr[:, :], "akrr")):
                ps, pk = pp.next()
                for kc in range(KC):
                    p.op("pe", lambda e, ps=ps, kc=kc, c0=c0, cw=cw, N=N: e.matmul(ps[0:cw, 0:N], lhsT=win[:, kc, c0:c0 + cw], rhs=hT[:, kc, 0:N],
                                                                                 start=(kc == 0), stop=(kc == KC - 1)), r=["mwin", "ahT"], w=[pk])
                p.op("act", lambda e, ps=ps, dst=dst, cw=cw, N=N: e.copy(out=dst[0:cw, 0:N], in_=ps[0:cw, 0:N]), r=[pk], w=[dk])

            def rmsn(src_chunks, src_key, ones_ap, gcols, dst_chunks, dst_key):
                n = len(src_chunks)
                for c in range(n):
                    p.op("act", lambda e, c=c: e.activation(out=sq[:, c, 0:N], in_=src_chunks[c], func=AF.Square), r=[src_key], w=["asq"])
                ps, pk = pp.next()
                for c in range(n):
                    p.op("pe", lambda e, ps=ps, c=c: e.matmul(ps[:, 0:N], lhsT=ones_ap, rhs=sq[:, c, 0:N], start=(c == 0), stop=(c == n - 1)),
                         r=["asq", "monesq", "masks"], w=[pk])
                r_, rk = rs.next()
                p.op("dve", lambda e: e.tensor_scalar(out=r_[:, 0:N], in0=ps[:, 0:N], scalar1=EPS, scalar2=None, op0=ALU.add), r=[pk], w=[rk])
                p.op("act", lambda e: e.activation(out=r_[:, 0:N], in_=r_[:, 0:N], func=AF.Sqrt), r=[rk], w=[rk])
                p.op("dve", lambda e: e.reciprocal(out=r_[:, 0:N], in_=r_[:, 0:N]), r=[rk], w=[rk])
                for c in range(n):
                    p.op("dve", lambda e, c=c: e.scalar_tensor_tensor(out=dst_chunks[c], in0=src_chunks[c], scalar=gcols[c], in1=r_[:, 0:N],
                                                                      op0=ALU.mult, op1=ALU.mult), r=[src_key, rk, "mg"], w=[dst_key])

            rmsn([ckv[:, 0:N]], "ackv", C["ones128"][:], [kg[:, 0:1]], [ckvn[:, 0:N]], "ackvn")
            if is_lat:
                rmsn([cq[:, 0, 0:N], cq[:, 1, 0:N]], "acq", onesq[:], [qg[:, 0:1], qg[:, 1:2]], [cqn[:, 0, 0:N], cqn[:, 1, 0:N]], "acqn")
                lat0 = t0 - n_ctx
                p.op("pool", lambda e, lat0=lat0: e.iota(tti[:, 0:N], pattern=[[1, N]], base=lat0, channel_multiplier=0), w=["atti"])
                p.op("dve", lambda e: e.tensor_single_scalar(out=tt2[:, 0:N], in_=tti[:, 0:N], scalar=63, op=ALU.bitwise_and), r=["atti"], w=["att2"])
                p.op("dve", lambda e: e.tensor_copy(out=cosT[:, 0:N], in_=tt2[:, 0:N]), r=["att2"], w=["trig"])
                p.op("dve", lambda e: e.tensor_single_scalar(out=tt2[:, 0:N], in_=tti[:, 0:N], scalar=6, op=ALU.arith_shift_right), r=["atti", "trig"], w=["att2"])
                p.op("dve", lambda e: e.tensor_copy(out=sinT[:, 0:N], in_=tt2[:, 0:N]), r=["att2"], w=["trig"])
                p.op("dve", lambda e: e.tensor_tensor(out=sinT[:, 0:N], in0=sinT[:, 0:N], in1=cosT[:, 0:N], op=ALU.subtract), r=["trig"], w=["trig"])
                p.op("dve", lambda e: e.scalar_tensor_tensor(out=ang[:, 0:N], in0=sinT[:, 0:N], scalar=C["isrow"][:, 0:1], in1=cosT[:, 0:N], op0=ALU.mult, op1=ALU.add),
                     r=["trig", "rope_c"], w=["aang"])
                p.op("dve", lambda e: e.tensor_scalar(out=ang[:, 0:N], in0=ang[:, 0:N], scalar1=C["invf"][:, 0:1], scalar2=None, op0=ALU.mult), r=["aang", "rope_c"], w=["aang"])
                for (dst, off) in ((sinT, 0.0), (cosT, 0.5 * math.pi)):
                    p.op("dve", lambda e, off=off: e.tensor_scalar(out=a2[:, 0:N], in0=ang[:, 0:N], scalar1=off, scalar2=None, op0=ALU.add), r=["aang"], w=["aa2"])
                    p.op("dve", lambda e: e.tensor_scalar(out=kf[:, 0:N], in0=a2[:, 0:N], scalar1=1.0 / (2 * math.pi), scalar2=None, op0=ALU.mult), r=["aa2"], w=["akf"])
                    p.op("dve", lambda e: e.tensor_copy(out=tt2[:, 0:N], in_=kf[:, 0:N]), r=["akf"], w=["att2"])
                    p.op("dve", lambda e: e.tensor_copy(out=kf[:, 0:N], in_=tt2[:, 0:N]), r=["att2"], w=["akf"])
                    p.op("dve", lambda e, dst=dst: e.scalar_tensor_tensor(out=dst[:, 0:N], in0=kf[:, 0:N], scalar=-2 * math.pi, in1=a2[:, 0:N], op0=ALU.mult, op1=ALU.add),
                         r=["akf", "aa2"], w=["trig"])
                    p.op("dve", lambda e, dst=dst: e.tensor_scalar(out=dst[:, 0:N], in0=dst[:, 0:N], scalar1=-math.pi, scalar2=math.pi, op0=ALU.max, op1=ALU.min), r=["trig"], w=["trig"])
                    p.op("act", lambda e, dst=dst: e.activation(out=dst[:, 0:N], in_=dst[:, 0:N], func=AF.Sin), r=["trig"], w=["trig"])
                lsl = slice(lat0, lat0 + N)
                for h in range(8):
                    ps, pk = pp.next()
                    for kc in range(2):
                        p.op("pe", lambda e, ps=ps, kc=kc, h=h: e.matmul(ps[:, 0:N], lhsT=wq[:, kc, h * 192:h * 192 + 128], rhs=cqn[:, kc, 0:N], start=(kc == 0), stop=(kc == 1)),
                             r=["mwq", "acqn"], w=[pk])
                    o, ok = ob.next()
                    p.op("act", lambda e, o=o, ps=ps: e.activation(out=o[:, 0:N], in_=ps[:, 0:N], func=AF.Copy, scale=MLA_SCALE), r=[pk], w=[ok])
                    p.dma("sp", lambda e, o=o, h=h, lsl=lsl: e.dma_start(out=QN[h, :, lsl], in_=o[:, 0:N]), r=[ok], w=["QN"])
                    ps, pk = pp.next()
                    for kc in range(2):
                        p.op("pe", lambda e, ps=ps, kc=kc, h=h: e.matmul(ps[0:64, 0:N], lhsT=wq[:, kc, h * 192 + 128:h * 192 + 192], rhs=cqn[:, kc, 0:N], start=(kc == 0), stop=(kc == 1)),
                             r=["mwq", "acqn"], w=[pk])
                    o, ok = ob.next()
                    x_, xk_ = rx.next()
                    p.op("act", lambda e, x_=x_, ps=ps: e.copy(out=x_[:, 0:N], in_=ps[0:64, 0:N]), r=[pk], w=[xk_])
                    rope(x_[:, 0:N], xk_, N, MLA_SCALE, o[0:64, 0:N], ok)
                    p.dma("sp", lambda e, o=o, h=h, lsl=lsl: e.dma_start(out=QR[h, :, lsl], in_=o[0:64, 0:N]), r=[ok], w=["QR"])
            for h in range(8):
                ps, pk = pp.next()
                p.op("pe", lambda e, ps=ps, h=h: e.matmul(ps[:, 0:N], lhsT=wkn[:, h, :], rhs=ckvn[:, 0:N], start=True, stop=True), r=["mwk", "ackvn"], w=[pk])
                o, ok = ob.next()
                p.op("act", lambda e, o=o, ps=ps: e.copy(out=o[:, 0:N], in_=ps[:, 0:N]), r=[pk], w=[ok])
                p.dma("sp", lambda e, o=o, h=h, t0=t0, N=N: e.dma_start(out=KN[h, :, t0:t0 + N], in_=o[:, 0:N]), r=[ok], w=["KN"])
            for j in range(N // 128):
                vt, vk = vb.next()
                for g in range(2):
                    ps, pk = pp.next()
                    p.op("pe", lambda e, ps=ps, j=j, g=g: e.matmul(ps[:, :], lhsT=ckvn[:, j * 128:(j + 1) * 128], rhs=wv[:, 4 * g:4 * g + 4, :], start=True, stop=True),
                         r=["mwk", "ackvn"], w=[pk])
                    p.op("dve", lambda e, vt=vt, ps=ps, g=g: e.tensor_copy(out=vt[:, g * 512:(g + 1) * 512], in_=ps[:, :]), r=[pk], w=[vk])
                tok = t0 + j * 128
                p.dma("sp", lambda e, vt=vt, tok=tok: e.dma_start(out=V[tok:tok + 128, :], in_=vt[:]), r=[vk], w=["Vt"])
            o, ok = ob.next()
            if is_lat:
                rope(krr[:, 0:N], "akrr", N, 1.0, o[0:64, 0:N], ok)
            else:
                p.op("dve", lambda e, o=o: e.tensor_copy(out=o[0:64, 0:N], in_=krr[:, 0:N]), r=["akrr"], w=[ok])
            p.dma("sp", lambda e, o=o, t0=t0, N=N: e.dma_start(out=KR[:, t0:t0 + N], in_=o[0:64, 0:N]), r=[ok], w=["KRo"])


        for (t0_, N_, lat_) in blocks:
            do_block(t0_, N_, lat_)

def phase_mla_attn(p, C, io, n_ctx, n_tok):
    QN, QR, KN, KR, V, MIXT = io["QN"], io["QR"], io["KN"], io["KRo"], io["Vt"], io["MIXT"]
    n_lat = n_tok - n_ctx
    nkt = n_tok // 128
    with p.phase():
        kr = p.sb("bkr", [64, n_tok], BF16)
        p.dma("sp", lambda e: e.dma_start(out=kr[:], in_=KR[:, :]), w=["bkr"])
        ones_bf = p.sb("bones", [128, 128], BF16)
        p.op("dve", lambda e: e.memset(ones_bf[:], 1.0), w=["bones"])
        kn = Ring(p, "bkn", [128, n_tok], BF16, n=2)
        vv = Ring(p, "bvv", [128, nkt, 128], BF16, n=2)
        qn = Ring(p, "bqn", [128, 512], BF16, n=2)
        qr = Ring(p, "bqr", [64, 512], BF16, n=2)
        pt = Ring(p, "bpt", [128, 512], BF16, n=4)
        rd = Ring(p, "brd", [128, 512], n=2)
        ob = Ring(p, "bob", [128, 512], BF16, n=2)
        psS = Ring(p, "bpsS", [128, 512], n=4, psum=True)
        psO = Ring(p, "bpsO", [128, 512], n=2, psum=True)
        psD = Ring(p, "bpsD", [128, 512], n=2, psum=True)
        for h in range(8):
            k_, kk = kn.next()
            v_, vk = vv.next()
            p.dma("sp", lambda e, k_=k_, h=h: e.dma_start(out=k_[:], in_=KN[h, :, :]), w=[kk])
            for j0 in range(0, nkt, 12):
                j1 = min(nkt, j0 + 12)
                p.dma("act", lambda e, v_=v_, h=h, j0=j0, j1=j1: e.dma_start(
                    out=v_[:, j0:j1, :], in_=V[j0 * 128:j1 * 128, h * 128:(h + 1) * 128].rearrange("(j q) c -> q j c", q=128)), w=[vk])
            for qb in range(n_lat // 512):
                qsl = slice(qb * 512, (qb + 1) * 512)
                a, ak = qn.next()
                b, bk = qr.next()
                p.dma("sp", lambda e, a=a, h=h, qsl=qsl: e.dma_start(out=a[:], in_=QN[h, :, qsl]), w=[ak])
                p.dma("sp", lambda e, b=b, h=h, qsl=qsl: e.dma_start(out=b[:], in_=QR[h, :, qsl]), w=[bk])
                po, pok = psO.next()
                pd, pdk = psD.next()
                def S_(kt):
                    ks = slice(kt * 128, (kt + 1) * 128)
                    ps, psk = psS.next()
                    p.op("pe", lambda e, ps=ps, k_=k_, ks=ks, a=a: e.matmul(ps[:], lhsT=k_[:, ks], rhs=a[:], start=True, stop=False), r=[kk, ak], w=[psk])
                    p.op("pe", lambda e, ps=ps, ks=ks, b=b: e.matmul(ps[:], lhsT=kr[:, ks], rhs=b[:], start=False, stop=True), r=["bkr", bk], w=[psk])
                    return ps, psk
                LA = 2
                pend = [S_(kt) for kt in range(min(LA, nkt))]
                for kt in range(nkt):
                    if kt + LA < nkt:
                        pend.append(S_(kt + LA))
                    ps, psk = pend.pop(0)
                    t, tk = pt.next()
                    p.op("act", lambda e, t=t, ps=ps: e.activation(out=t[:], in_=ps[:], func=AF.Exp), r=[psk], w=[tk])
                    p.op("pe", lambda e, po=po, v_=v_, kt=kt, t=t: e.matmul(po[:], lhsT=v_[:, kt, :], rhs=t[:], start=(kt == 0), stop=(kt == nkt - 1)), r=[vk, tk], w=[pok])
                    p.op("pe", lambda e, pd=pd, t=t, kt=kt: e.matmul(pd[:], lhsT=ones_bf[:], rhs=t[:], start=(kt == 0), stop=(kt == nkt - 1)), r=["bones", tk], w=[pdk])
                r_, rk = rd.next()
                p.op("dve", lambda e, r_=r_, pd=pd: e.reciprocal(out=r_[:], in_=pd[:]), r=[pdk], w=[rk])
                o, ok = ob.next()
                p.op("dve", lambda e, o=o, po=po, r_=r_: e.tensor_tensor(out=o[:], in0=po[:], in1=r_[:], op=ALU.mult), r=[pok, rk], w=[ok])
                p.dma("sp", lambda e, o=o, h=h, qb=qb: e.dma_start(out=MIXT[h * 128:(h + 1) * 128, n_ctx + qb * 512:n_ctx + (qb + 1) * 512], in_=o[:]), r=[ok], w=["MIXT"])

CAST_IN_GATHER = 1


def moe_tables(p, NE, max_tiles, max_nb):
    RT = {}
    for nm in ("e4", "rank4", "gate4", "destf"):
        RT[nm] = p.sb("rt_" + nm, [128, max_tiles, 4], F32)
    RT["desti"] = p.sb("rt_desti", [128, max_tiles, 4], I32)
    RT["pstart"] = p.sb("rt_pstart", [128, NE], F32)
    RT["blke"] = p.sb("rt_blke", [128, max_nb], F32)
    RT["widx"] = p.sb("rt_widx", [128, max_nb, 8], I32)
    return RT


def phase_moe_sparse_route(p, C, io, G, RT, l, NE, t_lo, t_hi, n_ctx, BLK=512):
    xres, HTOK, modrow = io["xres"], io["HTOK"], io["modrow"]
    T_ = t_hi - t_lo
    ntile = T_ // 128
    NB = (4 * T_ + BLK - 1) // BLK + NE
    LOG = BLK.bit_length() - 1
    e4, rank4, gate4 = RT["e4"], RT["rank4"], RT["gate4"]
    with p.phase():
        T = norm_scratch(p)
        wr = p.sb("wr", [128, KC, NE], F32)
        p.dma("sp", lambda e: e.dma_start(out=wr[:], in_=io["router_w"][l].rearrange("(c q) n -> q c n", q=128)), w=["wr"])
        br = p.sb("br", [128, NE], F32)
        p.dma("sp", lambda e: e.dma_start(out=br[:], in_=io["router_b"][l].partition_broadcast(128)), w=["wr"])
        Arow = p.sb("sArow", [128, 2, D], F32)
        Brow = p.sb("sBrow", [128, 2, D], F32)
        grow = p.sb("sgrow", [128, D], F32)
        p.dma("sp", lambda e: e.dma_start(out=grow[:], in_=io["norm2_g"][l].partition_broadcast(128)), w=["sgrow"])
        for r in range(2):
            p.dma("sp", lambda e, r=r: e.dma_start(out=Arow[:, r, :], in_=modrow[l, r, 4096:5120].partition_broadcast(128)), w=["sArow"])
            p.dma("sp", lambda e, r=r: e.dma_start(out=Brow[:, r, :], in_=modrow[l, r, 3072:4096].partition_broadcast(128)), w=["sBrow"])
            p.op("dve", lambda e, r=r: e.scalar_tensor_tensor(out=Arow[:, r, :], in0=Arow[:, r, :], scalar=1.0, in1=grow[:], op0=ALU.add, op1=ALU.mult),
                 r=["sArow", "sgrow"], w=["sArow"])
        cnt = p.sb("scnt", [128, NE], F32)
        p.op("dve", lambda e: e.memset(cnt[:], 0.0), w=["scnt"])
        xt = Ring(p, "sxt", [128, D], n=2)
        h32 = Ring(p, "sh32", [128, KC, 128], n=2)
        ht = Ring(p, "sht", [128, D], n=2)
        hb = Ring(p, "shb", [128, D], BF16, n=2)
        sm = {nm: Ring(p, "s_" + nm, [128, NE], n=2) for nm in ("lg", "ex", "mk", "rk", "oh", "tmp")}
        m8 = Ring(p, "sm8", [128, 8], n=2)
        sc = Ring(p, "ssc", [128, 2], n=2)
        psr = p.ps("spsr", [128, NE], F32)
        ps2 = p.ps("sps2", [128, NE], F32)
        ps3 = p.ps("sps3", [128, NE], F32)
        for ti in range(ntile):
            tok = t_lo + ti * 128
            r = 1 if tok < n_ctx else 0
            x, xk = xt.next()
            p.dma("sp", lambda e, x=x, tok=tok: e.dma_start(out=x[:], in_=xres[tok:tok + 128, :]), w=[xk])
            f, fk = h32.next()
            norm_tile(p, C, T, xk, x, G["A2"][:, l, r, :], G["B2"][:, l, r, :], None, fk, hT32_dst=lambda kc, f=f: f[:, kc, :], want_bf=False)
            fk32 = fk + "32"
            a, ak = ht.next()
            b, bk = hb.next()
            p.op("dve", lambda e, a=a, r=r: e.tensor_tensor(out=a[:], in0=T["xn"][:], in1=Arow[:, r, :], op=ALU.mult), r=["xn", "sArow"], w=[ak])
            p.op("pool", lambda e, a=a, b=b, r=r: e.tensor_tensor(out=b[:], in0=a[:], in1=Brow[:, r, :], op=ALU.add), r=[ak, "sBrow"], w=[bk])
            p.dma("sp", lambda e, b=b, tok=tok: e.dma_start(out=HTOK[tok:tok + 128, :], in_=b[:]), r=[bk], w=["HTOK"])
            for kc in range(KC):
                p.op("pe", lambda e, f=f, kc=kc: e.matmul(psr[:], lhsT=f[:, kc, :], rhs=wr[:, kc, :], start=(kc == 0), stop=(kc == KC - 1)),
                     r=[fk32, "wr"], w=["spsr"])
            g, gk = sm["lg"].next()
            p.op("dve", lambda e, g=g: e.tensor_tensor(out=g[:], in0=psr[:], in1=br[:], op=ALU.add), r=["spsr", "wr"], w=[gk])
            m, mk8 = m8.next()
            p.op("dve", lambda e, m=m, g=g: e.max(out=m[:], in_=g[:]), r=[gk], w=[mk8])
            msk, mskk = sm["mk"].next()
            p.op("dve", lambda e, msk=msk, g=g, m=m: e.tensor_scalar(out=msk[:], in0=g[:], scalar1=m[:, 3:4], scalar2=None, op0=ALU.is_ge), r=[gk, mk8], w=[mskk])
            s, sk = sc.next()
            p.op("dve", lambda e, s=s, m=m: e.tensor_scalar(out=s[:, 0:1], in0=m[:, 0:1], scalar1=-1.0, scalar2=None, op0=ALU.mult), r=[mk8], w=[sk])
            x_, xk_ = sm["ex"].next()
            p.op("act", lambda e, x_=x_, g=g, s=s: e.activation(out=x_[:], in_=g[:], func=AF.Exp, bias=s[:, 0:1]), r=[gk, sk], w=[xk_])
            p.op("dve", lambda e, x_=x_, msk=msk: e.tensor_tensor(out=x_[:], in0=x_[:], in1=msk[:], op=ALU.mult), r=[xk_, mskk], w=[xk_])
            p.op("dve", lambda e, s=s, x_=x_: e.reduce_sum(out=s[:, 1:2], in_=x_[:], axis=AX.X), r=[xk_], w=[sk])
            p.op("dve", lambda e, s=s: e.reciprocal(out=s[:, 1:2], in_=s[:, 1:2]), r=[sk], w=[sk])
            p.op("dve", lambda e, x_=x_, s=s: e.tensor_scalar(out=x_[:], in0=x_[:], scalar1=s[:, 1:2], scalar2=None, op0=ALU.mult), r=[xk_, sk], w=[xk_])
            p.op("pe", lambda e, msk=msk: e.matmul(ps2[:], lhsT=C["strict0"][:], rhs=msk[:], start=True, stop=True), r=["masks", mskk], w=["sps2"])
            p.op("pe", lambda e, msk=msk: e.matmul(ps3[:], lhsT=C["ones64"][:], rhs=msk[:], start=True, stop=True), r=["masks", mskk], w=["sps3"])
            rk, rkk = sm["rk"].next()
            p.op("dve", lambda e, rk=rk: e.tensor_tensor(out=rk[:], in0=ps2[:], in1=cnt[:], op=ALU.add), r=["sps2", "scnt"], w=[rkk])
            p.op("dve", lambda e: e.tensor_tensor(out=cnt[:], in0=ps3[:], in1=cnt[:], op=ALU.add), r=["sps3", "scnt", rkk], w=["scnt"])
            for k in range(4):
                oh, ohk = sm["oh"].next()
                p.op("dve", lambda e, oh=oh, g=g, m=m, k=k: e.tensor_scalar(out=oh[:], in0=g[:], scalar1=m[:, k:k + 1], scalar2=None, op0=ALU.is_equal), r=[gk, mk8], w=[ohk])
                for (src, srck, dst) in ((C["iota_f"][:, 0:NE], "iota_f", e4), (rk[:], rkk, rank4), (x_[:], xk_, gate4)):
                    tmp, tmpk = sm["tmp"].next()
                    p.op("dve", lambda e, tmp=tmp, oh=oh, src=src: e.tensor_tensor(out=tmp[:], in0=oh[:], in1=src, op=ALU.mult), r=[ohk, srck], w=[tmpk])
                    p.op("dve", lambda e, tmp=tmp, dst=dst, ti=ti, k=k: e.reduce_sum(out=dst[:, ti, k:k + 1], in_=tmp[:], axis=AX.X), r=[tmpk], w=["rt"])
        ci = p.sb("sci", [128, NE], I32)
        padf = p.sb("spadf", [128, NE], F32)
        ca = p.sb("sca", [128, NE], F32)
        cb = p.sb("scb", [128, NE], F32)
        p.op("dve", lambda e: e.tensor_copy(out=ci[:], in_=cnt[:]), r=["scnt"], w=["sci"])
        p.op("dve", lambda e: e.tensor_single_scalar(out=ci[:], in_=ci[:], scalar=BLK - 1, op=ALU.add), r=["sci"], w=["sci"])
        p.op("dve", lambda e: e.tensor_single_scalar(out=ci[:], in_=ci[:], scalar=LOG, op=ALU.arith_shift_right), r=["sci"], w=["sci"])
        p.op("dve", lambda e: e.tensor_single_scalar(out=ci[:], in_=ci[:], scalar=LOG, op=ALU.logical_shift_left), r=["sci"], w=["sci"])
        p.op("dve", lambda e: e.tensor_copy(out=padf[:], in_=ci[:]), r=["sci"], w=["spadf"])
        p.op("dve", lambda e: e.tensor_copy(out=ca[:], in_=padf[:]), r=["spadf"], w=["sca"])
        cur, curk, oth, othk = ca, "sca", cb, "scb"
        sft = 1
        while sft < NE:
            p.op("dve", lambda e, cur=cur, oth=oth: e.tensor_copy(out=oth[:], in_=cur[:]), r=[curk], w=[othk])
            p.op("dve", lambda e, cur=cur, oth=oth, sft=sft: e.tensor_tensor(out=oth[:, sft:NE], in0=cur[:, sft:NE], in1=cur[:, 0:NE - sft], op=ALU.add), r=[curk, othk], w=[othk])
            cur, curk, oth, othk = oth, othk, cur, curk
            sft *= 2
        pend, pendk = cur, curk
        p.op("dve", lambda e: e.tensor_tensor(out=RT["pstart"][:], in0=pend[:], in1=padf[:], op=ALU.subtract), r=[pendk, "spadf"], w=["rt"])
        bst = p.sb("sbst", [128, NB], F32)
        p.op("dve", lambda e: e.tensor_scalar(out=bst[:], in0=C["iota_f"][:, 0:NB], scalar1=float(BLK), scalar2=None, op0=ALU.mult), r=["iota_f"], w=["sbst"])
        blke = RT["blke"]
        p.op("dve", lambda e: e.memset(blke[:, 0:NB], 0.0), w=["rt"])
        for e_ in range(NE):
            p.op("dve", lambda e, e_=e_: e.scalar_tensor_tensor(out=blke[:, 0:NB], in0=bst[:], scalar=pend[:, e_:e_ + 1], in1=blke[:, 0:NB], op0=ALU.is_ge, op1=ALU.add),
                 r=["sbst", pendk, "rt"], w=["rt"])
        p.op("dve", lambda e: e.tensor_scalar(out=blke[:, 0:NB], in0=blke[:, 0:NB], scalar1=float(NE - 1), scalar2=None, op0=ALU.min), r=["rt"], w=["rt"])
        base8 = p.sb("sbase8", [128, 8], F32)
        p.op("dve", lambda e: e.tensor_scalar(out=base8[:], in0=C["iota_f"][:, 0:8], scalar1=128.0, scalar2=C["iota_p"][:, 0:1], op0=ALU.mult, op1=ALU.add),
             r=["iota_f", "iota_p"], w=["sbase8"])
        b1k = p.sb("sb1k", [128, NB], F32)
        lofs = float(l * NE * 1024) if CAST_IN_GATHER else 0.0
        p.op("dve", lambda e: e.tensor_scalar(out=b1k[:], in0=blke[:, 0:NB], scalar1=1024.0, scalar2=lofs, op0=ALU.mult, op1=ALU.add), r=["rt"], w=["sb1k"])
        wf = p.sb("swf", [128, NB, 8], F32)
        for b_ in range(NB):
            p.op("dve" if b_ % 2 == 0 else "pool", lambda e, b_=b_: e.tensor_scalar(out=wf[:, b_, :], in0=base8[:], scalar1=b1k[:, b_:b_ + 1], scalar2=None, op0=ALU.add),
                 r=["sbase8", "sb1k"], w=["swf"])
        p.op("dve", lambda e: e.tensor_copy(out=RT["widx"][:, 0:NB, :], in_=wf[:]), r=["swf"], w=["rt"])
        destf, desti = RT["destf"], RT["desti"]
        for ti in range(ntile):
            for k in range(4):
                oh, ohk = sm["oh"].next()
                p.op("dve", lambda e, oh=oh, ti=ti, k=k: e.tensor_scalar(out=oh[:], in0=C["iota_f"][:, 0:NE], scalar1=e4[:, ti, k:k + 1], scalar2=None, op0=ALU.is_equal),
                     r=["iota_f", "rt"], w=[ohk])
                tmp, tmpk = sm["tmp"].next()
                p.op("dve", lambda e, tmp=tmp, oh=oh: e.tensor_tensor(out=tmp[:], in0=oh[:], in1=RT["pstart"][:], op=ALU.mult), r=[ohk, "rt"], w=[tmpk])
                p.op("dve", lambda e, tmp=tmp, ti=ti, k=k: e.reduce_sum(out=destf[:, ti, k:k + 1], in_=tmp[:], axis=AX.X), r=[tmpk], w=["rt"])
        p.op("dve", lambda e: e.tensor_tensor(out=destf[:, 0:ntile, :], in0=destf[:, 0:ntile, :], in1=rank4[:, 0:ntile, :], op=ALU.add), r=["rt"], w=["rt"])
        p.op("dve", lambda e: e.tensor_scalar(out=destf[:, 0:ntile, :], in0=destf[:, 0:ntile, :], scalar1=float(NB * BLK - 1), scalar2=0.0, op0=ALU.min, op1=ALU.max), r=["rt"], w=["rt"])
        p.op("dve", lambda e: e.tensor_copy(out=desti[:, 0:ntile, :], in_=destf[:, 0:ntile, :]), r=["rt"], w=["rt"])
    return NB


def phase_moe_sparse_dispatch(p, C, io, RT, t_lo, t_hi, NB, BLK=512):
    HTOK, HS = io["HTOK"], io["HS"]
    ntile = (t_hi - t_lo) // 128
    with p.phase():
        z = p.sb("dz", [128, 4, D], BF16)
        p.op("pool", lambda e: e.memset(z[:], 0.0), w=["dz"])
        for b_ in range(NB * BLK // 512):
            p.dma("sp" if b_ % 2 == 0 else "act", lambda e, b_=b_: e.dma_start(out=HS[b_ * 512:(b_ + 1) * 512, :].rearrange("(j q) d -> q j d", q=128), in_=z[:]),
                  r=["dz"], w=[f"HSz{b_ % 8}"])
        hb = Ring(p, "dhb", [128, D], BF16, n=3)
        zk = [f"HSz{i}" for i in range(8)]
        for ti in range(ntile):
            tok = t_lo + ti * 128
            h, hk = hb.next()
            p.dma("sp", lambda e, h=h, tok=tok: e.dma_start(out=h[:], in_=HTOK[tok:tok + 128, :]), w=[hk])
            for k in range(4):
                p.dma("pool", lambda e, h=h, ti=ti, k=k: e.indirect_dma_start(
                    out=HS[:, :], out_offset=bass.IndirectOffsetOnAxis(ap=RT["desti"][:, ti, k:k + 1], axis=0), in_=h[:, :], in_offset=None),
                    r=[hk, "rt"] + zk, w=[f"HSs{(ti * 4 + k) % 8}"])


def phase_moe_sparse_experts(p, C, io, RT, l, NE, NB, BLK=512):
    HS, Y = io["HS"], io["Y"]
    if CAST_IN_GATHER:
        WGf = io["moe_gu_w"].rearrange("l e k n -> (l e k) n")
        WDf = io["moe_down_w"].rearrange("l e k n -> (l e k) n")
    else:
        WGf = io["WGU"].rearrange("e k n -> (e k) n")
        WDf = io["WDN"].rearrange("e k n -> (e k) n")
    blke = RT["blke"]
    with p.phase():
        b32 = p.sb("xb32", [NE, 2048], F32)
        bgu = p.sb("xbgu", [NE, 2048], BF16)
        bdn = p.sb("xbdn", [NE, 1024], BF16)
        p.dma("sp", lambda e: e.dma_start(out=b32[:], in_=io["moe_gu_b"][l]), w=["xb32"])
        p.op("dve", lambda e: e.tensor_copy(out=bgu[:], in_=b32[:]), r=["xb32"], w=["xbias"])
        p.dma("sp", lambda e: e.dma_start(out=b32[:, 0:1024], in_=io["moe_down_b"][l]), r=["xbias"], w=["xb32"])
        p.op("dve", lambda e: e.tensor_copy(out=bdn[:], in_=b32[:, 0:1024]), r=["xb32"], w=["xbias"])
        ones5 = p.sb("xones", [NE, BLK], F32)
        p.op("dve", lambda e: e.memset(ones5[:], 1.0), w=["xones"])
        wg = Ring(p, "xwg", [128, KC, 2048], BF16, n=2)
        wd = Ring(p, "xwd", [128, KC, 1024], BF16, n=2)
        hs = Ring(p, "xhs", [128, BLK // 128, D], BF16, n=2)
        hsT = Ring(p, "xhsT", [128, KC, BLK], BF16, n=2)
        ohT = Ring(p, "xohT", [NE, BLK], BF16, n=2)
        actT = Ring(p, "xactT", [128, KC, BLK], BF16, n=1)
        t1 = Ring(p, "xt1", [128, BLK], n=2)
        sgm = Ring(p, "xsg", [128, BLK], n=2)
        t2 = Ring(p, "xt2", [128, BLK], n=2)
        yo = Ring(p, "xyo", [128, D], n=2)
        pst = Ring(p, "xpst", [128, KC, 128], BF16, n=2, psum=True)
        psg = Ring(p, "xpsg", [128, 512], n=4, psum=True)
        psd = Ring(p, "xpsd", [128, 512], n=2, psum=True)
        ev = 0
        for b_ in range(NB):
            g_, gk = wg.next()
            d_, dk = wd.next()
            for kc in range(KC):
                p.dma("pool", lambda e, g_=g_, kc=kc, b_=b_: e.indirect_dma_start(
                    out=g_[:, kc, :], out_offset=None, in_=WGf[:, :], in_offset=bass.IndirectOffsetOnAxis(ap=RT["widx"][:, b_, kc:kc + 1], axis=0)), r=["rt"], w=[gk])
                p.dma("pool", lambda e, d_=d_, kc=kc, b_=b_: e.indirect_dma_start(
                    out=d_[:, kc, :], out_offset=None, in_=WDf[:, :], in_offset=bass.IndirectOffsetOnAxis(ap=RT["widx"][:, b_, kc:kc + 1], axis=0)), r=["rt"], w=[dk])
            h_, hk = hs.next()
            p.dma("sp", lambda e, h_=h_, b_=b_: e.dma_start(out=h_[:], in_=HS[b_ * BLK:(b_ + 1) * BLK, :].rearrange("(j q) d -> q j d", q=128)), w=[hk])
            o_, ok_ = ohT.next()
            p.op("dve", lambda e, o_=o_, b_=b_: e.tensor_scalar(out=o_[:], in0=ones5[:], scalar1=blke[0:NE, b_:b_ + 1], scalar2=C["iota_p"][0:NE, 0:1],
                                                                 op0=ALU.mult, op1=ALU.is_equal), r=["xones", "rt", "iota_p"], w=[ok_])
            hT, hTk = hsT.next()
            for j in range(BLK // 128):
                pt_, ptk = pst.next()
                for kc in range(KC):
                    p.op("pe", lambda e, pt_=pt_, h_=h_, j=j, kc=kc: e.transpose(pt_[:, kc, :], h_[:, j, kc * 128:(kc + 1) * 128], C["identb"][:]), r=[hk, "identb"], w=[ptk])
                if j % 2 == 0:
                    p.op("act", lambda e, hT=hT, pt_=pt_, j=j: e.copy(out=hT[:, :, j * 128:(j + 1) * 128], in_=pt_[:]), r=[ptk], w=[hTk])
                else:
                    p.op("dve", lambda e, hT=hT, pt_=pt_, j=j: e.tensor_copy(out=hT[:, :, j * 128:(j + 1) * 128], in_=pt_[:]), r=[ptk], w=[hTk])
            aT, aTk = actT.next()
            N = BLK
            for c in range(8):
                pa, pak = psg.next()
                pb, pbk = psg.next()
                for (ps_, pk_, col0) in ((pa, pak, c * 128), (pb, pbk, DE + c * 128)):
                    for kc in range(KC):
                        p.op("pe", lambda e, ps_=ps_, g_=g_, kc=kc, col0=col0, hT=hT: e.matmul(
                            ps_[:, 0:N], lhsT=g_[:, kc, col0:col0 + 128], rhs=hT[:, kc, :], start=(kc == 0), stop=False), r=[gk, hTk], w=[pk_])
                    p.op("pe", lambda e, ps_=ps_, col0=col0, o_=o_: e.matmul(ps_[:, 0:N], lhsT=bgu[:, col0:col0 + 128], rhs=o_[:], start=False, stop=True),
                         r=["xbias", ok_], w=[pk_])
                a1, a1k = t1.next()
                p.op("dve", lambda e, a1=a1, pa=pa: e.tensor_scalar(out=a1[:], in0=pa[:], scalar1=SW_LIMIT, scalar2=None, op0=ALU.min), r=[pak], w=[a1k])
                s_, sk_ = sgm.next()
                p.op("act", lambda e, s_=s_, a1=a1: e.activation(out=s_[:], in_=a1[:], func=AF.Sigmoid, scale=SW_ALPHA), r=[a1k], w=[sk_])
                a2, a2k = t2.next()
                p.op("dve", lambda e, a2=a2, pb=pb: e.tensor_scalar(out=a2[:], in0=pb[:], scalar1=SW_LIMIT, scalar2=-SW_LIMIT, op0=ALU.min, op1=ALU.max), r=[pbk], w=[a2k])
                p.op("dve", lambda e, a1=a1, s_=s_: e.tensor_tensor(out=a1[:], in0=a1[:], in1=s_[:], op=ALU.mult), r=[a1k, sk_], w=[a1k])
                p.op("dve", lambda e, aT=aT, a1=a1, a2=a2, c=c: e.scalar_tensor_tensor(out=aT[:, c, :], in0=a2[:], scalar=1.0, in1=a1[:], op0=ALU.add, op1=ALU.mult),
                     r=[a1k, a2k], w=[aTk])
            for j in range(BLK // 128):
                y_, yk = yo.next()
                for n in range(2):
                    pd, pdk = psd.next()
                    for kc in range(KC):
                        p.op("pe", lambda e, pd=pd, aT=aT, d_=d_, kc=kc, j=j, n=n: e.matmul(
                            pd[:], lhsT=aT[:, kc, j * 128:(j + 1) * 128], rhs=d_[:, kc, n * 512:(n + 1) * 512], start=(kc == 0), stop=False), r=[aTk, dk], w=[pdk])
                    p.op("pe", lambda e, pd=pd, o_=o_, n=n: e.matmul(pd[:], lhsT=o_[:, 0:128], rhs=bdn[:, n * 512:(n + 1) * 512], start=False, stop=True),
                         r=[ok_, "xbias"], w=[pdk])
                    ev += 1
                    if ev % 2 == 0:
                        p.op("act", lambda e, y_=y_, pd=pd, n=n: e.copy(out=y_[:, n * 512:(n + 1) * 512], in_=pd[:]), r=[pdk], w=[yk])
                    else:
                        p.op("dve", lambda e, y_=y_, pd=pd, n=n: e.tensor_copy(out=y_[:, n * 512:(n + 1) * 512], in_=pd[:]), r=[pdk], w=[yk])
                row = b_ * BLK + j * 128
                p.dma("sp", lambda e, y_=y_, row=row: e.dma_start(out=Y[row:row + 128, :], in_=y_[:]), r=[yk], w=["Y"])


def phase_moe_sparse_combine(p, C, io, RT, l, t_lo, t_hi, n_ctx):
    xres, Y, modrow = io["xres"], io["Y"], io["modrow"]
    ntile = (t_hi - t_lo) // 128
    with p.phase():
        gt = p.sb("cgt2", [128, 2, D], F32)
        for r in range(2):
            p.dma("sp", lambda e, r=r: e.dma_start(out=gt[:, r, :], in_=modrow[l, r, 5120:6144].partition_broadcast(128)), w=["cgt2"])
        yk_ = Ring(p, "cyk", [128, D], n=8)
        acc = Ring(p, "cacc", [128, D], n=2)
        xt = Ring(p, "cxt", [128, D], n=2)
        for ti in range(ntile):
            tok = t_lo + ti * 128
            r = 1 if tok < n_ctx else 0
            x, xk = xt.next()
            p.dma("sp", lambda e, x=x, tok=tok: e.dma_start(out=x[:], in_=xres[tok:tok + 128, :]), w=[xk])
            a, ak = acc.next()
            for k in range(4):
                y, yk = yk_.next()
                p.dma("pool", lambda e, y=y, ti=ti, k=k: e.indirect_dma_start(
                    out=y[:, :], out_offset=None, in_=Y[:, :], in_offset=bass.IndirectOffsetOnAxis(ap=RT["desti"][:, ti, k:k + 1], axis=0)), r=["rt", "Y"], w=[yk])
                if k == 0:
                    p.op("dve", lambda e, a=a, y=y, ti=ti: e.tensor_scalar(out=a[:], in0=y[:], scalar1=RT["gate4"][:, ti, 0:1], scalar2=None, op0=ALU.mult), r=[yk, "rt"], w=[ak])
                else:
                    p.op("dve", lambda e, a=a, y=y, ti=ti, k=k: e.scalar_tensor_tensor(out=a[:], in0=y[:], scalar=RT["gate4"][:, ti, k:k + 1], in1=a[:], op0=ALU.mult, op1=ALU.add),
                         r=[yk, "rt", ak], w=[ak])
            p.op("pool", lambda e, a=a, r=r: e.tensor_tensor(out=a[:], in0=a[:], in1=gt[:, r, :], op=ALU.mult), r=[ak, "cgt2"], w=[ak])
            p.op("dve", lambda e, a=a, x=x: e.tensor_tensor(out=x[:], in0=x[:], in1=a[:], op=ALU.add), r=[ak, xk], w=[xk])
            p.dma("sp", lambda e, x=x, tok=tok: e.dma_start(out=xres[tok:tok + 128, :], in_=x[:]), r=[xk], w=["xres"])

from concourse.bass_utils import run_bass_kernel_spmd

N_CTX = 256
N_LAT = 8192
DEPTH = 2
NEXP = 32
_W_NAMES = ["ada_w", "ada_b", "norm1_g", "norm2_g", "ev_w_in", "ev_w_out", "gla_gk_up", "gla_gk_b", "gla_norm_g",
            "rw_mu", "rw_w0", "rw_w2", "rw_a0", "rw_a2", "rw_k_k", "rw_k_a", "rw_r_k", "rw_g2", "rw_ln_w", "rw_ln_b",
            "od_w_in", "mla_q_norm", "mla_wq_up", "mla_kv_norm", "mla_wkv_up", "od_w_out",
            "router_w", "router_b", "moe_gu_w", "moe_gu_b", "moe_down_w", "moe_down_b", "final_norm_g"]


def build(shapes, n_ctx=N_CTX, n_lat=N_LAT, NE=NEXP):
    n_tok = n_ctx + n_lat
    L = DEPTH
    nc = bass.Bass("TRN2", target_bir_lowering=False)
    NBmax = (4 * n_tok + 511) // 512 + NE
    io = {}
    for k, shp in shapes.items():
        io[k] = dram(nc, k, list(shp), kind="ExternalInput")
    for name, shp, dt in (("xres", [n_tok, 1024], F32), ("modrow", [L, 2, 6144], F32), ("PT", [3376, n_tok], F32),
                          ("PTOK", [n_tok, 1536], F32), ("SMT", [1824, n_tok], F32), ("YTG", [2, 512, n_tok], F32),
                          ("YTR", [2, 512, n_tok], F32), ("RK", [2, 512, n_tok], F32), ("MIXT", [1024, n_tok], BF16),
                          ("HT", [1024, n_tok], BF16), ("GATE", [n_tok, NE], F32), ("WGU", [NE, 1024, 2048], BF16),
                          ("WDN", [NE, 1024, 1024], BF16), ("QN", [8, 128, n_lat], BF16), ("QR", [8, 64, n_lat], BF16),
                          ("KN", [8, 128, n_tok], BF16), ("KRo", [64, n_tok], BF16), ("Vt", [n_tok, 1024], BF16),
                          ("HTOK", [n_tok, 1024], BF16), ("HS", [NBmax * 512, 1024], BF16), ("Y", [NBmax * 512, 1024], F32)):
        io[name] = dram(nc, name, shp, dt)
    io["out"] = dram(nc, "out", [n_lat, 1024], kind="ExternalOutput")
    p = Prog(nc)
    C = make_consts(p)
    make_masks(p, C)
    make_rope_consts(p, C)
    G = {k: p.sb("G" + k, [128, L, 2, 8], F32) for k in ("A1", "B1", "A2", "B2")}
    RT = moe_tables(p, NE, n_tok // 128, NBmax)
    with p.phase():
        cp = Ring(p, "cpx", [128, 1024], n=3)
        for ti in range(n_tok // 128):
            x_, xk = cp.next()
            p.dma("sp", lambda e, x_=x_, ti=ti: e.dma_start(out=x_[:], in_=io["xin"][ti * 128:(ti + 1) * 128, :]), w=[xk])
            p.dma("sp", lambda e, x_=x_, ti=ti: e.dma_start(out=io["xres"][ti * 128:(ti + 1) * 128, :], in_=x_[:]), r=[xk], w=["xres"])
    phase_ada(p, C, io, L, G)
    phase_inproj(p, C, io, G, 0, n_ctx, n_tok)
    phase_gla(p, C, io, n_ctx, n_tok)
    phase_gla_finish(p, C, io, n_tok)
    phase_shift(p, C, io, n_ctx, n_tok)
    phase_rwkv(p, C, io, n_ctx, n_tok)
    phase_rwkv_finish(p, C, io, n_tok)
    phase_outproj(p, C, io, 0, io["ev_w_out"], n_ctx, n_tok)
    NB = phase_moe_sparse_route(p, C, io, G, RT, 0, NE, 0, n_tok, n_ctx)
    phase_moe_sparse_dispatch(p, C, io, RT, 0, n_tok, NB)
    phase_moe_sparse_experts(p, C, io, RT, 0, NE, NB)
    phase_moe_sparse_combine(p, C, io, RT, 0, 0, n_tok, n_ctx)
    phase_mla_proj(p, C, io, G, 1, n_ctx, n_tok)
    phase_mla_attn(p, C, io, n_ctx, n_tok)
    phase_outproj(p, C, io, 1, io["od_w_out"], n_ctx, n_tok, t_lo=n_ctx)
    NB = phase_moe_sparse_route(p, C, io, G, RT, 1, NE, n_ctx, n_tok, n_ctx)
    phase_moe_sparse_dispatch(p, C, io, RT, n_ctx, n_tok, NB)
    phase_moe_sparse_experts(p, C, io, RT, 1, NE, NB)
    phase_moe_sparse_combine(p, C, io, RT, 1, n_ctx, n_tok, n_ctx)
    phase_final(p, C, io, n_ctx, n_tok)
    p.finish()
    return nc


def kernel(**inputs):
    f = lambda a: np.ascontiguousarray(np.asarray(a, dtype=np.float32))
    x, c, ctx, c_ctx = f(inputs["x"]), f(inputs["c"]), f(inputs["ctx"]), f(inputs["c_ctx"])
    B = x.shape[0]
    shared = {}
    for k in _W_NAMES:
        a = f(inputs[k])
        if k.startswith(("ev_", "gla_", "rw_", "od_", "mla_")):
            a = np.ascontiguousarray(a[0])
        shared[k] = a
    in_maps = []
    for b in range(B):
        m = dict(shared)
        m["xin"] = np.ascontiguousarray(np.concatenate([ctx[b], x[b]], axis=0))
        m["cvec"] = np.ascontiguousarray(np.stack([c[b], c_ctx], axis=0))
        in_maps.append(m)
    shapes = {k: v.shape for k, v in in_maps[0].items()}
    nc = build(shapes, n_ctx=ctx.shape[1], n_lat=x.shape[1], NE=shared["router_w"].shape[-1])
    res = run_bass_kernel_spmd(nc, in_maps, core_ids=list(range(B)))
    return np.stack([np.asarray(r["out"], dtype=np.float32) for r in res.results], axis=0)
```

```python
import contextlib
import numpy as np
import concourse.bass as bass
import concourse.mybir as mybir

F32 = mybir.dt.float32
BF16 = mybir.dt.bfloat16
I32 = mybir.dt.int32
U32 = mybir.dt.uint32
ALU = mybir.AluOpType
AF = mybir.ActivationFunctionType
AX = mybir.AxisListType

ENGS = ("pe", "act", "dve", "pool", "sp")
RESET_THRESH = 20000


class Prog:
    def __init__(self, nc, n_dma_slots=10):
        self.nc = nc
        self.es = contextlib.ExitStack()
        self.cur = self.es
        self.streams = {e: [] for e in ENGS}
        self.count = {e: 0 for e in ENGS}
        self.seen = {e: {} for e in ENGS}
        self.bufs = {}
        self.sems = {}
        for e in ("pe", "act", "dve", "pool"):
            self.sems[e] = self.es.enter_context(nc.semaphore("s_" + e))
        self.dma_slots = {}
        self.dma_rr = {}
        for q in ("sp", "act", "pool"):
            self.dma_slots[q] = []
            for i in range(n_dma_slots):
                s = self.es.enter_context(nc.semaphore(f"d_{q}{i}"))
                self.sems[f"d_{q}{i}"] = s
                self.dma_slots[q].append([f"d_{q}{i}", 0])
            self.dma_rr[q] = 0
        self.n_instr = 0
        self.bsem = self.es.enter_context(nc.semaphore("s_bar"))
        self.gsem = self.es.enter_context(nc.semaphore("s_go"))
        self.nreset = 0
        self.reset_thresh = RESET_THRESH

    def _maybe_reset(self):
        if max(self.count.values()) >= self.reset_thresh or \
                max(v for q in self.dma_slots if q != "pool" for _, v in self.dma_slots[q]) >= 2 * self.reset_thresh:
            self.sync_reset()

    def sync_reset(self):
        self.barrier()
        self.nreset += 1
        k = self.nreset
        bs, gs = self.bsem, self.gsem
        for e in ENGS:
            self.streams[e].append(lambda eng, bs=bs: eng.sem_inc(bs, 1))
        self.streams["sp"].append(lambda eng, bs=bs, k=k: eng.wait_ge(bs, 5 * k))
        for name, sem in self.sems.items():
            if name.startswith("d_pool"):
                continue
            self.streams["sp"].append(lambda eng, sem=sem: eng.sem_clear(sem))
        self.streams["sp"].append(lambda eng, gs=gs: eng.sem_inc(gs, 1))
        for e in ENGS:
            if e != "sp":
                self.streams[e].append(lambda eng, gs=gs, k=k: eng.wait_ge(gs, k))
        self.count = {e: 0 for e in ENGS}
        self.seen = {e: {} for e in ENGS}
        self.bufs = {}
        for q in self.dma_slots:
            if q == "pool":
                continue
            for slot in self.dma_slots[q]:
                slot[1] = 0

    def sb(self, name, shape, dt=F32):
        self._uid = getattr(self, "_uid", 0) + 1
        name = f"{name}_u{self._uid}"
        return self.cur.enter_context(self.nc.sbuf_tensor(name, list(shape), dt))

    def ps(self, name, shape, dt=F32):
        self._uid = getattr(self, "_uid", 0) + 1
        name = f"{name}_u{self._uid}"
        return self.cur.enter_context(self.nc.psum_tensor(name, list(shape), dt))

    def _deps(self, r, w):
        deps = {}

        def add(tok):
            if tok is None:
                return
            s, v = tok
            if deps.get(s, 0) < v:
                deps[s] = v

        for k in r:
            b = self.bufs.setdefault(k, {"w": None, "r": {}})
            add(b["w"])
        for k in w:
            b = self.bufs.setdefault(k, {"w": None, "r": {}})
            add(b["w"])
            for s, v in b["r"].items():
                add((s, v))
        return deps

    def _commit(self, r, w, tok):
        for k in r:
            b = self.bufs[k]
            if b["r"].get(tok[0], 0) < tok[1]:
                b["r"][tok[0]] = tok[1]
        for k in w:
            b = self.bufs[k]
            b["w"] = tok
            b["r"] = {}

    def _emit_waits(self, eng, deps):
        seen = self.seen[eng]
        for s, v in deps.items():
            if eng == "pe" and s == "pe":
                continue
            if seen.get(s, 0) >= v:
                continue
            seen[s] = v
            sem = self.sems[s]
            self.streams[eng].append(lambda e, sem=sem, v=v: e.wait_ge(sem, v))

    def op(self, eng, fn, r=(), w=()):
        self._maybe_reset()
        deps = self._deps(r, w)
        self._emit_waits(eng, deps)
        self.count[eng] += 1
        n = self.count[eng]
        sem = self.sems[eng]
        self.streams[eng].append(lambda e, fn=fn, sem=sem: fn(e).then_inc(sem, 1))
        self._commit(r, w, (eng, n))
        self.n_instr += 1

    def dma(self, q, fn, r=(), w=()):
        self._maybe_reset()
        deps = self._deps(r, w)
        i = self.dma_rr[q]
        self.dma_rr[q] = (i + 1) % len(self.dma_slots[q])
        slot = self.dma_slots[q][i]
        if slot[1] > 0:
            if deps.get(slot[0], 0) < slot[1]:
                deps[slot[0]] = slot[1]
        self._emit_waits(q, deps)
        slot[1] += 16
        sem = self.sems[slot[0]]
        self.streams[q].append(lambda e, fn=fn, sem=sem: fn(e).then_inc(sem, 16))
        self._commit(r, w, (slot[0], slot[1]))
        self.n_instr += 1

    def wait_all(self, eng, keys):
        deps = self._deps(keys, ())
        self._emit_waits(eng, deps)

    def barrier(self):
        full = {}
        for e in ("pe", "act", "dve", "pool"):
            if self.count[e] > 0:
                full[e] = self.count[e]
        for q in self.dma_slots:
            for name, v in self.dma_slots[q]:
                if v > 0:
                    full[name] = v
        for e in ENGS:
            d = dict(full)
            d.pop(e, None)
            if e == "pe":
                pass
            self._emit_waits(e, d)

    @contextlib.contextmanager
    def phase(self):
        old = self.cur
        with contextlib.ExitStack() as st:
            self.cur = st
            yield
            self.barrier()
            self.flush()
        self.cur = old

    def flush(self):
        self._emit_block()
        self.streams = {e: [] for e in ENGS}

    def finish(self):
        self.barrier()
        self.flush()
        self.es.close()

    def _emit_block(self):
        nc = self.nc
        with nc.Block() as block:
            @block.tensor
            def _(e):
                for f in self.streams["pe"]:
                    f(e)

            @block.scalar
            def _(e):
                for f in self.streams["act"]:
                    f(e)

            @block.vector
            def _(e):
                for f in self.streams["dve"]:
                    f(e)

            @block.gpsimd
            def _(e):
                for f in self.streams["pool"]:
                    f(e)

            @block.sync
            def _(e):
                for f in self.streams["sp"]:
                    f(e)

import contextlib
import numpy as np

D = 1024
KC = 8
EPS = 1e-6
GLA_COLS = 1552
RW_COLS = 1824
EVEN_IN = 3376


def dram(nc, name, shape, dt=F32, kind="Internal"):
    return nc.dram_tensor(name, list(shape), dt, kind=kind).ap()


def make_consts(p):
    nc = p.nc
    C = {}
    iota_f = p.sb("iota_f", [128, 128], F32)
    iota_p = p.sb("iota_p", [128, 1], F32)
    ii = p.sb("iota_i", [128, 128], I32)
    ip = p.sb("iota_pi", [128, 1], I32)
    p.op("pool", lambda e: e.iota(ii[:], pattern=[[1, 128]], base=0, channel_multiplier=0), w=["iota_i"])
    p.op("pool", lambda e: e.iota(ip[:], pattern=[[1, 1]], base=0, channel_multiplier=1), w=["iota_pi"])
    p.op("dve", lambda e: e.tensor_copy(out=iota_f[:], in_=ii[:]), r=["iota_i"], w=["iota_f"])
    p.op("dve", lambda e: e.tensor_copy(out=iota_p[:], in_=ip[:]), r=["iota_pi"], w=["iota_p"])
    ident = p.sb("ident", [128, 128], F32)
    p.op("dve", lambda e: e.tensor_scalar(out=ident[:], in0=iota_f[:], scalar1=iota_p[:, 0:1], scalar2=None,
                                          op0=ALU.is_equal), r=["iota_f", "iota_p"], w=["ident"])
    identb = p.sb("identb", [128, 128], BF16)
    p.op("dve", lambda e: e.tensor_copy(out=identb[:], in_=ident[:]), r=["ident"], w=["identb"])
    C.update(iota_f=iota_f, iota_p=iota_p, ident=ident, identb=identb)
    return C


def phase_ada(p, C, io, L, G):
    nc = p.nc
    cvec, ada_w, ada_b = io["cvec"], io["ada_w"], io["ada_b"]
    modrow = io["modrow"]
    with p.phase():
        cT = p.sb("cT", [128, KC, 2], F32)
        sT = p.sb("sT", [128, KC, 2], F32)
        for r in range(2):
            p.dma("sp", lambda e, r=r: e.dma_start(out=cT[:, :, r], in_=cvec[r, :].rearrange("(c q) -> q c", q=128),
                                                   allow_slow_non_contiguous=True), w=["cT"])
        p.op("act", lambda e: e.activation(out=sT[:], in_=cT[:], func=AF.Silu), r=["cT"], w=["sT"])
        wts = [p.sb(f"adaw{i}", [128, KC, 512], F32) for i in range(2)]
        mrow = p.sb("mrow", [2, 6144], F32)
        brow = p.sb("brow", [2, 6144], F32)
        mps = p.ps("mps", [2, 512], F32)
        tps = p.ps("tps", [128, 48, 2], F32)
        mcol = p.sb("mcol", [128, 48, 2], F32)
        gcol = p.sb("gcol", [128, 2, KC], F32)
        it = 0
        for l in range(L):
            for r in range(2):
                p.dma("sp", lambda e, r=r, l=l: e.dma_start(out=brow[r:r + 1, :], in_=ada_b[l:l + 1, :]), w=["brow"])
            p.dma("sp", lambda e, l=l: e.dma_start(out=gcol[:, 0, :], in_=io["norm1_g"][l, :].rearrange("(c q) -> q c", q=128),
                                                   allow_slow_non_contiguous=True), w=["gcol"])
            p.dma("sp", lambda e, l=l: e.dma_start(out=gcol[:, 1, :], in_=io["norm2_g"][l, :].rearrange("(c q) -> q c", q=128),
                                                   allow_slow_non_contiguous=True), w=["gcol"])
            for cc in range(12):
                wt = wts[it % 2]
                wk = f"adaw{it % 2}"
                it += 1
                p.dma("sp", lambda e, wt=wt, l=l, cc=cc: e.dma_start(
                    out=wt[:], in_=ada_w[l, :, cc * 512:(cc + 1) * 512].rearrange("(c q) n -> q c n", q=128)), w=[wk])
                for kc in range(KC):
                    p.op("pe", lambda e, wt=wt, kc=kc: e.matmul(mps[:], lhsT=sT[:, kc, :], rhs=wt[:, kc, :],
                                                                 start=(kc == 0), stop=(kc == KC - 1)),
                         r=["sT", wk], w=["mps"])
                p.op("dve", lambda e, cc=cc: e.tensor_tensor(out=mrow[:, cc * 512:(cc + 1) * 512], in0=mps[:],
                                                             in1=brow[:, cc * 512:(cc + 1) * 512], op=ALU.add),
                     r=["mps", "brow"], w=["mrow"])
            p.dma("sp", lambda e, l=l: e.dma_start(out=modrow[l], in_=mrow[:]), r=["mrow"], w=[f"modrow{l}"])
            for c in range(48):
                p.op("pe", lambda e, c=c: e.transpose(tps[:, c, :], mrow[0:2, c * 128:(c + 1) * 128], C["ident"][0:2, 0:2]),
                     r=["mrow", "ident"], w=["tps"])
            p.op("dve", lambda e: e.tensor_copy(out=mcol[:], in_=tps[:]), r=["tps"], w=["mcol"])
            for r in range(2):
                for (nm, sc_c, sh_c, gi) in (("1", 1, 0, 0), ("2", 4, 3, 1)):
                    A = G["A" + nm]
                    B = G["B" + nm]
                    p.op("dve", lambda e, A=A, r=r, l=l, sc_c=sc_c, gi=gi: e.scalar_tensor_tensor(
                        out=A[:, l, r, :], in0=mcol[:, sc_c * 8:(sc_c + 1) * 8, r], scalar=1.0, in1=gcol[:, gi, :],
                        op0=ALU.add, op1=ALU.mult), r=["mcol", "gcol"], w=["G"])
                    p.op("dve", lambda e, B=B, r=r, l=l, sh_c=sh_c: e.tensor_copy(
                        out=B[:, l, r, :], in_=mcol[:, sh_c * 8:(sh_c + 1) * 8, r]), r=["mcol"], w=["G"])


def norm_tile(p, C, T, xt_key, xt, Acol, Bcol, hT_dst, hT_key, hT32_dst=None, want_bf=True):
    ss, rstd, xn, junk = T["ss"], T["rstd"], T["xn"], T["junk"]
    p.op("act", lambda e: e.activation(out=junk[:], in_=xt[:], func=AF.Square, accum_out=ss[:, 0:1]),
         r=[xt_key], w=["junk", "ss"])
    p.op("dve", lambda e: e.tensor_scalar(out=rstd[:], in0=ss[:], scalar1=1.0 / D, scalar2=EPS, op0=ALU.mult, op1=ALU.add),
         r=["ss"], w=["rstd"])
    p.op("act", lambda e: e.activation(out=ss[:], in_=rstd[:], func=AF.Sqrt), r=["rstd"], w=["ss"])
    p.op("dve", lambda e: e.reciprocal(out=rstd[:], in_=ss[:]), r=["ss"], w=["rstd"])
    p.op("dve", lambda e: e.tensor_scalar(out=xn[:], in0=xt[:], scalar1=rstd[:, 0:1], scalar2=None, op0=ALU.mult),
         r=[xt_key, "rstd"], w=["xn"])
    for half in range(2):
        tp = T["tp"][half]
        tk = f"tp{half}"
        for j in range(4):
            kc = half * 4 + j
            p.op("pe", lambda e, tp=tp, j=j, kc=kc: e.transpose(tp[:, j, :], xn[:, kc * 128:(kc + 1) * 128], C["ident"][:]),
                 r=["xn", "ident"], w=[tk])
        for j in range(4):
            kc = half * 4 + j
            if hT32_dst is None:
                p.op("act", lambda e, tp=tp, j=j, kc=kc: e.activation(out=hT_dst(kc), in_=tp[:, j, :], func=AF.Identity,
                                                                       scale=Acol[:, kc:kc + 1], bias=Bcol[:, kc:kc + 1]),
                     r=[tk, "G"], w=[hT_key])
            else:
                p.op("act", lambda e, tp=tp, j=j, kc=kc: e.activation(out=hT32_dst(kc), in_=tp[:, j, :], func=AF.Identity,
                                                                       scale=Acol[:, kc:kc + 1], bias=Bcol[:, kc:kc + 1]),
                     r=[tk, "G"], w=[hT_key + "32"])
                if want_bf:
                    p.op("pool", lambda e, kc=kc: e.tensor_copy(out=hT_dst(kc), in_=hT32_dst(kc)), r=[hT_key + "32"], w=[hT_key])


def norm_scratch(p):
    T = {}
    T["ss"] = p.sb("ss", [128, 1], F32)
    T["rstd"] = p.sb("rstd", [128, 1], F32)
    T["xn"] = p.sb("xn", [128, D], F32)
    T["junk"] = p.sb("junk", [128, D], BF16)
    T["tp"] = [p.ps(f"tp{h}", [128, 4, 128], F32) for h in range(2)]
    return T


def load_cast_weight(p, dst, dst_key, src_ap, rows_kc, ncols, stage, stage_key, eng_cycle=("dve", "pool")):
    for kc in range(rows_kc):
        st = stage[kc % len(stage)]
        sk = f"{stage_key}{kc % len(stage)}"
        p.dma("sp", lambda e, st=st, kc=kc: e.dma_start(out=st[:, 0:ncols], in_=src_ap[kc * 128:(kc + 1) * 128, :]), w=[sk])
        eng = eng_cycle[kc % len(eng_cycle)]
        p.op(eng, lambda e, st=st, kc=kc: e.tensor_copy(out=dst[:, kc, :], in_=st[:, 0:ncols]), r=[sk], w=[dst_key])


def phase_inproj(p, C, io, G, l, n_ctx, n_tok):
    xres, PT, PTOK, w_in = io["xres"], io["PT"], io["PTOK"], io["ev_w_in"]
    with p.phase():
        T = norm_scratch(p)
        wbf = p.sb("winbf", [128, KC, EVEN_IN], BF16)
        stage = [p.sb(f"wstage{i}", [128, EVEN_IN], F32) for i in range(2)]
        load_cast_weight(p, wbf, "winbf", w_in, KC, EVEN_IN, stage, "wstage")
        xts = [p.sb(f"xt{i}", [128, D], F32) for i in range(2)]
        hT = [p.sb(f"hT{i}", [128, KC, 512], BF16) for i in range(2)]
        pps = [p.ps(f"pps{i}", [128, 512], F32) for i in range(3)]
        ost = [p.sb(f"ost{i}", [128, 512], F32) for i in range(4)]
        ntile = n_tok // 128
        nsup = (ntile + 3) // 4
        oi = 0
        pi = 0
        tok_cols = [(512, 512), (1024, 512), (GLA_COLS + 1024, 512)]
        for s in range(nsup):
            tiles = list(range(s * 4, min(ntile, s * 4 + 4)))
            N = len(tiles) * 128
            h = hT[s % 2]
            hk = f"hT{s % 2}"
            for j, ti in enumerate(tiles):
                r = 1 if ti * 128 < n_ctx else 0
                xt = xts[ti % 2]
                xk = f"xt{ti % 2}"
                p.dma("sp", lambda e, xt=xt, ti=ti: e.dma_start(out=xt[:], in_=xres[ti * 128:(ti + 1) * 128, :]), w=[xk])
                norm_tile(p, C, T, xk, xt, G["A1"][:, l, r, :], G["B1"][:, l, r, :],
                          lambda kc, h=h, j=j: h[:, kc, j * 128:(j + 1) * 128], hk)
            ncc = (EVEN_IN + 127) // 128
            for cc in range(ncc):
                cw = min(128, EVEN_IN - cc * 128)
                ps = pps[pi % 3]
                pk = f"pps{pi % 3}"
                pi += 1
                for kc in range(KC):
                    p.op("pe", lambda e, ps=ps, kc=kc, cc=cc, cw=cw, h=h, N=N: e.matmul(
                        ps[0:cw, 0:N], lhsT=wbf[:, kc, cc * 128:cc * 128 + cw], rhs=h[:, kc, 0:N],
                        start=(kc == 0), stop=(kc == KC - 1)), r=["winbf", hk], w=[pk])
                o = ost[oi % 4]
                ok = f"ost{oi % 4}"
                eng = "dve" if oi % 2 == 0 else "act"
                oi += 1
                if eng == "dve":
                    p.op("dve", lambda e, o=o, ps=ps, cw=cw, N=N: e.tensor_copy(out=o[0:cw, 0:N], in_=ps[0:cw, 0:N]), r=[pk], w=[ok])
                else:
                    p.op("act", lambda e, o=o, ps=ps, cw=cw, N=N: e.copy(out=o[0:cw, 0:N], in_=ps[0:cw, 0:N]), r=[pk], w=[ok])
                p.dma("sp", lambda e, o=o, cc=cc, cw=cw, N=N, s=s: e.dma_start(
                    out=PT[cc * 128:cc * 128 + cw, s * 512:s * 512 + N], in_=o[0:cw, 0:N]), r=[ok], w=["PT"])
            for j, ti in enumerate(tiles):
                for gi, (c0, cn) in enumerate(tok_cols):
                    ps = pps[pi % 3]
                    pk = f"pps{pi % 3}"
                    pi += 1
                    for kc in range(KC):
                        p.op("pe", lambda e, ps=ps, kc=kc, c0=c0, cn=cn, h=h, j=j: e.matmul(
                            ps[:, 0:cn], lhsT=h[:, kc, j * 128:(j + 1) * 128], rhs=wbf[:, kc, c0:c0 + cn],
                            start=(kc == 0), stop=(kc == KC - 1)), r=["winbf", hk], w=[pk])
                    o = ost[oi % 4]
                    ok = f"ost{oi % 4}"
                    eng = "dve" if oi % 2 == 0 else "act"
                    oi += 1
                    if eng == "dve":
                        p.op("dve", lambda e, o=o, ps=ps, cn=cn: e.tensor_copy(out=o[:, 0:cn], in_=ps[:, 0:cn]), r=[pk], w=[ok])
                    else:
                        p.op("act", lambda e, o=o, ps=ps, cn=cn: e.copy(out=o[:, 0:cn], in_=ps[:, 0:cn]), r=[pk], w=[ok])
                    p.dma("sp", lambda e, o=o, ti=ti, gi=gi, cn=cn: e.dma_start(
                        out=PTOK[ti * 128:(ti + 1) * 128, gi * 512:gi * 512 + cn], in_=o[:, 0:cn]), r=[ok], w=["PTOK"])

import os


class Ring:
    def __init__(self, p, name, shape, dt=F32, n=2, psum=False):
        self.t = [(p.ps if psum else p.sb)(f"{name}{i}", shape, dt) for i in range(n)]
        self.k = [f"{name}{i}" for i in range(n)]
        self.i = 0

    def next(self):
        i = self.i
        self.i = (i + 1) % len(self.t)
        return self.t[i], self.k[i]


class PsRing:
    def __init__(self, p, name, nbanks):
        self.banks = [p.ps(f"{name}{i}", [128, 512], F32) for i in range(nbanks)]
        self.n = nbanks * 1
        self.sub = 1
        self.name = name
        self.i = 0

    def next(self):
        i = self.i
        self.i = (i + 1) % self.n
        sb_ = self.sub
        return self.banks[i // sb_][:, (i % sb_) * 128:(i % sb_ + 1) * 128], f"{self.name}_{i}"


def make_masks(p, C):
    f, q = C["iota_f"], C["iota_p"]
    for nm, op in (("incl0", ALU.is_ge), ("incl1", ALU.is_le), ("strict0", ALU.is_gt), ("strict1", ALU.is_lt)):
        m = p.sb("m_" + nm, [128, 128], F32)
        p.op("dve", lambda e, m=m, op=op: e.tensor_scalar(out=m[:], in0=f[:], scalar1=q[:, 0:1], scalar2=None, op0=op),
             r=["iota_f", "iota_p"], w=["masks"])
        C[nm] = m
    for nm, val in (("ones128", 1.0 / 128), ("ones64", 1.0), ("ones64m", 1.0 / 64)):
        m = p.sb("m_" + nm, [128, 128], F32)
        p.op("dve", lambda e, m=m, val=val: e.memset(m[:], val), w=["masks"])
        C[nm] = m


def chunk_order(n_ctx, n_tok):
    nc_, nt = n_ctx // 128, n_tok // 128
    fwd = list(range(nt))
    bwd = list(range(nc_ - 1, -1, -1)) + list(range(nt - 1, nc_ - 1, -1))
    return [fwd, bwd]


def phase_gla(p, C, io, n_ctx, n_tok):
    PT, PTOK, YT = io["PT"], io["PTOK"], io["YTG"]
    gk_up, gk_b = io["gla_gk_up"], io["gla_gk_b"]
    order = chunk_order(n_ctx, n_tok)
    nt = n_tok // 128
    with p.phase():
        gkup = p.sb("gkup", [16, 2, 256], F32)
        gkb = p.sb("gkb", [64, 2, 4], F32)
        for d in range(2):
            p.dma("sp", lambda e, d=d: e.dma_start(out=gkup[:, d, :], in_=gk_up[d]), w=["gkw"])
            p.dma("sp", lambda e, d=d: e.dma_start(out=gkb[:, d, :], in_=gk_b[d].rearrange("(h q) -> q h", q=64),
                                                   allow_slow_non_contiguous=True), w=["gkw"])
        S = [[p.sb(f"S{d}{h}", [64, 128], F32) for h in range(4)] for d in range(2)]
        for d in range(2):
            for h in range(4):
                p.op("pool", lambda e, t=S[d][h]: e.memset(t[:], 0.0), w=[f"S{d}{h}"])
        gd = Ring(p, "gd", [16, 128], n=4)
        qk = Ring(p, "qk", [64, 2, 128], n=9)
        vv = Ring(p, "vv", [128, 128], n=9)
        sg = Ring(p, "sg", [64, 128], n=9)
        lg = Ring(p, "lg", [64, 128], n=9)
        lgT = Ring(p, "lgT", [128, 64], n=9)
        eW = Ring(p, "eW", [64, 128], n=9)
        eWi = Ring(p, "eWi", [64, 128], n=9)
        rt = Ring(p, "rt", [64, 128], n=9)
        kt = Ring(p, "kt", [64, 128], n=9)
        ktok = Ring(p, "ktok", [128, 64], n=9)
        mrk = Ring(p, "mrk", [128, 128], n=9)
        yts = Ring(p, "yts", [128, 128], n=9)
        s2 = Ring(p, "s2", [64, 128], n=9)
        psA = Ring(p, "psA", [128, 128], n=8, psum=True)
        def unit(d, h, c, gdt, gdk):
            t0 = c * 128
            tend = 127 if d == 0 else 0
            Sk = f"S{d}{h}"
            St = S[d][h]
            qt, qkk = qk.next()
            p.dma("sp", lambda e, qt=qt, h=h, t0=t0: e.dma_start(out=qt[:, 0, :], in_=PT[h * 64:(h + 1) * 64, t0:t0 + 128]), w=[qkk])
            p.dma("sp", lambda e, qt=qt, h=h, t0=t0: e.dma_start(out=qt[:, 1, :], in_=PT[256 + h * 64:256 + (h + 1) * 64, t0:t0 + 128]), w=[qkk])
            vt, vk = vv.next()
            p.dma("sp", lambda e, vt=vt, h=h, t0=t0: e.dma_start(out=vt[:], in_=PTOK[t0:t0 + 128, h * 128:(h + 1) * 128]), w=[vk])
            yield
            ps, pk = psA.next()
            p.op("pe", lambda e, ps=ps, d=d, h=h, gdt=gdt: e.matmul(ps[0:64, :], lhsT=gkup[:, d, h * 64:(h + 1) * 64], rhs=gdt[:],
                                                                    start=True, stop=True), r=["gkw", gdk], w=[pk])
            sgt, sgk = sg.next()
            p.op("act", lambda e, sgt=sgt, ps=ps, d=d, h=h: e.activation(out=sgt[:], in_=ps[0:64, :], func=AF.Sigmoid,
                                                                        bias=gkb[:, d, h:h + 1]), r=[pk, "gkw"], w=[sgk])
            lgt, lgk = lg.next()
            p.op("act", lambda e, lgt=lgt, sgt=sgt: e.activation(out=lgt[:], in_=sgt[:], func=AF.Ln), r=[sgk], w=[lgk])
            yield
            ps, pk = psA.next()
            p.op("pe", lambda e, ps=ps, lgt=lgt: e.transpose(ps[:, 0:64], lgt[:], C["ident"][0:64, 0:64]), r=[lgk, "ident"], w=[pk])
            lTt, lTk = lgT.next()
            p.op("dve", lambda e, lTt=lTt, ps=ps: e.tensor_copy(out=lTt[:], in_=ps[:, 0:64]), r=[pk], w=[lTk])
            yield
            ps, pk = psA.next()
            p.op("pe", lambda e, ps=ps, lTt=lTt, d=d: e.matmul(ps[0:64, :], lhsT=lTt[:], rhs=C[f"incl{d}"][:], start=True, stop=True),
                 r=[lTk, "masks"], w=[pk])
            eWt, eWk = eW.next()
            eWit, eWik = eWi.next()
            p.op("act", lambda e, eWt=eWt, ps=ps: e.activation(out=eWt[:], in_=ps[0:64, :], func=AF.Exp, scale=1.0 / 16), r=[pk], w=[eWk])
            p.op("act", lambda e, eWit=eWit, ps=ps: e.activation(out=eWit[:], in_=ps[0:64, :], func=AF.Exp, scale=-1.0 / 16), r=[pk], w=[eWik])
            rtt, rtk = rt.next()
            ktt, ktk = kt.next()
            p.op("dve", lambda e, rtt=rtt, qt=qt, eWt=eWt: e.scalar_tensor_tensor(out=rtt[:], in0=qt[:, 0, :], scalar=0.125, in1=eWt[:],
                                                                                 op0=ALU.mult, op1=ALU.mult), r=[qkk, eWk], w=[rtk])
            p.op("dve", lambda e, ktt=ktt, qt=qt, eWit=eWit: e.tensor_tensor(out=ktt[:], in0=qt[:, 1, :], in1=eWit[:], op=ALU.mult),
                 r=[qkk, eWik], w=[ktk])
            yield
            ps, pk = psA.next()
            p.op("pe", lambda e, ps=ps, ktt=ktt: e.transpose(ps[:, 0:64], ktt[:], C["ident"][0:64, 0:64]), r=[ktk, "ident"], w=[pk])
            kTt, kTk = ktok.next()
            p.op("dve", lambda e, kTt=kTt, ps=ps: e.tensor_copy(out=kTt[:], in_=ps[:, 0:64]), r=[pk], w=[kTk])
            yield
            ps, pk = psA.next()
            p.op("pe", lambda e, ps=ps, ktt=ktt, rtt=rtt: e.matmul(ps[:], lhsT=ktt[:], rhs=rtt[:], start=True, stop=True), r=[ktk, rtk], w=[pk])
            mt, mk = mrk.next()
            p.op("dve", lambda e, mt=mt, ps=ps, d=d: e.tensor_tensor(out=mt[:], in0=ps[:], in1=C[f"incl{d}"][:], op=ALU.mult),
                 r=[pk, "masks"], w=[mk])
            yield
            ps, pk = psA.next()
            p.op("pe", lambda e, ps=ps, St=St, rtt=rtt: e.matmul(ps[:], lhsT=St[:], rhs=rtt[:], start=True, stop=False), r=[Sk, rtk], w=[pk])
            p.op("pe", lambda e, ps=ps, vt=vt, mt=mt: e.matmul(ps[:], lhsT=vt[:], rhs=mt[:], start=False, stop=True), r=[vk, mk], w=[pk])
            yt, yk = yts.next()
            p.op("act", lambda e, yt=yt, ps=ps: e.copy(out=yt[:], in_=ps[:]), r=[pk], w=[yk])
            p.dma("sp", lambda e, yt=yt, d=d, h=h, t0=t0: e.dma_start(out=YT[d, h * 128:(h + 1) * 128, t0:t0 + 128], in_=yt[:]),
                  r=[yk], w=["YTG"])
            yield
            ps, pk = psA.next()
            p.op("pe", lambda e, ps=ps, kTt=kTt, vt=vt: e.matmul(ps[0:64, :], lhsT=kTt[:], rhs=vt[:], start=True, stop=True), r=[kTk, vk], w=[pk])
            s2t, s2k = s2.next()
            p.op("dve", lambda e, s2t=s2t, St=St, eWt=eWt, tend=tend: e.tensor_scalar(out=s2t[:], in0=St[:], scalar1=eWt[:, tend:tend + 1],
                                                                                     scalar2=None, op0=ALU.mult), r=[Sk, eWk], w=[s2k])
            p.op("dve", lambda e, s2t=s2t, St=St, eWt=eWt, ps=ps, tend=tend: e.scalar_tensor_tensor(
                out=St[:], in0=ps[0:64, :], scalar=eWt[:, tend:tend + 1], in1=s2t[:], op0=ALU.mult, op1=ALU.add),
                r=[pk, eWk, s2k], w=[Sk])


        def drive(gens):
            gens = list(gens)
            while gens:
                for g_ in list(gens):
                    try:
                        next(g_)
                    except StopIteration:
                        gens.remove(g_)

        for i in range(nt):
            gens = []
            for d in range(2):
                c = order[d][i]
                t0 = c * 128
                gdt, gdk = gd.next()
                p.dma("sp", lambda e, gdt=gdt, t0=t0: e.dma_start(out=gdt[:], in_=PT[1536:1552, t0:t0 + 128]), w=[gdk])
                gens += [unit(d, h, c, gdt, gdk) for h in range(4)]
            drive(gens)


def phase_gla_finish(p, C, io, n_tok):
    PT, YT, MIXT = io["PT"], io["YTG"], io["MIXT"]
    with p.phase():
        gcol = p.sb("gng", [128, 1], F32)
        p.dma("sp", lambda e: e.dma_start(out=gcol[:, 0:1], in_=io["gla_norm_g"].rearrange("(q o) -> q o", o=1)), w=["gng"])
        y0 = Ring(p, "y0", [128, 512], n=2)
        y1 = Ring(p, "y1", [128, 512], n=2)
        og = Ring(p, "og", [128, 512], n=2)
        sq = Ring(p, "sq", [128, 512], n=2)
        rs = Ring(p, "rs", [128, 512], n=2)
        ob = Ring(p, "ob", [128, 512], BF16, n=2)
        psF = Ring(p, "psF", [128, 512], n=2, psum=True)
        for t0 in range(0, n_tok, 512):
            N = min(512, n_tok - t0)
            for h in range(4):
                a, ak = y0.next()
                b, bk = y1.next()
                o, ok = og.next()
                p.dma("sp", lambda e, a=a, h=h, t0=t0, N=N: e.dma_start(out=a[:, 0:N], in_=YT[0, h * 128:(h + 1) * 128, t0:t0 + N]), r=["YTG"], w=[ak])
                p.dma("sp", lambda e, b=b, h=h, t0=t0, N=N: e.dma_start(out=b[:, 0:N], in_=YT[1, h * 128:(h + 1) * 128, t0:t0 + N]), r=["YTG"], w=[bk])
                p.dma("sp", lambda e, o=o, h=h, t0=t0, N=N: e.dma_start(out=o[:, 0:N], in_=PT[1024 + h * 128:1024 + (h + 1) * 128, t0:t0 + N]), r=["PT"], w=[ok])
                p.op("dve", lambda e, a=a, b=b, N=N: e.tensor_tensor(out=a[:, 0:N], in0=a[:, 0:N], in1=b[:, 0:N], op=ALU.add), r=[ak, bk], w=[ak])
                s, sk = sq.next()
                p.op("act", lambda e, s=s, a=a, N=N: e.activation(out=s[:, 0:N], in_=a[:, 0:N], func=AF.Square), r=[ak], w=[sk])
                ps, pk = psF.next()
                p.op("pe", lambda e, ps=ps, s=s, N=N: e.matmul(ps[:, 0:N], lhsT=C["ones128"][:], rhs=s[:, 0:N], start=True, stop=True), r=[sk, "masks"], w=[pk])
                r_, rk = rs.next()
                p.op("dve", lambda e, r_=r_, ps=ps, N=N: e.tensor_scalar(out=r_[:, 0:N], in0=ps[:, 0:N], scalar1=EPS, scalar2=None, op0=ALU.add), r=[pk], w=[rk])
                p.op("act", lambda e, r_=r_, N=N: e.activation(out=r_[:, 0:N], in_=r_[:, 0:N], func=AF.Sqrt), r=[rk], w=[rk])
                p.op("dve", lambda e, r_=r_, N=N: e.reciprocal(out=r_[:, 0:N], in_=r_[:, 0:N]), r=[rk], w=[rk])
                p.op("dve", lambda e, a=a, r_=r_, N=N: e.scalar_tensor_tensor(out=a[:, 0:N], in0=a[:, 0:N], scalar=gcol[:, 0:1], in1=r_[:, 0:N],
                                                                             op0=ALU.mult, op1=ALU.mult), r=[ak, rk, "gng"], w=[ak])
                p.op("act", lambda e, s=s, o=o, N=N: e.activation(out=s[:, 0:N], in_=o[:, 0:N], func=AF.Silu), r=[ok], w=[sk])
                ot, otk = ob.next()
                p.op("dve", lambda e, ot=ot, a=a, s=s, N=N: e.tensor_tensor(out=ot[:, 0:N], in0=a[:, 0:N], in1=s[:, 0:N], op=ALU.mult), r=[ak, sk], w=[otk])
                p.dma("sp", lambda e, ot=ot, h=h, t0=t0, N=N: e.dma_start(out=MIXT[h * 128:(h + 1) * 128, t0:t0 + N], in_=ot[:, 0:N]), r=[otk], w=["MIXT"])


import os
STAGE = 99
R0 = GLA_COLS
NSQ = 12
NLW = 0.6065306597126334


def phase_shift(p, C, io, n_ctx, n_tok):
    PT, SMT, mu = io["PT"], io["SMT"], io["rw_mu"]
    with p.phase():
        nch = (RW_COLS + 127) // 128
        muc = p.sb("muc", [128, nch, 2], F32)
        p.op("pool", lambda e: e.memset(muc[:], 0.0), w=["muc"])
        for j in range(2):
            for c in range(nch):
                cw = min(128, RW_COLS - c * 128)
                p.dma("sp", lambda e, j=j, c=c, cw=cw: e.dma_start(out=muc[0:cw, c, j:j + 1],
                                                                   in_=mu[j, c * 128:c * 128 + cw].rearrange("(q o) -> q o", o=1)), w=["muc"])
        xin = Ring(p, "shx", [128, 514], n=3)
        d0 = Ring(p, "shd", [128, 512], n=3)
        so = Ring(p, "sho", [128, 512], n=3)
        blocks = []
        for (a, b) in ((0, n_ctx), (n_ctx, n_tok)):
            t = a
            while t < b:
                N = min(512, b - t)
                blocks.append((t, N, t == a, t + N == b))
                t += N
        for (t0, N, first, last) in blocks:
            for c in range(nch):
                cw = min(128, RW_COLS - c * 128)
                x, xk = xin.next()
                lo = 0 if not first else 1
                hi = N + 2 if not last else N + 1
                if first or last:
                    p.op("pool", lambda e, x=x: e.memset(x[:], 0.0), w=[xk])
                p.dma("sp", lambda e, x=x, c=c, cw=cw, lo=lo, hi=hi, t0=t0: e.dma_start(
                    out=x[0:cw, lo:hi], in_=PT[R0 + c * 128:R0 + c * 128 + cw, t0 - 1 + lo:t0 - 1 + hi]), w=[xk])
                dd, dk = d0.next()
                o, ok = so.next()
                p.op("dve", lambda e, dd=dd, x=x, cw=cw, N=N: e.tensor_tensor(out=dd[0:cw, 0:N], in0=x[0:cw, 0:N], in1=x[0:cw, 1:N + 1], op=ALU.subtract),
                     r=[xk], w=[dk])
                p.op("dve", lambda e, o=o, dd=dd, x=x, c=c, cw=cw, N=N: e.scalar_tensor_tensor(
                    out=o[0:cw, 0:N], in0=dd[0:cw, 0:N], scalar=muc[0:cw, c, 0:1], in1=x[0:cw, 1:N + 1], op0=ALU.mult, op1=ALU.add),
                    r=[dk, xk, "muc"], w=[ok])
                p.op("pool", lambda e, dd=dd, x=x, cw=cw, N=N: e.tensor_tensor(out=dd[0:cw, 0:N], in0=x[0:cw, 2:N + 2], in1=x[0:cw, 1:N + 1], op=ALU.subtract),
                     r=[xk, ok], w=[dk])
                p.op("dve", lambda e, o=o, dd=dd, c=c, cw=cw, N=N: e.scalar_tensor_tensor(
                    out=o[0:cw, 0:N], in0=dd[0:cw, 0:N], scalar=muc[0:cw, c, 1:2], in1=o[0:cw, 0:N], op0=ALU.mult, op1=ALU.add),
                    r=[dk, ok, "muc"], w=[ok])
                p.dma("sp", lambda e, o=o, c=c, cw=cw, N=N, t0=t0: e.dma_start(out=SMT[c * 128:c * 128 + cw, t0:t0 + N], in_=o[0:cw, 0:N]),
                      r=[ok], w=["SMT"])


def colload(p, dst, key, src_vec, nh, j=None):
    out = dst[:, :] if j is None else dst[:, j, :]
    p.dma("sp", lambda e: e.dma_start(out=out, in_=src_vec.rearrange("(h q) -> q h", q=64), allow_slow_non_contiguous=True), w=[key])


def phase_rwkv(p, C, io, n_ctx, n_tok):
    SMT, YT, RK = io["SMT"], io["YTR"], io["RK"]
    order = chunk_order(n_ctx, n_tok)
    nt = n_tok // 128
    with p.phase():
        w2 = p.sb("rw2", [64, 2, 512], F32)
        a2 = p.sb("ra2", [64, 2, 512], F32)
        for d in range(2):
            p.dma("sp", lambda e, d=d: e.dma_start(out=w2[:, d, :], in_=io["rw_w2"][d]), w=["rww"])
            p.dma("sp", lambda e, d=d: e.dma_start(out=a2[:, d, :], in_=io["rw_a2"][d]), w=["rww"])
        w0 = p.sb("rw0", [64, 2, 8], F32)
        a0 = p.sb("ra0", [64, 2, 8], F32)
        for d in range(2):
            colload(p, w0, "rww", io["rw_w0"][d], 8, d)
            colload(p, a0, "rww", io["rw_a0"][d], 8, d)
        kkc = p.sb("rkk", [64, 8], F32)
        kac = p.sb("rka", [64, 8], F32)
        kam = p.sb("rkam", [64, 8], F32)
        rkc = p.sb("rrk", [64, 8], F32)
        colload(p, kkc, "rww", io["rw_k_k"], 8)
        colload(p, kac, "rww", io["rw_k_a"], 8)
        colload(p, rkc, "rww", io["rw_r_k"].rearrange("h n -> (h n)"), 8)
        p.op("dve", lambda e: e.tensor_scalar(out=kam[:], in0=kac[:], scalar1=-1.0, scalar2=1.0, op0=ALU.mult, op1=ALU.add), r=["rww"], w=["rkam"])
        T = [[p.sb(f"T{d}{h}", [64, 64], F32) for h in range(8)] for d in range(2)]
        for d in range(2):
            for h in range(8):
                p.op("pool", lambda e, t=T[d][h]: e.memset(t[:], 0.0), w=[f"T{d}{h}"])
        wa = Ring(p, "wa", [64, 2, 128], n=3)
        rkv = Ring(p, "rkv", [64, 3, 128], n=9)
        f64 = {nm: Ring(p, nm, [64, 128], n=9) for nm in
               ("sgw", "alp", "kq", "sqk", "nrm", "kk", "tmk", "kd", "eW", "eWi", "eWx", "cx", "rt", "kt", "bt", "at", "rk_", "yo")}
        tokr = {nm: Ring(p, nm, [128, 64], n=9) for nm in ("sgT", "Ktok", "Btok", "Vtok", "Xs", "Us")}
        sqm = {nm: Ring(p, nm, [128, 128], n=9) for nm in ("Akt", "Mrbt", "Mrkt")}
        sqh = [{nm: Ring(p, f"{nm}h{h_}", [128, 128], n=3) for nm in ("At", "Am", "Pt")} for h_ in range(8)]
        tw = Ring(p, "tw", [64, 64], n=9)
        psA = PsRing(p, "psR", 8)

        def mm(out_ap, pk, lhsT, rhs, r, start=True, stop=True):
            p.op("pe", lambda e: e.matmul(out_ap, lhsT=lhsT, rhs=rhs, start=start, stop=stop), r=r, w=[pk])

        def tr(out_ap, pk, in_ap, n, r):
            p.op("pe", lambda e: e.transpose(out_ap, in_ap, C["ident"][0:n, 0:n]), r=r + ["ident"], w=[pk])

        def unit(d, h, c, wat, wak):
            t0 = c * 128
            tend = 127 if d == 0 else 0
            sl = slice(t0, t0 + 128)
            hs = slice(h * 64, (h + 1) * 64)
            Tt, Tk = T[d][h], f"T{d}{h}"
            x, xk = rkv.next()
            for j in range(3):
                p.dma("sp", lambda e, x=x, j=j, h=h, sl=sl: e.dma_start(out=x[:, j, :], in_=SMT[j * 512 + h * 64:j * 512 + (h + 1) * 64, sl]), w=[xk])
            r_, k_, v_ = x[:, 0, :], x[:, 1, :], x[:, 2, :]
            yield
            ps, pk = psA.next()
            mm(ps[0:64, :], pk, w2[:, d, hs], wat[:, 0, :], ["rww", wak])
            sgw, sgwk = f64["sgw"].next()
            p.op("act", lambda e, sgw=sgw, ps=ps, d=d, h=h: e.activation(out=sgw[:], in_=ps[0:64, :], func=AF.Sigmoid, bias=w0[:, d, h:h + 1]),
                 r=[pk, "rww"], w=[sgwk])
            yield
            ps, pk = psA.next()
            mm(ps[0:64, :], pk, a2[:, d, hs], wat[:, 1, :], ["rww", wak])
            alp, alpk = f64["alp"].next()
            p.op("act", lambda e, alp=alp, ps=ps, d=d, h=h: e.activation(out=alp[:], in_=ps[0:64, :], func=AF.Sigmoid, bias=a0[:, d, h:h + 1]),
                 r=[pk, "rww"], w=[alpk])
            kq, kqk = f64["kq"].next()
            p.op("dve", lambda e, kq=kq, k_=k_, h=h: e.tensor_scalar(out=kq[:], in0=k_, scalar1=kkc[:, h:h + 1], scalar2=None, op0=ALU.mult),
                 r=[xk, "rww"], w=[kqk])
            sqk, sqkk = f64["sqk"].next()
            p.op("act", lambda e, sqk=sqk, kq=kq: e.activation(out=sqk[:], in_=kq[:], func=AF.Square), r=[kqk], w=[sqkk])
            yield
            ps, pk = psA.next()
            mm(ps[0:64, :], pk, C["ones64"][0:64, 0:64], sqk[:], ["masks", sqkk])
            nrm, nrmk = f64["nrm"].next()
            p.op("act", lambda e, nrm=nrm, ps=ps: e.activation(out=nrm[:], in_=ps[0:64, :], func=AF.Sqrt), r=[pk], w=[nrmk])
            p.op("dve", lambda e, nrm=nrm: e.tensor_scalar(out=nrm[:], in0=nrm[:], scalar1=1e-12, scalar2=None, op0=ALU.max), r=[nrmk], w=[nrmk])
            p.op("dve", lambda e, nrm=nrm: e.reciprocal(out=nrm[:], in_=nrm[:]), r=[nrmk], w=[nrmk])
            kk, kkk = f64["kk"].next()
            p.op("dve", lambda e, kk=kk, kq=kq, nrm=nrm: e.tensor_tensor(out=kk[:], in0=kq[:], in1=nrm[:], op=ALU.mult), r=[kqk, nrmk], w=[kkk])
            tmk, tmkk = f64["tmk"].next()
            p.op("dve", lambda e, tmk=tmk, alp=alp, h=h: e.tensor_scalar(out=tmk[:], in0=alp[:], scalar1=kac[:, h:h + 1], scalar2=kam[:, h:h + 1],
                                                                        op0=ALU.mult, op1=ALU.add), r=[alpk, "rww", "rkam"], w=[tmkk])
            kd, kdk = f64["kd"].next()
            p.op("dve", lambda e, kd=kd, tmk=tmk, k_=k_: e.tensor_tensor(out=kd[:], in0=tmk[:], in1=k_, op=ALU.mult), r=[tmkk, xk], w=[kdk])
            rk_, rkk = f64["rk_"].next()
            p.op("dve", lambda e, rk_=rk_, r_=r_, kd=kd, h=h: e.scalar_tensor_tensor(out=rk_[:], in0=r_, scalar=rkc[:, h:h + 1], in1=kd[:],
                                                                                     op0=ALU.mult, op1=ALU.mult), r=[xk, kdk, "rww"], w=[rkk])
            p.dma("sp", lambda e, rk_=rk_, d=d, hs=hs, sl=sl: e.dma_start(out=RK[d, hs, sl], in_=rk_[:]), r=[rkk], w=["RK"])
            yield
            ps, pk = psA.next()
            tr(ps[:, 0:64], pk, sgw[:], 64, [sgwk])
            sgT, sgTk = tokr["sgT"].next()
            p.op("dve", lambda e, sgT=sgT, ps=ps: e.tensor_copy(out=sgT[:], in_=ps[:, 0:64]), r=[pk], w=[sgTk])
            yield
            ps, pk = psA.next()
            mm(ps[0:64, :], pk, sgT[:], C[f"incl{d}"][:], [sgTk, "masks"])
            eW, eWk = f64["eW"].next()
            eWi, eWik = f64["eWi"].next()
            cx, cxk = f64["cx"].next()
            eWx, eWxk = f64["eWx"].next()
            p.op("act", lambda e, eW=eW, ps=ps: e.activation(out=eW[:], in_=ps[0:64, :], func=AF.Exp, scale=-NLW), r=[pk], w=[eWk])
            p.op("act", lambda e, eWi=eWi, ps=ps: e.activation(out=eWi[:], in_=ps[0:64, :], func=AF.Exp, scale=NLW), r=[pk], w=[eWik])
            p.op("dve", lambda e, cx=cx, ps=ps, sgw=sgw: e.tensor_tensor(out=cx[:], in0=ps[0:64, :], in1=sgw[:], op=ALU.subtract), r=[pk, sgwk], w=[cxk])
            p.op("act", lambda e, eWx=eWx, cx=cx: e.activation(out=eWx[:], in_=cx[:], func=AF.Exp, scale=-NLW), r=[cxk], w=[eWxk])
            rt, rtk = f64["rt"].next()
            kt, ktk = f64["kt"].next()
            bt, btk = f64["bt"].next()
            at, atk = f64["at"].next()
            p.op("dve", lambda e, rt=rt, r_=r_, eW=eW: e.tensor_tensor(out=rt[:], in0=r_, in1=eW[:], op=ALU.mult), r=[xk, eWk], w=[rtk])
            p.op("pool", lambda e, kt=kt, kd=kd, eWi=eWi: e.tensor_tensor(out=kt[:], in0=kd[:], in1=eWi[:], op=ALU.mult), r=[kdk, eWik], w=[ktk])
            p.op("dve", lambda e, bt=bt, kk=kk, alp=alp: e.tensor_tensor(out=bt[:], in0=kk[:], in1=alp[:], op=ALU.mult), r=[kkk, alpk], w=[btk])
            p.op("dve", lambda e, bt=bt, eWi=eWi: e.tensor_tensor(out=bt[:], in0=bt[:], in1=eWi[:], op=ALU.mult), r=[btk, eWik], w=[btk])
            p.op("dve", lambda e, at=at, kk=kk, eWx=eWx: e.scalar_tensor_tensor(out=at[:], in0=kk[:], scalar=-1.0, in1=eWx[:], op0=ALU.mult, op1=ALU.mult),
                 r=[kkk, eWxk], w=[atk])
            toks = {}
            for nm, src, sk in (("Ktok", kt[:], ktk), ("Btok", bt[:], btk), ("Vtok", v_, xk)):
                yield
                ps, pk = psA.next()
                tr(ps[:, 0:64], pk, src, 64, [sk])
                tt, ttk = tokr[nm].next()
                p.op("act" if nm != "Vtok" else "dve", (lambda e, tt=tt, ps=ps: e.copy(out=tt[:], in_=ps[:, 0:64])) if nm != "Vtok" else
                     (lambda e, tt=tt, ps=ps: e.tensor_copy(out=tt[:], in_=ps[:, 0:64])), r=[pk], w=[ttk])
                toks[nm] = (tt, ttk)
            Ktok, Ktokk = toks["Ktok"]
            Btok, Btokk = toks["Btok"]
            Vtok, Vtokk = toks["Vtok"]
            def gram(nm, lhsT, lk, rhs, rk2, mask):
                ps, pk = psA.next()
                mm(ps[:, :], pk, lhsT, rhs, [lk, rk2])
                m, mk = (sqh[h][nm] if nm in sqh[h] else sqm[nm]).next()
                p.op("dve", lambda e: e.tensor_tensor(out=m[:], in0=ps[:, :], in1=C[mask][:], op=ALU.mult), r=[pk, "masks"], w=[mk])
                return m, mk
            At, Atk = gram("At", bt[:], btk, at[:], atk, f"strict{d}")
            yield
            Am, Amk = gram("Am", at[:], atk, bt[:], btk, f"strict{1 - d}")
            yield
            Akt, Aktk = gram("Akt", kt[:], ktk, at[:], atk, f"strict{d}")
            yield
            Mrbt, Mrbtk = gram("Mrbt", bt[:], btk, rt[:], rtk, f"incl{d}")
            yield
            Mrkt, Mrktk = gram("Mrkt", kt[:], ktk, rt[:], rtk, f"incl{d}")
            yield
            Pt, Ptk = sqh[h]["Pt"].next()
            p.op("pool", lambda e, Pt=Pt, At=At: e.tensor_tensor(out=Pt[:], in0=At[:], in1=C["ident"][:], op=ALU.add), r=[Atk, "ident"], w=[Ptk])
            for step in range(6):
                yield
                ps, pk = psA.next()
                mm(ps[:, :], pk, At[:], Am[:], [Atk, Amk])
                Am2, Am2k = sqh[h]["Am"].next()
                p.op("act", lambda e, Am2=Am2, ps=ps: e.copy(out=Am2[:], in_=ps[:, :]), r=[pk], w=[Am2k])
                if step < 5:
                    yield
                    ps, pk = psA.next()
                    mm(ps[:, :], pk, Am[:], At[:], [Atk, Amk])
                    At2, At2k = sqh[h]["At"].next()
                    p.op("dve", lambda e, At2=At2, ps=ps: e.tensor_copy(out=At2[:], in_=ps[:, :]), r=[pk], w=[At2k])
                yield
                ps, pk = psA.next()
                mm(ps[:, :], pk, Am2[:], Pt[:], [Am2k, Ptk])
                Pt2, Pt2k = sqh[h]["Pt"].next()
                p.op("dve", lambda e, Pt2=Pt2, ps=ps, Pt=Pt: e.tensor_tensor(out=Pt2[:], in0=ps[:, :], in1=Pt[:], op=ALU.add), r=[pk, Ptk], w=[Pt2k])
                Pt, Ptk = Pt2, Pt2k
                Am, Amk = Am2, Am2k
                if step < 5:
                    At, Atk = At2, At2k
            yield
            ps, pk = psA.next()
            mm(ps[:, 0:64], pk, at[:], Tt[:], [atk, Tk], start=True, stop=False)
            mm(ps[:, 0:64], pk, Akt[:], Vtok[:], [Aktk, Vtokk], start=False, stop=True)
            Xs, Xsk = tokr["Xs"].next()
            p.op("act", lambda e, Xs=Xs, ps=ps: e.copy(out=Xs[:], in_=ps[:, 0:64]), r=[pk], w=[Xsk])
            yield
            ps, pk = psA.next()
            mm(ps[:, 0:64], pk, Pt[:], Xs[:], [Ptk, Xsk])
            Us, Usk = tokr["Us"].next()
            p.op("dve", lambda e, Us=Us, ps=ps: e.tensor_copy(out=Us[:], in_=ps[:, 0:64]), r=[pk], w=[Usk])
            yield
            ps, pk = psA.next()
            mm(ps[0:64, :], pk, Tt[:], rt[:], [Tk, rtk], start=True, stop=False)
            mm(ps[0:64, :], pk, Us[:], Mrbt[:], [Usk, Mrbtk], start=False, stop=False)
            mm(ps[0:64, :], pk, Vtok[:], Mrkt[:], [Vtokk, Mrktk], start=False, stop=True)
            yo, yok = f64["yo"].next()
            p.op("act", lambda e, yo=yo, ps=ps: e.copy(out=yo[:], in_=ps[0:64, :]), r=[pk], w=[yok])
            p.dma("sp", lambda e, yo=yo, d=d, hs=hs, sl=sl: e.dma_start(out=YT[d, hs, sl], in_=yo[:]), r=[yok], w=["YTR"])
            yield
            ps, pk = psA.next()
            mm(ps[0:64, 0:64], pk, Btok[:], Us[:], [Btokk, Usk], start=True, stop=False)
            mm(ps[0:64, 0:64], pk, Ktok[:], Vtok[:], [Ktokk, Vtokk], start=False, stop=True)
            t2, t2k = tw.next()
            p.op("dve", lambda e, t2=t2, Tt=Tt, eW=eW, tend=tend: e.tensor_scalar(out=t2[:], in0=Tt[:], scalar1=eW[:, tend:tend + 1], scalar2=None, op0=ALU.mult),
                 r=[Tk, eWk], w=[t2k])
            p.op("dve", lambda e, t2=t2, Tt=Tt, eW=eW, ps=ps, tend=tend: e.scalar_tensor_tensor(
                out=Tt[:], in0=ps[0:64, 0:64], scalar=eW[:, tend:tend + 1], in1=t2[:], op0=ALU.mult, op1=ALU.add), r=[pk, eWk, t2k], w=[Tk])


        def drive(gens):
            gens = list(gens)
            while gens:
                for g_ in list(gens):
                    try:
                        next(g_)
                    except StopIteration:
                        gens.remove(g_)

        for i in range(nt):
            for d in range(2):
                c = order[d][i]
                t0 = c * 128
                tend = 127 if d == 0 else 0
                sl = slice(t0, t0 + 128)
                wat, wak = wa.next()
                p.dma("sp", lambda e, wat=wat, sl=sl: e.dma_start(out=wat[:, 0, :], in_=SMT[1536:1600, sl]), w=[wak])
                p.dma("sp", lambda e, wat=wat, sl=sl: e.dma_start(out=wat[:, 1, :], in_=SMT[1600:1664, sl]), w=[wak])
                p.op("act", lambda e, wat=wat: e.activation(out=wat[:, 0, :], in_=wat[:, 0, :], func=AF.Tanh), r=[wak], w=[wak])
                drive([unit(d, h, c, wat, wak) for h in range(8)])


def phase_rwkv_finish(p, C, io, n_tok):
    SMT, YT, RK, MIXT = io["SMT"], io["YTR"], io["RK"], io["MIXT"]
    with p.phase():
        g2a = p.sb("g2a", [128, 512], F32)
        g2b = p.sb("g2b", [32, 512], F32)
        p.dma("sp", lambda e: e.dma_start(out=g2a[:], in_=io["rw_g2"][0:128, :]), w=["fw"])
        p.dma("sp", lambda e: e.dma_start(out=g2b[:], in_=io["rw_g2"][128:160, :]), w=["fw"])
        lnw = p.sb("lnw", [64, 8], F32)
        lnb = p.sb("lnb", [64, 8], F32)
        colload(p, lnw, "fw", io["rw_ln_w"], 8)
        colload(p, lnb, "fw", io["rw_ln_b"], 8)
        sga = Ring(p, "sga", [128, 512], n=2)
        sgb = Ring(p, "sgb", [32, 512], n=2)
        R = {nm: Ring(p, nm, [64, 512], n=2) for nm in ("fy0", "fy1", "fr0", "fr1", "fv", "fyc", "fsq", "frs", "fbn")}
        ob = Ring(p, "fob", [64, 512], BF16, n=2)
        psF = Ring(p, "psG", [64, 512], n=4, psum=True)
        for t0 in range(0, n_tok, 512):
            N = min(512, n_tok - t0)
            sl = slice(t0, t0 + N)
            a_, ak = sga.next()
            b_, bk = sgb.next()
            p.dma("sp", lambda e, a_=a_, sl=sl, N=N: e.dma_start(out=a_[:, 0:N], in_=SMT[1664:1792, sl]), w=[ak])
            p.dma("sp", lambda e, b_=b_, sl=sl, N=N: e.dma_start(out=b_[:, 0:N], in_=SMT[1792:1824, sl]), w=[bk])
            p.op("act", lambda e, a_=a_, N=N: e.activation(out=a_[:, 0:N], in_=a_[:, 0:N], func=AF.Sigmoid), r=[ak], w=[ak])
            p.op("act", lambda e, b_=b_, N=N: e.activation(out=b_[:, 0:N], in_=b_[:, 0:N], func=AF.Sigmoid), r=[bk], w=[bk])
            for h in range(8):
                hs = slice(h * 64, (h + 1) * 64)
                y0, y0k = R["fy0"].next()
                y1, y1k = R["fy1"].next()
                r0, r0k = R["fr0"].next()
                r1, r1k = R["fr1"].next()
                v, vk = R["fv"].next()
                for (t, k_, src) in ((y0, y0k, YT[0, hs, sl]), (y1, y1k, YT[1, hs, sl]), (r0, r0k, RK[0, hs, sl]), (r1, r1k, RK[1, hs, sl]),
                                     (v, vk, SMT[1024 + h * 64:1024 + (h + 1) * 64, sl])):
                    p.dma("sp", lambda e, t=t, src=src, N=N: e.dma_start(out=t[:, 0:N], in_=src), w=[k_])
                p.op("dve", lambda e, y0=y0, y1=y1, N=N: e.tensor_tensor(out=y0[:, 0:N], in0=y0[:, 0:N], in1=y1[:, 0:N], op=ALU.add), r=[y0k, y1k], w=[y0k])
                p.op("pool", lambda e, r0=r0, r1=r1, N=N: e.tensor_tensor(out=r0[:, 0:N], in0=r0[:, 0:N], in1=r1[:, 0:N], op=ALU.add), r=[r0k, r1k], w=[r0k])
                ps, pk = psF.next()
                p.op("pe", lambda e, ps=ps, y0=y0, N=N: e.matmul(ps[:, 0:N], lhsT=C["ones64m"][0:64, 0:64], rhs=y0[:, 0:N], start=True, stop=True), r=[y0k, "masks"], w=[pk])
                yc, yck = R["fyc"].next()
                p.op("dve", lambda e, yc=yc, y0=y0, ps=ps, N=N: e.tensor_tensor(out=yc[:, 0:N], in0=y0[:, 0:N], in1=ps[:, 0:N], op=ALU.subtract), r=[y0k, pk], w=[yck])
                sq, sqk = R["fsq"].next()
                p.op("act", lambda e, sq=sq, yc=yc, N=N: e.activation(out=sq[:, 0:N], in_=yc[:, 0:N], func=AF.Square), r=[yck], w=[sqk])
                ps, pk = psF.next()
                p.op("pe", lambda e, ps=ps, sq=sq, N=N: e.matmul(ps[:, 0:N], lhsT=C["ones64m"][0:64, 0:64], rhs=sq[:, 0:N], start=True, stop=True), r=[sqk, "masks"], w=[pk])
                rs, rsk = R["frs"].next()
                p.op("dve", lambda e, rs=rs, ps=ps, N=N: e.tensor_scalar(out=rs[:, 0:N], in0=ps[:, 0:N], scalar1=64e-5, scalar2=None, op0=ALU.add), r=[pk], w=[rsk])
                p.op("act", lambda e, rs=rs, N=N: e.activation(out=rs[:, 0:N], in_=rs[:, 0:N], func=AF.Sqrt), r=[rsk], w=[rsk])
                p.op("dve", lambda e, rs=rs, N=N: e.reciprocal(out=rs[:, 0:N], in_=rs[:, 0:N]), r=[rsk], w=[rsk])
                p.op("dve", lambda e, yc=yc, rs=rs, N=N: e.tensor_tensor(out=yc[:, 0:N], in0=yc[:, 0:N], in1=rs[:, 0:N], op=ALU.mult), r=[yck, rsk], w=[yck])
                p.op("dve", lambda e, yc=yc, h=h, N=N: e.tensor_scalar(out=yc[:, 0:N], in0=yc[:, 0:N], scalar1=lnw[:, h:h + 1], scalar2=lnb[:, h:h + 1],
                                                                      op0=ALU.mult, op1=ALU.add), r=[yck, "fw"], w=[yck])
                ps, pk = psF.next()
                p.op("pe", lambda e, ps=ps, r0=r0, N=N: e.matmul(ps[:, 0:N], lhsT=C["ones64"][0:64, 0:64], rhs=r0[:, 0:N], start=True, stop=True), r=[r0k, "masks"], w=[pk])
                bn, bnk = R["fbn"].next()
                p.op("dve", lambda e, bn=bn, ps=ps, v=v, N=N: e.tensor_tensor(out=bn[:, 0:N], in0=ps[:, 0:N], in1=v[:, 0:N], op=ALU.mult), r=[pk, vk], w=[bnk])
                p.op("pool", lambda e, bn=bn, yc=yc, N=N: e.tensor_tensor(out=bn[:, 0:N], in0=bn[:, 0:N], in1=yc[:, 0:N], op=ALU.add), r=[bnk, yck], w=[bnk])
                ps, pk = psF.next()
                p.op("pe", lambda e, ps=ps, a_=a_, hs=hs, N=N: e.matmul(ps[:, 0:N], lhsT=g2a[:, hs], rhs=a_[:, 0:N], start=True, stop=False), r=["fw", ak], w=[pk])
                p.op("pe", lambda e, ps=ps, b_=b_, hs=hs, N=N: e.matmul(ps[:, 0:N], lhsT=g2b[:, hs], rhs=b_[:, 0:N], start=False, stop=True), r=["fw", bk], w=[pk])
                o, ok = ob.next()
                p.op("dve", lambda e, o=o, bn=bn, ps=ps, N=N: e.tensor_tensor(out=o[:, 0:N], in0=ps[:, 0:N], in1=bn[:, 0:N], op=ALU.mult), r=[pk, bnk], w=[ok])
                p.dma("sp", lambda e, o=o, h=h, sl=sl, N=N: e.dma_start(out=MIXT[512 + h * 64:512 + (h + 1) * 64, sl], in_=o[:, 0:N]), r=[ok], w=["MIXT"])


def phase_outproj(p, C, io, l, w_out, n_ctx, n_tok, t_lo=0):
    xres, MIXT, modrow = io["xres"], io["MIXT"], io["modrow"]
    with p.phase():
        wbf = p.sb("woutbf", [128, KC, D], BF16)
        stage = [p.sb(f"wostage{i}", [128, D], F32) for i in range(2)]
        load_cast_weight(p, wbf, "woutbf", w_out, KC, D, stage, "wostage")
        gt = p.sb("gt1row", [128, 2, D], F32)
        for r in range(2):
            p.dma("sp", lambda e, r=r: e.dma_start(out=gt[:, r, :], in_=modrow[l, r, 2048:3072].partition_broadcast(128)), w=["gt1row"])
        mx = Ring(p, "mx", [128, KC, 128], BF16, n=2)
        xt = Ring(p, "oxt", [128, D], n=2)
        tmp = Ring(p, "otmp", [128, D], n=2)
        pso = Ring(p, "pso", [128, 512], n=4, psum=True)
        for ti in range(t_lo // 128, n_tok // 128):
            r = 1 if ti * 128 < n_ctx else 0
            sl = slice(ti * 128, (ti + 1) * 128)
            m, mk = mx.next()
            p.dma("sp", lambda e, m=m, sl=sl: e.dma_start(out=m[:], in_=MIXT[:, sl].rearrange("(c q) t -> q c t", q=128)), w=[mk])
            x, xk = xt.next()
            p.dma("sp", lambda e, x=x, sl=sl: e.dma_start(out=x[:], in_=xres[sl, :]), w=[xk])
            t, tk = tmp.next()
            for n in range(2):
                ps, pk = pso.next()
                for kc in range(KC):
                    p.op("pe", lambda e, ps=ps, m=m, kc=kc, n=n: e.matmul(ps[:], lhsT=m[:, kc, :], rhs=wbf[:, kc, n * 512:(n + 1) * 512],
                                                                          start=(kc == 0), stop=(kc == KC - 1)), r=[mk, "woutbf"], w=[pk])
                p.op("dve", lambda e, t=t, ps=ps, n=n, r=r: e.tensor_tensor(out=t[:, n * 512:(n + 1) * 512], in0=ps[:], in1=gt[:, r, n * 512:(n + 1) * 512], op=ALU.mult),
                     r=[pk, "gt1row"], w=[tk])
            p.op("pool", lambda e, t=t, x=x: e.tensor_tensor(out=t[:], in0=t[:], in1=x[:], op=ALU.add), r=[tk, xk], w=[tk])
            p.dma("sp", lambda e, t=t, sl=sl: e.dma_start(out=xres[sl, :], in_=t[:]), r=[tk], w=["xres"])


def phase_final(p, C, io, n_ctx, n_tok):
    xres, out = io["xres"], io["out"]
    with p.phase():
        g = p.sb("fng", [128, D], F32)
        p.dma("sp", lambda e: e.dma_start(out=g[:], in_=io["final_norm_g"].partition_broadcast(128)), w=["fng"])
        xt = Ring(p, "fxt", [128, D], n=3)
        junk = p.sb("fjunk", [128, D], BF16)
        ss = Ring(p, "fss", [128, 1], n=3)
        rs = Ring(p, "frst", [128, 1], n=3)
        for ti in range(n_ctx // 128, n_tok // 128):
            sl = slice(ti * 128, (ti + 1) * 128)
            x, xk = xt.next()
            s, sk = ss.next()
            r_, rk = rs.next()
            p.dma("sp", lambda e, x=x, sl=sl: e.dma_start(out=x[:], in_=xres[sl, :]), w=[xk])
            p.op("act", lambda e, x=x, s=s: e.activation(out=junk[:], in_=x[:], func=AF.Square, accum_out=s[:, 0:1]), r=[xk], w=["fjunk", sk])
            p.op("dve", lambda e, r_=r_, s=s: e.tensor_scalar(out=r_[:], in0=s[:], scalar1=1.0 / D, scalar2=EPS, op0=ALU.mult, op1=ALU.add), r=[sk], w=[rk])
            p.op("act", lambda e, r_=r_: e.activation(out=r_[:], in_=r_[:], func=AF.Sqrt), r=[rk], w=[rk])
            p.op("dve", lambda e, r_=r_: e.reciprocal(out=r_[:], in_=r_[:]), r=[rk], w=[rk])
            p.op("dve", lambda e, x=x, r_=r_: e.scalar_tensor_tensor(out=x[:], in0=x[:], scalar=r_[:, 0:1], in1=g[:], op0=ALU.mult, op1=ALU.mult),
                 r=[xk, rk, "fng"], w=[xk])
            o0 = ti * 128 - n_ctx
            p.dma("sp", lambda e, x=x, o0=o0: e.dma_start(out=out[o0:o0 + 128, :], in_=x[:]), r=[xk], w=["out"])

RST = 9

DE = 1024
SW_LIMIT = 7.0
SW_ALPHA = 1.702


def phase_moe_cast(p, C, io, l, NE):
    gu, dn, WGU, WDN = io["moe_gu_w"], io["moe_down_w"], io["WGU"], io["WDN"]
    with p.phase():
        sg = Ring(p, "cg32", [128, 2, 2048], n=2)
        sb = Ring(p, "cg16", [128, 2, 2048], BF16, n=2)
        sd = Ring(p, "cd32", [128, 2, 1024], n=2)
        sdb = Ring(p, "cd16", [128, 2, 1024], BF16, n=2)
        engs = ("dve", "pool", "act")
        it = 0
        for e_ in range(NE):
            for k2 in range(4):
                for (src, dst, r32, r16) in ((gu, WGU, sg, sb), (dn, WDN, sd, sdb)):
                    a, ak = r32.next()
                    b, bk = r16.next()
                    rows = slice(k2 * 256, (k2 + 1) * 256)
                    p.dma("sp", lambda e, a=a, src=src, e_=e_, rows=rows: e.dma_start(out=a[:], in_=src[l, e_, rows, :].rearrange("(c q) n -> q c n", q=128)), w=[ak])
                    eng = engs[it % 3]
                    it += 1
                    if eng == "act":
                        p.op("act", lambda e, a=a, b=b: e.copy(out=b[:], in_=a[:]), r=[ak], w=[bk])
                    else:
                        p.op(eng, lambda e, a=a, b=b: e.tensor_copy(out=b[:], in_=a[:]), r=[ak], w=[bk])
                    p.dma("act", lambda e, b=b, dst=dst, e_=e_, rows=rows: e.dma_start(out=dst[e_, rows, :].rearrange("(c q) n -> q c n", q=128), in_=b[:]), r=[bk], w=["WBF"])


def phase_moe_route(p, C, io, G, l, NE, t_lo, t_hi, n_ctx):
    xres, HT, GATE = io["xres"], io["HT"], io["GATE"]
    with p.phase():
        T = norm_scratch(p)
        wr = p.sb("wr", [128, KC, NE], F32)
        p.dma("sp", lambda e: e.dma_start(out=wr[:], in_=io["router_w"][l].rearrange("(c q) n -> q c n", q=128)), w=["wr"])
        br = p.sb("br", [128, NE], F32)
        p.dma("sp", lambda e: e.dma_start(out=br[:], in_=io["router_b"][l].partition_broadcast(128)), w=["wr"])
        xt = Ring(p, "mxt", [128, D], n=2)
        hb = Ring(p, "mhb", [128, KC, 128], BF16, n=2)
        h32 = Ring(p, "mh32", [128, KC, 128], n=2)
        lg = Ring(p, "mlg", [128, NE], n=2)
        ex = Ring(p, "mex", [128, NE], n=2)
        mk = Ring(p, "mmk", [128, NE], n=2)
        m8 = Ring(p, "mm8", [128, 8], n=2)
        sc = Ring(p, "msc", [128, 2], n=2)
        psr = p.ps("psr", [128, NE], F32)
        for ti in range(t_lo // 128, t_hi // 128):
            r = 1 if ti * 128 < n_ctx else 0
            sl = slice(ti * 128, (ti + 1) * 128)
            x, xk = xt.next()
            p.dma("sp", lambda e, x=x, sl=sl: e.dma_start(out=x[:], in_=xres[sl, :]), w=[xk])
            b, bk = hb.next()
            f, fk = h32.next()
            norm_tile(p, C, T, xk, x, G["A2"][:, l, r, :], G["B2"][:, l, r, :], lambda kc, b=b: b[:, kc, :], bk,
                      hT32_dst=lambda kc, f=f: f[:, kc, :])
            fk32 = bk + "32"
            p.dma("sp", lambda e, b=b, sl=sl: e.dma_start(out=HT[:, sl].rearrange("(c q) t -> q c t", q=128), in_=b[:]), r=[bk], w=["HT"])
            if RST < 2:
                continue
            for kc in range(KC):
                p.op("pe", lambda e, f=f, kc=kc: e.matmul(psr[:], lhsT=f[:, kc, :], rhs=wr[:, kc, :], start=(kc == 0), stop=(kc == KC - 1)),
                     r=[fk32, "wr"], w=["psr"])
            g, gk = lg.next()
            p.op("dve", lambda e, g=g: e.tensor_tensor(out=g[:], in0=psr[:], in1=br[:], op=ALU.add), r=["psr", "wr"], w=[gk])
            if RST < 3:
                continue
            m, mk8 = m8.next()
            p.op("dve", lambda e, m=m, g=g: e.max(out=m[:], in_=g[:]), r=[gk], w=[mk8])
            msk, mskk = mk.next()
            p.op("dve", lambda e, msk=msk, g=g, m=m: e.tensor_scalar(out=msk[:], in0=g[:], scalar1=m[:, 3:4], scalar2=None, op0=ALU.is_ge), r=[gk, mk8], w=[mskk])
            if RST < 4:
                continue
            s, sk = sc.next()
            p.op("dve", lambda e, s=s, m=m: e.tensor_scalar(out=s[:, 0:1], in0=m[:, 0:1], scalar1=-1.0, scalar2=None, op0=ALU.mult), r=[mk8], w=[sk])
            x_, xk_ = ex.next()
            p.op("act", lambda e, x_=x_, g=g, s=s: e.activation(out=x_[:], in_=g[:], func=AF.Exp, bias=s[:, 0:1]), r=[gk, sk], w=[xk_])
            p.op("dve", lambda e, x_=x_, msk=msk: e.tensor_tensor(out=x_[:], in0=x_[:], in1=msk[:], op=ALU.mult), r=[xk_, mskk], w=[xk_])
            p.op("dve", lambda e, s=s, x_=x_: e.reduce_sum(out=s[:, 1:2], in_=x_[:], axis=AX.X), r=[xk_], w=[sk])
            p.op("dve", lambda e, s=s: e.reciprocal(out=s[:, 1:2], in_=s[:, 1:2]), r=[sk], w=[sk])
            p.op("dve", lambda e, x_=x_, s=s: e.tensor_scalar(out=x_[:], in0=x_[:], scalar1=s[:, 1:2], scalar2=None, op0=ALU.mult), r=[xk_, sk], w=[xk_])
            p.dma("sp", lambda e, x_=x_, sl=sl: e.dma_start(out=GATE[sl, :], in_=x_[:]), r=[xk_], w=["GATE"])


def phase_moe_experts(p, C, io, l, NE, t_lo, t_hi, n_ctx, TB=512):
    xres, HT, GATE, WGU, WDN, modrow = io["xres"], io["HT"], io["GATE"], io["WGU"], io["WDN"], io["modrow"]
    with p.phase():
        bg_rows = p.sb("bg_rows", [NE, 2048], F32)
        p.dma("sp", lambda e: e.dma_start(out=bg_rows[:], in_=io["moe_gu_b"][l]), w=["bg_rows"])
        bgc = p.sb("bgc", [128, 16, NE], F32)
        pst = p.ps("pst", [128, 16, NE], F32)
        for c in range(16):
            p.op("pe", lambda e, c=c: e.transpose(pst[:, c, :], bg_rows[:, c * 128:(c + 1) * 128], C["ident"][0:NE, 0:NE]), r=["bg_rows", "ident"], w=["pst"])
        p.op("dve", lambda e: e.tensor_copy(out=bgc[:], in_=pst[:]), r=["pst"], w=["bgc"])
        ones_b = p.sb("ones_b", [1, 128], BF16)
        p.op("dve", lambda e: e.memset(ones_b[:], 1.0), w=["ones_b"])
        gt = p.sb("gt2row", [128, 2, D], F32)
        for r in range(2):
            p.dma("sp", lambda e, r=r: e.dma_start(out=gt[:, r, :], in_=modrow[l, r, 5120:6144].partition_broadcast(128)), w=["gt2row"])
        wg = Ring(p, "wg", [128, KC, 2048], BF16, n=2)
        wd = Ring(p, "wd", [128, KC, 1024], BF16, n=2)
        bd32 = Ring(p, "bd32", [1, 1024], F32, n=2)
        bd16 = Ring(p, "bd16", [1, 1024], BF16, n=2)
        acc = p.sb("macc", [128, TB // 128, D], F32)
        hT = p.sb("mhT", [128, KC, TB], BF16)
        gate = p.sb("mgate", [128, TB // 128, NE], F32)
        actT = Ring(p, "actT", [128, KC, 512], BF16, n=2)
        t1 = Ring(p, "et1", [128, 512], n=2)
        sgm = Ring(p, "esg", [128, 512], n=2)
        t2 = Ring(p, "et2", [128, 512], n=2)
        xt = Ring(p, "ext", [128, D], n=2)
        psg = Ring(p, "psg", [128, 512], n=4, psum=True)
        psd = Ring(p, "psd", [128, 512], n=3, psum=True)
        t = t_lo
        while t < t_hi:
            nb = min(TB, t_hi - t)
            ntile = nb // 128
            p.dma("sp", lambda e, t=t, nb=nb: e.dma_start(out=hT[:, :, 0:nb], in_=HT[:, t:t + nb].rearrange("(c q) n -> q c n", q=128)), w=["mhT"])
            p.dma("sp", lambda e, t=t, nb=nb, ntile=ntile: e.dma_start(out=gate[:, 0:ntile, :], in_=GATE[t:t + nb, :].rearrange("(j q) n -> q j n", q=128)), w=["mgate"])
            p.op("pool", lambda e: e.memset(acc[:], 0.0), w=["macc"])
            for e_ in range(NE):
                g_, gk = wg.next()
                d_, dk = wd.next()
                b32, b32k = bd32.next()
                b16, b16k = bd16.next()
                p.dma("sp", lambda e, g_=g_, e_=e_: e.dma_start(out=g_[:], in_=WGU[e_].rearrange("(c q) n -> q c n", q=128)), w=[gk])
                p.dma("act", lambda e, d_=d_, e_=e_: e.dma_start(out=d_[:], in_=WDN[e_].rearrange("(c q) n -> q c n", q=128)), w=[dk])
                p.dma("sp", lambda e, b32=b32, e_=e_: e.dma_start(out=b32[:], in_=io["moe_down_b"][l, e_:e_ + 1, :]), w=[b32k])
                p.op("pool", lambda e, b32=b32, b16=b16: e.tensor_copy(out=b16[:], in_=b32[:]), r=[b32k], w=[b16k])
                for s0 in range(0, nb, 512):
                    N = min(512, nb - s0)
                    aT, aTk = actT.next()
                    for c in range(8):
                        pa, pak = psg.next()
                        pb, pbk = psg.next()
                        for (ps_, pk_, col0) in ((pa, pak, c * 128), (pb, pbk, DE + c * 128)):
                            for kc in range(KC):
                                p.op("pe", lambda e, ps_=ps_, g_=g_, kc=kc, col0=col0, s0=s0, N=N: e.matmul(
                                    ps_[:, 0:N], lhsT=g_[:, kc, col0:col0 + 128], rhs=hT[:, kc, s0:s0 + N], start=(kc == 0), stop=(kc == KC - 1)),
                                    r=[gk, "mhT"], w=[pk_])
                        a1, a1k = t1.next()
                        p.op("dve", lambda e, a1=a1, pa=pa, c=c, e_=e_, N=N: e.tensor_scalar(out=a1[:, 0:N], in0=pa[:, 0:N], scalar1=bgc[:, c, e_:e_ + 1], scalar2=SW_LIMIT,
                                                                                          op0=ALU.add, op1=ALU.min), r=[pak, "bgc"], w=[a1k])
                        s_, sk_ = sgm.next()
                        p.op("act", lambda e, s_=s_, a1=a1, N=N: e.activation(out=s_[:, 0:N], in_=a1[:, 0:N], func=AF.Sigmoid, scale=SW_ALPHA), r=[a1k], w=[sk_])
                        a2, a2k = t2.next()
                        p.op("dve", lambda e, a2=a2, pb=pb, c=c, e_=e_, N=N: e.tensor_scalar(out=a2[:, 0:N], in0=pb[:, 0:N], scalar1=bgc[:, 8 + c, e_:e_ + 1], scalar2=SW_LIMIT,
                                                                                          op0=ALU.add, op1=ALU.min), r=[pbk, "bgc"], w=[a2k])
                        p.op("pool", lambda e, a2=a2, N=N: e.tensor_scalar(out=a2[:, 0:N], in0=a2[:, 0:N], scalar1=-SW_LIMIT, scalar2=1.0, op0=ALU.max, op1=ALU.add),
                             r=[a2k], w=[a2k])
                        p.op("pool", lambda e, a1=a1, s_=s_, N=N: e.tensor_tensor(out=a1[:, 0:N], in0=a1[:, 0:N], in1=s_[:, 0:N], op=ALU.mult), r=[a1k, sk_], w=[a1k])
                        p.op("dve", lambda e, aT=aT, a1=a1, a2=a2, c=c, N=N: e.tensor_tensor(out=aT[:, c, 0:N], in0=a1[:, 0:N], in1=a2[:, 0:N], op=ALU.mult),
                             r=[a1k, a2k], w=[aTk])
                    for j in range(N // 128):
                        tile_i = (s0 // 128) + j
                        for n in range(2):
                            pd, pdk = psd.next()
                            for kc in range(KC):
                                p.op("pe", lambda e, pd=pd, aT=aT, d_=d_, kc=kc, j=j, n=n: e.matmul(
                                    pd[:], lhsT=aT[:, kc, j * 128:(j + 1) * 128], rhs=d_[:, kc, n * 512:(n + 1) * 512], start=(kc == 0), stop=False),
                                    r=[aTk, dk], w=[pdk])
                            p.op("pe", lambda e, pd=pd, b16=b16, n=n: e.matmul(pd[:], lhsT=ones_b[:], rhs=b16[:, n * 512:(n + 1) * 512], start=False, stop=True),
                                 r=["ones_b", b16k], w=[pdk])
                            p.op("dve", lambda e, pd=pd, tile_i=tile_i, n=n, e_=e_: e.scalar_tensor_tensor(
                                out=acc[:, tile_i, n * 512:(n + 1) * 512], in0=pd[:], scalar=gate[:, tile_i, e_:e_ + 1],
                                in1=acc[:, tile_i, n * 512:(n + 1) * 512], op0=ALU.mult, op1=ALU.add), r=[pdk, "mgate", "macc"], w=["macc"])
            for j in range(ntile):
                tok = t + j * 128
                r = 1 if tok < n_ctx else 0
                x, xk = xt.next()
                p.dma("sp", lambda e, x=x, tok=tok: e.dma_start(out=x[:], in_=xres[tok:tok + 128, :]), w=[xk])
                p.op("dve", lambda e, j=j, r=r: e.tensor_tensor(out=acc[:, j, :], in0=acc[:, j, :], in1=gt[:, r, :], op=ALU.mult), r=["macc", "gt2row"], w=["macc"])
                p.op("pool", lambda e, x=x, j=j: e.tensor_tensor(out=x[:], in0=x[:], in1=acc[:, j, :], op=ALU.add), r=[xk, "macc"], w=[xk])
                p.dma("sp", lambda e, x=x, tok=tok: e.dma_start(out=xres[tok:tok + 128, :], in_=x[:]), r=[xk], w=["xres"])
            t += nb

import math

MLA_SCALE = 192 ** -0.5


def make_rope_consts(p, C):
    f, q = C["iota_f"], C["iota_p"]
    d = p.sb("rp_d", [64, 64], F32)
    e1 = p.sb("rp_e1", [64, 64], F32)
    e2 = p.sb("rp_e2", [64, 64], F32)
    ge = p.sb("rp_ge", [64, 64], F32)
    PR = p.sb("rp_PR", [64, 64], F32)
    k = ["rope_c"]
    p.op("dve", lambda e: e.tensor_scalar(out=d[:], in0=f[0:64, 0:64], scalar1=q[0:64, 0:1], scalar2=None, op0=ALU.subtract), r=["iota_f", "iota_p"], w=k)
    p.op("dve", lambda e: e.tensor_scalar(out=e1[:], in0=d[:], scalar1=16.0, scalar2=None, op0=ALU.is_equal), r=k, w=k)
    p.op("dve", lambda e: e.tensor_scalar(out=e2[:], in0=d[:], scalar1=-16.0, scalar2=None, op0=ALU.is_equal), r=k, w=k)
    ii = p.sb("rp_ii", [64, 64], I32)
    p.op("pool", lambda e: e.iota(ii[:], pattern=[[1, 64]], base=0, channel_multiplier=0), w=["rp_ii"])
    p.op("dve", lambda e: e.tensor_single_scalar(out=ii[:], in_=ii[:], scalar=16, op=ALU.bitwise_and), r=["rp_ii"], w=["rp_ii"])
    p.op("dve", lambda e: e.tensor_copy(out=ge[:], in_=ii[:]), r=["rp_ii"], w=k)
    p.op("dve", lambda e: e.tensor_scalar(out=ge[:], in0=ge[:], scalar1=1.0 / 16, scalar2=None, op0=ALU.mult), r=k, w=k)
    p.op("dve", lambda e: e.tensor_tensor(out=e1[:], in0=e1[:], in1=ge[:], op=ALU.mult), r=k, w=k)
    p.op("dve", lambda e: e.tensor_scalar(out=ge[:], in0=ge[:], scalar1=-1.0, scalar2=1.0, op0=ALU.mult, op1=ALU.add), r=k, w=k)
    p.op("dve", lambda e: e.tensor_tensor(out=e2[:], in0=e2[:], in1=ge[:], op=ALU.mult), r=k, w=k)
    p.op("dve", lambda e: e.tensor_tensor(out=PR[:], in0=e1[:], in1=e2[:], op=ALU.subtract), r=k, w=k)
    invf = p.sb("rp_invf", [64, 1], F32)
    isrow = p.sb("rp_isrow", [64, 1], F32)
    pi_ = p.sb("rp_pi", [64, 1], I32)
    p.op("pool", lambda e: e.iota(pi_[:], pattern=[[1, 1]], base=0, channel_multiplier=1), w=["rp_pi"])
    p.op("dve", lambda e: e.tensor_single_scalar(out=pi_[:], in_=pi_[:], scalar=15, op=ALU.bitwise_and), r=["rp_pi"], w=["rp_pi"])
    p.op("dve", lambda e: e.tensor_copy(out=invf[:], in_=pi_[:]), r=["rp_pi"], w=k)
    p.op("act", lambda e: e.activation(out=invf[:], in_=invf[:], func=AF.Exp, scale=-math.log(10000.0) / 16.0), r=k, w=k)
    p.op("dve", lambda e: e.tensor_scalar(out=isrow[:], in0=q[0:64, 0:1], scalar1=32.0, scalar2=None, op0=ALU.is_lt), r=["iota_p"], w=k)
    C.update(PR=PR, invf=invf, isrow=isrow)


def phase_mla_proj(p, C, io, G, l, n_ctx, n_tok):
    xres = io["xres"]
    QN, QR, KN, KR, V = io["QN"], io["QR"], io["KN"], io["KRo"], io["Vt"]
    n_lat = n_tok - n_ctx
    with p.phase():
        T = norm_scratch(p)
        win = p.sb("mwin", [128, KC, 448], BF16)
        st = [p.sb(f"mst{i}", [128, 2048], F32) for i in range(2)]
        load_cast_weight(p, win, "mwin", io["od_w_in"], KC, 448, st, "mst")
        wq = p.sb("mwq", [128, 2, 1536], BF16)
        load_cast_weight(p, wq, "mwq", io["mla_wq_up"], 2, 1536, st, "mst")
        wkn = p.sb("mwkn", [128, 8, 128], BF16)
        wv = p.sb("mwv", [128, 8, 128], BF16)
        p.dma("sp", lambda e: e.dma_start(out=st[0][:, 0:2048], in_=io["mla_wkv_up"][:, :]), w=["mst0"])
        p.op("dve", lambda e: e.tensor_copy(out=wkn[:], in_=st[0][:, 0:2048].rearrange("q (h c) -> q h c", c=256)[:, :, 0:128]), r=["mst0"], w=["mwk"])
        p.op("pool", lambda e: e.tensor_copy(out=wv[:], in_=st[0][:, 0:2048].rearrange("q (h c) -> q h c", c=256)[:, :, 128:256]), r=["mst0"], w=["mwk"])
        qg = p.sb("mqg", [128, 2], F32)
        kg = p.sb("mkg", [128, 1], F32)
        p.dma("sp", lambda e: e.dma_start(out=qg[:], in_=io["mla_q_norm"].rearrange("(c q) -> q c", q=128), allow_slow_non_contiguous=True), w=["mg"])
        p.dma("sp", lambda e: e.dma_start(out=kg[:], in_=io["mla_kv_norm"].rearrange("(q o) -> q o", o=1)), w=["mg"])
        onesq = p.sb("monesq", [128, 128], F32)
        p.op("dve", lambda e: e.memset(onesq[:], 1.0 / 256), w=["monesq"])
        xts = Ring(p, "axt", [128, D], n=2)
        hT = p.sb("ahT", [128, KC, 512], BF16)
        cq = p.sb("acq", [128, 2, 512], F32)
        ckv = p.sb("ackv", [128, 512], F32)
        krr = p.sb("akrr", [64, 512], F32)
        sq = p.sb("asq", [128, 2, 512], F32)
        rs = Ring(p, "ars", [128, 512], n=2)
        cqn = p.sb("acqn", [128, 2, 512], BF16)
        ckvn = p.sb("ackvn", [128, 512], BF16)
        ang = p.sb("aang", [64, 512], F32)
        tti = p.sb("atti", [64, 512], I32)
        tt2 = p.sb("att2", [64, 512], I32)
        a2 = p.sb("aa2", [64, 512], F32)
        kf = p.sb("akf", [64, 512], F32)
        cosT = p.sb("acos", [64, 512], F32)
        sinT = p.sb("asin", [64, 512], F32)
        rx = Ring(p, "arx", [64, 512], n=2)
        ru = Ring(p, "aru", [64, 512], n=2)
        ob = Ring(p, "aob", [128, 512], BF16, n=3)
        vb = Ring(p, "avb", [128, 1024], BF16, n=2)
        pp = Ring(p, "app", [128, 512], n=5, psum=True)

        def rope(x, xk, N, scale, dst_ap, dst_key):
            p2, p2k = pp.next()
            p.op("pe", lambda e: e.matmul(p2[0:64, 0:N], lhsT=C["PR"][:], rhs=x, start=True, stop=True), r=["rope_c", xk], w=[p2k])
            u, uk = ru.next()
            p.op("dve", lambda e: e.tensor_tensor(out=u[:, 0:N], in0=p2[0:64, 0:N], in1=sinT[:, 0:N], op=ALU.mult), r=[p2k, "trig"], w=[uk])
            p.op("pool", lambda e: e.tensor_tensor(out=x, in0=x, in1=cosT[:, 0:N], op=ALU.mult), r=[xk, "trig"], w=[xk])
            if scale == 1.0:
                p.op("dve", lambda e: e.tensor_tensor(out=dst_ap, in0=x, in1=u[:, 0:N], op=ALU.add), r=[xk, uk], w=[dst_key])
            else:
                p.op("dve", lambda e: e.tensor_tensor(out=u[:, 0:N], in0=x, in1=u[:, 0:N], op=ALU.add), r=[xk, uk], w=[uk])
                p.op("dve", lambda e: e.tensor_scalar(out=dst_ap, in0=u[:, 0:N], scalar1=scale, scalar2=None, op0=ALU.mult), r=[uk], w=[dst_key])

        blocks = [(0, n_ctx, False)] + [(t, min(512, n_tok - t), True) for t in range(n_ctx, n_tok, 512)]
        def do_block(t0, N, is_lat):
            for j in range(N // 128):
                ti = t0 // 128 + j
                r = 0 if is_lat else 1
                x, xk = xts.next()
                p.dma("sp", lambda e, x=x, ti=ti: e.dma_start(out=x[:], in_=xres[ti * 128:(ti + 1) * 128, :]), w=[xk])
                norm_tile(p, C, T, xk, x, G["A1"][:, l, r, :], G["B1"][:, l, r, :], lambda kc, j=j: hT[:, kc, j * 128:(j + 1) * 128], "ahT")
            for (c0, cw, dst, dk) in ((0, 128, cq[:, 0, :], "acq"), (128, 128, cq[:, 1, :], "acq"), (256, 128, ckv[:, :], "ackv"), (384, 64, krr[:, :], "akrr")):
                ps, pk = pp.next()
                for kc in range(KC):
                    p.op("pe", lambda e, ps=ps, kc=kc, c0=c0, cw=cw, N=N: e.matmul(ps[0:cw, 0:N], lhsT=win[:, kc, c0:c0 + cw], rhs=hT[:, kc, 0:N],
                                                                                 start=(kc == 0), stop=(kc == KC - 1)), r=["mwin", "ahT"], w=[pk])
                p.op("act", lambda e, ps=ps, dst=dst, cw=cw, N=N: e.copy(out=dst[0:cw, 0:N], in_=ps[0:cw, 0:N]), r=[pk], w=[dk])

            def rmsn(src_chunks, src_key, ones_ap, gcols, dst_chunks, dst_key):
                n = len(src_chunks)
                for c in range(n):
                    p.op("act", lambda e, c=c: e.activation(out=sq[:, c, 0:N], in_=src_chunks[c], func=AF.Square), r=[src_key], w=["asq"])
                ps, pk = pp.next()
                for c in range(n):
                    p.op("pe", lambda e, ps=ps, c=c: e.matmul(ps[:, 0:N], lhsT=ones_ap, rhs=sq[:, c, 0:N], start=(c == 0), stop=(c == n - 1)),
                         r=["asq", "monesq", "masks"], w=[pk])
                r_, rk = rs.next()
                p.op("dve", lambda e: e.tensor_scalar(out=r_[:, 0:N], in0=ps[:, 0:N], scalar1=EPS, scalar2=None, op0=ALU.add), r=[pk], w=[rk])
                p.op("act", lambda e: e.activation(out=r_[:, 0:N], in_=r_[:, 0:N], func=AF.Sqrt), r=[rk], w=[rk])
                p.op("dve", lambda e: e.reciprocal(out=r_[:, 0:N], in_=r_[:, 0:N]), r=[rk], w=[rk])
                for c in range(n):
                    p.op("dve", lambda e, c=c: e.scalar_tensor_tensor(out=dst_chunks[c], in0=src_chunks[c], scalar=gcols[c], in1=r_[:, 0:N],
                                                                      op0=ALU.mult, op1=ALU.mult), r=[src_key, rk, "mg"], w=[dst_key])

            rmsn([ckv[:, 0:N]], "ackv", C["ones128"][:], [kg[:, 0:1]], [ckvn[:, 0:N]], "ackvn")
            if is_lat:
                rmsn([cq[:, 0, 0:N], cq[:, 1, 0:N]], "acq", onesq[:], [qg[:, 0:1], qg[:, 1:2]], [cqn[:, 0, 0:N], cqn[:, 1, 0:N]], "acqn")
                lat0 = t0 - n_ctx
                p.op("pool", lambda e, lat0=lat0: e.iota(tti[:, 0:N], pattern=[[1, N]], base=lat0, channel_multiplier=0), w=["atti"])
                p.op("dve", lambda e: e.tensor_single_scalar(out=tt2[:, 0:N], in_=tti[:, 0:N], scalar=63, op=ALU.bitwise_and), r=["atti"], w=["att2"])
                p.op("dve", lambda e: e.tensor_copy(out=cosT[:, 0:N], in_=tt2[:, 0:N]), r=["att2"], w=["trig"])
                p.op("dve", lambda e: e.tensor_single_scalar(out=tt2[:, 0:N], in_=tti[:, 0:N], scalar=6, op=ALU.arith_shift_right), r=["atti", "trig"], w=["att2"])
                p.op("dve", lambda e: e.tensor_copy(out=sinT[:, 0:N], in_=tt2[:, 0:N]), r=["att2"], w=["trig"])
                p.op("dve", lambda e: e.tensor_tensor(out=sinT[:, 0:N], in0=sinT[:, 0:N], in1=cosT[:, 0:N], op=ALU.subtract), r=["trig"], w=["trig"])
                p.op("dve", lambda e: e.scalar_tensor_tensor(out=ang[:, 0:N], in0=sinT[:, 0:N], scalar=C["isrow"][:, 0:1], in1=cosT[:, 0:N], op0=ALU.mult, op1=ALU.add),
                     r=["trig", "rope_c"], w=["aang"])
                p.op("dve", lambda e: e.tensor_scalar(out=ang[:, 0:N], in0=ang[:, 0:N], scalar1=C["invf"][:, 0:1], scalar2=None, op0=ALU.mult), r=["aang", "rope_c"], w=["aang"])
                for (dst, off) in ((sinT, 0.0), (cosT, 0.5 * math.pi)):
                    p.op("dve", lambda e, off=off: e.tensor_scalar(out=a2[:, 0:N], in0=ang[:, 0:N], scalar1=off, scalar2=None, op0=ALU.add), r=["aang"], w=["aa2"])
                    p.op("dve", lambda e: e.tensor_scalar(out=kf[:, 0:N], in0=a2[:, 0:N], scalar1=1.0 / (2 * math.pi), scalar2=None, op0=ALU.mult), r=["aa2"], w=["akf"])
                    p.op("dve", lambda e: e.tensor_copy(out=tt2[:, 0:N], in_=kf[:, 0:N]), r=["akf"], w=["att2"])
                    p.op("dve", lambda e: e.tensor_copy(out=kf[:, 0:N], in_=tt2[:, 0:N]), r=["att2"], w=["akf"])
                    p.op("dve", lambda e, dst=dst: e.scalar_tensor_tensor(out=dst[:, 0:N], in0=kf[:, 0:N], scalar=-2 * math.pi, in1=a2[:, 0:N], op0=ALU.mult, op1=ALU.add),
                         r=["akf", "aa2"], w=["trig"])
                    p.op("dve", lambda e, dst=dst: e.tensor_scalar(out=dst[:, 0:N], in0=dst[:, 0:N], scalar1=-math.pi, scalar2=math.pi, op0=ALU.max, op1=ALU.min), r=["trig"], w=["trig"])
                    p.op("act", lambda e, dst=dst: e.activation(out=dst[:, 0:N], in_=dst[:, 0:N], func=AF.Sin), r=["trig"], w=["trig"])
                lsl = slice(lat0, lat0 + N)
                for h in range(8):
                    ps, pk = pp.next()
                    for kc in range(2):
                        p.op("pe", lambda e, ps=ps, kc=kc, h=h: e.matmul(ps[:, 0:N], lhsT=wq[:, kc, h * 192:h * 192 + 128], rhs=cqn[:, kc, 0:N], start=(kc == 0), stop=(kc == 1)),
                             r=["mwq", "acqn"], w=[pk])
                    o, ok = ob.next()
                    p.op("act", lambda e, o=o, ps=ps: e.activation(out=o[:, 0:N], in_=ps[:, 0:N], func=AF.Copy, scale=MLA_SCALE), r=[pk], w=[ok])
                    p.dma("sp", lambda e, o=o, h=h, lsl=lsl: e.dma_start(out=QN[h, :, lsl], in_=o[:, 0:N]), r=[ok], w=["QN"])
                    ps, pk = pp.next()
                    for kc in range(2):
                        p.op("pe", lambda e, ps=ps, kc=kc, h=h: e.matmul(ps[0:64, 0:N], lhsT=wq[:, kc, h * 192 + 128:h * 192 + 192], rhs=cqn[:, kc, 0:N], start=(kc == 0), stop=(kc == 1)),
                             r=["mwq", "acqn"], w=[pk])
                    o, ok = ob.next()
                    x_, xk_ = rx.next()
                    p.op("act", lambda e, x_=x_, ps=ps: e.copy(out=x_[:, 0:N], in_=ps[0:64, 0:N]), r=[pk], w=[xk_])
                    rope(x_[:, 0:N], xk_, N, MLA_SCALE, o[0:64, 0:N], ok)
                    p.dma("sp", lambda e, o=o, h=h, lsl=lsl: e.dma_start(out=QR[h, :, lsl], in_=o[0:64, 0:N]), r=[ok], w=["QR"])
            for h in range(8):
                ps, pk = pp.next()
                p.op("pe", lambda e, ps=ps, h=h: e.matmul(ps[:, 0:N], lhsT=wkn[:, h, :], rhs=ckvn[:, 0:N], start=True, stop=True), r=["mwk", "ackvn"], w=[pk])
                o, ok = ob.next()
                p.op("act", lambda e, o=o, ps=ps: e.copy(out=o[:, 0:N], in_=ps[:, 0:N]), r=[pk], w=[ok])
                p.dma("sp", lambda e, o=o, h=h, t0=t0, N=N: e.dma_start(out=KN[h, :, t0:t0 + N], in_=o[:, 0:N]), r=[ok], w=["KN"])
            for j in range(N // 128):
                vt, vk = vb.next()
                for g in range(2):
                    ps, pk = pp.next()
                    p.op("pe", lambda e, ps=ps, j=j, g=g: e.matmul(ps[:, :], lhsT=ckvn[:, j * 128:(j + 1) * 128], rhs=wv[:, 4 * g:4 * g + 4, :], start=True, stop=True),
                         r=["mwk", "ackvn"], w=[pk])
                    p.op("dve", lambda e, vt=vt, ps=ps, g=g: e.tensor_copy(out=vt[:, g * 512:(g + 1) * 512], in_=ps[:, :]), r=[pk], w=[vk])
                tok = t0 + j * 128
                p.dma("sp", lambda e, vt=vt, tok=tok: e.dma_start(out=V[tok:tok + 128, :], in_=vt[:]), r=[vk], w=["Vt"])
            o, ok = ob.next()
            if is_lat:
                rope(krr[:, 0:N], "akrr", N, 1.0, o[0:64, 0:N], ok)
            else:
                p.op("dve", lambda e, o=o: e.tensor_copy(out=o[0:64, 0:N], in_=krr[:, 0:N]), r=["akrr"], w=[ok])
            p.dma("sp", lambda e, o=o, t0=t0, N=N: e.dma_start(out=KR[:, t0:t0 + N], in_=o[0:64, 0:N]), r=[ok], w=["KRo"])


        for (t0_, N_, lat_) in blocks:
            do_block(t0_, N_, lat_)

def phase_mla_attn(p, C, io, n_ctx, n_tok):
    QN, QR, KN, KR, V, MIXT = io["QN"], io["QR"], io["KN"], io["KRo"], io["Vt"], io["MIXT"]
    n_lat = n_tok - n_ctx
    nkt = n_tok // 128
    with p.phase():
        kr = p.sb("bkr", [64, n_tok], BF16)
        p.dma("sp", lambda e: e.dma_start(out=kr[:], in_=KR[:, :]), w=["bkr"])
        ones_bf = p.sb("bones", [128, 128], BF16)
        p.op("dve", lambda e: e.memset(ones_bf[:], 1.0), w=["bones"])
        kn = Ring(p, "bkn", [128, n_tok], BF16, n=2)
        vv = Ring(p, "bvv", [128, nkt, 128], BF16, n=2)
        qn = Ring(p, "bqn", [128, 512], BF16, n=2)
        qr = Ring(p, "bqr", [64, 512], BF16, n=2)
        pt = Ring(p, "bpt", [128, 512], BF16, n=4)
        rd = Ring(p, "brd", [128, 512], n=2)
        ob = Ring(p, "bob", [128, 512], BF16, n=2)
        psS = Ring(p, "bpsS", [128, 512], n=4, psum=True)
        psO = Ring(p, "bpsO", [128, 512], n=2, psum=True)
        psD = Ring(p, "bpsD", [128, 512], n=2, psum=True)
        for h in range(8):
            k_, kk = kn.next()
            v_, vk = vv.next()
            p.dma("sp", lambda e, k_=k_, h=h: e.dma_start(out=k_[:], in_=KN[h, :, :]), w=[kk])
            for j0 in range(0, nkt, 12):
                j1 = min(nkt, j0 + 12)
                p.dma("act", lambda e, v_=v_, h=h, j0=j0, j1=j1: e.dma_start(
                    out=v_[:, j0:j1, :], in_=V[j0 * 128:j1 * 128, h * 128:(h + 1) * 128].rearrange("(j q) c -> q j c", q=128)), w=[vk])
            for qb in range(n_lat // 512):
                qsl = slice(qb * 512, (qb + 1) * 512)
                a, ak = qn.next()
                b, bk = qr.next()
                p.dma("sp", lambda e, a=a, h=h, qsl=qsl: e.dma_start(out=a[:], in_=QN[h, :, qsl]), w=[ak])
                p.dma("sp", lambda e, b=b, h=h, qsl=qsl: e.dma_start(out=b[:], in_=QR[h, :, qsl]), w=[bk])
                po, pok = psO.next()
                pd, pdk = psD.next()
                def S_(kt):
                    ks = slice(kt * 128, (kt + 1) * 128)
                    ps, psk = psS.next()
                    p.op("pe", lambda e, ps=ps, k_=k_, ks=ks, a=a: e.matmul(ps[:], lhsT=k_[:, ks], rhs=a[:], start=True, stop=False), r=[kk, ak], w=[psk])
                    p.op("pe", lambda e, ps=ps, ks=ks, b=b: e.matmul(ps[:], lhsT=kr[:, ks], rhs=b[:], start=False, stop=True), r=["bkr", bk], w=[psk])
                    return ps, psk
                LA = 2
                pend = [S_(kt) for kt in range(min(LA, nkt))]
                for kt in range(nkt):
                    if kt + LA < nkt:
                        pend.append(S_(kt + LA))
                    ps, psk = pend.pop(0)
                    t, tk = pt.next()
                    p.op("act", lambda e, t=t, ps=ps: e.activation(out=t[:], in_=ps[:], func=AF.Exp), r=[psk], w=[tk])
                    p.op("pe", lambda e, po=po, v_=v_, kt=kt, t=t: e.matmul(po[:], lhsT=v_[:, kt, :], rhs=t[:], start=(kt == 0), stop=(kt == nkt - 1)), r=[vk, tk], w=[pok])
                    p.op("pe", lambda e, pd=pd, t=t, kt=kt: e.matmul(pd[:], lhsT=ones_bf[:], rhs=t[:], start=(kt == 0), stop=(kt == nkt - 1)), r=["bones", tk], w=[pdk])
                r_, rk = rd.next()
                p.op("dve", lambda e, r_=r_, pd=pd: e.reciprocal(out=r_[:], in_=pd[:]), r=[pdk], w=[rk])
                o, ok = ob.next()
                p.op("dve", lambda e, o=o, po=po, r_=r_: e.tensor_tensor(out=o[:], in0=po[:], in1=r_[:], op=ALU.mult), r=[pok, rk], w=[ok])
                p.dma("sp", lambda e, o=o, h=h, qb=qb: e.dma_start(out=MIXT[h * 128:(h + 1) * 128, n_ctx + qb * 512:n_ctx + (qb + 1) * 512], in_=o[:]), r=[ok], w=["MIXT"])

CAST_IN_GATHER = 1


def moe_tables(p, NE, max_tiles, max_nb):
    RT = {}
    for nm in ("e4", "rank4", "gate4", "destf"):
        RT[nm] = p.sb("rt_" + nm, [128, max_tiles, 4], F32)
    RT["desti"] = p.sb("rt_desti", [128, max_tiles, 4], I32)
    RT["pstart"] = p.sb("rt_pstart", [128, NE], F32)
    RT["blke"] = p.sb("rt_blke", [128, max_nb], F32)
    RT["widx"] = p.sb("rt_widx", [128, max_nb, 8], I32)
    return RT


def phase_moe_sparse_route(p, C, io, G, RT, l, NE, t_lo, t_hi, n_ctx, BLK=512):
    xres, HTOK, modrow = io["xres"], io["HTOK"], io["modrow"]
    T_ = t_hi - t_lo
    ntile = T_ // 128
    NB = (4 * T_ + BLK - 1) // BLK + NE
    LOG = BLK.bit_length() - 1
    e4, rank4, gate4 = RT["e4"], RT["rank4"], RT["gate4"]
    with p.phase():
        T = norm_scratch(p)
        wr = p.sb("wr", [128, KC, NE], F32)
        p.dma("sp", lambda e: e.dma_start(out=wr[:], in_=io["router_w"][l].rearrange("(c q) n -> q c n", q=128)), w=["wr"])
        br = p.sb("br", [128, NE], F32)
        p.dma("sp", lambda e: e.dma_start(out=br[:], in_=io["router_b"][l].partition_broadcast(128)), w=["wr"])
        Arow = p.sb("sArow", [128, 2, D], F32)
        Brow = p.sb("sBrow", [128, 2, D], F32)
        grow = p.sb("sgrow", [128, D], F32)
        p.dma("sp", lambda e: e.dma_start(out=grow[:], in_=io["norm2_g"][l].partition_broadcast(128)), w=["sgrow"])
        for r in range(2):
            p.dma("sp", lambda e, r=r: e.dma_start(out=Arow[:, r, :], in_=modrow[l, r, 4096:5120].partition_broadcast(128)), w=["sArow"])
            p.dma("sp", lambda e, r=r: e.dma_start(out=Brow[:, r, :], in_=modrow[l, r, 3072:4096].partition_broadcast(128)), w=["sBrow"])
            p.op("dve", lambda e, r=r: e.scalar_tensor_tensor(out=Arow[:, r, :], in0=Arow[:, r, :], scalar=1.0, in1=grow[:], op0=ALU.add, op1=ALU.mult),
                 r=["sArow", "sgrow"], w=["sArow"])
        cnt = p.sb("scnt", [128, NE], F32)
        p.op("dve", lambda e: e.memset(cnt[:], 0.0), w=["scnt"])
        xt = Ring(p, "sxt", [128, D], n=2)
        h32 = Ring(p, "sh32", [128, KC, 128], n=2)
        ht = Ring(p, "sht", [128, D], n=2)
        hb = Ring(p, "shb", [128, D], BF16, n=2)
        sm = {nm: Ring(p, "s_" + nm, [128, NE], n=2) for nm in ("lg", "ex", "mk", "rk", "oh", "tmp")}
        m8 = Ring(p, "sm8", [128, 8], n=2)
        sc = Ring(p, "ssc", [128, 2], n=2)
        psr = p.ps("spsr", [128, NE], F32)
        ps2 = p.ps("sps2", [128, NE], F32)
        ps3 = p.ps("sps3", [128, NE], F32)
        for ti in range(ntile):
            tok = t_lo + ti * 128
            r = 1 if tok < n_ctx else 0
            x, xk = xt.next()
            p.dma("sp", lambda e, x=x, tok=tok: e.dma_start(out=x[:], in_=xres[tok:tok + 128, :]), w=[xk])
            f, fk = h32.next()
            norm_tile(p, C, T, xk, x, G["A2"][:, l, r, :], G["B2"][:, l, r, :], None, fk, hT32_dst=lambda kc, f=f: f[:, kc, :], want_bf=False)
            fk32 = fk + "32"
            a, ak = ht.next()
            b, bk = hb.next()
            p.op("dve", lambda e, a=a, r=r: e.tensor_tensor(out=a[:], in0=T["xn"][:], in1=Arow[:, r, :], op=ALU.mult), r=["xn", "sArow"], w=[ak])
            p.op("pool", lambda e, a=a, b=b, r=r: e.tensor_tensor(out=b[:], in0=a[:], in1=Brow[:, r, :], op=ALU.add), r=[ak, "sBrow"], w=[bk])
            p.dma("sp", lambda e, b=b, tok=tok: e.dma_start(out=HTOK[tok:tok + 128, :], in_=b[:]), r=[bk], w=["HTOK"])
            for kc in range(KC):
                p.op("pe", lambda e, f=f, kc=kc: e.matmul(psr[:], lhsT=f[:, kc, :], rhs=wr[:, kc, :], start=(kc == 0), stop=(kc == KC - 1)),
                     r=[fk32, "wr"], w=["spsr"])
            g, gk = sm["lg"].next()
            p.op("dve", lambda e, g=g: e.tensor_tensor(out=g[:], in0=psr[:], in1=br[:], op=ALU.add), r=["spsr", "wr"], w=[gk])
            m, mk8 = m8.next()
            p.op("dve", lambda e, m=m, g=g: e.max(out=m[:], in_=g[:]), r=[gk], w=[mk8])
            msk, mskk = sm["mk"].next()
            p.op("dve", lambda e, msk=msk, g=g, m=m: e.tensor_scalar(out=msk[:], in0=g[:], scalar1=m[:, 3:4], scalar2=None, op0=ALU.is_ge), r=[gk, mk8], w=[mskk])
            s, sk = sc.next()
            p.op("dve", lambda e, s=s, m=m: e.tensor_scalar(out=s[:, 0:1], in0=m[:, 0:1], scalar1=-1.0, scalar2=None, op0=ALU.mult), r=[mk8], w=[sk])
            x_, xk_ = sm["ex"].next()
            p.op("act", lambda e, x_=x_, g=g, s=s: e.activation(out=x_[:], in_=g[:], func=AF.Exp, bias=s[:, 0:1]), r=[gk, sk], w=[xk_])
            p.op("dve", lambda e, x_=x_, msk=msk: e.tensor_tensor(out=x_[:], in0=x_[:], in1=msk[:], op=ALU.mult), r=[xk_, mskk], w=[xk_])
            p.op("dve", lambda e, s=s, x_=x_: e.reduce_sum(out=s[:, 1:2], in_=x_[:], axis=AX.X), r=[xk_], w=[sk])
            p.op("dve", lambda e, s=s: e.reciprocal(out=s[:, 1:2], in_=s[:, 1:2]), r=[sk], w=[sk])
            p.op("dve", lambda e, x_=x_, s=s: e.tensor_scalar(out=x_[:], in0=x_[:], scalar1=s[:, 1:2], scalar2=None, op0=ALU.mult), r=[xk_, sk], w=[xk_])
            p.op("pe", lambda e, msk=msk: e.matmul(ps2[:], lhsT=C["strict0"][:], rhs=msk[:], start=True, stop=True), r=["masks", mskk], w=["sps2"])
            p.op("pe", lambda e, msk=msk: e.matmul(ps3[:], lhsT=C["ones64"][:], rhs=msk[:], start=True, stop=True), r=["masks", mskk], w=["sps3"])
            rk, rkk = sm["rk"].next()
            p.op("dve", lambda e, rk=rk: e.tensor_tensor(out=rk[:], in0=ps2[:], in1=cnt[:], op=ALU.add), r=["sps2", "scnt"], w=[rkk])
            p.op("dve", lambda e: e.tensor_tensor(out=cnt[:], in0=ps3[:], in1=cnt[:], op=ALU.add), r=["sps3", "scnt", rkk], w=["scnt"])
            for k in range(4):
                oh, ohk = sm["oh"].next()
                p.op("dve", lambda e, oh=oh, g=g, m=m, k=k: e.tensor_scalar(out=oh[:], in0=g[:], scalar1=m[:, k:k + 1], scalar2=None, op0=ALU.is_equal), r=[gk, mk8], w=[ohk])
                for (src, srck, dst) in ((C["iota_f"][:, 0:NE], "iota_f", e4), (rk[:], rkk, rank4), (x_[:], xk_, gate4)):
                    tmp, tmpk = sm["tmp"].next()
                    p.op("dve", lambda e, tmp=tmp, oh=oh, src=src: e.tensor_tensor(out=tmp[:], in0=oh[:], in1=src, op=ALU.mult), r=[ohk, srck], w=[tmpk])
                    p.op("dve", lambda e, tmp=tmp, dst=dst, ti=ti, k=k: e.reduce_sum(out=dst[:, ti, k:k + 1], in_=tmp[:], axis=AX.X), r=[tmpk], w=["rt"])
        ci = p.sb("sci", [128, NE], I32)
        padf = p.sb("spadf", [128, NE], F32)
        ca = p.sb("sca", [128, NE], F32)
        cb = p.sb("scb", [128, NE], F32)
        p.op("dve", lambda e: e.tensor_copy(out=ci[:], in_=cnt[:]), r=["scnt"], w=["sci"])
        p.op("dve", lambda e: e.tensor_single_scalar(out=ci[:], in_=ci[:], scalar=BLK - 1, op=ALU.add), r=["sci"], w=["sci"])
        p.op("dve", lambda e: e.tensor_single_scalar(out=ci[:], in_=ci[:], scalar=LOG, op=ALU.arith_shift_right), r=["sci"], w=["sci"])
        p.op("dve", lambda e: e.tensor_single_scalar(out=ci[:], in_=ci[:], scalar=LOG, op=ALU.logical_shift_left), r=["sci"], w=["sci"])
        p.op("dve", lambda e: e.tensor_copy(out=padf[:], in_=ci[:]), r=["sci"], w=["spadf"])
        p.op("dve", lambda e: e.tensor_copy(out=ca[:], in_=padf[:]), r=["spadf"], w=["sca"])
        cur, curk, oth, othk = ca, "sca", cb, "scb"
        sft = 1
        while sft < NE:
            p.op("dve", lambda e, cur=cur, oth=oth: e.tensor_copy(out=oth[:], in_=cur[:]), r=[curk], w=[othk])
            p.op("dve", lambda e, cur=cur, oth=oth, sft=sft: e.tensor_tensor(out=oth[:, sft:NE], in0=cur[:, sft:NE], in1=cur[:, 0:NE - sft], op=ALU.add), r=[curk, othk], w=[othk])
            cur, curk, oth, othk = oth, othk, cur, curk
            sft *= 2
        pend, pendk = cur, curk
        p.op("dve", lambda e: e.tensor_tensor(out=RT["pstart"][:], in0=pend[:], in1=padf[:], op=ALU.subtract), r=[pendk, "spadf"], w=["rt"])
        bst = p.sb("sbst", [128, NB], F32)
        p.op("dve", lambda e: e.tensor_scalar(out=bst[:], in0=C["iota_f"][:, 0:NB], scalar1=float(BLK), scalar2=None, op0=ALU.mult), r=["iota_f"], w=["sbst"])
        blke = RT["blke"]
        p.op("dve", lambda e: e.memset(blke[:, 0:NB], 0.0), w=["rt"])
        for e_ in range(NE):
            p.op("dve", lambda e, e_=e_: e.scalar_tensor_tensor(out=blke[:, 0:NB], in0=bst[:], scalar=pend[:, e_:e_ + 1], in1=blke[:, 0:NB], op0=ALU.is_ge, op1=ALU.add),
                 r=["sbst", pendk, "rt"], w=["rt"])
        p.op("dve", lambda e: e.tensor_scalar(out=blke[:, 0:NB], in0=blke[:, 0:NB], scalar1=float(NE - 1), scalar2=None, op0=ALU.min), r=["rt"], w=["rt"])
        base8 = p.sb("sbase8", [128, 8], F32)
        p.op("dve", lambda e: e.tensor_scalar(out=base8[:], in0=C["iota_f"][:, 0:8], scalar1=128.0, scalar2=C["iota_p"][:, 0:1], op0=ALU.mult, op1=ALU.add),
             r=["iota_f", "iota_p"], w=["sbase8"])
        b1k = p.sb("sb1k", [128, NB], F32)
        lofs = float(l * NE * 1024) if CAST_IN_GATHER else 0.0
        p.op("dve", lambda e: e.tensor_scalar(out=b1k[:], in0=blke[:, 0:NB], scalar1=1024.0, scalar2=lofs, op0=ALU.mult, op1=ALU.add), r=["rt"], w=["sb1k"])
        wf = p.sb("swf", [128, NB, 8], F32)
        for b_ in range(NB):
            p.op("dve" if b_ % 2 == 0 else "pool", lambda e, b_=b_: e.tensor_scalar(out=wf[:, b_, :], in0=base8[:], scalar1=b1k[:, b_:b_ + 1], scalar2=None, op0=ALU.add),
                 r=["sbase8", "sb1k"], w=["swf"])
        p.op("dve", lambda e: e.tensor_copy(out=RT["widx"][:, 0:NB, :], in_=wf[:]), r=["swf"], w=["rt"])
        destf, desti = RT["destf"], RT["desti"]
        for ti in range(ntile):
            for k in range(4):
                oh, ohk = sm["oh"].next()
                p.op("dve", lambda e, oh=oh, ti=ti, k=k: e.tensor_scalar(out=oh[:], in0=C["iota_f"][:, 0:NE], scalar1=e4[:, ti, k:k + 1], scalar2=None, op0=ALU.is_equal),
                     r=["iota_f", "rt"], w=[ohk])
                tmp, tmpk = sm["tmp"].next()
                p.op("dve", lambda e, tmp=tmp, oh=oh: e.tensor_tensor(out=tmp[:], in0=oh[:], in1=RT["pstart"][:], op=ALU.mult), r=[ohk, "rt"], w=[tmpk])
                p.op("dve", lambda e, tmp=tmp, ti=ti, k=k: e.reduce_sum(out=destf[:, ti, k:k + 1], in_=tmp[:], axis=AX.X), r=[tmpk], w=["rt"])
        p.op("dve", lambda e: e.tensor_tensor(out=destf[:, 0:ntile, :], in0=destf[:, 0:ntile, :], in1=rank4[:, 0:ntile, :], op=ALU.add), r=["rt"], w=["rt"])
        p.op("dve", lambda e: e.tensor_scalar(out=destf[:, 0:ntile, :], in0=destf[:, 0:ntile, :], scalar1=float(NB * BLK - 1), scalar2=0.0, op0=ALU.min, op1=ALU.max), r=["rt"], w=["rt"])
        p.op("dve", lambda e: e.tensor_copy(out=desti[:, 0:ntile, :], in_=destf[:, 0:ntile, :]), r=["rt"], w=["rt"])
    return NB


def phase_moe_sparse_dispatch(p, C, io, RT, t_lo, t_hi, NB, BLK=512):
    HTOK, HS = io["HTOK"], io["HS"]
    ntile = (t_hi - t_lo) // 128
    with p.phase():
        z = p.sb("dz", [128, 4, D], BF16)
        p.op("pool", lambda e: e.memset(z[:], 0.0), w=["dz"])
        for b_ in range(NB * BLK // 512):
            p.dma("sp" if b_ % 2 == 0 else "act", lambda e, b_=b_: e.dma_start(out=HS[b_ * 512:(b_ + 1) * 512, :].rearrange("(j q) d -> q j d", q=128), in_=z[:]),
                  r=["dz"], w=[f"HSz{b_ % 8}"])
        hb = Ring(p, "dhb", [128, D], BF16, n=3)
        zk = [f"HSz{i}" for i in range(8)]
        for ti in range(ntile):
            tok = t_lo + ti * 128
            h, hk = hb.next()
            p.dma("sp", lambda e, h=h, tok=tok: e.dma_start(out=h[:], in_=HTOK[tok:tok + 128, :]), w=[hk])
            for k in range(4):
                p.dma("pool", lambda e, h=h, ti=ti, k=k: e.indirect_dma_start(
                    out=HS[:, :], out_offset=bass.IndirectOffsetOnAxis(ap=RT["desti"][:, ti, k:k + 1], axis=0), in_=h[:, :], in_offset=None),
                    r=[hk, "rt"] + zk, w=[f"HSs{(ti * 4 + k) % 8}"])


def phase_moe_sparse_experts(p, C, io, RT, l, NE, NB, BLK=512):
    HS, Y = io["HS"], io["Y"]
    if CAST_IN_GATHER:
        WGf = io["moe_gu_w"].rearrange("l e k n -> (l e k) n")
        WDf = io["moe_down_w"].rearrange("l e k n -> (l e k) n")
    else:
        WGf = io["WGU"].rearrange("e k n -> (e k) n")
        WDf = io["WDN"].rearrange("e k n -> (e k) n")
    blke = RT["blke"]
    with p.phase():
        b32 = p.sb("xb32", [NE, 2048], F32)
        bgu = p.sb("xbgu", [NE, 2048], BF16)
        bdn = p.sb("xbdn", [NE, 1024], BF16)
        p.dma("sp", lambda e: e.dma_start(out=b32[:], in_=io["moe_gu_b"][l]), w=["xb32"])
        p.op("dve", lambda e: e.tensor_copy(out=bgu[:], in_=b32[:]), r=["xb32"], w=["xbias"])
        p.dma("sp", lambda e: e.dma_start(out=b32[:, 0:1024], in_=io["moe_down_b"][l]), r=["xbias"], w=["xb32"])
        p.op("dve", lambda e: e.tensor_copy(out=bdn[:], in_=b32[:, 0:1024]), r=["xb32"], w=["xbias"])
        ones5 = p.sb("xones", [NE, BLK], F32)
        p.op("dve", lambda e: e.memset(ones5[:], 1.0), w=["xones"])
        wg = Ring(p, "xwg", [128, KC, 2048], BF16, n=2)
        wd = Ring(p, "xwd", [128, KC, 1024], BF16, n=2)
        hs = Ring(p, "xhs", [128, BLK // 128, D], BF16, n=2)
        hsT = Ring(p, "xhsT", [128, KC, BLK], BF16, n=2)
        ohT = Ring(p, "xohT", [NE, BLK], BF16, n=2)
        actT = Ring(p, "xactT", [128, KC, BLK], BF16, n=1)
        t1 = Ring(p, "xt1", [128, BLK], n=2)
        sgm = Ring(p, "xsg", [128, BLK], n=2)
        t2 = Ring(p, "xt2", [128, BLK], n=2)
        yo = Ring(p, "xyo", [128, D], n=2)
        pst = Ring(p, "xpst", [128, KC, 128], BF16, n=2, psum=True)
        psg = Ring(p, "xpsg", [128, 512], n=4, psum=True)
        psd = Ring(p, "xpsd", [128, 512], n=2, psum=True)
        ev = 0
        for b_ in range(NB):
            g_, gk = wg.next()
            d_, dk = wd.next()
            for kc in range(KC):
                p.dma("pool", lambda e, g_=g_, kc=kc, b_=b_: e.indirect_dma_start(
                    out=g_[:, kc, :], out_offset=None, in_=WGf[:, :], in_offset=bass.IndirectOffsetOnAxis(ap=RT["widx"][:, b_, kc:kc + 1], axis=0)), r=["rt"], w=[gk])
                p.dma("pool", lambda e, d_=d_, kc=kc, b_=b_: e.indirect_dma_start(
                    out=d_[:, kc, :], out_offset=None, in_=WDf[:, :], in_offset=bass.IndirectOffsetOnAxis(ap=RT["widx"][:, b_, kc:kc + 1], axis=0)), r=["rt"], w=[dk])
            h_, hk = hs.next()
            p.dma("sp", lambda e, h_=h_, b_=b_: e.dma_start(out=h_[:], in_=HS[b_ * BLK:(b_ + 1) * BLK, :].rearrange("(j q) d -> q j d", q=128)), w=[hk])
            o_, ok_ = ohT.next()
            p.op("dve", lambda e, o_=o_, b_=b_: e.tensor_scalar(out=o_[:], in0=ones5[:], scalar1=blke[0:NE, b_:b_ + 1], scalar2=C["iota_p"][0:NE, 0:1],
                                                                 op0=ALU.mult, op1=ALU.is_equal), r=["xones", "rt", "iota_p"], w=[ok_])
            hT, hTk = hsT.next()
            for j in range(BLK // 128):
                pt_, ptk = pst.next()
                for kc in range(KC):
                    p.op("pe", lambda e, pt_=pt_, h_=h_, j=j, kc=kc: e.transpose(pt_[:, kc, :], h_[:, j, kc * 128:(kc + 1) * 128], C["identb"][:]), r=[hk, "identb"], w=[ptk])
                if j % 2 == 0:
                    p.op("act", lambda e, hT=hT, pt_=pt_, j=j: e.copy(out=hT[:, :, j * 128:(j + 1) * 128], in_=pt_[:]), r=[ptk], w=[hTk])
                else:
                    p.op("dve", lambda e, hT=hT, pt_=pt_, j=j: e.tensor_copy(out=hT[:, :, j * 128:(j + 1) * 128], in_=pt_[:]), r=[ptk], w=[hTk])
            aT, aTk = actT.next()
            N = BLK
            for c in range(8):
                pa, pak = psg.next()
                pb, pbk = psg.next()
                for (ps_, pk_, col0) in ((pa, pak, c * 128), (pb, pbk, DE + c * 128)):
                    for kc in range(KC):
                        p.op("pe", lambda e, ps_=ps_, g_=g_, kc=kc, col0=col0, hT=hT: e.matmul(
                            ps_[:, 0:N], lhsT=g_[:, kc, col0:col0 + 128], rhs=hT[:, kc, :], start=(kc == 0), stop=False), r=[gk, hTk], w=[pk_])
                    p.op("pe", lambda e, ps_=ps_, col0=col0, o_=o_: e.matmul(ps_[:, 0:N], lhsT=bgu[:, col0:col0 + 128], rhs=o_[:], start=False, stop=True),
                         r=["xbias", ok_], w=[pk_])
                a1, a1k = t1.next()
                p.op("dve", lambda e, a1=a1, pa=pa: e.tensor_scalar(out=a1[:], in0=pa[:], scalar1=SW_LIMIT, scalar2=None, op0=ALU.min), r=[pak], w=[a1k])
                s_, sk_ = sgm.next()
                p.op("act", lambda e, s_=s_, a1=a1: e.activation(out=s_[:], in_=a1[:], func=AF.Sigmoid, scale=SW_ALPHA), r=[a1k], w=[sk_])
                a2, a2k = t2.next()
                p.op("dve", lambda e, a2=a2, pb=pb: e.tensor_scalar(out=a2[:], in0=pb[:], scalar1=SW_LIMIT, scalar2=-SW_LIMIT, op0=ALU.min, op1=ALU.max), r=[pbk], w=[a2k])
                p.op("dve", lambda e, a1=a1, s_=s_: e.tensor_tensor(out=a1[:], in0=a1[:], in1=s_[:], op=ALU.mult), r=[a1k, sk_], w=[a1k])
                p.op("dve", lambda e, aT=aT, a1=a1, a2=a2, c=c: e.scalar_tensor_tensor(out=aT[:, c, :], in0=a2[:], scalar=1.0, in1=a1[:], op0=ALU.add, op1=ALU.mult),
                     r=[a1k, a2k], w=[aTk])
            for j in range(BLK // 128):
                y_, yk = yo.next()
                for n in range(2):
                    pd, pdk = psd.next()
                    for kc in range(KC):
                        p.op("pe", lambda e, pd=pd, aT=aT, d_=d_, kc=kc, j=j, n=n: e.matmul(
                            pd[:], lhsT=aT[:, kc, j * 128:(j + 1) * 128], rhs=d_[:, kc, n * 512:(n + 1) * 512], start=(kc == 0), stop=False), r=[aTk, dk], w=[pdk])
                    p.op("pe", lambda e, pd=pd, o_=o_, n=n: e.matmul(pd[:], lhsT=o_[:, 0:128], rhs=bdn[:, n * 512:(n + 1) * 512], start=False, stop=True),
                         r=[ok_, "xbias"], w=[pdk])
                    ev += 1
                    if ev % 2 == 0:
                        p.op("act", lambda e, y_=y_, pd=pd, n=n: e.copy(out=y_[:, n * 512:(n + 1) * 512], in_=pd[:]), r=[pdk], w=[yk])
                    else:
                        p.op("dve", lambda e, y_=y_, pd=pd, n=n: e.tensor_copy(out=y_[:, n * 512:(n + 1) * 512], in_=pd[:]), r=[pdk], w=[yk])
                row = b_ * BLK + j * 128
                p.dma("sp", lambda e, y_=y_, row=row: e.dma_start(out=Y[row:row + 128, :], in_=y_[:]), r=[yk], w=["Y"])


def phase_moe_sparse_combine(p, C, io, RT, l, t_lo, t_hi, n_ctx):
    xres, Y, modrow = io["xres"], io["Y"], io["modrow"]
    ntile = (t_hi - t_lo) // 128
    with p.phase():
        gt = p.sb("cgt2", [128, 2, D], F32)
        for r in range(2):
            p.dma("sp", lambda e, r=r: e.dma_start(out=gt[:, r, :], in_=modrow[l, r, 5120:6144].partition_broadcast(128)), w=["cgt2"])
        yk_ = Ring(p, "cyk", [128, D], n=8)
        acc = Ring(p, "cacc", [128, D], n=2)
        xt = Ring(p, "cxt", [128, D], n=2)
        for ti in range(ntile):
            tok = t_lo + ti * 128
            r = 1 if tok < n_ctx else 0
            x, xk = xt.next()
            p.dma("sp", lambda e, x=x, tok=tok: e.dma_start(out=x[:], in_=xres[tok:tok + 128, :]), w=[xk])
            a, ak = acc.next()
            for k in range(4):
                y, yk = yk_.next()
                p.dma("pool", lambda e, y=y, ti=ti, k=k: e.indirect_dma_start(
                    out=y[:, :], out_offset=None, in_=Y[:, :], in_offset=bass.IndirectOffsetOnAxis(ap=RT["desti"][:, ti, k:k + 1], axis=0)), r=["rt", "Y"], w=[yk])
                if k == 0:
                    p.op("dve", lambda e, a=a, y=y, ti=ti: e.tensor_scalar(out=a[:], in0=y[:], scalar1=RT["gate4"][:, ti, 0:1], scalar2=None, op0=ALU.mult), r=[yk, "rt"], w=[ak])
                else:
                    p.op("dve", lambda e, a=a, y=y, ti=ti, k=k: e.scalar_tensor_tensor(out=a[:], in0=y[:], scalar=RT["gate4"][:, ti, k:k + 1], in1=a[:], op0=ALU.mult, op1=ALU.add),
                         r=[yk, "rt", ak], w=[ak])
            p.op("dve", lambda e, a=a, r=r: e.tensor_tensor(out=a[:], in0=a[:], in1=gt[:, r, :], op=ALU.mult), r=[ak, "cgt2"], w=[ak])
            p.op("dve", lambda e, a=a, x=x: e.tensor_tensor(out=x[:], in0=x[:], in1=a[:], op=ALU.add), r=[ak, xk], w=[xk])
            p.dma("sp", lambda e, x=x, tok=tok: e.dma_start(out=xres[tok:tok + 128, :], in_=x[:]), r=[xk], w=["xres"])

from concourse.bass_utils import run_bass_kernel_spmd

N_CTX = 256
N_LAT = 8192
DEPTH = 2
NEXP = 32
_W_NAMES = ["ada_w", "ada_b", "norm1_g", "norm2_g", "ev_w_in", "ev_w_out", "gla_gk_up", "gla_gk_b", "gla_norm_g",
            "rw_mu", "rw_w0", "rw_w2", "rw_a0", "rw_a2", "rw_k_k", "rw_k_a", "rw_r_k", "rw_g2", "rw_ln_w", "rw_ln_b",
            "od_w_in", "mla_q_norm", "mla_wq_up", "mla_kv_norm", "mla_wkv_up", "od_w_out",
            "router_w", "router_b", "moe_gu_w", "moe_gu_b", "moe_down_w", "moe_down_b", "final_norm_g"]


def build(shapes, n_ctx=N_CTX, n_lat=N_LAT, NE=NEXP):
    n_tok = n_ctx + n_lat
    L = DEPTH
    nc = bass.Bass("TRN2", target_bir_lowering=False)
    NBmax = (4 * n_tok + 511) // 512 + NE
    io = {}
    for k, shp in shapes.items():
        io[k] = dram(nc, k, list(shp), kind="ExternalInput")
    for name, shp, dt in (("xres", [n_tok, 1024], F32), ("modrow", [L, 2, 6144], F32), ("PT", [3376, n_tok], F32),
                          ("PTOK", [n_tok, 1536], F32), ("SMT", [1824, n_tok], F32), ("YTG", [2, 512, n_tok], F32),
                          ("YTR", [2, 512, n_tok], F32), ("RK", [2, 512, n_tok], F32), ("MIXT", [1024, n_tok], BF16),
                          ("HT", [1024, n_tok], BF16), ("GATE", [n_tok, NE], F32), ("WGU", [NE, 1024, 2048], BF16),
                          ("WDN", [NE, 1024, 1024], BF16), ("QN", [8, 128, n_lat], BF16), ("QR", [8, 64, n_lat], BF16),
                          ("KN", [8, 128, n_tok], BF16), ("KRo", [64, n_tok], BF16), ("Vt", [n_tok, 1024], BF16),
                          ("HTOK", [n_tok, 1024], BF16), ("HS", [NBmax * 512, 1024], BF16), ("Y", [NBmax * 512, 1024], F32)):
        io[name] = dram(nc, name, shp, dt)
    io["out"] = dram(nc, "out", [n_lat, 1024], kind="ExternalOutput")
    p = Prog(nc)
    C = make_consts(p)
    make_masks(p, C)
    make_rope_consts(p, C)
    G = {k: p.sb("G" + k, [128, L, 2, 8], F32) for k in ("A1", "B1", "A2", "B2")}
    RT = moe_tables(p, NE, n_tok // 128, NBmax)
    with p.phase():
        cp = Ring(p, "cpx", [128, 1024], n=3)
        for ti in range(n_tok // 128):
            x_, xk = cp.next()
            p.dma("sp", lambda e, x_=x_, ti=ti: e.dma_start(out=x_[:], in_=io["xin"][ti * 128:(ti + 1) * 128, :]), w=[xk])
            p.dma("sp", lambda e, x_=x_, ti=ti: e.dma_start(out=io["xres"][ti * 128:(ti + 1) * 128, :], in_=x_[:]), r=[xk], w=["xres"])
    phase_ada(p, C, io, L, G)
    phase_inproj(p, C, io, G, 0, n_ctx, n_tok)
    phase_gla(p, C, io, n_ctx, n_tok)
    phase_gla_finish(p, C, io, n_tok)
    phase_shift(p, C, io, n_ctx, n_tok)
    phase_rwkv(p, C, io, n_ctx, n_tok)
    phase_rwkv_finish(p, C, io, n_tok)
    phase_outproj(p, C, io, 0, io["ev_w_out"], n_ctx, n_tok)
    NB = phase_moe_sparse_route(p, C, io, G, RT, 0, NE, 0, n_tok, n_ctx)
    phase_moe_sparse_dispatch(p, C, io, RT, 0, n_tok, NB)
    phase_moe_sparse_experts(p, C, io, RT, 0, NE, NB)
    phase_moe_sparse_combine(p, C, io, RT, 0, 0, n_tok, n_ctx)
    phase_mla_proj(p, C, io, G, 1, n_ctx, n_tok)
    phase_mla_attn(p, C, io, n_ctx, n_tok)
    phase_outproj(p, C, io, 1, io["od_w_out"], n_ctx, n_tok, t_lo=n_ctx)
    NB = phase_moe_sparse_route(p, C, io, G, RT, 1, NE, n_ctx, n_tok, n_ctx)
    phase_moe_sparse_dispatch(p, C, io, RT, n_ctx, n_tok, NB)
    phase_moe_sparse_experts(p, C, io, RT, 1, NE, NB)
    phase_moe_sparse_combine(p, C, io, RT, 1, n_ctx, n_tok, n_ctx)
    phase_final(p, C, io, n_ctx, n_tok)
    p.finish()
    return nc


def kernel(**inputs):
    f = lambda a: np.ascontiguousarray(np.asarray(a, dtype=np.float32))
    x, c, ctx, c_ctx = f(inputs["x"]), f(inputs["c"]), f(inputs["ctx"]), f(inputs["c_ctx"])
    B = x.shape[0]
    shared = {}
    for k in _W_NAMES:
        a = f(inputs[k])
        if k.startswith(("ev_", "gla_", "rw_", "od_", "mla_")):
            a = np.ascontiguousarray(a[0])
        shared[k] = a
    in_maps = []
    for b in range(B):
        m = dict(shared)
        m["xin"] = np.ascontiguousarray(np.concatenate([ctx[b], x[b]], axis=0))
        m["cvec"] = np.ascontiguousarray(np.stack([c[b], c_ctx], axis=0))
        in_maps.append(m)
    shapes = {k: v.shape for k, v in in_maps[0].items()}
    nc = build(shapes, n_ctx=ctx.shape[1], n_lat=x.shape[1], NE=shared["router_w"].shape[-1])
    res = run_bass_kernel_spmd(nc, in_maps, core_ids=list(range(B)))
    return np.stack([np.asarray(r["out"], dtype=np.float32) for r in res.results], axis=0)
```
